# Optimizing a Trainium2 kernel written in Bass

```python
import jax, jax.numpy as jnp
from jax import lax
import numpy as np

D_MODEL = 1024
BATCH = 4
SEQ = 4096
DEPTH = 1

CHUNK = 64
Q_BLOCK = 128
HEAD_DIM = 64
ROPE_THETA = 10000.0
EPS = 1e-6

N_DIFF_HEADS = 4
DIFF_V_DIM = 2 * HEAD_DIM
DIFF_QK = N_DIFF_HEADS * 2 * HEAD_DIM
DIFF_V = N_DIFF_HEADS * DIFF_V_DIM
N_FOX_HEADS = 8
FOX_W = N_FOX_HEADS * HEAD_DIM
MIX_W = DIFF_V + FOX_W
PROJ_WIDTHS = (DIFF_QK, DIFF_QK, DIFF_V, FOX_W, FOX_W, FOX_W, N_FOX_HEADS)
D_IN = sum(PROJ_WIDTHS)

N_GROUPS = 4
EXPERTS_PER_GROUP = 4
N_EXPERTS = N_GROUPS * EXPERTS_PER_GROUP
TOP_K_IN_GROUP = 2
D_EXPERT = D_MODEL // 2

kernel_name = "hybrid_diffattn_fox_hiermoe_chunk_causal"


def _rmsnorm(x, g):
    xf = x.astype(jnp.float32)
    y = xf * lax.rsqrt(jnp.mean(xf * xf, axis=-1, keepdims=True) + EPS)
    return (y * g.astype(jnp.float32)).astype(x.dtype)


def _rotate_half(x):
    x1, x2 = jnp.split(x, 2, axis=-1)
    return jnp.concatenate([-x2, x1], axis=-1)


def _rope(x, cos, sin):
    return (x * cos + _rotate_half(x) * sin).astype(x.dtype)


def _diff_attention(q1, q2, k1, k2, v, lam):
    S = q1.shape[2]
    scale = HEAD_DIM ** -0.5
    outs = []
    for blk in range(S // Q_BLOCK):
        q0, q1_end = blk * Q_BLOCK, (blk + 1) * Q_BLOCK
        t = np.arange(q0, q1_end)
        s = np.arange(q1_end)
        mask = (s[None, :] // CHUNK) <= (t[:, None] // CHUNK)

        def probs(q, k):
            logits = jnp.einsum('bhqd,bhkd->bhqk', q[:, :, q0:q1_end], k[:, :, :q1_end]).astype(jnp.float32) * scale
            return jax.nn.softmax(jnp.where(mask, logits, -jnp.inf), axis=-1)

        w = probs(q1, k1) - lam * probs(q2, k2)
        outs.append(jnp.einsum('bhqk,bhkd->bhqd', w.astype(v.dtype), v[:, :, :q1_end]))
    return jnp.concatenate(outs, axis=2)


def _forgetting_attention(q, k, v, cum_logf):
    S = q.shape[2]
    scale = HEAD_DIM ** -0.5
    outs = []
    for blk in range(S // Q_BLOCK):
        q0, q_end = blk * Q_BLOCK, (blk + 1) * Q_BLOCK
        t = np.arange(q0, q_end)
        s = np.arange(q_end)
        mask = s[None, :] <= t[:, None]
        logits = jnp.einsum('bhqd,bhkd->bhqk', q[:, :, q0:q_end], k[:, :, :q_end]).astype(jnp.float32) * scale
        logits = logits + cum_logf[:, :, q0:q_end, None] - cum_logf[:, :, None, :q_end]
        p = jax.nn.softmax(jnp.where(mask, logits, -jnp.inf), axis=-1)
        outs.append(jnp.einsum('bhqk,bhkd->bhqd', p.astype(v.dtype), v[:, :, :q_end]))
    return jnp.concatenate(outs, axis=2)


def _hier_moe(h, wg_r, bg_r, we_r, be_r, w_gate, w_up, w_down):
    g_logits = (h @ wg_r).astype(jnp.float32) + bg_r.astype(jnp.float32)
    g_probs = jax.nn.softmax(g_logits, axis=-1)
    g_w, g_idx = lax.top_k(g_probs, 1)
    e_logits = (h @ we_r).astype(jnp.float32) + be_r.astype(jnp.float32)
    e_logits = e_logits.reshape(-1, N_GROUPS, EXPERTS_PER_GROUP)
    sel = jnp.take_along_axis(e_logits, g_idx[:, :, None], axis=1)[:, 0]
    top_v, top_i = lax.top_k(sel, TOP_K_IN_GROUP)
    weights = g_w * jax.nn.softmax(top_v, axis=-1)
    expert_ids = g_idx * EXPERTS_PER_GROUP + top_i
    combine = jnp.sum(jax.nn.one_hot(expert_ids, N_EXPERTS, dtype=jnp.float32) * weights[:, :, None], axis=1)
    y = jnp.zeros_like(h)
    for e in range(N_EXPERTS):
        a = jax.nn.silu(h @ w_gate[e]) * (h @ w_up[e])
        y = y + combine[:, e:e + 1].astype(h.dtype) * (a @ w_down[e])
    return y


def setup_inputs(seed: int = 0) -> dict:
    key = jax.random.key(seed)
    ks = jax.random.split(key, 20)
    f32 = jnp.float32
    nrm = lambda k, shape, s: jax.random.normal(k, shape, f32) * s
    gain = lambda k, shape: 1.0 + 0.02 * jax.random.normal(k, shape, f32)
    return {
        "x": jax.random.normal(ks[0], (BATCH, SEQ, D_MODEL), f32),
        "norm_attn_g": gain(ks[1], (DEPTH, D_MODEL)),
        "w_in": nrm(ks[2], (DEPTH, D_MODEL, D_IN), D_MODEL ** -0.5),
        "b_forget": jax.random.uniform(ks[3], (DEPTH, N_FOX_HEADS), f32, 1.0, 4.0),
        "lambda_q1": nrm(ks[4], (DEPTH, HEAD_DIM), 0.1),
        "lambda_k1": nrm(ks[5], (DEPTH, HEAD_DIM), 0.1),
        "lambda_q2": nrm(ks[6], (DEPTH, HEAD_DIM), 0.1),
        "lambda_k2": nrm(ks[7], (DEPTH, HEAD_DIM), 0.1),
        "diff_norm_g": gain(ks[8], (DEPTH, DIFF_V_DIM)),
        "w_out": nrm(ks[9], (DEPTH, MIX_W, D_MODEL), MIX_W ** -0.5),
        "norm_ffn_g": gain(ks[10], (DEPTH, D_MODEL)),
        "router_group_w": nrm(ks[11], (DEPTH, D_MODEL, N_GROUPS), D_MODEL ** -0.5),
        "router_group_b": nrm(ks[12], (DEPTH, N_GROUPS), 0.01),
        "router_expert_w": nrm(ks[13], (DEPTH, D_MODEL, N_EXPERTS), D_MODEL ** -0.5),
        "router_expert_b": nrm(ks[14], (DEPTH, N_EXPERTS), 0.01),
        "w_gate": nrm(ks[15], (DEPTH, N_EXPERTS, D_MODEL, D_EXPERT), D_MODEL ** -0.5),
        "w_up": nrm(ks[16], (DEPTH, N_EXPERTS, D_MODEL, D_EXPERT), D_MODEL ** -0.5),
        "w_down": nrm(ks[17], (DEPTH, N_EXPERTS, D_EXPERT, D_MODEL), D_EXPERT ** -0.5),
        "norm_final_g": gain(ks[18], (D_MODEL,)),
    }


def reference(x, norm_attn_g, w_in, b_forget, lambda_q1, lambda_k1, lambda_q2, lambda_k2,
              diff_norm_g, w_out, norm_ffn_g, router_group_w, router_group_b,
              router_expert_w, router_expert_b, w_gate, w_up, w_down, norm_final_g):
    B, S, D = x.shape
    pos = jnp.arange(S, dtype=jnp.float32)
    inv_freq = 1.0 / (ROPE_THETA ** (jnp.arange(0, HEAD_DIM, 2, dtype=jnp.float32) / HEAD_DIM))
    ang = pos[:, None] * inv_freq[None, :]
    ang = jnp.concatenate([ang, ang], axis=-1)
    cos, sin = jnp.cos(ang), jnp.sin(ang)
    splits = [int(i) for i in np.cumsum(PROJ_WIDTHS)[:-1]]

    for l in range(DEPTH):
        lam_init = 0.8 - 0.6 * float(np.exp(-0.3 * l))
        h = _rmsnorm(x, norm_attn_g[l])
        proj = h @ w_in[l]
        q_d, k_d, v_d, q_f, k_f, v_f, f_logit = jnp.split(proj, splits, axis=-1)

        q_d = _rope(q_d.reshape(B, S, N_DIFF_HEADS, 2, HEAD_DIM).transpose(3, 0, 2, 1, 4), cos, sin)
        k_d = _rope(k_d.reshape(B, S, N_DIFF_HEADS, 2, HEAD_DIM).transpose(3, 0, 2, 1, 4), cos, sin)
        v_d = v_d.reshape(B, S, N_DIFF_HEADS, DIFF_V_DIM).transpose(0, 2, 1, 3)
        lam = (jnp.exp(jnp.sum(lambda_q1[l].astype(jnp.float32) * lambda_k1[l].astype(jnp.float32)))
               - jnp.exp(jnp.sum(lambda_q2[l].astype(jnp.float32) * lambda_k2[l].astype(jnp.float32)))
               + lam_init)
        o_d = _diff_attention(q_d[0], q_d[1], k_d[0], k_d[1], v_d, lam)
        o_d = _rmsnorm(o_d, diff_norm_g[l]) * (1.0 - lam_init)
        o_d = o_d.transpose(0, 2, 1, 3).reshape(B, S, DIFF_V).astype(x.dtype)

        to_heads = lambda t: t.reshape(B, S, N_FOX_HEADS, HEAD_DIM).transpose(0, 2, 1, 3)
        log_f = jax.nn.log_sigmoid(f_logit.astype(jnp.float32) + b_forget[l].astype(jnp.float32))
        cum_logf = jnp.cumsum(log_f.transpose(0, 2, 1), axis=-1)
        o_f = _forgetting_attention(to_heads(q_f), to_heads(k_f), to_heads(v_f), cum_logf)
        o_f = o_f.transpose(0, 2, 1, 3).reshape(B, S, FOX_W).astype(x.dtype)

        x = x + jnp.concatenate([o_d, o_f], axis=-1) @ w_out[l]

        hm = _rmsnorm(x, norm_ffn_g[l]).reshape(B * S, D)
        y = _hier_moe(hm, router_group_w[l], router_group_b[l], router_expert_w[l], router_expert_b[l],
                      w_gate[l], w_up[l], w_down[l])
        x = x + y.reshape(B, S, D)

    return _rmsnorm(x, norm_final_g)
```

```python
import numpy as np
import ml_dtypes
from contextlib import ExitStack
import concourse.bass as bass
import concourse.mybir as mybir
from concourse.bass_utils import run_bass_kernel_spmd

F32 = mybir.dt.float32
BF16 = mybir.dt.bfloat16
I32 = mybir.dt.int32
AF = mybir.ActivationFunctionType
ALU = mybir.AluOpType
AX = mybir.AxisListType

ENGS = ("sync", "scalar", "vector", "gpsimd", "tensor")
EPS = 1e-6
LAM_INIT = 0.8 - 0.6 * 1.0
NEXP = 16
CAP = 512
NSLOT = NEXP * CAP


class Tile:
    __slots__ = ("name", "last_w", "readers", "dsem")

    def __init__(self, name):
        self.name = name
        self.last_w = None
        self.readers = {}
        self.dsem = None


class Sched:
    def __init__(self, nc, es):
        self.nc = nc
        self.es = es
        self.ops = {e: [] for e in ENGS}
        self.sems = {}
        self.cnt = {}
        self.waited = {e: {} for e in ENGS}
        self.pending = {e: False for e in ENGS}
        for e in ENGS:
            self._mksem("E:" + e)
        self.n_dsem = 0
        self.nops = 0
        self.tiles = []

    def _mksem(self, key):
        self.sems[key] = self.es.enter_context(self.nc.semaphore(key.replace(":", "_")))
        self.cnt[key] = 0

    def tile(self, name="t"):
        t = Tile(name)
        self.tiles.append(t)
        return t

    def _snapshot(self):
        return (dict(self.cnt), {e: dict(w) for e, w in self.waited.items()},
                [(t, t.last_w, dict(t.readers)) for t in self.tiles])

    def _restore(self, snap):
        self.cnt = dict(snap[0])
        for k in self.sems:
            self.cnt.setdefault(k, 0)
        self.waited = {e: dict(w) for e, w in snap[1].items()}
        for t, lw, rd in snap[2]:
            t.last_w = lw
            t.readers = dict(rd)

    def branch_begin(self):
        self.barrier()
        self._outer_ops = self.ops
        self.ops = {e: [] for e in ENGS}
        self._snap = self._snapshot()

    def branch_mid(self):
        self.barrier()
        self._A = (self.ops, dict(self.cnt))
        self.ops = {e: [] for e in ENGS}
        self._restore(self._snap)

    def branch_end(self, flag_ap, regs):
        self.barrier()
        opsA, cntA = self._A
        opsB, cntB = self.ops, dict(self.cnt)
        target = {k: max(cntA.get(k, 0), cntB.get(k, 0)) for k in set(cntA) | set(cntB)}

        def pads(cntX):
            out = {e: [] for e in ENGS}
            for k, v in target.items():
                d = v - cntX.get(k, 0)
                if d > 0:
                    owner = k[2:] if k.startswith("E:") else "gpsimd"
                    out[owner].append((k, d))
            return out
        pA, pB = pads(cntA), pads(cntB)
        self.ops = self._outer_ops
        for e in ENGS:
            self.ops[e].append(("branch", flag_ap, regs[e], opsA[e], pA[e], opsB[e], pB[e]))
        self.cnt = target
        for e in ENGS:
            self.waited[e] = dict(target)
        for t in self.tiles:
            t.last_w = None
            t.readers = {}

    def dsem_for(self, t):
        if t.dsem is None:
            key = "D:%d" % self.n_dsem
            self.n_dsem += 1
            self._mksem(key)
            t.dsem = key
        return t.dsem

    def _need(self, eng, waits, key, val):
        if eng == "tensor" and key == "E:tensor":
            return
        if self.cnt[key] < val:
            raise RuntimeError("wait on un-signalled event %s %d (cnt %d) from %s" % (key, val, self.cnt[key], eng))
        if self.waited[eng].get(key, 0) >= val:
            return
        self.waited[eng][key] = val
        waits[key] = max(waits.get(key, 0), val)

    def _deps(self, eng, reads, writes):
        waits = {}
        for t in reads:
            if t.last_w is not None:
                self._need(eng, waits, *t.last_w)
        for t in writes:
            if t.last_w is not None:
                self._need(eng, waits, *t.last_w)
            for k, v in t.readers.items():
                self._need(eng, waits, k, v)
        return list(waits.items())

    def _record(self, ev, reads, writes):
        for t in writes:
            t.last_w = ev
            t.readers = {}
        for t in reads:
            if t not in writes:
                if t.readers.get(ev[0], 0) < ev[1]:
                    t.readers[ev[0]] = ev[1]

    def op(self, eng, fn, reads=(), writes=(), signal=True):
        waits = self._deps(eng, reads, writes)
        key = "E:" + eng
        if signal:
            self.cnt[key] += 1
            ev = (key, self.cnt[key])
            inc = (key, 1)
            self.pending[eng] = False
        else:
            ev = (key, self.cnt[key] + 1)
            inc = None
            self.pending[eng] = True
        self._record(ev, reads, writes)
        self.ops[eng].append((waits, fn, inc))
        self.nops += 1

    def dma(self, eng, out, in_, reads=(), writes=(), semtile=None, **kw):
        waits = self._deps(eng, reads, writes)
        if semtile is None:
            semtile = writes[0] if writes else reads[0]
        key = self.dsem_for(semtile)
        self.cnt[key] += 16
        ev = (key, self.cnt[key])
        self._record(ev, reads, writes)

        def fn(e, out=out, in_=in_, kw=kw):
            return e.dma_start(out=out, in_=in_, **kw)
        self.ops[eng].append((waits, fn, (key, 16)))
        self.nops += 1

    def dma_fn(self, eng, fn, reads=(), writes=(), semtile=None):
        waits = self._deps(eng, reads, writes)
        key = self.dsem_for(semtile)
        self.cnt[key] += 16
        ev = (key, self.cnt[key])
        self._record(ev, reads, writes)
        self.ops[eng].append((waits, fn, (key, 16)))
        self.nops += 1

    def barrier(self, engs=ENGS):
        for e in ENGS:
            assert not self.pending[e], e
        for e in engs:
            waits = {}
            for key, c in self.cnt.items():
                if c > 0:
                    self._need(e, waits, key, c)
            self.ops[e].append((list(waits.items()), None, None))

    def emit(self):
        nc = self.nc
        sems = self.sems
        ops = self.ops
        with nc.Block() as block:
            def replay(e, lst):
                for ent in lst:
                    if ent[0] == "branch":
                        _, flag_ap, reg, oA, pA, oB, pB = ent
                        e.reg_load(reg, flag_ap)
                        with e.If_eq(reg, 0):
                            replay(e, oA)
                            for k, d in pA:
                                e.sem_inc(sems[k], d)
                            e.nop()
                        with e.Else():
                            replay(e, oB)
                            for k, d in pB:
                                e.sem_inc(sems[k], d)
                            e.nop()
                        continue
                    waits, fn, inc = ent
                    for key, val in waits:
                        e.wait_ge(sems[key], val)
                    if fn is None:
                        continue
                    inst = fn(e)
                    if inc is not None:
                        inst.then_inc(sems[inc[0]], inc[1])

            def mk(name):
                def body(e):
                    replay(e, ops[name])
                return body
            block.sync(mk("sync"))
            block.scalar(mk("scalar"))
            block.vector(mk("vector"))
            block.gpsimd(mk("gpsimd"))
            block.tensor(mk("tensor"))
        self.ops = {e: [] for e in ENGS}


def own_qtiles(hf):
    js = []
    for m in range(4):
        js += ([4 * m, 4 * m + 3] if hf == 0 else [4 * m + 1, 4 * m + 2])
    return js


def nk_of(i):
    return 8 * (i // 2) + (4 if i % 2 == 0 else 8)


def interleave(A, B):
    a, b = len(A), len(B)
    if a == 0:
        for f in B:
            f()
        return
    done = 0
    for k, f in enumerate(A):
        f()
        upto = ((k + 1) * b) // a
        while done < upto:
            B[done]()
            done += 1
    while done < b:
        B[done]()
        done += 1


def build(debug=(), stop_after=None, moe="sparse"):
    nc = bass.Bass("TRN2", target_bir_lowering=False)

    def din(name, shape, dt=F32):
        return nc.dram_tensor(name, list(shape), dt, kind="ExternalInput").ap()

    xb = din("xb", [4096, 1024])
    xo = din("xo", [2048, 1024])
    w_in = din("w_in", [1024, 3080])
    wqs_d = din("wqs", [1024, 512])
    wks_d = din("wks", [1024, 512])
    cosk = din("cosk", [128, 4096])
    sink = din("sink", [128, 4096])
    cosq = din("cosq", [128, 2048])
    sinq = din("sinq", [128, 2048])
    maskd_d = din("maskd", [128, 2, 1024], BF16)
    maskf_d = din("maskf", [128, 2, 1024], BF16)
    identb_d = din("identb", [128, 128], BF16)
    identf_d = din("identf", [128, 128])
    tri_d = din("tri", [128, 128])
    onesf_d = din("onesf", [128, 128])
    g_attn = din("g_attn", [1, 1024])
    g_ffn = din("g_ffn", [1, 1024])
    g_fin = din("g_fin", [1, 1024])
    bfor = din("bfor", [1, 8])
    lam_d = din("lamv", [1, 256])
    csel_d = din("csel", [1, 8 * 33])
    dng = din("dng", [1, 128])
    w_out = din("w_out", [1024, 1024])
    rw_d = din("rw", [1024, 20])
    rb_d = din("rb", [1, 20])
    wg_d = din("wg", [16, 1024, 512])
    wu_d = din("wu", [16, 1024, 512])
    wd_d = din("wd", [16, 512, 1024])
    ebase_d = din("ebase", [128, 256])
    ustrict_d = din("ustrict", [128, 128], BF16)
    onesb_d = din("onesb", [128, 128], BF16)
    xs_d = nc.dram_tensor("xs_scratch", [NSLOT, 1024], BF16).ap()
    ys_d = nc.dram_tensor("ys_scratch", [NSLOT, 1024], F32).ap()
    out = nc.dram_tensor("out", [2048, 1024], F32, kind="ExternalOutput").ap()
    dbg = {}

    def dbg_out(name, shape, dt=F32):
        if name in debug:
            dbg[name] = nc.dram_tensor("dbg_" + name, list(shape), dt, kind="ExternalOutput").ap()
            return dbg[name]
        return None

    with ExitStack() as es:
        S = Sched(nc, es)

        uniq = [0]

        def sb(sc, name, shape, dt):
            uniq[0] += 1
            return sc.enter_context(nc.sbuf_tensor("s%d_%s" % (uniq[0], name), list(shape), dt))

        banks = [es.enter_context(nc.psum_tensor("bank%d" % i, [128, 512], F32)) for i in range(8)]
        Tb = [S.tile("bank%d" % i) for i in range(8)]
        Tdbg = S.tile("dbg")

        def dump(name, src_ap, reads):
            if name in dbg:
                S.dma("sync", dbg[name], src_ap, reads=reads, writes=[Tdbg], semtile=Tdbg)

        def bcast_load(dst, src, T, n):
            S.dma("sync", dst, src.broadcast_to([128, n]), writes=[T])

        bcreg = es.enter_context(nc.gpsimd.register("bcreg"))
        brregs = {e: es.enter_context(getattr(nc, e).register("br_" + e)) for e in ENGS}

        def set_bcreg():
            S.ops["gpsimd"].append(([], lambda e: e.reg_mov(bcreg, NSLOT - 1), None))

        identb = sb(es, "identb", [128, 128], BF16); Tidb = S.tile("identb")
        S.dma("sync", identb[:], identb_d, writes=[Tidb])
        gb_attn = sb(es, "gb_attn", [128, 1024], F32); Tgba = S.tile("gba")
        bcast_load(gb_attn[:], g_attn, Tgba, 1024)
        OT = sb(es, "OT", [128, 8, 2048], BF16)
        TOT = [S.tile("OT%d" % q) for q in range(16)]

        def make_sweep(sc, pfx, pT_banks):
            st = {}
            st["xt"] = [sb(sc, pfx + "xt%d" % i, [128, 1024], F32) for i in range(4)]
            st["Txt"] = [S.tile() for _ in range(4)]
            st["hb"] = [sb(sc, pfx + "hb%d" % i, [128, 1024], BF16) for i in range(2)]
            st["Thb"] = [S.tile() for _ in range(2)]
            st["hT"] = [sb(sc, pfx + "hT%d" % i, [128, 8, 512], BF16) for i in range(2)]
            st["ThT"] = [[S.tile() for _ in range(4)] for _ in range(2)]
            st["junk"] = sb(sc, pfx + "junk", [128, 1024], BF16)
            st["ss"] = [sb(sc, pfx + "ss%d" % i, [128, 4], F32) for i in range(2)]
            st["Tss"] = [[S.tile() for _ in range(4)] for _ in range(2)]
            st["sq"] = [sb(sc, pfx + "sq%d" % i, [128, 4], F32) for i in range(2)]
            st["Tsq"] = [S.tile() for _ in range(2)]
            st["rs"] = [sb(sc, pfx + "rs%d" % i, [128, 4], F32) for i in range(2)]
            st["Trs"] = [S.tile() for _ in range(2)]
            st["pT"] = pT_banks
            return st

        def sweep_stage1(st, x_ap, blk, gb, Tgb):
            b2 = blk % 2
            for tt in range(4):
                n = blk * 4 + tt
                xt, Txt = st["xt"][n % 4], st["Txt"][n % 4]
                S.dma("sync", xt[:], x_ap[n * 128:(n + 1) * 128, :], writes=[Txt])
                S.op("scalar", lambda e, xt=xt, tt=tt: e.activation(out=st["junk"][:], in_=xt[:], func=AF.Square,
                                                                    accum_out=st["ss"][b2][:, tt:tt + 1]),
                     reads=[Txt], writes=[st["Tss"][b2][tt]])
            S.op("scalar", lambda e: e.activation(out=st["sq"][b2][:], in_=st["ss"][b2][:], func=AF.Sqrt,
                                                  scale=1.0 / 1024, bias=EPS),
                 reads=st["Tss"][b2], writes=[st["Tsq"][b2]])
            S.op("vector", lambda e: e.reciprocal(out=st["rs"][b2][:], in_=st["sq"][b2][:]),
                 reads=[st["Tsq"][b2]], writes=[st["Trs"][b2]])
            for tt in range(4):
                n = blk * 4 + tt
                xt, Txt = st["xt"][n % 4], st["Txt"][n % 4]
                hb, Thb = st["hb"][n % 2], st["Thb"][n % 2]
                bi = st["pT"][n % 2]
                S.op("vector", lambda e, xt=xt, hb=hb, tt=tt: e.scalar_tensor_tensor(
                    out=hb[:], in0=xt[:], scalar=st["rs"][b2][:, tt:tt + 1], in1=gb[:], op0=ALU.mult, op1=ALU.mult),
                    reads=[Txt, st["Trs"][b2], Tgb], writes=[Thb])
                pTv = banks[bi][:].bitcast(BF16)
                for c in range(8):
                    S.op("tensor", lambda e, c=c, hb=hb, pTv=pTv: e.transpose(
                        out=pTv[:, c * 128:(c + 1) * 128], in_=hb[:, c * 128:(c + 1) * 128], identity=identb[:]),
                        reads=[Thb, Tidb], writes=[Tb[bi]], signal=(c == 7))
                S.op("vector", lambda e, tt=tt, pTv=pTv: e.tensor_copy(
                    out=st["hT"][b2][:, :, tt * 128:(tt + 1) * 128], in_=pTv.rearrange("p (c t) -> p c t", c=8)),
                    reads=[Tb[bi]], writes=[st["ThT"][b2][tt]])

        def wload(dst, src_cols, T):
            S.dma("gpsimd", dst, src_cols.rearrange("(c p) n -> p c n", p=128), writes=[T])

        def mmgroup(out_ap, pairs, reads, writes):
            n = len(pairs)
            for k, (l, r) in enumerate(pairs):
                S.op("tensor", lambda e, l=l, r=r, k=k: e.matmul(out=out_ap, lhsT=l, rhs=r, start=(k == 0), stop=(k == n - 1)),
                     reads=reads, writes=writes, signal=(k == n - 1))

        def run_attention(units, PT, TPT, KT_of, QT_of, V_of, W, exp_emit, evac_emit, mask_emit, s_banks, o_banks):
            gctr = [0]

            def st_list(ui, u):
                buf = ui % 2
                nk = nk_of(u["i"])
                L = []
                for p in range(nk // 2):
                    def f(p=p):
                        bi = s_banks[gctr[0] % len(s_banks)]
                        gctr[0] += 1
                        for j in range(2):
                            kb = 2 * p + j
                            kap, Tk = KT_of(u, kb)
                            qap, Tq = QT_of(u)
                            S.op("tensor", lambda e, kap=kap, qap=qap, j=j, bi=bi: e.matmul(
                                out=banks[bi][:, j * 256:(j + 1) * 256], lhsT=kap, rhs=qap, start=True, stop=True),
                                reads=[Tk, Tq], writes=[Tb[bi]], signal=(j == 1))
                        exp_emit(u, p, bi, PT[buf], TPT[buf][p])
                    L.append(f)
                L.append(lambda: mask_emit(u, PT[buf], TPT[buf]))
                return L

            def pv_list(ui, u):
                buf = ui % 2
                nk = nk_of(u["i"])
                ob = o_banks[ui % len(o_banks)]
                L = []
                for s in range(2):
                    for k0 in range(0, nk, 4):
                        def f(s=s, k0=k0):
                            for kb in range(k0, min(nk, k0 + 4)):
                                vap, Tv = V_of(u, kb)
                                S.op("tensor", lambda e, kb=kb, s=s, vap=vap: e.matmul(
                                    out=banks[ob][:, s * W:(s + 1) * W], lhsT=PT[buf][:, kb, s * 128:(s + 1) * 128],
                                    rhs=vap, start=(kb == 0), stop=(kb == nk - 1)),
                                    reads=[TPT[buf][kb // 2], Tv], writes=[Tb[ob]], signal=(kb == nk - 1))
                        L.append(f)
                L.append(lambda: evac_emit(u, ob))
                return L

            prev = None
            for ui in range(len(units) + 1):
                A = st_list(ui, units[ui]) if ui < len(units) else []
                B = pv_list(ui - 1, units[ui - 1]) if ui >= 1 else []
                interleave(A, B)

        with ExitStack() as pa:
            KT = sb(pa, "KTd", [128, 4, 4096], BF16)
            TKT = [[S.tile() for _ in range(8)] for _ in range(4)]
            QT = sb(pa, "QTd", [128, 4, 2048], BF16)
            TQT = [[S.tile() for _ in range(4)] for _ in range(4)]
            Vd = sb(pa, "Vd", [128, 32, 4, 130], BF16)
            TV = [S.tile() for _ in range(32)]
            Tvones = S.tile()
            S.op("vector", lambda e: e.memset(Vd[:, :, :, 128:130], 1.0), writes=TV)
            lamt = sb(pa, "lamt", [128, 256], F32); Tlam = S.tile()
            bcast_load(lamt[:], lam_d, Tlam, 256)
            lj = sb(pa, "lj", [128, 64], F32)
            ls = sb(pa, "ls", [128, 2], F32); Tls = S.tile()
            le = sb(pa, "le", [128, 2], F32); Tle = S.tile()
            neglam = sb(pa, "neglam", [128, 1], F32); Tnl = S.tile()
            for z in range(2):
                S.op("vector", lambda e, z=z: e.scalar_tensor_tensor(
                    out=lj[:], in0=lamt[:, z * 128:z * 128 + 64], scalar=1.0, in1=lamt[:, z * 128 + 64:z * 128 + 128],
                    op0=ALU.mult, op1=ALU.mult, accum_out=ls[:, z:z + 1]), reads=[Tlam, Tls], writes=[Tls])
            S.op("scalar", lambda e: e.activation(out=le[:], in_=ls[:], func=AF.Exp), reads=[Tls], writes=[Tle])
            S.op("vector", lambda e: e.tensor_tensor(out=neglam[:], in0=le[:, 1:2], in1=le[:, 0:1], op=ALU.subtract),
                 reads=[Tle], writes=[Tnl])
            S.op("vector", lambda e: e.tensor_scalar(out=neglam[:], in0=neglam[:], scalar1=-LAM_INIT, scalar2=None, op0=ALU.add),
                 reads=[Tnl], writes=[Tnl])
            gsc = sb(pa, "gsc", [128, 128], F32); Tgsc = S.tile()
            bcast_load(gsc[:], dng, Tgsc, 128)
            S.op("vector", lambda e: e.tensor_scalar(out=gsc[:], in0=gsc[:], scalar1=1.0 - LAM_INIT, scalar2=None, op0=ALU.mult),
                 reads=[Tgsc], writes=[Tgsc])
            Txs = S.tile("xs")
            if moe == "sparse":
                zer = sb(pa, "zer", [128, 2048], BF16); Tzer = S.tile()
                S.op("vector", lambda e: e.memset(zer[:], 0.0), writes=[Tzer])
                for n in range(NSLOT // 256):
                    S.dma("sync", xs_d[n * 256:(n + 1) * 256, :].rearrange("(p r) d -> p (r d)", r=2), zer[:], reads=[Tzer], writes=[Txs], semtile=Tzer)
            maskd = sb(pa, "maskd", [128, 2, 1024], BF16); Tmd = S.tile()
            S.dma("sync", maskd[:], maskd_d, writes=[Tmd])

            with ExitStack() as sw:
                st = make_sweep(sw, "a", [0, 1])
                wA = sb(sw, "wA", [128, 8, 512], BF16); TwA = S.tile()
                wB = sb(sw, "wB", [128, 8, 512], BF16); TwB = S.tile()
                wC = sb(sw, "wC", [128, 8, 512], BF16); TwC = S.tile()
                wload(wA[:], w_in[:, 512:1024], TwA)
                wload(wB[:], wks_d, TwB)
                wload(wC[:], w_in[:, 1024:1536], TwC)
                ct = [sb(sw, "ct%d" % i, [128, 512], F32) for i in range(2)]; Tct = [S.tile() for _ in range(2)]
                sn = [sb(sw, "sn%d" % i, [128, 512], F32) for i in range(2)]; Tsn = [S.tile() for _ in range(2)]
                t1 = [sb(sw, "t1%d" % i, [128, 512], F32) for i in range(2)]; Tt1 = [S.tile() for _ in range(2)]
                t2 = [sb(sw, "t2%d" % i, [128, 512], F32) for i in range(2)]; Tt2 = [S.tile() for _ in range(2)]
                rctr = [0]

                def rope_proj(blk, hT, ThT, wq_, Tw_, ws_, Tws_, cos_d, sin_d, dstT, TdstT):
                    b2 = blk % 2
                    S.dma("sync", ct[b2][:], cos_d[:, blk * 512:(blk + 1) * 512], writes=[Tct[b2]])
                    S.dma("sync", sn[b2][:], sin_d[:, blk * 512:(blk + 1) * 512], writes=[Tsn[b2]])
                    for h in range(4):
                        ba, bb = (2, 3) if h % 2 == 0 else (4, 5)
                        mmgroup(banks[ba][:], [(wq_[:, c, h * 128:(h + 1) * 128], hT[:, c, :]) for c in range(8)],
                                reads=list(ThT) + [Tw_], writes=[Tb[ba]])
                        mmgroup(banks[bb][:], [(ws_[:, c, h * 128:(h + 1) * 128], hT[:, c, :]) for c in range(8)],
                                reads=list(ThT) + [Tws_], writes=[Tb[bb]])
                        r = rctr[0] % 2
                        rctr[0] += 1
                        S.op("vector", lambda e, r=r, ba=ba: e.tensor_tensor(out=t1[r][:], in0=banks[ba][:], in1=ct[b2][:], op=ALU.mult),
                             reads=[Tb[ba], Tct[b2]], writes=[Tt1[r]])
                        S.op("vector", lambda e, r=r, bb=bb: e.tensor_tensor(out=t2[r][:], in0=banks[bb][:], in1=sn[b2][:], op=ALU.mult),
                             reads=[Tb[bb], Tsn[b2]], writes=[Tt2[r]])
                        S.op("gpsimd", lambda e, r=r, h=h: e.tensor_tensor(out=dstT[:, h, blk * 512:(blk + 1) * 512], in0=t1[r][:], in1=t2[r][:], op=ALU.add),
                             reads=[Tt1[r], Tt2[r]], writes=[TdstT[h][blk]])

                def kv_proj(blk, hT, ThT):
                    rope_proj(blk, hT, ThT, wA, TwA, wB, TwB, cosk, sink, KT, TKT)
                    for tt in range(4):
                        n = blk * 4 + tt
                        bv = 6 + (tt % 2)
                        mmgroup(banks[bv][:], [(hT[:, c, tt * 128:(tt + 1) * 128], wC[:, c, :]) for c in range(8)],
                                reads=[ThT[tt], TwC], writes=[Tb[bv]])
                        S.op("scalar", lambda e, n=n, bv=bv: e.activation(
                            out=Vd[:, n, :, 0:128], in_=banks[bv][:].rearrange("p (h d) -> p h d", h=4), func=AF.Copy),
                            reads=[Tb[bv]], writes=[TV[n]])

                sweep_stage1(st, xb, 0, gb_attn, Tgba)
                for blk in range(8):
                    if blk + 1 < 8:
                        sweep_stage1(st, xb, blk + 1, gb_attn, Tgba)
                    kv_proj(blk, st["hT"][blk % 2], st["ThT"][blk % 2])
                wload(wA[:], w_in[:, 0:512], TwA)
                wload(wB[:], wqs_d, TwB)
                sweep_stage1(st, xo, 0, gb_attn, Tgba)
                for blk in range(4):
                    if blk + 1 < 4:
                        sweep_stage1(st, xo, blk + 1, gb_attn, Tgba)
                    rope_proj(blk, st["hT"][blk % 2], st["ThT"][blk % 2], wA, TwA, wB, TwB, cosq, sinq, QT, TQT)
                S.barrier()
                if "KTd" in debug:
                    dbg_out("KTd", [128, 4, 4096], BF16); dump("KTd", KT[:], [])
                    dbg_out("QTd", [128, 4, 2048], BF16); dump("QTd", QT[:], [])
                    dbg_out("Vd", [128, 32, 4, 130], BF16); dump("Vd", Vd[:], [])
                    S.barrier()
                S.emit()
            if stop_after == "Aproj":
                S.barrier(); S.emit()
                return nc

            with ExitStack() as at:
                PT = [sb(at, "PT%d" % i, [128, 32, 256], BF16) for i in range(2)]
                TPT = [[S.tile() for _ in range(16)] for _ in range(2)]
                oc = sb(at, "oc", [128, 16, 4, 128], F32); Toc = [[S.tile() for _ in range(4)] for _ in range(16)]
                ssq = sb(at, "ssq", [128, 64], F32); Tssq = S.tile()
                A1 = [sb(at, "A1%d" % i, [128, 128], F32) for i in range(2)]; TA1 = [S.tile() for _ in range(2)]
                rr = sb(at, "rr", [128, 4], F32); Trr = [S.tile() for _ in range(4)]
                sjunk = sb(at, "sjunk", [128, 128], F32)
                units = [dict(h=h, i=i, z=z) for h in range(4) for i in range(8) for z in range(2)]

                def KT_of(u, kb):
                    r0 = 64 * u["z"]
                    return KT[r0:r0 + 64, u["h"], kb * 128:(kb + 1) * 128], TKT[u["h"]][kb // 4]

                def QT_of(u):
                    r0 = 64 * u["z"]
                    return QT[r0:r0 + 64, u["h"], u["i"] * 256:(u["i"] + 1) * 256], TQT[u["h"]][u["i"] // 2]

                def V_of(u, kb):
                    return Vd[:, kb, u["h"], 0:129], TV[kb]

                def exp_emit(u, p, bi, PTb, Tp):
                    S.op("scalar", lambda e: e.activation(out=PTb[:, 2 * p:2 * p + 2, :].rearrange("p a b -> p (a b)"),
                                                          in_=banks[bi][:], func=AF.Exp),
                         reads=[Tb[bi]], writes=[Tp])

                def mask_emit(u, PTb, TPb):
                    i = u["i"]
                    lo = nk_of(i) - 4
                    S.op("vector", lambda e: e.tensor_tensor(out=PTb[:, lo:lo + 4, :], in0=PTb[:, lo:lo + 4, :],
                                                             in1=maskd[:, i % 2, :].rearrange("p (a b) -> p a b", a=4), op=ALU.min),
                         reads=[Tmd, TPb[lo // 2], TPb[lo // 2 + 1]], writes=[TPb[lo // 2], TPb[lo // 2 + 1]])

                def evac_emit(u, ob):
                    h, i, z = u["h"], u["i"], u["z"]
                    for s in range(2):
                        qb = 2 * i + s
                        o_ap = banks[ob][:, s * 129:s * 129 + 128]
                        sm_ap = banks[ob][:, s * 129 + 128:s * 129 + 129]
                        ri = 2 * z + s
                        S.op("vector", lambda e, ri=ri, sm_ap=sm_ap: e.reciprocal(out=rr[:, ri:ri + 1], in_=sm_ap),
                             reads=[Tb[ob]], writes=[Trr[ri]])
                        if z == 0:
                            S.op("vector", lambda e, ri=ri, o_ap=o_ap, s=s: e.tensor_scalar(
                                out=A1[s][:], in0=o_ap, scalar1=rr[:, ri:ri + 1], scalar2=None, op0=ALU.mult),
                                reads=[Tb[ob], Trr[ri]], writes=[TA1[s]])
                        else:
                            S.op("vector", lambda e, ri=ri: e.tensor_tensor(out=rr[:, ri:ri + 1], in0=rr[:, ri:ri + 1], in1=neglam[:], op=ALU.mult),
                                 reads=[Trr[ri], Tnl], writes=[Trr[ri]])
                            S.op("vector", lambda e, ri=ri, o_ap=o_ap, s=s, qb=qb: e.scalar_tensor_tensor(
                                out=oc[:, qb, h, :], in0=o_ap, scalar=rr[:, ri:ri + 1], in1=A1[s][:], op0=ALU.mult, op1=ALU.add),
                                reads=[Tb[ob], Trr[ri], TA1[s]], writes=[Toc[qb][h]])
                            S.op("vector", lambda e, qb=qb: e.scalar_tensor_tensor(
                                out=sjunk[:], in0=oc[:, qb, h, :], scalar=1.0, in1=oc[:, qb, h, :], op0=ALU.mult, op1=ALU.mult,
                                accum_out=ssq[:, qb * 4 + h:qb * 4 + h + 1]),
                                reads=[Toc[qb][h], Tssq], writes=[Tssq])

                run_attention(units, PT, TPT, KT_of, QT_of, V_of, 129, exp_emit, evac_emit, mask_emit,
                              s_banks=[0, 1, 2, 3], o_banks=[4, 5, 6, 7])
                S.op("scalar", lambda e: e.activation(out=ssq[:], in_=ssq[:], func=AF.Sqrt, scale=1.0 / 128, bias=EPS),
                     reads=[Tssq], writes=[Tssq])
                S.op("vector", lambda e: e.reciprocal(out=ssq[:], in_=ssq[:]), reads=[Tssq], writes=[Tssq])
                Otok = [sb(at, "Otok%d" % i, [128, 512], BF16) for i in range(2)]; TOtok = [S.tile() for _ in range(2)]
                for qb in range(16):
                    o2 = qb % 2
                    for h in range(4):
                        S.op("vector", lambda e, qb=qb, h=h, o2=o2: e.scalar_tensor_tensor(
                            out=Otok[o2][:, h * 128:(h + 1) * 128], in0=oc[:, qb, h, :], scalar=ssq[:, qb * 4 + h:qb * 4 + h + 1],
                            in1=gsc[:], op0=ALU.mult, op1=ALU.mult),
                            reads=[Toc[qb][h], Tssq, Tgsc], writes=[TOtok[o2]])
                    bi = o2
                    pTv = banks[bi][:].bitcast(BF16)
                    for c in range(4):
                        S.op("tensor", lambda e, c=c, o2=o2, pTv=pTv: e.transpose(
                            out=pTv[:, c * 128:(c + 1) * 128], in_=Otok[o2][:, c * 128:(c + 1) * 128], identity=identb[:]),
                            reads=[TOtok[o2], Tidb], writes=[Tb[bi]], signal=(c == 3))
                    S.op("vector", lambda e, qb=qb, pTv=pTv: e.tensor_copy(
                        out=OT[:, 0:4, qb * 128:(qb + 1) * 128], in_=pTv[:, 0:512].rearrange("p (c t) -> p c t", c=4)),
                        reads=[Tb[bi]], writes=[TOT[qb]])
                S.barrier()
                if "OTa" in debug:
                    dbg_out("OTa", [128, 8, 2048], BF16); dump("OTa", OT[:], []); S.barrier()
                S.emit()
        if stop_after == "A":
            return nc

        with ExitStack() as pb:
            KT = sb(pb, "KTf", [128, 4, 4096], BF16)
            TKT = [[S.tile() for _ in range(8)] for _ in range(4)]
            QT = sb(pb, "QTf", [128, 4, 2048], BF16)
            TQT = [[S.tile() for _ in range(4)] for _ in range(4)]
            Vf = sb(pb, "Vf", [128, 32, 8, 66], BF16)
            TV = [S.tile() for _ in range(32)]
            S.op("vector", lambda e: e.memset(Vf[:, :, :, 64:66], 1.0), writes=TV)
            zt = sb(pb, "zt", [128, 32, 8], F32); Tzt = [S.tile() for _ in range(32)]
            Fpos = sb(pb, "Fpos", [128, 32, 8], F32); TF = [S.tile() for _ in range(32)]
            Cpos = sb(pb, "Cpos", [128, 33, 8], F32); TC = [S.tile() for _ in range(33)]
            maskf = sb(pb, "maskf", [128, 2, 1024], BF16); Tmf = S.tile()
            S.dma("sync", maskf[:], maskf_d, writes=[Tmf])
            bfb = sb(pb, "bfb", [128, 8], F32); Tbfb = S.tile()
            bcast_load(bfb[:], bfor, Tbfb, 8)
            csel = sb(pb, "csel", [128, 8, 33], F32); Tcsel = S.tile()
            bcast_load(csel[:].rearrange("p a b -> p (a b)"), csel_d, Tcsel, 8 * 33)
            ctmp = sb(pb, "ctmp", [128, 8, 33], F32); Tctmp = S.tile()
            cq = sb(pb, "cq", [128, 8, 8], F32); Tcq = S.tile()
            tri = sb(pb, "tri", [128, 128], F32); Ttri = S.tile()
            S.dma("sync", tri[:], tri_d, writes=[Ttri])
            onesf = sb(pb, "onesf", [128, 128], F32); Tones = S.tile()
            S.dma("sync", onesf[:], onesf_d, writes=[Tones])

            with ExitStack() as sw:
                st = make_sweep(sw, "b", [0, 1])
                wA = sb(sw, "wA", [128, 8, 512], BF16); TwA = S.tile()
                wB = sb(sw, "wB", [128, 8, 512], BF16); TwB = S.tile()
                wF = sb(sw, "wF", [128, 8, 8], BF16); TwF = S.tile()
                wload(wA[:], w_in[:, 2048:2560], TwA)
                wload(wB[:], w_in[:, 2560:3072], TwB)
                wload(wF[:], w_in[:, 3072:3080], TwF)
                kctr = [0]

                def plain_proj(blk, hT, ThT, w_, Tw_, dstT, TdstT, scale):
                    for hp in range(4):
                        bk = 2 + (kctr[0] % 3)
                        kctr[0] += 1
                        mmgroup(banks[bk][:], [(w_[:, c, hp * 128:(hp + 1) * 128], hT[:, c, :]) for c in range(8)],
                                reads=list(ThT) + [Tw_], writes=[Tb[bk]])
                        S.op("scalar", lambda e, bk=bk, hp=hp: e.activation(
                            out=dstT[:, hp, blk * 512:(blk + 1) * 512], in_=banks[bk][:], func=AF.Copy, scale=scale),
                            reads=[Tb[bk]], writes=[TdstT[hp][blk]])

                def kvf_proj(blk, hT, ThT):
                    plain_proj(blk, hT, ThT, wA, TwA, KT, TKT, 1.0)
                    for tt in range(4):
                        n = blk * 4 + tt
                        bv = 6 + (tt % 2)
                        mmgroup(banks[bv][:], [(hT[:, c, tt * 128:(tt + 1) * 128], wB[:, c, :]) for c in range(8)],
                                reads=[ThT[tt], TwB], writes=[Tb[bv]])
                        S.op("vector", lambda e, n=n, bv=bv: e.tensor_copy(
                            out=Vf[:, n, :, 0:64], in_=banks[bv][:].rearrange("p (h d) -> p h d", h=8)),
                            reads=[Tb[bv]], writes=[TV[n]])
                        mmgroup(banks[5][:, 0:8], [(hT[:, c, tt * 128:(tt + 1) * 128], wF[:, c, :]) for c in range(8)],
                                reads=[ThT[tt], TwF], writes=[Tb[5]])
                        S.op("vector", lambda e, n=n: e.tensor_tensor(out=zt[:, n, :], in0=banks[5][:, 0:8], in1=bfb[:], op=ALU.add),
                             reads=[Tb[5], Tbfb], writes=[Tzt[n]])

                sweep_stage1(st, xb, 0, gb_attn, Tgba)
                for blk in range(8):
                    if blk + 1 < 8:
                        sweep_stage1(st, xb, blk + 1, gb_attn, Tgba)
                    kvf_proj(blk, st["hT"][blk % 2], st["ThT"][blk % 2])
                wload(wA[:], w_in[:, 1536:2048], TwA)
                sweep_stage1(st, xo, 0, gb_attn, Tgba)
                for blk in range(4):
                    if blk + 1 < 4:
                        sweep_stage1(st, xo, blk + 1, gb_attn, Tgba)
                    plain_proj(blk, st["hT"][blk % 2], st["ThT"][blk % 2], wA, TwA, QT, TQT, 0.125)
                ztf = zt[:].rearrange("p a b -> p (a b)")
                S.op("scalar", lambda e: e.activation(out=ztf, in_=ztf, func=AF.Exp, scale=-1.0), reads=Tzt, writes=Tzt)
                S.op("scalar", lambda e: e.activation(out=ztf, in_=ztf, func=AF.Ln, bias=1.0), reads=Tzt, writes=Tzt)
                S.op("vector", lambda e: e.memset(Cpos[:, 0, :], 0.0), writes=[TC[0]])
                for n in range(32):
                    bc = 2 + (n % 2)
                    S.op("tensor", lambda e, n=n, bc=bc: e.matmul(out=banks[bc][:, 0:8], lhsT=tri[:], rhs=zt[:, n, :], start=True, stop=True),
                         reads=[Ttri, Tzt[n]], writes=[Tb[bc]], signal=False)
                    S.op("tensor", lambda e, n=n, bc=bc: e.matmul(out=banks[bc][:, 8:16], lhsT=onesf[:], rhs=zt[:, n, :], start=True, stop=True),
                         reads=[Tones, Tzt[n]], writes=[Tb[bc]], signal=True)
                    S.op("vector", lambda e, n=n, bc=bc: e.tensor_tensor(out=Fpos[:, n, :], in0=banks[bc][:, 0:8], in1=Cpos[:, n, :], op=ALU.add),
                         reads=[Tb[bc], TC[n]], writes=[TF[n]])
                    S.op("vector", lambda e, n=n, bc=bc: e.tensor_tensor(out=Cpos[:, n + 1, :], in0=banks[bc][:, 8:16], in1=Cpos[:, n, :], op=ALU.add),
                         reads=[Tb[bc], TC[n]], writes=[TC[n + 1]])
                for i in range(8):
                    S.op("vector", lambda e, i=i: e.tensor_tensor(out=ctmp[:], in0=Cpos[:].rearrange("p n h -> p h n"),
                                                                  in1=csel[:, i, :].unsqueeze(1).broadcast_to([128, 8, 33]), op=ALU.mult),
                         reads=TC + [Tcsel, Tctmp], writes=[Tctmp])
                    S.op("vector", lambda e, i=i: e.tensor_reduce(out=cq[:, i, :], in_=ctmp[:], axis=AX.X, op=ALU.add),
                         reads=[Tctmp], writes=[Tcq])
                S.barrier()
                if "Fpos" in debug:
                    dbg_out("Fpos", [128, 32, 8]); dump("Fpos", Fpos[:], []); S.barrier()
                S.emit()

            with ExitStack() as at:
                PT = [sb(at, "PT%d" % i, [128, 32, 256], BF16) for i in range(2)]
                TPT = [[S.tile() for _ in range(16)] for _ in range(2)]
                Otf = sb(at, "Otf", [128, 16, 512], BF16); TOtf = [S.tile() for _ in range(16)]
                rr = sb(at, "rrf", [128, 2], F32); Trr = [S.tile() for _ in range(2)]
                biasb = [sb(at, "biasb%d" % i, [128, 32], F32) for i in range(2)]; Tbias = [S.tile() for _ in range(2)]
                units = [dict(hp=hp, hh=hh, i=i, head=2 * hp + hh) for hp in range(4) for hh in range(2) for i in range(8)]
                for ui, u in enumerate(units):
                    u["ui"] = ui

                def KT_of(u, kb):
                    r0 = 64 * u["hh"]
                    return KT[r0:r0 + 64, u["hp"], kb * 128:(kb + 1) * 128], TKT[u["hp"]][kb // 4]

                def QT_of(u):
                    r0 = 64 * u["hh"]
                    return QT[r0:r0 + 64, u["hp"], u["i"] * 256:(u["i"] + 1) * 256], TQT[u["hp"]][u["i"] // 2]

                def V_of(u, kb):
                    return Vf[:, kb, u["head"], 0:65], TV[kb]

                def exp_emit(u, p, bi, PTb, Tp):
                    b2 = u["ui"] % 2
                    hd = u["head"]
                    nk = nk_of(u["i"])
                    if p == 0:
                        S.op("vector", lambda e: e.tensor_scalar(out=biasb[b2][:, 0:nk], in0=Fpos[:, 0:nk, hd], scalar1=cq[:, u["i"], hd:hd + 1],
                                                                 scalar2=None, op0=ALU.subtract),
                             reads=TF[0:nk] + [Tcq], writes=[Tbias[b2]])
                    for j in range(2):
                        kb = 2 * p + j
                        S.op("scalar", lambda e, kb=kb, j=j: e.activation(out=PTb[:, kb, :], in_=banks[bi][:, j * 256:(j + 1) * 256],
                                                                          func=AF.Exp, bias=biasb[b2][:, kb:kb + 1]),
                             reads=[Tb[bi], Tbias[b2]], writes=[Tp])

                def mask_emit(u, PTb, TPb):
                    i = u["i"]
                    lo = nk_of(i) - 4
                    S.op("vector", lambda e: e.tensor_tensor(out=PTb[:, lo:lo + 4, :], in0=PTb[:, lo:lo + 4, :],
                                                             in1=maskf[:, i % 2, :].rearrange("p (a b) -> p a b", a=4), op=ALU.min),
                         reads=[Tmf, TPb[lo // 2], TPb[lo // 2 + 1]], writes=[TPb[lo // 2], TPb[lo // 2 + 1]])

                def evac_emit(u, ob):
                    hd, i = u["head"], u["i"]
                    for s in range(2):
                        qb = 2 * i + s
                        S.op("vector", lambda e, s=s: e.reciprocal(out=rr[:, s:s + 1], in_=banks[ob][:, s * 65 + 64:s * 65 + 65]),
                             reads=[Tb[ob]], writes=[Trr[s]])
                        S.op("vector", lambda e, s=s, qb=qb: e.tensor_scalar(
                            out=Otf[:, qb, hd * 64:(hd + 1) * 64], in0=banks[ob][:, s * 65:s * 65 + 64], scalar1=rr[:, s:s + 1],
                            scalar2=None, op0=ALU.mult),
                            reads=[Tb[ob], Trr[s]], writes=[TOtf[qb]])

                run_attention(units, PT, TPT, KT_of, QT_of, V_of, 65, exp_emit, evac_emit, mask_emit,
                              s_banks=[0, 1, 2, 3], o_banks=[4, 5, 6, 7])
                for qb in range(16):
                    bi = qb % 2
                    pTv = banks[bi][:].bitcast(BF16)
                    for c in range(4):
                        S.op("tensor", lambda e, c=c, qb=qb, pTv=pTv: e.transpose(
                            out=pTv[:, c * 128:(c + 1) * 128], in_=Otf[:, qb, c * 128:(c + 1) * 128], identity=identb[:]),
                            reads=[TOtf[qb], Tidb], writes=[Tb[bi]], signal=(c == 3))
                    S.op("vector", lambda e, qb=qb, pTv=pTv: e.tensor_copy(
                        out=OT[:, 4:8, qb * 128:(qb + 1) * 128], in_=pTv[:, 0:512].rearrange("p (c t) -> p c t", c=4)),
                        reads=[Tb[bi]], writes=[TOT[qb]])
                S.barrier()
                if "OTb" in debug:
                    dbg_out("OTb", [128, 8, 2048], BF16); dump("OTb", OT[:], []); S.barrier()
                S.emit()
        if stop_after == "B":
            return nc

        with ExitStack() as pc:
            x2 = sb(pc, "x2", [128, 16, 1024], F32); Tx2 = [[S.tile() for _ in range(2)] for _ in range(16)]
            hmT = OT
            ThmT = TOT
            ovf = sb(pc, "ovf", [128, 1], I32); Tovf = S.tile()
            w12 = sb(pc, "w12", [128, 2, 16], F32); Tw12 = S.tile()
            pos = sb(pc, "pos", [128, 2, 16], I32); Tpos = S.tile()
            comb = sb(pc, "comb", [128, 16, 16], F32); Tcomb = S.tile()
            junk = sb(pc, "junkc", [128, 1024], BF16)
            ssc = sb(pc, "ssc", [128, 16], F32); Tssc = [S.tile() for _ in range(16)]; Tsscall = S.tile()
            with ExitStack() as c1:
                wo = sb(c1, "wo", [128, 8, 1024], BF16); Two = S.tile()
                wload(wo[:], w_out, Two)
                xt = [sb(c1, "xc%d" % i, [128, 1024], F32) for i in range(2)]; Txt = [S.tile() for _ in range(2)]
                rw32 = sb(c1, "rw32", [128, 8, 20], F32); Trw = S.tile()
                S.dma("sync", rw32[:], rw_d.rearrange("(c p) n -> p c n", p=128), writes=[Trw])
                rbb = sb(c1, "rbb", [128, 20], F32); Trbb = S.tile()
                bcast_load(rbb[:], rb_d, Trbb, 20)
                gbf = sb(c1, "gbf", [128, 1024], F32); Tgbf = S.tile()
                bcast_load(gbf[:], g_ffn, Tgbf, 1024)
                identf = sb(c1, "identf", [128, 128], F32); Tidf = S.tile()
                S.dma("sync", identf[:], identf_d, writes=[Tidf])
                hm32 = [sb(c1, "hm32%d" % i, [128, 1024], F32) for i in range(2)]; Thm32 = [S.tile() for _ in range(2)]
                hmT32 = [sb(c1, "hmT32%d" % i, [128, 8, 128], F32) for i in range(2)]; ThmT32 = [S.tile() for _ in range(2)]
                Lall = sb(c1, "Lall", [128, 16, 20], F32); TL = [S.tile() for _ in range(16)]
                if moe == "sparse":
                    hmb = sb(c1, "hmb", [128, 16, 1024], BF16); Thmb = [S.tile() for _ in range(16)]
                for t in range(16):
                    S.dma("sync", xt[t % 2][:], xo[t * 128:(t + 1) * 128, :], writes=[Txt[t % 2]])
                    for hf in range(2):
                        mmgroup(banks[hf][:], [(OT[:, c, t * 128:(t + 1) * 128], wo[:, c, hf * 512:(hf + 1) * 512]) for c in range(8)],
                                reads=[TOT[t], Two], writes=[Tb[hf]])
                        S.op("vector", lambda e, t=t, hf=hf: e.tensor_tensor(
                            out=x2[:, t, hf * 512:(hf + 1) * 512], in0=banks[hf][:], in1=xt[t % 2][:, hf * 512:(hf + 1) * 512], op=ALU.add),
                            reads=[Tb[hf], Txt[t % 2]], writes=[Tx2[t][hf]])
                    S.op("scalar", lambda e, t=t: e.activation(out=junk[:], in_=x2[:, t, :], func=AF.Square, accum_out=ssc[:, t:t + 1]),
                         reads=Tx2[t], writes=[Tssc[t]])
                if "x2" in debug:
                    dbg_out("x2", [2048, 1024])
                    for t in range(16):
                        dump("x2", x2[:, t, :], Tx2[t]) if False else S.dma("sync", dbg["x2"][t * 128:(t + 1) * 128, :], x2[:, t, :], reads=Tx2[t], writes=[Tdbg], semtile=Tdbg)
                if stop_after == "C0":
                    S.barrier(); S.emit()
                    return nc
                S.op("scalar", lambda e: e.activation(out=ssc[:], in_=ssc[:], func=AF.Sqrt, scale=1.0 / 1024, bias=EPS),
                     reads=Tssc, writes=[Tsscall])
                S.op("vector", lambda e: e.reciprocal(out=ssc[:], in_=ssc[:]), reads=[Tsscall], writes=[Tsscall])
                for t in range(16):
                    t2 = t % 2
                    S.op("vector", lambda e, t=t, t2=t2: e.scalar_tensor_tensor(
                        out=hm32[t2][:], in0=x2[:, t, :], scalar=ssc[:, t:t + 1], in1=gbf[:], op0=ALU.mult, op1=ALU.mult),
                        reads=Tx2[t] + [Tsscall, Tgbf], writes=[Thm32[t2]])
                    ba, bb = (2, 3) if t2 == 0 else (4, 5)
                    for c in range(8):
                        bk = ba if c < 4 else bb
                        S.op("tensor", lambda e, c=c, t2=t2, bk=bk: e.transpose(
                            out=banks[bk][:, (c % 4) * 128:(c % 4 + 1) * 128], in_=hm32[t2][:, c * 128:(c + 1) * 128], identity=identf[:]),
                            reads=[Thm32[t2], Tidf], writes=[Tb[bk]], signal=(c % 4 == 3))
                    import os
                    CUT = int(os.environ.get("C1CUT", "9"))
                    if CUT < 2:
                        continue
                    for k, bk in enumerate((ba, bb)):
                        src = banks[bk][:].rearrange("p (c t) -> p c t", c=4)
                        S.op("scalar", lambda e, k=k, t2=t2, src=src: e.activation(out=hmT32[t2][:, 4 * k:4 * k + 4, :], in_=src, func=AF.Copy),
                             reads=[Tb[bk]], writes=[ThmT32[t2]])
                        S.op("vector", lambda e, k=k, t=t, t2=t2: e.tensor_copy(out=hmT[:, 4 * k:4 * k + 4, t * 128:(t + 1) * 128], in_=hmT32[t2][:, 4 * k:4 * k + 4, :]),
                             reads=[ThmT32[t2]], writes=[ThmT[t]])
                    if moe == "sparse":
                        S.op("gpsimd", lambda e, t=t, t2=t2: e.tensor_copy(out=hmb[:, t, :], in_=hm32[t2][:]), reads=[Thm32[t2]], writes=[Thmb[t]])
                    if CUT < 3:
                        continue
                    br = 6 + t2
                    mmgroup(banks[br][:, 0:20], [(hmT32[t2][:, c, :], rw32[:, c, :]) for c in range(8)],
                            reads=[ThmT32[t2], Trw], writes=[Tb[br]])
                    S.op("vector", lambda e, t=t, br=br: e.tensor_tensor(out=Lall[:, t, :], in0=banks[br][:, 0:20], in1=rbb[:], op=ALU.add),
                         reads=[Tb[br], Trbb], writes=[TL[t]])
                if stop_after == "C1a":
                    if "Lall" in debug:
                        dbg_out("Lall", [128, 320])
                        S.dma("sync", dbg["Lall"], Lall[:].rearrange("p a b -> p (a b)"), reads=[], writes=[Tdbg], semtile=Tdbg)
                    S.barrier(); S.emit()
                    return nc
                TR = S.tile()

                def rt(name, shape):
                    return sb(c1, "rt_" + name, shape, F32)
                gmax = rt("gmax", [128, 16]); gm = rt("gm", [128, 16, 4]); gd = rt("gd", [128, 16, 4])
                gsum = rt("gsum", [128, 16]); gw = rt("gw", [128, 16]); pen = rt("pen", [128, 16, 4])
                EL = rt("EL", [128, 16, 16]); EL2 = rt("EL2", [128, 16, 16]); m1 = rt("m1", [128, 16]); m2 = rt("m2", [128, 16])
                oh1 = rt("oh1", [128, 16, 16]); oh2 = rt("oh2", [128, 16, 16]); dd = rt("dd", [128, 16]); w1 = rt("w1", [128, 16]); w2 = rt("w2", [128, 16])
                LG = Lall[:, :, 0:4]
                LE4 = Lall[:, :, 4:20].rearrange("p t (g e) -> p t g e", g=4)
                EL4 = EL[:].rearrange("p t (g e) -> p t g e", g=4)

                def vop(fn, first=False):
                    S.op("vector", fn, reads=(TL + [TR]) if first else [TR], writes=[TR])

                def bc3(a, n):
                    return a[:].unsqueeze(2).broadcast_to([128, 16, n])
                vop(lambda e: e.tensor_reduce(out=gmax[:], in_=LG, axis=AX.X, op=ALU.max), first=True)
                vop(lambda e: e.tensor_tensor(out=gm[:], in0=LG, in1=bc3(gmax, 4), op=ALU.is_equal))
                vop(lambda e: e.tensor_tensor(out=gd[:], in0=LG, in1=bc3(gmax, 4), op=ALU.subtract))
                S.op("scalar", lambda e: e.activation(out=gd[:], in_=gd[:], func=AF.Exp), reads=[TR], writes=[TR])
                vop(lambda e: e.tensor_reduce(out=gsum[:], in_=gd[:], axis=AX.X, op=ALU.add))
                vop(lambda e: e.reciprocal(out=gw[:], in_=gsum[:]))
                vop(lambda e: e.tensor_scalar(out=pen[:], in0=gm[:], scalar1=1.0, scalar2=1e30, op0=ALU.subtract, op1=ALU.mult))
                vop(lambda e: e.tensor_tensor(out=EL4, in0=LE4, in1=gm[:].unsqueeze(3).broadcast_to([128, 16, 4, 4]), op=ALU.mult))
                vop(lambda e: e.tensor_tensor(out=EL4, in0=EL4, in1=pen[:].unsqueeze(3).broadcast_to([128, 16, 4, 4]), op=ALU.add))
                vop(lambda e: e.tensor_reduce(out=m1[:], in_=EL[:], axis=AX.X, op=ALU.max))
                vop(lambda e: e.tensor_tensor(out=oh1[:], in0=EL[:], in1=bc3(m1, 16), op=ALU.is_equal))
                vop(lambda e: e.scalar_tensor_tensor(out=EL2[:], in0=oh1[:], scalar=-1e30, in1=EL[:], op0=ALU.mult, op1=ALU.add))
                vop(lambda e: e.tensor_reduce(out=m2[:], in_=EL2[:], axis=AX.X, op=ALU.max))
                vop(lambda e: e.tensor_tensor(out=oh2[:], in0=EL2[:], in1=bc3(m2, 16), op=ALU.is_equal))
                vop(lambda e: e.tensor_tensor(out=dd[:], in0=m2[:], in1=m1[:], op=ALU.subtract))
                S.op("scalar", lambda e: e.activation(out=dd[:], in_=dd[:], func=AF.Exp), reads=[TR], writes=[TR])
                vop(lambda e: e.tensor_scalar(out=w1[:], in0=dd[:], scalar1=1.0, scalar2=None, op0=ALU.add))
                vop(lambda e: e.reciprocal(out=w1[:], in_=w1[:]))
                vop(lambda e: e.tensor_tensor(out=w1[:], in0=w1[:], in1=gw[:], op=ALU.mult))
                vop(lambda e: e.tensor_tensor(out=w2[:], in0=dd[:], in1=w1[:], op=ALU.mult))
                if moe == "sparse":
                    Mb = sb(c1, "Mb", [128, 16, 16], BF16)
                    ustrict = sb(c1, "ustrict", [128, 128], BF16); Tus = S.tile()
                    S.dma("sync", ustrict[:], ustrict_d, writes=[Tus])
                    onesb = sb(c1, "onesb", [128, 128], BF16); Tob_ = S.tile()
                    S.dma("sync", onesb[:], onesb_d, writes=[Tob_])
                    ebase = sb(c1, "ebase", [128, 16, 16], F32); Teb = S.tile()
                    S.dma("sync", ebase[:].rearrange("p a b -> p (a b)"), ebase_d, writes=[Teb])
                    slotf = rt("slotf", [128, 16, 16]); okf = rt("okf", [128, 16, 16]); posf = rt("posf", [128, 2, 16])
                    vop(lambda e: e.tensor_tensor(out=Mb[:], in0=oh1[:], in1=oh2[:], op=ALU.add))
                    for t in range(16):
                        prs = [(onesb[:], Mb[:, tp, :]) for tp in range(t)] + [(ustrict[:], Mb[:, t, :])]
                        n_ = len(prs)
                        for k_, (l_, r_) in enumerate(prs):
                            S.op("tensor", lambda e, l_=l_, r_=r_, k_=k_, n_=n_, t=t: e.matmul(out=banks[0][:, t * 16:(t + 1) * 16], lhsT=l_, rhs=r_,
                                                                                         start=(k_ == 0), stop=(k_ == n_ - 1)),
                                 reads=[TR, Tus, Tob_], writes=[Tb[0]], signal=(k_ == n_ - 1))
                    for tp in range(16):
                        S.op("tensor", lambda e, tp=tp: e.matmul(out=banks[1][:, 0:16], lhsT=onesb[:], rhs=Mb[:, tp, :], start=(tp == 0), stop=(tp == 15)),
                             reads=[TR, Tob_], writes=[Tb[1]], signal=(tp == 15))
                    cmax = rt("cmax", [128, 1])
                    S.op("vector", lambda e: e.tensor_reduce(out=cmax[:], in_=banks[1][:, 0:16], axis=AX.X, op=ALU.max), reads=[Tb[1], TR], writes=[TR])
                    import os as _os
                    thr = -1.0 if _os.environ.get("FORCE_DENSE") else float(CAP)
                    vop(lambda e: e.tensor_scalar(out=cmax[:], in0=cmax[:], scalar1=thr, scalar2=None, op0=ALU.is_gt))
                    S.op("vector", lambda e: e.tensor_copy(out=ovf[:], in_=cmax[:]), reads=[TR], writes=[Tovf])
                    rank = banks[0][:, 0:256].rearrange("p (a b) -> p a b", a=16)
                    S.op("vector", lambda e: e.tensor_tensor(out=slotf[:], in0=rank, in1=ebase[:], op=ALU.add), reads=[Tb[0], Teb, TR], writes=[TR])
                    vop(lambda e: e.tensor_scalar(out=okf[:], in0=slotf[:], scalar1=None, scalar2=None, op0=ALU.bypass) if False else
                        e.tensor_tensor(out=okf[:], in0=slotf[:], in1=ebase[:], op=ALU.subtract))
                    vop(lambda e: e.tensor_scalar(out=okf[:], in0=okf[:], scalar1=float(CAP), scalar2=1.0e6, op0=ALU.is_ge, op1=ALU.mult))
                    vop(lambda e: e.tensor_tensor(out=slotf[:], in0=slotf[:], in1=okf[:], op=ALU.add))
                    vop(lambda e: e.tensor_tensor(out=okf[:], in0=slotf[:], in1=oh1[:], op=ALU.mult))
                    vop(lambda e: e.tensor_reduce(out=posf[:, 0, :], in_=okf[:], axis=AX.X, op=ALU.add))
                    vop(lambda e: e.tensor_tensor(out=okf[:], in0=slotf[:], in1=oh2[:], op=ALU.mult))
                    vop(lambda e: e.tensor_reduce(out=posf[:, 1, :], in_=okf[:], axis=AX.X, op=ALU.add))
                    S.op("vector", lambda e: e.tensor_copy(out=pos[:], in_=posf[:]), reads=[TR], writes=[Tpos])
                    S.op("vector", lambda e: e.tensor_copy(out=w12[:, 0, :], in_=w1[:]), reads=[TR, Tw12], writes=[Tw12])
                    S.op("vector", lambda e: e.tensor_copy(out=w12[:, 1, :], in_=w2[:]), reads=[TR, Tw12], writes=[Tw12])
                    Tsc = [S.tile() for _ in range(32)]
                    set_bcreg()
                    for t in range(16):
                        for k_ in range(2):
                            S.dma_fn("gpsimd", lambda e, t=t, k_=k_: e.indirect_dma_start(
                                out=xs_d[:, :], out_offset=bass.IndirectOffsetOnAxis(ap=pos[:, k_, t:t + 1], axis=0),
                                in_=hmb[:, t, :], in_offset=None, bounds_check=bcreg, oob_is_err=False),
                                reads=[Thmb[t], Tpos, Txs], writes=[Tsc[2 * t + k_]], semtile=Thmb[t])
                vop(lambda e: e.tensor_tensor(out=oh1[:], in0=oh1[:], in1=bc3(w1, 16), op=ALU.mult))
                vop(lambda e: e.tensor_tensor(out=oh2[:], in0=oh2[:], in1=bc3(w2, 16), op=ALU.mult))
                S.op("vector", lambda e: e.tensor_tensor(out=comb[:], in0=oh1[:], in1=oh2[:], op=ALU.add), reads=[TR], writes=[Tcomb])
                S.barrier()
                if "comb" in debug:
                    dbg_out("comb", [128, 256])
                    S.dma("sync", dbg["comb"], comb[:].rearrange("p a b -> p (a b)"), reads=[Tcomb], writes=[Tdbg], semtile=Tdbg)
                    S.barrier()
                S.emit()
            if stop_after == "C1":
                return nc

            with ExitStack() as c2:
                wgb = [sb(c2, "wgb%d" % i, [128, 8, 512], BF16) for i in range(2)]; Twg = [S.tile() for _ in range(2)]
                wub = [sb(c2, "wub%d" % i, [128, 8, 512], BF16) for i in range(2)]; Twu = [S.tile() for _ in range(2)]
                wdb = [sb(c2, "wdb%d" % i, [128, 4, 1024], BF16) for i in range(2)]; Twd = [S.tile() for _ in range(2)]
                aT = [sb(c2, "aT%d" % i, [128, 4, 512], BF16) for i in range(2)]; TaT = [[S.tile() for _ in range(4)] for _ in range(2)]
                sg = [sb(c2, "sg%d" % i, [128, 512], F32) for i in range(2)]; Tsg = [S.tile() for _ in range(2)]
                xg = [sb(c2, "xg%d" % i, [128, 1024], BF16) for i in range(3)]; Txg = [S.tile() for _ in range(3)]
                xgT = [sb(c2, "xgT%d" % i, [128, 8, CAP], BF16) for i in range(2)]; TxgT = [[S.tile() for _ in range(CAP // 128)] for _ in range(2)]
                ysb = [sb(c2, "ysb%d" % i, [128, 1024], F32) for i in range(2)]; Tysb = [S.tile() for _ in range(2)]
                NJ = CAP // 128
                Tys = [S.tile() for _ in range(NEXP * NJ)]

                def load_w(ex):
                    b2 = ex % 2
                    wload(wgb[b2][:], wg_d[ex], Twg[b2])
                    wload(wub[b2][:], wu_d[ex], Twu[b2])
                    S.dma("gpsimd", wdb[b2][:], wd_d[ex].rearrange("(c p) n -> p c n", p=128), writes=[Twd[b2]])

                def gate_up(b2, rhs_of, Trhs, width, gq):
                    for ft in range(4):
                        bg, bu = (0, 1) if gq[0] % 2 == 0 else (2, 3)
                        s2 = gq[0] % 2
                        gq[0] += 1
                        mmgroup(banks[bg][:, 0:width], [(wgb[b2][:, c, ft * 128:(ft + 1) * 128], rhs_of(c)) for c in range(8)],
                                reads=Trhs + [Twg[b2]], writes=[Tb[bg]])
                        mmgroup(banks[bu][:, 0:width], [(wub[b2][:, c, ft * 128:(ft + 1) * 128], rhs_of(c)) for c in range(8)],
                                reads=Trhs + [Twu[b2]], writes=[Tb[bu]])
                        S.op("scalar", lambda e, bg=bg, s2=s2: e.activation(out=sg[s2][:, 0:width], in_=banks[bg][:, 0:width], func=AF.Silu),
                             reads=[Tb[bg]], writes=[Tsg[s2]])
                        S.op("vector", lambda e, bu=bu, s2=s2, ft=ft: e.tensor_tensor(out=aT[b2][:, ft, 0:width], in0=sg[s2][:, 0:width], in1=banks[bu][:, 0:width], op=ALU.mult),
                             reads=[Tsg[s2], Tb[bu]], writes=[TaT[b2][ft]])

                S.branch_begin()
                gq = [0]; yq = [0]; xq = [0]
                load_w(0)
                for ex in range(NEXP):
                    b2 = ex % 2
                    if ex + 1 < NEXP:
                        load_w(ex + 1)
                    for j in range(NJ):
                        xi = xq[0] % 3
                        xq[0] += 1
                        r0 = ex * CAP + j * 128
                        S.dma("sync", xg[xi][:], xs_d[r0:r0 + 128, :], reads=Tsc + [Txs], writes=[Txg[xi]])
                        bi = 6 + (xq[0] % 2)
                        pTv = banks[bi][:].bitcast(BF16)
                        for c in range(8):
                            S.op("tensor", lambda e, c=c, xi=xi, pTv=pTv: e.transpose(
                                out=pTv[:, c * 128:(c + 1) * 128], in_=xg[xi][:, c * 128:(c + 1) * 128], identity=identb[:]),
                                reads=[Txg[xi], Tidb], writes=[Tb[bi]], signal=(c == 7))
                        S.op("vector", lambda e, j=j, b2=b2, pTv=pTv: e.tensor_copy(
                            out=xgT[b2][:, :, j * 128:(j + 1) * 128], in_=pTv.rearrange("p (c t) -> p c t", c=8)),
                            reads=[Tb[bi]], writes=[TxgT[b2][j]])
                    gate_up(b2, lambda c, b2=b2: xgT[b2][:, c, :], TxgT[b2], CAP, gq)
                    for j in range(NJ):
                        y2 = yq[0] % 2
                        yq[0] += 1
                        for hf in range(2):
                            by = 4 + hf
                            mmgroup(banks[by][:], [(aT[b2][:, ft, j * 128:(j + 1) * 128], wdb[b2][:, ft, hf * 512:(hf + 1) * 512]) for ft in range(4)],
                                    reads=TaT[b2] + [Twd[b2]], writes=[Tb[by]])
                            if hf == 0:
                                S.op("vector", lambda e, y2=y2, by=by: e.tensor_copy(out=ysb[y2][:, 0:512], in_=banks[by][:]),
                                     reads=[Tb[by]], writes=[Tysb[y2]])
                            else:
                                S.op("scalar", lambda e, y2=y2, by=by: e.activation(out=ysb[y2][:, 512:1024], in_=banks[by][:], func=AF.Copy),
                                     reads=[Tb[by], Tysb[y2]], writes=[Tysb[y2]])
                        r0 = ex * CAP + j * 128
                        S.dma("sync", ys_d[r0:r0 + 128, :], ysb[y2][:], reads=[Tysb[y2]], writes=[Tys[ex * NJ + j]], semtile=Tysb[y2])
                S.barrier()
                ygl = []
                for wb in wgb + wub + wdb:
                    v = wb[:].rearrange("p a b -> p (a b)").bitcast(F32)
                    ygl += [v[:, 0:1024], v[:, 1024:2048]]
                Tyg = [S.tile() for _ in ygl]
                set_bcreg()
                def gath(i):
                    t, k_ = i // 2, i % 2
                    gi = i % len(ygl)
                    S.dma_fn("gpsimd", lambda e, t=t, k_=k_, gi=gi: e.indirect_dma_start(
                        out=ygl[gi], out_offset=None, in_=ys_d[:, :],
                        in_offset=bass.IndirectOffsetOnAxis(ap=pos[:, k_, t:t + 1], axis=0),
                        bounds_check=bcreg, oob_is_err=False),
                        reads=Tys + [Tpos], writes=[Tyg[gi]], semtile=Tyg[gi])

                def acc(i):
                    t, k_ = i // 2, i % 2
                    gi = i % len(ygl)
                    for hf in range(2):
                        S.op("vector", lambda e, t=t, k_=k_, gi=gi, hf=hf: e.scalar_tensor_tensor(
                            out=x2[:, t, hf * 512:(hf + 1) * 512], in0=ygl[gi][:, hf * 512:(hf + 1) * 512], scalar=w12[:, k_, t:t + 1],
                            in1=x2[:, t, hf * 512:(hf + 1) * 512], op0=ALU.mult, op1=ALU.add),
                            reads=[Tyg[gi], Tw12, Tx2[t][hf]], writes=[Tx2[t][hf]])
                depth = len(ygl) - 1
                for i in range(32 + depth):
                    if i < 32:
                        gath(i)
                    if i - depth >= 0:
                        acc(i - depth)
                S.branch_mid()
                gq = [0]; yq = [0]
                load_w(0)
                for ex in range(NEXP):
                    b2 = ex % 2
                    if ex + 1 < NEXP:
                        load_w(ex + 1)
                    for tb in range(4):
                        gate_up(b2, lambda c, tb=tb: hmT[:, c, tb * 512:(tb + 1) * 512], ThmT[tb * 4:tb * 4 + 4], 512, gq)
                        for tt in range(4):
                            t = tb * 4 + tt
                            for hf in range(2):
                                by = 4 + (yq[0] % 4)
                                yq[0] += 1
                                mmgroup(banks[by][:], [(aT[b2][:, ft, tt * 128:(tt + 1) * 128], wdb[b2][:, ft, hf * 512:(hf + 1) * 512]) for ft in range(4)],
                                        reads=TaT[b2] + [Twd[b2]], writes=[Tb[by]])
                                S.op("vector", lambda e, t=t, hf=hf, by=by, ex=ex: e.scalar_tensor_tensor(
                                    out=x2[:, t, hf * 512:(hf + 1) * 512], in0=banks[by][:], scalar=comb[:, t, ex:ex + 1],
                                    in1=x2[:, t, hf * 512:(hf + 1) * 512], op0=ALU.mult, op1=ALU.add),
                                    reads=[Tb[by], Tcomb, Tx2[t][hf]], writes=[Tx2[t][hf]])
                S.branch_end(ovf[0:1, 0:1], brregs)
                S.emit()

            with ExitStack() as c3:
                gbn = sb(c3, "gbn", [128, 1024], F32); Tgbn = S.tile()
                bcast_load(gbn[:], g_fin, Tgbn, 1024)
                ob = [sb(c3, "ob%d" % i, [128, 1024], F32) for i in range(2)]; Tob = [S.tile() for _ in range(2)]
                Tout = S.tile()
                for t in range(16):
                    S.op("scalar", lambda e, t=t: e.activation(out=junk[:], in_=x2[:, t, :], func=AF.Square, accum_out=ssc[:, t:t + 1]),
                         reads=Tx2[t] + [Tsscall], writes=[Tssc[t]])
                S.op("scalar", lambda e: e.activation(out=ssc[:], in_=ssc[:], func=AF.Sqrt, scale=1.0 / 1024, bias=EPS),
                     reads=Tssc, writes=[Tsscall])
                S.op("vector", lambda e: e.reciprocal(out=ssc[:], in_=ssc[:]), reads=[Tsscall], writes=[Tsscall])
                for t in range(16):
                    S.op("vector", lambda e, t=t: e.scalar_tensor_tensor(
                        out=ob[t % 2][:], in0=x2[:, t, :], scalar=ssc[:, t:t + 1], in1=gbn[:], op0=ALU.mult, op1=ALU.mult),
                        reads=Tx2[t] + [Tsscall, Tgbn], writes=[Tob[t % 2]])
                    S.dma("sync", out[t * 128:(t + 1) * 128, :], ob[t % 2][:], reads=[Tob[t % 2]], writes=[Tout], semtile=Tob[t % 2])
                S.barrier()
                S.emit()
    return nc


def _const_tables():
    f32 = np.float32
    inv_freq = (f32(1.0) / (f32(10000.0) ** (np.arange(0, 64, 2, dtype=f32) / f32(64)))).astype(f32)
    pos = np.arange(4096, dtype=f32)
    ang = (pos[:, None] * inv_freq[None, :]).astype(f32)
    cos = np.cos(ang).astype(f32)
    sin = np.sin(ang).astype(f32)
    r = np.arange(128)
    dh = r % 64
    cosT = cos[:, dh % 32].T.copy()
    sgn = np.where(dh < 32, -1.0, 1.0).astype(f32)
    sinT = (sin[:, dh % 32].T * sgn[:, None]).astype(f32)
    return cosT, sinT


def _masks(hf):
    k = np.arange(128)[:, None, None]
    r = np.arange(4)[None, :, None]
    q = np.arange(256)[None, None, :]
    md = np.zeros((128, 2, 4, 256), np.float32)
    mf = np.zeros((128, 2, 4, 256), np.float32)
    for par in range(2):
        if par == 0:
            kb = r
            j = 0 if hf == 0 else 1
        else:
            kb = 4 + r
            j = 3 if hf == 0 else 2
        s = kb * 128 + k
        t = j * 256 + q
        mf[:, par] = np.where(s <= t, 3e38, 0.0)
        md[:, par] = np.where((s // 64) <= (t // 64), 3e38, 0.0)
    return (md.reshape(128, 2, 1024).astype(ml_dtypes.bfloat16),
            mf.reshape(128, 2, 1024).astype(ml_dtypes.bfloat16))


def own_tokens(hf):
    return np.concatenate([np.arange(j * 256, (j + 1) * 256) for j in own_qtiles(hf)])


def prep(inputs):
    f32 = np.float32
    x = np.asarray(inputs["x"], f32)
    w_in = np.ascontiguousarray(np.asarray(inputs["w_in"], f32)[0])

    def swap_cols(w):
        return np.ascontiguousarray(w.reshape(1024, 8, 2, 32)[:, :, ::-1, :].reshape(1024, 512))

    cosT, sinT = _const_tables()
    common = {
        "w_in": w_in,
        "wqs": swap_cols(w_in[:, 0:512]),
        "wks": swap_cols(w_in[:, 512:1024]),
        "cosk": cosT, "sink": sinT,
        "identb": np.eye(128, dtype=f32).astype(ml_dtypes.bfloat16),
        "identf": np.eye(128, dtype=f32),
        "tri": np.triu(np.ones((128, 128), f32)),
        "onesf": np.ones((128, 128), f32),
        "onesb": np.ones((128, 128), f32).astype(ml_dtypes.bfloat16),
        "ustrict": np.triu(np.ones((128, 128), f32), 1).astype(ml_dtypes.bfloat16),
        "ebase": np.ascontiguousarray(np.broadcast_to((np.arange(16, dtype=f32) * CAP)[None, None, :], (128, 16, 16)).reshape(128, 256)),
        "g_attn": np.asarray(inputs["norm_attn_g"], f32).reshape(1, 1024),
        "g_ffn": np.asarray(inputs["norm_ffn_g"], f32).reshape(1, 1024),
        "g_fin": np.asarray(inputs["norm_final_g"], f32).reshape(1, 1024),
        "bfor": np.asarray(inputs["b_forget"], f32).reshape(1, 8),
        "lamv": np.concatenate([np.asarray(inputs[k], f32).reshape(1, 64) for k in
                                ("lambda_q1", "lambda_k1", "lambda_q2", "lambda_k2")], axis=1),
        "dng": np.asarray(inputs["diff_norm_g"], f32).reshape(1, 128),
        "w_out": np.ascontiguousarray(np.asarray(inputs["w_out"], f32)[0]),
        "rw": np.ascontiguousarray(np.concatenate([np.asarray(inputs["router_group_w"], f32)[0],
                                                   np.asarray(inputs["router_expert_w"], f32)[0]], axis=1)),
        "rb": np.concatenate([np.asarray(inputs["router_group_b"], f32).reshape(1, 4),
                              np.asarray(inputs["router_expert_b"], f32).reshape(1, 16)], axis=1),
        "wg": np.ascontiguousarray(np.asarray(inputs["w_gate"], f32)[0]),
        "wu": np.ascontiguousarray(np.asarray(inputs["w_up"], f32)[0]),
        "wd": np.ascontiguousarray(np.asarray(inputs["w_down"], f32)[0]),
    }
    in_maps = []
    for c in range(8):
        b, hf = c // 2, c % 2
        tok = own_tokens(hf)
        md, mf = _masks(hf)
        m = dict(common)
        m["xb"] = np.ascontiguousarray(x[b])
        m["xo"] = np.ascontiguousarray(x[b][tok])
        m["cosq"] = np.ascontiguousarray(cosT[:, tok] * f32(0.125))
        m["sinq"] = np.ascontiguousarray(sinT[:, tok] * f32(0.125))
        cs = np.zeros((8, 33), f32)
        for i, j in enumerate(own_qtiles(hf)):
            cs[i, 2 * j + 1] = 1.0
        m["csel"] = cs.reshape(1, 8 * 33)
        m["maskd"] = md
        m["maskf"] = mf
        in_maps.append(m)
    return in_maps


def kernel(**inputs):
    in_maps = prep(inputs)
    nc = build()
    res = run_bass_kernel_spmd(nc, in_maps, core_ids=list(range(8)))
    out = np.zeros((4, 4096, 1024), np.float32)
    for c in range(8):
        b, hf = c // 2, c % 2
        out[b, own_tokens(hf)] = res.results[c]["out"]
    return out
```

```python
import numpy as np
import ml_dtypes
from contextlib import ExitStack
import concourse.bass as bass
import concourse.mybir as mybir
from concourse.bass_utils import run_bass_kernel_spmd

F32 = mybir.dt.float32
BF16 = mybir.dt.bfloat16
I32 = mybir.dt.int32
AF = mybir.ActivationFunctionType
ALU = mybir.AluOpType
AX = mybir.AxisListType

ENGS = ("sync", "scalar", "vector", "gpsimd", "tensor")
EPS = 1e-6
LAM_INIT = 0.8 - 0.6 * 1.0
NEXP = 16
CAP = 512
NSLOT = NEXP * CAP


class Tile:
    __slots__ = ("name", "last_w", "readers", "dsem")

    def __init__(self, name):
        self.name = name
        self.last_w = None
        self.readers = {}
        self.dsem = None


class Sched:
    def __init__(self, nc, es):
        self.nc = nc
        self.es = es
        self.ops = {e: [] for e in ENGS}
        self.sems = {}
        self.cnt = {}
        self.waited = {e: {} for e in ENGS}
        self.pending = {e: False for e in ENGS}
        for e in ENGS:
            self._mksem("E:" + e)
        self.n_dsem = 0
        self.nops = 0
        self.tiles = []

    def _mksem(self, key):
        self.sems[key] = self.es.enter_context(self.nc.semaphore(key.replace(":", "_")))
        self.cnt[key] = 0

    def tile(self, name="t"):
        t = Tile(name)
        self.tiles.append(t)
        return t

    def _snapshot(self):
        return (dict(self.cnt), {e: dict(w) for e, w in self.waited.items()},
                [(t, t.last_w, dict(t.readers)) for t in self.tiles])

    def _restore(self, snap):
        self.cnt = dict(snap[0])
        for k in self.sems:
            self.cnt.setdefault(k, 0)
        self.waited = {e: dict(w) for e, w in snap[1].items()}
        for t, lw, rd in snap[2]:
            t.last_w = lw
            t.readers = dict(rd)

    def branch_begin(self):
        self.barrier()
        self._outer_ops = self.ops
        self.ops = {e: [] for e in ENGS}
        self._snap = self._snapshot()

    def branch_mid(self):
        self.barrier()
        self._A = (self.ops, dict(self.cnt))
        self.ops = {e: [] for e in ENGS}
        self._restore(self._snap)

    def branch_end(self, flag_ap, regs):
        self.barrier()
        opsA, cntA = self._A
        opsB, cntB = self.ops, dict(self.cnt)
        target = {k: max(cntA.get(k, 0), cntB.get(k, 0)) for k in set(cntA) | set(cntB)}

        def pads(cntX):
            out = {e: [] for e in ENGS}
            for k, v in target.items():
                d = v - cntX.get(k, 0)
                if d > 0:
                    owner = k[2:] if k.startswith("E:") else "gpsimd"
                    out[owner].append((k, d))
            return out
        pA, pB = pads(cntA), pads(cntB)
        self.ops = self._outer_ops
        for e in ENGS:
            self.ops[e].append(("branch", flag_ap, regs[e], opsA[e], pA[e], opsB[e], pB[e]))
        self.cnt = target
        for e in ENGS:
            self.waited[e] = dict(target)
        for t in self.tiles:
            t.last_w = None
            t.readers = {}

    def dsem_for(self, t):
        if t.dsem is None:
            key = "D:%d" % self.n_dsem
            self.n_dsem += 1
            self._mksem(key)
            t.dsem = key
        return t.dsem

    def _need(self, eng, waits, key, val):
        if eng == "tensor" and key == "E:tensor":
            return
        if self.cnt[key] < val:
            raise RuntimeError("wait on un-signalled event %s %d (cnt %d) from %s" % (key, val, self.cnt[key], eng))
        if self.waited[eng].get(key, 0) >= val:
            return
        self.waited[eng][key] = val
        waits[key] = max(waits.get(key, 0), val)

    def _deps(self, eng, reads, writes):
        waits = {}
        for t in reads:
            if t.last_w is not None:
                self._need(eng, waits, *t.last_w)
        for t in writes:
            if t.last_w is not None:
                self._need(eng, waits, *t.last_w)
            for k, v in t.readers.items():
                self._need(eng, waits, k, v)
        return list(waits.items())

    def _record(self, ev, reads, writes):
        for t in writes:
            t.last_w = ev
            t.readers = {}
        for t in reads:
            if t not in writes:
                if t.readers.get(ev[0], 0) < ev[1]:
                    t.readers[ev[0]] = ev[1]

    def op(self, eng, fn, reads=(), writes=(), signal=True):
        waits = self._deps(eng, reads, writes)
        key = "E:" + eng
        if signal:
            self.cnt[key] += 1
            ev = (key, self.cnt[key])
            inc = (key, 1)
            self.pending[eng] = False
        else:
            ev = (key, self.cnt[key] + 1)
            inc = None
            self.pending[eng] = True
        self._record(ev, reads, writes)
        self.ops[eng].append((waits, fn, inc))
        self.nops += 1

    def dma(self, eng, out, in_, reads=(), writes=(), semtile=None, **kw):
        waits = self._deps(eng, reads, writes)
        if semtile is None:
            semtile = writes[0] if writes else reads[0]
        key = self.dsem_for(semtile)
        self.cnt[key] += 16
        ev = (key, self.cnt[key])
        self._record(ev, reads, writes)

        def fn(e, out=out, in_=in_, kw=kw):
            return e.dma_start(out=out, in_=in_, **kw)
        self.ops[eng].append((waits, fn, (key, 16)))
        self.nops += 1

    def dma_fn(self, eng, fn, reads=(), writes=(), semtile=None):
        waits = self._deps(eng, reads, writes)
        key = self.dsem_for(semtile)
        self.cnt[key] += 16
        ev = (key, self.cnt[key])
        self._record(ev, reads, writes)
        self.ops[eng].append((waits, fn, (key, 16)))
        self.nops += 1

    def barrier(self, engs=ENGS):
        for e in ENGS:
            assert not self.pending[e], e
        for e in engs:
            waits = {}
            for key, c in self.cnt.items():
                if c > 0:
                    self._need(e, waits, key, c)
            self.ops[e].append((list(waits.items()), None, None))

    def emit(self):
        nc = self.nc
        sems = self.sems
        ops = self.ops
        with nc.Block() as block:
            def replay(e, lst):
                for ent in lst:
                    if ent[0] == "branch":
                        _, flag_ap, reg, oA, pA, oB, pB = ent
                        e.reg_load(reg, flag_ap)
                        with e.If_eq(reg, 0):
                            replay(e, oA)
                            for k, d in pA:
                                e.sem_inc(sems[k], d)
                            e.nop()
                        with e.Else():
                            replay(e, oB)
                            for k, d in pB:
                                e.sem_inc(sems[k], d)
                            e.nop()
                        continue
                    waits, fn, inc = ent
                    for key, val in waits:
                        e.wait_ge(sems[key], val)
                    if fn is None:
                        continue
                    inst = fn(e)
                    if inc is not None:
                        inst.then_inc(sems[inc[0]], inc[1])

            def mk(name):
                def body(e):
                    replay(e, ops[name])
                return body
            block.sync(mk("sync"))
            block.scalar(mk("scalar"))
            block.vector(mk("vector"))
            block.gpsimd(mk("gpsimd"))
            block.tensor(mk("tensor"))
        self.ops = {e: [] for e in ENGS}


def own_qtiles(hf):
    js = []
    for m in range(4):
        js += ([4 * m, 4 * m + 3] if hf == 0 else [4 * m + 1, 4 * m + 2])
    return js


def nk_of(i):
    return 8 * (i // 2) + (4 if i % 2 == 0 else 8)


def interleave(A, B):
    a, b = len(A), len(B)
    if a == 0:
        for f in B:
            f()
        return
    done = 0
    for k, f in enumerate(A):
        f()
        upto = ((k + 1) * b) // a
        while done < upto:
            B[done]()
            done += 1
    while done < b:
        B[done]()
        done += 1


def build(debug=(), stop_after=None, moe="sparse"):
    nc = bass.Bass("TRN2", target_bir_lowering=False)

    def din(name, shape, dt=F32):
        return nc.dram_tensor(name, list(shape), dt, kind="ExternalInput").ap()

    xb = din("xb", [4096, 1024])
    xo = din("xo", [2048, 1024])
    w_in = din("w_in", [1024, 3080])
    wqs_d = din("wqs", [1024, 512])
    wks_d = din("wks", [1024, 512])
    cosk = din("cosk", [128, 4096])
    sink = din("sink", [128, 4096])
    cosq = din("cosq", [128, 2048])
    sinq = din("sinq", [128, 2048])
    maskd_d = din("maskd", [128, 2, 1024], BF16)
    maskf_d = din("maskf", [128, 2, 1024], BF16)
    identb_d = din("identb", [128, 128], BF16)
    identf_d = din("identf", [128, 128])
    tri_d = din("tri", [128, 128])
    onesf_d = din("onesf", [128, 128])
    g_attn = din("g_attn", [1, 1024])
    g_ffn = din("g_ffn", [1, 1024])
    g_fin = din("g_fin", [1, 1024])
    bfor = din("bfor", [1, 8])
    lam_d = din("lamv", [1, 256])
    csel_d = din("csel", [1, 8 * 33])
    dng = din("dng", [1, 128])
    w_out = din("w_out", [1024, 1024])
    rw_d = din("rw", [1024, 20])
    rb_d = din("rb", [1, 20])
    wg_d = din("wg", [16, 1024, 512])
    wu_d = din("wu", [16, 1024, 512])
    wd_d = din("wd", [16, 512, 1024])
    ebase_d = din("ebase", [128, 256])
    ustrict_d = din("ustrict", [128, 128], BF16)
    onesb_d = din("onesb", [128, 128], BF16)
    xs_d = nc.dram_tensor("xs_scratch", [NSLOT, 1024], BF16).ap()
    ys_d = nc.dram_tensor("ys_scratch", [NSLOT, 1024], F32).ap()
    out = nc.dram_tensor("out", [2048, 1024], F32, kind="ExternalOutput").ap()
    dbg = {}

    def dbg_out(name, shape, dt=F32):
        if name in debug:
            dbg[name] = nc.dram_tensor("dbg_" + name, list(shape), dt, kind="ExternalOutput").ap()
            return dbg[name]
        return None

    with ExitStack() as es:
        S = Sched(nc, es)

        uniq = [0]

        def sb(sc, name, shape, dt):
            uniq[0] += 1
            return sc.enter_context(nc.sbuf_tensor("s%d_%s" % (uniq[0], name), list(shape), dt))

        banks = [es.enter_context(nc.psum_tensor("bank%d" % i, [128, 512], F32)) for i in range(8)]
        Tb = [S.tile("bank%d" % i) for i in range(8)]
        Tdbg = S.tile("dbg")

        def dump(name, src_ap, reads):
            if name in dbg:
                S.dma("sync", dbg[name], src_ap, reads=reads, writes=[Tdbg], semtile=Tdbg)

        def bcast_load(dst, src, T, n):
            S.dma("sync", dst, src.broadcast_to([128, n]), writes=[T])

        bcreg = es.enter_context(nc.gpsimd.register("bcreg"))
        brregs = {e: es.enter_context(getattr(nc, e).register("br_" + e)) for e in ENGS}

        def set_bcreg():
            S.ops["gpsimd"].append(([], lambda e: e.reg_mov(bcreg, NSLOT - 1), None))

        identb = sb(es, "identb", [128, 128], BF16); Tidb = S.tile("identb")
        S.dma("sync", identb[:], identb_d, writes=[Tidb])
        gb_attn = sb(es, "gb_attn", [128, 1024], F32); Tgba = S.tile("gba")
        bcast_load(gb_attn[:], g_attn, Tgba, 1024)
        OT = sb(es, "OT", [128, 8, 2048], BF16)
        TOT = [S.tile("OT%d" % q) for q in range(16)]

        def make_sweep(sc, pfx, pT_banks):
            st = {}
            st["xt"] = [sb(sc, pfx + "xt%d" % i, [128, 1024], F32) for i in range(4)]
            st["Txt"] = [S.tile() for _ in range(4)]
            st["hb"] = [sb(sc, pfx + "hb%d" % i, [128, 1024], BF16) for i in range(2)]
            st["Thb"] = [S.tile() for _ in range(2)]
            st["hT"] = [sb(sc, pfx + "hT%d" % i, [128, 8, 512], BF16) for i in range(3)]
            st["ThT"] = [[S.tile() for _ in range(4)] for _ in range(3)]
            st["junk"] = sb(sc, pfx + "junk", [128, 1024], BF16)
            st["ss"] = [sb(sc, pfx + "ss%d" % i, [128, 4], F32) for i in range(3)]
            st["Tss"] = [[S.tile() for _ in range(4)] for _ in range(3)]
            st["sq"] = [sb(sc, pfx + "sq%d" % i, [128, 4], F32) for i in range(3)]
            st["Tsq"] = [S.tile() for _ in range(3)]
            st["rs"] = [sb(sc, pfx + "rs%d" % i, [128, 4], F32) for i in range(3)]
            st["Trs"] = [S.tile() for _ in range(3)]
            st["pT"] = pT_banks
            return st

        def sweep_stage1(st, x_ap, blk, gb, Tgb):
            b2 = blk % 3
            for tt in range(4):
                n = blk * 4 + tt
                xt, Txt = st["xt"][n % 4], st["Txt"][n % 4]
                S.dma("sync", xt[:], x_ap[n * 128:(n + 1) * 128, :], writes=[Txt])
                S.op("scalar", lambda e, xt=xt, tt=tt: e.activation(out=st["junk"][:], in_=xt[:], func=AF.Square,
                                                                    accum_out=st["ss"][b2][:, tt:tt + 1]),
                     reads=[Txt], writes=[st["Tss"][b2][tt]])
            S.op("scalar", lambda e: e.activation(out=st["sq"][b2][:], in_=st["ss"][b2][:], func=AF.Sqrt,
                                                  scale=1.0 / 1024, bias=EPS),
                 reads=st["Tss"][b2], writes=[st["Tsq"][b2]])
            S.op("vector", lambda e: e.reciprocal(out=st["rs"][b2][:], in_=st["sq"][b2][:]),
                 reads=[st["Tsq"][b2]], writes=[st["Trs"][b2]])
            def stt(tt):
                n = blk * 4 + tt
                xt, Txt = st["xt"][n % 4], st["Txt"][n % 4]
                hb, Thb = st["hb"][n % 2], st["Thb"][n % 2]
                S.op("vector", lambda e, xt=xt, hb=hb, tt=tt: e.scalar_tensor_tensor(
                    out=hb[:], in0=xt[:], scalar=st["rs"][b2][:, tt:tt + 1], in1=gb[:], op0=ALU.mult, op1=ALU.mult),
                    reads=[Txt, st["Trs"][b2], Tgb], writes=[Thb])

            def tr(tt):
                n = blk * 4 + tt
                hb, Thb = st["hb"][n % 2], st["Thb"][n % 2]
                bi = st["pT"][n % 2]
                pTv = banks[bi][:].bitcast(BF16)
                for c in range(8):
                    S.op("tensor", lambda e, c=c, hb=hb, pTv=pTv: e.transpose(
                        out=pTv[:, c * 128:(c + 1) * 128], in_=hb[:, c * 128:(c + 1) * 128], identity=identb[:]),
                        reads=[Thb, Tidb], writes=[Tb[bi]], signal=(c == 7))

            def ev(tt):
                n = blk * 4 + tt
                bi = st["pT"][n % 2]
                pTv = banks[bi][:].bitcast(BF16)
                S.op("vector", lambda e, tt=tt, pTv=pTv: e.tensor_copy(
                    out=st["hT"][b2][:, :, tt * 128:(tt + 1) * 128], in_=pTv.rearrange("p (c t) -> p c t", c=8)),
                    reads=[Tb[bi]], writes=[st["ThT"][b2][tt]])
            for f, a in ((stt, 0), (stt, 1), (tr, 0), (ev, 0), (stt, 2), (tr, 1), (ev, 1), (stt, 3), (tr, 2), (ev, 2), (tr, 3), (ev, 3)):
                f(a)

        def wload(dst, src_cols, T):
            S.dma("gpsimd", dst, src_cols.rearrange("(c p) n -> p c n", p=128), writes=[T])

        def mmgroup(out_ap, pairs, reads, writes):
            n = len(pairs)
            for k, (l, r) in enumerate(pairs):
                S.op("tensor", lambda e, l=l, r=r, k=k: e.matmul(out=out_ap, lhsT=l, rhs=r, start=(k == 0), stop=(k == n - 1)),
                     reads=reads, writes=writes, signal=(k == n - 1))

        def run_attention(units, PT, TPT, KT_of, QT_of, V_of, W, exp_emit, evac_emit, mask_emit, s_banks, o_banks):
            gctr = [0]

            def st_list(ui, u):
                buf = ui % 2
                nk = nk_of(u["i"])
                L = []
                for p in range(nk // 2):
                    def f(p=p):
                        bi = s_banks[gctr[0] % len(s_banks)]
                        gctr[0] += 1
                        for j in range(2):
                            kb = 2 * p + j
                            kap, Tk = KT_of(u, kb)
                            qap, Tq = QT_of(u)
                            S.op("tensor", lambda e, kap=kap, qap=qap, j=j, bi=bi: e.matmul(
                                out=banks[bi][:, j * 256:(j + 1) * 256], lhsT=kap, rhs=qap, start=True, stop=True),
                                reads=[Tk, Tq], writes=[Tb[bi]], signal=(j == 1))
                        exp_emit(u, p, bi, PT[buf], TPT[buf][p])
                    L.append(f)
                L.append(lambda: mask_emit(u, PT[buf], TPT[buf]))
                return L

            def pv_list(ui, u):
                buf = ui % 2
                nk = nk_of(u["i"])
                ob = o_banks[ui % len(o_banks)]
                L = []
                for s in range(2):
                    for k0 in range(0, nk, 4):
                        def f(s=s, k0=k0):
                            for kb in range(k0, min(nk, k0 + 4)):
                                vap, Tv = V_of(u, kb)
                                S.op("tensor", lambda e, kb=kb, s=s, vap=vap: e.matmul(
                                    out=banks[ob][:, s * W:(s + 1) * W], lhsT=PT[buf][:, kb, s * 128:(s + 1) * 128],
                                    rhs=vap, start=(kb == 0), stop=(kb == nk - 1)),
                                    reads=[TPT[buf][kb // 2], Tv], writes=[Tb[ob]], signal=(kb == nk - 1))
                        L.append(f)
                L.append(lambda: evac_emit(u, ob))
                return L

            prev = None
            for ui in range(len(units) + 1):
                A = st_list(ui, units[ui]) if ui < len(units) else []
                B = pv_list(ui - 1, units[ui - 1]) if ui >= 1 else []
                interleave(A, B)

        with ExitStack() as pa:
            KT = sb(pa, "KTd", [128, 4, 4096], BF16)
            TKT = [[S.tile() for _ in range(8)] for _ in range(4)]
            QT = sb(pa, "QTd", [128, 4, 2048], BF16)
            TQT = [[S.tile() for _ in range(4)] for _ in range(4)]
            Vd = sb(pa, "Vd", [128, 32, 4, 130], BF16)
            TV = [S.tile() for _ in range(32)]
            Tvones = S.tile()
            S.op("vector", lambda e: e.memset(Vd[:, :, :, 128:130], 1.0), writes=TV)
            lamt = sb(pa, "lamt", [128, 256], F32); Tlam = S.tile()
            bcast_load(lamt[:], lam_d, Tlam, 256)
            lj = sb(pa, "lj", [128, 64], F32)
            ls = sb(pa, "ls", [128, 2], F32); Tls = S.tile()
            le = sb(pa, "le", [128, 2], F32); Tle = S.tile()
            neglam = sb(pa, "neglam", [128, 1], F32); Tnl = S.tile()
            for z in range(2):
                S.op("vector", lambda e, z=z: e.scalar_tensor_tensor(
                    out=lj[:], in0=lamt[:, z * 128:z * 128 + 64], scalar=1.0, in1=lamt[:, z * 128 + 64:z * 128 + 128],
                    op0=ALU.mult, op1=ALU.mult, accum_out=ls[:, z:z + 1]), reads=[Tlam, Tls], writes=[Tls])
            S.op("scalar", lambda e: e.activation(out=le[:], in_=ls[:], func=AF.Exp), reads=[Tls], writes=[Tle])
            S.op("vector", lambda e: e.tensor_tensor(out=neglam[:], in0=le[:, 1:2], in1=le[:, 0:1], op=ALU.subtract),
                 reads=[Tle], writes=[Tnl])
            S.op("vector", lambda e: e.tensor_scalar(out=neglam[:], in0=neglam[:], scalar1=-LAM_INIT, scalar2=None, op0=ALU.add),
                 reads=[Tnl], writes=[Tnl])
            gsc = sb(pa, "gsc", [128, 128], F32); Tgsc = S.tile()
            bcast_load(gsc[:], dng, Tgsc, 128)
            S.op("vector", lambda e: e.tensor_scalar(out=gsc[:], in0=gsc[:], scalar1=1.0 - LAM_INIT, scalar2=None, op0=ALU.mult),
                 reads=[Tgsc], writes=[Tgsc])
            Txs = S.tile("xs")

            with ExitStack() as sw:
                st = make_sweep(sw, "a", [0, 1])
                wA = sb(sw, "wA", [128, 8, 512], BF16); TwA = S.tile()
                wB = sb(sw, "wB", [128, 8, 512], BF16); TwB = S.tile()
                wC = sb(sw, "wC", [128, 8, 512], BF16); TwC = S.tile()
                wload(wA[:], w_in[:, 512:1024], TwA)
                wload(wB[:], wks_d, TwB)
                wload(wC[:], w_in[:, 1024:1536], TwC)
                ct = [sb(sw, "ct%d" % i, [128, 512], F32) for i in range(2)]; Tct = [S.tile() for _ in range(2)]
                sn = [sb(sw, "sn%d" % i, [128, 512], F32) for i in range(2)]; Tsn = [S.tile() for _ in range(2)]
                t1 = [sb(sw, "t1%d" % i, [128, 512], F32) for i in range(2)]; Tt1 = [S.tile() for _ in range(2)]
                t2 = [sb(sw, "t2%d" % i, [128, 512], F32) for i in range(2)]; Tt2 = [S.tile() for _ in range(2)]
                rctr = [0]

                def rope_proj(blk, hT, ThT, wq_, Tw_, ws_, Tws_, cos_d, sin_d, dstT, TdstT):
                    b2 = blk % 2
                    S.dma("sync", ct[b2][:], cos_d[:, blk * 512:(blk + 1) * 512], writes=[Tct[b2]])
                    S.dma("sync", sn[b2][:], sin_d[:, blk * 512:(blk + 1) * 512], writes=[Tsn[b2]])
                    for h in range(4):
                        ba, bb = (2, 3) if h % 2 == 0 else (4, 5)
                        mmgroup(banks[ba][:], [(wq_[:, c, h * 128:(h + 1) * 128], hT[:, c, :]) for c in range(8)],
                                reads=list(ThT) + [Tw_], writes=[Tb[ba]])
                        mmgroup(banks[bb][:], [(ws_[:, c, h * 128:(h + 1) * 128], hT[:, c, :]) for c in range(8)],
                                reads=list(ThT) + [Tws_], writes=[Tb[bb]])
                        r = rctr[0] % 2
                        rctr[0] += 1
                        S.op("vector", lambda e, r=r, ba=ba: e.tensor_tensor(out=t1[r][:], in0=banks[ba][:], in1=ct[b2][:], op=ALU.mult),
                             reads=[Tb[ba], Tct[b2]], writes=[Tt1[r]])
                        S.op("vector", lambda e, r=r, bb=bb: e.tensor_tensor(out=t2[r][:], in0=banks[bb][:], in1=sn[b2][:], op=ALU.mult),
                             reads=[Tb[bb], Tsn[b2]], writes=[Tt2[r]])
                        S.op("gpsimd", lambda e, r=r, h=h: e.tensor_tensor(out=dstT[:, h, blk * 512:(blk + 1) * 512], in0=t1[r][:], in1=t2[r][:], op=ALU.add),
                             reads=[Tt1[r], Tt2[r]], writes=[TdstT[h][blk]])

                def kv_proj(blk, hT, ThT):
                    rope_proj(blk, hT, ThT, wA, TwA, wB, TwB, cosk, sink, KT, TKT)
                    for tt in range(4):
                        n = blk * 4 + tt
                        bv = 6 + (tt % 2)
                        mmgroup(banks[bv][:], [(hT[:, c, tt * 128:(tt + 1) * 128], wC[:, c, :]) for c in range(8)],
                                reads=[ThT[tt], TwC], writes=[Tb[bv]])
                        S.op("scalar", lambda e, n=n, bv=bv: e.activation(
                            out=Vd[:, n, :, 0:128], in_=banks[bv][:].rearrange("p (h d) -> p h d", h=4), func=AF.Copy),
                            reads=[Tb[bv]], writes=[TV[n]])

                sweep_stage1(st, xb, 0, gb_attn, Tgba)
                sweep_stage1(st, xb, 1, gb_attn, Tgba)
                for blk in range(8):
                    if blk + 2 < 8:
                        sweep_stage1(st, xb, blk + 2, gb_attn, Tgba)
                    kv_proj(blk, st["hT"][blk % 3], st["ThT"][blk % 3])
                wload(wA[:], w_in[:, 0:512], TwA)
                wload(wB[:], wqs_d, TwB)
                sweep_stage1(st, xo, 0, gb_attn, Tgba)
                sweep_stage1(st, xo, 1, gb_attn, Tgba)
                for blk in range(4):
                    if blk + 2 < 4:
                        sweep_stage1(st, xo, blk + 2, gb_attn, Tgba)
                    rope_proj(blk, st["hT"][blk % 3], st["ThT"][blk % 3], wA, TwA, wB, TwB, cosq, sinq, QT, TQT)
                S.barrier()
                if "KTd" in debug:
                    dbg_out("KTd", [128, 4, 4096], BF16); dump("KTd", KT[:], [])
                    dbg_out("QTd", [128, 4, 2048], BF16); dump("QTd", QT[:], [])
                    dbg_out("Vd", [128, 32, 4, 130], BF16); dump("Vd", Vd[:], [])
                    S.barrier()
                S.emit()
            if stop_after == "Aproj":
                S.barrier(); S.emit()
                return nc

            with ExitStack() as at:
                if moe == "sparse":
                    zer = sb(at, "zer", [128, 2048], BF16); Tzer = S.tile()
                    S.op("vector", lambda e: e.memset(zer[:], 0.0), writes=[Tzer])
                    for n in range(NSLOT // 256):
                        S.dma("sync", xs_d[n * 256:(n + 1) * 256, :].rearrange("(p r) d -> p (r d)", r=2), zer[:], reads=[Tzer], writes=[Txs], semtile=Tzer)
                maskd = sb(at, "maskd", [128, 2, 1024], BF16); Tmd = S.tile()
                S.dma("sync", maskd[:], maskd_d, writes=[Tmd])
                PT = [sb(at, "PT%d" % i, [128, 32, 256], BF16) for i in range(2)]
                TPT = [[S.tile() for _ in range(16)] for _ in range(2)]
                oc = sb(at, "oc", [128, 16, 4, 128], F32); Toc = [[S.tile() for _ in range(4)] for _ in range(16)]
                ssq = sb(at, "ssq", [128, 64], F32); Tssq = S.tile()
                A1 = [sb(at, "A1%d" % i, [128, 128], F32) for i in range(2)]; TA1 = [S.tile() for _ in range(2)]
                rr = sb(at, "rr", [128, 4], F32); Trr = [S.tile() for _ in range(4)]
                sjunk = sb(at, "sjunk", [128, 128], F32)
                units = [dict(h=h, i=i, z=z) for h in range(4) for i in range(8) for z in range(2)]

                def KT_of(u, kb):
                    r0 = 64 * u["z"]
                    return KT[r0:r0 + 64, u["h"], kb * 128:(kb + 1) * 128], TKT[u["h"]][kb // 4]

                def QT_of(u):
                    r0 = 64 * u["z"]
                    return QT[r0:r0 + 64, u["h"], u["i"] * 256:(u["i"] + 1) * 256], TQT[u["h"]][u["i"] // 2]

                def V_of(u, kb):
                    return Vd[:, kb, u["h"], 0:129], TV[kb]

                def exp_emit(u, p, bi, PTb, Tp):
                    S.op("scalar", lambda e: e.activation(out=PTb[:, 2 * p:2 * p + 2, :].rearrange("p a b -> p (a b)"),
                                                          in_=banks[bi][:], func=AF.Exp),
                         reads=[Tb[bi]], writes=[Tp])

                def mask_emit(u, PTb, TPb):
                    i = u["i"]
                    lo = nk_of(i) - 4
                    S.op("vector", lambda e: e.tensor_tensor(out=PTb[:, lo:lo + 4, :], in0=PTb[:, lo:lo + 4, :],
                                                             in1=maskd[:, i % 2, :].rearrange("p (a b) -> p a b", a=4), op=ALU.min),
                         reads=[Tmd, TPb[lo // 2], TPb[lo // 2 + 1]], writes=[TPb[lo // 2], TPb[lo // 2 + 1]])

                def evac_emit(u, ob):
                    h, i, z = u["h"], u["i"], u["z"]
                    for s in range(2):
                        qb = 2 * i + s
                        o_ap = banks[ob][:, s * 129:s * 129 + 128]
                        sm_ap = banks[ob][:, s * 129 + 128:s * 129 + 129]
                        ri = 2 * z + s
                        S.op("vector", lambda e, ri=ri, sm_ap=sm_ap: e.reciprocal(out=rr[:, ri:ri + 1], in_=sm_ap),
                             reads=[Tb[ob]], writes=[Trr[ri]])
                        if z == 0:
                            S.op("vector", lambda e, ri=ri, o_ap=o_ap, s=s: e.tensor_scalar(
                                out=A1[s][:], in0=o_ap, scalar1=rr[:, ri:ri + 1], scalar2=None, op0=ALU.mult),
                                reads=[Tb[ob], Trr[ri]], writes=[TA1[s]])
                        else:
                            S.op("vector", lambda e, ri=ri: e.tensor_tensor(out=rr[:, ri:ri + 1], in0=rr[:, ri:ri + 1], in1=neglam[:], op=ALU.mult),
                                 reads=[Trr[ri], Tnl], writes=[Trr[ri]])
                            S.op("vector", lambda e, ri=ri, o_ap=o_ap, s=s, qb=qb: e.scalar_tensor_tensor(
                                out=oc[:, qb, h, :], in0=o_ap, scalar=rr[:, ri:ri + 1], in1=A1[s][:], op0=ALU.mult, op1=ALU.add),
                                reads=[Tb[ob], Trr[ri], TA1[s]], writes=[Toc[qb][h]])
                            S.op("vector", lambda e, qb=qb: e.scalar_tensor_tensor(
                                out=sjunk[:], in0=oc[:, qb, h, :], scalar=1.0, in1=oc[:, qb, h, :], op0=ALU.mult, op1=ALU.mult,
                                accum_out=ssq[:, qb * 4 + h:qb * 4 + h + 1]),
                                reads=[Toc[qb][h], Tssq], writes=[Tssq])

                run_attention(units, PT, TPT, KT_of, QT_of, V_of, 129, exp_emit, evac_emit, mask_emit,
                              s_banks=[0, 1, 2, 3], o_banks=[4, 5, 6, 7])
                S.op("scalar", lambda e: e.activation(out=ssq[:], in_=ssq[:], func=AF.Sqrt, scale=1.0 / 128, bias=EPS),
                     reads=[Tssq], writes=[Tssq])
                S.op("vector", lambda e: e.reciprocal(out=ssq[:], in_=ssq[:]), reads=[Tssq], writes=[Tssq])
                Otok = [sb(at, "Otok%d" % i, [128, 512], BF16) for i in range(2)]; TOtok = [S.tile() for _ in range(2)]
                for qb in range(16):
                    o2 = qb % 2
                    for h in range(4):
                        S.op("vector", lambda e, qb=qb, h=h, o2=o2: e.scalar_tensor_tensor(
                            out=Otok[o2][:, h * 128:(h + 1) * 128], in0=oc[:, qb, h, :], scalar=ssq[:, qb * 4 + h:qb * 4 + h + 1],
                            in1=gsc[:], op0=ALU.mult, op1=ALU.mult),
                            reads=[Toc[qb][h], Tssq, Tgsc], writes=[TOtok[o2]])
                    bi = o2
                    pTv = banks[bi][:].bitcast(BF16)
                    for c in range(4):
                        S.op("tensor", lambda e, c=c, o2=o2, pTv=pTv: e.transpose(
                            out=pTv[:, c * 128:(c + 1) * 128], in_=Otok[o2][:, c * 128:(c + 1) * 128], identity=identb[:]),
                            reads=[TOtok[o2], Tidb], writes=[Tb[bi]], signal=(c == 3))
                    S.op("vector", lambda e, qb=qb, pTv=pTv: e.tensor_copy(
                        out=OT[:, 0:4, qb * 128:(qb + 1) * 128], in_=pTv[:, 0:512].rearrange("p (c t) -> p c t", c=4)),
                        reads=[Tb[bi]], writes=[TOT[qb]])
                S.barrier()
                if "OTa" in debug:
                    dbg_out("OTa", [128, 8, 2048], BF16); dump("OTa", OT[:], []); S.barrier()
                S.emit()
        if stop_after == "A":
            return nc

        with ExitStack() as pb:
            KT = sb(pb, "KTf", [128, 4, 4096], BF16)
            TKT = [[S.tile() for _ in range(8)] for _ in range(4)]
            QT = sb(pb, "QTf", [128, 4, 2048], BF16)
            TQT = [[S.tile() for _ in range(4)] for _ in range(4)]
            Vf = sb(pb, "Vf", [128, 32, 8, 66], BF16)
            TV = [S.tile() for _ in range(32)]
            S.op("vector", lambda e: e.memset(Vf[:, :, :, 64:66], 1.0), writes=TV)
            zt = sb(pb, "zt", [128, 32, 8], F32); Tzt = [S.tile() for _ in range(32)]
            Fpos = sb(pb, "Fpos", [128, 32, 8], F32); TF = [S.tile() for _ in range(32)]
            Cpos = sb(pb, "Cpos", [128, 33, 8], F32); TC = [S.tile() for _ in range(33)]
            maskf = sb(pb, "maskf", [128, 2, 1024], BF16); Tmf = S.tile()
            S.dma("sync", maskf[:], maskf_d, writes=[Tmf])
            bfb = sb(pb, "bfb", [128, 8], F32); Tbfb = S.tile()
            bcast_load(bfb[:], bfor, Tbfb, 8)
            csel = sb(pb, "csel", [128, 8, 33], F32); Tcsel = S.tile()
            bcast_load(csel[:].rearrange("p a b -> p (a b)"), csel_d, Tcsel, 8 * 33)
            ctmp = sb(pb, "ctmp", [128, 8, 33], F32); Tctmp = S.tile()
            cq = sb(pb, "cq", [128, 8, 8], F32); Tcq = S.tile()
            tri = sb(pb, "tri", [128, 128], F32); Ttri = S.tile()
            S.dma("sync", tri[:], tri_d, writes=[Ttri])
            onesf = sb(pb, "onesf", [128, 128], F32); Tones = S.tile()
            S.dma("sync", onesf[:], onesf_d, writes=[Tones])

            with ExitStack() as sw:
                st = make_sweep(sw, "b", [0, 1])
                wA = sb(sw, "wA", [128, 8, 512], BF16); TwA = S.tile()
                wB = sb(sw, "wB", [128, 8, 512], BF16); TwB = S.tile()
                wF = sb(sw, "wF", [128, 8, 8], BF16); TwF = S.tile()
                wload(wA[:], w_in[:, 2048:2560], TwA)
                wload(wB[:], w_in[:, 2560:3072], TwB)
                wload(wF[:], w_in[:, 3072:3080], TwF)
                kctr = [0]

                def plain_proj(blk, hT, ThT, w_, Tw_, dstT, TdstT, scale):
                    for hp in range(4):
                        bk = 2 + (kctr[0] % 3)
                        kctr[0] += 1
                        mmgroup(banks[bk][:], [(w_[:, c, hp * 128:(hp + 1) * 128], hT[:, c, :]) for c in range(8)],
                                reads=list(ThT) + [Tw_], writes=[Tb[bk]])
                        S.op("scalar", lambda e, bk=bk, hp=hp: e.activation(
                            out=dstT[:, hp, blk * 512:(blk + 1) * 512], in_=banks[bk][:], func=AF.Copy, scale=scale),
                            reads=[Tb[bk]], writes=[TdstT[hp][blk]])

                def kvf_proj(blk, hT, ThT):
                    plain_proj(blk, hT, ThT, wA, TwA, KT, TKT, 1.0)
                    for tt in range(4):
                        n = blk * 4 + tt
                        bv = 6 + (tt % 2)
                        mmgroup(banks[bv][:], [(hT[:, c, tt * 128:(tt + 1) * 128], wB[:, c, :]) for c in range(8)],
                                reads=[ThT[tt], TwB], writes=[Tb[bv]])
                        S.op("vector", lambda e, n=n, bv=bv: e.tensor_copy(
                            out=Vf[:, n, :, 0:64], in_=banks[bv][:].rearrange("p (h d) -> p h d", h=8)),
                            reads=[Tb[bv]], writes=[TV[n]])
                        mmgroup(banks[5][:, 0:8], [(hT[:, c, tt * 128:(tt + 1) * 128], wF[:, c, :]) for c in range(8)],
                                reads=[ThT[tt], TwF], writes=[Tb[5]])
                        S.op("vector", lambda e, n=n: e.tensor_tensor(out=zt[:, n, :], in0=banks[5][:, 0:8], in1=bfb[:], op=ALU.add),
                             reads=[Tb[5], Tbfb], writes=[Tzt[n]])

                sweep_stage1(st, xb, 0, gb_attn, Tgba)
                sweep_stage1(st, xb, 1, gb_attn, Tgba)
                for blk in range(8):
                    if blk + 2 < 8:
                        sweep_stage1(st, xb, blk + 2, gb_attn, Tgba)
                    kvf_proj(blk, st["hT"][blk % 3], st["ThT"][blk % 3])
                wload(wA[:], w_in[:, 1536:2048], TwA)
                sweep_stage1(st, xo, 0, gb_attn, Tgba)
                sweep_stage1(st, xo, 1, gb_attn, Tgba)
                for blk in range(4):
                    if blk + 2 < 4:
                        sweep_stage1(st, xo, blk + 2, gb_attn, Tgba)
                    plain_proj(blk, st["hT"][blk % 3], st["ThT"][blk % 3], wA, TwA, QT, TQT, 0.125)
                ztf = zt[:].rearrange("p a b -> p (a b)")
                S.op("scalar", lambda e: e.activation(out=ztf, in_=ztf, func=AF.Exp, scale=-1.0), reads=Tzt, writes=Tzt)
                S.op("scalar", lambda e: e.activation(out=ztf, in_=ztf, func=AF.Ln, bias=1.0), reads=Tzt, writes=Tzt)
                S.op("vector", lambda e: e.memset(Cpos[:, 0, :], 0.0), writes=[TC[0]])
                for n in range(32):
                    bc = 2 + (n % 2)
                    S.op("tensor", lambda e, n=n, bc=bc: e.matmul(out=banks[bc][:, 0:8], lhsT=tri[:], rhs=zt[:, n, :], start=True, stop=True),
                         reads=[Ttri, Tzt[n]], writes=[Tb[bc]], signal=False)
                    S.op("tensor", lambda e, n=n, bc=bc: e.matmul(out=banks[bc][:, 8:16], lhsT=onesf[:], rhs=zt[:, n, :], start=True, stop=True),
                         reads=[Tones, Tzt[n]], writes=[Tb[bc]], signal=True)
                    S.op("vector", lambda e, n=n, bc=bc: e.tensor_tensor(out=Fpos[:, n, :], in0=banks[bc][:, 0:8], in1=Cpos[:, n, :], op=ALU.add),
                         reads=[Tb[bc], TC[n]], writes=[TF[n]])
                    S.op("vector", lambda e, n=n, bc=bc: e.tensor_tensor(out=Cpos[:, n + 1, :], in0=banks[bc][:, 8:16], in1=Cpos[:, n, :], op=ALU.add),
                         reads=[Tb[bc], TC[n]], writes=[TC[n + 1]])
                for i in range(8):
                    S.op("vector", lambda e, i=i: e.tensor_tensor(out=ctmp[:], in0=Cpos[:].rearrange("p n h -> p h n"),
                                                                  in1=csel[:, i, :].unsqueeze(1).broadcast_to([128, 8, 33]), op=ALU.mult),
                         reads=TC + [Tcsel, Tctmp], writes=[Tctmp])
                    S.op("vector", lambda e, i=i: e.tensor_reduce(out=cq[:, i, :], in_=ctmp[:], axis=AX.X, op=ALU.add),
                         reads=[Tctmp], writes=[Tcq])
                S.barrier()
                if "Fpos" in debug:
                    dbg_out("Fpos", [128, 32, 8]); dump("Fpos", Fpos[:], []); S.barrier()
                S.emit()

            with ExitStack() as at:
                PT = [sb(at, "PT%d" % i, [128, 32, 256], BF16) for i in range(2)]
                TPT = [[S.tile() for _ in range(16)] for _ in range(2)]
                Otf = sb(at, "Otf", [128, 16, 512], BF16); TOtf = [S.tile() for _ in range(16)]
                rr = sb(at, "rrf", [128, 2], F32); Trr = [S.tile() for _ in range(2)]
                biasb = [sb(at, "biasb%d" % i, [128, 32], F32) for i in range(2)]; Tbias = [S.tile() for _ in range(2)]
                units = [dict(hp=hp, hh=hh, i=i, head=2 * hp + hh) for hp in range(4) for hh in range(2) for i in range(8)]
                for ui, u in enumerate(units):
                    u["ui"] = ui

                def KT_of(u, kb):
                    r0 = 64 * u["hh"]
                    return KT[r0:r0 + 64, u["hp"], kb * 128:(kb + 1) * 128], TKT[u["hp"]][kb // 4]

                def QT_of(u):
                    r0 = 64 * u["hh"]
                    return QT[r0:r0 + 64, u["hp"], u["i"] * 256:(u["i"] + 1) * 256], TQT[u["hp"]][u["i"] // 2]

                def V_of(u, kb):
                    return Vf[:, kb, u["head"], 0:65], TV[kb]

                def exp_emit(u, p, bi, PTb, Tp):
                    b2 = u["ui"] % 2
                    hd = u["head"]
                    nk = nk_of(u["i"])
                    if p == 0:
                        S.op("vector", lambda e: e.tensor_scalar(out=biasb[b2][:, 0:nk], in0=Fpos[:, 0:nk, hd], scalar1=cq[:, u["i"], hd:hd + 1],
                                                                 scalar2=None, op0=ALU.subtract),
                             reads=TF[0:nk] + [Tcq], writes=[Tbias[b2]])
                    for j in range(2):
                        kb = 2 * p + j
                        S.op("scalar", lambda e, kb=kb, j=j: e.activation(out=PTb[:, kb, :], in_=banks[bi][:, j * 256:(j + 1) * 256],
                                                                          func=AF.Exp, bias=biasb[b2][:, kb:kb + 1]),
                             reads=[Tb[bi], Tbias[b2]], writes=[Tp])

                def mask_emit(u, PTb, TPb):
                    i = u["i"]
                    lo = nk_of(i) - 4
                    S.op("vector", lambda e: e.tensor_tensor(out=PTb[:, lo:lo + 4, :], in0=PTb[:, lo:lo + 4, :],
                                                             in1=maskf[:, i % 2, :].rearrange("p (a b) -> p a b", a=4), op=ALU.min),
                         reads=[Tmf, TPb[lo // 2], TPb[lo // 2 + 1]], writes=[TPb[lo // 2], TPb[lo // 2 + 1]])

                def evac_emit(u, ob):
                    hd, i = u["head"], u["i"]
                    for s in range(2):
                        qb = 2 * i + s
                        S.op("vector", lambda e, s=s: e.reciprocal(out=rr[:, s:s + 1], in_=banks[ob][:, s * 65 + 64:s * 65 + 65]),
                             reads=[Tb[ob]], writes=[Trr[s]])
                        S.op("vector", lambda e, s=s, qb=qb: e.tensor_scalar(
                            out=Otf[:, qb, hd * 64:(hd + 1) * 64], in0=banks[ob][:, s * 65:s * 65 + 64], scalar1=rr[:, s:s + 1],
                            scalar2=None, op0=ALU.mult),
                            reads=[Tb[ob], Trr[s]], writes=[TOtf[qb]])

                run_attention(units, PT, TPT, KT_of, QT_of, V_of, 65, exp_emit, evac_emit, mask_emit,
                              s_banks=[0, 1, 2, 3], o_banks=[4, 5, 6, 7])
                for qb in range(16):
                    bi = qb % 2
                    pTv = banks[bi][:].bitcast(BF16)
                    for c in range(4):
                        S.op("tensor", lambda e, c=c, qb=qb, pTv=pTv: e.transpose(
                            out=pTv[:, c * 128:(c + 1) * 128], in_=Otf[:, qb, c * 128:(c + 1) * 128], identity=identb[:]),
                            reads=[TOtf[qb], Tidb], writes=[Tb[bi]], signal=(c == 3))
                    S.op("vector", lambda e, qb=qb, pTv=pTv: e.tensor_copy(
                        out=OT[:, 4:8, qb * 128:(qb + 1) * 128], in_=pTv[:, 0:512].rearrange("p (c t) -> p c t", c=4)),
                        reads=[Tb[bi]], writes=[TOT[qb]])
                S.barrier()
                if "OTb" in debug:
                    dbg_out("OTb", [128, 8, 2048], BF16); dump("OTb", OT[:], []); S.barrier()
                S.emit()
        if stop_after == "B":
            return nc

        with ExitStack() as pc:
            x2 = sb(pc, "x2", [128, 16, 1024], F32); Tx2 = [[S.tile() for _ in range(2)] for _ in range(16)]
            hmT = OT
            ThmT = TOT
            ovf = sb(pc, "ovf", [128, 1], I32); Tovf = S.tile()
            w12 = sb(pc, "w12", [128, 2, 16], F32); Tw12 = S.tile()
            pos = sb(pc, "pos", [128, 2, 16], I32); Tpos = S.tile()
            comb = sb(pc, "comb", [128, 16, 16], F32); Tcomb = S.tile()
            junk = sb(pc, "junkc", [128, 1024], BF16)
            ssc = sb(pc, "ssc", [128, 16], F32); Tssc = [S.tile() for _ in range(16)]; Tsscall = S.tile()
            with ExitStack() as c1:
                wo = sb(c1, "wo", [128, 8, 1024], BF16); Two = S.tile()
                wload(wo[:], w_out, Two)
                xt = [sb(c1, "xc%d" % i, [128, 1024], F32) for i in range(2)]; Txt = [S.tile() for _ in range(2)]
                rw32 = sb(c1, "rw32", [128, 8, 20], F32); Trw = S.tile()
                S.dma("sync", rw32[:], rw_d.rearrange("(c p) n -> p c n", p=128), writes=[Trw])
                rbb = sb(c1, "rbb", [128, 20], F32); Trbb = S.tile()
                bcast_load(rbb[:], rb_d, Trbb, 20)
                gbf = sb(c1, "gbf", [128, 1024], F32); Tgbf = S.tile()
                bcast_load(gbf[:], g_ffn, Tgbf, 1024)
                identf = sb(c1, "identf", [128, 128], F32); Tidf = S.tile()
                S.dma("sync", identf[:], identf_d, writes=[Tidf])
                hm32 = [sb(c1, "hm32%d" % i, [128, 1024], F32) for i in range(2)]; Thm32 = [S.tile() for _ in range(2)]
                hmT32 = [sb(c1, "hmT32%d" % i, [128, 8, 128], F32) for i in range(2)]; ThmT32 = [S.tile() for _ in range(2)]
                Lall = sb(c1, "Lall", [128, 16, 20], F32); TL = [S.tile() for _ in range(16)]
                if moe == "sparse":
                    hmb = sb(c1, "hmb", [128, 16, 1024], BF16); Thmb = [S.tile() for _ in range(16)]
                for t in range(16):
                    S.dma("sync", xt[t % 2][:], xo[t * 128:(t + 1) * 128, :], writes=[Txt[t % 2]])
                    for hf in range(2):
                        mmgroup(banks[hf][:], [(OT[:, c, t * 128:(t + 1) * 128], wo[:, c, hf * 512:(hf + 1) * 512]) for c in range(8)],
                                reads=[TOT[t], Two], writes=[Tb[hf]])
                        S.op("vector", lambda e, t=t, hf=hf: e.tensor_tensor(
                            out=x2[:, t, hf * 512:(hf + 1) * 512], in0=banks[hf][:], in1=xt[t % 2][:, hf * 512:(hf + 1) * 512], op=ALU.add),
                            reads=[Tb[hf], Txt[t % 2]], writes=[Tx2[t][hf]])
                    S.op("scalar", lambda e, t=t: e.activation(out=junk[:], in_=x2[:, t, :], func=AF.Square, accum_out=ssc[:, t:t + 1]),
                         reads=Tx2[t], writes=[Tssc[t]])
                if "x2" in debug:
                    dbg_out("x2", [2048, 1024])
                    for t in range(16):
                        dump("x2", x2[:, t, :], Tx2[t]) if False else S.dma("sync", dbg["x2"][t * 128:(t + 1) * 128, :], x2[:, t, :], reads=Tx2[t], writes=[Tdbg], semtile=Tdbg)
                if stop_after == "C0":
                    S.barrier(); S.emit()
                    return nc
                S.op("scalar", lambda e: e.activation(out=ssc[:], in_=ssc[:], func=AF.Sqrt, scale=1.0 / 1024, bias=EPS),
                     reads=Tssc, writes=[Tsscall])
                S.op("vector", lambda e: e.reciprocal(out=ssc[:], in_=ssc[:]), reads=[Tsscall], writes=[Tsscall])
                for t in range(16):
                    t2 = t % 2
                    S.op("vector", lambda e, t=t, t2=t2: e.scalar_tensor_tensor(
                        out=hm32[t2][:], in0=x2[:, t, :], scalar=ssc[:, t:t + 1], in1=gbf[:], op0=ALU.mult, op1=ALU.mult),
                        reads=Tx2[t] + [Tsscall, Tgbf], writes=[Thm32[t2]])
                    ba, bb = (2, 3) if t2 == 0 else (4, 5)
                    for c in range(8):
                        bk = ba if c < 4 else bb
                        S.op("tensor", lambda e, c=c, t2=t2, bk=bk: e.transpose(
                            out=banks[bk][:, (c % 4) * 128:(c % 4 + 1) * 128], in_=hm32[t2][:, c * 128:(c + 1) * 128], identity=identf[:]),
                            reads=[Thm32[t2], Tidf], writes=[Tb[bk]], signal=(c % 4 == 3))
                    import os
                    CUT = int(os.environ.get("C1CUT", "9"))
                    if CUT < 2:
                        continue
                    for k, bk in enumerate((ba, bb)):
                        src = banks[bk][:].rearrange("p (c t) -> p c t", c=4)
                        S.op("scalar", lambda e, k=k, t2=t2, src=src: e.activation(out=hmT32[t2][:, 4 * k:4 * k + 4, :], in_=src, func=AF.Copy),
                             reads=[Tb[bk]], writes=[ThmT32[t2]])
                        S.op("vector", lambda e, k=k, t=t, t2=t2: e.tensor_copy(out=hmT[:, 4 * k:4 * k + 4, t * 128:(t + 1) * 128], in_=hmT32[t2][:, 4 * k:4 * k + 4, :]),
                             reads=[ThmT32[t2]], writes=[ThmT[t]])
                    if moe == "sparse":
                        S.op("gpsimd", lambda e, t=t, t2=t2: e.tensor_copy(out=hmb[:, t, :], in_=hm32[t2][:]), reads=[Thm32[t2]], writes=[Thmb[t]])
                    if CUT < 3:
                        continue
                    br = 6 + t2
                    mmgroup(banks[br][:, 0:20], [(hmT32[t2][:, c, :], rw32[:, c, :]) for c in range(8)],
                            reads=[ThmT32[t2], Trw], writes=[Tb[br]])
                    S.op("vector", lambda e, t=t, br=br: e.tensor_tensor(out=Lall[:, t, :], in0=banks[br][:, 0:20], in1=rbb[:], op=ALU.add),
                         reads=[Tb[br], Trbb], writes=[TL[t]])
                if stop_after == "C1a":
                    if "Lall" in debug:
                        dbg_out("Lall", [128, 320])
                        S.dma("sync", dbg["Lall"], Lall[:].rearrange("p a b -> p (a b)"), reads=[], writes=[Tdbg], semtile=Tdbg)
                    S.barrier(); S.emit()
                    return nc
                TR = S.tile()

                def rt(name, shape):
                    return sb(c1, "rt_" + name, shape, F32)
                gmax = rt("gmax", [128, 16]); gm = rt("gm", [128, 16, 4]); gd = rt("gd", [128, 16, 4])
                gsum = rt("gsum", [128, 16]); gw = rt("gw", [128, 16]); pen = rt("pen", [128, 16, 4])
                EL = rt("EL", [128, 16, 16]); EL2 = rt("EL2", [128, 16, 16]); m1 = rt("m1", [128, 16]); m2 = rt("m2", [128, 16])
                oh1 = rt("oh1", [128, 16, 16]); oh2 = rt("oh2", [128, 16, 16]); dd = rt("dd", [128, 16]); w1 = rt("w1", [128, 16]); w2 = rt("w2", [128, 16])
                LG = Lall[:, :, 0:4]
                LE4 = Lall[:, :, 4:20].rearrange("p t (g e) -> p t g e", g=4)
                EL4 = EL[:].rearrange("p t (g e) -> p t g e", g=4)

                def vop(fn, first=False):
                    S.op("vector", fn, reads=(TL + [TR]) if first else [TR], writes=[TR])

                def bc3(a, n):
                    return a[:].unsqueeze(2).broadcast_to([128, 16, n])
                vop(lambda e: e.tensor_reduce(out=gmax[:], in_=LG, axis=AX.X, op=ALU.max), first=True)
                vop(lambda e: e.tensor_tensor(out=gm[:], in0=LG, in1=bc3(gmax, 4), op=ALU.is_equal))
                vop(lambda e: e.tensor_tensor(out=gd[:], in0=LG, in1=bc3(gmax, 4), op=ALU.subtract))
                S.op("scalar", lambda e: e.activation(out=gd[:], in_=gd[:], func=AF.Exp), reads=[TR], writes=[TR])
                vop(lambda e: e.tensor_reduce(out=gsum[:], in_=gd[:], axis=AX.X, op=ALU.add))
                vop(lambda e: e.reciprocal(out=gw[:], in_=gsum[:]))
                vop(lambda e: e.tensor_scalar(out=pen[:], in0=gm[:], scalar1=1.0, scalar2=1e30, op0=ALU.subtract, op1=ALU.mult))
                vop(lambda e: e.tensor_tensor(out=EL4, in0=LE4, in1=gm[:].unsqueeze(3).broadcast_to([128, 16, 4, 4]), op=ALU.mult))
                vop(lambda e: e.tensor_tensor(out=EL4, in0=EL4, in1=pen[:].unsqueeze(3).broadcast_to([128, 16, 4, 4]), op=ALU.add))
                vop(lambda e: e.tensor_reduce(out=m1[:], in_=EL[:], axis=AX.X, op=ALU.max))
                vop(lambda e: e.tensor_tensor(out=oh1[:], in0=EL[:], in1=bc3(m1, 16), op=ALU.is_equal))
                vop(lambda e: e.scalar_tensor_tensor(out=EL2[:], in0=oh1[:], scalar=-1e30, in1=EL[:], op0=ALU.mult, op1=ALU.add))
                vop(lambda e: e.tensor_reduce(out=m2[:], in_=EL2[:], axis=AX.X, op=ALU.max))
                vop(lambda e: e.tensor_tensor(out=oh2[:], in0=EL2[:], in1=bc3(m2, 16), op=ALU.is_equal))
                vop(lambda e: e.tensor_tensor(out=dd[:], in0=m2[:], in1=m1[:], op=ALU.subtract))
                S.op("scalar", lambda e: e.activation(out=dd[:], in_=dd[:], func=AF.Exp), reads=[TR], writes=[TR])
                vop(lambda e: e.tensor_scalar(out=w1[:], in0=dd[:], scalar1=1.0, scalar2=None, op0=ALU.add))
                vop(lambda e: e.reciprocal(out=w1[:], in_=w1[:]))
                vop(lambda e: e.tensor_tensor(out=w1[:], in0=w1[:], in1=gw[:], op=ALU.mult))
                vop(lambda e: e.tensor_tensor(out=w2[:], in0=dd[:], in1=w1[:], op=ALU.mult))
                if moe == "sparse":
                    Mb = sb(c1, "Mb", [128, 16, 16], BF16)
                    ustrict = sb(c1, "ustrict", [128, 128], BF16); Tus = S.tile()
                    S.dma("sync", ustrict[:], ustrict_d, writes=[Tus])
                    onesb = sb(c1, "onesb", [128, 128], BF16); Tob_ = S.tile()
                    S.dma("sync", onesb[:], onesb_d, writes=[Tob_])
                    ebase = sb(c1, "ebase", [128, 16, 16], F32); Teb = S.tile()
                    S.dma("sync", ebase[:].rearrange("p a b -> p (a b)"), ebase_d, writes=[Teb])
                    slotf = rt("slotf", [128, 16, 16]); okf = rt("okf", [128, 16, 16]); posf = rt("posf", [128, 2, 16])
                    vop(lambda e: e.tensor_tensor(out=Mb[:], in0=oh1[:], in1=oh2[:], op=ALU.add))
                    for t in range(16):
                        prs = [(onesb[:], Mb[:, tp, :]) for tp in range(t)] + [(ustrict[:], Mb[:, t, :])]
                        n_ = len(prs)
                        for k_, (l_, r_) in enumerate(prs):
                            S.op("tensor", lambda e, l_=l_, r_=r_, k_=k_, n_=n_, t=t: e.matmul(out=banks[0][:, t * 16:(t + 1) * 16], lhsT=l_, rhs=r_,
                                                                                         start=(k_ == 0), stop=(k_ == n_ - 1)),
                                 reads=[TR, Tus, Tob_], writes=[Tb[0]], signal=(k_ == n_ - 1))
                    for tp in range(16):
                        S.op("tensor", lambda e, tp=tp: e.matmul(out=banks[1][:, 0:16], lhsT=onesb[:], rhs=Mb[:, tp, :], start=(tp == 0), stop=(tp == 15)),
                             reads=[TR, Tob_], writes=[Tb[1]], signal=(tp == 15))
                    cmax = rt("cmax", [128, 1])
                    S.op("vector", lambda e: e.tensor_reduce(out=cmax[:], in_=banks[1][:, 0:16], axis=AX.X, op=ALU.max), reads=[Tb[1], TR], writes=[TR])
                    import os as _os
                    thr = -1.0 if _os.environ.get("FORCE_DENSE") else float(CAP)
                    vop(lambda e: e.tensor_scalar(out=cmax[:], in0=cmax[:], scalar1=thr, scalar2=None, op0=ALU.is_gt))
                    S.op("vector", lambda e: e.tensor_copy(out=ovf[:], in_=cmax[:]), reads=[TR], writes=[Tovf])
                    rank = banks[0][:, 0:256].rearrange("p (a b) -> p a b", a=16)
                    S.op("vector", lambda e: e.tensor_tensor(out=slotf[:], in0=rank, in1=ebase[:], op=ALU.add), reads=[Tb[0], Teb, TR], writes=[TR])
                    vop(lambda e: e.tensor_scalar(out=okf[:], in0=slotf[:], scalar1=None, scalar2=None, op0=ALU.bypass) if False else
                        e.tensor_tensor(out=okf[:], in0=slotf[:], in1=ebase[:], op=ALU.subtract))
                    vop(lambda e: e.tensor_scalar(out=okf[:], in0=okf[:], scalar1=float(CAP), scalar2=1.0e6, op0=ALU.is_ge, op1=ALU.mult))
                    vop(lambda e: e.tensor_tensor(out=slotf[:], in0=slotf[:], in1=okf[:], op=ALU.add))
                    vop(lambda e: e.tensor_tensor(out=okf[:], in0=slotf[:], in1=oh1[:], op=ALU.mult))
                    vop(lambda e: e.tensor_reduce(out=posf[:, 0, :], in_=okf[:], axis=AX.X, op=ALU.add))
                    vop(lambda e: e.tensor_tensor(out=okf[:], in0=slotf[:], in1=oh2[:], op=ALU.mult))
                    vop(lambda e: e.tensor_reduce(out=posf[:, 1, :], in_=okf[:], axis=AX.X, op=ALU.add))
                    S.op("vector", lambda e: e.tensor_copy(out=pos[:], in_=posf[:]), reads=[TR], writes=[Tpos])
                    S.op("vector", lambda e: e.tensor_copy(out=w12[:, 0, :], in_=w1[:]), reads=[TR, Tw12], writes=[Tw12])
                    S.op("vector", lambda e: e.tensor_copy(out=w12[:, 1, :], in_=w2[:]), reads=[TR, Tw12], writes=[Tw12])
                    Tsc = [S.tile() for _ in range(32)]
                    set_bcreg()
                    for t in range(16):
                        for k_ in range(2):
                            S.dma_fn("gpsimd", lambda e, t=t, k_=k_: e.indirect_dma_start(
                                out=xs_d[:, :], out_offset=bass.IndirectOffsetOnAxis(ap=pos[:, k_, t:t + 1], axis=0),
                                in_=hmb[:, t, :], in_offset=None, bounds_check=bcreg, oob_is_err=False),
                                reads=[Thmb[t], Tpos, Txs], writes=[Tsc[2 * t + k_]], semtile=Thmb[t])
                vop(lambda e: e.tensor_tensor(out=oh1[:], in0=oh1[:], in1=bc3(w1, 16), op=ALU.mult))
                vop(lambda e: e.tensor_tensor(out=oh2[:], in0=oh2[:], in1=bc3(w2, 16), op=ALU.mult))
                S.op("vector", lambda e: e.tensor_tensor(out=comb[:], in0=oh1[:], in1=oh2[:], op=ALU.add), reads=[TR], writes=[Tcomb])
                S.barrier()
                if "comb" in debug:
                    dbg_out("comb", [128, 256])
                    S.dma("sync", dbg["comb"], comb[:].rearrange("p a b -> p (a b)"), reads=[Tcomb], writes=[Tdbg], semtile=Tdbg)
                    S.barrier()
                S.emit()
            if stop_after == "C1":
                return nc

            with ExitStack() as c2:
                wgb = [sb(c2, "wgb%d" % i, [128, 8, 512], BF16) for i in range(2)]; Twg4 = [[S.tile() for _ in range(4)] for _ in range(2)]
                wub = [sb(c2, "wub%d" % i, [128, 8, 512], BF16) for i in range(2)]; Twu4 = [[S.tile() for _ in range(4)] for _ in range(2)]
                wdb = [sb(c2, "wdb%d" % i, [128, 4, 1024], BF16) for i in range(2)]; Twd4 = [[S.tile() for _ in range(4)] for _ in range(2)]
                stg = [sb(c2, "stg%d" % i, [128, 1024], F32) for i in range(3)]; Tstg = [S.tile() for _ in range(3)]
                sq_ = [0]
                aT = [sb(c2, "aT%d" % i, [128, 4, 512], BF16) for i in range(2)]; TaT = [[S.tile() for _ in range(4)] for _ in range(2)]
                sg = [sb(c2, "sg%d" % i, [128, 512], F32) for i in range(2)]; Tsg = [S.tile() for _ in range(2)]
                xg = [sb(c2, "xg%d" % i, [128, 1024], BF16) for i in range(2)]; Txg = [S.tile() for _ in range(2)]
                xgT = [sb(c2, "xgT%d" % i, [128, 8, CAP], BF16) for i in range(2)]; TxgT = [[S.tile() for _ in range(CAP // 128)] for _ in range(2)]
                ysb = [sb(c2, "ysb%d" % i, [128, 1024], F32) for i in range(2)]; Tysb = [S.tile() for _ in range(2)]
                NJ = CAP // 128
                Tys = [S.tile() for _ in range(NEXP * NJ)]

                def load_w(ex):
                    b2 = ex % 2
                    for k in range(12):
                        si = sq_[0] % 3
                        sq_[0] += 1
                        if k < 8:
                            srcw = (wg_d if k < 4 else wu_d)[ex]
                            kk = k % 4
                            src = srcw[kk * 256:(kk + 1) * 256, :].rearrange("(c p) n -> p c n", p=128)
                            dstb = (wgb if k < 4 else wub)[b2][:, 2 * kk:2 * kk + 2, :]
                            Td = (Twg4 if k < 4 else Twu4)[b2][kk]
                            sv = stg[si][:].rearrange("p (c n) -> p c n", c=2)
                        else:
                            kk = k - 8
                            src = wd_d[ex][kk * 128:(kk + 1) * 128, :]
                            dstb = wdb[b2][:, kk, :]
                            Td = Twd4[b2][kk]
                            sv = stg[si][:]
                        S.dma("sync", sv, src, writes=[Tstg[si]])
                        eng = ("vector", "scalar", "gpsimd")[k % 3]
                        if eng == "scalar":
                            S.op("scalar", lambda e, dstb=dstb, sv=sv: e.activation(out=dstb, in_=sv, func=AF.Copy), reads=[Tstg[si]], writes=[Td])
                        else:
                            S.op(eng, lambda e, dstb=dstb, sv=sv: e.tensor_copy(out=dstb, in_=sv), reads=[Tstg[si]], writes=[Td])

                def gate_up(b2, rhs_of, Trhs, width, gq):
                    for ft in range(4):
                        bg, bu = (0, 1) if gq[0] % 2 == 0 else (2, 3)
                        s2 = gq[0] % 2
                        gq[0] += 1
                        mmgroup(banks[bg][:, 0:width], [(wgb[b2][:, c, ft * 128:(ft + 1) * 128], rhs_of(c)) for c in range(8)],
                                reads=Trhs + Twg4[b2], writes=[Tb[bg]])
                        mmgroup(banks[bu][:, 0:width], [(wub[b2][:, c, ft * 128:(ft + 1) * 128], rhs_of(c)) for c in range(8)],
                                reads=Trhs + Twu4[b2], writes=[Tb[bu]])
                        S.op("scalar", lambda e, bg=bg, s2=s2: e.activation(out=sg[s2][:, 0:width], in_=banks[bg][:, 0:width], func=AF.Silu),
                             reads=[Tb[bg]], writes=[Tsg[s2]])
                        S.op("vector", lambda e, bu=bu, s2=s2, ft=ft: e.tensor_tensor(out=aT[b2][:, ft, 0:width], in0=sg[s2][:, 0:width], in1=banks[bu][:, 0:width], op=ALU.mult),
                             reads=[Tsg[s2], Tb[bu]], writes=[TaT[b2][ft]])

                S.branch_begin()
                gq = [0]; yq = [0]; xq = [0]
                load_w(0)
                for ex in range(NEXP):
                    b2 = ex % 2
                    for j in range(NJ):
                        xi = xq[0] % 2
                        xq[0] += 1
                        r0 = ex * CAP + j * 128
                        S.dma("sync", xg[xi][:], xs_d[r0:r0 + 128, :], reads=Tsc + [Txs], writes=[Txg[xi]])
                        bi = 6 + (xq[0] % 2)
                        pTv = banks[bi][:].bitcast(BF16)
                        for c in range(8):
                            S.op("tensor", lambda e, c=c, xi=xi, pTv=pTv: e.transpose(
                                out=pTv[:, c * 128:(c + 1) * 128], in_=xg[xi][:, c * 128:(c + 1) * 128], identity=identb[:]),
                                reads=[Txg[xi], Tidb], writes=[Tb[bi]], signal=(c == 7))
                        S.op("vector", lambda e, j=j, b2=b2, pTv=pTv: e.tensor_copy(
                            out=xgT[b2][:, :, j * 128:(j + 1) * 128], in_=pTv.rearrange("p (c t) -> p c t", c=8)),
                            reads=[Tb[bi]], writes=[TxgT[b2][j]])
                    if ex + 1 < NEXP:
                        load_w(ex + 1)
                    gate_up(b2, lambda c, b2=b2: xgT[b2][:, c, :], TxgT[b2], CAP, gq)
                    for j in range(NJ):
                        y2 = yq[0] % 2
                        yq[0] += 1
                        for hf in range(2):
                            by = 4 + hf
                            mmgroup(banks[by][:], [(aT[b2][:, ft, j * 128:(j + 1) * 128], wdb[b2][:, ft, hf * 512:(hf + 1) * 512]) for ft in range(4)],
                                    reads=TaT[b2] + Twd4[b2], writes=[Tb[by]])
                            if hf == 0:
                                S.op("vector", lambda e, y2=y2, by=by: e.tensor_copy(out=ysb[y2][:, 0:512], in_=banks[by][:]),
                                     reads=[Tb[by]], writes=[Tysb[y2]])
                            else:
                                S.op("scalar", lambda e, y2=y2, by=by: e.activation(out=ysb[y2][:, 512:1024], in_=banks[by][:], func=AF.Copy),
                                     reads=[Tb[by], Tysb[y2]], writes=[Tysb[y2]])
                        r0 = ex * CAP + j * 128
                        S.dma("sync", ys_d[r0:r0 + 128, :], ysb[y2][:], reads=[Tysb[y2]], writes=[Tys[ex * NJ + j]], semtile=Tysb[y2])
                S.barrier()
                ygl = []
                for wb in wgb + wub + wdb:
                    v = wb[:].rearrange("p a b -> p (a b)").bitcast(F32)
                    ygl += [v[:, 0:1024], v[:, 1024:2048]]
                Tyg = [S.tile() for _ in ygl]
                set_bcreg()
                def gath(i):
                    t, k_ = i // 2, i % 2
                    gi = i % len(ygl)
                    S.dma_fn("gpsimd", lambda e, t=t, k_=k_, gi=gi: e.indirect_dma_start(
                        out=ygl[gi], out_offset=None, in_=ys_d[:, :],
                        in_offset=bass.IndirectOffsetOnAxis(ap=pos[:, k_, t:t + 1], axis=0),
                        bounds_check=bcreg, oob_is_err=False),
                        reads=Tys + [Tpos], writes=[Tyg[gi]], semtile=Tyg[gi])

                def acc(i):
                    t, k_ = i // 2, i % 2
                    gi = i % len(ygl)
                    for hf in range(2):
                        S.op("vector", lambda e, t=t, k_=k_, gi=gi, hf=hf: e.scalar_tensor_tensor(
                            out=x2[:, t, hf * 512:(hf + 1) * 512], in0=ygl[gi][:, hf * 512:(hf + 1) * 512], scalar=w12[:, k_, t:t + 1],
                            in1=x2[:, t, hf * 512:(hf + 1) * 512], op0=ALU.mult, op1=ALU.add),
                            reads=[Tyg[gi], Tw12, Tx2[t][hf]], writes=[Tx2[t][hf]])
                depth = len(ygl) - 1
                for i in range(32 + depth):
                    if i < 32:
                        gath(i)
                    if i - depth >= 0:
                        acc(i - depth)
                S.branch_mid()
                gq = [0]; yq = [0]
                load_w(0)
                for ex in range(NEXP):
                    b2 = ex % 2
                    if ex + 1 < NEXP:
                        load_w(ex + 1)
                    for tb in range(4):
                        gate_up(b2, lambda c, tb=tb: hmT[:, c, tb * 512:(tb + 1) * 512], ThmT[tb * 4:tb * 4 + 4], 512, gq)
                        for tt in range(4):
                            t = tb * 4 + tt
                            for hf in range(2):
                                by = 4 + (yq[0] % 4)
                                yq[0] += 1
                                mmgroup(banks[by][:], [(aT[b2][:, ft, tt * 128:(tt + 1) * 128], wdb[b2][:, ft, hf * 512:(hf + 1) * 512]) for ft in range(4)],
                                        reads=TaT[b2] + Twd4[b2], writes=[Tb[by]])
                                S.op("vector", lambda e, t=t, hf=hf, by=by, ex=ex: e.scalar_tensor_tensor(
                                    out=x2[:, t, hf * 512:(hf + 1) * 512], in0=banks[by][:], scalar=comb[:, t, ex:ex + 1],
                                    in1=x2[:, t, hf * 512:(hf + 1) * 512], op0=ALU.mult, op1=ALU.add),
                                    reads=[Tb[by], Tcomb, Tx2[t][hf]], writes=[Tx2[t][hf]])
                S.branch_end(ovf[0:1, 0:1], brregs)
                S.emit()

            with ExitStack() as c3:
                gbn = sb(c3, "gbn", [128, 1024], F32); Tgbn = S.tile()
                bcast_load(gbn[:], g_fin, Tgbn, 1024)
                ob = [sb(c3, "ob%d" % i, [128, 1024], F32) for i in range(2)]; Tob = [S.tile() for _ in range(2)]
                Tout = S.tile()
                for t in range(16):
                    S.op("scalar", lambda e, t=t: e.activation(out=junk[:], in_=x2[:, t, :], func=AF.Square, accum_out=ssc[:, t:t + 1]),
                         reads=Tx2[t] + [Tsscall], writes=[Tssc[t]])
                S.op("scalar", lambda e: e.activation(out=ssc[:], in_=ssc[:], func=AF.Sqrt, scale=1.0 / 1024, bias=EPS),
                     reads=Tssc, writes=[Tsscall])
                S.op("vector", lambda e: e.reciprocal(out=ssc[:], in_=ssc[:]), reads=[Tsscall], writes=[Tsscall])
                for t in range(16):
                    S.op("vector", lambda e, t=t: e.scalar_tensor_tensor(
                        out=ob[t % 2][:], in0=x2[:, t, :], scalar=ssc[:, t:t + 1], in1=gbn[:], op0=ALU.mult, op1=ALU.mult),
                        reads=Tx2[t] + [Tsscall, Tgbn], writes=[Tob[t % 2]])
                    S.dma("sync", out[t * 128:(t + 1) * 128, :], ob[t % 2][:], reads=[Tob[t % 2]], writes=[Tout], semtile=Tob[t % 2])
                S.barrier()
                S.emit()
    return nc


def _const_tables():
    f32 = np.float32
    inv_freq = (f32(1.0) / (f32(10000.0) ** (np.arange(0, 64, 2, dtype=f32) / f32(64)))).astype(f32)
    pos = np.arange(4096, dtype=f32)
    ang = (pos[:, None] * inv_freq[None, :]).astype(f32)
    cos = np.cos(ang).astype(f32)
    sin = np.sin(ang).astype(f32)
    r = np.arange(128)
    dh = r % 64
    cosT = cos[:, dh % 32].T.copy()
    sgn = np.where(dh < 32, -1.0, 1.0).astype(f32)
    sinT = (sin[:, dh % 32].T * sgn[:, None]).astype(f32)
    return cosT, sinT


def _masks(hf):
    k = np.arange(128)[:, None, None]
    r = np.arange(4)[None, :, None]
    q = np.arange(256)[None, None, :]
    md = np.zeros((128, 2, 4, 256), np.float32)
    mf = np.zeros((128, 2, 4, 256), np.float32)
    for par in range(2):
        if par == 0:
            kb = r
            j = 0 if hf == 0 else 1
        else:
            kb = 4 + r
            j = 3 if hf == 0 else 2
        s = kb * 128 + k
        t = j * 256 + q
        mf[:, par] = np.where(s <= t, 3e38, 0.0)
        md[:, par] = np.where((s // 64) <= (t // 64), 3e38, 0.0)
    return (md.reshape(128, 2, 1024).astype(ml_dtypes.bfloat16),
            mf.reshape(128, 2, 1024).astype(ml_dtypes.bfloat16))


def own_tokens(hf):
    return np.concatenate([np.arange(j * 256, (j + 1) * 256) for j in own_qtiles(hf)])


def prep(inputs):
    f32 = np.float32
    x = np.asarray(inputs["x"], f32)
    w_in = np.ascontiguousarray(np.asarray(inputs["w_in"], f32)[0])

    def swap_cols(w):
        return np.ascontiguousarray(w.reshape(1024, 8, 2, 32)[:, :, ::-1, :].reshape(1024, 512))

    cosT, sinT = _const_tables()
    common = {
        "w_in": w_in,
        "wqs": swap_cols(w_in[:, 0:512]),
        "wks": swap_cols(w_in[:, 512:1024]),
        "cosk": cosT, "sink": sinT,
        "identb": np.eye(128, dtype=f32).astype(ml_dtypes.bfloat16),
        "identf": np.eye(128, dtype=f32),
        "tri": np.triu(np.ones((128, 128), f32)),
        "onesf": np.ones((128, 128), f32),
        "onesb": np.ones((128, 128), f32).astype(ml_dtypes.bfloat16),
        "ustrict": np.triu(np.ones((128, 128), f32), 1).astype(ml_dtypes.bfloat16),
        "ebase": np.ascontiguousarray(np.broadcast_to((np.arange(16, dtype=f32) * CAP)[None, None, :], (128, 16, 16)).reshape(128, 256)),
        "g_attn": np.asarray(inputs["norm_attn_g"], f32).reshape(1, 1024),
        "g_ffn": np.asarray(inputs["norm_ffn_g"], f32).reshape(1, 1024),
        "g_fin": np.asarray(inputs["norm_final_g"], f32).reshape(1, 1024),
        "bfor": np.asarray(inputs["b_forget"], f32).reshape(1, 8),
        "lamv": np.concatenate([np.asarray(inputs[k], f32).reshape(1, 64) for k in
                                ("lambda_q1", "lambda_k1", "lambda_q2", "lambda_k2")], axis=1),
        "dng": np.asarray(inputs["diff_norm_g"], f32).reshape(1, 128),
        "w_out": np.ascontiguousarray(np.asarray(inputs["w_out"], f32)[0]),
        "rw": np.ascontiguousarray(np.concatenate([np.asarray(inputs["router_group_w"], f32)[0],
                                                   np.asarray(inputs["router_expert_w"], f32)[0]], axis=1)),
        "rb": np.concatenate([np.asarray(inputs["router_group_b"], f32).reshape(1, 4),
                              np.asarray(inputs["router_expert_b"], f32).reshape(1, 16)], axis=1),
        "wg": np.ascontiguousarray(np.asarray(inputs["w_gate"], f32)[0]),
        "wu": np.ascontiguousarray(np.asarray(inputs["w_up"], f32)[0]),
        "wd": np.ascontiguousarray(np.asarray(inputs["w_down"], f32)[0]),
    }
    in_maps = []
    for c in range(8):
        b, hf = c // 2, c % 2
        tok = own_tokens(hf)
        md, mf = _masks(hf)
        m = dict(common)
        m["xb"] = np.ascontiguousarray(x[b])
        m["xo"] = np.ascontiguousarray(x[b][tok])
        m["cosq"] = np.ascontiguousarray(cosT[:, tok] * f32(0.125))
        m["sinq"] = np.ascontiguousarray(sinT[:, tok] * f32(0.125))
        cs = np.zeros((8, 33), f32)
        for i, j in enumerate(own_qtiles(hf)):
            cs[i, 2 * j + 1] = 1.0
        m["csel"] = cs.reshape(1, 8 * 33)
        m["maskd"] = md
        m["maskf"] = mf
        in_maps.append(m)
    return in_maps


def kernel(**inputs):
    in_maps = prep(inputs)
    nc = build()
    res = run_bass_kernel_spmd(nc, in_maps, core_ids=list(range(8)))
    out = np.zeros((4, 4096, 1024), np.float32)
    for c in range(8):
        b, hf = c // 2, c % 2
        out[b, own_tokens(hf)] = res.results[c]["out"]
    return out
```

```python
import numpy as np
import ml_dtypes
from contextlib import ExitStack
import concourse.bass as bass
import concourse.mybir as mybir
from concourse.bass_utils import run_bass_kernel_spmd

F32 = mybir.dt.float32
BF16 = mybir.dt.bfloat16
I32 = mybir.dt.int32
AF = mybir.ActivationFunctionType
ALU = mybir.AluOpType
AX = mybir.AxisListType

ENGS = ("sync", "scalar", "vector", "gpsimd", "tensor")
EPS = 1e-6
LAM_INIT = 0.8 - 0.6 * 1.0
NEXP = 16
CAP = 512
NSLOT = NEXP * CAP


class Tile:
    __slots__ = ("name", "last_w", "readers", "dsem")

    def __init__(self, name):
        self.name = name
        self.last_w = None
        self.readers = {}
        self.dsem = None


class Sched:
    def __init__(self, nc, es):
        self.nc = nc
        self.es = es
        self.ops = {e: [] for e in ENGS}
        self.sems = {}
        self.cnt = {}
        self.waited = {e: {} for e in ENGS}
        self.pending = {e: False for e in ENGS}
        for e in ENGS:
            self._mksem("E:" + e)
        self.n_dsem = 0
        self.nops = 0
        self.tiles = []

    def _mksem(self, key):
        self.sems[key] = self.es.enter_context(self.nc.semaphore(key.replace(":", "_")))
        self.cnt[key] = 0

    def tile(self, name="t"):
        t = Tile(name)
        self.tiles.append(t)
        return t

    def _snapshot(self):
        return (dict(self.cnt), {e: dict(w) for e, w in self.waited.items()},
                [(t, t.last_w, dict(t.readers)) for t in self.tiles])

    def _restore(self, snap):
        self.cnt = dict(snap[0])
        for k in self.sems:
            self.cnt.setdefault(k, 0)
        self.waited = {e: dict(w) for e, w in snap[1].items()}
        for t, lw, rd in snap[2]:
            t.last_w = lw
            t.readers = dict(rd)

    def branch_begin(self):
        self.barrier()
        self._outer_ops = self.ops
        self.ops = {e: [] for e in ENGS}
        self._snap = self._snapshot()

    def branch_mid(self):
        self.barrier()
        self._A = (self.ops, dict(self.cnt))
        self.ops = {e: [] for e in ENGS}
        self._restore(self._snap)

    def branch_end(self, flag_ap, regs):
        self.barrier()
        opsA, cntA = self._A
        opsB, cntB = self.ops, dict(self.cnt)
        target = {k: max(cntA.get(k, 0), cntB.get(k, 0)) for k in set(cntA) | set(cntB)}

        def pads(cntX):
            out = {e: [] for e in ENGS}
            for k, v in target.items():
                d = v - cntX.get(k, 0)
                if d > 0:
                    owner = k[2:] if k.startswith("E:") else "gpsimd"
                    out[owner].append((k, d))
            return out
        pA, pB = pads(cntA), pads(cntB)
        self.ops = self._outer_ops
        for e in ENGS:
            self.ops[e].append(("branch", flag_ap, regs[e], opsA[e], pA[e], opsB[e], pB[e]))
        self.cnt = target
        for e in ENGS:
            self.waited[e] = dict(target)
        for t in self.tiles:
            t.last_w = None
            t.readers = {}

    def dsem_for(self, t):
        if t.dsem is None:
            key = "D:%d" % self.n_dsem
            self.n_dsem += 1
            self._mksem(key)
            t.dsem = key
        return t.dsem

    def _need(self, eng, waits, key, val):
        if eng == "tensor" and key == "E:tensor":
            return
        if self.cnt[key] < val:
            raise RuntimeError("wait on un-signalled event %s %d (cnt %d) from %s" % (key, val, self.cnt[key], eng))
        if self.waited[eng].get(key, 0) >= val:
            return
        self.waited[eng][key] = val
        waits[key] = max(waits.get(key, 0), val)

    def _deps(self, eng, reads, writes):
        waits = {}
        for t in reads:
            if t.last_w is not None:
                self._need(eng, waits, *t.last_w)
        for t in writes:
            if t.last_w is not None:
                self._need(eng, waits, *t.last_w)
            for k, v in t.readers.items():
                self._need(eng, waits, k, v)
        return list(waits.items())

    def _record(self, ev, reads, writes):
        for t in writes:
            t.last_w = ev
            t.readers = {}
        for t in reads:
            if t not in writes:
                if t.readers.get(ev[0], 0) < ev[1]:
                    t.readers[ev[0]] = ev[1]

    def op(self, eng, fn, reads=(), writes=(), signal=True):
        waits = self._deps(eng, reads, writes)
        key = "E:" + eng
        if signal:
            self.cnt[key] += 1
            ev = (key, self.cnt[key])
            inc = (key, 1)
            self.pending[eng] = False
        else:
            ev = (key, self.cnt[key] + 1)
            inc = None
            self.pending[eng] = True
        self._record(ev, reads, writes)
        self.ops[eng].append((waits, fn, inc))
        self.nops += 1

    def dma(self, eng, out, in_, reads=(), writes=(), semtile=None, **kw):
        waits = self._deps(eng, reads, writes)
        if semtile is None:
            semtile = writes[0] if writes else reads[0]
        key = self.dsem_for(semtile)
        self.cnt[key] += 16
        ev = (key, self.cnt[key])
        self._record(ev, reads, writes)

        def fn(e, out=out, in_=in_, kw=kw):
            return e.dma_start(out=out, in_=in_, **kw)
        self.ops[eng].append((waits, fn, (key, 16)))
        self.nops += 1

    def dma_fn(self, eng, fn, reads=(), writes=(), semtile=None):
        waits = self._deps(eng, reads, writes)
        key = self.dsem_for(semtile)
        self.cnt[key] += 16
        ev = (key, self.cnt[key])
        self._record(ev, reads, writes)
        self.ops[eng].append((waits, fn, (key, 16)))
        self.nops += 1

    def barrier(self, engs=ENGS):
        for e in ENGS:
            assert not self.pending[e], e
        for e in engs:
            waits = {}
            for key, c in self.cnt.items():
                if c > 0:
                    self._need(e, waits, key, c)
            self.ops[e].append((list(waits.items()), None, None))

    def emit(self):
        nc = self.nc
        sems = self.sems
        ops = self.ops
        with nc.Block() as block:
            def replay(e, lst):
                for ent in lst:
                    if ent[0] == "branch":
                        _, flag_ap, reg, oA, pA, oB, pB = ent
                        e.reg_load(reg, flag_ap)
                        with e.If_eq(reg, 0):
                            replay(e, oA)
                            for k, d in pA:
                                e.sem_inc(sems[k], d)
                            e.nop()
                        with e.Else():
                            replay(e, oB)
                            for k, d in pB:
                                e.sem_inc(sems[k], d)
                            e.nop()
                        continue
                    waits, fn, inc = ent
                    for key, val in waits:
                        e.wait_ge(sems[key], val)
                    if fn is None:
                        continue
                    inst = fn(e)
                    if inc is not None:
                        inst.then_inc(sems[inc[0]], inc[1])

            def mk(name):
                def body(e):
                    replay(e, ops[name])
                return body
            block.sync(mk("sync"))
            block.scalar(mk("scalar"))
            block.vector(mk("vector"))
            block.gpsimd(mk("gpsimd"))
            block.tensor(mk("tensor"))
        self.ops = {e: [] for e in ENGS}


def own_qtiles(hf):
    js = []
    for m in range(4):
        js += ([4 * m, 4 * m + 3] if hf == 0 else [4 * m + 1, 4 * m + 2])
    return js


def nk_of(i):
    return 8 * (i // 2) + (4 if i % 2 == 0 else 8)


def interleave(A, B):
    a, b = len(A), len(B)
    if a == 0:
        for f in B:
            f()
        return
    done = 0
    for k, f in enumerate(A):
        f()
        upto = ((k + 1) * b) // a
        while done < upto:
            B[done]()
            done += 1
    while done < b:
        B[done]()
        done += 1


def build(debug=(), stop_after=None, moe="sparse"):
    nc = bass.Bass("TRN2", target_bir_lowering=False)

    def din(name, shape, dt=F32):
        return nc.dram_tensor(name, list(shape), dt, kind="ExternalInput").ap()

    xb = din("xb", [4096, 1024])
    xo = din("xo", [2048, 1024])
    w_in = din("w_in", [1024, 3080])
    wqs_d = din("wqs", [1024, 512])
    wks_d = din("wks", [1024, 512])
    cosk = din("cosk", [128, 4096])
    sink = din("sink", [128, 4096])
    cosq = din("cosq", [128, 2048])
    sinq = din("sinq", [128, 2048])
    maskd_d = din("maskd", [128, 2, 1024], BF16)
    maskf_d = din("maskf", [128, 2, 1024], BF16)
    identb_d = din("identb", [128, 128], BF16)
    identf_d = din("identf", [128, 128])
    tri_d = din("tri", [128, 128])
    onesf_d = din("onesf", [128, 128])
    g_attn = din("g_attn", [1, 1024])
    g_ffn = din("g_ffn", [1, 1024])
    g_fin = din("g_fin", [1, 1024])
    bfor = din("bfor", [1, 8])
    lam_d = din("lamv", [1, 256])
    csel_d = din("csel", [1, 8 * 33])
    dng = din("dng", [1, 128])
    w_out = din("w_out", [1024, 1024])
    rw_d = din("rw", [1024, 20])
    rb_d = din("rb", [1, 20])
    wg_d = din("wg", [16, 1024, 512])
    wu_d = din("wu", [16, 1024, 512])
    wd_d = din("wd", [16, 512, 1024])
    ebase_d = din("ebase", [128, 256])
    ustrict_d = din("ustrict", [128, 128], BF16)
    onesb_d = din("onesb", [128, 128], BF16)
    xs_d = nc.dram_tensor("xs_scratch", [NSLOT, 1024], BF16).ap()
    ys_d = nc.dram_tensor("ys_scratch", [NSLOT, 1024], F32).ap()
    out = nc.dram_tensor("out", [2048, 1024], F32, kind="ExternalOutput").ap()
    dbg = {}

    def dbg_out(name, shape, dt=F32):
        if name in debug:
            dbg[name] = nc.dram_tensor("dbg_" + name, list(shape), dt, kind="ExternalOutput").ap()
            return dbg[name]
        return None

    with ExitStack() as es:
        S = Sched(nc, es)

        uniq = [0]

        def sb(sc, name, shape, dt):
            uniq[0] += 1
            return sc.enter_context(nc.sbuf_tensor("s%d_%s" % (uniq[0], name), list(shape), dt))

        banks = [es.enter_context(nc.psum_tensor("bank%d" % i, [128, 512], F32)) for i in range(8)]
        Tb = [S.tile("bank%d" % i) for i in range(8)]
        Tdbg = S.tile("dbg")

        def dump(name, src_ap, reads):
            if name in dbg:
                S.dma("sync", dbg[name], src_ap, reads=reads, writes=[Tdbg], semtile=Tdbg)

        def bcast_load(dst, src, T, n):
            S.dma("sync", dst, src.broadcast_to([128, n]), writes=[T])

        bcreg = es.enter_context(nc.gpsimd.register("bcreg"))
        brregs = {e: es.enter_context(getattr(nc, e).register("br_" + e)) for e in ENGS}

        def set_bcreg():
            S.ops["gpsimd"].append(([], lambda e: e.reg_mov(bcreg, NSLOT - 1), None))

        identb = sb(es, "identb", [128, 128], BF16); Tidb = S.tile("identb")
        S.dma("sync", identb[:], identb_d, writes=[Tidb])
        gb_attn = sb(es, "gb_attn", [128, 1024], F32); Tgba = S.tile("gba")
        bcast_load(gb_attn[:], g_attn, Tgba, 1024)
        OT = sb(es, "OT", [128, 8, 2048], BF16)
        TOT = [S.tile("OT%d" % q) for q in range(16)]

        def make_sweep(sc, pfx, pT_banks):
            st = {}
            st["xt"] = [sb(sc, pfx + "xt%d" % i, [128, 1024], F32) for i in range(4)]
            st["Txt"] = [S.tile() for _ in range(4)]
            st["hb"] = [sb(sc, pfx + "hb%d" % i, [128, 1024], BF16) for i in range(2)]
            st["Thb"] = [S.tile() for _ in range(2)]
            st["hT"] = [sb(sc, pfx + "hT%d" % i, [128, 8, 512], BF16) for i in range(3)]
            st["ThT"] = [[S.tile() for _ in range(4)] for _ in range(3)]
            st["junk"] = sb(sc, pfx + "junk", [128, 1024], BF16)
            st["ss"] = [sb(sc, pfx + "ss%d" % i, [128, 4], F32) for i in range(3)]
            st["Tss"] = [[S.tile() for _ in range(4)] for _ in range(3)]
            st["sq"] = [sb(sc, pfx + "sq%d" % i, [128, 4], F32) for i in range(3)]
            st["Tsq"] = [S.tile() for _ in range(3)]
            st["rs"] = [sb(sc, pfx + "rs%d" % i, [128, 4], F32) for i in range(3)]
            st["Trs"] = [S.tile() for _ in range(3)]
            st["pT"] = pT_banks
            return st

        def sweep_stage1(st, x_ap, blk, gb, Tgb):
            b2 = blk % 3
            for tt in range(4):
                n = blk * 4 + tt
                xt, Txt = st["xt"][n % 4], st["Txt"][n % 4]
                S.dma("sync", xt[:], x_ap[n * 128:(n + 1) * 128, :], writes=[Txt])
                S.op("scalar", lambda e, xt=xt, tt=tt: e.activation(out=st["junk"][:], in_=xt[:], func=AF.Square,
                                                                    accum_out=st["ss"][b2][:, tt:tt + 1]),
                     reads=[Txt], writes=[st["Tss"][b2][tt]])
            S.op("scalar", lambda e: e.activation(out=st["sq"][b2][:], in_=st["ss"][b2][:], func=AF.Sqrt,
                                                  scale=1.0 / 1024, bias=EPS),
                 reads=st["Tss"][b2], writes=[st["Tsq"][b2]])
            S.op("vector", lambda e: e.reciprocal(out=st["rs"][b2][:], in_=st["sq"][b2][:]),
                 reads=[st["Tsq"][b2]], writes=[st["Trs"][b2]])
            def stt(tt):
                n = blk * 4 + tt
                xt, Txt = st["xt"][n % 4], st["Txt"][n % 4]
                hb, Thb = st["hb"][n % 2], st["Thb"][n % 2]
                S.op("vector", lambda e, xt=xt, hb=hb, tt=tt: e.scalar_tensor_tensor(
                    out=hb[:], in0=xt[:], scalar=st["rs"][b2][:, tt:tt + 1], in1=gb[:], op0=ALU.mult, op1=ALU.mult),
                    reads=[Txt, st["Trs"][b2], Tgb], writes=[Thb])

            def tr(tt):
                n = blk * 4 + tt
                hb, Thb = st["hb"][n % 2], st["Thb"][n % 2]
                bi = st["pT"][n % 2]
                pTv = banks[bi][:].bitcast(BF16)
                for c in range(8):
                    S.op("tensor", lambda e, c=c, hb=hb, pTv=pTv: e.transpose(
                        out=pTv[:, c * 128:(c + 1) * 128], in_=hb[:, c * 128:(c + 1) * 128], identity=identb[:]),
                        reads=[Thb, Tidb], writes=[Tb[bi]], signal=(c == 7))

            def ev(tt):
                n = blk * 4 + tt
                bi = st["pT"][n % 2]
                pTv = banks[bi][:].bitcast(BF16)
                S.op("vector", lambda e, tt=tt, pTv=pTv: e.tensor_copy(
                    out=st["hT"][b2][:, :, tt * 128:(tt + 1) * 128], in_=pTv.rearrange("p (c t) -> p c t", c=8)),
                    reads=[Tb[bi]], writes=[st["ThT"][b2][tt]])
            for f, a in ((stt, 0), (stt, 1), (tr, 0), (ev, 0), (stt, 2), (tr, 1), (ev, 1), (stt, 3), (tr, 2), (ev, 2), (tr, 3), (ev, 3)):
                f(a)

        def wload(dst, src_cols, T):
            S.dma("gpsimd", dst, src_cols.rearrange("(c p) n -> p c n", p=128), writes=[T])

        def mmgroup(out_ap, pairs, reads, writes):
            n = len(pairs)
            for k, (l, r) in enumerate(pairs):
                S.op("tensor", lambda e, l=l, r=r, k=k: e.matmul(out=out_ap, lhsT=l, rhs=r, start=(k == 0), stop=(k == n - 1)),
                     reads=reads, writes=writes, signal=(k == n - 1))

        def run_attention(units, PT, TPT, KT_of, QT_of, V_of, W, exp_emit, evac_emit, mask_emit, s_banks, o_banks):
            gctr = [0]

            def st_list(ui, u):
                buf = ui % 2
                nk = nk_of(u["i"])
                L = []
                for p in range(nk // 2):
                    def f(p=p):
                        bi = s_banks[gctr[0] % len(s_banks)]
                        gctr[0] += 1
                        for j in range(2):
                            kb = 2 * p + j
                            kap, Tk = KT_of(u, kb)
                            qap, Tq = QT_of(u)
                            S.op("tensor", lambda e, kap=kap, qap=qap, j=j, bi=bi: e.matmul(
                                out=banks[bi][:, j * 256:(j + 1) * 256], lhsT=kap, rhs=qap, start=True, stop=True),
                                reads=[Tk, Tq], writes=[Tb[bi]], signal=(j == 1))
                        exp_emit(u, p, bi, PT[buf], TPT[buf][p])
                    L.append(f)
                L.append(lambda: mask_emit(u, PT[buf], TPT[buf]))
                return L

            def pv_list(ui, u):
                buf = ui % 2
                nk = nk_of(u["i"])
                ob = o_banks[ui % len(o_banks)]
                L = []
                for s in range(2):
                    for k0 in range(0, nk, 4):
                        def f(s=s, k0=k0):
                            for kb in range(k0, min(nk, k0 + 4)):
                                vap, Tv = V_of(u, kb)
                                S.op("tensor", lambda e, kb=kb, s=s, vap=vap: e.matmul(
                                    out=banks[ob][:, s * W:(s + 1) * W], lhsT=PT[buf][:, kb, s * 128:(s + 1) * 128],
                                    rhs=vap, start=(kb == 0), stop=(kb == nk - 1)),
                                    reads=[TPT[buf][kb // 2], Tv], writes=[Tb[ob]], signal=(kb == nk - 1))
                        L.append(f)
                L.append(lambda: evac_emit(u, ob))
                return L

            prev = None
            for ui in range(len(units) + 1):
                A = st_list(ui, units[ui]) if ui < len(units) else []
                B = pv_list(ui - 1, units[ui - 1]) if ui >= 1 else []
                interleave(A, B)

        with ExitStack() as pa:
            KT = sb(pa, "KTd", [128, 4, 4096], BF16)
            TKT = [[S.tile() for _ in range(8)] for _ in range(4)]
            QT = sb(pa, "QTd", [128, 4, 2048], BF16)
            TQT = [[S.tile() for _ in range(4)] for _ in range(4)]
            Vd = sb(pa, "Vd", [128, 32, 4, 130], BF16)
            TV = [S.tile() for _ in range(32)]
            Tvones = S.tile()
            S.op("vector", lambda e: e.memset(Vd[:, :, :, 128:130], 1.0), writes=TV)
            lamt = sb(pa, "lamt", [128, 256], F32); Tlam = S.tile()
            bcast_load(lamt[:], lam_d, Tlam, 256)
            lj = sb(pa, "lj", [128, 64], F32)
            ls = sb(pa, "ls", [128, 2], F32); Tls = S.tile()
            le = sb(pa, "le", [128, 2], F32); Tle = S.tile()
            neglam = sb(pa, "neglam", [128, 1], F32); Tnl = S.tile()
            for z in range(2):
                S.op("vector", lambda e, z=z: e.scalar_tensor_tensor(
                    out=lj[:], in0=lamt[:, z * 128:z * 128 + 64], scalar=1.0, in1=lamt[:, z * 128 + 64:z * 128 + 128],
                    op0=ALU.mult, op1=ALU.mult, accum_out=ls[:, z:z + 1]), reads=[Tlam, Tls], writes=[Tls])
            S.op("scalar", lambda e: e.activation(out=le[:], in_=ls[:], func=AF.Exp), reads=[Tls], writes=[Tle])
            S.op("vector", lambda e: e.tensor_tensor(out=neglam[:], in0=le[:, 1:2], in1=le[:, 0:1], op=ALU.subtract),
                 reads=[Tle], writes=[Tnl])
            S.op("vector", lambda e: e.tensor_scalar(out=neglam[:], in0=neglam[:], scalar1=-LAM_INIT, scalar2=None, op0=ALU.add),
                 reads=[Tnl], writes=[Tnl])
            gsc = sb(pa, "gsc", [128, 128], F32); Tgsc = S.tile()
            bcast_load(gsc[:], dng, Tgsc, 128)
            S.op("vector", lambda e: e.tensor_scalar(out=gsc[:], in0=gsc[:], scalar1=1.0 - LAM_INIT, scalar2=None, op0=ALU.mult),
                 reads=[Tgsc], writes=[Tgsc])
            Txs = S.tile("xs")

            with ExitStack() as sw:
                st = make_sweep(sw, "a", [0, 1])
                wA = sb(sw, "wA", [128, 8, 512], BF16); TwA = S.tile()
                wB = sb(sw, "wB", [128, 8, 512], BF16); TwB = S.tile()
                wC = sb(sw, "wC", [128, 8, 512], BF16); TwC = S.tile()
                wload(wA[:], w_in[:, 512:1024], TwA)
                wload(wB[:], wks_d, TwB)
                wload(wC[:], w_in[:, 1024:1536], TwC)
                ct = [sb(sw, "ct%d" % i, [128, 512], F32) for i in range(2)]; Tct = [S.tile() for _ in range(2)]
                sn = [sb(sw, "sn%d" % i, [128, 512], F32) for i in range(2)]; Tsn = [S.tile() for _ in range(2)]
                t1 = [sb(sw, "t1%d" % i, [128, 512], F32) for i in range(2)]; Tt1 = [S.tile() for _ in range(2)]
                t2 = [sb(sw, "t2%d" % i, [128, 512], F32) for i in range(2)]; Tt2 = [S.tile() for _ in range(2)]
                rctr = [0]

                def rope_proj(blk, hT, ThT, wq_, Tw_, ws_, Tws_, cos_d, sin_d, dstT, TdstT):
                    b2 = blk % 2
                    S.dma("sync", ct[b2][:], cos_d[:, blk * 512:(blk + 1) * 512], writes=[Tct[b2]])
                    S.dma("sync", sn[b2][:], sin_d[:, blk * 512:(blk + 1) * 512], writes=[Tsn[b2]])
                    for h in range(4):
                        ba, bb = (2, 3) if h % 2 == 0 else (4, 5)
                        mmgroup(banks[ba][:], [(wq_[:, c, h * 128:(h + 1) * 128], hT[:, c, :]) for c in range(8)],
                                reads=list(ThT) + [Tw_], writes=[Tb[ba]])
                        mmgroup(banks[bb][:], [(ws_[:, c, h * 128:(h + 1) * 128], hT[:, c, :]) for c in range(8)],
                                reads=list(ThT) + [Tws_], writes=[Tb[bb]])
                        r = rctr[0] % 2
                        rctr[0] += 1
                        S.op("vector", lambda e, r=r, ba=ba: e.tensor_tensor(out=t1[r][:], in0=banks[ba][:], in1=ct[b2][:], op=ALU.mult),
                             reads=[Tb[ba], Tct[b2]], writes=[Tt1[r]])
                        S.op("vector", lambda e, r=r, bb=bb: e.tensor_tensor(out=t2[r][:], in0=banks[bb][:], in1=sn[b2][:], op=ALU.mult),
                             reads=[Tb[bb], Tsn[b2]], writes=[Tt2[r]])
                        S.op("gpsimd", lambda e, r=r, h=h: e.tensor_tensor(out=dstT[:, h, blk * 512:(blk + 1) * 512], in0=t1[r][:], in1=t2[r][:], op=ALU.add),
                             reads=[Tt1[r], Tt2[r]], writes=[TdstT[h][blk]])

                def kv_proj(blk, hT, ThT):
                    rope_proj(blk, hT, ThT, wA, TwA, wB, TwB, cosk, sink, KT, TKT)
                    for tt in range(4):
                        n = blk * 4 + tt
                        bv = 6 + (tt % 2)
                        mmgroup(banks[bv][:], [(hT[:, c, tt * 128:(tt + 1) * 128], wC[:, c, :]) for c in range(8)],
                                reads=[ThT[tt], TwC], writes=[Tb[bv]])
                        S.op("scalar", lambda e, n=n, bv=bv: e.activation(
                            out=Vd[:, n, :, 0:128], in_=banks[bv][:].rearrange("p (h d) -> p h d", h=4), func=AF.Copy),
                            reads=[Tb[bv]], writes=[TV[n]])

                sweep_stage1(st, xb, 0, gb_attn, Tgba)
                sweep_stage1(st, xb, 1, gb_attn, Tgba)
                for blk in range(8):
                    if blk + 2 < 8:
                        sweep_stage1(st, xb, blk + 2, gb_attn, Tgba)
                    kv_proj(blk, st["hT"][blk % 3], st["ThT"][blk % 3])
                wload(wA[:], w_in[:, 0:512], TwA)
                wload(wB[:], wqs_d, TwB)
                sweep_stage1(st, xo, 0, gb_attn, Tgba)
                sweep_stage1(st, xo, 1, gb_attn, Tgba)
                for blk in range(4):
                    if blk + 2 < 4:
                        sweep_stage1(st, xo, blk + 2, gb_attn, Tgba)
                    rope_proj(blk, st["hT"][blk % 3], st["ThT"][blk % 3], wA, TwA, wB, TwB, cosq, sinq, QT, TQT)
                S.barrier()
                if "KTd" in debug:
                    dbg_out("KTd", [128, 4, 4096], BF16); dump("KTd", KT[:], [])
                    dbg_out("QTd", [128, 4, 2048], BF16); dump("QTd", QT[:], [])
                    dbg_out("Vd", [128, 32, 4, 130], BF16); dump("Vd", Vd[:], [])
                    S.barrier()
                S.emit()
            if stop_after == "Aproj":
                S.barrier(); S.emit()
                return nc

            with ExitStack() as at:
                if moe == "sparse":
                    zer = sb(at, "zer", [128, 2048], BF16); Tzer = S.tile()
                    S.op("vector", lambda e: e.memset(zer[:], 0.0), writes=[Tzer])
                    for n in range(NSLOT // 256):
                        S.dma("sync", xs_d[n * 256:(n + 1) * 256, :].rearrange("(p r) d -> p (r d)", r=2), zer[:], reads=[Tzer], writes=[Txs], semtile=Tzer)
                maskd = sb(at, "maskd", [128, 2, 1024], BF16); Tmd = S.tile()
                S.dma("sync", maskd[:], maskd_d, writes=[Tmd])
                PT = [sb(at, "PT%d" % i, [128, 32, 256], BF16) for i in range(2)]
                TPT = [[S.tile() for _ in range(16)] for _ in range(2)]
                oc = sb(at, "oc", [128, 16, 4, 128], F32); Toc = [[S.tile() for _ in range(4)] for _ in range(16)]
                ssq = sb(at, "ssq", [128, 64], F32); Tssq = S.tile()
                A1 = [sb(at, "A1%d" % i, [128, 128], F32) for i in range(2)]; TA1 = [S.tile() for _ in range(2)]
                rr = sb(at, "rr", [128, 4], F32); Trr = [S.tile() for _ in range(4)]
                sjunk = sb(at, "sjunk", [128, 128], F32)
                units = [dict(h=h, i=i, z=z) for h in range(4) for i in range(8) for z in range(2)]

                def KT_of(u, kb):
                    r0 = 64 * u["z"]
                    return KT[r0:r0 + 64, u["h"], kb * 128:(kb + 1) * 128], TKT[u["h"]][kb // 4]

                def QT_of(u):
                    r0 = 64 * u["z"]
                    return QT[r0:r0 + 64, u["h"], u["i"] * 256:(u["i"] + 1) * 256], TQT[u["h"]][u["i"] // 2]

                def V_of(u, kb):
                    return Vd[:, kb, u["h"], 0:129], TV[kb]

                def exp_emit(u, p, bi, PTb, Tp):
                    S.op("scalar", lambda e: e.activation(out=PTb[:, 2 * p:2 * p + 2, :].rearrange("p a b -> p (a b)"),
                                                          in_=banks[bi][:], func=AF.Exp),
                         reads=[Tb[bi]], writes=[Tp])

                def mask_emit(u, PTb, TPb):
                    i = u["i"]
                    lo = nk_of(i) - 4
                    S.op("vector", lambda e: e.tensor_tensor(out=PTb[:, lo:lo + 4, :], in0=PTb[:, lo:lo + 4, :],
                                                             in1=maskd[:, i % 2, :].rearrange("p (a b) -> p a b", a=4), op=ALU.min),
                         reads=[Tmd, TPb[lo // 2], TPb[lo // 2 + 1]], writes=[TPb[lo // 2], TPb[lo // 2 + 1]])

                def evac_emit(u, ob):
                    h, i, z = u["h"], u["i"], u["z"]
                    for s in range(2):
                        qb = 2 * i + s
                        o_ap = banks[ob][:, s * 129:s * 129 + 128]
                        sm_ap = banks[ob][:, s * 129 + 128:s * 129 + 129]
                        ri = 2 * z + s
                        S.op("vector", lambda e, ri=ri, sm_ap=sm_ap: e.reciprocal(out=rr[:, ri:ri + 1], in_=sm_ap),
                             reads=[Tb[ob]], writes=[Trr[ri]])
                        if z == 0:
                            S.op("vector", lambda e, ri=ri, o_ap=o_ap, s=s: e.tensor_scalar(
                                out=A1[s][:], in0=o_ap, scalar1=rr[:, ri:ri + 1], scalar2=None, op0=ALU.mult),
                                reads=[Tb[ob], Trr[ri]], writes=[TA1[s]])
                        else:
                            S.op("vector", lambda e, ri=ri: e.tensor_tensor(out=rr[:, ri:ri + 1], in0=rr[:, ri:ri + 1], in1=neglam[:], op=ALU.mult),
                                 reads=[Trr[ri], Tnl], writes=[Trr[ri]])
                            S.op("vector", lambda e, ri=ri, o_ap=o_ap, s=s, qb=qb: e.scalar_tensor_tensor(
                                out=oc[:, qb, h, :], in0=o_ap, scalar=rr[:, ri:ri + 1], in1=A1[s][:], op0=ALU.mult, op1=ALU.add),
                                reads=[Tb[ob], Trr[ri], TA1[s]], writes=[Toc[qb][h]])
                            S.op("vector", lambda e, qb=qb: e.scalar_tensor_tensor(
                                out=sjunk[:], in0=oc[:, qb, h, :], scalar=1.0, in1=oc[:, qb, h, :], op0=ALU.mult, op1=ALU.mult,
                                accum_out=ssq[:, qb * 4 + h:qb * 4 + h + 1]),
                                reads=[Toc[qb][h], Tssq], writes=[Tssq])

                run_attention(units, PT, TPT, KT_of, QT_of, V_of, 129, exp_emit, evac_emit, mask_emit,
                              s_banks=[0, 1, 2, 3], o_banks=[4, 5, 6, 7])
                S.op("scalar", lambda e: e.activation(out=ssq[:], in_=ssq[:], func=AF.Sqrt, scale=1.0 / 128, bias=EPS),
                     reads=[Tssq], writes=[Tssq])
                S.op("vector", lambda e: e.reciprocal(out=ssq[:], in_=ssq[:]), reads=[Tssq], writes=[Tssq])
                Otok = [sb(at, "Otok%d" % i, [128, 512], BF16) for i in range(2)]; TOtok = [S.tile() for _ in range(2)]
                for qb in range(16):
                    o2 = qb % 2
                    for h in range(4):
                        S.op("vector", lambda e, qb=qb, h=h, o2=o2: e.scalar_tensor_tensor(
                            out=Otok[o2][:, h * 128:(h + 1) * 128], in0=oc[:, qb, h, :], scalar=ssq[:, qb * 4 + h:qb * 4 + h + 1],
                            in1=gsc[:], op0=ALU.mult, op1=ALU.mult),
                            reads=[Toc[qb][h], Tssq, Tgsc], writes=[TOtok[o2]])
                    bi = o2
                    pTv = banks[bi][:].bitcast(BF16)
                    for c in range(4):
                        S.op("tensor", lambda e, c=c, o2=o2, pTv=pTv: e.transpose(
                            out=pTv[:, c * 128:(c + 1) * 128], in_=Otok[o2][:, c * 128:(c + 1) * 128], identity=identb[:]),
                            reads=[TOtok[o2], Tidb], writes=[Tb[bi]], signal=(c == 3))
                    S.op("vector", lambda e, qb=qb, pTv=pTv: e.tensor_copy(
                        out=OT[:, 0:4, qb * 128:(qb + 1) * 128], in_=pTv[:, 0:512].rearrange("p (c t) -> p c t", c=4)),
                        reads=[Tb[bi]], writes=[TOT[qb]])
                S.barrier()
                if "OTa" in debug:
                    dbg_out("OTa", [128, 8, 2048], BF16); dump("OTa", OT[:], []); S.barrier()
                S.emit()
        if stop_after == "A":
            return nc

        with ExitStack() as pb:
            KT = sb(pb, "KTf", [128, 4, 4096], BF16)
            TKT = [[S.tile() for _ in range(8)] for _ in range(4)]
            QT = sb(pb, "QTf", [128, 4, 2048], BF16)
            TQT = [[S.tile() for _ in range(4)] for _ in range(4)]
            Vf = sb(pb, "Vf", [128, 32, 8, 66], BF16)
            TV = [S.tile() for _ in range(32)]
            S.op("vector", lambda e: e.memset(Vf[:, :, :, 64:66], 1.0), writes=TV)
            zt = sb(pb, "zt", [128, 32, 8], F32); Tzt = [S.tile() for _ in range(32)]
            Fpos = sb(pb, "Fpos", [128, 32, 8], F32); TF = [S.tile() for _ in range(32)]
            Cpos = sb(pb, "Cpos", [128, 33, 8], F32); TC = [S.tile() for _ in range(33)]
            maskf = sb(pb, "maskf", [128, 2, 1024], BF16); Tmf = S.tile()
            S.dma("sync", maskf[:], maskf_d, writes=[Tmf])
            bfb = sb(pb, "bfb", [128, 8], F32); Tbfb = S.tile()
            bcast_load(bfb[:], bfor, Tbfb, 8)
            csel = sb(pb, "csel", [128, 8, 33], F32); Tcsel = S.tile()
            bcast_load(csel[:].rearrange("p a b -> p (a b)"), csel_d, Tcsel, 8 * 33)
            ctmp = sb(pb, "ctmp", [128, 8, 33], F32); Tctmp = S.tile()
            cq = sb(pb, "cq", [128, 8, 8], F32); Tcq = S.tile()
            tri = sb(pb, "tri", [128, 128], F32); Ttri = S.tile()
            S.dma("sync", tri[:], tri_d, writes=[Ttri])
            onesf = sb(pb, "onesf", [128, 128], F32); Tones = S.tile()
            S.dma("sync", onesf[:], onesf_d, writes=[Tones])

            with ExitStack() as sw:
                st = make_sweep(sw, "b", [0, 1])
                wA = sb(sw, "wA", [128, 8, 512], BF16); TwA = S.tile()
                wB = sb(sw, "wB", [128, 8, 512], BF16); TwB = S.tile()
                wF = sb(sw, "wF", [128, 8, 8], BF16); TwF = S.tile()
                wload(wA[:], w_in[:, 2048:2560], TwA)
                wload(wB[:], w_in[:, 2560:3072], TwB)
                wload(wF[:], w_in[:, 3072:3080], TwF)
                kctr = [0]

                def plain_proj(blk, hT, ThT, w_, Tw_, dstT, TdstT, scale):
                    for hp in range(4):
                        bk = 2 + (kctr[0] % 3)
                        kctr[0] += 1
                        mmgroup(banks[bk][:], [(w_[:, c, hp * 128:(hp + 1) * 128], hT[:, c, :]) for c in range(8)],
                                reads=list(ThT) + [Tw_], writes=[Tb[bk]])
                        S.op("scalar", lambda e, bk=bk, hp=hp: e.activation(
                            out=dstT[:, hp, blk * 512:(blk + 1) * 512], in_=banks[bk][:], func=AF.Copy, scale=scale),
                            reads=[Tb[bk]], writes=[TdstT[hp][blk]])

                def kvf_proj(blk, hT, ThT):
                    plain_proj(blk, hT, ThT, wA, TwA, KT, TKT, 1.0)
                    for tt in range(4):
                        n = blk * 4 + tt
                        bv = 6 + (tt % 2)
                        mmgroup(banks[bv][:], [(hT[:, c, tt * 128:(tt + 1) * 128], wB[:, c, :]) for c in range(8)],
                                reads=[ThT[tt], TwB], writes=[Tb[bv]])
                        S.op("vector", lambda e, n=n, bv=bv: e.tensor_copy(
                            out=Vf[:, n, :, 0:64], in_=banks[bv][:].rearrange("p (h d) -> p h d", h=8)),
                            reads=[Tb[bv]], writes=[TV[n]])
                        mmgroup(banks[5][:, 0:8], [(hT[:, c, tt * 128:(tt + 1) * 128], wF[:, c, :]) for c in range(8)],
                                reads=[ThT[tt], TwF], writes=[Tb[5]])
                        S.op("vector", lambda e, n=n: e.tensor_tensor(out=zt[:, n, :], in0=banks[5][:, 0:8], in1=bfb[:], op=ALU.add),
                             reads=[Tb[5], Tbfb], writes=[Tzt[n]])

                sweep_stage1(st, xb, 0, gb_attn, Tgba)
                sweep_stage1(st, xb, 1, gb_attn, Tgba)
                for blk in range(8):
                    if blk + 2 < 8:
                        sweep_stage1(st, xb, blk + 2, gb_attn, Tgba)
                    kvf_proj(blk, st["hT"][blk % 3], st["ThT"][blk % 3])
                wload(wA[:], w_in[:, 1536:2048], TwA)
                sweep_stage1(st, xo, 0, gb_attn, Tgba)
                sweep_stage1(st, xo, 1, gb_attn, Tgba)
                for blk in range(4):
                    if blk + 2 < 4:
                        sweep_stage1(st, xo, blk + 2, gb_attn, Tgba)
                    plain_proj(blk, st["hT"][blk % 3], st["ThT"][blk % 3], wA, TwA, QT, TQT, 0.125)
                ztf = zt[:].rearrange("p a b -> p (a b)")
                S.op("scalar", lambda e: e.activation(out=ztf, in_=ztf, func=AF.Exp, scale=-1.0), reads=Tzt, writes=Tzt)
                S.op("scalar", lambda e: e.activation(out=ztf, in_=ztf, func=AF.Ln, bias=1.0), reads=Tzt, writes=Tzt)
                S.op("vector", lambda e: e.memset(Cpos[:, 0, :], 0.0), writes=[TC[0]])
                for n in range(32):
                    bc = 2 + (n % 2)
                    S.op("tensor", lambda e, n=n, bc=bc: e.matmul(out=banks[bc][:, 0:8], lhsT=tri[:], rhs=zt[:, n, :], start=True, stop=True),
                         reads=[Ttri, Tzt[n]], writes=[Tb[bc]], signal=False)
                    S.op("tensor", lambda e, n=n, bc=bc: e.matmul(out=banks[bc][:, 8:16], lhsT=onesf[:], rhs=zt[:, n, :], start=True, stop=True),
                         reads=[Tones, Tzt[n]], writes=[Tb[bc]], signal=True)
                    S.op("vector", lambda e, n=n, bc=bc: e.tensor_tensor(out=Fpos[:, n, :], in0=banks[bc][:, 0:8], in1=Cpos[:, n, :], op=ALU.add),
                         reads=[Tb[bc], TC[n]], writes=[TF[n]])
                    S.op("vector", lambda e, n=n, bc=bc: e.tensor_tensor(out=Cpos[:, n + 1, :], in0=banks[bc][:, 8:16], in1=Cpos[:, n, :], op=ALU.add),
                         reads=[Tb[bc], TC[n]], writes=[TC[n + 1]])
                for i in range(8):
                    S.op("vector", lambda e, i=i: e.tensor_tensor(out=ctmp[:], in0=Cpos[:].rearrange("p n h -> p h n"),
                                                                  in1=csel[:, i, :].unsqueeze(1).broadcast_to([128, 8, 33]), op=ALU.mult),
                         reads=TC + [Tcsel, Tctmp], writes=[Tctmp])
                    S.op("vector", lambda e, i=i: e.tensor_reduce(out=cq[:, i, :], in_=ctmp[:], axis=AX.X, op=ALU.add),
                         reads=[Tctmp], writes=[Tcq])
                S.barrier()
                if "Fpos" in debug:
                    dbg_out("Fpos", [128, 32, 8]); dump("Fpos", Fpos[:], []); S.barrier()
                S.emit()

            with ExitStack() as at:
                PT = [sb(at, "PT%d" % i, [128, 32, 256], BF16) for i in range(2)]
                TPT = [[S.tile() for _ in range(16)] for _ in range(2)]
                Otf = sb(at, "Otf", [128, 16, 512], BF16); TOtf = [S.tile() for _ in range(16)]
                rr = sb(at, "rrf", [128, 2], F32); Trr = [S.tile() for _ in range(2)]
                biasb = [sb(at, "biasb%d" % i, [128, 32], F32) for i in range(2)]; Tbias = [S.tile() for _ in range(2)]
                units = [dict(hp=hp, hh=hh, i=i, head=2 * hp + hh) for hp in range(4) for hh in range(2) for i in range(8)]
                for ui, u in enumerate(units):
                    u["ui"] = ui

                def KT_of(u, kb):
                    r0 = 64 * u["hh"]
                    return KT[r0:r0 + 64, u["hp"], kb * 128:(kb + 1) * 128], TKT[u["hp"]][kb // 4]

                def QT_of(u):
                    r0 = 64 * u["hh"]
                    return QT[r0:r0 + 64, u["hp"], u["i"] * 256:(u["i"] + 1) * 256], TQT[u["hp"]][u["i"] // 2]

                def V_of(u, kb):
                    return Vf[:, kb, u["head"], 0:65], TV[kb]

                def exp_emit(u, p, bi, PTb, Tp):
                    b2 = u["ui"] % 2
                    hd = u["head"]
                    nk = nk_of(u["i"])
                    if p == 0:
                        S.op("vector", lambda e: e.tensor_scalar(out=biasb[b2][:, 0:nk], in0=Fpos[:, 0:nk, hd], scalar1=cq[:, u["i"], hd:hd + 1],
                                                                 scalar2=None, op0=ALU.subtract),
                             reads=TF[0:nk] + [Tcq], writes=[Tbias[b2]])
                    for j in range(2):
                        kb = 2 * p + j
                        S.op("scalar", lambda e, kb=kb, j=j: e.activation(out=PTb[:, kb, :], in_=banks[bi][:, j * 256:(j + 1) * 256],
                                                                          func=AF.Exp, bias=biasb[b2][:, kb:kb + 1]),
                             reads=[Tb[bi], Tbias[b2]], writes=[Tp])

                def mask_emit(u, PTb, TPb):
                    i = u["i"]
                    lo = nk_of(i) - 4
                    S.op("vector", lambda e: e.tensor_tensor(out=PTb[:, lo:lo + 4, :], in0=PTb[:, lo:lo + 4, :],
                                                             in1=maskf[:, i % 2, :].rearrange("p (a b) -> p a b", a=4), op=ALU.min),
                         reads=[Tmf, TPb[lo // 2], TPb[lo // 2 + 1]], writes=[TPb[lo // 2], TPb[lo // 2 + 1]])

                def evac_emit(u, ob):
                    hd, i = u["head"], u["i"]
                    for s in range(2):
                        qb = 2 * i + s
                        S.op("vector", lambda e, s=s: e.reciprocal(out=rr[:, s:s + 1], in_=banks[ob][:, s * 65 + 64:s * 65 + 65]),
                             reads=[Tb[ob]], writes=[Trr[s]])
                        S.op("vector", lambda e, s=s, qb=qb: e.tensor_scalar(
                            out=Otf[:, qb, hd * 64:(hd + 1) * 64], in0=banks[ob][:, s * 65:s * 65 + 64], scalar1=rr[:, s:s + 1],
                            scalar2=None, op0=ALU.mult),
                            reads=[Tb[ob], Trr[s]], writes=[TOtf[qb]])

                run_attention(units, PT, TPT, KT_of, QT_of, V_of, 65, exp_emit, evac_emit, mask_emit,
                              s_banks=[0, 1, 2, 3], o_banks=[4, 5, 6, 7])
                for qb in range(16):
                    bi = qb % 2
                    pTv = banks[bi][:].bitcast(BF16)
                    for c in range(4):
                        S.op("tensor", lambda e, c=c, qb=qb, pTv=pTv: e.transpose(
                            out=pTv[:, c * 128:(c + 1) * 128], in_=Otf[:, qb, c * 128:(c + 1) * 128], identity=identb[:]),
                            reads=[TOtf[qb], Tidb], writes=[Tb[bi]], signal=(c == 3))
                    S.op("vector", lambda e, qb=qb, pTv=pTv: e.tensor_copy(
                        out=OT[:, 4:8, qb * 128:(qb + 1) * 128], in_=pTv[:, 0:512].rearrange("p (c t) -> p c t", c=4)),
                        reads=[Tb[bi]], writes=[TOT[qb]])
                S.barrier()
                if "OTb" in debug:
                    dbg_out("OTb", [128, 8, 2048], BF16); dump("OTb", OT[:], []); S.barrier()
                S.emit()
        if stop_after == "B":
            return nc

        with ExitStack() as pc:
            x2 = sb(pc, "x2", [128, 16, 1024], F32); Tx2 = [[S.tile() for _ in range(2)] for _ in range(16)]
            hmT = OT
            ThmT = TOT
            ovf = sb(pc, "ovf", [128, 1], I32); Tovf = S.tile()
            w12 = sb(pc, "w12", [128, 2, 16], F32); Tw12 = S.tile()
            pos = sb(pc, "pos", [128, 2, 16], I32); Tpos = S.tile()
            comb = sb(pc, "comb", [128, 16, 16], F32); Tcomb = S.tile()
            junk = sb(pc, "junkc", [128, 1024], BF16)
            ssc = sb(pc, "ssc", [128, 16], F32); Tssc = [S.tile() for _ in range(16)]; Tsscall = S.tile()
            with ExitStack() as c1:
                wo = sb(c1, "wo", [128, 8, 1024], BF16); Two = S.tile()
                wload(wo[:], w_out, Two)
                xt = [sb(c1, "xc%d" % i, [128, 1024], F32) for i in range(2)]; Txt = [S.tile() for _ in range(2)]
                rw32 = sb(c1, "rw32", [128, 8, 20], F32); Trw = S.tile()
                S.dma("sync", rw32[:], rw_d.rearrange("(c p) n -> p c n", p=128), writes=[Trw])
                rbb = sb(c1, "rbb", [128, 20], F32); Trbb = S.tile()
                bcast_load(rbb[:], rb_d, Trbb, 20)
                gbf = sb(c1, "gbf", [128, 1024], F32); Tgbf = S.tile()
                bcast_load(gbf[:], g_ffn, Tgbf, 1024)
                identf = sb(c1, "identf", [128, 128], F32); Tidf = S.tile()
                S.dma("sync", identf[:], identf_d, writes=[Tidf])
                hm32 = [sb(c1, "hm32%d" % i, [128, 1024], F32) for i in range(2)]; Thm32 = [S.tile() for _ in range(2)]
                hmT32 = [sb(c1, "hmT32%d" % i, [128, 8, 128], F32) for i in range(2)]; ThmT32 = [S.tile() for _ in range(2)]
                Lall = sb(c1, "Lall", [128, 16, 20], F32); TL = [S.tile() for _ in range(16)]
                if moe == "sparse":
                    hmb = sb(c1, "hmb", [128, 16, 1024], BF16); Thmb = [S.tile() for _ in range(16)]
                for t in range(16):
                    S.dma("sync", xt[t % 2][:], xo[t * 128:(t + 1) * 128, :], writes=[Txt[t % 2]])
                    for hf in range(2):
                        mmgroup(banks[hf][:], [(OT[:, c, t * 128:(t + 1) * 128], wo[:, c, hf * 512:(hf + 1) * 512]) for c in range(8)],
                                reads=[TOT[t], Two], writes=[Tb[hf]])
                        S.op("vector", lambda e, t=t, hf=hf: e.tensor_tensor(
                            out=x2[:, t, hf * 512:(hf + 1) * 512], in0=banks[hf][:], in1=xt[t % 2][:, hf * 512:(hf + 1) * 512], op=ALU.add),
                            reads=[Tb[hf], Txt[t % 2]], writes=[Tx2[t][hf]])
                    S.op("scalar", lambda e, t=t: e.activation(out=junk[:], in_=x2[:, t, :], func=AF.Square, accum_out=ssc[:, t:t + 1]),
                         reads=Tx2[t], writes=[Tssc[t]])
                if "x2" in debug:
                    dbg_out("x2", [2048, 1024])
                    for t in range(16):
                        dump("x2", x2[:, t, :], Tx2[t]) if False else S.dma("sync", dbg["x2"][t * 128:(t + 1) * 128, :], x2[:, t, :], reads=Tx2[t], writes=[Tdbg], semtile=Tdbg)
                if stop_after == "C0":
                    S.barrier(); S.emit()
                    return nc
                S.op("scalar", lambda e: e.activation(out=ssc[:], in_=ssc[:], func=AF.Sqrt, scale=1.0 / 1024, bias=EPS),
                     reads=Tssc, writes=[Tsscall])
                S.op("vector", lambda e: e.reciprocal(out=ssc[:], in_=ssc[:]), reads=[Tsscall], writes=[Tsscall])
                for t in range(16):
                    t2 = t % 2
                    S.op("vector", lambda e, t=t, t2=t2: e.scalar_tensor_tensor(
                        out=hm32[t2][:], in0=x2[:, t, :], scalar=ssc[:, t:t + 1], in1=gbf[:], op0=ALU.mult, op1=ALU.mult),
                        reads=Tx2[t] + [Tsscall, Tgbf], writes=[Thm32[t2]])
                    ba, bb = (2, 3) if t2 == 0 else (4, 5)
                    for c in range(8):
                        bk = ba if c < 4 else bb
                        S.op("tensor", lambda e, c=c, t2=t2, bk=bk: e.transpose(
                            out=banks[bk][:, (c % 4) * 128:(c % 4 + 1) * 128], in_=hm32[t2][:, c * 128:(c + 1) * 128], identity=identf[:]),
                            reads=[Thm32[t2], Tidf], writes=[Tb[bk]], signal=(c % 4 == 3))
                    import os
                    CUT = int(os.environ.get("C1CUT", "9"))
                    if CUT < 2:
                        continue
                    for k, bk in enumerate((ba, bb)):
                        src = banks[bk][:].rearrange("p (c t) -> p c t", c=4)
                        S.op("scalar", lambda e, k=k, t2=t2, src=src: e.activation(out=hmT32[t2][:, 4 * k:4 * k + 4, :], in_=src, func=AF.Copy),
                             reads=[Tb[bk]], writes=[ThmT32[t2]])
                        S.op("vector", lambda e, k=k, t=t, t2=t2: e.tensor_copy(out=hmT[:, 4 * k:4 * k + 4, t * 128:(t + 1) * 128], in_=hmT32[t2][:, 4 * k:4 * k + 4, :]),
                             reads=[ThmT32[t2]], writes=[ThmT[t]])
                    if moe == "sparse":
                        S.op("gpsimd", lambda e, t=t, t2=t2: e.tensor_copy(out=hmb[:, t, :], in_=hm32[t2][:]), reads=[Thm32[t2]], writes=[Thmb[t]])
                    if CUT < 3:
                        continue
                    br = 6 + t2
                    mmgroup(banks[br][:, 0:20], [(hmT32[t2][:, c, :], rw32[:, c, :]) for c in range(8)],
                            reads=[ThmT32[t2], Trw], writes=[Tb[br]])
                    S.op("vector", lambda e, t=t, br=br: e.tensor_tensor(out=Lall[:, t, :], in0=banks[br][:, 0:20], in1=rbb[:], op=ALU.add),
                         reads=[Tb[br], Trbb], writes=[TL[t]])
                if stop_after == "C1a":
                    if "Lall" in debug:
                        dbg_out("Lall", [128, 320])
                        S.dma("sync", dbg["Lall"], Lall[:].rearrange("p a b -> p (a b)"), reads=[], writes=[Tdbg], semtile=Tdbg)
                    S.barrier(); S.emit()
                    return nc
                TR = S.tile()

                def rt(name, shape):
                    return sb(c1, "rt_" + name, shape, F32)
                gmax = rt("gmax", [128, 16]); gm = rt("gm", [128, 16, 4]); gd = rt("gd", [128, 16, 4])
                gsum = rt("gsum", [128, 16]); gw = rt("gw", [128, 16]); pen = rt("pen", [128, 16, 4])
                EL = rt("EL", [128, 16, 16]); EL2 = rt("EL2", [128, 16, 16]); m1 = rt("m1", [128, 16]); m2 = rt("m2", [128, 16])
                oh1 = rt("oh1", [128, 16, 16]); oh2 = rt("oh2", [128, 16, 16]); dd = rt("dd", [128, 16]); w1 = rt("w1", [128, 16]); w2 = rt("w2", [128, 16])
                LG = Lall[:, :, 0:4]
                LE4 = Lall[:, :, 4:20].rearrange("p t (g e) -> p t g e", g=4)
                EL4 = EL[:].rearrange("p t (g e) -> p t g e", g=4)

                def vop(fn, first=False):
                    S.op("vector", fn, reads=(TL + [TR]) if first else [TR], writes=[TR])

                def bc3(a, n):
                    return a[:].unsqueeze(2).broadcast_to([128, 16, n])
                vop(lambda e: e.tensor_reduce(out=gmax[:], in_=LG, axis=AX.X, op=ALU.max), first=True)
                vop(lambda e: e.tensor_tensor(out=gm[:], in0=LG, in1=bc3(gmax, 4), op=ALU.is_equal))
                vop(lambda e: e.tensor_tensor(out=gd[:], in0=LG, in1=bc3(gmax, 4), op=ALU.subtract))
                S.op("scalar", lambda e: e.activation(out=gd[:], in_=gd[:], func=AF.Exp), reads=[TR], writes=[TR])
                vop(lambda e: e.tensor_reduce(out=gsum[:], in_=gd[:], axis=AX.X, op=ALU.add))
                vop(lambda e: e.reciprocal(out=gw[:], in_=gsum[:]))
                vop(lambda e: e.tensor_scalar(out=pen[:], in0=gm[:], scalar1=1.0, scalar2=1e30, op0=ALU.subtract, op1=ALU.mult))
                vop(lambda e: e.tensor_tensor(out=EL4, in0=LE4, in1=gm[:].unsqueeze(3).broadcast_to([128, 16, 4, 4]), op=ALU.mult))
                vop(lambda e: e.tensor_tensor(out=EL4, in0=EL4, in1=pen[:].unsqueeze(3).broadcast_to([128, 16, 4, 4]), op=ALU.add))
                vop(lambda e: e.tensor_reduce(out=m1[:], in_=EL[:], axis=AX.X, op=ALU.max))
                vop(lambda e: e.tensor_tensor(out=oh1[:], in0=EL[:], in1=bc3(m1, 16), op=ALU.is_equal))
                vop(lambda e: e.scalar_tensor_tensor(out=EL2[:], in0=oh1[:], scalar=-1e30, in1=EL[:], op0=ALU.mult, op1=ALU.add))
                vop(lambda e: e.tensor_reduce(out=m2[:], in_=EL2[:], axis=AX.X, op=ALU.max))
                vop(lambda e: e.tensor_tensor(out=oh2[:], in0=EL2[:], in1=bc3(m2, 16), op=ALU.is_equal))
                vop(lambda e: e.tensor_tensor(out=dd[:], in0=m2[:], in1=m1[:], op=ALU.subtract))
                S.op("scalar", lambda e: e.activation(out=dd[:], in_=dd[:], func=AF.Exp), reads=[TR], writes=[TR])
                vop(lambda e: e.tensor_scalar(out=w1[:], in0=dd[:], scalar1=1.0, scalar2=None, op0=ALU.add))
                vop(lambda e: e.reciprocal(out=w1[:], in_=w1[:]))
                vop(lambda e: e.tensor_tensor(out=w1[:], in0=w1[:], in1=gw[:], op=ALU.mult))
                vop(lambda e: e.tensor_tensor(out=w2[:], in0=dd[:], in1=w1[:], op=ALU.mult))
                if moe == "sparse":
                    Mb = sb(c1, "Mb", [128, 16, 16], BF16)
                    ustrict = sb(c1, "ustrict", [128, 128], BF16); Tus = S.tile()
                    S.dma("sync", ustrict[:], ustrict_d, writes=[Tus])
                    onesb = sb(c1, "onesb", [128, 128], BF16); Tob_ = S.tile()
                    S.dma("sync", onesb[:], onesb_d, writes=[Tob_])
                    ebase = sb(c1, "ebase", [128, 16, 16], F32); Teb = S.tile()
                    S.dma("sync", ebase[:].rearrange("p a b -> p (a b)"), ebase_d, writes=[Teb])
                    slotf = rt("slotf", [128, 16, 16]); okf = rt("okf", [128, 16, 16]); posf = rt("posf", [128, 2, 16])
                    vop(lambda e: e.tensor_tensor(out=Mb[:], in0=oh1[:], in1=oh2[:], op=ALU.add))
                    for t in range(16):
                        prs = [(onesb[:], Mb[:, tp, :]) for tp in range(t)] + [(ustrict[:], Mb[:, t, :])]
                        n_ = len(prs)
                        for k_, (l_, r_) in enumerate(prs):
                            S.op("tensor", lambda e, l_=l_, r_=r_, k_=k_, n_=n_, t=t: e.matmul(out=banks[0][:, t * 16:(t + 1) * 16], lhsT=l_, rhs=r_,
                                                                                         start=(k_ == 0), stop=(k_ == n_ - 1)),
                                 reads=[TR, Tus, Tob_], writes=[Tb[0]], signal=(k_ == n_ - 1))
                    for tp in range(16):
                        S.op("tensor", lambda e, tp=tp: e.matmul(out=banks[1][:, 0:16], lhsT=onesb[:], rhs=Mb[:, tp, :], start=(tp == 0), stop=(tp == 15)),
                             reads=[TR, Tob_], writes=[Tb[1]], signal=(tp == 15))
                    cmax = rt("cmax", [128, 1])
                    S.op("vector", lambda e: e.tensor_reduce(out=cmax[:], in_=banks[1][:, 0:16], axis=AX.X, op=ALU.max), reads=[Tb[1], TR], writes=[TR])
                    import os as _os
                    thr = -1.0 if _os.environ.get("FORCE_DENSE") else float(CAP)
                    vop(lambda e: e.tensor_scalar(out=cmax[:], in0=cmax[:], scalar1=thr, scalar2=None, op0=ALU.is_gt))
                    S.op("vector", lambda e: e.tensor_copy(out=ovf[:], in_=cmax[:]), reads=[TR], writes=[Tovf])
                    rank = banks[0][:, 0:256].rearrange("p (a b) -> p a b", a=16)
                    S.op("vector", lambda e: e.tensor_tensor(out=slotf[:], in0=rank, in1=ebase[:], op=ALU.add), reads=[Tb[0], Teb, TR], writes=[TR])
                    vop(lambda e: e.tensor_scalar(out=okf[:], in0=slotf[:], scalar1=None, scalar2=None, op0=ALU.bypass) if False else
                        e.tensor_tensor(out=okf[:], in0=slotf[:], in1=ebase[:], op=ALU.subtract))
                    vop(lambda e: e.tensor_scalar(out=okf[:], in0=okf[:], scalar1=float(CAP), scalar2=1.0e6, op0=ALU.is_ge, op1=ALU.mult))
                    vop(lambda e: e.tensor_tensor(out=slotf[:], in0=slotf[:], in1=okf[:], op=ALU.add))
                    vop(lambda e: e.tensor_tensor(out=okf[:], in0=slotf[:], in1=oh1[:], op=ALU.mult))
                    vop(lambda e: e.tensor_reduce(out=posf[:, 0, :], in_=okf[:], axis=AX.X, op=ALU.add))
                    vop(lambda e: e.tensor_tensor(out=okf[:], in0=slotf[:], in1=oh2[:], op=ALU.mult))
                    vop(lambda e: e.tensor_reduce(out=posf[:, 1, :], in_=okf[:], axis=AX.X, op=ALU.add))
                    S.op("vector", lambda e: e.tensor_copy(out=pos[:], in_=posf[:]), reads=[TR], writes=[Tpos])
                    S.op("vector", lambda e: e.tensor_copy(out=w12[:, 0, :], in_=w1[:]), reads=[TR, Tw12], writes=[Tw12])
                    S.op("vector", lambda e: e.tensor_copy(out=w12[:, 1, :], in_=w2[:]), reads=[TR, Tw12], writes=[Tw12])
                    Tsc = [S.tile() for _ in range(32)]
                    set_bcreg()
                    for t in range(16):
                        for k_ in range(2):
                            S.dma_fn("gpsimd", lambda e, t=t, k_=k_: e.indirect_dma_start(
                                out=xs_d[:, :], out_offset=bass.IndirectOffsetOnAxis(ap=pos[:, k_, t:t + 1], axis=0),
                                in_=hmb[:, t, :], in_offset=None, bounds_check=bcreg, oob_is_err=False),
                                reads=[Thmb[t], Tpos, Txs], writes=[Tsc[2 * t + k_]], semtile=Thmb[t])
                vop(lambda e: e.tensor_tensor(out=oh1[:], in0=oh1[:], in1=bc3(w1, 16), op=ALU.mult))
                vop(lambda e: e.tensor_tensor(out=oh2[:], in0=oh2[:], in1=bc3(w2, 16), op=ALU.mult))
                S.op("vector", lambda e: e.tensor_tensor(out=comb[:], in0=oh1[:], in1=oh2[:], op=ALU.add), reads=[TR], writes=[Tcomb])
                S.barrier()
                if "comb" in debug:
                    dbg_out("comb", [128, 256])
                    S.dma("sync", dbg["comb"], comb[:].rearrange("p a b -> p (a b)"), reads=[Tcomb], writes=[Tdbg], semtile=Tdbg)
                    S.barrier()
                S.emit()
            if stop_after == "C1":
                return nc

            with ExitStack() as c2:
                wgb = [sb(c2, "wgb%d" % i, [128, 8, 512], BF16) for i in range(2)]; Twg4 = [[S.tile() for _ in range(4)] for _ in range(2)]
                wub = [sb(c2, "wub%d" % i, [128, 8, 512], BF16) for i in range(2)]; Twu4 = [[S.tile() for _ in range(4)] for _ in range(2)]
                wdb = [sb(c2, "wdb%d" % i, [128, 4, 1024], BF16) for i in range(2)]; Twd4 = [[S.tile() for _ in range(4)] for _ in range(2)]
                stg = [sb(c2, "stg%d" % i, [128, 1024], F32) for i in range(3)]; Tstg = [S.tile() for _ in range(3)]
                sq_ = [0]
                aT = [sb(c2, "aT%d" % i, [128, 4, 512], BF16) for i in range(2)]; TaT = [[S.tile() for _ in range(4)] for _ in range(2)]
                sg = [sb(c2, "sg%d" % i, [128, 512], F32) for i in range(2)]; Tsg = [S.tile() for _ in range(2)]
                xg = [sb(c2, "xg%d" % i, [128, 1024], BF16) for i in range(4)]; Txg = [S.tile() for _ in range(4)]
                xgT = [sb(c2, "xgT%d" % i, [128, 8, CAP], BF16) for i in range(2)]; TxgT = [[S.tile() for _ in range(CAP // 128)] for _ in range(2)]
                ysb = [sb(c2, "ysb%d" % i, [128, 1024], F32) for i in range(2)]; Tysb = [S.tile() for _ in range(2)]
                NJ = CAP // 128
                Tys = [S.tile() for _ in range(NEXP * NJ)]

                def w_steps(ex):
                    b2 = ex % 2
                    dmas, casts = [], []
                    for k in range(12):
                        def mk(k=k):
                            if k < 8:
                                srcw = (wg_d if k < 4 else wu_d)[ex]
                                kk = k % 4
                                src = srcw[kk * 256:(kk + 1) * 256, :].rearrange("(c p) n -> p c n", p=128)
                                dst_of = lambda: (wgb if k < 4 else wub)[b2][:, 2 * kk:2 * kk + 2, :]
                                Td = (Twg4 if k < 4 else Twu4)[b2][kk]
                                view = lambda t_: t_[:].rearrange("p (c n) -> p c n", c=2)
                            else:
                                kk = k - 8
                                src = wd_d[ex][kk * 128:(kk + 1) * 128, :]
                                dst_of = lambda: wdb[b2][:, kk, :]
                                Td = Twd4[b2][kk]
                                view = lambda t_: t_[:]
                            cell = {}

                            def d():
                                si = sq_[0] % 3
                                sq_[0] += 1
                                cell["si"] = si
                                S.dma("sync", view(stg[si]), src, writes=[Tstg[si]])

                            def c():
                                si = cell["si"]
                                sv = view(stg[si])
                                dstb = dst_of()
                                if k % 2 == 1:
                                    S.op("scalar", lambda e: e.activation(out=dstb, in_=sv, func=AF.Copy), reads=[Tstg[si]], writes=[Td])
                                else:
                                    S.op("vector", lambda e: e.tensor_copy(out=dstb, in_=sv), reads=[Tstg[si]], writes=[Td])
                            return d, c
                        d, c = mk()
                        dmas.append(d)
                        casts.append(c)
                    steps = dmas[0:3]
                    for k in range(12):
                        steps.append(casts[k])
                        if k + 3 < 12:
                            steps.append(dmas[k + 3])
                    return steps

                def load_w(ex):
                    for f in w_steps(ex):
                        f()

                def gate_up(b2, rhs_of, Trhs, width, gq, slot=None):
                    for ft in range(4):
                        bg, bu = (0, 1) if gq[0] % 2 == 0 else (2, 3)
                        s2 = gq[0] % 2
                        gq[0] += 1
                        mmgroup(banks[bg][:, 0:width], [(wgb[b2][:, c, ft * 128:(ft + 1) * 128], rhs_of(c)) for c in range(8)],
                                reads=Trhs + Twg4[b2], writes=[Tb[bg]])
                        mmgroup(banks[bu][:, 0:width], [(wub[b2][:, c, ft * 128:(ft + 1) * 128], rhs_of(c)) for c in range(8)],
                                reads=Trhs + Twu4[b2], writes=[Tb[bu]])
                        S.op("scalar", lambda e, bg=bg, s2=s2: e.activation(out=sg[s2][:, 0:width], in_=banks[bg][:, 0:width], func=AF.Silu),
                             reads=[Tb[bg]], writes=[Tsg[s2]])
                        S.op("vector", lambda e, bu=bu, s2=s2, ft=ft: e.tensor_tensor(out=aT[b2][:, ft, 0:width], in0=sg[s2][:, 0:width], in1=banks[bu][:, 0:width], op=ALU.mult),
                             reads=[Tsg[s2], Tb[bu]], writes=[TaT[b2][ft]])
                        if slot is not None:
                            slot()

                S.branch_begin()
                gq = [0]; yq = [0]; xq = [0]

                def prep(ex):
                    b2 = ex % 2
                    for j in range(NJ):
                        xi = xq[0] % 4
                        xq[0] += 1
                        r0 = ex * CAP + j * 128
                        S.dma("gpsimd", xg[xi][:], xs_d[r0:r0 + 128, :], reads=Tsc + [Txs], writes=[Txg[xi]])
                        bi = 6 + (xq[0] % 2)
                        pTv = banks[bi][:].bitcast(BF16)
                        for c in range(8):
                            S.op("tensor", lambda e, c=c, xi=xi, pTv=pTv: e.transpose(
                                out=pTv[:, c * 128:(c + 1) * 128], in_=xg[xi][:, c * 128:(c + 1) * 128], identity=identb[:]),
                                reads=[Txg[xi], Tidb], writes=[Tb[bi]], signal=(c == 7))
                        S.op("vector", lambda e, j=j, b2=b2, pTv=pTv: e.tensor_copy(
                            out=xgT[b2][:, :, j * 128:(j + 1) * 128], in_=pTv.rearrange("p (c t) -> p c t", c=8)),
                            reads=[Tb[bi]], writes=[TxgT[b2][j]])
                prep(0)
                load_w(0)
                for ex in range(NEXP):
                    b2 = ex % 2
                    wq = w_steps(ex + 1) if ex + 1 < NEXP else []

                    def pop(n):
                        for _ in range(n):
                            if wq:
                                wq.pop(0)()
                    pop(3)
                    gate_up(b2, lambda c, b2=b2: xgT[b2][:, c, :], TxgT[b2], CAP, gq, slot=lambda: pop(3))
                    if ex + 1 < NEXP:
                        prep(ex + 1)
                    for j in range(NJ):
                        y2 = yq[0] % 2
                        yq[0] += 1
                        for hf in range(2):
                            by = 4 + hf
                            mmgroup(banks[by][:], [(aT[b2][:, ft, j * 128:(j + 1) * 128], wdb[b2][:, ft, hf * 512:(hf + 1) * 512]) for ft in range(4)],
                                    reads=TaT[b2] + Twd4[b2], writes=[Tb[by]])
                            if hf == 0:
                                S.op("vector", lambda e, y2=y2, by=by: e.tensor_copy(out=ysb[y2][:, 0:512], in_=banks[by][:]),
                                     reads=[Tb[by]], writes=[Tysb[y2]])
                            else:
                                S.op("scalar", lambda e, y2=y2, by=by: e.activation(out=ysb[y2][:, 512:1024], in_=banks[by][:], func=AF.Copy),
                                     reads=[Tb[by], Tysb[y2]], writes=[Tysb[y2]])
                        r0 = ex * CAP + j * 128
                        S.dma("gpsimd", ys_d[r0:r0 + 128, :], ysb[y2][:], reads=[Tysb[y2]], writes=[Tys[ex * NJ + j]], semtile=Tysb[y2])
                        pop(3)
                    pop(99)
                S.barrier()
                ygl = []
                for wb in wgb + wub + wdb:
                    v = wb[:].rearrange("p a b -> p (a b)").bitcast(F32)
                    ygl += [v[:, 0:1024], v[:, 1024:2048]]
                Tyg = [S.tile() for _ in ygl]
                set_bcreg()
                def gath(i):
                    t, k_ = i // 2, i % 2
                    gi = i % len(ygl)
                    S.dma_fn("gpsimd", lambda e, t=t, k_=k_, gi=gi: e.indirect_dma_start(
                        out=ygl[gi], out_offset=None, in_=ys_d[:, :],
                        in_offset=bass.IndirectOffsetOnAxis(ap=pos[:, k_, t:t + 1], axis=0),
                        bounds_check=bcreg, oob_is_err=False),
                        reads=Tys + [Tpos], writes=[Tyg[gi]], semtile=Tyg[gi])

                def acc(i):
                    t, k_ = i // 2, i % 2
                    gi = i % len(ygl)
                    for hf in range(2):
                        S.op("vector", lambda e, t=t, k_=k_, gi=gi, hf=hf: e.scalar_tensor_tensor(
                            out=x2[:, t, hf * 512:(hf + 1) * 512], in0=ygl[gi][:, hf * 512:(hf + 1) * 512], scalar=w12[:, k_, t:t + 1],
                            in1=x2[:, t, hf * 512:(hf + 1) * 512], op0=ALU.mult, op1=ALU.add),
                            reads=[Tyg[gi], Tw12, Tx2[t][hf]], writes=[Tx2[t][hf]])
                depth = len(ygl) - 1
                for i in range(32 + depth):
                    if i < 32:
                        gath(i)
                    if i - depth >= 0:
                        acc(i - depth)
                S.branch_mid()
                gq = [0]; yq = [0]
                load_w(0)
                for ex in range(NEXP):
                    b2 = ex % 2
                    if ex + 1 < NEXP:
                        load_w(ex + 1)
                    for tb in range(4):
                        gate_up(b2, lambda c, tb=tb: hmT[:, c, tb * 512:(tb + 1) * 512], ThmT[tb * 4:tb * 4 + 4], 512, gq)
                        for tt in range(4):
                            t = tb * 4 + tt
                            for hf in range(2):
                                by = 4 + (yq[0] % 4)
                                yq[0] += 1
                                mmgroup(banks[by][:], [(aT[b2][:, ft, tt * 128:(tt + 1) * 128], wdb[b2][:, ft, hf * 512:(hf + 1) * 512]) for ft in range(4)],
                                        reads=TaT[b2] + Twd4[b2], writes=[Tb[by]])
                                S.op("vector", lambda e, t=t, hf=hf, by=by, ex=ex: e.scalar_tensor_tensor(
                                    out=x2[:, t, hf * 512:(hf + 1) * 512], in0=banks[by][:], scalar=comb[:, t, ex:ex + 1],
                                    in1=x2[:, t, hf * 512:(hf + 1) * 512], op0=ALU.mult, op1=ALU.add),
                                    reads=[Tb[by], Tcomb, Tx2[t][hf]], writes=[Tx2[t][hf]])
                S.branch_end(ovf[0:1, 0:1], brregs)
                S.emit()

            with ExitStack() as c3:
                gbn = sb(c3, "gbn", [128, 1024], F32); Tgbn = S.tile()
                bcast_load(gbn[:], g_fin, Tgbn, 1024)
                ob = [sb(c3, "ob%d" % i, [128, 1024], F32) for i in range(2)]; Tob = [S.tile() for _ in range(2)]
                Tout = S.tile()
                for t in range(16):
                    S.op("scalar", lambda e, t=t: e.activation(out=junk[:], in_=x2[:, t, :], func=AF.Square, accum_out=ssc[:, t:t + 1]),
                         reads=Tx2[t] + [Tsscall], writes=[Tssc[t]])
                S.op("scalar", lambda e: e.activation(out=ssc[:], in_=ssc[:], func=AF.Sqrt, scale=1.0 / 1024, bias=EPS),
                     reads=Tssc, writes=[Tsscall])
                S.op("vector", lambda e: e.reciprocal(out=ssc[:], in_=ssc[:]), reads=[Tsscall], writes=[Tsscall])
                for t in range(16):
                    S.op("vector", lambda e, t=t: e.scalar_tensor_tensor(
                        out=ob[t % 2][:], in0=x2[:, t, :], scalar=ssc[:, t:t + 1], in1=gbn[:], op0=ALU.mult, op1=ALU.mult),
                        reads=Tx2[t] + [Tsscall, Tgbn], writes=[Tob[t % 2]])
                    S.dma("sync", out[t * 128:(t + 1) * 128, :], ob[t % 2][:], reads=[Tob[t % 2]], writes=[Tout], semtile=Tob[t % 2])
                S.barrier()
                S.emit()
    return nc


def _const_tables():
    f32 = np.float32
    inv_freq = (f32(1.0) / (f32(10000.0) ** (np.arange(0, 64, 2, dtype=f32) / f32(64)))).astype(f32)
    pos = np.arange(4096, dtype=f32)
    ang = (pos[:, None] * inv_freq[None, :]).astype(f32)
    cos = np.cos(ang).astype(f32)
    sin = np.sin(ang).astype(f32)
    r = np.arange(128)
    dh = r % 64
    cosT = cos[:, dh % 32].T.copy()
    sgn = np.where(dh < 32, -1.0, 1.0).astype(f32)
    sinT = (sin[:, dh % 32].T * sgn[:, None]).astype(f32)
    return cosT, sinT


def _masks(hf):
    k = np.arange(128)[:, None, None]
    r = np.arange(4)[None, :, None]
    q = np.arange(256)[None, None, :]
    md = np.zeros((128, 2, 4, 256), np.float32)
    mf = np.zeros((128, 2, 4, 256), np.float32)
    for par in range(2):
        if par == 0:
            kb = r
            j = 0 if hf == 0 else 1
        else:
            kb = 4 + r
            j = 3 if hf == 0 else 2
        s = kb * 128 + k
        t = j * 256 + q
        mf[:, par] = np.where(s <= t, 3e38, 0.0)
        md[:, par] = np.where((s // 64) <= (t // 64), 3e38, 0.0)
    return (md.reshape(128, 2, 1024).astype(ml_dtypes.bfloat16),
            mf.reshape(128, 2, 1024).astype(ml_dtypes.bfloat16))


def own_tokens(hf):
    return np.concatenate([np.arange(j * 256, (j + 1) * 256) for j in own_qtiles(hf)])


def prep(inputs):
    f32 = np.float32
    x = np.asarray(inputs["x"], f32)
    w_in = np.ascontiguousarray(np.asarray(inputs["w_in"], f32)[0])

    def swap_cols(w):
        return np.ascontiguousarray(w.reshape(1024, 8, 2, 32)[:, :, ::-1, :].reshape(1024, 512))

    cosT, sinT = _const_tables()
    common = {
        "w_in": w_in,
        "wqs": swap_cols(w_in[:, 0:512]),
        "wks": swap_cols(w_in[:, 512:1024]),
        "cosk": cosT, "sink": sinT,
        "identb": np.eye(128, dtype=f32).astype(ml_dtypes.bfloat16),
        "identf": np.eye(128, dtype=f32),
        "tri": np.triu(np.ones((128, 128), f32)),
        "onesf": np.ones((128, 128), f32),
        "onesb": np.ones((128, 128), f32).astype(ml_dtypes.bfloat16),
        "ustrict": np.triu(np.ones((128, 128), f32), 1).astype(ml_dtypes.bfloat16),
        "ebase": np.ascontiguousarray(np.broadcast_to((np.arange(16, dtype=f32) * CAP)[None, None, :], (128, 16, 16)).reshape(128, 256)),
        "g_attn": np.asarray(inputs["norm_attn_g"], f32).reshape(1, 1024),
        "g_ffn": np.asarray(inputs["norm_ffn_g"], f32).reshape(1, 1024),
        "g_fin": np.asarray(inputs["norm_final_g"], f32).reshape(1, 1024),
        "bfor": np.asarray(inputs["b_forget"], f32).reshape(1, 8),
        "lamv": np.concatenate([np.asarray(inputs[k], f32).reshape(1, 64) for k in
                                ("lambda_q1", "lambda_k1", "lambda_q2", "lambda_k2")], axis=1),
        "dng": np.asarray(inputs["diff_norm_g"], f32).reshape(1, 128),
        "w_out": np.ascontiguousarray(np.asarray(inputs["w_out"], f32)[0]),
        "rw": np.ascontiguousarray(np.concatenate([np.asarray(inputs["router_group_w"], f32)[0],
                                                   np.asarray(inputs["router_expert_w"], f32)[0]], axis=1)),
        "rb": np.concatenate([np.asarray(inputs["router_group_b"], f32).reshape(1, 4),
                              np.asarray(inputs["router_expert_b"], f32).reshape(1, 16)], axis=1),
        "wg": np.ascontiguousarray(np.asarray(inputs["w_gate"], f32)[0]),
        "wu": np.ascontiguousarray(np.asarray(inputs["w_up"], f32)[0]),
        "wd": np.ascontiguousarray(np.asarray(inputs["w_down"], f32)[0]),
    }
    in_maps = []
    for c in range(8):
        b, hf = c // 2, c % 2
        tok = own_tokens(hf)
        md, mf = _masks(hf)
        m = dict(common)
        m["xb"] = np.ascontiguousarray(x[b])
        m["xo"] = np.ascontiguousarray(x[b][tok])
        m["cosq"] = np.ascontiguousarray(cosT[:, tok] * f32(0.125))
        m["sinq"] = np.ascontiguousarray(sinT[:, tok] * f32(0.125))
        cs = np.zeros((8, 33), f32)
        for i, j in enumerate(own_qtiles(hf)):
            cs[i, 2 * j + 1] = 1.0
        m["csel"] = cs.reshape(1, 8 * 33)
        m["maskd"] = md
        m["maskf"] = mf
        in_maps.append(m)
    return in_maps


def kernel(**inputs):
    in_maps = prep(inputs)
    nc = build()
    res = run_bass_kernel_spmd(nc, in_maps, core_ids=list(range(8)))
    out = np.zeros((4, 4096, 1024), np.float32)
    for c in range(8):
        b, hf = c // 2, c % 2
        out[b, own_tokens(hf)] = res.results[c]["out"]
    return out
```

```python
import numpy as np
import ml_dtypes
from contextlib import ExitStack
import concourse.bass as bass
import concourse.mybir as mybir
from concourse.bass_utils import run_bass_kernel_spmd

F32 = mybir.dt.float32
BF16 = mybir.dt.bfloat16
I32 = mybir.dt.int32
AF = mybir.ActivationFunctionType
ALU = mybir.AluOpType
AX = mybir.AxisListType

ENGS = ("sync", "scalar", "vector", "gpsimd", "tensor")
EPS = 1e-6
LAM_INIT = 0.8 - 0.6 * 1.0
NEXP = 16
CAP = 512
NSLOT = NEXP * CAP


class Tile:
    __slots__ = ("name", "last_w", "readers", "dsem")

    def __init__(self, name):
        self.name = name
        self.last_w = None
        self.readers = {}
        self.dsem = None


class Sched:
    def __init__(self, nc, es):
        self.nc = nc
        self.es = es
        self.ops = {e: [] for e in ENGS}
        self.sems = {}
        self.cnt = {}
        self.waited = {e: {} for e in ENGS}
        self.pending = {e: False for e in ENGS}
        for e in ENGS:
            self._mksem("E:" + e)
        self.n_dsem = 0
        self.nops = 0
        self.tiles = []

    def _mksem(self, key):
        self.sems[key] = self.es.enter_context(self.nc.semaphore(key.replace(":", "_")))
        self.cnt[key] = 0

    def tile(self, name="t"):
        t = Tile(name)
        self.tiles.append(t)
        return t

    def _snapshot(self):
        return (dict(self.cnt), {e: dict(w) for e, w in self.waited.items()},
                [(t, t.last_w, dict(t.readers)) for t in self.tiles])

    def _restore(self, snap):
        self.cnt = dict(snap[0])
        for k in self.sems:
            self.cnt.setdefault(k, 0)
        self.waited = {e: dict(w) for e, w in snap[1].items()}
        for t, lw, rd in snap[2]:
            t.last_w = lw
            t.readers = dict(rd)

    def branch_begin(self):
        self.barrier()
        self._outer_ops = self.ops
        self.ops = {e: [] for e in ENGS}
        self._snap = self._snapshot()

    def branch_mid(self):
        self.barrier()
        self._A = (self.ops, dict(self.cnt))
        self.ops = {e: [] for e in ENGS}
        self._restore(self._snap)

    def branch_end(self, flag_ap, regs):
        self.barrier()
        opsA, cntA = self._A
        opsB, cntB = self.ops, dict(self.cnt)
        target = {k: max(cntA.get(k, 0), cntB.get(k, 0)) for k in set(cntA) | set(cntB)}

        def pads(cntX):
            out = {e: [] for e in ENGS}
            for k, v in target.items():
                d = v - cntX.get(k, 0)
                if d > 0:
                    owner = k[2:] if k.startswith("E:") else "gpsimd"
                    out[owner].append((k, d))
            return out
        pA, pB = pads(cntA), pads(cntB)
        self.ops = self._outer_ops
        for e in ENGS:
            self.ops[e].append(("branch", flag_ap, regs[e], opsA[e], pA[e], opsB[e], pB[e]))
        self.cnt = target
        for e in ENGS:
            self.waited[e] = dict(target)
        for t in self.tiles:
            t.last_w = None
            t.readers = {}

    def dsem_for(self, t):
        if t.dsem is None:
            key = "D:%d" % self.n_dsem
            self.n_dsem += 1
            self._mksem(key)
            t.dsem = key
        return t.dsem

    def _need(self, eng, waits, key, val):
        if eng == "tensor" and key == "E:tensor":
            return
        if self.cnt[key] < val:
            raise RuntimeError("wait on un-signalled event %s %d (cnt %d) from %s" % (key, val, self.cnt[key], eng))
        if self.waited[eng].get(key, 0) >= val:
            return
        self.waited[eng][key] = val
        waits[key] = max(waits.get(key, 0), val)

    def _deps(self, eng, reads, writes):
        waits = {}
        for t in reads:
            if t.last_w is not None:
                self._need(eng, waits, *t.last_w)
        for t in writes:
            if t.last_w is not None:
                self._need(eng, waits, *t.last_w)
            for k, v in t.readers.items():
                self._need(eng, waits, k, v)
        return list(waits.items())

    def _record(self, ev, reads, writes):
        for t in writes:
            t.last_w = ev
            t.readers = {}
        for t in reads:
            if t not in writes:
                if t.readers.get(ev[0], 0) < ev[1]:
                    t.readers[ev[0]] = ev[1]

    def op(self, eng, fn, reads=(), writes=(), signal=True):
        waits = self._deps(eng, reads, writes)
        key = "E:" + eng
        if signal:
            self.cnt[key] += 1
            ev = (key, self.cnt[key])
            inc = (key, 1)
            self.pending[eng] = False
        else:
            ev = (key, self.cnt[key] + 1)
            inc = None
            self.pending[eng] = True
        self._record(ev, reads, writes)
        self.ops[eng].append((waits, fn, inc))
        self.nops += 1

    def dma(self, eng, out, in_, reads=(), writes=(), semtile=None, **kw):
        waits = self._deps(eng, reads, writes)
        if semtile is None:
            semtile = writes[0] if writes else reads[0]
        key = self.dsem_for(semtile)
        self.cnt[key] += 16
        ev = (key, self.cnt[key])
        self._record(ev, reads, writes)

        def fn(e, out=out, in_=in_, kw=kw):
            return e.dma_start(out=out, in_=in_, **kw)
        self.ops[eng].append((waits, fn, (key, 16)))
        self.nops += 1

    def dma_fn(self, eng, fn, reads=(), writes=(), semtile=None):
        waits = self._deps(eng, reads, writes)
        key = self.dsem_for(semtile)
        self.cnt[key] += 16
        ev = (key, self.cnt[key])
        self._record(ev, reads, writes)
        self.ops[eng].append((waits, fn, (key, 16)))
        self.nops += 1

    def barrier(self, engs=ENGS):
        for e in ENGS:
            assert not self.pending[e], e
        for e in engs:
            waits = {}
            for key, c in self.cnt.items():
                if c > 0:
                    self._need(e, waits, key, c)
            self.ops[e].append((list(waits.items()), None, None))

    def emit(self):
        nc = self.nc
        sems = self.sems
        ops = self.ops
        with nc.Block() as block:
            def replay(e, lst):
                for ent in lst:
                    if ent[0] == "branch":
                        _, flag_ap, reg, oA, pA, oB, pB = ent
                        e.reg_load(reg, flag_ap)
                        with e.If_eq(reg, 0):
                            replay(e, oA)
                            for k, d in pA:
                                e.sem_inc(sems[k], d)
                            e.nop()
                        with e.Else():
                            replay(e, oB)
                            for k, d in pB:
                                e.sem_inc(sems[k], d)
                            e.nop()
                        continue
                    waits, fn, inc = ent
                    for key, val in waits:
                        e.wait_ge(sems[key], val)
                    if fn is None:
                        continue
                    inst = fn(e)
                    if inc is not None:
                        inst.then_inc(sems[inc[0]], inc[1])

            def mk(name):
                def body(e):
                    replay(e, ops[name])
                return body
            block.sync(mk("sync"))
            block.scalar(mk("scalar"))
            block.vector(mk("vector"))
            block.gpsimd(mk("gpsimd"))
            block.tensor(mk("tensor"))
        self.ops = {e: [] for e in ENGS}


def own_qtiles(hf):
    js = []
    for m in range(4):
        js += ([4 * m, 4 * m + 3] if hf == 0 else [4 * m + 1, 4 * m + 2])
    return js


def nk_of(i):
    return 8 * (i // 2) + (4 if i % 2 == 0 else 8)


def interleave(A, B):
    a, b = len(A), len(B)
    if a == 0:
        for f in B:
            f()
        return
    done = 0
    for k, f in enumerate(A):
        f()
        upto = ((k + 1) * b) // a
        while done < upto:
            B[done]()
            done += 1
    while done < b:
        B[done]()
        done += 1


def build(debug=(), stop_after=None, moe="sparse"):
    nc = bass.Bass("TRN2", target_bir_lowering=False)

    def din(name, shape, dt=F32):
        return nc.dram_tensor(name, list(shape), dt, kind="ExternalInput").ap()

    xb = din("xb", [4096, 1024])
    xo = din("xo", [2048, 1024])
    w_in = din("w_in", [1024, 3080])
    wqs_d = din("wqs", [1024, 512])
    wks_d = din("wks", [1024, 512])
    cosk = din("cosk", [128, 4096])
    sink = din("sink", [128, 4096])
    cosq = din("cosq", [128, 2048])
    sinq = din("sinq", [128, 2048])
    maskd_d = din("maskd", [128, 2, 1024], BF16)
    maskf_d = din("maskf", [128, 2, 1024], BF16)
    identb_d = din("identb", [128, 128], BF16)
    identf_d = din("identf", [128, 128])
    tri_d = din("tri", [128, 128])
    onesf_d = din("onesf", [128, 128])
    g_attn = din("g_attn", [1, 1024])
    g_ffn = din("g_ffn", [1, 1024])
    g_fin = din("g_fin", [1, 1024])
    bfor = din("bfor", [1, 8])
    lam_d = din("lamv", [1, 256])
    csel_d = din("csel", [1, 8 * 33])
    dng = din("dng", [1, 128])
    w_out = din("w_out", [1024, 1024])
    rw_d = din("rw", [1024, 20])
    rb_d = din("rb", [1, 20])
    wg_d = din("wg", [16, 1024, 512])
    wu_d = din("wu", [16, 1024, 512])
    wd_d = din("wd", [16, 512, 1024])
    ebase_d = din("ebase", [128, 256])
    ustrict_d = din("ustrict", [128, 128], BF16)
    onesb_d = din("onesb", [128, 128], BF16)
    xs_d = nc.dram_tensor("xs_scratch", [NSLOT, 1024], BF16).ap()
    ys_d = nc.dram_tensor("ys_scratch", [NSLOT, 1024], F32).ap()
    out = nc.dram_tensor("out", [2048, 1024], F32, kind="ExternalOutput").ap()
    dbg = {}

    def dbg_out(name, shape, dt=F32):
        if name in debug:
            dbg[name] = nc.dram_tensor("dbg_" + name, list(shape), dt, kind="ExternalOutput").ap()
            return dbg[name]
        return None

    with ExitStack() as es:
        S = Sched(nc, es)

        uniq = [0]

        def sb(sc, name, shape, dt):
            uniq[0] += 1
            return sc.enter_context(nc.sbuf_tensor("s%d_%s" % (uniq[0], name), list(shape), dt))

        banks = [es.enter_context(nc.psum_tensor("bank%d" % i, [128, 512], F32)) for i in range(8)]
        Tb = [S.tile("bank%d" % i) for i in range(8)]
        Tdbg = S.tile("dbg")

        def dump(name, src_ap, reads):
            if name in dbg:
                S.dma("sync", dbg[name], src_ap, reads=reads, writes=[Tdbg], semtile=Tdbg)

        def bcast_load(dst, src, T, n):
            S.dma("sync", dst, src.broadcast_to([128, n]), writes=[T])

        bcreg = es.enter_context(nc.gpsimd.register("bcreg"))
        brregs = {e: es.enter_context(getattr(nc, e).register("br_" + e)) for e in ENGS}

        def set_bcreg():
            S.ops["gpsimd"].append(([], lambda e: e.reg_mov(bcreg, NSLOT - 1), None))

        identb = sb(es, "identb", [128, 128], BF16); Tidb = S.tile("identb")
        S.dma("sync", identb[:], identb_d, writes=[Tidb])
        gb_attn = sb(es, "gb_attn", [128, 1024], F32); Tgba = S.tile("gba")
        bcast_load(gb_attn[:], g_attn, Tgba, 1024)
        OT = sb(es, "OT", [128, 8, 2048], BF16)
        TOT = [S.tile("OT%d" % q) for q in range(16)]

        def make_sweep(sc, pfx, pT_banks):
            st = {}
            st["xt"] = [sb(sc, pfx + "xt%d" % i, [128, 1024], F32) for i in range(4)]
            st["Txt"] = [S.tile() for _ in range(4)]
            st["hb"] = [sb(sc, pfx + "hb%d" % i, [128, 1024], BF16) for i in range(2)]
            st["Thb"] = [S.tile() for _ in range(2)]
            st["hT"] = [sb(sc, pfx + "hT%d" % i, [128, 8, 512], BF16) for i in range(3)]
            st["ThT"] = [[S.tile() for _ in range(4)] for _ in range(3)]
            st["junk"] = sb(sc, pfx + "junk", [128, 1024], BF16)
            st["ss"] = [sb(sc, pfx + "ss%d" % i, [128, 4], F32) for i in range(3)]
            st["Tss"] = [[S.tile() for _ in range(4)] for _ in range(3)]
            st["sq"] = [sb(sc, pfx + "sq%d" % i, [128, 4], F32) for i in range(3)]
            st["Tsq"] = [S.tile() for _ in range(3)]
            st["rs"] = [sb(sc, pfx + "rs%d" % i, [128, 4], F32) for i in range(3)]
            st["Trs"] = [S.tile() for _ in range(3)]
            st["pT"] = pT_banks
            return st

        def sweep_stage1(st, x_ap, blk, gb, Tgb):
            b2 = blk % 3
            for tt in range(4):
                n = blk * 4 + tt
                xt, Txt = st["xt"][n % 4], st["Txt"][n % 4]
                S.dma("sync", xt[:], x_ap[n * 128:(n + 1) * 128, :], writes=[Txt])
                S.op("scalar", lambda e, xt=xt, tt=tt: e.activation(out=st["junk"][:], in_=xt[:], func=AF.Square,
                                                                    accum_out=st["ss"][b2][:, tt:tt + 1]),
                     reads=[Txt], writes=[st["Tss"][b2][tt]])
            S.op("scalar", lambda e: e.activation(out=st["sq"][b2][:], in_=st["ss"][b2][:], func=AF.Sqrt,
                                                  scale=1.0 / 1024, bias=EPS),
                 reads=st["Tss"][b2], writes=[st["Tsq"][b2]])
            S.op("vector", lambda e: e.reciprocal(out=st["rs"][b2][:], in_=st["sq"][b2][:]),
                 reads=[st["Tsq"][b2]], writes=[st["Trs"][b2]])
            def stt(tt):
                n = blk * 4 + tt
                xt, Txt = st["xt"][n % 4], st["Txt"][n % 4]
                hb, Thb = st["hb"][n % 2], st["Thb"][n % 2]
                S.op("vector", lambda e, xt=xt, hb=hb, tt=tt: e.scalar_tensor_tensor(
                    out=hb[:], in0=xt[:], scalar=st["rs"][b2][:, tt:tt + 1], in1=gb[:], op0=ALU.mult, op1=ALU.mult),
                    reads=[Txt, st["Trs"][b2], Tgb], writes=[Thb])

            def tr(tt):
                n = blk * 4 + tt
                hb, Thb = st["hb"][n % 2], st["Thb"][n % 2]
                bi = st["pT"][n % 2]
                pTv = banks[bi][:].bitcast(BF16)
                for c in range(8):
                    S.op("tensor", lambda e, c=c, hb=hb, pTv=pTv: e.transpose(
                        out=pTv[:, c * 128:(c + 1) * 128], in_=hb[:, c * 128:(c + 1) * 128], identity=identb[:]),
                        reads=[Thb, Tidb], writes=[Tb[bi]], signal=(c == 7))

            def ev(tt):
                n = blk * 4 + tt
                bi = st["pT"][n % 2]
                pTv = banks[bi][:].bitcast(BF16)
                S.op("vector", lambda e, tt=tt, pTv=pTv: e.tensor_copy(
                    out=st["hT"][b2][:, :, tt * 128:(tt + 1) * 128], in_=pTv.rearrange("p (c t) -> p c t", c=8)),
                    reads=[Tb[bi]], writes=[st["ThT"][b2][tt]])
            for f, a in ((stt, 0), (stt, 1), (tr, 0), (ev, 0), (stt, 2), (tr, 1), (ev, 1), (stt, 3), (tr, 2), (ev, 2), (tr, 3), (ev, 3)):
                f(a)

        def wload(dst, src_cols, T):
            S.dma("gpsimd", dst, src_cols.rearrange("(c p) n -> p c n", p=128), writes=[T])

        def mmgroup(out_ap, pairs, reads, writes):
            n = len(pairs)
            for k, (l, r) in enumerate(pairs):
                S.op("tensor", lambda e, l=l, r=r, k=k: e.matmul(out=out_ap, lhsT=l, rhs=r, start=(k == 0), stop=(k == n - 1)),
                     reads=reads, writes=writes, signal=(k == n - 1))

        def run_attention(units, PT, TPT, KT_of, QT_of, V_of, W, exp_emit, evac_emit, mask_emit, s_banks, o_banks,
                          OTs=None, TOTs=None, smr=None, Tsmr=None, identf=None, Tidf=None, Vsum_of=None):
            gctr = [0]
            dv = W - 1
            MO = dv if Vsum_of is not None else W
            deferred = []

            def st_list(ui, u):
                buf = ui % 2
                nk = nk_of(u["i"])
                L = []
                for p in range(nk // 2):
                    def f(p=p):
                        bi = s_banks[gctr[0] % len(s_banks)]
                        gctr[0] += 1
                        for j in range(2):
                            kb = 2 * p + j
                            kap, Tk = KT_of(u, kb)
                            qap, Tq = QT_of(u)
                            S.op("tensor", lambda e, kap=kap, qap=qap, j=j, bi=bi: e.matmul(
                                out=banks[bi][:, j * 256:(j + 1) * 256], lhsT=kap, rhs=qap, start=True, stop=True),
                                reads=[Tk, Tq], writes=[Tb[bi]], signal=(j == 1))
                        exp_emit(u, p, bi, PT[buf], TPT[buf][p])
                    L.append(f)
                L.append(lambda: mask_emit(u, PT[buf], TPT[buf]))
                return L

            def pv_list(ui, u):
                buf = ui % 2
                nk = nk_of(u["i"])
                ba = o_banks[(ui % 2) * 2]
                bb = o_banks[(ui % 2) * 2 + 1]
                o2 = ui % 2
                L = []
                for k0 in range(0, nk, 4):
                    def f(k0=k0):
                        for kb in range(k0, min(nk, k0 + 4)):
                            vap, Tv = V_of(u, kb)
                            S.op("tensor", lambda e, kb=kb, vap=vap: e.matmul(
                                out=banks[ba][0:MO, 0:256], lhsT=vap, rhs=PT[buf][:, kb, :], start=(kb == 0), stop=(kb == nk - 1)),
                                reads=[TPT[buf][kb // 2], Tv], writes=[Tb[ba]], signal=(kb == nk - 1))
                    L.append(f)
                if Vsum_of is not None:
                    for k0 in range(0, nk, 4):
                        def g(k0=k0):
                            for kb in range(k0, min(nk, k0 + 4)):
                                vap, Tv = Vsum_of(u, kb)
                                S.op("tensor", lambda e, kb=kb, vap=vap: e.matmul(
                                    out=banks[bb][0:1, 256:512], lhsT=vap, rhs=PT[buf][:, kb, :], start=(kb == 0), stop=(kb == nk - 1)),
                                    reads=[TPT[buf][kb // 2], Tv], writes=[Tb[bb]], signal=(kb == nk - 1))
                        L.append(g)

                def copy_out():
                    S.op("vector", lambda e: e.tensor_copy(out=OTs[o2][0:MO, :], in_=banks[ba][0:MO, 0:256]), reads=[Tb[ba]], writes=[TOTs[o2]])
                    if Vsum_of is not None:
                        S.op("vector", lambda e: e.tensor_copy(out=smr[o2][:], in_=banks[bb][0:1, 256:512]), reads=[Tb[bb]], writes=[Tsmr[o2]])

                def transpose_back():
                    for s in range(2):
                        last = (Vsum_of is None)
                        S.op("tensor", lambda e, s=s: e.transpose(out=banks[bb][:, s * W:s * W + MO], in_=OTs[o2][0:MO, s * 128:(s + 1) * 128],
                                                                  identity=identf[0:MO, 0:MO]),
                             reads=[TOTs[o2], Tidf], writes=[Tb[bb]], signal=(last and s == 1))
                        if Vsum_of is not None:
                            S.op("tensor", lambda e, s=s: e.transpose(out=banks[bb][:, s * W + dv:s * W + W], in_=smr[o2][0:1, s * 128:(s + 1) * 128],
                                                                      identity=identf[0:1, 0:1]),
                                 reads=[Tsmr[o2], Tidf], writes=[Tb[bb]], signal=(s == 1))
                    evac_emit(u, bb)
                L.append(copy_out)
                deferred.append(transpose_back)
                return L

            for ui in range(len(units) + 1):
                A = st_list(ui, units[ui]) if ui < len(units) else []
                B = pv_list(ui - 1, units[ui - 1]) if ui >= 1 else []
                if len(deferred) > (1 if ui >= 1 else 0):
                    B.insert(min(2, len(B)), deferred.pop(0))
                interleave(A, B)
            while deferred:
                deferred.pop(0)()

        with ExitStack() as pa:
            KT = sb(pa, "KTd", [128, 4, 4096], BF16)
            TKT = [[S.tile() for _ in range(8)] for _ in range(4)]
            QT = sb(pa, "QTd", [128, 4, 2048], BF16)
            TQT = [[S.tile() for _ in range(4)] for _ in range(4)]
            Vd = sb(pa, "Vd", [128, 32, 4, 130], BF16)
            TV = [S.tile() for _ in range(32)]
            Tvones = S.tile()
            S.op("vector", lambda e: e.memset(Vd[:, :, :, 128:130], 1.0), writes=TV)
            lamt = sb(pa, "lamt", [128, 256], F32); Tlam = S.tile()
            bcast_load(lamt[:], lam_d, Tlam, 256)
            lj = sb(pa, "lj", [128, 64], F32)
            ls = sb(pa, "ls", [128, 2], F32); Tls = S.tile()
            le = sb(pa, "le", [128, 2], F32); Tle = S.tile()
            neglam = sb(pa, "neglam", [128, 1], F32); Tnl = S.tile()
            for z in range(2):
                S.op("vector", lambda e, z=z: e.scalar_tensor_tensor(
                    out=lj[:], in0=lamt[:, z * 128:z * 128 + 64], scalar=1.0, in1=lamt[:, z * 128 + 64:z * 128 + 128],
                    op0=ALU.mult, op1=ALU.mult, accum_out=ls[:, z:z + 1]), reads=[Tlam, Tls], writes=[Tls])
            S.op("scalar", lambda e: e.activation(out=le[:], in_=ls[:], func=AF.Exp), reads=[Tls], writes=[Tle])
            S.op("vector", lambda e: e.tensor_tensor(out=neglam[:], in0=le[:, 1:2], in1=le[:, 0:1], op=ALU.subtract),
                 reads=[Tle], writes=[Tnl])
            S.op("vector", lambda e: e.tensor_scalar(out=neglam[:], in0=neglam[:], scalar1=-LAM_INIT, scalar2=None, op0=ALU.add),
                 reads=[Tnl], writes=[Tnl])
            gsc = sb(pa, "gsc", [128, 128], F32); Tgsc = S.tile()
            bcast_load(gsc[:], dng, Tgsc, 128)
            S.op("vector", lambda e: e.tensor_scalar(out=gsc[:], in0=gsc[:], scalar1=1.0 - LAM_INIT, scalar2=None, op0=ALU.mult),
                 reads=[Tgsc], writes=[Tgsc])
            Txs = S.tile("xs")

            with ExitStack() as sw:
                st = make_sweep(sw, "a", [0, 1])
                wA = sb(sw, "wA", [128, 8, 512], BF16); TwA = S.tile()
                wB = sb(sw, "wB", [128, 8, 512], BF16); TwB = S.tile()
                wC = sb(sw, "wC", [128, 8, 512], BF16); TwC = S.tile()
                wload(wA[:], w_in[:, 512:1024], TwA)
                wload(wB[:], wks_d, TwB)
                wload(wC[:], w_in[:, 1024:1536], TwC)
                ct = [sb(sw, "ct%d" % i, [128, 512], F32) for i in range(2)]; Tct = [S.tile() for _ in range(2)]
                sn = [sb(sw, "sn%d" % i, [128, 512], F32) for i in range(2)]; Tsn = [S.tile() for _ in range(2)]
                t1 = [sb(sw, "t1%d" % i, [128, 512], F32) for i in range(2)]; Tt1 = [S.tile() for _ in range(2)]
                t2 = [sb(sw, "t2%d" % i, [128, 512], F32) for i in range(2)]; Tt2 = [S.tile() for _ in range(2)]
                rctr = [0]

                def rope_proj(blk, hT, ThT, wq_, Tw_, ws_, Tws_, cos_d, sin_d, dstT, TdstT):
                    b2 = blk % 2
                    S.dma("sync", ct[b2][:], cos_d[:, blk * 512:(blk + 1) * 512], writes=[Tct[b2]])
                    S.dma("sync", sn[b2][:], sin_d[:, blk * 512:(blk + 1) * 512], writes=[Tsn[b2]])
                    for h in range(4):
                        ba, bb = (2, 3) if h % 2 == 0 else (4, 5)
                        mmgroup(banks[ba][:], [(wq_[:, c, h * 128:(h + 1) * 128], hT[:, c, :]) for c in range(8)],
                                reads=list(ThT) + [Tw_], writes=[Tb[ba]])
                        mmgroup(banks[bb][:], [(ws_[:, c, h * 128:(h + 1) * 128], hT[:, c, :]) for c in range(8)],
                                reads=list(ThT) + [Tws_], writes=[Tb[bb]])
                        r = rctr[0] % 2
                        rctr[0] += 1
                        S.op("vector", lambda e, r=r, ba=ba: e.tensor_tensor(out=t1[r][:], in0=banks[ba][:], in1=ct[b2][:], op=ALU.mult),
                             reads=[Tb[ba], Tct[b2]], writes=[Tt1[r]])
                        S.op("vector", lambda e, r=r, bb=bb: e.tensor_tensor(out=t2[r][:], in0=banks[bb][:], in1=sn[b2][:], op=ALU.mult),
                             reads=[Tb[bb], Tsn[b2]], writes=[Tt2[r]])
                        S.op("gpsimd", lambda e, r=r, h=h: e.tensor_tensor(out=dstT[:, h, blk * 512:(blk + 1) * 512], in0=t1[r][:], in1=t2[r][:], op=ALU.add),
                             reads=[Tt1[r], Tt2[r]], writes=[TdstT[h][blk]])

                def kv_proj(blk, hT, ThT):
                    rope_proj(blk, hT, ThT, wA, TwA, wB, TwB, cosk, sink, KT, TKT)
                    for tt in range(4):
                        n = blk * 4 + tt
                        bv = 6 + (tt % 2)
                        mmgroup(banks[bv][:], [(hT[:, c, tt * 128:(tt + 1) * 128], wC[:, c, :]) for c in range(8)],
                                reads=[ThT[tt], TwC], writes=[Tb[bv]])
                        S.op("scalar", lambda e, n=n, bv=bv: e.activation(
                            out=Vd[:, n, :, 0:128], in_=banks[bv][:].rearrange("p (h d) -> p h d", h=4), func=AF.Copy),
                            reads=[Tb[bv]], writes=[TV[n]])

                sweep_stage1(st, xb, 0, gb_attn, Tgba)
                sweep_stage1(st, xb, 1, gb_attn, Tgba)
                for blk in range(8):
                    if blk + 2 < 8:
                        sweep_stage1(st, xb, blk + 2, gb_attn, Tgba)
                    kv_proj(blk, st["hT"][blk % 3], st["ThT"][blk % 3])
                wload(wA[:], w_in[:, 0:512], TwA)
                wload(wB[:], wqs_d, TwB)
                sweep_stage1(st, xo, 0, gb_attn, Tgba)
                sweep_stage1(st, xo, 1, gb_attn, Tgba)
                for blk in range(4):
                    if blk + 2 < 4:
                        sweep_stage1(st, xo, blk + 2, gb_attn, Tgba)
                    rope_proj(blk, st["hT"][blk % 3], st["ThT"][blk % 3], wA, TwA, wB, TwB, cosq, sinq, QT, TQT)
                S.barrier()
                if "KTd" in debug:
                    dbg_out("KTd", [128, 4, 4096], BF16); dump("KTd", KT[:], [])
                    dbg_out("QTd", [128, 4, 2048], BF16); dump("QTd", QT[:], [])
                    dbg_out("Vd", [128, 32, 4, 130], BF16); dump("Vd", Vd[:], [])
                    S.barrier()
                S.emit()
            if stop_after == "Aproj":
                S.barrier(); S.emit()
                return nc

            with ExitStack() as at:
                if moe == "sparse":
                    zer = sb(at, "zer", [128, 2048], BF16); Tzer = S.tile()
                    S.op("vector", lambda e: e.memset(zer[:], 0.0), writes=[Tzer])
                    for n in range(NSLOT // 256):
                        S.dma("sync", xs_d[n * 256:(n + 1) * 256, :].rearrange("(p r) d -> p (r d)", r=2), zer[:], reads=[Tzer], writes=[Txs], semtile=Tzer)
                maskd = sb(at, "maskd", [128, 2, 1024], BF16); Tmd = S.tile()
                S.dma("sync", maskd[:], maskd_d, writes=[Tmd])
                PT = [sb(at, "PT%d" % i, [128, 32, 256], BF16) for i in range(2)]
                TPT = [[S.tile() for _ in range(16)] for _ in range(2)]
                oc = sb(at, "oc", [128, 16, 4, 128], F32); Toc = [[S.tile() for _ in range(4)] for _ in range(16)]
                ssq = sb(at, "ssq", [128, 64], F32); Tssq = S.tile()
                A1 = [sb(at, "A1%d" % i, [128, 128], F32) for i in range(2)]; TA1 = [S.tile() for _ in range(2)]
                rr = sb(at, "rr", [128, 4], F32); Trr = [S.tile() for _ in range(4)]
                sjunk = sb(at, "sjunk", [128, 128], F32)
                units = [dict(h=h, i=i, z=z) for h in range(4) for i in range(8) for z in range(2)]

                def KT_of(u, kb):
                    r0 = 64 * u["z"]
                    return KT[r0:r0 + 64, u["h"], kb * 128:(kb + 1) * 128], TKT[u["h"]][kb // 4]

                def QT_of(u):
                    r0 = 64 * u["z"]
                    return QT[r0:r0 + 64, u["h"], u["i"] * 256:(u["i"] + 1) * 256], TQT[u["h"]][u["i"] // 2]

                def V_of(u, kb):
                    return Vd[:, kb, u["h"], 0:128], TV[kb]

                def exp_emit(u, p, bi, PTb, Tp):
                    S.op("scalar", lambda e: e.activation(out=PTb[:, 2 * p:2 * p + 2, :].rearrange("p a b -> p (a b)"),
                                                          in_=banks[bi][:], func=AF.Exp),
                         reads=[Tb[bi]], writes=[Tp])

                def mask_emit(u, PTb, TPb):
                    i = u["i"]
                    lo = nk_of(i) - 4
                    S.op("vector", lambda e: e.tensor_tensor(out=PTb[:, lo:lo + 4, :], in0=PTb[:, lo:lo + 4, :],
                                                             in1=maskd[:, i % 2, :].rearrange("p (a b) -> p a b", a=4), op=ALU.min),
                         reads=[Tmd, TPb[lo // 2], TPb[lo // 2 + 1]], writes=[TPb[lo // 2], TPb[lo // 2 + 1]])

                def evac_emit(u, ob):
                    h, i, z = u["h"], u["i"], u["z"]
                    for s in range(2):
                        qb = 2 * i + s
                        o_ap = banks[ob][:, s * 129:s * 129 + 128]
                        sm_ap = banks[ob][:, s * 129 + 128:s * 129 + 129]
                        ri = 2 * z + s
                        S.op("vector", lambda e, ri=ri, sm_ap=sm_ap: e.reciprocal(out=rr[:, ri:ri + 1], in_=sm_ap),
                             reads=[Tb[ob]], writes=[Trr[ri]])
                        if z == 0:
                            S.op("vector", lambda e, ri=ri, o_ap=o_ap, s=s: e.tensor_scalar(
                                out=A1[s][:], in0=o_ap, scalar1=rr[:, ri:ri + 1], scalar2=None, op0=ALU.mult),
                                reads=[Tb[ob], Trr[ri]], writes=[TA1[s]])
                        else:
                            S.op("vector", lambda e, ri=ri: e.tensor_tensor(out=rr[:, ri:ri + 1], in0=rr[:, ri:ri + 1], in1=neglam[:], op=ALU.mult),
                                 reads=[Trr[ri], Tnl], writes=[Trr[ri]])
                            S.op("vector", lambda e, ri=ri, o_ap=o_ap, s=s, qb=qb: e.scalar_tensor_tensor(
                                out=oc[:, qb, h, :], in0=o_ap, scalar=rr[:, ri:ri + 1], in1=A1[s][:], op0=ALU.mult, op1=ALU.add),
                                reads=[Tb[ob], Trr[ri], TA1[s]], writes=[Toc[qb][h]])
                            S.op("vector", lambda e, qb=qb: e.scalar_tensor_tensor(
                                out=sjunk[:], in0=oc[:, qb, h, :], scalar=1.0, in1=oc[:, qb, h, :], op0=ALU.mult, op1=ALU.mult,
                                accum_out=ssq[:, qb * 4 + h:qb * 4 + h + 1]),
                                reads=[Toc[qb][h], Tssq], writes=[Tssq])

                OTs = [sb(at, "OTs%d" % i, [128, 256], F32) for i in range(2)]; TOTs = [S.tile() for _ in range(2)]
                smr = [sb(at, "smr%d" % i, [1, 256], F32) for i in range(2)]; Tsmr = [S.tile() for _ in range(2)]
                identf_a = sb(at, "identf_a", [128, 128], F32); Tidf_a = S.tile()
                S.dma("sync", identf_a[:], identf_d, writes=[Tidf_a])

                def Vsum_of(u, kb):
                    return Vd[:, kb, u["h"], 128:129], TV[kb]
                run_attention(units, PT, TPT, KT_of, QT_of, V_of, 129, exp_emit, evac_emit, mask_emit,
                              s_banks=[0, 1, 2, 3], o_banks=[4, 5, 6, 7], OTs=OTs, TOTs=TOTs, smr=smr, Tsmr=Tsmr,
                              identf=identf_a, Tidf=Tidf_a, Vsum_of=Vsum_of)
                S.op("scalar", lambda e: e.activation(out=ssq[:], in_=ssq[:], func=AF.Sqrt, scale=1.0 / 128, bias=EPS),
                     reads=[Tssq], writes=[Tssq])
                S.op("vector", lambda e: e.reciprocal(out=ssq[:], in_=ssq[:]), reads=[Tssq], writes=[Tssq])
                Otok = [sb(at, "Otok%d" % i, [128, 512], BF16) for i in range(2)]; TOtok = [S.tile() for _ in range(2)]
                for qb in range(16):
                    o2 = qb % 2
                    for h in range(4):
                        S.op("vector", lambda e, qb=qb, h=h, o2=o2: e.scalar_tensor_tensor(
                            out=Otok[o2][:, h * 128:(h + 1) * 128], in0=oc[:, qb, h, :], scalar=ssq[:, qb * 4 + h:qb * 4 + h + 1],
                            in1=gsc[:], op0=ALU.mult, op1=ALU.mult),
                            reads=[Toc[qb][h], Tssq, Tgsc], writes=[TOtok[o2]])
                    bi = o2
                    pTv = banks[bi][:].bitcast(BF16)
                    for c in range(4):
                        S.op("tensor", lambda e, c=c, o2=o2, pTv=pTv: e.transpose(
                            out=pTv[:, c * 128:(c + 1) * 128], in_=Otok[o2][:, c * 128:(c + 1) * 128], identity=identb[:]),
                            reads=[TOtok[o2], Tidb], writes=[Tb[bi]], signal=(c == 3))
                    S.op("vector", lambda e, qb=qb, pTv=pTv: e.tensor_copy(
                        out=OT[:, 0:4, qb * 128:(qb + 1) * 128], in_=pTv[:, 0:512].rearrange("p (c t) -> p c t", c=4)),
                        reads=[Tb[bi]], writes=[TOT[qb]])
                S.barrier()
                if "OTa" in debug:
                    dbg_out("OTa", [128, 8, 2048], BF16); dump("OTa", OT[:], []); S.barrier()
                S.emit()
        if stop_after == "A":
            return nc

        with ExitStack() as pb:
            KT = sb(pb, "KTf", [128, 4, 4096], BF16)
            TKT = [[S.tile() for _ in range(8)] for _ in range(4)]
            QT = sb(pb, "QTf", [128, 4, 2048], BF16)
            TQT = [[S.tile() for _ in range(4)] for _ in range(4)]
            Vf = sb(pb, "Vf", [128, 32, 8, 66], BF16)
            TV = [S.tile() for _ in range(32)]
            S.op("vector", lambda e: e.memset(Vf[:, :, :, 64:66], 1.0), writes=TV)
            zt = sb(pb, "zt", [128, 32, 8], F32); Tzt = [S.tile() for _ in range(32)]
            Fpos = sb(pb, "Fpos", [128, 32, 8], F32); TF = [S.tile() for _ in range(32)]
            Cpos = sb(pb, "Cpos", [128, 33, 8], F32); TC = [S.tile() for _ in range(33)]
            maskf = sb(pb, "maskf", [128, 2, 1024], BF16); Tmf = S.tile()
            S.dma("sync", maskf[:], maskf_d, writes=[Tmf])
            bfb = sb(pb, "bfb", [128, 8], F32); Tbfb = S.tile()
            bcast_load(bfb[:], bfor, Tbfb, 8)
            csel = sb(pb, "csel", [128, 8, 33], F32); Tcsel = S.tile()
            bcast_load(csel[:].rearrange("p a b -> p (a b)"), csel_d, Tcsel, 8 * 33)
            ctmp = sb(pb, "ctmp", [128, 8, 33], F32); Tctmp = S.tile()
            cq = sb(pb, "cq", [128, 8, 8], F32); Tcq = S.tile()
            tri = sb(pb, "tri", [128, 128], F32); Ttri = S.tile()
            S.dma("sync", tri[:], tri_d, writes=[Ttri])
            onesf = sb(pb, "onesf", [128, 128], F32); Tones = S.tile()
            S.dma("sync", onesf[:], onesf_d, writes=[Tones])

            with ExitStack() as sw:
                st = make_sweep(sw, "b", [0, 1])
                wA = sb(sw, "wA", [128, 8, 512], BF16); TwA = S.tile()
                wB = sb(sw, "wB", [128, 8, 512], BF16); TwB = S.tile()
                wF = sb(sw, "wF", [128, 8, 8], BF16); TwF = S.tile()
                wload(wA[:], w_in[:, 2048:2560], TwA)
                wload(wB[:], w_in[:, 2560:3072], TwB)
                wload(wF[:], w_in[:, 3072:3080], TwF)
                kctr = [0]

                def plain_proj(blk, hT, ThT, w_, Tw_, dstT, TdstT, scale):
                    for hp in range(4):
                        bk = 2 + (kctr[0] % 3)
                        kctr[0] += 1
                        mmgroup(banks[bk][:], [(w_[:, c, hp * 128:(hp + 1) * 128], hT[:, c, :]) for c in range(8)],
                                reads=list(ThT) + [Tw_], writes=[Tb[bk]])
                        S.op("scalar", lambda e, bk=bk, hp=hp: e.activation(
                            out=dstT[:, hp, blk * 512:(blk + 1) * 512], in_=banks[bk][:], func=AF.Copy, scale=scale),
                            reads=[Tb[bk]], writes=[TdstT[hp][blk]])

                def kvf_proj(blk, hT, ThT):
                    plain_proj(blk, hT, ThT, wA, TwA, KT, TKT, 1.0)
                    for tt in range(4):
                        n = blk * 4 + tt
                        bv = 6 + (tt % 2)
                        mmgroup(banks[bv][:], [(hT[:, c, tt * 128:(tt + 1) * 128], wB[:, c, :]) for c in range(8)],
                                reads=[ThT[tt], TwB], writes=[Tb[bv]])
                        S.op("vector", lambda e, n=n, bv=bv: e.tensor_copy(
                            out=Vf[:, n, :, 0:64], in_=banks[bv][:].rearrange("p (h d) -> p h d", h=8)),
                            reads=[Tb[bv]], writes=[TV[n]])
                        mmgroup(banks[5][:, 0:8], [(hT[:, c, tt * 128:(tt + 1) * 128], wF[:, c, :]) for c in range(8)],
                                reads=[ThT[tt], TwF], writes=[Tb[5]])
                        S.op("vector", lambda e, n=n: e.tensor_tensor(out=zt[:, n, :], in0=banks[5][:, 0:8], in1=bfb[:], op=ALU.add),
                             reads=[Tb[5], Tbfb], writes=[Tzt[n]])

                sweep_stage1(st, xb, 0, gb_attn, Tgba)
                sweep_stage1(st, xb, 1, gb_attn, Tgba)
                for blk in range(8):
                    if blk + 2 < 8:
                        sweep_stage1(st, xb, blk + 2, gb_attn, Tgba)
                    kvf_proj(blk, st["hT"][blk % 3], st["ThT"][blk % 3])
                wload(wA[:], w_in[:, 1536:2048], TwA)
                sweep_stage1(st, xo, 0, gb_attn, Tgba)
                sweep_stage1(st, xo, 1, gb_attn, Tgba)
                for blk in range(4):
                    if blk + 2 < 4:
                        sweep_stage1(st, xo, blk + 2, gb_attn, Tgba)
                    plain_proj(blk, st["hT"][blk % 3], st["ThT"][blk % 3], wA, TwA, QT, TQT, 0.125)
                ztf = zt[:].rearrange("p a b -> p (a b)")
                S.op("scalar", lambda e: e.activation(out=ztf, in_=ztf, func=AF.Exp, scale=-1.0), reads=Tzt, writes=Tzt)
                S.op("scalar", lambda e: e.activation(out=ztf, in_=ztf, func=AF.Ln, bias=1.0), reads=Tzt, writes=Tzt)
                S.op("vector", lambda e: e.memset(Cpos[:, 0, :], 0.0), writes=[TC[0]])
                for n in range(32):
                    bc = 2 + (n % 2)
                    S.op("tensor", lambda e, n=n, bc=bc: e.matmul(out=banks[bc][:, 0:8], lhsT=tri[:], rhs=zt[:, n, :], start=True, stop=True),
                         reads=[Ttri, Tzt[n]], writes=[Tb[bc]], signal=False)
                    S.op("tensor", lambda e, n=n, bc=bc: e.matmul(out=banks[bc][:, 8:16], lhsT=onesf[:], rhs=zt[:, n, :], start=True, stop=True),
                         reads=[Tones, Tzt[n]], writes=[Tb[bc]], signal=True)
                    S.op("vector", lambda e, n=n, bc=bc: e.tensor_tensor(out=Fpos[:, n, :], in0=banks[bc][:, 0:8], in1=Cpos[:, n, :], op=ALU.add),
                         reads=[Tb[bc], TC[n]], writes=[TF[n]])
                    S.op("vector", lambda e, n=n, bc=bc: e.tensor_tensor(out=Cpos[:, n + 1, :], in0=banks[bc][:, 8:16], in1=Cpos[:, n, :], op=ALU.add),
                         reads=[Tb[bc], TC[n]], writes=[TC[n + 1]])
                for i in range(8):
                    S.op("vector", lambda e, i=i: e.tensor_tensor(out=ctmp[:], in0=Cpos[:].rearrange("p n h -> p h n"),
                                                                  in1=csel[:, i, :].unsqueeze(1).broadcast_to([128, 8, 33]), op=ALU.mult),
                         reads=TC + [Tcsel, Tctmp], writes=[Tctmp])
                    S.op("vector", lambda e, i=i: e.tensor_reduce(out=cq[:, i, :], in_=ctmp[:], axis=AX.X, op=ALU.add),
                         reads=[Tctmp], writes=[Tcq])
                S.barrier()
                if "Fpos" in debug:
                    dbg_out("Fpos", [128, 32, 8]); dump("Fpos", Fpos[:], []); S.barrier()
                S.emit()

            with ExitStack() as at:
                PT = [sb(at, "PT%d" % i, [128, 32, 256], BF16) for i in range(2)]
                TPT = [[S.tile() for _ in range(16)] for _ in range(2)]
                Otf = sb(at, "Otf", [128, 16, 512], BF16); TOtf = [S.tile() for _ in range(16)]
                rr = sb(at, "rrf", [128, 2], F32); Trr = [S.tile() for _ in range(2)]
                biasb = [sb(at, "biasb%d" % i, [128, 32], F32) for i in range(2)]; Tbias = [S.tile() for _ in range(2)]
                units = [dict(hp=hp, hh=hh, i=i, head=2 * hp + hh) for hp in range(4) for hh in range(2) for i in range(8)]
                for ui, u in enumerate(units):
                    u["ui"] = ui

                def KT_of(u, kb):
                    r0 = 64 * u["hh"]
                    return KT[r0:r0 + 64, u["hp"], kb * 128:(kb + 1) * 128], TKT[u["hp"]][kb // 4]

                def QT_of(u):
                    r0 = 64 * u["hh"]
                    return QT[r0:r0 + 64, u["hp"], u["i"] * 256:(u["i"] + 1) * 256], TQT[u["hp"]][u["i"] // 2]

                def V_of(u, kb):
                    return Vf[:, kb, u["head"], 0:65], TV[kb]

                def exp_emit(u, p, bi, PTb, Tp):
                    b2 = u["ui"] % 2
                    hd = u["head"]
                    nk = nk_of(u["i"])
                    if p == 0:
                        S.op("vector", lambda e: e.tensor_scalar(out=biasb[b2][:, 0:nk], in0=Fpos[:, 0:nk, hd], scalar1=cq[:, u["i"], hd:hd + 1],
                                                                 scalar2=None, op0=ALU.subtract),
                             reads=TF[0:nk] + [Tcq], writes=[Tbias[b2]])
                    for j in range(2):
                        kb = 2 * p + j
                        S.op("scalar", lambda e, kb=kb, j=j: e.activation(out=PTb[:, kb, :], in_=banks[bi][:, j * 256:(j + 1) * 256],
                                                                          func=AF.Exp, bias=biasb[b2][:, kb:kb + 1]),
                             reads=[Tb[bi], Tbias[b2]], writes=[Tp])

                def mask_emit(u, PTb, TPb):
                    i = u["i"]
                    lo = nk_of(i) - 4
                    S.op("vector", lambda e: e.tensor_tensor(out=PTb[:, lo:lo + 4, :], in0=PTb[:, lo:lo + 4, :],
                                                             in1=maskf[:, i % 2, :].rearrange("p (a b) -> p a b", a=4), op=ALU.min),
                         reads=[Tmf, TPb[lo // 2], TPb[lo // 2 + 1]], writes=[TPb[lo // 2], TPb[lo // 2 + 1]])

                def evac_emit(u, ob):
                    hd, i = u["head"], u["i"]
                    for s in range(2):
                        qb = 2 * i + s
                        S.op("vector", lambda e, s=s: e.reciprocal(out=rr[:, s:s + 1], in_=banks[ob][:, s * 65 + 64:s * 65 + 65]),
                             reads=[Tb[ob]], writes=[Trr[s]])
                        S.op("vector", lambda e, s=s, qb=qb: e.tensor_scalar(
                            out=Otf[:, qb, hd * 64:(hd + 1) * 64], in0=banks[ob][:, s * 65:s * 65 + 64], scalar1=rr[:, s:s + 1],
                            scalar2=None, op0=ALU.mult),
                            reads=[Tb[ob], Trr[s]], writes=[TOtf[qb]])

                OTs = [sb(at, "OTsf%d" % i, [128, 256], F32) for i in range(2)]; TOTs = [S.tile() for _ in range(2)]
                identf_b = sb(at, "identf_b", [128, 128], F32); Tidf_b = S.tile()
                S.dma("sync", identf_b[:], identf_d, writes=[Tidf_b])
                run_attention(units, PT, TPT, KT_of, QT_of, V_of, 65, exp_emit, evac_emit, mask_emit,
                              s_banks=[0, 1, 2, 3], o_banks=[4, 5, 6, 7], OTs=OTs, TOTs=TOTs, identf=identf_b, Tidf=Tidf_b)
                for qb in range(16):
                    bi = qb % 2
                    pTv = banks[bi][:].bitcast(BF16)
                    for c in range(4):
                        S.op("tensor", lambda e, c=c, qb=qb, pTv=pTv: e.transpose(
                            out=pTv[:, c * 128:(c + 1) * 128], in_=Otf[:, qb, c * 128:(c + 1) * 128], identity=identb[:]),
                            reads=[TOtf[qb], Tidb], writes=[Tb[bi]], signal=(c == 3))
                    S.op("vector", lambda e, qb=qb, pTv=pTv: e.tensor_copy(
                        out=OT[:, 4:8, qb * 128:(qb + 1) * 128], in_=pTv[:, 0:512].rearrange("p (c t) -> p c t", c=4)),
                        reads=[Tb[bi]], writes=[TOT[qb]])
                S.barrier()
                if "OTb" in debug:
                    dbg_out("OTb", [128, 8, 2048], BF16); dump("OTb", OT[:], []); S.barrier()
                S.emit()
        if stop_after == "B":
            return nc

        with ExitStack() as pc:
            x2 = sb(pc, "x2", [128, 16, 1024], F32); Tx2 = [[S.tile() for _ in range(2)] for _ in range(16)]
            hmT = OT
            ThmT = TOT
            ovf = sb(pc, "ovf", [128, 1], I32); Tovf = S.tile()
            w12 = sb(pc, "w12", [128, 2, 16], F32); Tw12 = S.tile()
            pos = sb(pc, "pos", [128, 2, 16], I32); Tpos = S.tile()
            comb = sb(pc, "comb", [128, 16, 16], F32); Tcomb = S.tile()
            junk = sb(pc, "junkc", [128, 1024], BF16)
            ssc = sb(pc, "ssc", [128, 16], F32); Tssc = [S.tile() for _ in range(16)]; Tsscall = S.tile()
            with ExitStack() as c1:
                wo = sb(c1, "wo", [128, 8, 1024], BF16); Two = S.tile()
                wload(wo[:], w_out, Two)
                xt = [sb(c1, "xc%d" % i, [128, 1024], F32) for i in range(2)]; Txt = [S.tile() for _ in range(2)]
                rw32 = sb(c1, "rw32", [128, 8, 20], F32); Trw = S.tile()
                S.dma("sync", rw32[:], rw_d.rearrange("(c p) n -> p c n", p=128), writes=[Trw])
                rbb = sb(c1, "rbb", [128, 20], F32); Trbb = S.tile()
                bcast_load(rbb[:], rb_d, Trbb, 20)
                gbf = sb(c1, "gbf", [128, 1024], F32); Tgbf = S.tile()
                bcast_load(gbf[:], g_ffn, Tgbf, 1024)
                identf = sb(c1, "identf", [128, 128], F32); Tidf = S.tile()
                S.dma("sync", identf[:], identf_d, writes=[Tidf])
                hm32 = [sb(c1, "hm32%d" % i, [128, 1024], F32) for i in range(2)]; Thm32 = [S.tile() for _ in range(2)]
                hmT32 = [sb(c1, "hmT32%d" % i, [128, 8, 128], F32) for i in range(2)]; ThmT32 = [S.tile() for _ in range(2)]
                Lall = sb(c1, "Lall", [128, 16, 20], F32); TL = [S.tile() for _ in range(16)]
                if moe == "sparse":
                    hmb = sb(c1, "hmb", [128, 16, 1024], BF16); Thmb = [S.tile() for _ in range(16)]
                for t in range(16):
                    S.dma("sync", xt[t % 2][:], xo[t * 128:(t + 1) * 128, :], writes=[Txt[t % 2]])
                    for hf in range(2):
                        mmgroup(banks[hf][:], [(OT[:, c, t * 128:(t + 1) * 128], wo[:, c, hf * 512:(hf + 1) * 512]) for c in range(8)],
                                reads=[TOT[t], Two], writes=[Tb[hf]])
                        S.op("vector", lambda e, t=t, hf=hf: e.tensor_tensor(
                            out=x2[:, t, hf * 512:(hf + 1) * 512], in0=banks[hf][:], in1=xt[t % 2][:, hf * 512:(hf + 1) * 512], op=ALU.add),
                            reads=[Tb[hf], Txt[t % 2]], writes=[Tx2[t][hf]])
                    S.op("scalar", lambda e, t=t: e.activation(out=junk[:], in_=x2[:, t, :], func=AF.Square, accum_out=ssc[:, t:t + 1]),
                         reads=Tx2[t], writes=[Tssc[t]])
                if "x2" in debug:
                    dbg_out("x2", [2048, 1024])
                    for t in range(16):
                        dump("x2", x2[:, t, :], Tx2[t]) if False else S.dma("sync", dbg["x2"][t * 128:(t + 1) * 128, :], x2[:, t, :], reads=Tx2[t], writes=[Tdbg], semtile=Tdbg)
                if stop_after == "C0":
                    S.barrier(); S.emit()
                    return nc
                S.op("scalar", lambda e: e.activation(out=ssc[:], in_=ssc[:], func=AF.Sqrt, scale=1.0 / 1024, bias=EPS),
                     reads=Tssc, writes=[Tsscall])
                S.op("vector", lambda e: e.reciprocal(out=ssc[:], in_=ssc[:]), reads=[Tsscall], writes=[Tsscall])
                for t in range(16):
                    t2 = t % 2
                    S.op("vector", lambda e, t=t, t2=t2: e.scalar_tensor_tensor(
                        out=hm32[t2][:], in0=x2[:, t, :], scalar=ssc[:, t:t + 1], in1=gbf[:], op0=ALU.mult, op1=ALU.mult),
                        reads=Tx2[t] + [Tsscall, Tgbf], writes=[Thm32[t2]])
                    ba, bb = (2, 3) if t2 == 0 else (4, 5)
                    for c in range(8):
                        bk = ba if c < 4 else bb
                        S.op("tensor", lambda e, c=c, t2=t2, bk=bk: e.transpose(
                            out=banks[bk][:, (c % 4) * 128:(c % 4 + 1) * 128], in_=hm32[t2][:, c * 128:(c + 1) * 128], identity=identf[:]),
                            reads=[Thm32[t2], Tidf], writes=[Tb[bk]], signal=(c % 4 == 3))
                    import os
                    CUT = int(os.environ.get("C1CUT", "9"))
                    if CUT < 2:
                        continue
                    for k, bk in enumerate((ba, bb)):
                        src = banks[bk][:].rearrange("p (c t) -> p c t", c=4)
                        S.op("scalar", lambda e, k=k, t2=t2, src=src: e.activation(out=hmT32[t2][:, 4 * k:4 * k + 4, :], in_=src, func=AF.Copy),
                             reads=[Tb[bk]], writes=[ThmT32[t2]])
                        S.op("vector", lambda e, k=k, t=t, t2=t2: e.tensor_copy(out=hmT[:, 4 * k:4 * k + 4, t * 128:(t + 1) * 128], in_=hmT32[t2][:, 4 * k:4 * k + 4, :]),
                             reads=[ThmT32[t2]], writes=[ThmT[t]])
                    if moe == "sparse":
                        S.op("gpsimd", lambda e, t=t, t2=t2: e.tensor_copy(out=hmb[:, t, :], in_=hm32[t2][:]), reads=[Thm32[t2]], writes=[Thmb[t]])
                    if CUT < 3:
                        continue
                    br = 6 + t2
                    mmgroup(banks[br][:, 0:20], [(hmT32[t2][:, c, :], rw32[:, c, :]) for c in range(8)],
                            reads=[ThmT32[t2], Trw], writes=[Tb[br]])
                    S.op("vector", lambda e, t=t, br=br: e.tensor_tensor(out=Lall[:, t, :], in0=banks[br][:, 0:20], in1=rbb[:], op=ALU.add),
                         reads=[Tb[br], Trbb], writes=[TL[t]])
                if stop_after == "C1a":
                    if "Lall" in debug:
                        dbg_out("Lall", [128, 320])
                        S.dma("sync", dbg["Lall"], Lall[:].rearrange("p a b -> p (a b)"), reads=[], writes=[Tdbg], semtile=Tdbg)
                    S.barrier(); S.emit()
                    return nc
                TR = S.tile()

                def rt(name, shape):
                    return sb(c1, "rt_" + name, shape, F32)
                gmax = rt("gmax", [128, 16]); gm = rt("gm", [128, 16, 4]); gd = rt("gd", [128, 16, 4])
                gsum = rt("gsum", [128, 16]); gw = rt("gw", [128, 16]); pen = rt("pen", [128, 16, 4])
                EL = rt("EL", [128, 16, 16]); EL2 = rt("EL2", [128, 16, 16]); m1 = rt("m1", [128, 16]); m2 = rt("m2", [128, 16])
                oh1 = rt("oh1", [128, 16, 16]); oh2 = rt("oh2", [128, 16, 16]); dd = rt("dd", [128, 16]); w1 = rt("w1", [128, 16]); w2 = rt("w2", [128, 16])
                LG = Lall[:, :, 0:4]
                LE4 = Lall[:, :, 4:20].rearrange("p t (g e) -> p t g e", g=4)
                EL4 = EL[:].rearrange("p t (g e) -> p t g e", g=4)

                def vop(fn, first=False):
                    S.op("vector", fn, reads=(TL + [TR]) if first else [TR], writes=[TR])

                def bc3(a, n):
                    return a[:].unsqueeze(2).broadcast_to([128, 16, n])
                vop(lambda e: e.tensor_reduce(out=gmax[:], in_=LG, axis=AX.X, op=ALU.max), first=True)
                vop(lambda e: e.tensor_tensor(out=gm[:], in0=LG, in1=bc3(gmax, 4), op=ALU.is_equal))
                vop(lambda e: e.tensor_tensor(out=gd[:], in0=LG, in1=bc3(gmax, 4), op=ALU.subtract))
                S.op("scalar", lambda e: e.activation(out=gd[:], in_=gd[:], func=AF.Exp), reads=[TR], writes=[TR])
                vop(lambda e: e.tensor_reduce(out=gsum[:], in_=gd[:], axis=AX.X, op=ALU.add))
                vop(lambda e: e.reciprocal(out=gw[:], in_=gsum[:]))
                vop(lambda e: e.tensor_scalar(out=pen[:], in0=gm[:], scalar1=1.0, scalar2=1e30, op0=ALU.subtract, op1=ALU.mult))
                vop(lambda e: e.tensor_tensor(out=EL4, in0=LE4, in1=gm[:].unsqueeze(3).broadcast_to([128, 16, 4, 4]), op=ALU.mult))
                vop(lambda e: e.tensor_tensor(out=EL4, in0=EL4, in1=pen[:].unsqueeze(3).broadcast_to([128, 16, 4, 4]), op=ALU.add))
                vop(lambda e: e.tensor_reduce(out=m1[:], in_=EL[:], axis=AX.X, op=ALU.max))
                vop(lambda e: e.tensor_tensor(out=oh1[:], in0=EL[:], in1=bc3(m1, 16), op=ALU.is_equal))
                vop(lambda e: e.scalar_tensor_tensor(out=EL2[:], in0=oh1[:], scalar=-1e30, in1=EL[:], op0=ALU.mult, op1=ALU.add))
                vop(lambda e: e.tensor_reduce(out=m2[:], in_=EL2[:], axis=AX.X, op=ALU.max))
                vop(lambda e: e.tensor_tensor(out=oh2[:], in0=EL2[:], in1=bc3(m2, 16), op=ALU.is_equal))
                vop(lambda e: e.tensor_tensor(out=dd[:], in0=m2[:], in1=m1[:], op=ALU.subtract))
                S.op("scalar", lambda e: e.activation(out=dd[:], in_=dd[:], func=AF.Exp), reads=[TR], writes=[TR])
                vop(lambda e: e.tensor_scalar(out=w1[:], in0=dd[:], scalar1=1.0, scalar2=None, op0=ALU.add))
                vop(lambda e: e.reciprocal(out=w1[:], in_=w1[:]))
                vop(lambda e: e.tensor_tensor(out=w1[:], in0=w1[:], in1=gw[:], op=ALU.mult))
                vop(lambda e: e.tensor_tensor(out=w2[:], in0=dd[:], in1=w1[:], op=ALU.mult))
                if moe == "sparse":
                    Mb = sb(c1, "Mb", [128, 16, 16], BF16)
                    ustrict = sb(c1, "ustrict", [128, 128], BF16); Tus = S.tile()
                    S.dma("sync", ustrict[:], ustrict_d, writes=[Tus])
                    onesb = sb(c1, "onesb", [128, 128], BF16); Tob_ = S.tile()
                    S.dma("sync", onesb[:], onesb_d, writes=[Tob_])
                    ebase = sb(c1, "ebase", [128, 16, 16], F32); Teb = S.tile()
                    S.dma("sync", ebase[:].rearrange("p a b -> p (a b)"), ebase_d, writes=[Teb])
                    slotf = rt("slotf", [128, 16, 16]); okf = rt("okf", [128, 16, 16]); posf = rt("posf", [128, 2, 16])
                    vop(lambda e: e.tensor_tensor(out=Mb[:], in0=oh1[:], in1=oh2[:], op=ALU.add))
                    for t in range(16):
                        prs = [(onesb[:], Mb[:, tp, :]) for tp in range(t)] + [(ustrict[:], Mb[:, t, :])]
                        n_ = len(prs)
                        for k_, (l_, r_) in enumerate(prs):
                            S.op("tensor", lambda e, l_=l_, r_=r_, k_=k_, n_=n_, t=t: e.matmul(out=banks[0][:, t * 16:(t + 1) * 16], lhsT=l_, rhs=r_,
                                                                                         start=(k_ == 0), stop=(k_ == n_ - 1)),
                                 reads=[TR, Tus, Tob_], writes=[Tb[0]], signal=(k_ == n_ - 1))
                    for tp in range(16):
                        S.op("tensor", lambda e, tp=tp: e.matmul(out=banks[1][:, 0:16], lhsT=onesb[:], rhs=Mb[:, tp, :], start=(tp == 0), stop=(tp == 15)),
                             reads=[TR, Tob_], writes=[Tb[1]], signal=(tp == 15))
                    cmax = rt("cmax", [128, 1])
                    S.op("vector", lambda e: e.tensor_reduce(out=cmax[:], in_=banks[1][:, 0:16], axis=AX.X, op=ALU.max), reads=[Tb[1], TR], writes=[TR])
                    import os as _os
                    thr = -1.0 if _os.environ.get("FORCE_DENSE") else float(CAP)
                    vop(lambda e: e.tensor_scalar(out=cmax[:], in0=cmax[:], scalar1=thr, scalar2=None, op0=ALU.is_gt))
                    S.op("vector", lambda e: e.tensor_copy(out=ovf[:], in_=cmax[:]), reads=[TR], writes=[Tovf])
                    rank = banks[0][:, 0:256].rearrange("p (a b) -> p a b", a=16)
                    S.op("vector", lambda e: e.tensor_tensor(out=slotf[:], in0=rank, in1=ebase[:], op=ALU.add), reads=[Tb[0], Teb, TR], writes=[TR])
                    vop(lambda e: e.tensor_scalar(out=okf[:], in0=slotf[:], scalar1=None, scalar2=None, op0=ALU.bypass) if False else
                        e.tensor_tensor(out=okf[:], in0=slotf[:], in1=ebase[:], op=ALU.subtract))
                    vop(lambda e: e.tensor_scalar(out=okf[:], in0=okf[:], scalar1=float(CAP), scalar2=1.0e6, op0=ALU.is_ge, op1=ALU.mult))
                    vop(lambda e: e.tensor_tensor(out=slotf[:], in0=slotf[:], in1=okf[:], op=ALU.add))
                    vop(lambda e: e.tensor_tensor(out=okf[:], in0=slotf[:], in1=oh1[:], op=ALU.mult))
                    vop(lambda e: e.tensor_reduce(out=posf[:, 0, :], in_=okf[:], axis=AX.X, op=ALU.add))
                    vop(lambda e: e.tensor_tensor(out=okf[:], in0=slotf[:], in1=oh2[:], op=ALU.mult))
                    vop(lambda e: e.tensor_reduce(out=posf[:, 1, :], in_=okf[:], axis=AX.X, op=ALU.add))
                    S.op("vector", lambda e: e.tensor_copy(out=pos[:], in_=posf[:]), reads=[TR], writes=[Tpos])
                    S.op("vector", lambda e: e.tensor_copy(out=w12[:, 0, :], in_=w1[:]), reads=[TR, Tw12], writes=[Tw12])
                    S.op("vector", lambda e: e.tensor_copy(out=w12[:, 1, :], in_=w2[:]), reads=[TR, Tw12], writes=[Tw12])
                    Tsc = [S.tile() for _ in range(32)]
                    set_bcreg()
                    for t in range(16):
                        for k_ in range(2):
                            S.dma_fn("gpsimd", lambda e, t=t, k_=k_: e.indirect_dma_start(
                                out=xs_d[:, :], out_offset=bass.IndirectOffsetOnAxis(ap=pos[:, k_, t:t + 1], axis=0),
                                in_=hmb[:, t, :], in_offset=None, bounds_check=bcreg, oob_is_err=False),
                                reads=[Thmb[t], Tpos, Txs], writes=[Tsc[2 * t + k_]], semtile=Thmb[t])
                vop(lambda e: e.tensor_tensor(out=oh1[:], in0=oh1[:], in1=bc3(w1, 16), op=ALU.mult))
                vop(lambda e: e.tensor_tensor(out=oh2[:], in0=oh2[:], in1=bc3(w2, 16), op=ALU.mult))
                S.op("vector", lambda e: e.tensor_tensor(out=comb[:], in0=oh1[:], in1=oh2[:], op=ALU.add), reads=[TR], writes=[Tcomb])
                S.barrier()
                if "comb" in debug:
                    dbg_out("comb", [128, 256])
                    S.dma("sync", dbg["comb"], comb[:].rearrange("p a b -> p (a b)"), reads=[Tcomb], writes=[Tdbg], semtile=Tdbg)
                    S.barrier()
                S.emit()
            if stop_after == "C1":
                return nc

            with ExitStack() as c2:
                wgb = [sb(c2, "wgb%d" % i, [128, 8, 512], BF16) for i in range(2)]; Twg4 = [[S.tile() for _ in range(4)] for _ in range(2)]
                wub = [sb(c2, "wub%d" % i, [128, 8, 512], BF16) for i in range(2)]; Twu4 = [[S.tile() for _ in range(4)] for _ in range(2)]
                wdb = [sb(c2, "wdb%d" % i, [128, 4, 1024], BF16) for i in range(2)]; Twd4 = [[S.tile() for _ in range(4)] for _ in range(2)]
                stg = [sb(c2, "stg%d" % i, [128, 1024], F32) for i in range(3)]; Tstg = [S.tile() for _ in range(3)]
                sq_ = [0]
                aT = [sb(c2, "aT%d" % i, [128, 4, 512], BF16) for i in range(2)]; TaT = [[S.tile() for _ in range(4)] for _ in range(2)]
                sg = [sb(c2, "sg%d" % i, [128, 512], F32) for i in range(2)]; Tsg = [S.tile() for _ in range(2)]
                xg = [sb(c2, "xg%d" % i, [128, 1024], BF16) for i in range(4)]; Txg = [S.tile() for _ in range(4)]
                xgT = [sb(c2, "xgT%d" % i, [128, 8, CAP], BF16) for i in range(2)]; TxgT = [[S.tile() for _ in range(CAP // 128)] for _ in range(2)]
                ysb = [sb(c2, "ysb%d" % i, [128, 1024], F32) for i in range(2)]; Tysb = [S.tile() for _ in range(2)]
                NJ = CAP // 128
                Tys = [S.tile() for _ in range(NEXP * NJ)]

                def w_steps(ex):
                    b2 = ex % 2
                    dmas, casts = [], []
                    for k in range(12):
                        def mk(k=k):
                            if k < 8:
                                srcw = (wg_d if k < 4 else wu_d)[ex]
                                kk = k % 4
                                src = srcw[kk * 256:(kk + 1) * 256, :].rearrange("(c p) n -> p c n", p=128)
                                dst_of = lambda: (wgb if k < 4 else wub)[b2][:, 2 * kk:2 * kk + 2, :]
                                Td = (Twg4 if k < 4 else Twu4)[b2][kk]
                                view = lambda t_: t_[:].rearrange("p (c n) -> p c n", c=2)
                            else:
                                kk = k - 8
                                src = wd_d[ex][kk * 128:(kk + 1) * 128, :]
                                dst_of = lambda: wdb[b2][:, kk, :]
                                Td = Twd4[b2][kk]
                                view = lambda t_: t_[:]
                            cell = {}

                            def d():
                                si = sq_[0] % 3
                                sq_[0] += 1
                                cell["si"] = si
                                S.dma("sync", view(stg[si]), src, writes=[Tstg[si]])

                            def c():
                                si = cell["si"]
                                sv = view(stg[si])
                                dstb = dst_of()
                                if k % 2 == 1:
                                    S.op("scalar", lambda e: e.activation(out=dstb, in_=sv, func=AF.Copy), reads=[Tstg[si]], writes=[Td])
                                else:
                                    S.op("vector", lambda e: e.tensor_copy(out=dstb, in_=sv), reads=[Tstg[si]], writes=[Td])
                            return d, c
                        d, c = mk()
                        dmas.append(d)
                        casts.append(c)
                    steps = dmas[0:3]
                    for k in range(12):
                        steps.append(casts[k])
                        if k + 3 < 12:
                            steps.append(dmas[k + 3])
                    return steps

                def load_w(ex):
                    for f in w_steps(ex):
                        f()

                def gate_up(b2, rhs_of, Trhs, width, gq, slot=None):
                    for ft in range(4):
                        bg, bu = (0, 1) if gq[0] % 2 == 0 else (2, 3)
                        s2 = gq[0] % 2
                        gq[0] += 1
                        mmgroup(banks[bg][:, 0:width], [(wgb[b2][:, c, ft * 128:(ft + 1) * 128], rhs_of(c)) for c in range(8)],
                                reads=Trhs + Twg4[b2], writes=[Tb[bg]])
                        mmgroup(banks[bu][:, 0:width], [(wub[b2][:, c, ft * 128:(ft + 1) * 128], rhs_of(c)) for c in range(8)],
                                reads=Trhs + Twu4[b2], writes=[Tb[bu]])
                        S.op("scalar", lambda e, bg=bg, s2=s2: e.activation(out=sg[s2][:, 0:width], in_=banks[bg][:, 0:width], func=AF.Silu),
                             reads=[Tb[bg]], writes=[Tsg[s2]])
                        S.op("vector", lambda e, bu=bu, s2=s2, ft=ft: e.tensor_tensor(out=aT[b2][:, ft, 0:width], in0=sg[s2][:, 0:width], in1=banks[bu][:, 0:width], op=ALU.mult),
                             reads=[Tsg[s2], Tb[bu]], writes=[TaT[b2][ft]])
                        if slot is not None:
                            slot()

                S.branch_begin()
                gq = [0]; yq = [0]; xq = [0]

                def prep(ex):
                    b2 = ex % 2
                    for j in range(NJ):
                        xi = xq[0] % 4
                        xq[0] += 1
                        r0 = ex * CAP + j * 128
                        S.dma("gpsimd", xg[xi][:], xs_d[r0:r0 + 128, :], reads=Tsc + [Txs], writes=[Txg[xi]])
                        bi = 6 + (xq[0] % 2)
                        pTv = banks[bi][:].bitcast(BF16)
                        for c in range(8):
                            S.op("tensor", lambda e, c=c, xi=xi, pTv=pTv: e.transpose(
                                out=pTv[:, c * 128:(c + 1) * 128], in_=xg[xi][:, c * 128:(c + 1) * 128], identity=identb[:]),
                                reads=[Txg[xi], Tidb], writes=[Tb[bi]], signal=(c == 7))
                        S.op("vector", lambda e, j=j, b2=b2, pTv=pTv: e.tensor_copy(
                            out=xgT[b2][:, :, j * 128:(j + 1) * 128], in_=pTv.rearrange("p (c t) -> p c t", c=8)),
                            reads=[Tb[bi]], writes=[TxgT[b2][j]])
                prep(0)
                load_w(0)
                for ex in range(NEXP):
                    b2 = ex % 2
                    wq = w_steps(ex + 1) if ex + 1 < NEXP else []

                    def pop(n):
                        for _ in range(n):
                            if wq:
                                wq.pop(0)()
                    pop(3)
                    gate_up(b2, lambda c, b2=b2: xgT[b2][:, c, :], TxgT[b2], CAP, gq, slot=lambda: pop(3))
                    if ex + 1 < NEXP:
                        prep(ex + 1)
                    for j in range(NJ):
                        y2 = yq[0] % 2
                        yq[0] += 1
                        for hf in range(2):
                            by = 4 + hf
                            mmgroup(banks[by][:], [(aT[b2][:, ft, j * 128:(j + 1) * 128], wdb[b2][:, ft, hf * 512:(hf + 1) * 512]) for ft in range(4)],
                                    reads=TaT[b2] + Twd4[b2], writes=[Tb[by]])
                            if hf == 0:
                                S.op("vector", lambda e, y2=y2, by=by: e.tensor_copy(out=ysb[y2][:, 0:512], in_=banks[by][:]),
                                     reads=[Tb[by]], writes=[Tysb[y2]])
                            else:
                                S.op("scalar", lambda e, y2=y2, by=by: e.activation(out=ysb[y2][:, 512:1024], in_=banks[by][:], func=AF.Copy),
                                     reads=[Tb[by], Tysb[y2]], writes=[Tysb[y2]])
                        r0 = ex * CAP + j * 128
                        S.dma("gpsimd", ys_d[r0:r0 + 128, :], ysb[y2][:], reads=[Tysb[y2]], writes=[Tys[ex * NJ + j]], semtile=Tysb[y2])
                        pop(3)
                    pop(99)
                S.barrier()
                ygl = []
                for wb in wgb + wub + wdb:
                    v = wb[:].rearrange("p a b -> p (a b)").bitcast(F32)
                    ygl += [v[:, 0:1024], v[:, 1024:2048]]
                Tyg = [S.tile() for _ in ygl]
                set_bcreg()
                def gath(i):
                    t, k_ = i // 2, i % 2
                    gi = i % len(ygl)
                    S.dma_fn("gpsimd", lambda e, t=t, k_=k_, gi=gi: e.indirect_dma_start(
                        out=ygl[gi], out_offset=None, in_=ys_d[:, :],
                        in_offset=bass.IndirectOffsetOnAxis(ap=pos[:, k_, t:t + 1], axis=0),
                        bounds_check=bcreg, oob_is_err=False),
                        reads=Tys + [Tpos], writes=[Tyg[gi]], semtile=Tyg[gi])

                def acc(i):
                    t, k_ = i // 2, i % 2
                    gi = i % len(ygl)
                    for hf in range(2):
                        S.op("vector", lambda e, t=t, k_=k_, gi=gi, hf=hf: e.scalar_tensor_tensor(
                            out=x2[:, t, hf * 512:(hf + 1) * 512], in0=ygl[gi][:, hf * 512:(hf + 1) * 512], scalar=w12[:, k_, t:t + 1],
                            in1=x2[:, t, hf * 512:(hf + 1) * 512], op0=ALU.mult, op1=ALU.add),
                            reads=[Tyg[gi], Tw12, Tx2[t][hf]], writes=[Tx2[t][hf]])
                depth = len(ygl) - 1
                for i in range(32 + depth):
                    if i < 32:
                        gath(i)
                    if i - depth >= 0:
                        acc(i - depth)
                S.branch_mid()
                gq = [0]; yq = [0]
                load_w(0)
                for ex in range(NEXP):
                    b2 = ex % 2
                    if ex + 1 < NEXP:
                        load_w(ex + 1)
                    for tb in range(4):
                        gate_up(b2, lambda c, tb=tb: hmT[:, c, tb * 512:(tb + 1) * 512], ThmT[tb * 4:tb * 4 + 4], 512, gq)
                        for tt in range(4):
                            t = tb * 4 + tt
                            for hf in range(2):
                                by = 4 + (yq[0] % 4)
                                yq[0] += 1
                                mmgroup(banks[by][:], [(aT[b2][:, ft, tt * 128:(tt + 1) * 128], wdb[b2][:, ft, hf * 512:(hf + 1) * 512]) for ft in range(4)],
                                        reads=TaT[b2] + Twd4[b2], writes=[Tb[by]])
                                S.op("vector", lambda e, t=t, hf=hf, by=by, ex=ex: e.scalar_tensor_tensor(
                                    out=x2[:, t, hf * 512:(hf + 1) * 512], in0=banks[by][:], scalar=comb[:, t, ex:ex + 1],
                                    in1=x2[:, t, hf * 512:(hf + 1) * 512], op0=ALU.mult, op1=ALU.add),
                                    reads=[Tb[by], Tcomb, Tx2[t][hf]], writes=[Tx2[t][hf]])
                S.branch_end(ovf[0:1, 0:1], brregs)
                S.emit()

            with ExitStack() as c3:
                gbn = sb(c3, "gbn", [128, 1024], F32); Tgbn = S.tile()
                bcast_load(gbn[:], g_fin, Tgbn, 1024)
                ob = [sb(c3, "ob%d" % i, [128, 1024], F32) for i in range(2)]; Tob = [S.tile() for _ in range(2)]
                Tout = S.tile()
                for t in range(16):
                    S.op("scalar", lambda e, t=t: e.activation(out=junk[:], in_=x2[:, t, :], func=AF.Square, accum_out=ssc[:, t:t + 1]),
                         reads=Tx2[t] + [Tsscall], writes=[Tssc[t]])
                S.op("scalar", lambda e: e.activation(out=ssc[:], in_=ssc[:], func=AF.Sqrt, scale=1.0 / 1024, bias=EPS),
                     reads=Tssc, writes=[Tsscall])
                S.op("vector", lambda e: e.reciprocal(out=ssc[:], in_=ssc[:]), reads=[Tsscall], writes=[Tsscall])
                for t in range(16):
                    S.op("vector", lambda e, t=t: e.scalar_tensor_tensor(
                        out=ob[t % 2][:], in0=x2[:, t, :], scalar=ssc[:, t:t + 1], in1=gbn[:], op0=ALU.mult, op1=ALU.mult),
                        reads=Tx2[t] + [Tsscall, Tgbn], writes=[Tob[t % 2]])
                    S.dma("sync", out[t * 128:(t + 1) * 128, :], ob[t % 2][:], reads=[Tob[t % 2]], writes=[Tout], semtile=Tob[t % 2])
                S.barrier()
                S.emit()
    return nc


def _const_tables():
    f32 = np.float32
    inv_freq = (f32(1.0) / (f32(10000.0) ** (np.arange(0, 64, 2, dtype=f32) / f32(64)))).astype(f32)
    pos = np.arange(4096, dtype=f32)
    ang = (pos[:, None] * inv_freq[None, :]).astype(f32)
    cos = np.cos(ang).astype(f32)
    sin = np.sin(ang).astype(f32)
    r = np.arange(128)
    dh = r % 64
    cosT = cos[:, dh % 32].T.copy()
    sgn = np.where(dh < 32, -1.0, 1.0).astype(f32)
    sinT = (sin[:, dh % 32].T * sgn[:, None]).astype(f32)
    return cosT, sinT


def _masks(hf):
    k = np.arange(128)[:, None, None]
    r = np.arange(4)[None, :, None]
    q = np.arange(256)[None, None, :]
    md = np.zeros((128, 2, 4, 256), np.float32)
    mf = np.zeros((128, 2, 4, 256), np.float32)
    for par in range(2):
        if par == 0:
            kb = r
            j = 0 if hf == 0 else 1
        else:
            kb = 4 + r
            j = 3 if hf == 0 else 2
        s = kb * 128 + k
        t = j * 256 + q
        mf[:, par] = np.where(s <= t, 3e38, 0.0)
        md[:, par] = np.where((s // 64) <= (t // 64), 3e38, 0.0)
    return (md.reshape(128, 2, 1024).astype(ml_dtypes.bfloat16),
            mf.reshape(128, 2, 1024).astype(ml_dtypes.bfloat16))


def own_tokens(hf):
    return np.concatenate([np.arange(j * 256, (j + 1) * 256) for j in own_qtiles(hf)])


def prep(inputs):
    f32 = np.float32
    x = np.asarray(inputs["x"], f32)
    w_in = np.ascontiguousarray(np.asarray(inputs["w_in"], f32)[0])

    def swap_cols(w):
        return np.ascontiguousarray(w.reshape(1024, 8, 2, 32)[:, :, ::-1, :].reshape(1024, 512))

    cosT, sinT = _const_tables()
    common = {
        "w_in": w_in,
        "wqs": swap_cols(w_in[:, 0:512]),
        "wks": swap_cols(w_in[:, 512:1024]),
        "cosk": cosT, "sink": sinT,
        "identb": np.eye(128, dtype=f32).astype(ml_dtypes.bfloat16),
        "identf": np.eye(128, dtype=f32),
        "tri": np.triu(np.ones((128, 128), f32)),
        "onesf": np.ones((128, 128), f32),
        "onesb": np.ones((128, 128), f32).astype(ml_dtypes.bfloat16),
        "ustrict": np.triu(np.ones((128, 128), f32), 1).astype(ml_dtypes.bfloat16),
        "ebase": np.ascontiguousarray(np.broadcast_to((np.arange(16, dtype=f32) * CAP)[None, None, :], (128, 16, 16)).reshape(128, 256)),
        "g_attn": np.asarray(inputs["norm_attn_g"], f32).reshape(1, 1024),
        "g_ffn": np.asarray(inputs["norm_ffn_g"], f32).reshape(1, 1024),
        "g_fin": np.asarray(inputs["norm_final_g"], f32).reshape(1, 1024),
        "bfor": np.asarray(inputs["b_forget"], f32).reshape(1, 8),
        "lamv": np.concatenate([np.asarray(inputs[k], f32).reshape(1, 64) for k in
                                ("lambda_q1", "lambda_k1", "lambda_q2", "lambda_k2")], axis=1),
        "dng": np.asarray(inputs["diff_norm_g"], f32).reshape(1, 128),
        "w_out": np.ascontiguousarray(np.asarray(inputs["w_out"], f32)[0]),
        "rw": np.ascontiguousarray(np.concatenate([np.asarray(inputs["router_group_w"], f32)[0],
                                                   np.asarray(inputs["router_expert_w"], f32)[0]], axis=1)),
        "rb": np.concatenate([np.asarray(inputs["router_group_b"], f32).reshape(1, 4),
                              np.asarray(inputs["router_expert_b"], f32).reshape(1, 16)], axis=1),
        "wg": np.ascontiguousarray(np.asarray(inputs["w_gate"], f32)[0]),
        "wu": np.ascontiguousarray(np.asarray(inputs["w_up"], f32)[0]),
        "wd": np.ascontiguousarray(np.asarray(inputs["w_down"], f32)[0]),
    }
    in_maps = []
    for c in range(8):
        b, hf = c // 2, c % 2
        tok = own_tokens(hf)
        md, mf = _masks(hf)
        m = dict(common)
        m["xb"] = np.ascontiguousarray(x[b])
        m["xo"] = np.ascontiguousarray(x[b][tok])
        m["cosq"] = np.ascontiguousarray(cosT[:, tok] * f32(0.125))
        m["sinq"] = np.ascontiguousarray(sinT[:, tok] * f32(0.125))
        cs = np.zeros((8, 33), f32)
        for i, j in enumerate(own_qtiles(hf)):
            cs[i, 2 * j + 1] = 1.0
        m["csel"] = cs.reshape(1, 8 * 33)
        m["maskd"] = md
        m["maskf"] = mf
        in_maps.append(m)
    return in_maps


def kernel(**inputs):
    in_maps = prep(inputs)
    nc = build()
    res = run_bass_kernel_spmd(nc, in_maps, core_ids=list(range(8)))
    out = np.zeros((4, 4096, 1024), np.float32)
    for c in range(8):
        b, hf = c // 2, c % 2
        out[b, own_tokens(hf)] = res.results[c]["out"]
    return out
```

```python
import numpy as np
import ml_dtypes
from contextlib import ExitStack
import concourse.bass as bass
import concourse.mybir as mybir
from concourse.bass_utils import run_bass_kernel_spmd

F32 = mybir.dt.float32
BF16 = mybir.dt.bfloat16
I32 = mybir.dt.int32
AF = mybir.ActivationFunctionType
ALU = mybir.AluOpType
AX = mybir.AxisListType

ENGS = ("sync", "scalar", "vector", "gpsimd", "tensor")
EPS = 1e-6
LAM_INIT = 0.8 - 0.6 * 1.0
NEXP = 16
CAP = 512
NSLOT = NEXP * CAP


class Tile:
    __slots__ = ("name", "last_w", "readers", "dsem")

    def __init__(self, name):
        self.name = name
        self.last_w = None
        self.readers = {}
        self.dsem = None


class Sched:
    def __init__(self, nc, es):
        self.nc = nc
        self.es = es
        self.ops = {e: [] for e in ENGS}
        self.sems = {}
        self.cnt = {}
        self.waited = {e: {} for e in ENGS}
        self.pending = {e: False for e in ENGS}
        for e in ENGS:
            self._mksem("E:" + e)
        self.n_dsem = 0
        self.nops = 0
        self.tiles = []

    def _mksem(self, key):
        self.sems[key] = self.es.enter_context(self.nc.semaphore(key.replace(":", "_")))
        self.cnt[key] = 0

    def tile(self, name="t"):
        t = Tile(name)
        self.tiles.append(t)
        return t

    def _snapshot(self):
        return (dict(self.cnt), {e: dict(w) for e, w in self.waited.items()},
                [(t, t.last_w, dict(t.readers)) for t in self.tiles])

    def _restore(self, snap):
        self.cnt = dict(snap[0])
        for k in self.sems:
            self.cnt.setdefault(k, 0)
        self.waited = {e: dict(w) for e, w in snap[1].items()}
        for t, lw, rd in snap[2]:
            t.last_w = lw
            t.readers = dict(rd)

    def branch_begin(self):
        self.barrier()
        self._outer_ops = self.ops
        self.ops = {e: [] for e in ENGS}
        self._snap = self._snapshot()

    def branch_mid(self):
        self.barrier()
        self._A = (self.ops, dict(self.cnt))
        self.ops = {e: [] for e in ENGS}
        self._restore(self._snap)

    def branch_end(self, flag_ap, regs):
        self.barrier()
        opsA, cntA = self._A
        opsB, cntB = self.ops, dict(self.cnt)
        target = {k: max(cntA.get(k, 0), cntB.get(k, 0)) for k in set(cntA) | set(cntB)}

        def pads(cntX):
            out = {e: [] for e in ENGS}
            for k, v in target.items():
                d = v - cntX.get(k, 0)
                if d > 0:
                    owner = k[2:] if k.startswith("E:") else "gpsimd"
                    out[owner].append((k, d))
            return out
        pA, pB = pads(cntA), pads(cntB)
        self.ops = self._outer_ops
        for e in ENGS:
            self.ops[e].append(("branch", flag_ap, regs[e], opsA[e], pA[e], opsB[e], pB[e]))
        self.cnt = target
        for e in ENGS:
            self.waited[e] = dict(target)
        for t in self.tiles:
            t.last_w = None
            t.readers = {}

    def dsem_for(self, t):
        if t.dsem is None:
            key = "D:%d" % self.n_dsem
            self.n_dsem += 1
            self._mksem(key)
            t.dsem = key
        return t.dsem

    def _need(self, eng, waits, key, val):
        if eng == "tensor" and key == "E:tensor":
            return
        if self.cnt[key] < val:
            raise RuntimeError("wait on un-signalled event %s %d (cnt %d) from %s" % (key, val, self.cnt[key], eng))
        if self.waited[eng].get(key, 0) >= val:
            return
        self.waited[eng][key] = val
        waits[key] = max(waits.get(key, 0), val)

    def _deps(self, eng, reads, writes):
        waits = {}
        for t in reads:
            if t.last_w is not None:
                self._need(eng, waits, *t.last_w)
        for t in writes:
            if t.last_w is not None:
                self._need(eng, waits, *t.last_w)
            for k, v in t.readers.items():
                self._need(eng, waits, k, v)
        return list(waits.items())

    def _record(self, ev, reads, writes):
        for t in writes:
            t.last_w = ev
            t.readers = {}
        for t in reads:
            if t not in writes:
                if t.readers.get(ev[0], 0) < ev[1]:
                    t.readers[ev[0]] = ev[1]

    def op(self, eng, fn, reads=(), writes=(), signal=True):
        waits = self._deps(eng, reads, writes)
        key = "E:" + eng
        if signal:
            self.cnt[key] += 1
            ev = (key, self.cnt[key])
            inc = (key, 1)
            self.pending[eng] = False
        else:
            ev = (key, self.cnt[key] + 1)
            inc = None
            self.pending[eng] = True
        self._record(ev, reads, writes)
        self.ops[eng].append((waits, fn, inc))
        self.nops += 1

    def dma(self, eng, out, in_, reads=(), writes=(), semtile=None, **kw):
        waits = self._deps(eng, reads, writes)
        if semtile is None:
            semtile = writes[0] if writes else reads[0]
        key = self.dsem_for(semtile)
        self.cnt[key] += 16
        ev = (key, self.cnt[key])
        self._record(ev, reads, writes)

        def fn(e, out=out, in_=in_, kw=kw):
            return e.dma_start(out=out, in_=in_, **kw)
        self.ops[eng].append((waits, fn, (key, 16)))
        self.nops += 1

    def dma_fn(self, eng, fn, reads=(), writes=(), semtile=None):
        waits = self._deps(eng, reads, writes)
        key = self.dsem_for(semtile)
        self.cnt[key] += 16
        ev = (key, self.cnt[key])
        self._record(ev, reads, writes)
        self.ops[eng].append((waits, fn, (key, 16)))
        self.nops += 1

    def barrier(self, engs=ENGS):
        for e in ENGS:
            assert not self.pending[e], e
        for e in engs:
            waits = {}
            for key, c in self.cnt.items():
                if c > 0:
                    self._need(e, waits, key, c)
            self.ops[e].append((list(waits.items()), None, None))

    def emit(self):
        nc = self.nc
        sems = self.sems
        ops = self.ops
        with nc.Block() as block:
            def replay(e, lst):
                for ent in lst:
                    if ent[0] == "branch":
                        _, flag_ap, reg, oA, pA, oB, pB = ent
                        e.reg_load(reg, flag_ap)
                        with e.If_eq(reg, 0):
                            replay(e, oA)
                            for k, d in pA:
                                e.sem_inc(sems[k], d)
                            e.nop()
                        with e.Else():
                            replay(e, oB)
                            for k, d in pB:
                                e.sem_inc(sems[k], d)
                            e.nop()
                        continue
                    waits, fn, inc = ent
                    for key, val in waits:
                        e.wait_ge(sems[key], val)
                    if fn is None:
                        continue
                    inst = fn(e)
                    if inc is not None:
                        inst.then_inc(sems[inc[0]], inc[1])

            def mk(name):
                def body(e):
                    replay(e, ops[name])
                return body
            block.sync(mk("sync"))
            block.scalar(mk("scalar"))
            block.vector(mk("vector"))
            block.gpsimd(mk("gpsimd"))
            block.tensor(mk("tensor"))
        self.ops = {e: [] for e in ENGS}


def own_qtiles(hf):
    js = []
    for m in range(4):
        js += ([4 * m, 4 * m + 3] if hf == 0 else [4 * m + 1, 4 * m + 2])
    return js


def nk_of(i):
    return 8 * (i // 2) + (4 if i % 2 == 0 else 8)


def interleave(A, B):
    a, b = len(A), len(B)
    if a == 0:
        for f in B:
            f()
        return
    done = 0
    for k, f in enumerate(A):
        f()
        upto = ((k + 1) * b) // a
        while done < upto:
            B[done]()
            done += 1
    while done < b:
        B[done]()
        done += 1


def build(debug=(), stop_after=None, moe="sparse"):
    nc = bass.Bass("TRN2", target_bir_lowering=False)

    def din(name, shape, dt=F32):
        return nc.dram_tensor(name, list(shape), dt, kind="ExternalInput").ap()

    xb = din("xb", [4096, 1024])
    xo = din("xo", [2048, 1024])
    w_in = din("w_in", [1024, 3080])
    wqs_d = din("wqs", [1024, 512])
    wks_d = din("wks", [1024, 512])
    cosk = din("cosk", [128, 4096])
    sink = din("sink", [128, 4096])
    cosq = din("cosq", [128, 2048])
    sinq = din("sinq", [128, 2048])
    maskd_d = din("maskd", [128, 2, 1024], BF16)
    maskf_d = din("maskf", [128, 2, 1024], BF16)
    identb_d = din("identb", [128, 128], BF16)
    identf_d = din("identf", [128, 128])
    tri_d = din("tri", [128, 128])
    onesf_d = din("onesf", [128, 128])
    g_attn = din("g_attn", [1, 1024])
    g_ffn = din("g_ffn", [1, 1024])
    g_fin = din("g_fin", [1, 1024])
    bfor = din("bfor", [1, 8])
    lam_d = din("lamv", [1, 256])
    csel_d = din("csel", [1, 8 * 33])
    dng = din("dng", [1, 128])
    w_out = din("w_out", [1024, 1024])
    rw_d = din("rw", [1024, 20])
    rb_d = din("rb", [1, 20])
    wg_d = din("wg", [16, 1024, 512])
    wu_d = din("wu", [16, 1024, 512])
    wd_d = din("wd", [16, 512, 1024])
    ebase_d = din("ebase", [128, 256])
    ustrict_d = din("ustrict", [128, 128], BF16)
    onesb_d = din("onesb", [128, 128], BF16)
    xs_d = nc.dram_tensor("xs_scratch", [NSLOT, 1024], BF16).ap()
    ys_d = nc.dram_tensor("ys_scratch", [NSLOT, 1024], F32).ap()
    out = nc.dram_tensor("out", [2048, 1024], F32, kind="ExternalOutput").ap()
    dbg = {}

    def dbg_out(name, shape, dt=F32):
        if name in debug:
            dbg[name] = nc.dram_tensor("dbg_" + name, list(shape), dt, kind="ExternalOutput").ap()
            return dbg[name]
        return None

    with ExitStack() as es:
        S = Sched(nc, es)

        uniq = [0]

        def sb(sc, name, shape, dt):
            uniq[0] += 1
            return sc.enter_context(nc.sbuf_tensor("s%d_%s" % (uniq[0], name), list(shape), dt))

        banks = [es.enter_context(nc.psum_tensor("bank%d" % i, [128, 512], F32)) for i in range(8)]
        Tb = [S.tile("bank%d" % i) for i in range(8)]
        Tdbg = S.tile("dbg")

        def dump(name, src_ap, reads):
            if name in dbg:
                S.dma("sync", dbg[name], src_ap, reads=reads, writes=[Tdbg], semtile=Tdbg)

        def bcast_load(dst, src, T, n):
            S.dma("sync", dst, src.broadcast_to([128, n]), writes=[T])

        bcreg = es.enter_context(nc.gpsimd.register("bcreg"))
        brregs = {e: es.enter_context(getattr(nc, e).register("br_" + e)) for e in ENGS}

        def set_bcreg():
            S.ops["gpsimd"].append(([], lambda e: e.reg_mov(bcreg, NSLOT - 1), None))

        identb = sb(es, "identb", [128, 128], BF16); Tidb = S.tile("identb")
        S.dma("sync", identb[:], identb_d, writes=[Tidb])
        gb_attn = sb(es, "gb_attn", [128, 1024], F32); Tgba = S.tile("gba")
        bcast_load(gb_attn[:], g_attn, Tgba, 1024)
        OT = sb(es, "OT", [128, 8, 2048], BF16)
        TOT = [S.tile("OT%d" % q) for q in range(16)]

        def make_sweep(sc, pfx, pT_banks):
            st = {}
            st["xt"] = [sb(sc, pfx + "xt%d" % i, [128, 1024], F32) for i in range(4)]
            st["Txt"] = [S.tile() for _ in range(4)]
            st["hb"] = [sb(sc, pfx + "hb%d" % i, [128, 1024], BF16) for i in range(2)]
            st["Thb"] = [S.tile() for _ in range(2)]
            st["hT"] = [sb(sc, pfx + "hT%d" % i, [128, 8, 512], BF16) for i in range(3)]
            st["ThT"] = [[S.tile() for _ in range(4)] for _ in range(3)]
            st["junk"] = sb(sc, pfx + "junk", [128, 1024], BF16)
            st["ss"] = [sb(sc, pfx + "ss%d" % i, [128, 4], F32) for i in range(3)]
            st["Tss"] = [[S.tile() for _ in range(4)] for _ in range(3)]
            st["sq"] = [sb(sc, pfx + "sq%d" % i, [128, 4], F32) for i in range(3)]
            st["Tsq"] = [S.tile() for _ in range(3)]
            st["rs"] = [sb(sc, pfx + "rs%d" % i, [128, 4], F32) for i in range(3)]
            st["Trs"] = [S.tile() for _ in range(3)]
            st["pT"] = pT_banks
            return st

        def sweep_stage1(st, x_ap, blk, gb, Tgb):
            b2 = blk % 3
            for tt in range(4):
                n = blk * 4 + tt
                xt, Txt = st["xt"][n % 4], st["Txt"][n % 4]
                S.dma("sync", xt[:], x_ap[n * 128:(n + 1) * 128, :], writes=[Txt])
                S.op("scalar", lambda e, xt=xt, tt=tt: e.activation(out=st["junk"][:], in_=xt[:], func=AF.Square,
                                                                    accum_out=st["ss"][b2][:, tt:tt + 1]),
                     reads=[Txt], writes=[st["Tss"][b2][tt]])
            S.op("scalar", lambda e: e.activation(out=st["sq"][b2][:], in_=st["ss"][b2][:], func=AF.Sqrt,
                                                  scale=1.0 / 1024, bias=EPS),
                 reads=st["Tss"][b2], writes=[st["Tsq"][b2]])
            S.op("vector", lambda e: e.reciprocal(out=st["rs"][b2][:], in_=st["sq"][b2][:]),
                 reads=[st["Tsq"][b2]], writes=[st["Trs"][b2]])
            def stt(tt):
                n = blk * 4 + tt
                xt, Txt = st["xt"][n % 4], st["Txt"][n % 4]
                hb, Thb = st["hb"][n % 2], st["Thb"][n % 2]
                S.op("vector", lambda e, xt=xt, hb=hb, tt=tt: e.scalar_tensor_tensor(
                    out=hb[:], in0=xt[:], scalar=st["rs"][b2][:, tt:tt + 1], in1=gb[:], op0=ALU.mult, op1=ALU.mult),
                    reads=[Txt, st["Trs"][b2], Tgb], writes=[Thb])

            def tr(tt):
                n = blk * 4 + tt
                hb, Thb = st["hb"][n % 2], st["Thb"][n % 2]
                bi = st["pT"][n % 2]
                pTv = banks[bi][:].bitcast(BF16)
                for c in range(8):
                    S.op("tensor", lambda e, c=c, hb=hb, pTv=pTv: e.transpose(
                        out=pTv[:, c * 128:(c + 1) * 128], in_=hb[:, c * 128:(c + 1) * 128], identity=identb[:]),
                        reads=[Thb, Tidb], writes=[Tb[bi]], signal=(c == 7))

            def ev(tt):
                n = blk * 4 + tt
                bi = st["pT"][n % 2]
                pTv = banks[bi][:].bitcast(BF16)
                S.op("vector", lambda e, tt=tt, pTv=pTv: e.tensor_copy(
                    out=st["hT"][b2][:, :, tt * 128:(tt + 1) * 128], in_=pTv.rearrange("p (c t) -> p c t", c=8)),
                    reads=[Tb[bi]], writes=[st["ThT"][b2][tt]])
            for f, a in ((stt, 0), (stt, 1), (tr, 0), (ev, 0), (stt, 2), (tr, 1), (ev, 1), (stt, 3), (tr, 2), (ev, 2), (tr, 3), (ev, 3)):
                f(a)

        def wload(dst, src_cols, T):
            S.dma("gpsimd", dst, src_cols.rearrange("(c p) n -> p c n", p=128), writes=[T])

        def mmgroup(out_ap, pairs, reads, writes):
            n = len(pairs)
            for k, (l, r) in enumerate(pairs):
                S.op("tensor", lambda e, l=l, r=r, k=k: e.matmul(out=out_ap, lhsT=l, rhs=r, start=(k == 0), stop=(k == n - 1)),
                     reads=reads, writes=writes, signal=(k == n - 1))

        def run_attention(units, PT, TPT, KT_of, QT_of, V_of, W, exp_emit, evac_emit, mask_emit, s_banks, o_banks,
                          OTs=None, TOTs=None, smr=None, Tsmr=None, identf=None, Tidf=None, Vsum_of=None):
            gctr = [0]
            dv = W - 1
            MO = dv if Vsum_of is not None else W
            deferred = []

            def st_list(ui, u):
                buf = ui % 2
                nk = nk_of(u["i"])
                L = []
                for p in range(nk // 2):
                    def f(p=p):
                        bi = s_banks[gctr[0] % len(s_banks)]
                        gctr[0] += 1
                        for j in range(2):
                            kb = 2 * p + j
                            kap, Tk = KT_of(u, kb)
                            qap, Tq = QT_of(u)
                            S.op("tensor", lambda e, kap=kap, qap=qap, j=j, bi=bi: e.matmul(
                                out=banks[bi][:, j * 256:(j + 1) * 256], lhsT=kap, rhs=qap, start=True, stop=True),
                                reads=[Tk, Tq], writes=[Tb[bi]], signal=(j == 1))
                        exp_emit(u, p, bi, PT[buf], TPT[buf][p])
                    L.append(f)
                L.append(lambda: mask_emit(u, PT[buf], TPT[buf]))
                return L

            def pv_list(ui, u):
                buf = ui % 2
                nk = nk_of(u["i"])
                ba = o_banks[(ui % 2) * 2]
                bb = o_banks[(ui % 2) * 2 + 1]
                o2 = ui % 2
                L = []
                for k0 in range(0, nk, 4):
                    def f(k0=k0):
                        for kb in range(k0, min(nk, k0 + 4)):
                            vap, Tv = V_of(u, kb)
                            S.op("tensor", lambda e, kb=kb, vap=vap: e.matmul(
                                out=banks[ba][0:MO, 0:256], lhsT=vap, rhs=PT[buf][:, kb, :], start=(kb == 0), stop=(kb == nk - 1)),
                                reads=[TPT[buf][kb // 2], Tv], writes=[Tb[ba]], signal=(kb == nk - 1))
                    L.append(f)
                if Vsum_of is not None:
                    for k0 in range(0, nk, 4):
                        def g(k0=k0):
                            for kb in range(k0, min(nk, k0 + 4)):
                                vap, Tv = Vsum_of(u, kb)
                                S.op("tensor", lambda e, kb=kb, vap=vap: e.matmul(
                                    out=banks[bb][0:1, 256:512], lhsT=vap, rhs=PT[buf][:, kb, :], start=(kb == 0), stop=(kb == nk - 1)),
                                    reads=[TPT[buf][kb // 2], Tv], writes=[Tb[bb]], signal=(kb == nk - 1))
                        L.append(g)

                def copy_out():
                    S.op("vector", lambda e: e.tensor_copy(out=OTs[o2][0:MO, :], in_=banks[ba][0:MO, 0:256]), reads=[Tb[ba]], writes=[TOTs[o2]])
                    if Vsum_of is not None:
                        S.op("vector", lambda e: e.tensor_copy(out=smr[o2][:], in_=banks[bb][0:1, 256:512]), reads=[Tb[bb]], writes=[Tsmr[o2]])

                def transpose_back():
                    for s in range(2):
                        last = (Vsum_of is None)
                        S.op("tensor", lambda e, s=s: e.transpose(out=banks[bb][:, s * W:s * W + MO], in_=OTs[o2][0:MO, s * 128:(s + 1) * 128],
                                                                  identity=identf[0:MO, 0:MO]),
                             reads=[TOTs[o2], Tidf], writes=[Tb[bb]], signal=(last and s == 1))
                        if Vsum_of is not None:
                            S.op("tensor", lambda e, s=s: e.transpose(out=banks[bb][:, s * W + dv:s * W + W], in_=smr[o2][0:1, s * 128:(s + 1) * 128],
                                                                      identity=identf[0:1, 0:1]),
                                 reads=[Tsmr[o2], Tidf], writes=[Tb[bb]], signal=(s == 1))
                    evac_emit(u, bb)
                L.append(copy_out)
                deferred.append(transpose_back)
                return L

            for ui in range(len(units) + 1):
                A = st_list(ui, units[ui]) if ui < len(units) else []
                B = pv_list(ui - 1, units[ui - 1]) if ui >= 1 else []
                if len(deferred) > (1 if ui >= 1 else 0):
                    B.insert(min(2, len(B)), deferred.pop(0))
                interleave(A, B)
            while deferred:
                deferred.pop(0)()

        with ExitStack() as pa:
            KT = sb(pa, "KTd", [128, 4, 4096], BF16)
            TKT = [[S.tile() for _ in range(8)] for _ in range(4)]
            QT = sb(pa, "QTd", [128, 4, 2048], BF16)
            TQT = [[S.tile() for _ in range(4)] for _ in range(4)]
            Vd = sb(pa, "Vd", [128, 32, 4, 130], BF16)
            TV = [S.tile() for _ in range(32)]
            Tvones = S.tile()
            S.op("vector", lambda e: e.memset(Vd[:, :, :, 128:130], 1.0), writes=TV)
            lamt = sb(pa, "lamt", [128, 256], F32); Tlam = S.tile()
            bcast_load(lamt[:], lam_d, Tlam, 256)
            lj = sb(pa, "lj", [128, 64], F32)
            ls = sb(pa, "ls", [128, 2], F32); Tls = S.tile()
            le = sb(pa, "le", [128, 2], F32); Tle = S.tile()
            neglam = sb(pa, "neglam", [128, 1], F32); Tnl = S.tile()
            for z in range(2):
                S.op("vector", lambda e, z=z: e.scalar_tensor_tensor(
                    out=lj[:], in0=lamt[:, z * 128:z * 128 + 64], scalar=1.0, in1=lamt[:, z * 128 + 64:z * 128 + 128],
                    op0=ALU.mult, op1=ALU.mult, accum_out=ls[:, z:z + 1]), reads=[Tlam, Tls], writes=[Tls])
            S.op("scalar", lambda e: e.activation(out=le[:], in_=ls[:], func=AF.Exp), reads=[Tls], writes=[Tle])
            S.op("vector", lambda e: e.tensor_tensor(out=neglam[:], in0=le[:, 1:2], in1=le[:, 0:1], op=ALU.subtract),
                 reads=[Tle], writes=[Tnl])
            S.op("vector", lambda e: e.tensor_scalar(out=neglam[:], in0=neglam[:], scalar1=-LAM_INIT, scalar2=None, op0=ALU.add),
                 reads=[Tnl], writes=[Tnl])
            gsc = sb(pa, "gsc", [128, 128], F32); Tgsc = S.tile()
            bcast_load(gsc[:], dng, Tgsc, 128)
            S.op("vector", lambda e: e.tensor_scalar(out=gsc[:], in0=gsc[:], scalar1=1.0 - LAM_INIT, scalar2=None, op0=ALU.mult),
                 reads=[Tgsc], writes=[Tgsc])
            Txs = S.tile("xs")

            with ExitStack() as sw:
                st = make_sweep(sw, "a", [0, 1])
                wA = sb(sw, "wA", [128, 8, 512], BF16); TwA = S.tile()
                wB = sb(sw, "wB", [128, 8, 512], BF16); TwB = S.tile()
                wC = sb(sw, "wC", [128, 8, 512], BF16); TwC = S.tile()
                wload(wA[:], w_in[:, 512:1024], TwA)
                wload(wB[:], wks_d, TwB)
                wload(wC[:], w_in[:, 1024:1536], TwC)
                ct = [sb(sw, "ct%d" % i, [128, 512], F32) for i in range(2)]; Tct = [S.tile() for _ in range(2)]
                sn = [sb(sw, "sn%d" % i, [128, 512], F32) for i in range(2)]; Tsn = [S.tile() for _ in range(2)]
                t1 = [sb(sw, "t1%d" % i, [128, 512], F32) for i in range(2)]; Tt1 = [S.tile() for _ in range(2)]
                t2 = [sb(sw, "t2%d" % i, [128, 512], F32) for i in range(2)]; Tt2 = [S.tile() for _ in range(2)]
                rctr = [0]

                def rope_proj(blk, hT, ThT, wq_, Tw_, ws_, Tws_, cos_d, sin_d, dstT, TdstT):
                    b2 = blk % 2
                    S.dma("sync", ct[b2][:], cos_d[:, blk * 512:(blk + 1) * 512], writes=[Tct[b2]])
                    S.dma("sync", sn[b2][:], sin_d[:, blk * 512:(blk + 1) * 512], writes=[Tsn[b2]])
                    for h in range(4):
                        ba, bb = (2, 3) if h % 2 == 0 else (4, 5)
                        mmgroup(banks[ba][:], [(wq_[:, c, h * 128:(h + 1) * 128], hT[:, c, :]) for c in range(8)],
                                reads=list(ThT) + [Tw_], writes=[Tb[ba]])
                        mmgroup(banks[bb][:], [(ws_[:, c, h * 128:(h + 1) * 128], hT[:, c, :]) for c in range(8)],
                                reads=list(ThT) + [Tws_], writes=[Tb[bb]])
                        r = rctr[0] % 2
                        rctr[0] += 1
                        S.op("vector", lambda e, r=r, ba=ba: e.tensor_tensor(out=t1[r][:], in0=banks[ba][:], in1=ct[b2][:], op=ALU.mult),
                             reads=[Tb[ba], Tct[b2]], writes=[Tt1[r]])
                        S.op("vector", lambda e, r=r, bb=bb: e.tensor_tensor(out=t2[r][:], in0=banks[bb][:], in1=sn[b2][:], op=ALU.mult),
                             reads=[Tb[bb], Tsn[b2]], writes=[Tt2[r]])
                        S.op("gpsimd", lambda e, r=r, h=h: e.tensor_tensor(out=dstT[:, h, blk * 512:(blk + 1) * 512], in0=t1[r][:], in1=t2[r][:], op=ALU.add),
                             reads=[Tt1[r], Tt2[r]], writes=[TdstT[h][blk]])

                def kv_proj(blk, hT, ThT):
                    rope_proj(blk, hT, ThT, wA, TwA, wB, TwB, cosk, sink, KT, TKT)
                    for tt in range(4):
                        n = blk * 4 + tt
                        bv = 6 + (tt % 2)
                        mmgroup(banks[bv][:], [(hT[:, c, tt * 128:(tt + 1) * 128], wC[:, c, :]) for c in range(8)],
                                reads=[ThT[tt], TwC], writes=[Tb[bv]])
                        S.op("scalar", lambda e, n=n, bv=bv: e.activation(
                            out=Vd[:, n, :, 0:128], in_=banks[bv][:].rearrange("p (h d) -> p h d", h=4), func=AF.Copy),
                            reads=[Tb[bv]], writes=[TV[n]])

                sweep_stage1(st, xb, 0, gb_attn, Tgba)
                sweep_stage1(st, xb, 1, gb_attn, Tgba)
                for blk in range(8):
                    if blk + 2 < 8:
                        sweep_stage1(st, xb, blk + 2, gb_attn, Tgba)
                    kv_proj(blk, st["hT"][blk % 3], st["ThT"][blk % 3])
                wload(wA[:], w_in[:, 0:512], TwA)
                wload(wB[:], wqs_d, TwB)
                sweep_stage1(st, xo, 0, gb_attn, Tgba)
                sweep_stage1(st, xo, 1, gb_attn, Tgba)
                for blk in range(4):
                    if blk + 2 < 4:
                        sweep_stage1(st, xo, blk + 2, gb_attn, Tgba)
                    rope_proj(blk, st["hT"][blk % 3], st["ThT"][blk % 3], wA, TwA, wB, TwB, cosq, sinq, QT, TQT)
                S.barrier()
                if "KTd" in debug:
                    dbg_out("KTd", [128, 4, 4096], BF16); dump("KTd", KT[:], [])
                    dbg_out("QTd", [128, 4, 2048], BF16); dump("QTd", QT[:], [])
                    dbg_out("Vd", [128, 32, 4, 130], BF16); dump("Vd", Vd[:], [])
                    S.barrier()
                S.emit()
            if stop_after == "Aproj":
                S.barrier(); S.emit()
                return nc

            with ExitStack() as at:
                if moe == "sparse":
                    zer = sb(at, "zer", [128, 2048], BF16); Tzer = S.tile()
                    S.op("vector", lambda e: e.memset(zer[:], 0.0), writes=[Tzer])
                    for n in range(NSLOT // 256):
                        S.dma("sync", xs_d[n * 256:(n + 1) * 256, :].rearrange("(p r) d -> p (r d)", r=2), zer[:], reads=[Tzer], writes=[Txs], semtile=Tzer)
                maskd = sb(at, "maskd", [128, 2, 1024], BF16); Tmd = S.tile()
                S.dma("sync", maskd[:], maskd_d, writes=[Tmd])
                PT = [sb(at, "PT%d" % i, [128, 32, 256], BF16) for i in range(2)]
                TPT = [[S.tile() for _ in range(16)] for _ in range(2)]
                oc = sb(at, "oc", [128, 16, 4, 128], F32); Toc = [[S.tile() for _ in range(4)] for _ in range(16)]
                ssq = sb(at, "ssq", [128, 64], F32); Tssq = S.tile()
                A1 = [sb(at, "A1%d" % i, [128, 128], F32) for i in range(2)]; TA1 = [S.tile() for _ in range(2)]
                rr = sb(at, "rr", [128, 4], F32); Trr = [S.tile() for _ in range(4)]
                sjunk = sb(at, "sjunk", [128, 128], F32)
                units = [dict(h=h, i=i, z=z) for h in range(4) for i in range(8) for z in range(2)]

                def KT_of(u, kb):
                    r0 = 64 * u["z"]
                    return KT[r0:r0 + 64, u["h"], kb * 128:(kb + 1) * 128], TKT[u["h"]][kb // 4]

                def QT_of(u):
                    r0 = 64 * u["z"]
                    return QT[r0:r0 + 64, u["h"], u["i"] * 256:(u["i"] + 1) * 256], TQT[u["h"]][u["i"] // 2]

                def V_of(u, kb):
                    return Vd[:, kb, u["h"], 0:128], TV[kb]

                def exp_emit(u, p, bi, PTb, Tp):
                    S.op("scalar", lambda e: e.activation(out=PTb[:, 2 * p:2 * p + 2, :].rearrange("p a b -> p (a b)"),
                                                          in_=banks[bi][:], func=AF.Exp),
                         reads=[Tb[bi]], writes=[Tp])

                def mask_emit(u, PTb, TPb):
                    i = u["i"]
                    lo = nk_of(i) - 4
                    S.op("vector", lambda e: e.tensor_tensor(out=PTb[:, lo:lo + 4, :], in0=PTb[:, lo:lo + 4, :],
                                                             in1=maskd[:, i % 2, :].rearrange("p (a b) -> p a b", a=4), op=ALU.min),
                         reads=[Tmd, TPb[lo // 2], TPb[lo // 2 + 1]], writes=[TPb[lo // 2], TPb[lo // 2 + 1]])

                def evac_emit(u, ob):
                    h, i, z = u["h"], u["i"], u["z"]
                    for s in range(2):
                        qb = 2 * i + s
                        o_ap = banks[ob][:, s * 129:s * 129 + 128]
                        sm_ap = banks[ob][:, s * 129 + 128:s * 129 + 129]
                        ri = 2 * z + s
                        S.op("vector", lambda e, ri=ri, sm_ap=sm_ap: e.reciprocal(out=rr[:, ri:ri + 1], in_=sm_ap),
                             reads=[Tb[ob]], writes=[Trr[ri]])
                        if z == 0:
                            S.op("vector", lambda e, ri=ri, o_ap=o_ap, s=s: e.tensor_scalar(
                                out=A1[s][:], in0=o_ap, scalar1=rr[:, ri:ri + 1], scalar2=None, op0=ALU.mult),
                                reads=[Tb[ob], Trr[ri]], writes=[TA1[s]])
                        else:
                            S.op("vector", lambda e, ri=ri: e.tensor_tensor(out=rr[:, ri:ri + 1], in0=rr[:, ri:ri + 1], in1=neglam[:], op=ALU.mult),
                                 reads=[Trr[ri], Tnl], writes=[Trr[ri]])
                            S.op("vector", lambda e, ri=ri, o_ap=o_ap, s=s, qb=qb: e.scalar_tensor_tensor(
                                out=oc[:, qb, h, :], in0=o_ap, scalar=rr[:, ri:ri + 1], in1=A1[s][:], op0=ALU.mult, op1=ALU.add),
                                reads=[Tb[ob], Trr[ri], TA1[s]], writes=[Toc[qb][h]])
                            S.op("vector", lambda e, qb=qb: e.scalar_tensor_tensor(
                                out=sjunk[:], in0=oc[:, qb, h, :], scalar=1.0, in1=oc[:, qb, h, :], op0=ALU.mult, op1=ALU.mult,
                                accum_out=ssq[:, qb * 4 + h:qb * 4 + h + 1]),
                                reads=[Toc[qb][h], Tssq], writes=[Tssq])

                OTs = [sb(at, "OTs%d" % i, [128, 256], F32) for i in range(2)]; TOTs = [S.tile() for _ in range(2)]
                smr = [sb(at, "smr%d" % i, [1, 256], F32) for i in range(2)]; Tsmr = [S.tile() for _ in range(2)]
                identf_a = sb(at, "identf_a", [128, 128], F32); Tidf_a = S.tile()
                S.dma("sync", identf_a[:], identf_d, writes=[Tidf_a])

                def Vsum_of(u, kb):
                    return Vd[:, kb, u["h"], 128:129], TV[kb]
                run_attention(units, PT, TPT, KT_of, QT_of, V_of, 129, exp_emit, evac_emit, mask_emit,
                              s_banks=[0, 1, 2, 3], o_banks=[4, 5, 6, 7], OTs=OTs, TOTs=TOTs, smr=smr, Tsmr=Tsmr,
                              identf=identf_a, Tidf=Tidf_a, Vsum_of=Vsum_of)
                S.op("scalar", lambda e: e.activation(out=ssq[:], in_=ssq[:], func=AF.Sqrt, scale=1.0 / 128, bias=EPS),
                     reads=[Tssq], writes=[Tssq])
                S.op("vector", lambda e: e.reciprocal(out=ssq[:], in_=ssq[:]), reads=[Tssq], writes=[Tssq])
                Otok = [sb(at, "Otok%d" % i, [128, 512], BF16) for i in range(2)]; TOtok = [S.tile() for _ in range(2)]
                for qb in range(16):
                    o2 = qb % 2
                    for h in range(4):
                        S.op("vector", lambda e, qb=qb, h=h, o2=o2: e.scalar_tensor_tensor(
                            out=Otok[o2][:, h * 128:(h + 1) * 128], in0=oc[:, qb, h, :], scalar=ssq[:, qb * 4 + h:qb * 4 + h + 1],
                            in1=gsc[:], op0=ALU.mult, op1=ALU.mult),
                            reads=[Toc[qb][h], Tssq, Tgsc], writes=[TOtok[o2]])
                    bi = o2
                    pTv = banks[bi][:].bitcast(BF16)
                    for c in range(4):
                        S.op("tensor", lambda e, c=c, o2=o2, pTv=pTv: e.transpose(
                            out=pTv[:, c * 128:(c + 1) * 128], in_=Otok[o2][:, c * 128:(c + 1) * 128], identity=identb[:]),
                            reads=[TOtok[o2], Tidb], writes=[Tb[bi]], signal=(c == 3))
                    S.op("vector", lambda e, qb=qb, pTv=pTv: e.tensor_copy(
                        out=OT[:, 0:4, qb * 128:(qb + 1) * 128], in_=pTv[:, 0:512].rearrange("p (c t) -> p c t", c=4)),
                        reads=[Tb[bi]], writes=[TOT[qb]])
                S.barrier()
                if "OTa" in debug:
                    dbg_out("OTa", [128, 8, 2048], BF16); dump("OTa", OT[:], []); S.barrier()
                S.emit()
        if stop_after == "A":
            return nc

        with ExitStack() as pb:
            KT = sb(pb, "KTf", [128, 4, 4096], BF16)
            TKT = [[S.tile() for _ in range(8)] for _ in range(4)]
            QT = sb(pb, "QTf", [128, 4, 2048], BF16)
            TQT = [[S.tile() for _ in range(4)] for _ in range(4)]
            Vf = sb(pb, "Vf", [128, 32, 8, 66], BF16)
            TV = [S.tile() for _ in range(32)]
            S.op("vector", lambda e: e.memset(Vf[:, :, :, 64:66], 1.0), writes=TV)
            zt = sb(pb, "zt", [128, 32, 8], F32); Tzt = [S.tile() for _ in range(32)]
            Fpos = sb(pb, "Fpos", [128, 32, 8], F32); TF = [S.tile() for _ in range(32)]
            Cpos = sb(pb, "Cpos", [128, 33, 8], F32); TC = [S.tile() for _ in range(33)]
            maskf = sb(pb, "maskf", [128, 2, 1024], BF16); Tmf = S.tile()
            S.dma("sync", maskf[:], maskf_d, writes=[Tmf])
            bfb = sb(pb, "bfb", [128, 8], F32); Tbfb = S.tile()
            bcast_load(bfb[:], bfor, Tbfb, 8)
            csel = sb(pb, "csel", [128, 8, 33], F32); Tcsel = S.tile()
            bcast_load(csel[:].rearrange("p a b -> p (a b)"), csel_d, Tcsel, 8 * 33)
            ctmp = sb(pb, "ctmp", [128, 8, 33], F32); Tctmp = S.tile()
            cq = sb(pb, "cq", [128, 8, 8], F32); Tcq = S.tile()
            tri = sb(pb, "tri", [128, 128], F32); Ttri = S.tile()
            S.dma("sync", tri[:], tri_d, writes=[Ttri])
            onesf = sb(pb, "onesf", [128, 128], F32); Tones = S.tile()
            S.dma("sync", onesf[:], onesf_d, writes=[Tones])

            with ExitStack() as sw:
                st = make_sweep(sw, "b", [0, 1])
                wA = sb(sw, "wA", [128, 8, 512], BF16); TwA = S.tile()
                wB = sb(sw, "wB", [128, 8, 512], BF16); TwB = S.tile()
                wF = sb(sw, "wF", [128, 8, 8], BF16); TwF = S.tile()
                wload(wA[:], w_in[:, 2048:2560], TwA)
                wload(wB[:], w_in[:, 2560:3072], TwB)
                wload(wF[:], w_in[:, 3072:3080], TwF)
                kctr = [0]

                def plain_proj(blk, hT, ThT, w_, Tw_, dstT, TdstT, scale):
                    for hp in range(4):
                        bk = 2 + (kctr[0] % 3)
                        kctr[0] += 1
                        mmgroup(banks[bk][:], [(w_[:, c, hp * 128:(hp + 1) * 128], hT[:, c, :]) for c in range(8)],
                                reads=list(ThT) + [Tw_], writes=[Tb[bk]])
                        S.op("scalar", lambda e, bk=bk, hp=hp: e.activation(
                            out=dstT[:, hp, blk * 512:(blk + 1) * 512], in_=banks[bk][:], func=AF.Copy, scale=scale),
                            reads=[Tb[bk]], writes=[TdstT[hp][blk]])

                def kvf_proj(blk, hT, ThT):
                    plain_proj(blk, hT, ThT, wA, TwA, KT, TKT, 1.0)
                    for tt in range(4):
                        n = blk * 4 + tt
                        bv = 6 + (tt % 2)
                        mmgroup(banks[bv][:], [(hT[:, c, tt * 128:(tt + 1) * 128], wB[:, c, :]) for c in range(8)],
                                reads=[ThT[tt], TwB], writes=[Tb[bv]])
                        S.op("vector", lambda e, n=n, bv=bv: e.tensor_copy(
                            out=Vf[:, n, :, 0:64], in_=banks[bv][:].rearrange("p (h d) -> p h d", h=8)),
                            reads=[Tb[bv]], writes=[TV[n]])
                        mmgroup(banks[5][:, 0:8], [(hT[:, c, tt * 128:(tt + 1) * 128], wF[:, c, :]) for c in range(8)],
                                reads=[ThT[tt], TwF], writes=[Tb[5]])
                        S.op("vector", lambda e, n=n: e.tensor_tensor(out=zt[:, n, :], in0=banks[5][:, 0:8], in1=bfb[:], op=ALU.add),
                             reads=[Tb[5], Tbfb], writes=[Tzt[n]])

                sweep_stage1(st, xb, 0, gb_attn, Tgba)
                sweep_stage1(st, xb, 1, gb_attn, Tgba)
                for blk in range(8):
                    if blk + 2 < 8:
                        sweep_stage1(st, xb, blk + 2, gb_attn, Tgba)
                    kvf_proj(blk, st["hT"][blk % 3], st["ThT"][blk % 3])
                wload(wA[:], w_in[:, 1536:2048], TwA)
                sweep_stage1(st, xo, 0, gb_attn, Tgba)
                sweep_stage1(st, xo, 1, gb_attn, Tgba)
                for blk in range(4):
                    if blk + 2 < 4:
                        sweep_stage1(st, xo, blk + 2, gb_attn, Tgba)
                    plain_proj(blk, st["hT"][blk % 3], st["ThT"][blk % 3], wA, TwA, QT, TQT, 0.125)
                ztf = zt[:].rearrange("p a b -> p (a b)")
                S.op("scalar", lambda e: e.activation(out=ztf, in_=ztf, func=AF.Exp, scale=-1.0), reads=Tzt, writes=Tzt)
                S.op("scalar", lambda e: e.activation(out=ztf, in_=ztf, func=AF.Ln, bias=1.0), reads=Tzt, writes=Tzt)
                S.op("vector", lambda e: e.memset(Cpos[:, 0, :], 0.0), writes=[TC[0]])
                for n in range(32):
                    bc = 2 + (n % 2)
                    S.op("tensor", lambda e, n=n, bc=bc: e.matmul(out=banks[bc][:, 0:8], lhsT=tri[:], rhs=zt[:, n, :], start=True, stop=True),
                         reads=[Ttri, Tzt[n]], writes=[Tb[bc]], signal=False)
                    S.op("tensor", lambda e, n=n, bc=bc: e.matmul(out=banks[bc][:, 8:16], lhsT=onesf[:], rhs=zt[:, n, :], start=True, stop=True),
                         reads=[Tones, Tzt[n]], writes=[Tb[bc]], signal=True)
                    S.op("vector", lambda e, n=n, bc=bc: e.tensor_tensor(out=Fpos[:, n, :], in0=banks[bc][:, 0:8], in1=Cpos[:, n, :], op=ALU.add),
                         reads=[Tb[bc], TC[n]], writes=[TF[n]])
                    S.op("vector", lambda e, n=n, bc=bc: e.tensor_tensor(out=Cpos[:, n + 1, :], in0=banks[bc][:, 8:16], in1=Cpos[:, n, :], op=ALU.add),
                         reads=[Tb[bc], TC[n]], writes=[TC[n + 1]])
                for i in range(8):
                    S.op("vector", lambda e, i=i: e.tensor_tensor(out=ctmp[:], in0=Cpos[:].rearrange("p n h -> p h n"),
                                                                  in1=csel[:, i, :].unsqueeze(1).broadcast_to([128, 8, 33]), op=ALU.mult),
                         reads=TC + [Tcsel, Tctmp], writes=[Tctmp])
                    S.op("vector", lambda e, i=i: e.tensor_reduce(out=cq[:, i, :], in_=ctmp[:], axis=AX.X, op=ALU.add),
                         reads=[Tctmp], writes=[Tcq])
                S.barrier()
                if "Fpos" in debug:
                    dbg_out("Fpos", [128, 32, 8]); dump("Fpos", Fpos[:], []); S.barrier()
                S.emit()

            with ExitStack() as at:
                PT = [sb(at, "PT%d" % i, [128, 32, 256], BF16) for i in range(2)]
                TPT = [[S.tile() for _ in range(16)] for _ in range(2)]
                Otf = sb(at, "Otf", [128, 16, 512], BF16); TOtf = [S.tile() for _ in range(16)]
                rr = sb(at, "rrf", [128, 2], F32); Trr = [S.tile() for _ in range(2)]
                biasb = [sb(at, "biasb%d" % i, [128, 32], F32) for i in range(2)]; Tbias = [S.tile() for _ in range(2)]
                units = [dict(hp=hp, hh=hh, i=i, head=2 * hp + hh) for hp in range(4) for hh in range(2) for i in range(8)]
                for ui, u in enumerate(units):
                    u["ui"] = ui

                def KT_of(u, kb):
                    r0 = 64 * u["hh"]
                    return KT[r0:r0 + 64, u["hp"], kb * 128:(kb + 1) * 128], TKT[u["hp"]][kb // 4]

                def QT_of(u):
                    r0 = 64 * u["hh"]
                    return QT[r0:r0 + 64, u["hp"], u["i"] * 256:(u["i"] + 1) * 256], TQT[u["hp"]][u["i"] // 2]

                ebb = [sb(at, "ebb%d" % i, [128, 32], F32) for i in range(2)]; Teb = [S.tile() for _ in range(2)]
                Vp = [sb(at, "Vp%d" % i, [128, 32, 65], BF16) for i in range(2)]; TVp = [S.tile() for _ in range(2)]

                def V_of(u, kb):
                    return Vp[u["ui"] % 2][:, kb, :], TVp[u["ui"] % 2]

                def exp_emit(u, p, bi, PTb, Tp):
                    b2 = u["ui"] % 2
                    hd = u["head"]
                    nk = nk_of(u["i"])
                    if p == 0:
                        S.op("vector", lambda e: e.tensor_scalar(out=biasb[b2][:, 0:nk], in0=Fpos[:, 0:nk, hd], scalar1=cq[:, u["i"], hd:hd + 1],
                                                                 scalar2=70.0, op0=ALU.subtract, op1=ALU.min),
                             reads=TF[0:nk] + [Tcq], writes=[Tbias[b2]])
                        S.op("scalar", lambda e: e.activation(out=ebb[b2][:, 0:nk], in_=biasb[b2][:, 0:nk], func=AF.Exp),
                             reads=[Tbias[b2]], writes=[Teb[b2]])
                        S.op("vector", lambda e: e.tensor_tensor(out=Vp[b2][:, 0:nk, :], in0=Vf[:, 0:nk, hd, 0:65],
                                                                 in1=ebb[b2][:, 0:nk].unsqueeze(2).broadcast_to([128, nk, 65]), op=ALU.mult),
                             reads=TV[0:nk] + [Teb[b2]], writes=[TVp[b2]])
                    S.op("scalar", lambda e: e.activation(out=PTb[:, 2 * p:2 * p + 2, :].rearrange("p a b -> p (a b)"),
                                                          in_=banks[bi][:], func=AF.Exp),
                         reads=[Tb[bi]], writes=[Tp])

                def mask_emit(u, PTb, TPb):
                    i = u["i"]
                    lo = nk_of(i) - 4
                    S.op("vector", lambda e: e.tensor_tensor(out=PTb[:, lo:lo + 4, :], in0=PTb[:, lo:lo + 4, :],
                                                             in1=maskf[:, i % 2, :].rearrange("p (a b) -> p a b", a=4), op=ALU.min),
                         reads=[Tmf, TPb[lo // 2], TPb[lo // 2 + 1]], writes=[TPb[lo // 2], TPb[lo // 2 + 1]])

                def evac_emit(u, ob):
                    hd, i = u["head"], u["i"]
                    for s in range(2):
                        qb = 2 * i + s
                        S.op("vector", lambda e, s=s: e.reciprocal(out=rr[:, s:s + 1], in_=banks[ob][:, s * 65 + 64:s * 65 + 65]),
                             reads=[Tb[ob]], writes=[Trr[s]])
                        S.op("vector", lambda e, s=s, qb=qb: e.tensor_scalar(
                            out=Otf[:, qb, hd * 64:(hd + 1) * 64], in0=banks[ob][:, s * 65:s * 65 + 64], scalar1=rr[:, s:s + 1],
                            scalar2=None, op0=ALU.mult),
                            reads=[Tb[ob], Trr[s]], writes=[TOtf[qb]])

                OTs = [sb(at, "OTsf%d" % i, [128, 256], F32) for i in range(2)]; TOTs = [S.tile() for _ in range(2)]
                identf_b = sb(at, "identf_b", [128, 128], F32); Tidf_b = S.tile()
                S.dma("sync", identf_b[:], identf_d, writes=[Tidf_b])
                run_attention(units, PT, TPT, KT_of, QT_of, V_of, 65, exp_emit, evac_emit, mask_emit,
                              s_banks=[0, 1, 2, 3], o_banks=[4, 5, 6, 7], OTs=OTs, TOTs=TOTs, identf=identf_b, Tidf=Tidf_b)
                for qb in range(16):
                    bi = qb % 2
                    pTv = banks[bi][:].bitcast(BF16)
                    for c in range(4):
                        S.op("tensor", lambda e, c=c, qb=qb, pTv=pTv: e.transpose(
                            out=pTv[:, c * 128:(c + 1) * 128], in_=Otf[:, qb, c * 128:(c + 1) * 128], identity=identb[:]),
                            reads=[TOtf[qb], Tidb], writes=[Tb[bi]], signal=(c == 3))
                    S.op("vector", lambda e, qb=qb, pTv=pTv: e.tensor_copy(
                        out=OT[:, 4:8, qb * 128:(qb + 1) * 128], in_=pTv[:, 0:512].rearrange("p (c t) -> p c t", c=4)),
                        reads=[Tb[bi]], writes=[TOT[qb]])
                S.barrier()
                if "OTb" in debug:
                    dbg_out("OTb", [128, 8, 2048], BF16); dump("OTb", OT[:], []); S.barrier()
                S.emit()
        if stop_after == "B":
            return nc

        with ExitStack() as pc:
            x2 = sb(pc, "x2", [128, 16, 1024], F32); Tx2 = [[S.tile() for _ in range(2)] for _ in range(16)]
            hmT = OT
            ThmT = TOT
            ovf = sb(pc, "ovf", [128, 1], I32); Tovf = S.tile()
            w12 = sb(pc, "w12", [128, 2, 16], F32); Tw12 = S.tile()
            pos = sb(pc, "pos", [128, 2, 16], I32); Tpos = S.tile()
            comb = sb(pc, "comb", [128, 16, 16], F32); Tcomb = S.tile()
            junk = sb(pc, "junkc", [128, 1024], BF16)
            ssc = sb(pc, "ssc", [128, 16], F32); Tssc = [S.tile() for _ in range(16)]; Tsscall = S.tile()
            with ExitStack() as c1:
                wo = sb(c1, "wo", [128, 8, 1024], BF16); Two = S.tile()
                wload(wo[:], w_out, Two)
                xt = [sb(c1, "xc%d" % i, [128, 1024], F32) for i in range(2)]; Txt = [S.tile() for _ in range(2)]
                rw32 = sb(c1, "rw32", [128, 8, 20], F32); Trw = S.tile()
                S.dma("sync", rw32[:], rw_d.rearrange("(c p) n -> p c n", p=128), writes=[Trw])
                rbb = sb(c1, "rbb", [128, 20], F32); Trbb = S.tile()
                bcast_load(rbb[:], rb_d, Trbb, 20)
                gbf = sb(c1, "gbf", [128, 1024], F32); Tgbf = S.tile()
                bcast_load(gbf[:], g_ffn, Tgbf, 1024)
                identf = sb(c1, "identf", [128, 128], F32); Tidf = S.tile()
                S.dma("sync", identf[:], identf_d, writes=[Tidf])
                hm32 = [sb(c1, "hm32%d" % i, [128, 1024], F32) for i in range(2)]; Thm32 = [S.tile() for _ in range(2)]
                hmT32 = [sb(c1, "hmT32%d" % i, [128, 8, 128], F32) for i in range(2)]; ThmT32 = [S.tile() for _ in range(2)]
                Lall = sb(c1, "Lall", [128, 16, 20], F32); TL = [S.tile() for _ in range(16)]
                if moe == "sparse":
                    hmb = sb(c1, "hmb", [128, 16, 1024], BF16); Thmb = [S.tile() for _ in range(16)]
                for t in range(16):
                    S.dma("sync", xt[t % 2][:], xo[t * 128:(t + 1) * 128, :], writes=[Txt[t % 2]])
                    for hf in range(2):
                        mmgroup(banks[hf][:], [(OT[:, c, t * 128:(t + 1) * 128], wo[:, c, hf * 512:(hf + 1) * 512]) for c in range(8)],
                                reads=[TOT[t], Two], writes=[Tb[hf]])
                        S.op("vector", lambda e, t=t, hf=hf: e.tensor_tensor(
                            out=x2[:, t, hf * 512:(hf + 1) * 512], in0=banks[hf][:], in1=xt[t % 2][:, hf * 512:(hf + 1) * 512], op=ALU.add),
                            reads=[Tb[hf], Txt[t % 2]], writes=[Tx2[t][hf]])
                    S.op("scalar", lambda e, t=t: e.activation(out=junk[:], in_=x2[:, t, :], func=AF.Square, accum_out=ssc[:, t:t + 1]),
                         reads=Tx2[t], writes=[Tssc[t]])
                if "x2" in debug:
                    dbg_out("x2", [2048, 1024])
                    for t in range(16):
                        dump("x2", x2[:, t, :], Tx2[t]) if False else S.dma("sync", dbg["x2"][t * 128:(t + 1) * 128, :], x2[:, t, :], reads=Tx2[t], writes=[Tdbg], semtile=Tdbg)
                if stop_after == "C0":
                    S.barrier(); S.emit()
                    return nc
                S.op("scalar", lambda e: e.activation(out=ssc[:], in_=ssc[:], func=AF.Sqrt, scale=1.0 / 1024, bias=EPS),
                     reads=Tssc, writes=[Tsscall])
                S.op("vector", lambda e: e.reciprocal(out=ssc[:], in_=ssc[:]), reads=[Tsscall], writes=[Tsscall])
                for t in range(16):
                    t2 = t % 2
                    S.op("vector", lambda e, t=t, t2=t2: e.scalar_tensor_tensor(
                        out=hm32[t2][:], in0=x2[:, t, :], scalar=ssc[:, t:t + 1], in1=gbf[:], op0=ALU.mult, op1=ALU.mult),
                        reads=Tx2[t] + [Tsscall, Tgbf], writes=[Thm32[t2]])
                    ba, bb = (2, 3) if t2 == 0 else (4, 5)
                    for c in range(8):
                        bk = ba if c < 4 else bb
                        S.op("tensor", lambda e, c=c, t2=t2, bk=bk: e.transpose(
                            out=banks[bk][:, (c % 4) * 128:(c % 4 + 1) * 128], in_=hm32[t2][:, c * 128:(c + 1) * 128], identity=identf[:]),
                            reads=[Thm32[t2], Tidf], writes=[Tb[bk]], signal=(c % 4 == 3))
                    import os
                    CUT = int(os.environ.get("C1CUT", "9"))
                    if CUT < 2:
                        continue
                    for k, bk in enumerate((ba, bb)):
                        src = banks[bk][:].rearrange("p (c t) -> p c t", c=4)
                        S.op("scalar", lambda e, k=k, t2=t2, src=src: e.activation(out=hmT32[t2][:, 4 * k:4 * k + 4, :], in_=src, func=AF.Copy),
                             reads=[Tb[bk]], writes=[ThmT32[t2]])
                        S.op("vector", lambda e, k=k, t=t, t2=t2: e.tensor_copy(out=hmT[:, 4 * k:4 * k + 4, t * 128:(t + 1) * 128], in_=hmT32[t2][:, 4 * k:4 * k + 4, :]),
                             reads=[ThmT32[t2]], writes=[ThmT[t]])
                    if moe == "sparse":
                        S.op("gpsimd", lambda e, t=t, t2=t2: e.tensor_copy(out=hmb[:, t, :], in_=hm32[t2][:]), reads=[Thm32[t2]], writes=[Thmb[t]])
                    if CUT < 3:
                        continue
                    br = 6 + t2
                    mmgroup(banks[br][:, 0:20], [(hmT32[t2][:, c, :], rw32[:, c, :]) for c in range(8)],
                            reads=[ThmT32[t2], Trw], writes=[Tb[br]])
                    S.op("vector", lambda e, t=t, br=br: e.tensor_tensor(out=Lall[:, t, :], in0=banks[br][:, 0:20], in1=rbb[:], op=ALU.add),
                         reads=[Tb[br], Trbb], writes=[TL[t]])
                if stop_after == "C1a":
                    if "Lall" in debug:
                        dbg_out("Lall", [128, 320])
                        S.dma("sync", dbg["Lall"], Lall[:].rearrange("p a b -> p (a b)"), reads=[], writes=[Tdbg], semtile=Tdbg)
                    S.barrier(); S.emit()
                    return nc
                TR = S.tile()

                def rt(name, shape):
                    return sb(c1, "rt_" + name, shape, F32)
                gmax = rt("gmax", [128, 16]); gm = rt("gm", [128, 16, 4]); gd = rt("gd", [128, 16, 4])
                gsum = rt("gsum", [128, 16]); gw = rt("gw", [128, 16]); pen = rt("pen", [128, 16, 4])
                EL = rt("EL", [128, 16, 16]); EL2 = rt("EL2", [128, 16, 16]); m1 = rt("m1", [128, 16]); m2 = rt("m2", [128, 16])
                oh1 = rt("oh1", [128, 16, 16]); oh2 = rt("oh2", [128, 16, 16]); dd = rt("dd", [128, 16]); w1 = rt("w1", [128, 16]); w2 = rt("w2", [128, 16])
                LG = Lall[:, :, 0:4]
                LE4 = Lall[:, :, 4:20].rearrange("p t (g e) -> p t g e", g=4)
                EL4 = EL[:].rearrange("p t (g e) -> p t g e", g=4)

                def vop(fn, first=False):
                    S.op("vector", fn, reads=(TL + [TR]) if first else [TR], writes=[TR])

                def bc3(a, n):
                    return a[:].unsqueeze(2).broadcast_to([128, 16, n])
                vop(lambda e: e.tensor_reduce(out=gmax[:], in_=LG, axis=AX.X, op=ALU.max), first=True)
                vop(lambda e: e.tensor_tensor(out=gm[:], in0=LG, in1=bc3(gmax, 4), op=ALU.is_equal))
                vop(lambda e: e.tensor_tensor(out=gd[:], in0=LG, in1=bc3(gmax, 4), op=ALU.subtract))
                S.op("scalar", lambda e: e.activation(out=gd[:], in_=gd[:], func=AF.Exp), reads=[TR], writes=[TR])
                vop(lambda e: e.tensor_reduce(out=gsum[:], in_=gd[:], axis=AX.X, op=ALU.add))
                vop(lambda e: e.reciprocal(out=gw[:], in_=gsum[:]))
                vop(lambda e: e.tensor_scalar(out=pen[:], in0=gm[:], scalar1=1.0, scalar2=1e30, op0=ALU.subtract, op1=ALU.mult))
                vop(lambda e: e.tensor_tensor(out=EL4, in0=LE4, in1=gm[:].unsqueeze(3).broadcast_to([128, 16, 4, 4]), op=ALU.mult))
                vop(lambda e: e.tensor_tensor(out=EL4, in0=EL4, in1=pen[:].unsqueeze(3).broadcast_to([128, 16, 4, 4]), op=ALU.add))
                vop(lambda e: e.tensor_reduce(out=m1[:], in_=EL[:], axis=AX.X, op=ALU.max))
                vop(lambda e: e.tensor_tensor(out=oh1[:], in0=EL[:], in1=bc3(m1, 16), op=ALU.is_equal))
                vop(lambda e: e.scalar_tensor_tensor(out=EL2[:], in0=oh1[:], scalar=-1e30, in1=EL[:], op0=ALU.mult, op1=ALU.add))
                vop(lambda e: e.tensor_reduce(out=m2[:], in_=EL2[:], axis=AX.X, op=ALU.max))
                vop(lambda e: e.tensor_tensor(out=oh2[:], in0=EL2[:], in1=bc3(m2, 16), op=ALU.is_equal))
                vop(lambda e: e.tensor_tensor(out=dd[:], in0=m2[:], in1=m1[:], op=ALU.subtract))
                S.op("scalar", lambda e: e.activation(out=dd[:], in_=dd[:], func=AF.Exp), reads=[TR], writes=[TR])
                vop(lambda e: e.tensor_scalar(out=w1[:], in0=dd[:], scalar1=1.0, scalar2=None, op0=ALU.add))
                vop(lambda e: e.reciprocal(out=w1[:], in_=w1[:]))
                vop(lambda e: e.tensor_tensor(out=w1[:], in0=w1[:], in1=gw[:], op=ALU.mult))
                vop(lambda e: e.tensor_tensor(out=w2[:], in0=dd[:], in1=w1[:], op=ALU.mult))
                if moe == "sparse":
                    Mb = sb(c1, "Mb", [128, 16, 16], BF16)
                    ustrict = sb(c1, "ustrict", [128, 128], BF16); Tus = S.tile()
                    S.dma("sync", ustrict[:], ustrict_d, writes=[Tus])
                    onesb = sb(c1, "onesb", [128, 128], BF16); Tob_ = S.tile()
                    S.dma("sync", onesb[:], onesb_d, writes=[Tob_])
                    ebase = sb(c1, "ebase", [128, 16, 16], F32); Teb = S.tile()
                    S.dma("sync", ebase[:].rearrange("p a b -> p (a b)"), ebase_d, writes=[Teb])
                    slotf = rt("slotf", [128, 16, 16]); okf = rt("okf", [128, 16, 16]); posf = rt("posf", [128, 2, 16])
                    vop(lambda e: e.tensor_tensor(out=Mb[:], in0=oh1[:], in1=oh2[:], op=ALU.add))
                    for t in range(16):
                        prs = [(onesb[:], Mb[:, tp, :]) for tp in range(t)] + [(ustrict[:], Mb[:, t, :])]
                        n_ = len(prs)
                        for k_, (l_, r_) in enumerate(prs):
                            S.op("tensor", lambda e, l_=l_, r_=r_, k_=k_, n_=n_, t=t: e.matmul(out=banks[0][:, t * 16:(t + 1) * 16], lhsT=l_, rhs=r_,
                                                                                         start=(k_ == 0), stop=(k_ == n_ - 1)),
                                 reads=[TR, Tus, Tob_], writes=[Tb[0]], signal=(k_ == n_ - 1))
                    for tp in range(16):
                        S.op("tensor", lambda e, tp=tp: e.matmul(out=banks[1][:, 0:16], lhsT=onesb[:], rhs=Mb[:, tp, :], start=(tp == 0), stop=(tp == 15)),
                             reads=[TR, Tob_], writes=[Tb[1]], signal=(tp == 15))
                    cmax = rt("cmax", [128, 1])
                    S.op("vector", lambda e: e.tensor_reduce(out=cmax[:], in_=banks[1][:, 0:16], axis=AX.X, op=ALU.max), reads=[Tb[1], TR], writes=[TR])
                    import os as _os
                    thr = -1.0 if _os.environ.get("FORCE_DENSE") else float(CAP)
                    vop(lambda e: e.tensor_scalar(out=cmax[:], in0=cmax[:], scalar1=thr, scalar2=None, op0=ALU.is_gt))
                    S.op("vector", lambda e: e.tensor_copy(out=ovf[:], in_=cmax[:]), reads=[TR], writes=[Tovf])
                    rank = banks[0][:, 0:256].rearrange("p (a b) -> p a b", a=16)
                    S.op("vector", lambda e: e.tensor_tensor(out=slotf[:], in0=rank, in1=ebase[:], op=ALU.add), reads=[Tb[0], Teb, TR], writes=[TR])
                    vop(lambda e: e.tensor_scalar(out=okf[:], in0=slotf[:], scalar1=None, scalar2=None, op0=ALU.bypass) if False else
                        e.tensor_tensor(out=okf[:], in0=slotf[:], in1=ebase[:], op=ALU.subtract))
                    vop(lambda e: e.tensor_scalar(out=okf[:], in0=okf[:], scalar1=float(CAP), scalar2=1.0e6, op0=ALU.is_ge, op1=ALU.mult))
                    vop(lambda e: e.tensor_tensor(out=slotf[:], in0=slotf[:], in1=okf[:], op=ALU.add))
                    vop(lambda e: e.tensor_tensor(out=okf[:], in0=slotf[:], in1=oh1[:], op=ALU.mult))
                    vop(lambda e: e.tensor_reduce(out=posf[:, 0, :], in_=okf[:], axis=AX.X, op=ALU.add))
                    vop(lambda e: e.tensor_tensor(out=okf[:], in0=slotf[:], in1=oh2[:], op=ALU.mult))
                    vop(lambda e: e.tensor_reduce(out=posf[:, 1, :], in_=okf[:], axis=AX.X, op=ALU.add))
                    S.op("vector", lambda e: e.tensor_copy(out=pos[:], in_=posf[:]), reads=[TR], writes=[Tpos])
                    S.op("vector", lambda e: e.tensor_copy(out=w12[:, 0, :], in_=w1[:]), reads=[TR, Tw12], writes=[Tw12])
                    S.op("vector", lambda e: e.tensor_copy(out=w12[:, 1, :], in_=w2[:]), reads=[TR, Tw12], writes=[Tw12])
                    Tsc = [S.tile() for _ in range(32)]
                    set_bcreg()
                    for t in range(16):
                        for k_ in range(2):
                            S.dma_fn("gpsimd", lambda e, t=t, k_=k_: e.indirect_dma_start(
                                out=xs_d[:, :], out_offset=bass.IndirectOffsetOnAxis(ap=pos[:, k_, t:t + 1], axis=0),
                                in_=hmb[:, t, :], in_offset=None, bounds_check=bcreg, oob_is_err=False),
                                reads=[Thmb[t], Tpos, Txs], writes=[Tsc[2 * t + k_]], semtile=Thmb[t])
                vop(lambda e: e.tensor_tensor(out=oh1[:], in0=oh1[:], in1=bc3(w1, 16), op=ALU.mult))
                vop(lambda e: e.tensor_tensor(out=oh2[:], in0=oh2[:], in1=bc3(w2, 16), op=ALU.mult))
                S.op("vector", lambda e: e.tensor_tensor(out=comb[:], in0=oh1[:], in1=oh2[:], op=ALU.add), reads=[TR], writes=[Tcomb])
                S.barrier()
                if "comb" in debug:
                    dbg_out("comb", [128, 256])
                    S.dma("sync", dbg["comb"], comb[:].rearrange("p a b -> p (a b)"), reads=[Tcomb], writes=[Tdbg], semtile=Tdbg)
                    S.barrier()
                S.emit()
            if stop_after == "C1":
                return nc

            with ExitStack() as c2:
                wgb = [sb(c2, "wgb%d" % i, [128, 8, 512], BF16) for i in range(2)]; Twg4 = [[S.tile() for _ in range(4)] for _ in range(2)]
                wub = [sb(c2, "wub%d" % i, [128, 8, 512], BF16) for i in range(2)]; Twu4 = [[S.tile() for _ in range(4)] for _ in range(2)]
                wdb = [sb(c2, "wdb%d" % i, [128, 4, 1024], BF16) for i in range(2)]; Twd4 = [[S.tile() for _ in range(4)] for _ in range(2)]
                stg = [sb(c2, "stg%d" % i, [128, 1024], F32) for i in range(3)]; Tstg = [S.tile() for _ in range(3)]
                sq_ = [0]
                aT = [sb(c2, "aT%d" % i, [128, 4, 512], BF16) for i in range(2)]; TaT = [[S.tile() for _ in range(4)] for _ in range(2)]
                sg = [sb(c2, "sg%d" % i, [128, 512], F32) for i in range(2)]; Tsg = [S.tile() for _ in range(2)]
                xg = [sb(c2, "xg%d" % i, [128, 1024], BF16) for i in range(4)]; Txg = [S.tile() for _ in range(4)]
                xgT = [sb(c2, "xgT%d" % i, [128, 8, CAP], BF16) for i in range(2)]; TxgT = [[S.tile() for _ in range(CAP // 128)] for _ in range(2)]
                ysb = [sb(c2, "ysb%d" % i, [128, 1024], F32) for i in range(2)]; Tysb = [S.tile() for _ in range(2)]
                NJ = CAP // 128
                Tys = [S.tile() for _ in range(NEXP * NJ)]

                def w_steps(ex):
                    b2 = ex % 2
                    dmas, casts = [], []
                    for k in range(12):
                        def mk(k=k):
                            if k < 8:
                                srcw = (wg_d if k < 4 else wu_d)[ex]
                                kk = k % 4
                                src = srcw[kk * 256:(kk + 1) * 256, :].rearrange("(c p) n -> p c n", p=128)
                                dst_of = lambda: (wgb if k < 4 else wub)[b2][:, 2 * kk:2 * kk + 2, :]
                                Td = (Twg4 if k < 4 else Twu4)[b2][kk]
                                view = lambda t_: t_[:].rearrange("p (c n) -> p c n", c=2)
                            else:
                                kk = k - 8
                                src = wd_d[ex][kk * 128:(kk + 1) * 128, :]
                                dst_of = lambda: wdb[b2][:, kk, :]
                                Td = Twd4[b2][kk]
                                view = lambda t_: t_[:]
                            cell = {}

                            def d():
                                si = sq_[0] % 3
                                sq_[0] += 1
                                cell["si"] = si
                                S.dma("sync", view(stg[si]), src, writes=[Tstg[si]])

                            def c():
                                si = cell["si"]
                                sv = view(stg[si])
                                dstb = dst_of()
                                if k % 2 == 1:
                                    S.op("scalar", lambda e: e.activation(out=dstb, in_=sv, func=AF.Copy), reads=[Tstg[si]], writes=[Td])
                                else:
                                    S.op("vector", lambda e: e.tensor_copy(out=dstb, in_=sv), reads=[Tstg[si]], writes=[Td])
                            return d, c
                        d, c = mk()
                        dmas.append(d)
                        casts.append(c)
                    steps = dmas[0:3]
                    for k in range(12):
                        steps.append(casts[k])
                        if k + 3 < 12:
                            steps.append(dmas[k + 3])
                    return steps

                def load_w(ex):
                    for f in w_steps(ex):
                        f()

                def gate_up(b2, rhs_of, Trhs, width, gq, slot=None):
                    for ft in range(4):
                        bg, bu = (0, 1) if gq[0] % 2 == 0 else (2, 3)
                        s2 = gq[0] % 2
                        gq[0] += 1
                        mmgroup(banks[bg][:, 0:width], [(wgb[b2][:, c, ft * 128:(ft + 1) * 128], rhs_of(c)) for c in range(8)],
                                reads=Trhs + Twg4[b2], writes=[Tb[bg]])
                        mmgroup(banks[bu][:, 0:width], [(wub[b2][:, c, ft * 128:(ft + 1) * 128], rhs_of(c)) for c in range(8)],
                                reads=Trhs + Twu4[b2], writes=[Tb[bu]])
                        S.op("scalar", lambda e, bg=bg, s2=s2: e.activation(out=sg[s2][:, 0:width], in_=banks[bg][:, 0:width], func=AF.Silu),
                             reads=[Tb[bg]], writes=[Tsg[s2]])
                        S.op("vector", lambda e, bu=bu, s2=s2, ft=ft: e.tensor_tensor(out=aT[b2][:, ft, 0:width], in0=sg[s2][:, 0:width], in1=banks[bu][:, 0:width], op=ALU.mult),
                             reads=[Tsg[s2], Tb[bu]], writes=[TaT[b2][ft]])
                        if slot is not None:
                            slot()

                S.branch_begin()
                gq = [0]; yq = [0]; xq = [0]

                def prep(ex):
                    b2 = ex % 2
                    for j in range(NJ):
                        xi = xq[0] % 4
                        xq[0] += 1
                        r0 = ex * CAP + j * 128
                        S.dma("gpsimd", xg[xi][:], xs_d[r0:r0 + 128, :], reads=Tsc + [Txs], writes=[Txg[xi]])
                        bi = 6 + (xq[0] % 2)
                        pTv = banks[bi][:].bitcast(BF16)
                        for c in range(8):
                            S.op("tensor", lambda e, c=c, xi=xi, pTv=pTv: e.transpose(
                                out=pTv[:, c * 128:(c + 1) * 128], in_=xg[xi][:, c * 128:(c + 1) * 128], identity=identb[:]),
                                reads=[Txg[xi], Tidb], writes=[Tb[bi]], signal=(c == 7))
                        S.op("vector", lambda e, j=j, b2=b2, pTv=pTv: e.tensor_copy(
                            out=xgT[b2][:, :, j * 128:(j + 1) * 128], in_=pTv.rearrange("p (c t) -> p c t", c=8)),
                            reads=[Tb[bi]], writes=[TxgT[b2][j]])
                prep(0)
                load_w(0)
                for ex in range(NEXP):
                    b2 = ex % 2
                    wq = w_steps(ex + 1) if ex + 1 < NEXP else []

                    def pop(n):
                        for _ in range(n):
                            if wq:
                                wq.pop(0)()
                    pop(3)
                    gate_up(b2, lambda c, b2=b2: xgT[b2][:, c, :], TxgT[b2], CAP, gq, slot=lambda: pop(3))
                    if ex + 1 < NEXP:
                        prep(ex + 1)
                    for j in range(NJ):
                        y2 = yq[0] % 2
                        yq[0] += 1
                        for hf in range(2):
                            by = 4 + hf
                            mmgroup(banks[by][:], [(aT[b2][:, ft, j * 128:(j + 1) * 128], wdb[b2][:, ft, hf * 512:(hf + 1) * 512]) for ft in range(4)],
                                    reads=TaT[b2] + Twd4[b2], writes=[Tb[by]])
                            if hf == 0:
                                S.op("vector", lambda e, y2=y2, by=by: e.tensor_copy(out=ysb[y2][:, 0:512], in_=banks[by][:]),
                                     reads=[Tb[by]], writes=[Tysb[y2]])
                            else:
                                S.op("scalar", lambda e, y2=y2, by=by: e.activation(out=ysb[y2][:, 512:1024], in_=banks[by][:], func=AF.Copy),
                                     reads=[Tb[by], Tysb[y2]], writes=[Tysb[y2]])
                        r0 = ex * CAP + j * 128
                        S.dma("gpsimd", ys_d[r0:r0 + 128, :], ysb[y2][:], reads=[Tysb[y2]], writes=[Tys[ex * NJ + j]], semtile=Tysb[y2])
                        pop(3)
                    pop(99)
                S.barrier()
                ygl = []
                for wb in wgb + wub + wdb:
                    v = wb[:].rearrange("p a b -> p (a b)").bitcast(F32)
                    ygl += [v[:, 0:1024], v[:, 1024:2048]]
                Tyg = [S.tile() for _ in ygl]
                set_bcreg()
                def gath(i):
                    t, k_ = i // 2, i % 2
                    gi = i % len(ygl)
                    S.dma_fn("gpsimd", lambda e, t=t, k_=k_, gi=gi: e.indirect_dma_start(
                        out=ygl[gi], out_offset=None, in_=ys_d[:, :],
                        in_offset=bass.IndirectOffsetOnAxis(ap=pos[:, k_, t:t + 1], axis=0),
                        bounds_check=bcreg, oob_is_err=False),
                        reads=Tys + [Tpos], writes=[Tyg[gi]], semtile=Tyg[gi])

                def acc(i):
                    t, k_ = i // 2, i % 2
                    gi = i % len(ygl)
                    for hf in range(2):
                        S.op("vector", lambda e, t=t, k_=k_, gi=gi, hf=hf: e.scalar_tensor_tensor(
                            out=x2[:, t, hf * 512:(hf + 1) * 512], in0=ygl[gi][:, hf * 512:(hf + 1) * 512], scalar=w12[:, k_, t:t + 1],
                            in1=x2[:, t, hf * 512:(hf + 1) * 512], op0=ALU.mult, op1=ALU.add),
                            reads=[Tyg[gi], Tw12, Tx2[t][hf]], writes=[Tx2[t][hf]])
                depth = len(ygl) - 1
                for i in range(32 + depth):
                    if i < 32:
                        gath(i)
                    if i - depth >= 0:
                        acc(i - depth)
                S.branch_mid()
                gq = [0]; yq = [0]
                load_w(0)
                for ex in range(NEXP):
                    b2 = ex % 2
                    if ex + 1 < NEXP:
                        load_w(ex + 1)
                    for tb in range(4):
                        gate_up(b2, lambda c, tb=tb: hmT[:, c, tb * 512:(tb + 1) * 512], ThmT[tb * 4:tb * 4 + 4], 512, gq)
                        for tt in range(4):
                            t = tb * 4 + tt
                            for hf in range(2):
                                by = 4 + (yq[0] % 4)
                                yq[0] += 1
                                mmgroup(banks[by][:], [(aT[b2][:, ft, tt * 128:(tt + 1) * 128], wdb[b2][:, ft, hf * 512:(hf + 1) * 512]) for ft in range(4)],
                                        reads=TaT[b2] + Twd4[b2], writes=[Tb[by]])
                                S.op("vector", lambda e, t=t, hf=hf, by=by, ex=ex: e.scalar_tensor_tensor(
                                    out=x2[:, t, hf * 512:(hf + 1) * 512], in0=banks[by][:], scalar=comb[:, t, ex:ex + 1],
                                    in1=x2[:, t, hf * 512:(hf + 1) * 512], op0=ALU.mult, op1=ALU.add),
                                    reads=[Tb[by], Tcomb, Tx2[t][hf]], writes=[Tx2[t][hf]])
                S.branch_end(ovf[0:1, 0:1], brregs)
                S.emit()

            with ExitStack() as c3:
                gbn = sb(c3, "gbn", [128, 1024], F32); Tgbn = S.tile()
                bcast_load(gbn[:], g_fin, Tgbn, 1024)
                ob = [sb(c3, "ob%d" % i, [128, 1024], F32) for i in range(2)]; Tob = [S.tile() for _ in range(2)]
                Tout = S.tile()
                for t in range(16):
                    S.op("scalar", lambda e, t=t: e.activation(out=junk[:], in_=x2[:, t, :], func=AF.Square, accum_out=ssc[:, t:t + 1]),
                         reads=Tx2[t] + [Tsscall], writes=[Tssc[t]])
                S.op("scalar", lambda e: e.activation(out=ssc[:], in_=ssc[:], func=AF.Sqrt, scale=1.0 / 1024, bias=EPS),
                     reads=Tssc, writes=[Tsscall])
                S.op("vector", lambda e: e.reciprocal(out=ssc[:], in_=ssc[:]), reads=[Tsscall], writes=[Tsscall])
                for t in range(16):
                    S.op("vector", lambda e, t=t: e.scalar_tensor_tensor(
                        out=ob[t % 2][:], in0=x2[:, t, :], scalar=ssc[:, t:t + 1], in1=gbn[:], op0=ALU.mult, op1=ALU.mult),
                        reads=Tx2[t] + [Tsscall, Tgbn], writes=[Tob[t % 2]])
                    S.dma("sync", out[t * 128:(t + 1) * 128, :], ob[t % 2][:], reads=[Tob[t % 2]], writes=[Tout], semtile=Tob[t % 2])
                S.barrier()
                S.emit()
    return nc


def _const_tables():
    f32 = np.float32
    inv_freq = (f32(1.0) / (f32(10000.0) ** (np.arange(0, 64, 2, dtype=f32) / f32(64)))).astype(f32)
    pos = np.arange(4096, dtype=f32)
    ang = (pos[:, None] * inv_freq[None, :]).astype(f32)
    cos = np.cos(ang).astype(f32)
    sin = np.sin(ang).astype(f32)
    r = np.arange(128)
    dh = r % 64
    cosT = cos[:, dh % 32].T.copy()
    sgn = np.where(dh < 32, -1.0, 1.0).astype(f32)
    sinT = (sin[:, dh % 32].T * sgn[:, None]).astype(f32)
    return cosT, sinT


def _masks(hf):
    k = np.arange(128)[:, None, None]
    r = np.arange(4)[None, :, None]
    q = np.arange(256)[None, None, :]
    md = np.zeros((128, 2, 4, 256), np.float32)
    mf = np.zeros((128, 2, 4, 256), np.float32)
    for par in range(2):
        if par == 0:
            kb = r
            j = 0 if hf == 0 else 1
        else:
            kb = 4 + r
            j = 3 if hf == 0 else 2
        s = kb * 128 + k
        t = j * 256 + q
        mf[:, par] = np.where(s <= t, 3e38, 0.0)
        md[:, par] = np.where((s // 64) <= (t // 64), 3e38, 0.0)
    return (md.reshape(128, 2, 1024).astype(ml_dtypes.bfloat16),
            mf.reshape(128, 2, 1024).astype(ml_dtypes.bfloat16))


def own_tokens(hf):
    return np.concatenate([np.arange(j * 256, (j + 1) * 256) for j in own_qtiles(hf)])


def prep(inputs):
    f32 = np.float32
    x = np.asarray(inputs["x"], f32)
    w_in = np.ascontiguousarray(np.asarray(inputs["w_in"], f32)[0])

    def swap_cols(w):
        return np.ascontiguousarray(w.reshape(1024, 8, 2, 32)[:, :, ::-1, :].reshape(1024, 512))

    cosT, sinT = _const_tables()
    common = {
        "w_in": w_in,
        "wqs": swap_cols(w_in[:, 0:512]),
        "wks": swap_cols(w_in[:, 512:1024]),
        "cosk": cosT, "sink": sinT,
        "identb": np.eye(128, dtype=f32).astype(ml_dtypes.bfloat16),
        "identf": np.eye(128, dtype=f32),
        "tri": np.triu(np.ones((128, 128), f32)),
        "onesf": np.ones((128, 128), f32),
        "onesb": np.ones((128, 128), f32).astype(ml_dtypes.bfloat16),
        "ustrict": np.triu(np.ones((128, 128), f32), 1).astype(ml_dtypes.bfloat16),
        "ebase": np.ascontiguousarray(np.broadcast_to((np.arange(16, dtype=f32) * CAP)[None, None, :], (128, 16, 16)).reshape(128, 256)),
        "g_attn": np.asarray(inputs["norm_attn_g"], f32).reshape(1, 1024),
        "g_ffn": np.asarray(inputs["norm_ffn_g"], f32).reshape(1, 1024),
        "g_fin": np.asarray(inputs["norm_final_g"], f32).reshape(1, 1024),
        "bfor": np.asarray(inputs["b_forget"], f32).reshape(1, 8),
        "lamv": np.concatenate([np.asarray(inputs[k], f32).reshape(1, 64) for k in
                                ("lambda_q1", "lambda_k1", "lambda_q2", "lambda_k2")], axis=1),
        "dng": np.asarray(inputs["diff_norm_g"], f32).reshape(1, 128),
        "w_out": np.ascontiguousarray(np.asarray(inputs["w_out"], f32)[0]),
        "rw": np.ascontiguousarray(np.concatenate([np.asarray(inputs["router_group_w"], f32)[0],
                                                   np.asarray(inputs["router_expert_w"], f32)[0]], axis=1)),
        "rb": np.concatenate([np.asarray(inputs["router_group_b"], f32).reshape(1, 4),
                              np.asarray(inputs["router_expert_b"], f32).reshape(1, 16)], axis=1),
        "wg": np.ascontiguousarray(np.asarray(inputs["w_gate"], f32)[0]),
        "wu": np.ascontiguousarray(np.asarray(inputs["w_up"], f32)[0]),
        "wd": np.ascontiguousarray(np.asarray(inputs["w_down"], f32)[0]),
    }
    in_maps = []
    for c in range(8):
        b, hf = c // 2, c % 2
        tok = own_tokens(hf)
        md, mf = _masks(hf)
        m = dict(common)
        m["xb"] = np.ascontiguousarray(x[b])
        m["xo"] = np.ascontiguousarray(x[b][tok])
        m["cosq"] = np.ascontiguousarray(cosT[:, tok] * f32(0.125))
        m["sinq"] = np.ascontiguousarray(sinT[:, tok] * f32(0.125))
        cs = np.zeros((8, 33), f32)
        for i, j in enumerate(own_qtiles(hf)):
            cs[i, 2 * j + 1] = 1.0
        m["csel"] = cs.reshape(1, 8 * 33)
        m["maskd"] = md
        m["maskf"] = mf
        in_maps.append(m)
    return in_maps


def kernel(**inputs):
    in_maps = prep(inputs)
    nc = build()
    res = run_bass_kernel_spmd(nc, in_maps, core_ids=list(range(8)))
    out = np.zeros((4, 4096, 1024), np.float32)
    for c in range(8):
        b, hf = c // 2, c % 2
        out[b, own_tokens(hf)] = res.results[c]["out"]
    return out
```

```python
import numpy as np
import ml_dtypes
from contextlib import ExitStack
import concourse.bass as bass
import concourse.mybir as mybir
from concourse.bass_utils import run_bass_kernel_spmd

F32 = mybir.dt.float32
BF16 = mybir.dt.bfloat16
I32 = mybir.dt.int32
AF = mybir.ActivationFunctionType
ALU = mybir.AluOpType
AX = mybir.AxisListType

ENGS = ("sync", "scalar", "vector", "gpsimd", "tensor")
EPS = 1e-6
LAM_INIT = 0.8 - 0.6 * 1.0
NEXP = 16
CAP = 512
NSLOT = NEXP * CAP


class Tile:
    __slots__ = ("name", "last_w", "readers", "dsem")

    def __init__(self, name):
        self.name = name
        self.last_w = None
        self.readers = {}
        self.dsem = None


class Sched:
    def __init__(self, nc, es):
        self.nc = nc
        self.es = es
        self.ops = {e: [] for e in ENGS}
        self.sems = {}
        self.cnt = {}
        self.waited = {e: {} for e in ENGS}
        self.pending = {e: False for e in ENGS}
        for e in ENGS:
            self._mksem("E:" + e)
        self.n_dsem = 0
        self.nops = 0
        self.tiles = []

    def _mksem(self, key):
        self.sems[key] = self.es.enter_context(self.nc.semaphore(key.replace(":", "_")))
        self.cnt[key] = 0

    def tile(self, name="t"):
        t = Tile(name)
        self.tiles.append(t)
        return t

    def _snapshot(self):
        return (dict(self.cnt), {e: dict(w) for e, w in self.waited.items()},
                [(t, t.last_w, dict(t.readers)) for t in self.tiles])

    def _restore(self, snap):
        self.cnt = dict(snap[0])
        for k in self.sems:
            self.cnt.setdefault(k, 0)
        self.waited = {e: dict(w) for e, w in snap[1].items()}
        for t, lw, rd in snap[2]:
            t.last_w = lw
            t.readers = dict(rd)

    def branch_begin(self):
        self.barrier()
        self._outer_ops = self.ops
        self.ops = {e: [] for e in ENGS}
        self._snap = self._snapshot()

    def branch_mid(self):
        self.barrier()
        self._A = (self.ops, dict(self.cnt))
        self.ops = {e: [] for e in ENGS}
        self._restore(self._snap)

    def branch_end(self, flag_ap, regs):
        self.barrier()
        opsA, cntA = self._A
        opsB, cntB = self.ops, dict(self.cnt)
        target = {k: max(cntA.get(k, 0), cntB.get(k, 0)) for k in set(cntA) | set(cntB)}

        def pads(cntX):
            out = {e: [] for e in ENGS}
            for k, v in target.items():
                d = v - cntX.get(k, 0)
                if d > 0:
                    owner = k[2:] if k.startswith("E:") else "gpsimd"
                    out[owner].append((k, d))
            return out
        pA, pB = pads(cntA), pads(cntB)
        self.ops = self._outer_ops
        for e in ENGS:
            self.ops[e].append(("branch", flag_ap, regs[e], opsA[e], pA[e], opsB[e], pB[e]))
        self.cnt = target
        for e in ENGS:
            self.waited[e] = dict(target)
        for t in self.tiles:
            t.last_w = None
            t.readers = {}

    def dsem_for(self, t):
        if t.dsem is None:
            key = "D:%d" % self.n_dsem
            self.n_dsem += 1
            self._mksem(key)
            t.dsem = key
        return t.dsem

    def _need(self, eng, waits, key, val):
        if eng == "tensor" and key == "E:tensor":
            return
        if self.cnt[key] < val:
            raise RuntimeError("wait on un-signalled event %s %d (cnt %d) from %s" % (key, val, self.cnt[key], eng))
        if self.waited[eng].get(key, 0) >= val:
            return
        self.waited[eng][key] = val
        waits[key] = max(waits.get(key, 0), val)

    def _deps(self, eng, reads, writes):
        waits = {}
        for t in reads:
            if t.last_w is not None:
                self._need(eng, waits, *t.last_w)
        for t in writes:
            if t.last_w is not None:
                self._need(eng, waits, *t.last_w)
            for k, v in t.readers.items():
                self._need(eng, waits, k, v)
        return list(waits.items())

    def _record(self, ev, reads, writes):
        for t in writes:
            t.last_w = ev
            t.readers = {}
        for t in reads:
            if t not in writes:
                if t.readers.get(ev[0], 0) < ev[1]:
                    t.readers[ev[0]] = ev[1]

    def op(self, eng, fn, reads=(), writes=(), signal=True):
        waits = self._deps(eng, reads, writes)
        key = "E:" + eng
        if signal:
            self.cnt[key] += 1
            ev = (key, self.cnt[key])
            inc = (key, 1)
            self.pending[eng] = False
        else:
            ev = (key, self.cnt[key] + 1)
            inc = None
            self.pending[eng] = True
        self._record(ev, reads, writes)
        self.ops[eng].append((waits, fn, inc))
        self.nops += 1

    def dma(self, eng, out, in_, reads=(), writes=(), semtile=None, **kw):
        waits = self._deps(eng, reads, writes)
        if semtile is None:
            semtile = writes[0] if writes else reads[0]
        key = self.dsem_for(semtile)
        self.cnt[key] += 16
        ev = (key, self.cnt[key])
        self._record(ev, reads, writes)

        def fn(e, out=out, in_=in_, kw=kw):
            return e.dma_start(out=out, in_=in_, **kw)
        self.ops[eng].append((waits, fn, (key, 16)))
        self.nops += 1

    def dma_fn(self, eng, fn, reads=(), writes=(), semtile=None):
        waits = self._deps(eng, reads, writes)
        key = self.dsem_for(semtile)
        self.cnt[key] += 16
        ev = (key, self.cnt[key])
        self._record(ev, reads, writes)
        self.ops[eng].append((waits, fn, (key, 16)))
        self.nops += 1

    def barrier(self, engs=ENGS):
        for e in ENGS:
            assert not self.pending[e], e
        for e in engs:
            waits = {}
            for key, c in self.cnt.items():
                if c > 0:
                    self._need(e, waits, key, c)
            self.ops[e].append((list(waits.items()), None, None))

    def emit(self):
        nc = self.nc
        sems = self.sems
        ops = self.ops
        with nc.Block() as block:
            def replay(e, lst):
                for ent in lst:
                    if ent[0] == "branch":
                        _, flag_ap, reg, oA, pA, oB, pB = ent
                        e.reg_load(reg, flag_ap)
                        with e.If_eq(reg, 0):
                            replay(e, oA)
                            for k, d in pA:
                                e.sem_inc(sems[k], d)
                            e.nop()
                        with e.Else():
                            replay(e, oB)
                            for k, d in pB:
                                e.sem_inc(sems[k], d)
                            e.nop()
                        continue
                    waits, fn, inc = ent
                    for key, val in waits:
                        e.wait_ge(sems[key], val)
                    if fn is None:
                        continue
                    inst = fn(e)
                    if inc is not None:
                        inst.then_inc(sems[inc[0]], inc[1])

            def mk(name):
                def body(e):
                    replay(e, ops[name])
                return body
            block.sync(mk("sync"))
            block.scalar(mk("scalar"))
            block.vector(mk("vector"))
            block.gpsimd(mk("gpsimd"))
            block.tensor(mk("tensor"))
        self.ops = {e: [] for e in ENGS}


def own_qtiles(hf):
    js = []
    for m in range(4):
        js += ([4 * m, 4 * m + 3] if hf == 0 else [4 * m + 1, 4 * m + 2])
    return js


def nk_of(i):
    return 8 * (i // 2) + (4 if i % 2 == 0 else 8)


def interleave(A, B):
    a, b = len(A), len(B)
    if a == 0:
        for f in B:
            f()
        return
    done = 0
    for k, f in enumerate(A):
        f()
        upto = ((k + 1) * b) // a
        while done < upto:
            B[done]()
            done += 1
    while done < b:
        B[done]()
        done += 1


def build(debug=(), stop_after=None, moe="sparse"):
    nc = bass.Bass("TRN2", target_bir_lowering=False)

    def din(name, shape, dt=F32):
        return nc.dram_tensor(name, list(shape), dt, kind="ExternalInput").ap()

    xb = din("xb", [4096, 1024])
    xo = din("xo", [2048, 1024])
    w_in = din("w_in", [1024, 3080])
    wqs_d = din("wqs", [1024, 512])
    wks_d = din("wks", [1024, 512])
    cosk = din("cosk", [128, 4096])
    sink = din("sink", [128, 4096])
    cosq = din("cosq", [128, 2048])
    sinq = din("sinq", [128, 2048])
    maskd_d = din("maskd", [128, 2, 1024], BF16)
    maskf_d = din("maskf", [128, 2, 1024], BF16)
    identb_d = din("identb", [128, 128], BF16)
    identf_d = din("identf", [128, 128])
    tri_d = din("tri", [128, 128])
    onesf_d = din("onesf", [128, 128])
    g_attn = din("g_attn", [1, 1024])
    g_ffn = din("g_ffn", [1, 1024])
    g_fin = din("g_fin", [1, 1024])
    bfor = din("bfor", [1, 8])
    lam_d = din("lamv", [1, 256])
    csel_d = din("csel", [1, 8 * 33])
    dng = din("dng", [1, 128])
    w_out = din("w_out", [1024, 1024])
    rw_d = din("rw", [1024, 20])
    rb_d = din("rb", [1, 20])
    wg_d = din("wg", [16, 1024, 512])
    wu_d = din("wu", [16, 1024, 512])
    wd_d = din("wd", [16, 512, 1024])
    ebase_d = din("ebase", [128, 256])
    ustrict_d = din("ustrict", [128, 128], BF16)
    onesb_d = din("onesb", [128, 128], BF16)
    xs_d = nc.dram_tensor("xs_scratch", [NSLOT, 1024], BF16).ap()
    ys_d = nc.dram_tensor("ys_scratch", [NSLOT, 1024], F32).ap()
    out = nc.dram_tensor("out", [2048, 1024], F32, kind="ExternalOutput").ap()
    dbg = {}

    def dbg_out(name, shape, dt=F32):
        if name in debug:
            dbg[name] = nc.dram_tensor("dbg_" + name, list(shape), dt, kind="ExternalOutput").ap()
            return dbg[name]
        return None

    with ExitStack() as es:
        S = Sched(nc, es)

        uniq = [0]

        def sb(sc, name, shape, dt):
            uniq[0] += 1
            return sc.enter_context(nc.sbuf_tensor("s%d_%s" % (uniq[0], name), list(shape), dt))

        banks = [es.enter_context(nc.psum_tensor("bank%d" % i, [128, 512], F32)) for i in range(8)]
        Tb = [S.tile("bank%d" % i) for i in range(8)]
        Tdbg = S.tile("dbg")

        def dump(name, src_ap, reads):
            if name in dbg:
                S.dma("sync", dbg[name], src_ap, reads=reads, writes=[Tdbg], semtile=Tdbg)

        def bcast_load(dst, src, T, n):
            S.dma("sync", dst, src.broadcast_to([128, n]), writes=[T])

        bcreg = es.enter_context(nc.gpsimd.register("bcreg"))
        brregs = {e: es.enter_context(getattr(nc, e).register("br_" + e)) for e in ENGS}

        def set_bcreg():
            S.ops["gpsimd"].append(([], lambda e: e.reg_mov(bcreg, NSLOT - 1), None))

        identb = sb(es, "identb", [128, 128], BF16); Tidb = S.tile("identb")
        S.dma("sync", identb[:], identb_d, writes=[Tidb])
        gb_attn = sb(es, "gb_attn", [128, 1024], F32); Tgba = S.tile("gba")
        bcast_load(gb_attn[:], g_attn, Tgba, 1024)
        OT = sb(es, "OT", [128, 8, 2048], BF16)
        TOT = [S.tile("OT%d" % q) for q in range(16)]

        def make_sweep(sc, pfx, pT_banks):
            st = {}
            st["xt"] = [sb(sc, pfx + "xt%d" % i, [128, 1024], F32) for i in range(4)]
            st["Txt"] = [S.tile() for _ in range(4)]
            st["hb"] = [sb(sc, pfx + "hb%d" % i, [128, 1024], BF16) for i in range(2)]
            st["Thb"] = [S.tile() for _ in range(2)]
            st["hT"] = [sb(sc, pfx + "hT%d" % i, [128, 8, 512], BF16) for i in range(3)]
            st["ThT"] = [[S.tile() for _ in range(4)] for _ in range(3)]
            st["junk"] = sb(sc, pfx + "junk", [128, 1024], BF16)
            st["Tjunk"] = S.tile()
            st["ss"] = [sb(sc, pfx + "ss%d" % i, [128, 4], F32) for i in range(3)]
            st["Tss"] = [[S.tile() for _ in range(4)] for _ in range(3)]
            st["sq"] = [sb(sc, pfx + "sq%d" % i, [128, 4], F32) for i in range(3)]
            st["Tsq"] = [S.tile() for _ in range(3)]
            st["rs"] = [sb(sc, pfx + "rs%d" % i, [128, 4], F32) for i in range(3)]
            st["Trs"] = [S.tile() for _ in range(3)]
            st["pT"] = pT_banks
            return st

        def sweep_stage1(st, x_ap, blk, gb, Tgb):
            b2 = blk % 3
            for tt in range(4):
                n = blk * 4 + tt
                xt, Txt = st["xt"][n % 4], st["Txt"][n % 4]
                S.dma("sync", xt[:], x_ap[n * 128:(n + 1) * 128, :], writes=[Txt])
                S.op("scalar", lambda e, xt=xt, tt=tt: e.activation(out=st["junk"][:], in_=xt[:], func=AF.Square,
                                                                    accum_out=st["ss"][b2][:, tt:tt + 1]),
                     reads=[Txt], writes=[st["Tss"][b2][tt], st["Tjunk"]])
            S.op("scalar", lambda e: e.activation(out=st["sq"][b2][:], in_=st["ss"][b2][:], func=AF.Sqrt,
                                                  scale=1.0 / 1024, bias=EPS),
                 reads=st["Tss"][b2], writes=[st["Tsq"][b2]])
            S.op("vector", lambda e: e.reciprocal(out=st["rs"][b2][:], in_=st["sq"][b2][:]),
                 reads=[st["Tsq"][b2]], writes=[st["Trs"][b2]])
            def stt(tt):
                n = blk * 4 + tt
                xt, Txt = st["xt"][n % 4], st["Txt"][n % 4]
                hb, Thb = st["hb"][n % 2], st["Thb"][n % 2]
                S.op("vector", lambda e, xt=xt, hb=hb, tt=tt: e.scalar_tensor_tensor(
                    out=hb[:], in0=xt[:], scalar=st["rs"][b2][:, tt:tt + 1], in1=gb[:], op0=ALU.mult, op1=ALU.mult),
                    reads=[Txt, st["Trs"][b2], Tgb], writes=[Thb])

            def tr(tt):
                n = blk * 4 + tt
                hb, Thb = st["hb"][n % 2], st["Thb"][n % 2]
                bi = st["pT"][n % 2]
                pTv = banks[bi][:].bitcast(BF16)
                for c in range(8):
                    S.op("tensor", lambda e, c=c, hb=hb, pTv=pTv: e.transpose(
                        out=pTv[:, c * 128:(c + 1) * 128], in_=hb[:, c * 128:(c + 1) * 128], identity=identb[:]),
                        reads=[Thb, Tidb], writes=[Tb[bi]], signal=(c == 7))

            def ev(tt):
                n = blk * 4 + tt
                bi = st["pT"][n % 2]
                pTv = banks[bi][:].bitcast(BF16)
                S.op("vector", lambda e, tt=tt, pTv=pTv: e.tensor_copy(
                    out=st["hT"][b2][:, :, tt * 128:(tt + 1) * 128], in_=pTv.rearrange("p (c t) -> p c t", c=8)),
                    reads=[Tb[bi]], writes=[st["ThT"][b2][tt]])
            for f, a in ((stt, 0), (stt, 1), (tr, 0), (ev, 0), (stt, 2), (tr, 1), (ev, 1), (stt, 3), (tr, 2), (ev, 2), (tr, 3), (ev, 3)):
                f(a)

        def wload(dst, src_cols, T):
            S.dma("gpsimd", dst, src_cols.rearrange("(c p) n -> p c n", p=128), writes=[T])

        def mmgroup(out_ap, pairs, reads, writes):
            n = len(pairs)
            for k, (l, r) in enumerate(pairs):
                S.op("tensor", lambda e, l=l, r=r, k=k: e.matmul(out=out_ap, lhsT=l, rhs=r, start=(k == 0), stop=(k == n - 1)),
                     reads=reads, writes=writes, signal=(k == n - 1))

        def run_attention(units, PT, TPT, KT_of, QT_of, V_of, W, exp_emit, evac_emit, mask_emit, s_banks, o_banks,
                          OTs=None, TOTs=None, smr=None, Tsmr=None, identf=None, Tidf=None, Vsum_of=None):
            gctr = [0]
            dv = W - 1
            MO = dv if Vsum_of is not None else W
            deferred = []

            def st_list(ui, u):
                buf = ui % 2
                nk = nk_of(u["i"])
                L = []
                for p in range(nk // 2):
                    def f(p=p):
                        bi = s_banks[gctr[0] % len(s_banks)]
                        gctr[0] += 1
                        for j in range(2):
                            kb = 2 * p + j
                            kap, Tk = KT_of(u, kb)
                            qap, Tq = QT_of(u)
                            S.op("tensor", lambda e, kap=kap, qap=qap, j=j, bi=bi: e.matmul(
                                out=banks[bi][:, j * 256:(j + 1) * 256], lhsT=kap, rhs=qap, start=True, stop=True),
                                reads=[Tk, Tq], writes=[Tb[bi]], signal=(j == 1))
                        exp_emit(u, p, bi, PT[buf], TPT[buf][p])
                    L.append(f)
                L.append(lambda: mask_emit(u, PT[buf], TPT[buf]))
                return L

            def pv_list(ui, u):
                buf = ui % 2
                nk = nk_of(u["i"])
                ba = o_banks[(ui % 2) * 2]
                bb = o_banks[(ui % 2) * 2 + 1]
                o2 = ui % 2
                L = []
                for k0 in range(0, nk, 4):
                    def f(k0=k0):
                        for kb in range(k0, min(nk, k0 + 4)):
                            vap, Tv = V_of(u, kb)
                            S.op("tensor", lambda e, kb=kb, vap=vap: e.matmul(
                                out=banks[ba][0:MO, 0:256], lhsT=vap, rhs=PT[buf][:, kb, :], start=(kb == 0), stop=(kb == nk - 1)),
                                reads=[TPT[buf][kb // 2], Tv], writes=[Tb[ba]], signal=(kb == nk - 1))
                    L.append(f)
                if Vsum_of is not None:
                    for k0 in range(0, nk, 4):
                        def g(k0=k0):
                            for kb in range(k0, min(nk, k0 + 4)):
                                vap, Tv = Vsum_of(u, kb)
                                S.op("tensor", lambda e, kb=kb, vap=vap: e.matmul(
                                    out=banks[bb][0:1, 256:512], lhsT=vap, rhs=PT[buf][:, kb, :], start=(kb == 0), stop=(kb == nk - 1)),
                                    reads=[TPT[buf][kb // 2], Tv], writes=[Tb[bb]], signal=(kb == nk - 1))
                        L.append(g)

                def copy_out():
                    S.op("vector", lambda e: e.tensor_copy(out=OTs[o2][0:MO, :], in_=banks[ba][0:MO, 0:256]), reads=[Tb[ba]], writes=[TOTs[o2]])
                    if Vsum_of is not None:
                        S.op("vector", lambda e: e.tensor_copy(out=smr[o2][:], in_=banks[bb][0:1, 256:512]), reads=[Tb[bb]], writes=[Tsmr[o2]])

                def transpose_back():
                    for s in range(2):
                        last = (Vsum_of is None)
                        S.op("tensor", lambda e, s=s: e.transpose(out=banks[bb][:, s * W:s * W + MO], in_=OTs[o2][0:MO, s * 128:(s + 1) * 128],
                                                                  identity=identf[0:MO, 0:MO]),
                             reads=[TOTs[o2], Tidf], writes=[Tb[bb]], signal=(last and s == 1))
                        if Vsum_of is not None:
                            S.op("tensor", lambda e, s=s: e.transpose(out=banks[bb][:, s * W + dv:s * W + W], in_=smr[o2][0:1, s * 128:(s + 1) * 128],
                                                                      identity=identf[0:1, 0:1]),
                                 reads=[Tsmr[o2], Tidf], writes=[Tb[bb]], signal=(s == 1))
                    evac_emit(u, bb)
                L.append(copy_out)
                deferred.append(transpose_back)
                return L

            for ui in range(len(units) + 1):
                A = st_list(ui, units[ui]) if ui < len(units) else []
                B = pv_list(ui - 1, units[ui - 1]) if ui >= 1 else []
                if len(deferred) > (1 if ui >= 1 else 0):
                    B.insert(min(2, len(B)), deferred.pop(0))
                interleave(A, B)
            while deferred:
                deferred.pop(0)()

        with ExitStack() as pa:
            KT = sb(pa, "KTd", [128, 4, 4096], BF16)
            TKT = [[S.tile() for _ in range(8)] for _ in range(4)]
            QT = sb(pa, "QTd", [128, 4, 2048], BF16)
            TQT = [[S.tile() for _ in range(4)] for _ in range(4)]
            Vd = sb(pa, "Vd", [128, 32, 4, 130], BF16)
            TV = [S.tile() for _ in range(32)]
            Tvones = S.tile()
            S.op("vector", lambda e: e.memset(Vd[:, :, :, 128:130], 1.0), writes=TV)
            lamt = sb(pa, "lamt", [128, 256], F32); Tlam = S.tile()
            bcast_load(lamt[:], lam_d, Tlam, 256)
            lj = sb(pa, "lj", [128, 64], F32)
            ls = sb(pa, "ls", [128, 2], F32); Tls = S.tile()
            le = sb(pa, "le", [128, 2], F32); Tle = S.tile()
            neglam = sb(pa, "neglam", [128, 1], F32); Tnl = S.tile()
            for z in range(2):
                S.op("vector", lambda e, z=z: e.scalar_tensor_tensor(
                    out=lj[:], in0=lamt[:, z * 128:z * 128 + 64], scalar=1.0, in1=lamt[:, z * 128 + 64:z * 128 + 128],
                    op0=ALU.mult, op1=ALU.mult, accum_out=ls[:, z:z + 1]), reads=[Tlam, Tls], writes=[Tls])
            S.op("scalar", lambda e: e.activation(out=le[:], in_=ls[:], func=AF.Exp), reads=[Tls], writes=[Tle])
            S.op("vector", lambda e: e.tensor_tensor(out=neglam[:], in0=le[:, 1:2], in1=le[:, 0:1], op=ALU.subtract),
                 reads=[Tle], writes=[Tnl])
            S.op("vector", lambda e: e.tensor_scalar(out=neglam[:], in0=neglam[:], scalar1=-LAM_INIT, scalar2=None, op0=ALU.add),
                 reads=[Tnl], writes=[Tnl])
            gsc = sb(pa, "gsc", [128, 128], F32); Tgsc = S.tile()
            bcast_load(gsc[:], dng, Tgsc, 128)
            S.op("vector", lambda e: e.tensor_scalar(out=gsc[:], in0=gsc[:], scalar1=1.0 - LAM_INIT, scalar2=None, op0=ALU.mult),
                 reads=[Tgsc], writes=[Tgsc])
            Txs = S.tile("xs")

            with ExitStack() as sw:
                st = make_sweep(sw, "a", [0, 1])
                wA = sb(sw, "wA", [128, 8, 512], BF16); TwA = S.tile()
                wB = sb(sw, "wB", [128, 8, 512], BF16); TwB = S.tile()
                wC = sb(sw, "wC", [128, 8, 512], BF16); TwC = S.tile()
                wload(wA[:], w_in[:, 512:1024], TwA)
                wload(wB[:], wks_d, TwB)
                wload(wC[:], w_in[:, 1024:1536], TwC)
                ct = [sb(sw, "ct%d" % i, [128, 512], F32) for i in range(2)]; Tct = [S.tile() for _ in range(2)]
                sn = [sb(sw, "sn%d" % i, [128, 512], F32) for i in range(2)]; Tsn = [S.tile() for _ in range(2)]
                t1 = [sb(sw, "t1%d" % i, [128, 512], F32) for i in range(2)]; Tt1 = [S.tile() for _ in range(2)]
                t2 = [sb(sw, "t2%d" % i, [128, 512], F32) for i in range(2)]; Tt2 = [S.tile() for _ in range(2)]
                rctr = [0]

                def rope_proj(blk, hT, ThT, wq_, Tw_, ws_, Tws_, cos_d, sin_d, dstT, TdstT):
                    b2 = blk % 2
                    S.dma("sync", ct[b2][:], cos_d[:, blk * 512:(blk + 1) * 512], writes=[Tct[b2]])
                    S.dma("sync", sn[b2][:], sin_d[:, blk * 512:(blk + 1) * 512], writes=[Tsn[b2]])
                    for h in range(4):
                        ba, bb = (2, 3) if h % 2 == 0 else (4, 5)
                        mmgroup(banks[ba][:], [(wq_[:, c, h * 128:(h + 1) * 128], hT[:, c, :]) for c in range(8)],
                                reads=list(ThT) + [Tw_], writes=[Tb[ba]])
                        mmgroup(banks[bb][:], [(ws_[:, c, h * 128:(h + 1) * 128], hT[:, c, :]) for c in range(8)],
                                reads=list(ThT) + [Tws_], writes=[Tb[bb]])
                        r = rctr[0] % 2
                        rctr[0] += 1
                        S.op("vector", lambda e, r=r, ba=ba: e.tensor_tensor(out=t1[r][:], in0=banks[ba][:], in1=ct[b2][:], op=ALU.mult),
                             reads=[Tb[ba], Tct[b2]], writes=[Tt1[r]])
                        S.op("vector", lambda e, r=r, bb=bb: e.tensor_tensor(out=t2[r][:], in0=banks[bb][:], in1=sn[b2][:], op=ALU.mult),
                             reads=[Tb[bb], Tsn[b2]], writes=[Tt2[r]])
                        S.op("gpsimd", lambda e, r=r, h=h: e.tensor_tensor(out=dstT[:, h, blk * 512:(blk + 1) * 512], in0=t1[r][:], in1=t2[r][:], op=ALU.add),
                             reads=[Tt1[r], Tt2[r]], writes=[TdstT[h][blk]])

                def kv_proj(blk, hT, ThT):
                    rope_proj(blk, hT, ThT, wA, TwA, wB, TwB, cosk, sink, KT, TKT)
                    for tt in range(4):
                        n = blk * 4 + tt
                        bv = 6 + (tt % 2)
                        mmgroup(banks[bv][:], [(hT[:, c, tt * 128:(tt + 1) * 128], wC[:, c, :]) for c in range(8)],
                                reads=[ThT[tt], TwC], writes=[Tb[bv]])
                        S.op("scalar", lambda e, n=n, bv=bv: e.activation(
                            out=Vd[:, n, :, 0:128], in_=banks[bv][:].rearrange("p (h d) -> p h d", h=4), func=AF.Copy),
                            reads=[Tb[bv]], writes=[TV[n]])

                sweep_stage1(st, xb, 0, gb_attn, Tgba)
                sweep_stage1(st, xb, 1, gb_attn, Tgba)
                for blk in range(8):
                    if blk + 2 < 8:
                        sweep_stage1(st, xb, blk + 2, gb_attn, Tgba)
                    kv_proj(blk, st["hT"][blk % 3], st["ThT"][blk % 3])
                wload(wA[:], w_in[:, 0:512], TwA)
                wload(wB[:], wqs_d, TwB)
                sweep_stage1(st, xo, 0, gb_attn, Tgba)
                sweep_stage1(st, xo, 1, gb_attn, Tgba)
                for blk in range(4):
                    if blk + 2 < 4:
                        sweep_stage1(st, xo, blk + 2, gb_attn, Tgba)
                    rope_proj(blk, st["hT"][blk % 3], st["ThT"][blk % 3], wA, TwA, wB, TwB, cosq, sinq, QT, TQT)
                S.barrier()
                if "KTd" in debug:
                    dbg_out("KTd", [128, 4, 4096], BF16); dump("KTd", KT[:], [])
                    dbg_out("QTd", [128, 4, 2048], BF16); dump("QTd", QT[:], [])
                    dbg_out("Vd", [128, 32, 4, 130], BF16); dump("Vd", Vd[:], [])
                    S.barrier()
                S.emit()
            if stop_after == "Aproj":
                S.barrier(); S.emit()
                return nc

            with ExitStack() as at:
                if moe == "sparse":
                    zer = sb(at, "zer", [128, 2048], BF16); Tzer = S.tile()
                    S.op("vector", lambda e: e.memset(zer[:], 0.0), writes=[Tzer])
                    for n in range(NSLOT // 256):
                        S.dma("sync", xs_d[n * 256:(n + 1) * 256, :].rearrange("(p r) d -> p (r d)", r=2), zer[:], reads=[Tzer], writes=[Txs], semtile=Tzer)
                maskd = sb(at, "maskd", [128, 2, 1024], BF16); Tmd = S.tile()
                S.dma("sync", maskd[:], maskd_d, writes=[Tmd])
                PT = [sb(at, "PT%d" % i, [128, 32, 256], BF16) for i in range(2)]
                TPT = [[S.tile() for _ in range(16)] for _ in range(2)]
                oc = sb(at, "oc", [128, 16, 4, 128], F32); Toc = [[S.tile() for _ in range(4)] for _ in range(16)]
                ssq = sb(at, "ssq", [128, 64], F32); Tssq = S.tile()
                A1 = [sb(at, "A1%d" % i, [128, 128], F32) for i in range(2)]; TA1 = [S.tile() for _ in range(2)]
                rr = sb(at, "rr", [128, 4], F32); Trr = [S.tile() for _ in range(4)]
                sjunk = sb(at, "sjunk", [128, 128], F32); Tsjunk = S.tile()
                units = [dict(h=h, i=i, z=z) for h in range(4) for i in range(8) for z in range(2)]

                def KT_of(u, kb):
                    r0 = 64 * u["z"]
                    return KT[r0:r0 + 64, u["h"], kb * 128:(kb + 1) * 128], TKT[u["h"]][kb // 4]

                def QT_of(u):
                    r0 = 64 * u["z"]
                    return QT[r0:r0 + 64, u["h"], u["i"] * 256:(u["i"] + 1) * 256], TQT[u["h"]][u["i"] // 2]

                def V_of(u, kb):
                    return Vd[:, kb, u["h"], 0:128], TV[kb]

                def exp_emit(u, p, bi, PTb, Tp):
                    S.op("scalar", lambda e: e.activation(out=PTb[:, 2 * p:2 * p + 2, :].rearrange("p a b -> p (a b)"),
                                                          in_=banks[bi][:], func=AF.Exp),
                         reads=[Tb[bi]], writes=[Tp])

                def mask_emit(u, PTb, TPb):
                    i = u["i"]
                    lo = nk_of(i) - 4
                    S.op("vector", lambda e: e.tensor_tensor(out=PTb[:, lo:lo + 4, :], in0=PTb[:, lo:lo + 4, :],
                                                             in1=maskd[:, i % 2, :].rearrange("p (a b) -> p a b", a=4), op=ALU.min),
                         reads=[Tmd, TPb[lo // 2], TPb[lo // 2 + 1]], writes=[TPb[lo // 2], TPb[lo // 2 + 1]])

                def evac_emit(u, ob):
                    h, i, z = u["h"], u["i"], u["z"]
                    for s in range(2):
                        qb = 2 * i + s
                        o_ap = banks[ob][:, s * 129:s * 129 + 128]
                        sm_ap = banks[ob][:, s * 129 + 128:s * 129 + 129]
                        ri = 2 * z + s
                        S.op("vector", lambda e, ri=ri, sm_ap=sm_ap: e.reciprocal(out=rr[:, ri:ri + 1], in_=sm_ap),
                             reads=[Tb[ob]], writes=[Trr[ri]])
                        if z == 0:
                            S.op("vector", lambda e, ri=ri, o_ap=o_ap, s=s: e.tensor_scalar(
                                out=A1[s][:], in0=o_ap, scalar1=rr[:, ri:ri + 1], scalar2=None, op0=ALU.mult),
                                reads=[Tb[ob], Trr[ri]], writes=[TA1[s]])
                        else:
                            S.op("vector", lambda e, ri=ri: e.tensor_tensor(out=rr[:, ri:ri + 1], in0=rr[:, ri:ri + 1], in1=neglam[:], op=ALU.mult),
                                 reads=[Trr[ri], Tnl], writes=[Trr[ri]])
                            S.op("vector", lambda e, ri=ri, o_ap=o_ap, s=s, qb=qb: e.scalar_tensor_tensor(
                                out=oc[:, qb, h, :], in0=o_ap, scalar=rr[:, ri:ri + 1], in1=A1[s][:], op0=ALU.mult, op1=ALU.add),
                                reads=[Tb[ob], Trr[ri], TA1[s]], writes=[Toc[qb][h]])
                            S.op("vector", lambda e, qb=qb: e.scalar_tensor_tensor(
                                out=sjunk[:], in0=oc[:, qb, h, :], scalar=1.0, in1=oc[:, qb, h, :], op0=ALU.mult, op1=ALU.mult,
                                accum_out=ssq[:, qb * 4 + h:qb * 4 + h + 1]),
                                reads=[Toc[qb][h], Tssq], writes=[Tssq, Tsjunk])

                OTs = [sb(at, "OTs%d" % i, [128, 256], F32) for i in range(2)]; TOTs = [S.tile() for _ in range(2)]
                smr = [sb(at, "smr%d" % i, [1, 256], F32) for i in range(2)]; Tsmr = [S.tile() for _ in range(2)]
                identf_a = sb(at, "identf_a", [128, 128], F32); Tidf_a = S.tile()
                S.dma("sync", identf_a[:], identf_d, writes=[Tidf_a])

                def Vsum_of(u, kb):
                    return Vd[:, kb, u["h"], 128:129], TV[kb]
                run_attention(units, PT, TPT, KT_of, QT_of, V_of, 129, exp_emit, evac_emit, mask_emit,
                              s_banks=[0, 1, 2, 3], o_banks=[4, 5, 6, 7], OTs=OTs, TOTs=TOTs, smr=smr, Tsmr=Tsmr,
                              identf=identf_a, Tidf=Tidf_a, Vsum_of=Vsum_of)
                S.op("scalar", lambda e: e.activation(out=ssq[:], in_=ssq[:], func=AF.Sqrt, scale=1.0 / 128, bias=EPS),
                     reads=[Tssq], writes=[Tssq])
                S.op("vector", lambda e: e.reciprocal(out=ssq[:], in_=ssq[:]), reads=[Tssq], writes=[Tssq])
                Otok = [sb(at, "Otok%d" % i, [128, 512], BF16) for i in range(2)]; TOtok = [S.tile() for _ in range(2)]
                for qb in range(16):
                    o2 = qb % 2
                    for h in range(4):
                        S.op("vector", lambda e, qb=qb, h=h, o2=o2: e.scalar_tensor_tensor(
                            out=Otok[o2][:, h * 128:(h + 1) * 128], in0=oc[:, qb, h, :], scalar=ssq[:, qb * 4 + h:qb * 4 + h + 1],
                            in1=gsc[:], op0=ALU.mult, op1=ALU.mult),
                            reads=[Toc[qb][h], Tssq, Tgsc], writes=[TOtok[o2]])
                    bi = o2
                    pTv = banks[bi][:].bitcast(BF16)
                    for c in range(4):
                        S.op("tensor", lambda e, c=c, o2=o2, pTv=pTv: e.transpose(
                            out=pTv[:, c * 128:(c + 1) * 128], in_=Otok[o2][:, c * 128:(c + 1) * 128], identity=identb[:]),
                            reads=[TOtok[o2], Tidb], writes=[Tb[bi]], signal=(c == 3))
                    S.op("vector", lambda e, qb=qb, pTv=pTv: e.tensor_copy(
                        out=OT[:, 0:4, qb * 128:(qb + 1) * 128], in_=pTv[:, 0:512].rearrange("p (c t) -> p c t", c=4)),
                        reads=[Tb[bi]], writes=[TOT[qb]])
                S.barrier()
                if "OTa" in debug:
                    dbg_out("OTa", [128, 8, 2048], BF16); dump("OTa", OT[:], []); S.barrier()
                S.emit()
        if stop_after == "A":
            return nc

        with ExitStack() as pb:
            KT = sb(pb, "KTf", [128, 4, 4096], BF16)
            TKT = [[S.tile() for _ in range(8)] for _ in range(4)]
            QT = sb(pb, "QTf", [128, 4, 2048], BF16)
            TQT = [[S.tile() for _ in range(4)] for _ in range(4)]
            Vf = sb(pb, "Vf", [128, 32, 8, 66], BF16)
            TV = [S.tile() for _ in range(32)]
            S.op("vector", lambda e: e.memset(Vf[:, :, :, 64:66], 1.0), writes=TV)
            zt = sb(pb, "zt", [128, 32, 8], F32); Tzt = [S.tile() for _ in range(32)]
            Fpos = sb(pb, "Fpos", [128, 32, 8], F32); TF = [S.tile() for _ in range(32)]
            Cpos = sb(pb, "Cpos", [128, 33, 8], F32); TC = [S.tile() for _ in range(33)]
            maskf = sb(pb, "maskf", [128, 2, 1024], BF16); Tmf = S.tile()
            S.dma("sync", maskf[:], maskf_d, writes=[Tmf])
            bfb = sb(pb, "bfb", [128, 8], F32); Tbfb = S.tile()
            bcast_load(bfb[:], bfor, Tbfb, 8)
            csel = sb(pb, "csel", [128, 8, 33], F32); Tcsel = S.tile()
            bcast_load(csel[:].rearrange("p a b -> p (a b)"), csel_d, Tcsel, 8 * 33)
            ctmp = sb(pb, "ctmp", [128, 8, 33], F32); Tctmp = S.tile()
            cq = sb(pb, "cq", [128, 8, 8], F32); Tcq = S.tile()
            tri = sb(pb, "tri", [128, 128], F32); Ttri = S.tile()
            S.dma("sync", tri[:], tri_d, writes=[Ttri])
            onesf = sb(pb, "onesf", [128, 128], F32); Tones = S.tile()
            S.dma("sync", onesf[:], onesf_d, writes=[Tones])

            with ExitStack() as sw:
                st = make_sweep(sw, "b", [0, 1])
                wA = sb(sw, "wA", [128, 8, 512], BF16); TwA = S.tile()
                wB = sb(sw, "wB", [128, 8, 512], BF16); TwB = S.tile()
                wF = sb(sw, "wF", [128, 8, 8], BF16); TwF = S.tile()
                wload(wA[:], w_in[:, 2048:2560], TwA)
                wload(wB[:], w_in[:, 2560:3072], TwB)
                wload(wF[:], w_in[:, 3072:3080], TwF)
                kctr = [0]

                def plain_proj(blk, hT, ThT, w_, Tw_, dstT, TdstT, scale):
                    for hp in range(4):
                        bk = 2 + (kctr[0] % 3)
                        kctr[0] += 1
                        mmgroup(banks[bk][:], [(w_[:, c, hp * 128:(hp + 1) * 128], hT[:, c, :]) for c in range(8)],
                                reads=list(ThT) + [Tw_], writes=[Tb[bk]])
                        S.op("scalar", lambda e, bk=bk, hp=hp: e.activation(
                            out=dstT[:, hp, blk * 512:(blk + 1) * 512], in_=banks[bk][:], func=AF.Copy, scale=scale),
                            reads=[Tb[bk]], writes=[TdstT[hp][blk]])

                def kvf_proj(blk, hT, ThT):
                    plain_proj(blk, hT, ThT, wA, TwA, KT, TKT, 1.0)
                    for tt in range(4):
                        n = blk * 4 + tt
                        bv = 6 + (tt % 2)
                        mmgroup(banks[bv][:], [(hT[:, c, tt * 128:(tt + 1) * 128], wB[:, c, :]) for c in range(8)],
                                reads=[ThT[tt], TwB], writes=[Tb[bv]])
                        S.op("vector", lambda e, n=n, bv=bv: e.tensor_copy(
                            out=Vf[:, n, :, 0:64], in_=banks[bv][:].rearrange("p (h d) -> p h d", h=8)),
                            reads=[Tb[bv]], writes=[TV[n]])
                        mmgroup(banks[5][:, 0:8], [(hT[:, c, tt * 128:(tt + 1) * 128], wF[:, c, :]) for c in range(8)],
                                reads=[ThT[tt], TwF], writes=[Tb[5]])
                        S.op("vector", lambda e, n=n: e.tensor_tensor(out=zt[:, n, :], in0=banks[5][:, 0:8], in1=bfb[:], op=ALU.add),
                             reads=[Tb[5], Tbfb], writes=[Tzt[n]])

                sweep_stage1(st, xb, 0, gb_attn, Tgba)
                sweep_stage1(st, xb, 1, gb_attn, Tgba)
                for blk in range(8):
                    if blk + 2 < 8:
                        sweep_stage1(st, xb, blk + 2, gb_attn, Tgba)
                    kvf_proj(blk, st["hT"][blk % 3], st["ThT"][blk % 3])
                wload(wA[:], w_in[:, 1536:2048], TwA)
                sweep_stage1(st, xo, 0, gb_attn, Tgba)
                sweep_stage1(st, xo, 1, gb_attn, Tgba)
                for blk in range(4):
                    if blk + 2 < 4:
                        sweep_stage1(st, xo, blk + 2, gb_attn, Tgba)
                    plain_proj(blk, st["hT"][blk % 3], st["ThT"][blk % 3], wA, TwA, QT, TQT, 0.125)
                ztf = zt[:].rearrange("p a b -> p (a b)")
                S.op("scalar", lambda e: e.activation(out=ztf, in_=ztf, func=AF.Exp, scale=-1.0), reads=Tzt, writes=Tzt)
                S.op("scalar", lambda e: e.activation(out=ztf, in_=ztf, func=AF.Ln, bias=1.0), reads=Tzt, writes=Tzt)
                S.op("vector", lambda e: e.memset(Cpos[:, 0, :], 0.0), writes=[TC[0]])
                for n in range(32):
                    bc = 2 + (n % 2)
                    S.op("tensor", lambda e, n=n, bc=bc: e.matmul(out=banks[bc][:, 0:8], lhsT=tri[:], rhs=zt[:, n, :], start=True, stop=True),
                         reads=[Ttri, Tzt[n]], writes=[Tb[bc]], signal=False)
                    S.op("tensor", lambda e, n=n, bc=bc: e.matmul(out=banks[bc][:, 8:16], lhsT=onesf[:], rhs=zt[:, n, :], start=True, stop=True),
                         reads=[Tones, Tzt[n]], writes=[Tb[bc]], signal=True)
                    S.op("vector", lambda e, n=n, bc=bc: e.tensor_tensor(out=Fpos[:, n, :], in0=banks[bc][:, 0:8], in1=Cpos[:, n, :], op=ALU.add),
                         reads=[Tb[bc], TC[n]], writes=[TF[n]])
                    S.op("vector", lambda e, n=n, bc=bc: e.tensor_tensor(out=Cpos[:, n + 1, :], in0=banks[bc][:, 8:16], in1=Cpos[:, n, :], op=ALU.add),
                         reads=[Tb[bc], TC[n]], writes=[TC[n + 1]])
                for i in range(8):
                    S.op("vector", lambda e, i=i: e.tensor_tensor(out=ctmp[:], in0=Cpos[:].rearrange("p n h -> p h n"),
                                                                  in1=csel[:, i, :].unsqueeze(1).broadcast_to([128, 8, 33]), op=ALU.mult),
                         reads=TC + [Tcsel, Tctmp], writes=[Tctmp])
                    S.op("vector", lambda e, i=i: e.tensor_reduce(out=cq[:, i, :], in_=ctmp[:], axis=AX.X, op=ALU.add),
                         reads=[Tctmp], writes=[Tcq])
                S.barrier()
                if "Fpos" in debug:
                    dbg_out("Fpos", [128, 32, 8]); dump("Fpos", Fpos[:], []); S.barrier()
                S.emit()

            with ExitStack() as at:
                PT = [sb(at, "PT%d" % i, [128, 32, 256], BF16) for i in range(2)]
                TPT = [[S.tile() for _ in range(16)] for _ in range(2)]
                Otf = sb(at, "Otf", [128, 16, 512], BF16); TOtf = [S.tile() for _ in range(16)]
                rr = sb(at, "rrf", [128, 2], F32); Trr = [S.tile() for _ in range(2)]
                biasb = [sb(at, "biasb%d" % i, [128, 32], F32) for i in range(2)]; Tbias = [S.tile() for _ in range(2)]
                units = [dict(hp=hp, hh=hh, i=i, head=2 * hp + hh) for hp in range(4) for hh in range(2) for i in range(8)]
                for ui, u in enumerate(units):
                    u["ui"] = ui

                def KT_of(u, kb):
                    r0 = 64 * u["hh"]
                    return KT[r0:r0 + 64, u["hp"], kb * 128:(kb + 1) * 128], TKT[u["hp"]][kb // 4]

                def QT_of(u):
                    r0 = 64 * u["hh"]
                    return QT[r0:r0 + 64, u["hp"], u["i"] * 256:(u["i"] + 1) * 256], TQT[u["hp"]][u["i"] // 2]

                ebb = [sb(at, "ebb%d" % i, [128, 32], F32) for i in range(2)]; Teb = [S.tile() for _ in range(2)]
                Vp = [sb(at, "Vp%d" % i, [128, 32, 65], BF16) for i in range(2)]; TVp = [S.tile() for _ in range(2)]

                def V_of(u, kb):
                    return Vp[u["ui"] % 2][:, kb, :], TVp[u["ui"] % 2]

                def exp_emit(u, p, bi, PTb, Tp):
                    b2 = u["ui"] % 2
                    hd = u["head"]
                    nk = nk_of(u["i"])
                    if p == 0:
                        S.op("vector", lambda e: e.tensor_scalar(out=biasb[b2][:, 0:nk], in0=Fpos[:, 0:nk, hd], scalar1=cq[:, u["i"], hd:hd + 1],
                                                                 scalar2=70.0, op0=ALU.subtract, op1=ALU.min),
                             reads=TF[0:nk] + [Tcq], writes=[Tbias[b2]])
                        S.op("scalar", lambda e: e.activation(out=ebb[b2][:, 0:nk], in_=biasb[b2][:, 0:nk], func=AF.Exp),
                             reads=[Tbias[b2]], writes=[Teb[b2]])
                        S.op("vector", lambda e: e.tensor_tensor(out=Vp[b2][:, 0:nk, :], in0=Vf[:, 0:nk, hd, 0:65],
                                                                 in1=ebb[b2][:, 0:nk].unsqueeze(2).broadcast_to([128, nk, 65]), op=ALU.mult),
                             reads=TV[0:nk] + [Teb[b2]], writes=[TVp[b2]])
                    S.op("scalar", lambda e: e.activation(out=PTb[:, 2 * p:2 * p + 2, :].rearrange("p a b -> p (a b)"),
                                                          in_=banks[bi][:], func=AF.Exp),
                         reads=[Tb[bi]], writes=[Tp])

                def mask_emit(u, PTb, TPb):
                    i = u["i"]
                    lo = nk_of(i) - 4
                    S.op("vector", lambda e: e.tensor_tensor(out=PTb[:, lo:lo + 4, :], in0=PTb[:, lo:lo + 4, :],
                                                             in1=maskf[:, i % 2, :].rearrange("p (a b) -> p a b", a=4), op=ALU.min),
                         reads=[Tmf, TPb[lo // 2], TPb[lo // 2 + 1]], writes=[TPb[lo // 2], TPb[lo // 2 + 1]])

                def evac_emit(u, ob):
                    hd, i = u["head"], u["i"]
                    for s in range(2):
                        qb = 2 * i + s
                        S.op("vector", lambda e, s=s: e.reciprocal(out=rr[:, s:s + 1], in_=banks[ob][:, s * 65 + 64:s * 65 + 65]),
                             reads=[Tb[ob]], writes=[Trr[s]])
                        S.op("vector", lambda e, s=s, qb=qb: e.tensor_scalar(
                            out=Otf[:, qb, hd * 64:(hd + 1) * 64], in0=banks[ob][:, s * 65:s * 65 + 64], scalar1=rr[:, s:s + 1],
                            scalar2=None, op0=ALU.mult),
                            reads=[Tb[ob], Trr[s]], writes=[TOtf[qb]])

                OTs = [sb(at, "OTsf%d" % i, [128, 256], F32) for i in range(2)]; TOTs = [S.tile() for _ in range(2)]
                identf_b = sb(at, "identf_b", [128, 128], F32); Tidf_b = S.tile()
                S.dma("sync", identf_b[:], identf_d, writes=[Tidf_b])
                run_attention(units, PT, TPT, KT_of, QT_of, V_of, 65, exp_emit, evac_emit, mask_emit,
                              s_banks=[0, 1, 2, 3], o_banks=[4, 5, 6, 7], OTs=OTs, TOTs=TOTs, identf=identf_b, Tidf=Tidf_b)
                for qb in range(16):
                    bi = qb % 2
                    pTv = banks[bi][:].bitcast(BF16)
                    for c in range(4):
                        S.op("tensor", lambda e, c=c, qb=qb, pTv=pTv: e.transpose(
                            out=pTv[:, c * 128:(c + 1) * 128], in_=Otf[:, qb, c * 128:(c + 1) * 128], identity=identb[:]),
                            reads=[TOtf[qb], Tidb], writes=[Tb[bi]], signal=(c == 3))
                    S.op("vector", lambda e, qb=qb, pTv=pTv: e.tensor_copy(
                        out=OT[:, 4:8, qb * 128:(qb + 1) * 128], in_=pTv[:, 0:512].rearrange("p (c t) -> p c t", c=4)),
                        reads=[Tb[bi]], writes=[TOT[qb]])
                S.barrier()
                if "OTb" in debug:
                    dbg_out("OTb", [128, 8, 2048], BF16); dump("OTb", OT[:], []); S.barrier()
                S.emit()
        if stop_after == "B":
            return nc

        with ExitStack() as pc:
            x2 = sb(pc, "x2", [128, 16, 1024], F32); Tx2 = [[S.tile() for _ in range(2)] for _ in range(16)]
            hmT = OT
            ThmT = TOT
            ovf = sb(pc, "ovf", [128, 1], I32); Tovf = S.tile()
            w12 = sb(pc, "w12", [128, 2, 16], F32); Tw12 = S.tile()
            pos = sb(pc, "pos", [128, 2, 16], I32); Tpos = S.tile()
            comb = sb(pc, "comb", [128, 16, 16], F32); Tcomb = S.tile()
            junk = sb(pc, "junkc", [128, 1024], BF16); Tjunkc = S.tile()
            ssc = sb(pc, "ssc", [128, 16], F32); Tssc = [S.tile() for _ in range(16)]; Tsscall = S.tile()
            with ExitStack() as c1:
                wo = sb(c1, "wo", [128, 8, 1024], BF16); Two = S.tile()
                wload(wo[:], w_out, Two)
                xt = [sb(c1, "xc%d" % i, [128, 1024], F32) for i in range(2)]; Txt = [S.tile() for _ in range(2)]
                rw32 = sb(c1, "rw32", [128, 8, 20], F32); Trw = S.tile()
                S.dma("sync", rw32[:], rw_d.rearrange("(c p) n -> p c n", p=128), writes=[Trw])
                rbb = sb(c1, "rbb", [128, 20], F32); Trbb = S.tile()
                bcast_load(rbb[:], rb_d, Trbb, 20)
                gbf = sb(c1, "gbf", [128, 1024], F32); Tgbf = S.tile()
                bcast_load(gbf[:], g_ffn, Tgbf, 1024)
                identf = sb(c1, "identf", [128, 128], F32); Tidf = S.tile()
                S.dma("sync", identf[:], identf_d, writes=[Tidf])
                hm32 = [sb(c1, "hm32%d" % i, [128, 1024], F32) for i in range(2)]; Thm32 = [S.tile() for _ in range(2)]
                hmT32 = [sb(c1, "hmT32%d" % i, [128, 8, 128], F32) for i in range(2)]; ThmT32 = [S.tile() for _ in range(2)]
                Lall = sb(c1, "Lall", [128, 16, 20], F32); TL = [S.tile() for _ in range(16)]
                if moe == "sparse":
                    hmb = sb(c1, "hmb", [128, 16, 1024], BF16); Thmb = [S.tile() for _ in range(16)]
                for t in range(16):
                    S.dma("sync", xt[t % 2][:], xo[t * 128:(t + 1) * 128, :], writes=[Txt[t % 2]])
                    for hf in range(2):
                        mmgroup(banks[hf][:], [(OT[:, c, t * 128:(t + 1) * 128], wo[:, c, hf * 512:(hf + 1) * 512]) for c in range(8)],
                                reads=[TOT[t], Two], writes=[Tb[hf]])
                        S.op("vector", lambda e, t=t, hf=hf: e.tensor_tensor(
                            out=x2[:, t, hf * 512:(hf + 1) * 512], in0=banks[hf][:], in1=xt[t % 2][:, hf * 512:(hf + 1) * 512], op=ALU.add),
                            reads=[Tb[hf], Txt[t % 2]], writes=[Tx2[t][hf]])
                    S.op("scalar", lambda e, t=t: e.activation(out=junk[:], in_=x2[:, t, :], func=AF.Square, accum_out=ssc[:, t:t + 1]),
                         reads=Tx2[t], writes=[Tssc[t], Tjunkc])
                if "x2" in debug:
                    dbg_out("x2", [2048, 1024])
                    for t in range(16):
                        dump("x2", x2[:, t, :], Tx2[t]) if False else S.dma("sync", dbg["x2"][t * 128:(t + 1) * 128, :], x2[:, t, :], reads=Tx2[t], writes=[Tdbg], semtile=Tdbg)
                if stop_after == "C0":
                    S.barrier(); S.emit()
                    return nc
                S.op("scalar", lambda e: e.activation(out=ssc[:], in_=ssc[:], func=AF.Sqrt, scale=1.0 / 1024, bias=EPS),
                     reads=Tssc, writes=[Tsscall])
                S.op("vector", lambda e: e.reciprocal(out=ssc[:], in_=ssc[:]), reads=[Tsscall], writes=[Tsscall])
                for t in range(16):
                    t2 = t % 2
                    S.op("vector", lambda e, t=t, t2=t2: e.scalar_tensor_tensor(
                        out=hm32[t2][:], in0=x2[:, t, :], scalar=ssc[:, t:t + 1], in1=gbf[:], op0=ALU.mult, op1=ALU.mult),
                        reads=Tx2[t] + [Tsscall, Tgbf], writes=[Thm32[t2]])
                    ba, bb = (2, 3) if t2 == 0 else (4, 5)
                    for c in range(8):
                        bk = ba if c < 4 else bb
                        S.op("tensor", lambda e, c=c, t2=t2, bk=bk: e.transpose(
                            out=banks[bk][:, (c % 4) * 128:(c % 4 + 1) * 128], in_=hm32[t2][:, c * 128:(c + 1) * 128], identity=identf[:]),
                            reads=[Thm32[t2], Tidf], writes=[Tb[bk]], signal=(c % 4 == 3))
                    import os
                    CUT = int(os.environ.get("C1CUT", "9"))
                    if CUT < 2:
                        continue
                    for k, bk in enumerate((ba, bb)):
                        src = banks[bk][:].rearrange("p (c t) -> p c t", c=4)
                        S.op("scalar", lambda e, k=k, t2=t2, src=src: e.activation(out=hmT32[t2][:, 4 * k:4 * k + 4, :], in_=src, func=AF.Copy),
                             reads=[Tb[bk]], writes=[ThmT32[t2]])
                        S.op("vector", lambda e, k=k, t=t, t2=t2: e.tensor_copy(out=hmT[:, 4 * k:4 * k + 4, t * 128:(t + 1) * 128], in_=hmT32[t2][:, 4 * k:4 * k + 4, :]),
                             reads=[ThmT32[t2]], writes=[ThmT[t]])
                    if moe == "sparse":
                        S.op("gpsimd", lambda e, t=t, t2=t2: e.tensor_copy(out=hmb[:, t, :], in_=hm32[t2][:]), reads=[Thm32[t2]], writes=[Thmb[t]])
                    if CUT < 3:
                        continue
                    br = 6 + t2
                    mmgroup(banks[br][:, 0:20], [(hmT32[t2][:, c, :], rw32[:, c, :]) for c in range(8)],
                            reads=[ThmT32[t2], Trw], writes=[Tb[br]])
                    S.op("vector", lambda e, t=t, br=br: e.tensor_tensor(out=Lall[:, t, :], in0=banks[br][:, 0:20], in1=rbb[:], op=ALU.add),
                         reads=[Tb[br], Trbb], writes=[TL[t]])
                if stop_after == "C1a":
                    if "Lall" in debug:
                        dbg_out("Lall", [128, 320])
                        S.dma("sync", dbg["Lall"], Lall[:].rearrange("p a b -> p (a b)"), reads=[], writes=[Tdbg], semtile=Tdbg)
                    S.barrier(); S.emit()
                    return nc
                TR = S.tile()

                def rt(name, shape):
                    return sb(c1, "rt_" + name, shape, F32)
                gmax = rt("gmax", [128, 16]); gm = rt("gm", [128, 16, 4]); gd = rt("gd", [128, 16, 4])
                gsum = rt("gsum", [128, 16]); gw = rt("gw", [128, 16]); pen = rt("pen", [128, 16, 4])
                EL = rt("EL", [128, 16, 16]); EL2 = rt("EL2", [128, 16, 16]); m1 = rt("m1", [128, 16]); m2 = rt("m2", [128, 16])
                oh1 = rt("oh1", [128, 16, 16]); oh2 = rt("oh2", [128, 16, 16]); dd = rt("dd", [128, 16]); w1 = rt("w1", [128, 16]); w2 = rt("w2", [128, 16])
                LG = Lall[:, :, 0:4]
                LE4 = Lall[:, :, 4:20].rearrange("p t (g e) -> p t g e", g=4)
                EL4 = EL[:].rearrange("p t (g e) -> p t g e", g=4)

                def vop(fn, first=False):
                    S.op("vector", fn, reads=(TL + [TR]) if first else [TR], writes=[TR])

                def bc3(a, n):
                    return a[:].unsqueeze(2).broadcast_to([128, 16, n])
                vop(lambda e: e.tensor_reduce(out=gmax[:], in_=LG, axis=AX.X, op=ALU.max), first=True)
                vop(lambda e: e.tensor_tensor(out=gm[:], in0=LG, in1=bc3(gmax, 4), op=ALU.is_equal))
                vop(lambda e: e.tensor_tensor(out=gd[:], in0=LG, in1=bc3(gmax, 4), op=ALU.subtract))
                S.op("scalar", lambda e: e.activation(out=gd[:], in_=gd[:], func=AF.Exp), reads=[TR], writes=[TR])
                vop(lambda e: e.tensor_reduce(out=gsum[:], in_=gd[:], axis=AX.X, op=ALU.add))
                vop(lambda e: e.reciprocal(out=gw[:], in_=gsum[:]))
                vop(lambda e: e.tensor_scalar(out=pen[:], in0=gm[:], scalar1=1.0, scalar2=1e30, op0=ALU.subtract, op1=ALU.mult))
                vop(lambda e: e.tensor_tensor(out=EL4, in0=LE4, in1=gm[:].unsqueeze(3).broadcast_to([128, 16, 4, 4]), op=ALU.mult))
                vop(lambda e: e.tensor_tensor(out=EL4, in0=EL4, in1=pen[:].unsqueeze(3).broadcast_to([128, 16, 4, 4]), op=ALU.add))
                vop(lambda e: e.tensor_reduce(out=m1[:], in_=EL[:], axis=AX.X, op=ALU.max))
                vop(lambda e: e.tensor_tensor(out=oh1[:], in0=EL[:], in1=bc3(m1, 16), op=ALU.is_equal))
                vop(lambda e: e.scalar_tensor_tensor(out=EL2[:], in0=oh1[:], scalar=-1e30, in1=EL[:], op0=ALU.mult, op1=ALU.add))
                vop(lambda e: e.tensor_reduce(out=m2[:], in_=EL2[:], axis=AX.X, op=ALU.max))
                vop(lambda e: e.tensor_tensor(out=oh2[:], in0=EL2[:], in1=bc3(m2, 16), op=ALU.is_equal))
                vop(lambda e: e.tensor_tensor(out=dd[:], in0=m2[:], in1=m1[:], op=ALU.subtract))
                S.op("scalar", lambda e: e.activation(out=dd[:], in_=dd[:], func=AF.Exp), reads=[TR], writes=[TR])
                vop(lambda e: e.tensor_scalar(out=w1[:], in0=dd[:], scalar1=1.0, scalar2=None, op0=ALU.add))
                vop(lambda e: e.reciprocal(out=w1[:], in_=w1[:]))
                vop(lambda e: e.tensor_tensor(out=w1[:], in0=w1[:], in1=gw[:], op=ALU.mult))
                vop(lambda e: e.tensor_tensor(out=w2[:], in0=dd[:], in1=w1[:], op=ALU.mult))
                if moe == "sparse":
                    Mb = sb(c1, "Mb", [128, 16, 16], BF16)
                    ustrict = sb(c1, "ustrict", [128, 128], BF16); Tus = S.tile()
                    S.dma("sync", ustrict[:], ustrict_d, writes=[Tus])
                    onesb = sb(c1, "onesb", [128, 128], BF16); Tob_ = S.tile()
                    S.dma("sync", onesb[:], onesb_d, writes=[Tob_])
                    ebase = sb(c1, "ebase", [128, 16, 16], F32); Teb = S.tile()
                    S.dma("sync", ebase[:].rearrange("p a b -> p (a b)"), ebase_d, writes=[Teb])
                    slotf = rt("slotf", [128, 16, 16]); okf = rt("okf", [128, 16, 16]); posf = rt("posf", [128, 2, 16])
                    vop(lambda e: e.tensor_tensor(out=Mb[:], in0=oh1[:], in1=oh2[:], op=ALU.add))
                    for t in range(16):
                        prs = [(onesb[:], Mb[:, tp, :]) for tp in range(t)] + [(ustrict[:], Mb[:, t, :])]
                        n_ = len(prs)
                        for k_, (l_, r_) in enumerate(prs):
                            S.op("tensor", lambda e, l_=l_, r_=r_, k_=k_, n_=n_, t=t: e.matmul(out=banks[0][:, t * 16:(t + 1) * 16], lhsT=l_, rhs=r_,
                                                                                         start=(k_ == 0), stop=(k_ == n_ - 1)),
                                 reads=[TR, Tus, Tob_], writes=[Tb[0]], signal=(k_ == n_ - 1))
                    for tp in range(16):
                        S.op("tensor", lambda e, tp=tp: e.matmul(out=banks[1][:, 0:16], lhsT=onesb[:], rhs=Mb[:, tp, :], start=(tp == 0), stop=(tp == 15)),
                             reads=[TR, Tob_], writes=[Tb[1]], signal=(tp == 15))
                    cmax = rt("cmax", [128, 1])
                    S.op("vector", lambda e: e.tensor_reduce(out=cmax[:], in_=banks[1][:, 0:16], axis=AX.X, op=ALU.max), reads=[Tb[1], TR], writes=[TR])
                    import os as _os
                    thr = -1.0 if _os.environ.get("FORCE_DENSE") else float(CAP)
                    vop(lambda e: e.tensor_scalar(out=cmax[:], in0=cmax[:], scalar1=thr, scalar2=None, op0=ALU.is_gt))
                    S.op("vector", lambda e: e.tensor_copy(out=ovf[:], in_=cmax[:]), reads=[TR], writes=[Tovf])
                    rank = banks[0][:, 0:256].rearrange("p (a b) -> p a b", a=16)
                    S.op("vector", lambda e: e.tensor_tensor(out=slotf[:], in0=rank, in1=ebase[:], op=ALU.add), reads=[Tb[0], Teb, TR], writes=[TR])
                    vop(lambda e: e.tensor_scalar(out=okf[:], in0=slotf[:], scalar1=None, scalar2=None, op0=ALU.bypass) if False else
                        e.tensor_tensor(out=okf[:], in0=slotf[:], in1=ebase[:], op=ALU.subtract))
                    vop(lambda e: e.tensor_scalar(out=okf[:], in0=okf[:], scalar1=float(CAP), scalar2=1.0e6, op0=ALU.is_ge, op1=ALU.mult))
                    vop(lambda e: e.tensor_tensor(out=slotf[:], in0=slotf[:], in1=okf[:], op=ALU.add))
                    vop(lambda e: e.tensor_tensor(out=okf[:], in0=slotf[:], in1=oh1[:], op=ALU.mult))
                    vop(lambda e: e.tensor_reduce(out=posf[:, 0, :], in_=okf[:], axis=AX.X, op=ALU.add))
                    vop(lambda e: e.tensor_tensor(out=okf[:], in0=slotf[:], in1=oh2[:], op=ALU.mult))
                    vop(lambda e: e.tensor_reduce(out=posf[:, 1, :], in_=okf[:], axis=AX.X, op=ALU.add))
                    S.op("vector", lambda e: e.tensor_copy(out=pos[:], in_=posf[:]), reads=[TR], writes=[Tpos])
                    S.op("vector", lambda e: e.tensor_copy(out=w12[:, 0, :], in_=w1[:]), reads=[TR, Tw12], writes=[Tw12])
                    S.op("vector", lambda e: e.tensor_copy(out=w12[:, 1, :], in_=w2[:]), reads=[TR, Tw12], writes=[Tw12])
                    Tsc = [S.tile() for _ in range(32)]
                    set_bcreg()
                    for t in range(16):
                        for k_ in range(2):
                            S.dma_fn("gpsimd", lambda e, t=t, k_=k_: e.indirect_dma_start(
                                out=xs_d[:, :], out_offset=bass.IndirectOffsetOnAxis(ap=pos[:, k_, t:t + 1], axis=0),
                                in_=hmb[:, t, :], in_offset=None, bounds_check=bcreg, oob_is_err=False),
                                reads=[Thmb[t], Tpos, Txs], writes=[Tsc[2 * t + k_]], semtile=Thmb[t])
                vop(lambda e: e.tensor_tensor(out=oh1[:], in0=oh1[:], in1=bc3(w1, 16), op=ALU.mult))
                vop(lambda e: e.tensor_tensor(out=oh2[:], in0=oh2[:], in1=bc3(w2, 16), op=ALU.mult))
                S.op("vector", lambda e: e.tensor_tensor(out=comb[:], in0=oh1[:], in1=oh2[:], op=ALU.add), reads=[TR], writes=[Tcomb])
                S.barrier()
                if "comb" in debug:
                    dbg_out("comb", [128, 256])
                    S.dma("sync", dbg["comb"], comb[:].rearrange("p a b -> p (a b)"), reads=[Tcomb], writes=[Tdbg], semtile=Tdbg)
                    S.barrier()
                S.emit()
            if stop_after == "C1":
                return nc

            with ExitStack() as c2:
                wgb = [sb(c2, "wgb%d" % i, [128, 8, 512], BF16) for i in range(2)]; Twg4 = [[S.tile() for _ in range(4)] for _ in range(2)]
                wub = [sb(c2, "wub%d" % i, [128, 8, 512], BF16) for i in range(2)]; Twu4 = [[S.tile() for _ in range(4)] for _ in range(2)]
                wdb = [sb(c2, "wdb%d" % i, [128, 4, 1024], BF16) for i in range(2)]; Twd4 = [[S.tile() for _ in range(4)] for _ in range(2)]
                stg = [sb(c2, "stg%d" % i, [128, 1024], F32) for i in range(3)]; Tstg = [S.tile() for _ in range(3)]
                sq_ = [0]
                aT = [sb(c2, "aT%d" % i, [128, 4, 512], BF16) for i in range(2)]; TaT = [[S.tile() for _ in range(4)] for _ in range(2)]
                sg = [sb(c2, "sg%d" % i, [128, 512], F32) for i in range(2)]; Tsg = [S.tile() for _ in range(2)]
                xg = [sb(c2, "xg%d" % i, [128, 1024], BF16) for i in range(4)]; Txg = [S.tile() for _ in range(4)]
                xgT = [sb(c2, "xgT%d" % i, [128, 8, CAP], BF16) for i in range(2)]; TxgT = [[S.tile() for _ in range(CAP // 128)] for _ in range(2)]
                ysb = [sb(c2, "ysb%d" % i, [128, 1024], F32) for i in range(2)]; Tysb = [S.tile() for _ in range(2)]
                NJ = CAP // 128
                Tys = [S.tile() for _ in range(NEXP * NJ)]

                def w_steps(ex):
                    b2 = ex % 2
                    dmas, casts = [], []
                    for k in range(12):
                        def mk(k=k):
                            if k < 8:
                                srcw = (wg_d if k < 4 else wu_d)[ex]
                                kk = k % 4
                                src = srcw[kk * 256:(kk + 1) * 256, :].rearrange("(c p) n -> p c n", p=128)
                                dst_of = lambda: (wgb if k < 4 else wub)[b2][:, 2 * kk:2 * kk + 2, :]
                                Td = (Twg4 if k < 4 else Twu4)[b2][kk]
                                view = lambda t_: t_[:].rearrange("p (c n) -> p c n", c=2)
                            else:
                                kk = k - 8
                                src = wd_d[ex][kk * 128:(kk + 1) * 128, :]
                                dst_of = lambda: wdb[b2][:, kk, :]
                                Td = Twd4[b2][kk]
                                view = lambda t_: t_[:]
                            cell = {}

                            def d():
                                si = sq_[0] % 3
                                sq_[0] += 1
                                cell["si"] = si
                                S.dma("sync", view(stg[si]), src, writes=[Tstg[si]])

                            def c():
                                si = cell["si"]
                                sv = view(stg[si])
                                dstb = dst_of()
                                if k % 2 == 1:
                                    S.op("scalar", lambda e: e.activation(out=dstb, in_=sv, func=AF.Copy), reads=[Tstg[si]], writes=[Td])
                                else:
                                    S.op("vector", lambda e: e.tensor_copy(out=dstb, in_=sv), reads=[Tstg[si]], writes=[Td])
                            return d, c
                        d, c = mk()
                        dmas.append(d)
                        casts.append(c)
                    steps = dmas[0:3]
                    for k in range(12):
                        steps.append(casts[k])
                        if k + 3 < 12:
                            steps.append(dmas[k + 3])
                    return steps

                def load_w(ex):
                    for f in w_steps(ex):
                        f()

                def gate_up(b2, rhs_of, Trhs, width, gq, slot=None):
                    for ft in range(4):
                        bg, bu = (0, 1) if gq[0] % 2 == 0 else (2, 3)
                        s2 = gq[0] % 2
                        gq[0] += 1
                        mmgroup(banks[bg][:, 0:width], [(wgb[b2][:, c, ft * 128:(ft + 1) * 128], rhs_of(c)) for c in range(8)],
                                reads=Trhs + Twg4[b2], writes=[Tb[bg]])
                        mmgroup(banks[bu][:, 0:width], [(wub[b2][:, c, ft * 128:(ft + 1) * 128], rhs_of(c)) for c in range(8)],
                                reads=Trhs + Twu4[b2], writes=[Tb[bu]])
                        S.op("scalar", lambda e, bg=bg, s2=s2: e.activation(out=sg[s2][:, 0:width], in_=banks[bg][:, 0:width], func=AF.Silu),
                             reads=[Tb[bg]], writes=[Tsg[s2]])
                        S.op("vector", lambda e, bu=bu, s2=s2, ft=ft: e.tensor_tensor(out=aT[b2][:, ft, 0:width], in0=sg[s2][:, 0:width], in1=banks[bu][:, 0:width], op=ALU.mult),
                             reads=[Tsg[s2], Tb[bu]], writes=[TaT[b2][ft]])
                        if slot is not None:
                            slot()

                S.branch_begin()
                gq = [0]; yq = [0]; xq = [0]

                def prep(ex):
                    b2 = ex % 2
                    for j in range(NJ):
                        xi = xq[0] % 4
                        xq[0] += 1
                        r0 = ex * CAP + j * 128
                        S.dma("gpsimd", xg[xi][:], xs_d[r0:r0 + 128, :], reads=Tsc + [Txs], writes=[Txg[xi]])
                        bi = 6 + (xq[0] % 2)
                        pTv = banks[bi][:].bitcast(BF16)
                        for c in range(8):
                            S.op("tensor", lambda e, c=c, xi=xi, pTv=pTv: e.transpose(
                                out=pTv[:, c * 128:(c + 1) * 128], in_=xg[xi][:, c * 128:(c + 1) * 128], identity=identb[:]),
                                reads=[Txg[xi], Tidb], writes=[Tb[bi]], signal=(c == 7))
                        S.op("vector", lambda e, j=j, b2=b2, pTv=pTv: e.tensor_copy(
                            out=xgT[b2][:, :, j * 128:(j + 1) * 128], in_=pTv.rearrange("p (c t) -> p c t", c=8)),
                            reads=[Tb[bi]], writes=[TxgT[b2][j]])
                prep(0)
                load_w(0)
                for ex in range(NEXP):
                    b2 = ex % 2
                    wq = w_steps(ex + 1) if ex + 1 < NEXP else []

                    def pop(n):
                        for _ in range(n):
                            if wq:
                                wq.pop(0)()
                    pop(3)
                    gate_up(b2, lambda c, b2=b2: xgT[b2][:, c, :], TxgT[b2], CAP, gq, slot=lambda: pop(3))
                    if ex + 1 < NEXP:
                        prep(ex + 1)
                    for j in range(NJ):
                        y2 = yq[0] % 2
                        yq[0] += 1
                        for hf in range(2):
                            by = 4 + hf
                            mmgroup(banks[by][:], [(aT[b2][:, ft, j * 128:(j + 1) * 128], wdb[b2][:, ft, hf * 512:(hf + 1) * 512]) for ft in range(4)],
                                    reads=TaT[b2] + Twd4[b2], writes=[Tb[by]])
                            if hf == 0:
                                S.op("vector", lambda e, y2=y2, by=by: e.tensor_copy(out=ysb[y2][:, 0:512], in_=banks[by][:]),
                                     reads=[Tb[by]], writes=[Tysb[y2]])
                            else:
                                S.op("scalar", lambda e, y2=y2, by=by: e.activation(out=ysb[y2][:, 512:1024], in_=banks[by][:], func=AF.Copy),
                                     reads=[Tb[by], Tysb[y2]], writes=[Tysb[y2]])
                        r0 = ex * CAP + j * 128
                        S.dma("gpsimd", ys_d[r0:r0 + 128, :], ysb[y2][:], reads=[Tysb[y2]], writes=[Tys[ex * NJ + j]], semtile=Tysb[y2])
                        pop(3)
                    pop(99)
                S.barrier()
                ygl = []
                for wb in wgb + wub + wdb:
                    v = wb[:].rearrange("p a b -> p (a b)").bitcast(F32)
                    ygl += [v[:, 0:1024], v[:, 1024:2048]]
                Tyg = [S.tile() for _ in ygl]
                set_bcreg()
                def gath(i):
                    t, k_ = i // 2, i % 2
                    gi = i % len(ygl)
                    S.dma_fn("gpsimd", lambda e, t=t, k_=k_, gi=gi: e.indirect_dma_start(
                        out=ygl[gi], out_offset=None, in_=ys_d[:, :],
                        in_offset=bass.IndirectOffsetOnAxis(ap=pos[:, k_, t:t + 1], axis=0),
                        bounds_check=bcreg, oob_is_err=False),
                        reads=Tys + [Tpos], writes=[Tyg[gi]], semtile=Tyg[gi])

                def acc(i):
                    t, k_ = i // 2, i % 2
                    gi = i % len(ygl)
                    for hf in range(2):
                        S.op("vector", lambda e, t=t, k_=k_, gi=gi, hf=hf: e.scalar_tensor_tensor(
                            out=x2[:, t, hf * 512:(hf + 1) * 512], in0=ygl[gi][:, hf * 512:(hf + 1) * 512], scalar=w12[:, k_, t:t + 1],
                            in1=x2[:, t, hf * 512:(hf + 1) * 512], op0=ALU.mult, op1=ALU.add),
                            reads=[Tyg[gi], Tw12, Tx2[t][hf]], writes=[Tx2[t][hf]])
                depth = len(ygl) - 1
                for i in range(32 + depth):
                    if i < 32:
                        gath(i)
                    if i - depth >= 0:
                        acc(i - depth)
                S.branch_mid()
                gq = [0]; yq = [0]
                load_w(0)
                for ex in range(NEXP):
                    b2 = ex % 2
                    if ex + 1 < NEXP:
                        load_w(ex + 1)
                    for tb in range(4):
                        gate_up(b2, lambda c, tb=tb: hmT[:, c, tb * 512:(tb + 1) * 512], ThmT[tb * 4:tb * 4 + 4], 512, gq)
                        for tt in range(4):
                            t = tb * 4 + tt
                            for hf in range(2):
                                by = 4 + (yq[0] % 4)
                                yq[0] += 1
                                mmgroup(banks[by][:], [(aT[b2][:, ft, tt * 128:(tt + 1) * 128], wdb[b2][:, ft, hf * 512:(hf + 1) * 512]) for ft in range(4)],
                                        reads=TaT[b2] + Twd4[b2], writes=[Tb[by]])
                                S.op("vector", lambda e, t=t, hf=hf, by=by, ex=ex: e.scalar_tensor_tensor(
                                    out=x2[:, t, hf * 512:(hf + 1) * 512], in0=banks[by][:], scalar=comb[:, t, ex:ex + 1],
                                    in1=x2[:, t, hf * 512:(hf + 1) * 512], op0=ALU.mult, op1=ALU.add),
                                    reads=[Tb[by], Tcomb, Tx2[t][hf]], writes=[Tx2[t][hf]])
                S.branch_end(ovf[0:1, 0:1], brregs)
                S.emit()

            with ExitStack() as c3:
                gbn = sb(c3, "gbn", [128, 1024], F32); Tgbn = S.tile()
                bcast_load(gbn[:], g_fin, Tgbn, 1024)
                ob = [sb(c3, "ob%d" % i, [128, 1024], F32) for i in range(2)]; Tob = [S.tile() for _ in range(2)]
                Tout = S.tile()
                for t in range(16):
                    S.op("scalar", lambda e, t=t: e.activation(out=junk[:], in_=x2[:, t, :], func=AF.Square, accum_out=ssc[:, t:t + 1]),
                         reads=Tx2[t] + [Tsscall], writes=[Tssc[t], Tjunkc])
                S.op("scalar", lambda e: e.activation(out=ssc[:], in_=ssc[:], func=AF.Sqrt, scale=1.0 / 1024, bias=EPS),
                     reads=Tssc, writes=[Tsscall])
                S.op("vector", lambda e: e.reciprocal(out=ssc[:], in_=ssc[:]), reads=[Tsscall], writes=[Tsscall])
                for t in range(16):
                    S.op("vector", lambda e, t=t: e.scalar_tensor_tensor(
                        out=ob[t % 2][:], in0=x2[:, t, :], scalar=ssc[:, t:t + 1], in1=gbn[:], op0=ALU.mult, op1=ALU.mult),
                        reads=Tx2[t] + [Tsscall, Tgbn], writes=[Tob[t % 2]])
                    S.dma("sync", out[t * 128:(t + 1) * 128, :], ob[t % 2][:], reads=[Tob[t % 2]], writes=[Tout], semtile=Tob[t % 2])
                S.barrier()
                S.emit()
    return nc


def _const_tables():
    f32 = np.float32
    inv_freq = (f32(1.0) / (f32(10000.0) ** (np.arange(0, 64, 2, dtype=f32) / f32(64)))).astype(f32)
    pos = np.arange(4096, dtype=f32)
    ang = (pos[:, None] * inv_freq[None, :]).astype(f32)
    cos = np.cos(ang).astype(f32)
    sin = np.sin(ang).astype(f32)
    r = np.arange(128)
    dh = r % 64
    cosT = cos[:, dh % 32].T.copy()
    sgn = np.where(dh < 32, -1.0, 1.0).astype(f32)
    sinT = (sin[:, dh % 32].T * sgn[:, None]).astype(f32)
    return cosT, sinT


def _masks(hf):
    k = np.arange(128)[:, None, None]
    r = np.arange(4)[None, :, None]
    q = np.arange(256)[None, None, :]
    md = np.zeros((128, 2, 4, 256), np.float32)
    mf = np.zeros((128, 2, 4, 256), np.float32)
    for par in range(2):
        if par == 0:
            kb = r
            j = 0 if hf == 0 else 1
        else:
            kb = 4 + r
            j = 3 if hf == 0 else 2
        s = kb * 128 + k
        t = j * 256 + q
        mf[:, par] = np.where(s <= t, 3e38, 0.0)
        md[:, par] = np.where((s // 64) <= (t // 64), 3e38, 0.0)
    return (md.reshape(128, 2, 1024).astype(ml_dtypes.bfloat16),
            mf.reshape(128, 2, 1024).astype(ml_dtypes.bfloat16))


def own_tokens(hf):
    return np.concatenate([np.arange(j * 256, (j + 1) * 256) for j in own_qtiles(hf)])


def prep(inputs):
    f32 = np.float32
    x = np.asarray(inputs["x"], f32)
    w_in = np.ascontiguousarray(np.asarray(inputs["w_in"], f32)[0])

    def swap_cols(w):
        return np.ascontiguousarray(w.reshape(1024, 8, 2, 32)[:, :, ::-1, :].reshape(1024, 512))

    cosT, sinT = _const_tables()
    common = {
        "w_in": w_in,
        "wqs": swap_cols(w_in[:, 0:512]),
        "wks": swap_cols(w_in[:, 512:1024]),
        "cosk": cosT, "sink": sinT,
        "identb": np.eye(128, dtype=f32).astype(ml_dtypes.bfloat16),
        "identf": np.eye(128, dtype=f32),
        "tri": np.triu(np.ones((128, 128), f32)),
        "onesf": np.ones((128, 128), f32),
        "onesb": np.ones((128, 128), f32).astype(ml_dtypes.bfloat16),
        "ustrict": np.triu(np.ones((128, 128), f32), 1).astype(ml_dtypes.bfloat16),
        "ebase": np.ascontiguousarray(np.broadcast_to((np.arange(16, dtype=f32) * CAP)[None, None, :], (128, 16, 16)).reshape(128, 256)),
        "g_attn": np.asarray(inputs["norm_attn_g"], f32).reshape(1, 1024),
        "g_ffn": np.asarray(inputs["norm_ffn_g"], f32).reshape(1, 1024),
        "g_fin": np.asarray(inputs["norm_final_g"], f32).reshape(1, 1024),
        "bfor": np.asarray(inputs["b_forget"], f32).reshape(1, 8),
        "lamv": np.concatenate([np.asarray(inputs[k], f32).reshape(1, 64) for k in
                                ("lambda_q1", "lambda_k1", "lambda_q2", "lambda_k2")], axis=1),
        "dng": np.asarray(inputs["diff_norm_g"], f32).reshape(1, 128),
        "w_out": np.ascontiguousarray(np.asarray(inputs["w_out"], f32)[0]),
        "rw": np.ascontiguousarray(np.concatenate([np.asarray(inputs["router_group_w"], f32)[0],
                                                   np.asarray(inputs["router_expert_w"], f32)[0]], axis=1)),
        "rb": np.concatenate([np.asarray(inputs["router_group_b"], f32).reshape(1, 4),
                              np.asarray(inputs["router_expert_b"], f32).reshape(1, 16)], axis=1),
        "wg": np.ascontiguousarray(np.asarray(inputs["w_gate"], f32)[0]),
        "wu": np.ascontiguousarray(np.asarray(inputs["w_up"], f32)[0]),
        "wd": np.ascontiguousarray(np.asarray(inputs["w_down"], f32)[0]),
    }
    in_maps = []
    for c in range(8):
        b, hf = c // 2, c % 2
        tok = own_tokens(hf)
        md, mf = _masks(hf)
        m = dict(common)
        m["xb"] = np.ascontiguousarray(x[b])
        m["xo"] = np.ascontiguousarray(x[b][tok])
        m["cosq"] = np.ascontiguousarray(cosT[:, tok] * f32(0.125))
        m["sinq"] = np.ascontiguousarray(sinT[:, tok] * f32(0.125))
        cs = np.zeros((8, 33), f32)
        for i, j in enumerate(own_qtiles(hf)):
            cs[i, 2 * j + 1] = 1.0
        m["csel"] = cs.reshape(1, 8 * 33)
        m["maskd"] = md
        m["maskf"] = mf
        in_maps.append(m)
    return in_maps


def kernel(**inputs):
    in_maps = prep(inputs)
    nc = build()
    res = run_bass_kernel_spmd(nc, in_maps, core_ids=list(range(8)))
    out = np.zeros((4, 4096, 1024), np.float32)
    for c in range(8):
        b, hf = c // 2, c % 2
        out[b, own_tokens(hf)] = res.results[c]["out"]
    return out
```

```python
import numpy as np
import ml_dtypes
from contextlib import ExitStack
import concourse.bass as bass
import concourse.mybir as mybir
from concourse.bass_utils import run_bass_kernel_spmd

F32 = mybir.dt.float32
BF16 = mybir.dt.bfloat16
I32 = mybir.dt.int32
AF = mybir.ActivationFunctionType
ALU = mybir.AluOpType
AX = mybir.AxisListType

ENGS = ("sync", "scalar", "vector", "gpsimd", "tensor")
EPS = 1e-6
LAM_INIT = 0.8 - 0.6 * 1.0
NEXP = 16
CAP = 512
NSLOT = NEXP * CAP


class Tile:
    __slots__ = ("name", "last_w", "readers", "dsem")

    def __init__(self, name):
        self.name = name
        self.last_w = None
        self.readers = {}
        self.dsem = None


class Sched:
    def __init__(self, nc, es):
        self.nc = nc
        self.es = es
        self.ops = {e: [] for e in ENGS}
        self.sems = {}
        self.cnt = {}
        self.waited = {e: {} for e in ENGS}
        self.pending = {e: False for e in ENGS}
        for e in ENGS:
            self._mksem("E:" + e)
        self.n_dsem = 0
        self.nops = 0
        self.tiles = []

    def _mksem(self, key):
        self.sems[key] = self.es.enter_context(self.nc.semaphore(key.replace(":", "_")))
        self.cnt[key] = 0

    def tile(self, name="t"):
        t = Tile(name)
        self.tiles.append(t)
        return t

    def _snapshot(self):
        return (dict(self.cnt), {e: dict(w) for e, w in self.waited.items()},
                [(t, t.last_w, dict(t.readers)) for t in self.tiles])

    def _restore(self, snap):
        self.cnt = dict(snap[0])
        for k in self.sems:
            self.cnt.setdefault(k, 0)
        self.waited = {e: dict(w) for e, w in snap[1].items()}
        for t, lw, rd in snap[2]:
            t.last_w = lw
            t.readers = dict(rd)

    def branch_begin(self):
        self.barrier()
        self._outer_ops = self.ops
        self.ops = {e: [] for e in ENGS}
        self._snap = self._snapshot()

    def branch_mid(self):
        self.barrier()
        self._A = (self.ops, dict(self.cnt))
        self.ops = {e: [] for e in ENGS}
        self._restore(self._snap)

    def branch_end(self, flag_ap, regs):
        self.barrier()
        opsA, cntA = self._A
        opsB, cntB = self.ops, dict(self.cnt)
        target = {k: max(cntA.get(k, 0), cntB.get(k, 0)) for k in set(cntA) | set(cntB)}

        def pads(cntX):
            out = {e: [] for e in ENGS}
            for k, v in target.items():
                d = v - cntX.get(k, 0)
                if d > 0:
                    owner = k[2:] if k.startswith("E:") else "gpsimd"
                    out[owner].append((k, d))
            return out
        pA, pB = pads(cntA), pads(cntB)
        self.ops = self._outer_ops
        for e in ENGS:
            self.ops[e].append(("branch", flag_ap, regs[e], opsA[e], pA[e], opsB[e], pB[e]))
        self.cnt = target
        for e in ENGS:
            self.waited[e] = dict(target)
        for t in self.tiles:
            t.last_w = None
            t.readers = {}

    def dsem_for(self, t):
        if t.dsem is None:
            key = "D:%d" % self.n_dsem
            self.n_dsem += 1
            self._mksem(key)
            t.dsem = key
        return t.dsem

    def _need(self, eng, waits, key, val):
        if eng == "tensor" and key == "E:tensor":
            return
        if self.cnt[key] < val:
            raise RuntimeError("wait on un-signalled event %s %d (cnt %d) from %s" % (key, val, self.cnt[key], eng))
        if self.waited[eng].get(key, 0) >= val:
            return
        self.waited[eng][key] = val
        waits[key] = max(waits.get(key, 0), val)

    def _deps(self, eng, reads, writes):
        waits = {}
        for t in reads:
            if t.last_w is not None:
                self._need(eng, waits, *t.last_w)
        for t in writes:
            if t.last_w is not None:
                self._need(eng, waits, *t.last_w)
            for k, v in t.readers.items():
                self._need(eng, waits, k, v)
        return list(waits.items())

    def _record(self, ev, reads, writes):
        for t in writes:
            t.last_w = ev
            t.readers = {}
        for t in reads:
            if t not in writes:
                if t.readers.get(ev[0], 0) < ev[1]:
                    t.readers[ev[0]] = ev[1]

    def op(self, eng, fn, reads=(), writes=(), signal=True):
        waits = self._deps(eng, reads, writes)
        key = "E:" + eng
        if signal:
            self.cnt[key] += 1
            ev = (key, self.cnt[key])
            inc = (key, 1)
            self.pending[eng] = False
        else:
            ev = (key, self.cnt[key] + 1)
            inc = None
            self.pending[eng] = True
        self._record(ev, reads, writes)
        self.ops[eng].append((waits, fn, inc))
        self.nops += 1

    def dma(self, eng, out, in_, reads=(), writes=(), semtile=None, **kw):
        waits = self._deps(eng, reads, writes)
        if semtile is None:
            semtile = writes[0] if writes else reads[0]
        key = self.dsem_for(semtile)
        self.cnt[key] += 16
        ev = (key, self.cnt[key])
        self._record(ev, reads, writes)

        def fn(e, out=out, in_=in_, kw=kw):
            return e.dma_start(out=out, in_=in_, **kw)
        self.ops[eng].append((waits, fn, (key, 16)))
        self.nops += 1

    def dma_fn(self, eng, fn, reads=(), writes=(), semtile=None):
        waits = self._deps(eng, reads, writes)
        key = self.dsem_for(semtile)
        self.cnt[key] += 16
        ev = (key, self.cnt[key])
        self._record(ev, reads, writes)
        self.ops[eng].append((waits, fn, (key, 16)))
        self.nops += 1

    def barrier(self, engs=ENGS):
        for e in ENGS:
            assert not self.pending[e], e
        for e in engs:
            waits = {}
            for key, c in self.cnt.items():
                if c > 0:
                    self._need(e, waits, key, c)
            self.ops[e].append((list(waits.items()), None, None))

    def emit(self):
        nc = self.nc
        sems = self.sems
        ops = self.ops
        with nc.Block() as block:
            def replay(e, lst):
                for ent in lst:
                    if ent[0] == "branch":
                        _, flag_ap, reg, oA, pA, oB, pB = ent
                        e.reg_load(reg, flag_ap)
                        with e.If_eq(reg, 0):
                            replay(e, oA)
                            for k, d in pA:
                                e.sem_inc(sems[k], d)
                            e.nop()
                        with e.Else():
                            replay(e, oB)
                            for k, d in pB:
                                e.sem_inc(sems[k], d)
                            e.nop()
                        continue
                    waits, fn, inc = ent
                    for key, val in waits:
                        e.wait_ge(sems[key], val)
                    if fn is None:
                        continue
                    inst = fn(e)
                    if inc is not None:
                        inst.then_inc(sems[inc[0]], inc[1])

            def mk(name):
                def body(e):
                    replay(e, ops[name])
                return body
            block.sync(mk("sync"))
            block.scalar(mk("scalar"))
            block.vector(mk("vector"))
            block.gpsimd(mk("gpsimd"))
            block.tensor(mk("tensor"))
        self.ops = {e: [] for e in ENGS}


def own_qtiles(hf):
    js = []
    for m in range(4):
        js += ([4 * m, 4 * m + 3] if hf == 0 else [4 * m + 1, 4 * m + 2])
    return js


def nk_of(i):
    return 8 * (i // 2) + (4 if i % 2 == 0 else 8)


def interleave(A, B):
    a, b = len(A), len(B)
    if a == 0:
        for f in B:
            f()
        return
    done = 0
    for k, f in enumerate(A):
        f()
        upto = ((k + 1) * b) // a
        while done < upto:
            B[done]()
            done += 1
    while done < b:
        B[done]()
        done += 1


def build(debug=(), stop_after=None, moe="sparse"):
    nc = bass.Bass("TRN2", target_bir_lowering=False)

    def din(name, shape, dt=F32):
        return nc.dram_tensor(name, list(shape), dt, kind="ExternalInput").ap()

    xb = din("xb", [4096, 1024])
    xo = din("xo", [2048, 1024])
    w_in = din("w_in", [1024, 3080])
    wqs_d = din("wqs", [1024, 512])
    wks_d = din("wks", [1024, 512])
    cosk = din("cosk", [128, 4096])
    sink = din("sink", [128, 4096])
    cosq = din("cosq", [128, 2048])
    sinq = din("sinq", [128, 2048])
    maskd_d = din("maskd", [128, 2, 1024], BF16)
    maskf_d = din("maskf", [128, 2, 1024], BF16)
    identb_d = din("identb", [128, 128], BF16)
    identf_d = din("identf", [128, 128])
    tri_d = din("tri", [128, 128])
    onesf_d = din("onesf", [128, 128])
    g_attn = din("g_attn", [1, 1024])
    g_ffn = din("g_ffn", [1, 1024])
    g_fin = din("g_fin", [1, 1024])
    bfor = din("bfor", [1, 8])
    lam_d = din("lamv", [1, 256])
    csel_d = din("csel", [1, 8 * 33])
    dng = din("dng", [1, 128])
    w_out = din("w_out", [1024, 1024])
    rw_d = din("rw", [1024, 20])
    rb_d = din("rb", [1, 20])
    wg_d = din("wg", [16, 1024, 512])
    wu_d = din("wu", [16, 1024, 512])
    wd_d = din("wd", [16, 512, 1024])
    ebase_d = din("ebase", [128, 256])
    ustrict_d = din("ustrict", [128, 128], BF16)
    onesb_d = din("onesb", [128, 128], BF16)
    xs_d = nc.dram_tensor("xs_scratch", [NSLOT, 1024], BF16).ap()
    ys_d = nc.dram_tensor("ys_scratch", [NSLOT, 1024], F32).ap()
    out = nc.dram_tensor("out", [2048, 1024], F32, kind="ExternalOutput").ap()
    dbg = {}

    def dbg_out(name, shape, dt=F32):
        if name in debug:
            dbg[name] = nc.dram_tensor("dbg_" + name, list(shape), dt, kind="ExternalOutput").ap()
            return dbg[name]
        return None

    with ExitStack() as es:
        S = Sched(nc, es)

        uniq = [0]

        def sb(sc, name, shape, dt):
            uniq[0] += 1
            return sc.enter_context(nc.sbuf_tensor("s%d_%s" % (uniq[0], name), list(shape), dt))

        banks = [es.enter_context(nc.psum_tensor("bank%d" % i, [128, 512], F32)) for i in range(8)]
        Tb = [S.tile("bank%d" % i) for i in range(8)]
        Tdbg = S.tile("dbg")

        def dump(name, src_ap, reads):
            if name in dbg:
                S.dma("sync", dbg[name], src_ap, reads=reads, writes=[Tdbg], semtile=Tdbg)

        def bcast_load(dst, src, T, n):
            S.dma("sync", dst, src.broadcast_to([128, n]), writes=[T])

        bcreg = es.enter_context(nc.gpsimd.register("bcreg"))
        brregs = {e: es.enter_context(getattr(nc, e).register("br_" + e)) for e in ENGS}

        def set_bcreg():
            S.ops["gpsimd"].append(([], lambda e: e.reg_mov(bcreg, NSLOT - 1), None))

        identb = sb(es, "identb", [128, 128], BF16); Tidb = S.tile("identb")
        S.dma("sync", identb[:], identb_d, writes=[Tidb])
        gb_attn = sb(es, "gb_attn", [128, 1024], F32); Tgba = S.tile("gba")
        bcast_load(gb_attn[:], g_attn, Tgba, 1024)
        OT = sb(es, "OT", [128, 8, 2048], BF16)
        TOT = [S.tile("OT%d" % q) for q in range(16)]

        def make_sweep(sc, pfx, pT_banks):
            st = {}
            st["xt"] = [sb(sc, pfx + "xt%d" % i, [128, 1024], F32) for i in range(4)]
            st["Txt"] = [S.tile() for _ in range(4)]
            st["hb"] = [sb(sc, pfx + "hb%d" % i, [128, 1024], BF16) for i in range(2)]
            st["Thb"] = [S.tile() for _ in range(2)]
            st["hT"] = [sb(sc, pfx + "hT%d" % i, [128, 8, 512], BF16) for i in range(3)]
            st["ThT"] = [[S.tile() for _ in range(4)] for _ in range(3)]
            st["junk"] = sb(sc, pfx + "junk", [128, 1024], BF16)
            st["Tjunk"] = S.tile()
            st["ss"] = [sb(sc, pfx + "ss%d" % i, [128, 4], F32) for i in range(3)]
            st["Tss"] = [[S.tile() for _ in range(4)] for _ in range(3)]
            st["sq"] = [sb(sc, pfx + "sq%d" % i, [128, 4], F32) for i in range(3)]
            st["Tsq"] = [S.tile() for _ in range(3)]
            st["rs"] = [sb(sc, pfx + "rs%d" % i, [128, 4], F32) for i in range(3)]
            st["Trs"] = [S.tile() for _ in range(3)]
            st["pT"] = pT_banks
            return st

        def sweep_stage1(st, x_ap, blk, gb, Tgb):
            b2 = blk % 3
            for tt in range(4):
                n = blk * 4 + tt
                xt, Txt = st["xt"][n % 4], st["Txt"][n % 4]
                S.dma("sync", xt[:], x_ap[n * 128:(n + 1) * 128, :], writes=[Txt])
                S.op("scalar", lambda e, xt=xt, tt=tt: e.activation(out=st["junk"][:], in_=xt[:], func=AF.Square,
                                                                    accum_out=st["ss"][b2][:, tt:tt + 1]),
                     reads=[Txt], writes=[st["Tss"][b2][tt], st["Tjunk"]])
            S.op("scalar", lambda e: e.activation(out=st["sq"][b2][:], in_=st["ss"][b2][:], func=AF.Sqrt,
                                                  scale=1.0 / 1024, bias=EPS),
                 reads=st["Tss"][b2], writes=[st["Tsq"][b2]])
            S.op("vector", lambda e: e.reciprocal(out=st["rs"][b2][:], in_=st["sq"][b2][:]),
                 reads=[st["Tsq"][b2]], writes=[st["Trs"][b2]])
            def stt(tt):
                n = blk * 4 + tt
                xt, Txt = st["xt"][n % 4], st["Txt"][n % 4]
                hb, Thb = st["hb"][n % 2], st["Thb"][n % 2]
                S.op("vector", lambda e, xt=xt, hb=hb, tt=tt: e.scalar_tensor_tensor(
                    out=hb[:], in0=xt[:], scalar=st["rs"][b2][:, tt:tt + 1], in1=gb[:], op0=ALU.mult, op1=ALU.mult),
                    reads=[Txt, st["Trs"][b2], Tgb], writes=[Thb])

            def tr(tt):
                n = blk * 4 + tt
                hb, Thb = st["hb"][n % 2], st["Thb"][n % 2]
                bi = st["pT"][n % 2]
                pTv = banks[bi][:].bitcast(BF16)
                for c in range(8):
                    S.op("tensor", lambda e, c=c, hb=hb, pTv=pTv: e.transpose(
                        out=pTv[:, c * 128:(c + 1) * 128], in_=hb[:, c * 128:(c + 1) * 128], identity=identb[:]),
                        reads=[Thb, Tidb], writes=[Tb[bi]], signal=(c == 7))

            def ev(tt):
                n = blk * 4 + tt
                bi = st["pT"][n % 2]
                pTv = banks[bi][:].bitcast(BF16)
                S.op("vector", lambda e, tt=tt, pTv=pTv: e.tensor_copy(
                    out=st["hT"][b2][:, :, tt * 128:(tt + 1) * 128], in_=pTv.rearrange("p (c t) -> p c t", c=8)),
                    reads=[Tb[bi]], writes=[st["ThT"][b2][tt]])
            for f, a in ((stt, 0), (stt, 1), (tr, 0), (ev, 0), (stt, 2), (tr, 1), (ev, 1), (stt, 3), (tr, 2), (ev, 2), (tr, 3), (ev, 3)):
                f(a)

        def wload(dst, src_cols, T):
            S.dma("gpsimd", dst, src_cols.rearrange("(c p) n -> p c n", p=128), writes=[T])

        def mmgroup(out_ap, pairs, reads, writes):
            n = len(pairs)
            for k, (l, r) in enumerate(pairs):
                S.op("tensor", lambda e, l=l, r=r, k=k: e.matmul(out=out_ap, lhsT=l, rhs=r, start=(k == 0), stop=(k == n - 1)),
                     reads=reads, writes=writes, signal=(k == n - 1))

        def run_attention(units, PT, TPT, KT_of, QT_of, V_of, W, exp_emit, evac_emit, mask_emit, s_banks, o_banks,
                          OTs=None, TOTs=None, smr=None, Tsmr=None, identf=None, Tidf=None, Vsum_of=None):
            gctr = [0]
            dv = W - 1
            MO = dv if Vsum_of is not None else W
            deferred = []

            def st_list(ui, u):
                buf = ui % 2
                nk = nk_of(u["i"])
                L = []
                for p in range(nk // 2):
                    def f(p=p):
                        bi = s_banks[gctr[0] % len(s_banks)]
                        gctr[0] += 1
                        for j in range(2):
                            kb = 2 * p + j
                            kap, Tk = KT_of(u, kb)
                            qap, Tq = QT_of(u)
                            S.op("tensor", lambda e, kap=kap, qap=qap, j=j, bi=bi: e.matmul(
                                out=banks[bi][:, j * 256:(j + 1) * 256], lhsT=kap, rhs=qap, start=True, stop=True),
                                reads=[Tk, Tq], writes=[Tb[bi]], signal=(j == 1))
                        exp_emit(u, p, bi, PT[buf], TPT[buf][p])
                    L.append(f)
                L.append(lambda: mask_emit(u, PT[buf], TPT[buf]))
                return L

            def pv_list(ui, u):
                buf = ui % 2
                nk = nk_of(u["i"])
                ba = o_banks[(ui % 2) * 2]
                bb = o_banks[(ui % 2) * 2 + 1]
                o2 = ui % 2
                L = []
                for k0 in range(0, nk, 4):
                    def f(k0=k0):
                        for kb in range(k0, min(nk, k0 + 4)):
                            vap, Tv = V_of(u, kb)
                            S.op("tensor", lambda e, kb=kb, vap=vap: e.matmul(
                                out=banks[ba][0:MO, 0:256], lhsT=vap, rhs=PT[buf][:, kb, :], start=(kb == 0), stop=(kb == nk - 1)),
                                reads=[TPT[buf][kb // 2], Tv], writes=[Tb[ba]], signal=(kb == nk - 1))
                    L.append(f)
                if Vsum_of is not None:
                    for k0 in range(0, nk, 4):
                        def g(k0=k0):
                            for kb in range(k0, min(nk, k0 + 4)):
                                vap, Tv = Vsum_of(u, kb)
                                S.op("tensor", lambda e, kb=kb, vap=vap: e.matmul(
                                    out=banks[bb][0:1, 256:512], lhsT=vap, rhs=PT[buf][:, kb, :], start=(kb == 0), stop=(kb == nk - 1)),
                                    reads=[TPT[buf][kb // 2], Tv], writes=[Tb[bb]], signal=(kb == nk - 1))
                        L.append(g)

                def copy_out():
                    S.op("vector", lambda e: e.tensor_copy(out=OTs[o2][0:MO, :], in_=banks[ba][0:MO, 0:256]), reads=[Tb[ba]], writes=[TOTs[o2]])
                    if Vsum_of is not None:
                        S.op("vector", lambda e: e.tensor_copy(out=smr[o2][:], in_=banks[bb][0:1, 256:512]), reads=[Tb[bb]], writes=[Tsmr[o2]])

                def transpose_back():
                    for s in range(2):
                        last = (Vsum_of is None)
                        S.op("tensor", lambda e, s=s: e.transpose(out=banks[bb][:, s * W:s * W + MO], in_=OTs[o2][0:MO, s * 128:(s + 1) * 128],
                                                                  identity=identf[0:MO, 0:MO]),
                             reads=[TOTs[o2], Tidf], writes=[Tb[bb]], signal=(last and s == 1))
                        if Vsum_of is not None:
                            S.op("tensor", lambda e, s=s: e.transpose(out=banks[bb][:, s * W + dv:s * W + W], in_=smr[o2][0:1, s * 128:(s + 1) * 128],
                                                                      identity=identf[0:1, 0:1]),
                                 reads=[Tsmr[o2], Tidf], writes=[Tb[bb]], signal=(s == 1))
                    evac_emit(u, bb)
                L.append(copy_out)
                deferred.append(transpose_back)
                return L

            for ui in range(len(units) + 1):
                A = st_list(ui, units[ui]) if ui < len(units) else []
                B = pv_list(ui - 1, units[ui - 1]) if ui >= 1 else []
                if len(deferred) > (1 if ui >= 1 else 0):
                    B.insert(min(2, len(B)), deferred.pop(0))
                interleave(A, B)
            while deferred:
                deferred.pop(0)()

        with ExitStack() as pa:
            KT = sb(pa, "KTd", [128, 4, 4096], BF16)
            TKT = [[S.tile() for _ in range(8)] for _ in range(4)]
            QT = sb(pa, "QTd", [128, 4, 2048], BF16)
            TQT = [[S.tile() for _ in range(4)] for _ in range(4)]
            Vd = sb(pa, "Vd", [128, 32, 4, 130], BF16)
            TV = [S.tile() for _ in range(32)]
            Tvones = S.tile()
            S.op("vector", lambda e: e.memset(Vd[:, :, :, 128:130], 1.0), writes=TV)
            lamt = sb(pa, "lamt", [128, 256], F32); Tlam = S.tile()
            bcast_load(lamt[:], lam_d, Tlam, 256)
            lj = sb(pa, "lj", [128, 64], F32)
            ls = sb(pa, "ls", [128, 2], F32); Tls = S.tile()
            le = sb(pa, "le", [128, 2], F32); Tle = S.tile()
            neglam = sb(pa, "neglam", [128, 1], F32); Tnl = S.tile()
            for z in range(2):
                S.op("vector", lambda e, z=z: e.scalar_tensor_tensor(
                    out=lj[:], in0=lamt[:, z * 128:z * 128 + 64], scalar=1.0, in1=lamt[:, z * 128 + 64:z * 128 + 128],
                    op0=ALU.mult, op1=ALU.mult, accum_out=ls[:, z:z + 1]), reads=[Tlam, Tls], writes=[Tls])
            S.op("scalar", lambda e: e.activation(out=le[:], in_=ls[:], func=AF.Exp), reads=[Tls], writes=[Tle])
            S.op("vector", lambda e: e.tensor_tensor(out=neglam[:], in0=le[:, 1:2], in1=le[:, 0:1], op=ALU.subtract),
                 reads=[Tle], writes=[Tnl])
            S.op("vector", lambda e: e.tensor_scalar(out=neglam[:], in0=neglam[:], scalar1=-LAM_INIT, scalar2=None, op0=ALU.add),
                 reads=[Tnl], writes=[Tnl])
            gsc = sb(pa, "gsc", [128, 128], F32); Tgsc = S.tile()
            bcast_load(gsc[:], dng, Tgsc, 128)
            S.op("vector", lambda e: e.tensor_scalar(out=gsc[:], in0=gsc[:], scalar1=1.0 - LAM_INIT, scalar2=None, op0=ALU.mult),
                 reads=[Tgsc], writes=[Tgsc])
            Txs = S.tile("xs")

            with ExitStack() as sw:
                st = make_sweep(sw, "a", [0, 1])
                wA = sb(sw, "wA", [128, 8, 512], BF16); TwA = S.tile()
                wB = sb(sw, "wB", [128, 8, 512], BF16); TwB = S.tile()
                wC = sb(sw, "wC", [128, 8, 512], BF16); TwC = S.tile()
                wload(wA[:], w_in[:, 512:1024], TwA)
                wload(wB[:], wks_d, TwB)
                wload(wC[:], w_in[:, 1024:1536], TwC)
                ct = [sb(sw, "ct%d" % i, [128, 512], F32) for i in range(2)]; Tct = [S.tile() for _ in range(2)]
                sn = [sb(sw, "sn%d" % i, [128, 512], F32) for i in range(2)]; Tsn = [S.tile() for _ in range(2)]
                t1 = [sb(sw, "t1%d" % i, [128, 512], F32) for i in range(2)]; Tt1 = [S.tile() for _ in range(2)]
                t2 = [sb(sw, "t2%d" % i, [128, 512], F32) for i in range(2)]; Tt2 = [S.tile() for _ in range(2)]
                rctr = [0]

                def rope_proj(blk, hT, ThT, wq_, Tw_, ws_, Tws_, cos_d, sin_d, dstT, TdstT):
                    b2 = blk % 2
                    S.dma("sync", ct[b2][:], cos_d[:, blk * 512:(blk + 1) * 512], writes=[Tct[b2]])
                    S.dma("sync", sn[b2][:], sin_d[:, blk * 512:(blk + 1) * 512], writes=[Tsn[b2]])
                    for h in range(4):
                        ba, bb = (2, 3) if h % 2 == 0 else (4, 5)
                        mmgroup(banks[ba][:], [(wq_[:, c, h * 128:(h + 1) * 128], hT[:, c, :]) for c in range(8)],
                                reads=list(ThT) + [Tw_], writes=[Tb[ba]])
                        mmgroup(banks[bb][:], [(ws_[:, c, h * 128:(h + 1) * 128], hT[:, c, :]) for c in range(8)],
                                reads=list(ThT) + [Tws_], writes=[Tb[bb]])
                        r = rctr[0] % 2
                        rctr[0] += 1
                        S.op("vector", lambda e, r=r, ba=ba: e.tensor_tensor(out=t1[r][:], in0=banks[ba][:], in1=ct[b2][:], op=ALU.mult),
                             reads=[Tb[ba], Tct[b2]], writes=[Tt1[r]])
                        S.op("vector", lambda e, r=r, bb=bb: e.tensor_tensor(out=t2[r][:], in0=banks[bb][:], in1=sn[b2][:], op=ALU.mult),
                             reads=[Tb[bb], Tsn[b2]], writes=[Tt2[r]])
                        S.op("gpsimd", lambda e, r=r, h=h: e.tensor_tensor(out=dstT[:, h, blk * 512:(blk + 1) * 512], in0=t1[r][:], in1=t2[r][:], op=ALU.add),
                             reads=[Tt1[r], Tt2[r]], writes=[TdstT[h][blk]])

                def kv_proj(blk, hT, ThT):
                    rope_proj(blk, hT, ThT, wA, TwA, wB, TwB, cosk, sink, KT, TKT)
                    for tt in range(4):
                        n = blk * 4 + tt
                        bv = 6 + (tt % 2)
                        mmgroup(banks[bv][:], [(hT[:, c, tt * 128:(tt + 1) * 128], wC[:, c, :]) for c in range(8)],
                                reads=[ThT[tt], TwC], writes=[Tb[bv]])
                        S.op("scalar", lambda e, n=n, bv=bv: e.activation(
                            out=Vd[:, n, :, 0:128], in_=banks[bv][:].rearrange("p (h d) -> p h d", h=4), func=AF.Copy),
                            reads=[Tb[bv]], writes=[TV[n]])

                sweep_stage1(st, xb, 0, gb_attn, Tgba)
                sweep_stage1(st, xb, 1, gb_attn, Tgba)
                for blk in range(8):
                    if blk + 2 < 8:
                        sweep_stage1(st, xb, blk + 2, gb_attn, Tgba)
                    kv_proj(blk, st["hT"][blk % 3], st["ThT"][blk % 3])
                wload(wA[:], w_in[:, 0:512], TwA)
                wload(wB[:], wqs_d, TwB)
                sweep_stage1(st, xo, 0, gb_attn, Tgba)
                sweep_stage1(st, xo, 1, gb_attn, Tgba)
                for blk in range(4):
                    if blk + 2 < 4:
                        sweep_stage1(st, xo, blk + 2, gb_attn, Tgba)
                    rope_proj(blk, st["hT"][blk % 3], st["ThT"][blk % 3], wA, TwA, wB, TwB, cosq, sinq, QT, TQT)
                S.barrier()
                if "KTd" in debug:
                    dbg_out("KTd", [128, 4, 4096], BF16); dump("KTd", KT[:], [])
                    dbg_out("QTd", [128, 4, 2048], BF16); dump("QTd", QT[:], [])
                    dbg_out("Vd", [128, 32, 4, 130], BF16); dump("Vd", Vd[:], [])
                    S.barrier()
                S.emit()
            if stop_after == "Aproj":
                S.barrier(); S.emit()
                return nc

            with ExitStack() as at:
                maskd = sb(at, "maskd", [128, 2, 1024], BF16); Tmd = S.tile()
                S.dma("sync", maskd[:], maskd_d, writes=[Tmd])
                if moe == "sparse":
                    zer = sb(at, "zer", [128, 2048], BF16); Tzer = S.tile()
                    S.op("gpsimd", lambda e: e.memset(zer[:], 0.0), writes=[Tzer])
                    for n in range(NSLOT // 256):
                        S.dma("gpsimd", xs_d[n * 256:(n + 1) * 256, :].rearrange("(p r) d -> p (r d)", r=2), zer[:], reads=[Tzer], writes=[Txs], semtile=Tzer)
                PT = [sb(at, "PT%d" % i, [128, 32, 256], BF16) for i in range(2)]
                TPT = [[S.tile() for _ in range(16)] for _ in range(2)]
                oc = sb(at, "oc", [128, 16, 4, 128], F32); Toc = [[S.tile() for _ in range(4)] for _ in range(16)]
                ssq = sb(at, "ssq", [128, 64], F32); Tssq = S.tile()
                A1 = [sb(at, "A1%d" % i, [128, 128], F32) for i in range(2)]; TA1 = [S.tile() for _ in range(2)]
                rr = sb(at, "rr", [128, 4], F32); Trr = [S.tile() for _ in range(4)]
                sjunk = sb(at, "sjunk", [128, 128], F32); Tsjunk = S.tile()
                units = [dict(h=h, i=i, z=z) for h in range(4) for i in range(8) for z in range(2)]

                def KT_of(u, kb):
                    r0 = 64 * u["z"]
                    return KT[r0:r0 + 64, u["h"], kb * 128:(kb + 1) * 128], TKT[u["h"]][kb // 4]

                def QT_of(u):
                    r0 = 64 * u["z"]
                    return QT[r0:r0 + 64, u["h"], u["i"] * 256:(u["i"] + 1) * 256], TQT[u["h"]][u["i"] // 2]

                def V_of(u, kb):
                    return Vd[:, kb, u["h"], 0:128], TV[kb]

                def exp_emit(u, p, bi, PTb, Tp):
                    S.op("scalar", lambda e: e.activation(out=PTb[:, 2 * p:2 * p + 2, :].rearrange("p a b -> p (a b)"),
                                                          in_=banks[bi][:], func=AF.Exp),
                         reads=[Tb[bi]], writes=[Tp])

                def mask_emit(u, PTb, TPb):
                    i = u["i"]
                    lo = nk_of(i) - 4
                    S.op("vector", lambda e: e.tensor_tensor(out=PTb[:, lo:lo + 4, :], in0=PTb[:, lo:lo + 4, :],
                                                             in1=maskd[:, i % 2, :].rearrange("p (a b) -> p a b", a=4), op=ALU.min),
                         reads=[Tmd, TPb[lo // 2], TPb[lo // 2 + 1]], writes=[TPb[lo // 2], TPb[lo // 2 + 1]])

                def evac_emit(u, ob):
                    h, i, z = u["h"], u["i"], u["z"]
                    for s in range(2):
                        qb = 2 * i + s
                        o_ap = banks[ob][:, s * 129:s * 129 + 128]
                        sm_ap = banks[ob][:, s * 129 + 128:s * 129 + 129]
                        ri = 2 * z + s
                        S.op("vector", lambda e, ri=ri, sm_ap=sm_ap: e.reciprocal(out=rr[:, ri:ri + 1], in_=sm_ap),
                             reads=[Tb[ob]], writes=[Trr[ri]])
                        if z == 0:
                            S.op("vector", lambda e, ri=ri, o_ap=o_ap, s=s: e.tensor_scalar(
                                out=A1[s][:], in0=o_ap, scalar1=rr[:, ri:ri + 1], scalar2=None, op0=ALU.mult),
                                reads=[Tb[ob], Trr[ri]], writes=[TA1[s]])
                        else:
                            S.op("vector", lambda e, ri=ri: e.tensor_tensor(out=rr[:, ri:ri + 1], in0=rr[:, ri:ri + 1], in1=neglam[:], op=ALU.mult),
                                 reads=[Trr[ri], Tnl], writes=[Trr[ri]])
                            S.op("vector", lambda e, ri=ri, o_ap=o_ap, s=s, qb=qb: e.scalar_tensor_tensor(
                                out=oc[:, qb, h, :], in0=o_ap, scalar=rr[:, ri:ri + 1], in1=A1[s][:], op0=ALU.mult, op1=ALU.add),
                                reads=[Tb[ob], Trr[ri], TA1[s]], writes=[Toc[qb][h]])
                            S.op("vector", lambda e, qb=qb: e.scalar_tensor_tensor(
                                out=sjunk[:], in0=oc[:, qb, h, :], scalar=1.0, in1=oc[:, qb, h, :], op0=ALU.mult, op1=ALU.mult,
                                accum_out=ssq[:, qb * 4 + h:qb * 4 + h + 1]),
                                reads=[Toc[qb][h], Tssq], writes=[Tssq, Tsjunk])

                OTs = [sb(at, "OTs%d" % i, [128, 256], F32) for i in range(2)]; TOTs = [S.tile() for _ in range(2)]
                smr = [sb(at, "smr%d" % i, [1, 256], F32) for i in range(2)]; Tsmr = [S.tile() for _ in range(2)]
                identf_a = sb(at, "identf_a", [128, 128], F32); Tidf_a = S.tile()
                S.dma("sync", identf_a[:], identf_d, writes=[Tidf_a])

                def Vsum_of(u, kb):
                    return Vd[:, kb, u["h"], 128:129], TV[kb]
                run_attention(units, PT, TPT, KT_of, QT_of, V_of, 129, exp_emit, evac_emit, mask_emit,
                              s_banks=[0, 1, 2, 3], o_banks=[4, 5, 6, 7], OTs=OTs, TOTs=TOTs, smr=smr, Tsmr=Tsmr,
                              identf=identf_a, Tidf=Tidf_a, Vsum_of=Vsum_of)
                S.op("scalar", lambda e: e.activation(out=ssq[:], in_=ssq[:], func=AF.Sqrt, scale=1.0 / 128, bias=EPS),
                     reads=[Tssq], writes=[Tssq])
                S.op("vector", lambda e: e.reciprocal(out=ssq[:], in_=ssq[:]), reads=[Tssq], writes=[Tssq])
                Otok = [sb(at, "Otok%d" % i, [128, 512], BF16) for i in range(2)]; TOtok = [S.tile() for _ in range(2)]
                for qb in range(16):
                    o2 = qb % 2
                    for h in range(4):
                        S.op("vector", lambda e, qb=qb, h=h, o2=o2: e.scalar_tensor_tensor(
                            out=Otok[o2][:, h * 128:(h + 1) * 128], in0=oc[:, qb, h, :], scalar=ssq[:, qb * 4 + h:qb * 4 + h + 1],
                            in1=gsc[:], op0=ALU.mult, op1=ALU.mult),
                            reads=[Toc[qb][h], Tssq, Tgsc], writes=[TOtok[o2]])
                    bi = o2
                    pTv = banks[bi][:].bitcast(BF16)
                    for c in range(4):
                        S.op("tensor", lambda e, c=c, o2=o2, pTv=pTv: e.transpose(
                            out=pTv[:, c * 128:(c + 1) * 128], in_=Otok[o2][:, c * 128:(c + 1) * 128], identity=identb[:]),
                            reads=[TOtok[o2], Tidb], writes=[Tb[bi]], signal=(c == 3))
                    S.op("vector", lambda e, qb=qb, pTv=pTv: e.tensor_copy(
                        out=OT[:, 0:4, qb * 128:(qb + 1) * 128], in_=pTv[:, 0:512].rearrange("p (c t) -> p c t", c=4)),
                        reads=[Tb[bi]], writes=[TOT[qb]])
                S.barrier()
                if "OTa" in debug:
                    dbg_out("OTa", [128, 8, 2048], BF16); dump("OTa", OT[:], []); S.barrier()
                S.emit()
        if stop_after == "A":
            return nc

        with ExitStack() as pb:
            KT = sb(pb, "KTf", [128, 4, 4096], BF16)
            TKT = [[S.tile() for _ in range(8)] for _ in range(4)]
            QT = sb(pb, "QTf", [128, 4, 2048], BF16)
            TQT = [[S.tile() for _ in range(4)] for _ in range(4)]
            Vf = sb(pb, "Vf", [128, 32, 8, 66], BF16)
            TV = [S.tile() for _ in range(32)]
            S.op("vector", lambda e: e.memset(Vf[:, :, :, 64:66], 1.0), writes=TV)
            zt = sb(pb, "zt", [128, 32, 8], F32); Tzt = [S.tile() for _ in range(32)]
            Fpos = sb(pb, "Fpos", [128, 32, 8], F32); TF = [S.tile() for _ in range(32)]
            Cpos = sb(pb, "Cpos", [128, 33, 8], F32); TC = [S.tile() for _ in range(33)]
            maskf = sb(pb, "maskf", [128, 2, 1024], BF16); Tmf = S.tile()
            S.dma("sync", maskf[:], maskf_d, writes=[Tmf])
            bfb = sb(pb, "bfb", [128, 8], F32); Tbfb = S.tile()
            bcast_load(bfb[:], bfor, Tbfb, 8)
            csel = sb(pb, "csel", [128, 8, 33], F32); Tcsel = S.tile()
            bcast_load(csel[:].rearrange("p a b -> p (a b)"), csel_d, Tcsel, 8 * 33)
            ctmp = sb(pb, "ctmp", [128, 8, 33], F32); Tctmp = S.tile()
            cq = sb(pb, "cq", [128, 8, 8], F32); Tcq = S.tile()
            tri = sb(pb, "tri", [128, 128], F32); Ttri = S.tile()
            S.dma("sync", tri[:], tri_d, writes=[Ttri])
            onesf = sb(pb, "onesf", [128, 128], F32); Tones = S.tile()
            S.dma("sync", onesf[:], onesf_d, writes=[Tones])

            with ExitStack() as sw:
                st = make_sweep(sw, "b", [0, 1])
                wA = sb(sw, "wA", [128, 8, 512], BF16); TwA = S.tile()
                wB = sb(sw, "wB", [128, 8, 512], BF16); TwB = S.tile()
                wF = sb(sw, "wF", [128, 8, 8], BF16); TwF = S.tile()
                wload(wA[:], w_in[:, 2048:2560], TwA)
                wload(wB[:], w_in[:, 2560:3072], TwB)
                wload(wF[:], w_in[:, 3072:3080], TwF)
                kctr = [0]

                def plain_proj(blk, hT, ThT, w_, Tw_, dstT, TdstT, scale):
                    for hp in range(4):
                        bk = 2 + (kctr[0] % 3)
                        kctr[0] += 1
                        mmgroup(banks[bk][:], [(w_[:, c, hp * 128:(hp + 1) * 128], hT[:, c, :]) for c in range(8)],
                                reads=list(ThT) + [Tw_], writes=[Tb[bk]])
                        S.op("scalar", lambda e, bk=bk, hp=hp: e.activation(
                            out=dstT[:, hp, blk * 512:(blk + 1) * 512], in_=banks[bk][:], func=AF.Copy, scale=scale),
                            reads=[Tb[bk]], writes=[TdstT[hp][blk]])

                def kvf_proj(blk, hT, ThT):
                    plain_proj(blk, hT, ThT, wA, TwA, KT, TKT, 1.0)
                    for tt in range(4):
                        n = blk * 4 + tt
                        bv = 6 + (tt % 2)
                        mmgroup(banks[bv][:], [(hT[:, c, tt * 128:(tt + 1) * 128], wB[:, c, :]) for c in range(8)],
                                reads=[ThT[tt], TwB], writes=[Tb[bv]])
                        S.op("vector", lambda e, n=n, bv=bv: e.tensor_copy(
                            out=Vf[:, n, :, 0:64], in_=banks[bv][:].rearrange("p (h d) -> p h d", h=8)),
                            reads=[Tb[bv]], writes=[TV[n]])
                        mmgroup(banks[5][:, 0:8], [(hT[:, c, tt * 128:(tt + 1) * 128], wF[:, c, :]) for c in range(8)],
                                reads=[ThT[tt], TwF], writes=[Tb[5]])
                        S.op("vector", lambda e, n=n: e.tensor_tensor(out=zt[:, n, :], in0=banks[5][:, 0:8], in1=bfb[:], op=ALU.add),
                             reads=[Tb[5], Tbfb], writes=[Tzt[n]])

                sweep_stage1(st, xb, 0, gb_attn, Tgba)
                sweep_stage1(st, xb, 1, gb_attn, Tgba)
                for blk in range(8):
                    if blk + 2 < 8:
                        sweep_stage1(st, xb, blk + 2, gb_attn, Tgba)
                    kvf_proj(blk, st["hT"][blk % 3], st["ThT"][blk % 3])
                wload(wA[:], w_in[:, 1536:2048], TwA)
                sweep_stage1(st, xo, 0, gb_attn, Tgba)
                sweep_stage1(st, xo, 1, gb_attn, Tgba)
                for blk in range(4):
                    if blk + 2 < 4:
                        sweep_stage1(st, xo, blk + 2, gb_attn, Tgba)
                    plain_proj(blk, st["hT"][blk % 3], st["ThT"][blk % 3], wA, TwA, QT, TQT, 0.125)
                ztf = zt[:].rearrange("p a b -> p (a b)")
                S.op("scalar", lambda e: e.activation(out=ztf, in_=ztf, func=AF.Exp, scale=-1.0), reads=Tzt, writes=Tzt)
                S.op("scalar", lambda e: e.activation(out=ztf, in_=ztf, func=AF.Ln, bias=1.0), reads=Tzt, writes=Tzt)
                S.op("vector", lambda e: e.memset(Cpos[:, 0, :], 0.0), writes=[TC[0]])
                for n in range(32):
                    bc = 2 + (n % 2)
                    S.op("tensor", lambda e, n=n, bc=bc: e.matmul(out=banks[bc][:, 0:8], lhsT=tri[:], rhs=zt[:, n, :], start=True, stop=True),
                         reads=[Ttri, Tzt[n]], writes=[Tb[bc]], signal=False)
                    S.op("tensor", lambda e, n=n, bc=bc: e.matmul(out=banks[bc][:, 8:16], lhsT=onesf[:], rhs=zt[:, n, :], start=True, stop=True),
                         reads=[Tones, Tzt[n]], writes=[Tb[bc]], signal=True)
                    S.op("vector", lambda e, n=n, bc=bc: e.tensor_tensor(out=Fpos[:, n, :], in0=banks[bc][:, 0:8], in1=Cpos[:, n, :], op=ALU.add),
                         reads=[Tb[bc], TC[n]], writes=[TF[n]])
                    S.op("vector", lambda e, n=n, bc=bc: e.tensor_tensor(out=Cpos[:, n + 1, :], in0=banks[bc][:, 8:16], in1=Cpos[:, n, :], op=ALU.add),
                         reads=[Tb[bc], TC[n]], writes=[TC[n + 1]])
                for i in range(8):
                    S.op("vector", lambda e, i=i: e.tensor_tensor(out=ctmp[:], in0=Cpos[:].rearrange("p n h -> p h n"),
                                                                  in1=csel[:, i, :].unsqueeze(1).broadcast_to([128, 8, 33]), op=ALU.mult),
                         reads=TC + [Tcsel, Tctmp], writes=[Tctmp])
                    S.op("vector", lambda e, i=i: e.tensor_reduce(out=cq[:, i, :], in_=ctmp[:], axis=AX.X, op=ALU.add),
                         reads=[Tctmp], writes=[Tcq])
                S.barrier()
                if "Fpos" in debug:
                    dbg_out("Fpos", [128, 32, 8]); dump("Fpos", Fpos[:], []); S.barrier()
                S.emit()

            with ExitStack() as at:
                PT = [sb(at, "PT%d" % i, [128, 32, 256], BF16) for i in range(2)]
                TPT = [[S.tile() for _ in range(16)] for _ in range(2)]
                Otf = sb(at, "Otf", [128, 16, 512], BF16); TOtf = [S.tile() for _ in range(16)]
                rr = sb(at, "rrf", [128, 2], F32); Trr = [S.tile() for _ in range(2)]
                biasb = [sb(at, "biasb%d" % i, [128, 32], F32) for i in range(2)]; Tbias = [S.tile() for _ in range(2)]
                units = [dict(hp=hp, hh=hh, i=i, head=2 * hp + hh) for hp in range(4) for hh in range(2) for i in range(8)]
                for ui, u in enumerate(units):
                    u["ui"] = ui

                def KT_of(u, kb):
                    r0 = 64 * u["hh"]
                    return KT[r0:r0 + 64, u["hp"], kb * 128:(kb + 1) * 128], TKT[u["hp"]][kb // 4]

                def QT_of(u):
                    r0 = 64 * u["hh"]
                    return QT[r0:r0 + 64, u["hp"], u["i"] * 256:(u["i"] + 1) * 256], TQT[u["hp"]][u["i"] // 2]

                ebb = [sb(at, "ebb%d" % i, [128, 32], F32) for i in range(2)]; Teb = [S.tile() for _ in range(2)]
                Vp = [sb(at, "Vp%d" % i, [128, 32, 65], BF16) for i in range(2)]; TVp = [S.tile() for _ in range(2)]

                def V_of(u, kb):
                    return Vp[u["ui"] % 2][:, kb, :], TVp[u["ui"] % 2]

                def exp_emit(u, p, bi, PTb, Tp):
                    b2 = u["ui"] % 2
                    hd = u["head"]
                    nk = nk_of(u["i"])
                    if p == 0:
                        S.op("vector", lambda e: e.tensor_scalar(out=biasb[b2][:, 0:nk], in0=Fpos[:, 0:nk, hd], scalar1=cq[:, u["i"], hd:hd + 1],
                                                                 scalar2=70.0, op0=ALU.subtract, op1=ALU.min),
                             reads=TF[0:nk] + [Tcq], writes=[Tbias[b2]])
                        S.op("scalar", lambda e: e.activation(out=ebb[b2][:, 0:nk], in_=biasb[b2][:, 0:nk], func=AF.Exp),
                             reads=[Tbias[b2]], writes=[Teb[b2]])
                        S.op("vector", lambda e: e.tensor_tensor(out=Vp[b2][:, 0:nk, :], in0=Vf[:, 0:nk, hd, 0:65],
                                                                 in1=ebb[b2][:, 0:nk].unsqueeze(2).broadcast_to([128, nk, 65]), op=ALU.mult),
                             reads=TV[0:nk] + [Teb[b2]], writes=[TVp[b2]])
                    S.op("scalar", lambda e: e.activation(out=PTb[:, 2 * p:2 * p + 2, :].rearrange("p a b -> p (a b)"),
                                                          in_=banks[bi][:], func=AF.Exp),
                         reads=[Tb[bi]], writes=[Tp])

                def mask_emit(u, PTb, TPb):
                    i = u["i"]
                    lo = nk_of(i) - 4
                    S.op("vector", lambda e: e.tensor_tensor(out=PTb[:, lo:lo + 4, :], in0=PTb[:, lo:lo + 4, :],
                                                             in1=maskf[:, i % 2, :].rearrange("p (a b) -> p a b", a=4), op=ALU.min),
                         reads=[Tmf, TPb[lo // 2], TPb[lo // 2 + 1]], writes=[TPb[lo // 2], TPb[lo // 2 + 1]])

                def evac_emit(u, ob):
                    hd, i = u["head"], u["i"]
                    for s in range(2):
                        qb = 2 * i + s
                        S.op("vector", lambda e, s=s: e.reciprocal(out=rr[:, s:s + 1], in_=banks[ob][:, s * 65 + 64:s * 65 + 65]),
                             reads=[Tb[ob]], writes=[Trr[s]])
                        S.op("vector", lambda e, s=s, qb=qb: e.tensor_scalar(
                            out=Otf[:, qb, hd * 64:(hd + 1) * 64], in0=banks[ob][:, s * 65:s * 65 + 64], scalar1=rr[:, s:s + 1],
                            scalar2=None, op0=ALU.mult),
                            reads=[Tb[ob], Trr[s]], writes=[TOtf[qb]])

                OTs = [sb(at, "OTsf%d" % i, [128, 256], F32) for i in range(2)]; TOTs = [S.tile() for _ in range(2)]
                identf_b = sb(at, "identf_b", [128, 128], F32); Tidf_b = S.tile()
                S.dma("sync", identf_b[:], identf_d, writes=[Tidf_b])
                run_attention(units, PT, TPT, KT_of, QT_of, V_of, 65, exp_emit, evac_emit, mask_emit,
                              s_banks=[0, 1, 2, 3], o_banks=[4, 5, 6, 7], OTs=OTs, TOTs=TOTs, identf=identf_b, Tidf=Tidf_b)
                for qb in range(16):
                    bi = qb % 2
                    pTv = banks[bi][:].bitcast(BF16)
                    for c in range(4):
                        S.op("tensor", lambda e, c=c, qb=qb, pTv=pTv: e.transpose(
                            out=pTv[:, c * 128:(c + 1) * 128], in_=Otf[:, qb, c * 128:(c + 1) * 128], identity=identb[:]),
                            reads=[TOtf[qb], Tidb], writes=[Tb[bi]], signal=(c == 3))
                    S.op("vector", lambda e, qb=qb, pTv=pTv: e.tensor_copy(
                        out=OT[:, 4:8, qb * 128:(qb + 1) * 128], in_=pTv[:, 0:512].rearrange("p (c t) -> p c t", c=4)),
                        reads=[Tb[bi]], writes=[TOT[qb]])
                S.barrier()
                if "OTb" in debug:
                    dbg_out("OTb", [128, 8, 2048], BF16); dump("OTb", OT[:], []); S.barrier()
                S.emit()
        if stop_after == "B":
            return nc

        with ExitStack() as pc:
            x2 = sb(pc, "x2", [128, 16, 1024], F32); Tx2 = [[S.tile() for _ in range(2)] for _ in range(16)]
            hmT = OT
            ThmT = TOT
            ovf = sb(pc, "ovf", [128, 1], I32); Tovf = S.tile()
            w12 = sb(pc, "w12", [128, 2, 16], F32); Tw12 = S.tile()
            pos = sb(pc, "pos", [128, 2, 16], I32); Tpos = S.tile()
            comb = sb(pc, "comb", [128, 16, 16], F32); Tcomb = S.tile()
            junk = sb(pc, "junkc", [128, 1024], BF16); Tjunkc = S.tile()
            ssc = sb(pc, "ssc", [128, 16], F32); Tssc = [S.tile() for _ in range(16)]; Tsscall = S.tile()
            with ExitStack() as c1:
                wo = sb(c1, "wo", [128, 8, 1024], BF16); Two = S.tile()
                wload(wo[:], w_out, Two)
                xt = [sb(c1, "xc%d" % i, [128, 1024], F32) for i in range(2)]; Txt = [S.tile() for _ in range(2)]
                rw32 = sb(c1, "rw32", [128, 8, 20], F32); Trw = S.tile()
                S.dma("sync", rw32[:], rw_d.rearrange("(c p) n -> p c n", p=128), writes=[Trw])
                rbb = sb(c1, "rbb", [128, 20], F32); Trbb = S.tile()
                bcast_load(rbb[:], rb_d, Trbb, 20)
                gbf = sb(c1, "gbf", [128, 1024], F32); Tgbf = S.tile()
                bcast_load(gbf[:], g_ffn, Tgbf, 1024)
                identf = sb(c1, "identf", [128, 128], F32); Tidf = S.tile()
                S.dma("sync", identf[:], identf_d, writes=[Tidf])
                hm32 = [sb(c1, "hm32%d" % i, [128, 1024], F32) for i in range(2)]; Thm32 = [S.tile() for _ in range(2)]
                hmT32 = [sb(c1, "hmT32%d" % i, [128, 8, 128], F32) for i in range(2)]; ThmT32 = [S.tile() for _ in range(2)]
                Lall = sb(c1, "Lall", [128, 16, 20], F32); TL = [S.tile() for _ in range(16)]
                if moe == "sparse":
                    hmb = sb(c1, "hmb", [128, 16, 1024], BF16); Thmb = [S.tile() for _ in range(16)]
                for t in range(16):
                    S.dma("sync", xt[t % 2][:], xo[t * 128:(t + 1) * 128, :], writes=[Txt[t % 2]])
                    for hf in range(2):
                        mmgroup(banks[hf][:], [(OT[:, c, t * 128:(t + 1) * 128], wo[:, c, hf * 512:(hf + 1) * 512]) for c in range(8)],
                                reads=[TOT[t], Two], writes=[Tb[hf]])
                        S.op("vector", lambda e, t=t, hf=hf: e.tensor_tensor(
                            out=x2[:, t, hf * 512:(hf + 1) * 512], in0=banks[hf][:], in1=xt[t % 2][:, hf * 512:(hf + 1) * 512], op=ALU.add),
                            reads=[Tb[hf], Txt[t % 2]], writes=[Tx2[t][hf]])
                    S.op("scalar", lambda e, t=t: e.activation(out=junk[:], in_=x2[:, t, :], func=AF.Square, accum_out=ssc[:, t:t + 1]),
                         reads=Tx2[t], writes=[Tssc[t], Tjunkc])
                if "x2" in debug:
                    dbg_out("x2", [2048, 1024])
                    for t in range(16):
                        dump("x2", x2[:, t, :], Tx2[t]) if False else S.dma("sync", dbg["x2"][t * 128:(t + 1) * 128, :], x2[:, t, :], reads=Tx2[t], writes=[Tdbg], semtile=Tdbg)
                if stop_after == "C0":
                    S.barrier(); S.emit()
                    return nc
                S.op("scalar", lambda e: e.activation(out=ssc[:], in_=ssc[:], func=AF.Sqrt, scale=1.0 / 1024, bias=EPS),
                     reads=Tssc, writes=[Tsscall])
                S.op("vector", lambda e: e.reciprocal(out=ssc[:], in_=ssc[:]), reads=[Tsscall], writes=[Tsscall])
                for t in range(16):
                    t2 = t % 2
                    S.op("vector", lambda e, t=t, t2=t2: e.scalar_tensor_tensor(
                        out=hm32[t2][:], in0=x2[:, t, :], scalar=ssc[:, t:t + 1], in1=gbf[:], op0=ALU.mult, op1=ALU.mult),
                        reads=Tx2[t] + [Tsscall, Tgbf], writes=[Thm32[t2]])
                    ba, bb = (2, 3) if t2 == 0 else (4, 5)
                    for c in range(8):
                        bk = ba if c < 4 else bb
                        S.op("tensor", lambda e, c=c, t2=t2, bk=bk: e.transpose(
                            out=banks[bk][:, (c % 4) * 128:(c % 4 + 1) * 128], in_=hm32[t2][:, c * 128:(c + 1) * 128], identity=identf[:]),
                            reads=[Thm32[t2], Tidf], writes=[Tb[bk]], signal=(c % 4 == 3))
                    import os
                    CUT = int(os.environ.get("C1CUT", "9"))
                    if CUT < 2:
                        continue
                    for k, bk in enumerate((ba, bb)):
                        src = banks[bk][:].rearrange("p (c t) -> p c t", c=4)
                        S.op("scalar", lambda e, k=k, t2=t2, src=src: e.activation(out=hmT32[t2][:, 4 * k:4 * k + 4, :], in_=src, func=AF.Copy),
                             reads=[Tb[bk]], writes=[ThmT32[t2]])
                        S.op("vector", lambda e, k=k, t=t, t2=t2: e.tensor_copy(out=hmT[:, 4 * k:4 * k + 4, t * 128:(t + 1) * 128], in_=hmT32[t2][:, 4 * k:4 * k + 4, :]),
                             reads=[ThmT32[t2]], writes=[ThmT[t]])
                    if moe == "sparse":
                        S.op("gpsimd", lambda e, t=t, t2=t2: e.tensor_copy(out=hmb[:, t, :], in_=hm32[t2][:]), reads=[Thm32[t2]], writes=[Thmb[t]])
                    if CUT < 3:
                        continue
                    br = 6 + t2
                    mmgroup(banks[br][:, 0:20], [(hmT32[t2][:, c, :], rw32[:, c, :]) for c in range(8)],
                            reads=[ThmT32[t2], Trw], writes=[Tb[br]])
                    S.op("vector", lambda e, t=t, br=br: e.tensor_tensor(out=Lall[:, t, :], in0=banks[br][:, 0:20], in1=rbb[:], op=ALU.add),
                         reads=[Tb[br], Trbb], writes=[TL[t]])
                if stop_after == "C1a":
                    if "Lall" in debug:
                        dbg_out("Lall", [128, 320])
                        S.dma("sync", dbg["Lall"], Lall[:].rearrange("p a b -> p (a b)"), reads=[], writes=[Tdbg], semtile=Tdbg)
                    S.barrier(); S.emit()
                    return nc
                TR = S.tile()

                def rt(name, shape):
                    return sb(c1, "rt_" + name, shape, F32)
                gmax = rt("gmax", [128, 16]); gm = rt("gm", [128, 16, 4]); gd = rt("gd", [128, 16, 4])
                gsum = rt("gsum", [128, 16]); gw = rt("gw", [128, 16]); pen = rt("pen", [128, 16, 4])
                EL = rt("EL", [128, 16, 16]); EL2 = rt("EL2", [128, 16, 16]); m1 = rt("m1", [128, 16]); m2 = rt("m2", [128, 16])
                oh1 = rt("oh1", [128, 16, 16]); oh2 = rt("oh2", [128, 16, 16]); dd = rt("dd", [128, 16]); w1 = rt("w1", [128, 16]); w2 = rt("w2", [128, 16])
                LG = Lall[:, :, 0:4]
                LE4 = Lall[:, :, 4:20].rearrange("p t (g e) -> p t g e", g=4)
                EL4 = EL[:].rearrange("p t (g e) -> p t g e", g=4)

                def vop(fn, first=False):
                    S.op("vector", fn, reads=(TL + [TR]) if first else [TR], writes=[TR])

                def bc3(a, n):
                    return a[:].unsqueeze(2).broadcast_to([128, 16, n])
                vop(lambda e: e.tensor_reduce(out=gmax[:], in_=LG, axis=AX.X, op=ALU.max), first=True)
                vop(lambda e: e.tensor_tensor(out=gm[:], in0=LG, in1=bc3(gmax, 4), op=ALU.is_equal))
                vop(lambda e: e.tensor_tensor(out=gd[:], in0=LG, in1=bc3(gmax, 4), op=ALU.subtract))
                S.op("scalar", lambda e: e.activation(out=gd[:], in_=gd[:], func=AF.Exp), reads=[TR], writes=[TR])
                vop(lambda e: e.tensor_reduce(out=gsum[:], in_=gd[:], axis=AX.X, op=ALU.add))
                vop(lambda e: e.reciprocal(out=gw[:], in_=gsum[:]))
                vop(lambda e: e.tensor_scalar(out=pen[:], in0=gm[:], scalar1=1.0, scalar2=1e30, op0=ALU.subtract, op1=ALU.mult))
                vop(lambda e: e.tensor_tensor(out=EL4, in0=LE4, in1=gm[:].unsqueeze(3).broadcast_to([128, 16, 4, 4]), op=ALU.mult))
                vop(lambda e: e.tensor_tensor(out=EL4, in0=EL4, in1=pen[:].unsqueeze(3).broadcast_to([128, 16, 4, 4]), op=ALU.add))
                vop(lambda e: e.tensor_reduce(out=m1[:], in_=EL[:], axis=AX.X, op=ALU.max))
                vop(lambda e: e.tensor_tensor(out=oh1[:], in0=EL[:], in1=bc3(m1, 16), op=ALU.is_equal))
                vop(lambda e: e.scalar_tensor_tensor(out=EL2[:], in0=oh1[:], scalar=-1e30, in1=EL[:], op0=ALU.mult, op1=ALU.add))
                vop(lambda e: e.tensor_reduce(out=m2[:], in_=EL2[:], axis=AX.X, op=ALU.max))
                vop(lambda e: e.tensor_tensor(out=oh2[:], in0=EL2[:], in1=bc3(m2, 16), op=ALU.is_equal))
                vop(lambda e: e.tensor_tensor(out=dd[:], in0=m2[:], in1=m1[:], op=ALU.subtract))
                S.op("scalar", lambda e: e.activation(out=dd[:], in_=dd[:], func=AF.Exp), reads=[TR], writes=[TR])
                vop(lambda e: e.tensor_scalar(out=w1[:], in0=dd[:], scalar1=1.0, scalar2=None, op0=ALU.add))
                vop(lambda e: e.reciprocal(out=w1[:], in_=w1[:]))
                vop(lambda e: e.tensor_tensor(out=w1[:], in0=w1[:], in1=gw[:], op=ALU.mult))
                vop(lambda e: e.tensor_tensor(out=w2[:], in0=dd[:], in1=w1[:], op=ALU.mult))
                if moe == "sparse":
                    Mb = sb(c1, "Mb", [128, 16, 16], BF16)
                    ustrict = sb(c1, "ustrict", [128, 128], BF16); Tus = S.tile()
                    S.dma("sync", ustrict[:], ustrict_d, writes=[Tus])
                    onesb = sb(c1, "onesb", [128, 128], BF16); Tob_ = S.tile()
                    S.dma("sync", onesb[:], onesb_d, writes=[Tob_])
                    ebase = sb(c1, "ebase", [128, 16, 16], F32); Teb = S.tile()
                    S.dma("sync", ebase[:].rearrange("p a b -> p (a b)"), ebase_d, writes=[Teb])
                    slotf = rt("slotf", [128, 16, 16]); okf = rt("okf", [128, 16, 16]); posf = rt("posf", [128, 2, 16])
                    vop(lambda e: e.tensor_tensor(out=Mb[:], in0=oh1[:], in1=oh2[:], op=ALU.add))
                    for t in range(16):
                        prs = [(onesb[:], Mb[:, tp, :]) for tp in range(t)] + [(ustrict[:], Mb[:, t, :])]
                        n_ = len(prs)
                        for k_, (l_, r_) in enumerate(prs):
                            S.op("tensor", lambda e, l_=l_, r_=r_, k_=k_, n_=n_, t=t: e.matmul(out=banks[0][:, t * 16:(t + 1) * 16], lhsT=l_, rhs=r_,
                                                                                         start=(k_ == 0), stop=(k_ == n_ - 1)),
                                 reads=[TR, Tus, Tob_], writes=[Tb[0]], signal=(k_ == n_ - 1))
                    for tp in range(16):
                        S.op("tensor", lambda e, tp=tp: e.matmul(out=banks[1][:, 0:16], lhsT=onesb[:], rhs=Mb[:, tp, :], start=(tp == 0), stop=(tp == 15)),
                             reads=[TR, Tob_], writes=[Tb[1]], signal=(tp == 15))
                    cmax = rt("cmax", [128, 1])
                    S.op("vector", lambda e: e.tensor_reduce(out=cmax[:], in_=banks[1][:, 0:16], axis=AX.X, op=ALU.max), reads=[Tb[1], TR], writes=[TR])
                    import os as _os
                    thr = -1.0 if _os.environ.get("FORCE_DENSE") else float(CAP)
                    vop(lambda e: e.tensor_scalar(out=cmax[:], in0=cmax[:], scalar1=thr, scalar2=None, op0=ALU.is_gt))
                    S.op("vector", lambda e: e.tensor_copy(out=ovf[:], in_=cmax[:]), reads=[TR], writes=[Tovf])
                    rank = banks[0][:, 0:256].rearrange("p (a b) -> p a b", a=16)
                    S.op("vector", lambda e: e.tensor_tensor(out=slotf[:], in0=rank, in1=ebase[:], op=ALU.add), reads=[Tb[0], Teb, TR], writes=[TR])
                    vop(lambda e: e.tensor_scalar(out=okf[:], in0=slotf[:], scalar1=None, scalar2=None, op0=ALU.bypass) if False else
                        e.tensor_tensor(out=okf[:], in0=slotf[:], in1=ebase[:], op=ALU.subtract))
                    vop(lambda e: e.tensor_scalar(out=okf[:], in0=okf[:], scalar1=float(CAP), scalar2=1.0e6, op0=ALU.is_ge, op1=ALU.mult))
                    vop(lambda e: e.tensor_tensor(out=slotf[:], in0=slotf[:], in1=okf[:], op=ALU.add))
                    vop(lambda e: e.tensor_tensor(out=okf[:], in0=slotf[:], in1=oh1[:], op=ALU.mult))
                    vop(lambda e: e.tensor_reduce(out=posf[:, 0, :], in_=okf[:], axis=AX.X, op=ALU.add))
                    vop(lambda e: e.tensor_tensor(out=okf[:], in0=slotf[:], in1=oh2[:], op=ALU.mult))
                    vop(lambda e: e.tensor_reduce(out=posf[:, 1, :], in_=okf[:], axis=AX.X, op=ALU.add))
                    S.op("vector", lambda e: e.tensor_copy(out=pos[:], in_=posf[:]), reads=[TR], writes=[Tpos])
                    S.op("vector", lambda e: e.tensor_copy(out=w12[:, 0, :], in_=w1[:]), reads=[TR, Tw12], writes=[Tw12])
                    S.op("vector", lambda e: e.tensor_copy(out=w12[:, 1, :], in_=w2[:]), reads=[TR, Tw12], writes=[Tw12])
                    Tsc = [S.tile() for _ in range(32)]
                    set_bcreg()
                    for t in range(16):
                        for k_ in range(2):
                            S.dma_fn("gpsimd", lambda e, t=t, k_=k_: e.indirect_dma_start(
                                out=xs_d[:, :], out_offset=bass.IndirectOffsetOnAxis(ap=pos[:, k_, t:t + 1], axis=0),
                                in_=hmb[:, t, :], in_offset=None, bounds_check=bcreg, oob_is_err=False),
                                reads=[Thmb[t], Tpos, Txs], writes=[Tsc[2 * t + k_]], semtile=Thmb[t])
                vop(lambda e: e.tensor_tensor(out=oh1[:], in0=oh1[:], in1=bc3(w1, 16), op=ALU.mult))
                vop(lambda e: e.tensor_tensor(out=oh2[:], in0=oh2[:], in1=bc3(w2, 16), op=ALU.mult))
                S.op("vector", lambda e: e.tensor_tensor(out=comb[:], in0=oh1[:], in1=oh2[:], op=ALU.add), reads=[TR], writes=[Tcomb])
                S.barrier()
                if "comb" in debug:
                    dbg_out("comb", [128, 256])
                    S.dma("sync", dbg["comb"], comb[:].rearrange("p a b -> p (a b)"), reads=[Tcomb], writes=[Tdbg], semtile=Tdbg)
                    S.barrier()
                S.emit()
            if stop_after == "C1":
                return nc

            with ExitStack() as c2:
                wgb = [sb(c2, "wgb%d" % i, [128, 8, 512], BF16) for i in range(2)]; Twg4 = [[S.tile() for _ in range(4)] for _ in range(2)]
                wub = [sb(c2, "wub%d" % i, [128, 8, 512], BF16) for i in range(2)]; Twu4 = [[S.tile() for _ in range(4)] for _ in range(2)]
                wdb = [sb(c2, "wdb%d" % i, [128, 4, 1024], BF16) for i in range(2)]; Twd4 = [[S.tile() for _ in range(4)] for _ in range(2)]
                stg = [sb(c2, "stg%d" % i, [128, 1024], F32) for i in range(3)]; Tstg = [S.tile() for _ in range(3)]
                sq_ = [0]
                aT = [sb(c2, "aT%d" % i, [128, 4, 512], BF16) for i in range(2)]; TaT = [[S.tile() for _ in range(4)] for _ in range(2)]
                sg = [sb(c2, "sg%d" % i, [128, 512], F32) for i in range(2)]; Tsg = [S.tile() for _ in range(2)]
                xg = [sb(c2, "xg%d" % i, [128, 1024], BF16) for i in range(4)]; Txg = [S.tile() for _ in range(4)]
                xgT = [sb(c2, "xgT%d" % i, [128, 8, CAP], BF16) for i in range(2)]; TxgT = [[S.tile() for _ in range(CAP // 128)] for _ in range(2)]
                ysb = [sb(c2, "ysb%d" % i, [128, 1024], F32) for i in range(2)]; Tysb = [S.tile() for _ in range(2)]
                NJ = CAP // 128
                Tys = [S.tile() for _ in range(NEXP * NJ)]

                def w_steps(ex):
                    b2 = ex % 2
                    dmas, casts = [], []
                    for k in range(12):
                        def mk(k=k):
                            if k < 8:
                                srcw = (wg_d if k < 4 else wu_d)[ex]
                                kk = k % 4
                                src = srcw[kk * 256:(kk + 1) * 256, :].rearrange("(c p) n -> p c n", p=128)
                                dst_of = lambda: (wgb if k < 4 else wub)[b2][:, 2 * kk:2 * kk + 2, :]
                                Td = (Twg4 if k < 4 else Twu4)[b2][kk]
                                view = lambda t_: t_[:].rearrange("p (c n) -> p c n", c=2)
                            else:
                                kk = k - 8
                                src = wd_d[ex][kk * 128:(kk + 1) * 128, :]
                                dst_of = lambda: wdb[b2][:, kk, :]
                                Td = Twd4[b2][kk]
                                view = lambda t_: t_[:]
                            cell = {}

                            def d():
                                si = sq_[0] % 3
                                sq_[0] += 1
                                cell["si"] = si
                                S.dma("sync", view(stg[si]), src, writes=[Tstg[si]])

                            def c():
                                si = cell["si"]
                                sv = view(stg[si])
                                dstb = dst_of()
                                if k % 2 == 1:
                                    S.op("scalar", lambda e: e.activation(out=dstb, in_=sv, func=AF.Copy), reads=[Tstg[si]], writes=[Td])
                                else:
                                    S.op("vector", lambda e: e.tensor_copy(out=dstb, in_=sv), reads=[Tstg[si]], writes=[Td])
                            return d, c
                        d, c = mk()
                        dmas.append(d)
                        casts.append(c)
                    steps = dmas[0:3]
                    for k in range(12):
                        steps.append(casts[k])
                        if k + 3 < 12:
                            steps.append(dmas[k + 3])
                    return steps

                def load_w(ex):
                    for f in w_steps(ex):
                        f()

                def gate_up(b2, rhs_of, Trhs, width, gq, slot=None):
                    for ft in range(4):
                        bg, bu = (0, 1) if gq[0] % 2 == 0 else (2, 3)
                        s2 = gq[0] % 2
                        gq[0] += 1
                        mmgroup(banks[bg][:, 0:width], [(wgb[b2][:, c, ft * 128:(ft + 1) * 128], rhs_of(c)) for c in range(8)],
                                reads=Trhs + Twg4[b2], writes=[Tb[bg]])
                        mmgroup(banks[bu][:, 0:width], [(wub[b2][:, c, ft * 128:(ft + 1) * 128], rhs_of(c)) for c in range(8)],
                                reads=Trhs + Twu4[b2], writes=[Tb[bu]])
                        S.op("scalar", lambda e, bg=bg, s2=s2: e.activation(out=sg[s2][:, 0:width], in_=banks[bg][:, 0:width], func=AF.Silu),
                             reads=[Tb[bg]], writes=[Tsg[s2]])
                        S.op("vector", lambda e, bu=bu, s2=s2, ft=ft: e.tensor_tensor(out=aT[b2][:, ft, 0:width], in0=sg[s2][:, 0:width], in1=banks[bu][:, 0:width], op=ALU.mult),
                             reads=[Tsg[s2], Tb[bu]], writes=[TaT[b2][ft]])
                        if slot is not None:
                            slot()

                S.branch_begin()
                gq = [0]; yq = [0]; xq = [0]

                def prep(ex):
                    b2 = ex % 2
                    for j in range(NJ):
                        xi = xq[0] % 4
                        xq[0] += 1
                        r0 = ex * CAP + j * 128
                        S.dma("gpsimd", xg[xi][:], xs_d[r0:r0 + 128, :], reads=Tsc + [Txs], writes=[Txg[xi]])
                        bi = 6 + (xq[0] % 2)
                        pTv = banks[bi][:].bitcast(BF16)
                        for c in range(8):
                            S.op("tensor", lambda e, c=c, xi=xi, pTv=pTv: e.transpose(
                                out=pTv[:, c * 128:(c + 1) * 128], in_=xg[xi][:, c * 128:(c + 1) * 128], identity=identb[:]),
                                reads=[Txg[xi], Tidb], writes=[Tb[bi]], signal=(c == 7))
                        S.op("vector", lambda e, j=j, b2=b2, pTv=pTv: e.tensor_copy(
                            out=xgT[b2][:, :, j * 128:(j + 1) * 128], in_=pTv.rearrange("p (c t) -> p c t", c=8)),
                            reads=[Tb[bi]], writes=[TxgT[b2][j]])
                prep(0)
                load_w(0)
                for ex in range(NEXP):
                    b2 = ex % 2
                    wq = w_steps(ex + 1) if ex + 1 < NEXP else []

                    def pop(n):
                        for _ in range(n):
                            if wq:
                                wq.pop(0)()
                    pop(3)
                    gate_up(b2, lambda c, b2=b2: xgT[b2][:, c, :], TxgT[b2], CAP, gq, slot=lambda: pop(3))
                    if ex + 1 < NEXP:
                        prep(ex + 1)
                    for j in range(NJ):
                        y2 = yq[0] % 2
                        yq[0] += 1
                        for hf in range(2):
                            by = 4 + hf
                            mmgroup(banks[by][:], [(aT[b2][:, ft, j * 128:(j + 1) * 128], wdb[b2][:, ft, hf * 512:(hf + 1) * 512]) for ft in range(4)],
                                    reads=TaT[b2] + Twd4[b2], writes=[Tb[by]])
                            if hf == 0:
                                S.op("vector", lambda e, y2=y2, by=by: e.tensor_copy(out=ysb[y2][:, 0:512], in_=banks[by][:]),
                                     reads=[Tb[by]], writes=[Tysb[y2]])
                            else:
                                S.op("scalar", lambda e, y2=y2, by=by: e.activation(out=ysb[y2][:, 512:1024], in_=banks[by][:], func=AF.Copy),
                                     reads=[Tb[by], Tysb[y2]], writes=[Tysb[y2]])
                        r0 = ex * CAP + j * 128
                        S.dma("gpsimd", ys_d[r0:r0 + 128, :], ysb[y2][:], reads=[Tysb[y2]], writes=[Tys[ex * NJ + j]], semtile=Tysb[y2])
                        pop(3)
                    pop(99)
                S.barrier()
                ygl = []
                for wb in wgb + wub + wdb:
                    v = wb[:].rearrange("p a b -> p (a b)").bitcast(F32)
                    ygl += [v[:, 0:1024], v[:, 1024:2048]]
                Tyg = [S.tile() for _ in ygl]
                set_bcreg()
                def gath(i):
                    t, k_ = i // 2, i % 2
                    gi = i % len(ygl)
                    S.dma_fn("gpsimd", lambda e, t=t, k_=k_, gi=gi: e.indirect_dma_start(
                        out=ygl[gi], out_offset=None, in_=ys_d[:, :],
                        in_offset=bass.IndirectOffsetOnAxis(ap=pos[:, k_, t:t + 1], axis=0),
                        bounds_check=bcreg, oob_is_err=False),
                        reads=Tys + [Tpos], writes=[Tyg[gi]], semtile=Tyg[gi])

                def acc(i):
                    t, k_ = i // 2, i % 2
                    gi = i % len(ygl)
                    for hf in range(2):
                        S.op("vector", lambda e, t=t, k_=k_, gi=gi, hf=hf: e.scalar_tensor_tensor(
                            out=x2[:, t, hf * 512:(hf + 1) * 512], in0=ygl[gi][:, hf * 512:(hf + 1) * 512], scalar=w12[:, k_, t:t + 1],
                            in1=x2[:, t, hf * 512:(hf + 1) * 512], op0=ALU.mult, op1=ALU.add),
                            reads=[Tyg[gi], Tw12, Tx2[t][hf]], writes=[Tx2[t][hf]])
                depth = len(ygl) - 1
                for i in range(32 + depth):
                    if i < 32:
                        gath(i)
                    if i - depth >= 0:
                        acc(i - depth)
                S.branch_mid()
                gq = [0]; yq = [0]
                load_w(0)
                for ex in range(NEXP):
                    b2 = ex % 2
                    if ex + 1 < NEXP:
                        load_w(ex + 1)
                    for tb in range(4):
                        gate_up(b2, lambda c, tb=tb: hmT[:, c, tb * 512:(tb + 1) * 512], ThmT[tb * 4:tb * 4 + 4], 512, gq)
                        for tt in range(4):
                            t = tb * 4 + tt
                            for hf in range(2):
                                by = 4 + (yq[0] % 4)
                                yq[0] += 1
                                mmgroup(banks[by][:], [(aT[b2][:, ft, tt * 128:(tt + 1) * 128], wdb[b2][:, ft, hf * 512:(hf + 1) * 512]) for ft in range(4)],
                                        reads=TaT[b2] + Twd4[b2], writes=[Tb[by]])
                                S.op("vector", lambda e, t=t, hf=hf, by=by, ex=ex: e.scalar_tensor_tensor(
                                    out=x2[:, t, hf * 512:(hf + 1) * 512], in0=banks[by][:], scalar=comb[:, t, ex:ex + 1],
                                    in1=x2[:, t, hf * 512:(hf + 1) * 512], op0=ALU.mult, op1=ALU.add),
                                    reads=[Tb[by], Tcomb, Tx2[t][hf]], writes=[Tx2[t][hf]])
                S.branch_end(ovf[0:1, 0:1], brregs)
                S.emit()

            with ExitStack() as c3:
                gbn = sb(c3, "gbn", [128, 1024], F32); Tgbn = S.tile()
                bcast_load(gbn[:], g_fin, Tgbn, 1024)
                ob = [sb(c3, "ob%d" % i, [128, 1024], F32) for i in range(2)]; Tob = [S.tile() for _ in range(2)]
                Tout = S.tile()
                for t in range(16):
                    S.op("scalar", lambda e, t=t: e.activation(out=junk[:], in_=x2[:, t, :], func=AF.Square, accum_out=ssc[:, t:t + 1]),
                         reads=Tx2[t] + [Tsscall], writes=[Tssc[t], Tjunkc])
                S.op("scalar", lambda e: e.activation(out=ssc[:], in_=ssc[:], func=AF.Sqrt, scale=1.0 / 1024, bias=EPS),
                     reads=Tssc, writes=[Tsscall])
                S.op("vector", lambda e: e.reciprocal(out=ssc[:], in_=ssc[:]), reads=[Tsscall], writes=[Tsscall])
                for t in range(16):
                    S.op("vector", lambda e, t=t: e.scalar_tensor_tensor(
                        out=ob[t % 2][:], in0=x2[:, t, :], scalar=ssc[:, t:t + 1], in1=gbn[:], op0=ALU.mult, op1=ALU.mult),
                        reads=Tx2[t] + [Tsscall, Tgbn], writes=[Tob[t % 2]])
                    S.dma("sync", out[t * 128:(t + 1) * 128, :], ob[t % 2][:], reads=[Tob[t % 2]], writes=[Tout], semtile=Tob[t % 2])
                S.barrier()
                S.emit()
    return nc


def _const_tables():
    f32 = np.float32
    inv_freq = (f32(1.0) / (f32(10000.0) ** (np.arange(0, 64, 2, dtype=f32) / f32(64)))).astype(f32)
    pos = np.arange(4096, dtype=f32)
    ang = (pos[:, None] * inv_freq[None, :]).astype(f32)
    cos = np.cos(ang).astype(f32)
    sin = np.sin(ang).astype(f32)
    r = np.arange(128)
    dh = r % 64
    cosT = cos[:, dh % 32].T.copy()
    sgn = np.where(dh < 32, -1.0, 1.0).astype(f32)
    sinT = (sin[:, dh % 32].T * sgn[:, None]).astype(f32)
    return cosT, sinT


def _masks(hf):
    k = np.arange(128)[:, None, None]
    r = np.arange(4)[None, :, None]
    q = np.arange(256)[None, None, :]
    md = np.zeros((128, 2, 4, 256), np.float32)
    mf = np.zeros((128, 2, 4, 256), np.float32)
    for par in range(2):
        if par == 0:
            kb = r
            j = 0 if hf == 0 else 1
        else:
            kb = 4 + r
            j = 3 if hf == 0 else 2
        s = kb * 128 + k
        t = j * 256 + q
        mf[:, par] = np.where(s <= t, 3e38, 0.0)
        md[:, par] = np.where((s // 64) <= (t // 64), 3e38, 0.0)
    return (md.reshape(128, 2, 1024).astype(ml_dtypes.bfloat16),
            mf.reshape(128, 2, 1024).astype(ml_dtypes.bfloat16))


def own_tokens(hf):
    return np.concatenate([np.arange(j * 256, (j + 1) * 256) for j in own_qtiles(hf)])


def prep(inputs):
    f32 = np.float32
    x = np.asarray(inputs["x"], f32)
    w_in = np.ascontiguousarray(np.asarray(inputs["w_in"], f32)[0])

    def swap_cols(w):
        return np.ascontiguousarray(w.reshape(1024, 8, 2, 32)[:, :, ::-1, :].reshape(1024, 512))

    cosT, sinT = _const_tables()
    common = {
        "w_in": w_in,
        "wqs": swap_cols(w_in[:, 0:512]),
        "wks": swap_cols(w_in[:, 512:1024]),
        "cosk": cosT, "sink": sinT,
        "identb": np.eye(128, dtype=f32).astype(ml_dtypes.bfloat16),
        "identf": np.eye(128, dtype=f32),
        "tri": np.triu(np.ones((128, 128), f32)),
        "onesf": np.ones((128, 128), f32),
        "onesb": np.ones((128, 128), f32).astype(ml_dtypes.bfloat16),
        "ustrict": np.triu(np.ones((128, 128), f32), 1).astype(ml_dtypes.bfloat16),
        "ebase": np.ascontiguousarray(np.broadcast_to((np.arange(16, dtype=f32) * CAP)[None, None, :], (128, 16, 16)).reshape(128, 256)),
        "g_attn": np.asarray(inputs["norm_attn_g"], f32).reshape(1, 1024),
        "g_ffn": np.asarray(inputs["norm_ffn_g"], f32).reshape(1, 1024),
        "g_fin": np.asarray(inputs["norm_final_g"], f32).reshape(1, 1024),
        "bfor": np.asarray(inputs["b_forget"], f32).reshape(1, 8),
        "lamv": np.concatenate([np.asarray(inputs[k], f32).reshape(1, 64) for k in
                                ("lambda_q1", "lambda_k1", "lambda_q2", "lambda_k2")], axis=1),
        "dng": np.asarray(inputs["diff_norm_g"], f32).reshape(1, 128),
        "w_out": np.ascontiguousarray(np.asarray(inputs["w_out"], f32)[0]),
        "rw": np.ascontiguousarray(np.concatenate([np.asarray(inputs["router_group_w"], f32)[0],
                                                   np.asarray(inputs["router_expert_w"], f32)[0]], axis=1)),
        "rb": np.concatenate([np.asarray(inputs["router_group_b"], f32).reshape(1, 4),
                              np.asarray(inputs["router_expert_b"], f32).reshape(1, 16)], axis=1),
        "wg": np.ascontiguousarray(np.asarray(inputs["w_gate"], f32)[0]),
        "wu": np.ascontiguousarray(np.asarray(inputs["w_up"], f32)[0]),
        "wd": np.ascontiguousarray(np.asarray(inputs["w_down"], f32)[0]),
    }
    in_maps = []
    for c in range(8):
        b, hf = c // 2, c % 2
        tok = own_tokens(hf)
        md, mf = _masks(hf)
        m = dict(common)
        m["xb"] = np.ascontiguousarray(x[b])
        m["xo"] = np.ascontiguousarray(x[b][tok])
        m["cosq"] = np.ascontiguousarray(cosT[:, tok] * f32(0.125))
        m["sinq"] = np.ascontiguousarray(sinT[:, tok] * f32(0.125))
        cs = np.zeros((8, 33), f32)
        for i, j in enumerate(own_qtiles(hf)):
            cs[i, 2 * j + 1] = 1.0
        m["csel"] = cs.reshape(1, 8 * 33)
        m["maskd"] = md
        m["maskf"] = mf
        in_maps.append(m)
    return in_maps


def kernel(**inputs):
    in_maps = prep(inputs)
    nc = build()
    res = run_bass_kernel_spmd(nc, in_maps, core_ids=list(range(8)))
    out = np.zeros((4, 4096, 1024), np.float32)
    for c in range(8):
        b, hf = c // 2, c % 2
        out[b, own_tokens(hf)] = res.results[c]["out"]
    return out
```

```python
import numpy as np
import ml_dtypes
from contextlib import ExitStack
import concourse.bass as bass
import concourse.mybir as mybir
from concourse.bass_utils import run_bass_kernel_spmd

F32 = mybir.dt.float32
BF16 = mybir.dt.bfloat16
I32 = mybir.dt.int32
AF = mybir.ActivationFunctionType
ALU = mybir.AluOpType
AX = mybir.AxisListType

ENGS = ("sync", "scalar", "vector", "gpsimd", "tensor")
EPS = 1e-6
LAM_INIT = 0.8 - 0.6 * 1.0
NEXP = 16
CAP = 512
NSLOT = NEXP * CAP


class Tile:
    __slots__ = ("name", "last_w", "readers", "dsem")

    def __init__(self, name):
        self.name = name
        self.last_w = None
        self.readers = {}
        self.dsem = None


class Sched:
    def __init__(self, nc, es):
        self.nc = nc
        self.es = es
        self.ops = {e: [] for e in ENGS}
        self.sems = {}
        self.cnt = {}
        self.waited = {e: {} for e in ENGS}
        self.pending = {e: False for e in ENGS}
        for e in ENGS:
            self._mksem("E:" + e)
        self.n_dsem = 0
        self.nops = 0
        self.tiles = []

    def _mksem(self, key):
        self.sems[key] = self.es.enter_context(self.nc.semaphore(key.replace(":", "_")))
        self.cnt[key] = 0

    def tile(self, name="t"):
        t = Tile(name)
        self.tiles.append(t)
        return t

    def _snapshot(self):
        return (dict(self.cnt), {e: dict(w) for e, w in self.waited.items()},
                [(t, t.last_w, dict(t.readers)) for t in self.tiles])

    def _restore(self, snap):
        self.cnt = dict(snap[0])
        for k in self.sems:
            self.cnt.setdefault(k, 0)
        self.waited = {e: dict(w) for e, w in snap[1].items()}
        for t, lw, rd in snap[2]:
            t.last_w = lw
            t.readers = dict(rd)

    def branch_begin(self):
        self.barrier()
        self._outer_ops = self.ops
        self.ops = {e: [] for e in ENGS}
        self._snap = self._snapshot()

    def branch_mid(self):
        self.barrier()
        self._A = (self.ops, dict(self.cnt))
        self.ops = {e: [] for e in ENGS}
        self._restore(self._snap)

    def branch_end(self, flag_ap, regs):
        self.barrier()
        opsA, cntA = self._A
        opsB, cntB = self.ops, dict(self.cnt)
        target = {k: max(cntA.get(k, 0), cntB.get(k, 0)) for k in set(cntA) | set(cntB)}

        def pads(cntX):
            out = {e: [] for e in ENGS}
            for k, v in target.items():
                d = v - cntX.get(k, 0)
                if d > 0:
                    owner = k[2:] if k.startswith("E:") else "gpsimd"
                    out[owner].append((k, d))
            return out
        pA, pB = pads(cntA), pads(cntB)
        self.ops = self._outer_ops
        for e in ENGS:
            self.ops[e].append(("branch", flag_ap, regs[e], opsA[e], pA[e], opsB[e], pB[e]))
        self.cnt = target
        for e in ENGS:
            self.waited[e] = dict(target)
        for t in self.tiles:
            t.last_w = None
            t.readers = {}

    def dsem_for(self, t):
        if t.dsem is None:
            key = "D:%d" % self.n_dsem
            self.n_dsem += 1
            self._mksem(key)
            t.dsem = key
        return t.dsem

    def _need(self, eng, waits, key, val):
        if eng == "tensor" and key == "E:tensor":
            return
        if self.cnt[key] < val:
            raise RuntimeError("wait on un-signalled event %s %d (cnt %d) from %s" % (key, val, self.cnt[key], eng))
        if self.waited[eng].get(key, 0) >= val:
            return
        self.waited[eng][key] = val
        waits[key] = max(waits.get(key, 0), val)

    def _deps(self, eng, reads, writes):
        waits = {}
        for t in reads:
            if t.last_w is not None:
                self._need(eng, waits, *t.last_w)
        for t in writes:
            if t.last_w is not None:
                self._need(eng, waits, *t.last_w)
            for k, v in t.readers.items():
                self._need(eng, waits, k, v)
        return list(waits.items())

    def _record(self, ev, reads, writes):
        for t in writes:
            t.last_w = ev
            t.readers = {}
        for t in reads:
            if t not in writes:
                if t.readers.get(ev[0], 0) < ev[1]:
                    t.readers[ev[0]] = ev[1]

    def op(self, eng, fn, reads=(), writes=(), signal=True):
        waits = self._deps(eng, reads, writes)
        key = "E:" + eng
        if signal:
            self.cnt[key] += 1
            ev = (key, self.cnt[key])
            inc = (key, 1)
            self.pending[eng] = False
        else:
            ev = (key, self.cnt[key] + 1)
            inc = None
            self.pending[eng] = True
        self._record(ev, reads, writes)
        self.ops[eng].append((waits, fn, inc))
        self.nops += 1

    def dma(self, eng, out, in_, reads=(), writes=(), semtile=None, **kw):
        waits = self._deps(eng, reads, writes)
        if semtile is None:
            semtile = writes[0] if writes else reads[0]
        key = self.dsem_for(semtile)
        self.cnt[key] += 16
        ev = (key, self.cnt[key])
        self._record(ev, reads, writes)

        def fn(e, out=out, in_=in_, kw=kw):
            return e.dma_start(out=out, in_=in_, **kw)
        self.ops[eng].append((waits, fn, (key, 16)))
        self.nops += 1

    def dma_fn(self, eng, fn, reads=(), writes=(), semtile=None):
        waits = self._deps(eng, reads, writes)
        key = self.dsem_for(semtile)
        self.cnt[key] += 16
        ev = (key, self.cnt[key])
        self._record(ev, reads, writes)
        self.ops[eng].append((waits, fn, (key, 16)))
        self.nops += 1

    def barrier(self, engs=ENGS):
        for e in ENGS:
            assert not self.pending[e], e
        for e in engs:
            waits = {}
            for key, c in self.cnt.items():
                if c > 0:
                    self._need(e, waits, key, c)
            self.ops[e].append((list(waits.items()), None, None))

    def emit(self):
        nc = self.nc
        sems = self.sems
        ops = self.ops
        with nc.Block() as block:
            def replay(e, lst):
                for ent in lst:
                    if ent[0] == "branch":
                        _, flag_ap, reg, oA, pA, oB, pB = ent
                        e.reg_load(reg, flag_ap)
                        with e.If_eq(reg, 0):
                            replay(e, oA)
                            for k, d in pA:
                                e.sem_inc(sems[k], d)
                            e.nop()
                        with e.Else():
                            replay(e, oB)
                            for k, d in pB:
                                e.sem_inc(sems[k], d)
                            e.nop()
                        continue
                    waits, fn, inc = ent
                    for key, val in waits:
                        e.wait_ge(sems[key], val)
                    if fn is None:
                        continue
                    inst = fn(e)
                    if inc is not None:
                        inst.then_inc(sems[inc[0]], inc[1])

            def mk(name):
                def body(e):
                    replay(e, ops[name])
                return body
            block.sync(mk("sync"))
            block.scalar(mk("scalar"))
            block.vector(mk("vector"))
            block.gpsimd(mk("gpsimd"))
            block.tensor(mk("tensor"))
        self.ops = {e: [] for e in ENGS}


def own_qtiles(hf):
    js = []
    for m in range(4):
        js += ([4 * m, 4 * m + 3] if hf == 0 else [4 * m + 1, 4 * m + 2])
    return js


def nk_of(i):
    return 8 * (i // 2) + (4 if i % 2 == 0 else 8)


def interleave(A, B):
    a, b = len(A), len(B)
    if a == 0:
        for f in B:
            f()
        return
    done = 0
    for k, f in enumerate(A):
        f()
        upto = ((k + 1) * b) // a
        while done < upto:
            B[done]()
            done += 1
    while done < b:
        B[done]()
        done += 1


def build(debug=(), stop_after=None, moe="sparse"):
    nc = bass.Bass("TRN2", target_bir_lowering=False)

    def din(name, shape, dt=F32):
        return nc.dram_tensor(name, list(shape), dt, kind="ExternalInput").ap()

    xb = din("xb", [4096, 1024])
    xo = din("xo", [2048, 1024])
    w_in = din("w_in", [1024, 3080])
    wqs_d = din("wqs", [1024, 512])
    wks_d = din("wks", [1024, 512])
    cosk = din("cosk", [128, 4096])
    sink = din("sink", [128, 4096])
    cosq = din("cosq", [128, 2048])
    sinq = din("sinq", [128, 2048])
    maskd_d = din("maskd", [128, 2, 1024], BF16)
    maskf_d = din("maskf", [128, 2, 1024], BF16)
    identb_d = din("identb", [128, 128], BF16)
    identf_d = din("identf", [128, 128])
    tri_d = din("tri", [128, 128])
    onesf_d = din("onesf", [128, 128])
    g_attn = din("g_attn", [1, 1024])
    g_ffn = din("g_ffn", [1, 1024])
    g_fin = din("g_fin", [1, 1024])
    bfor = din("bfor", [1, 8])
    lam_d = din("lamv", [1, 256])
    csel_d = din("csel", [1, 8 * 33])
    dng = din("dng", [1, 128])
    w_out = din("w_out", [1024, 1024])
    rw_d = din("rw", [1024, 20])
    rb_d = din("rb", [1, 20])
    wg_d = din("wg", [16, 1024, 512])
    wu_d = din("wu", [16, 1024, 512])
    wd_d = din("wd", [16, 512, 1024])
    ebase_d = din("ebase", [128, 256])
    ustrict_d = din("ustrict", [128, 128], BF16)
    onesb_d = din("onesb", [128, 128], BF16)
    xs_d = nc.dram_tensor("xs_scratch", [NSLOT, 1024], BF16).ap()
    ys_d = nc.dram_tensor("ys_scratch", [NSLOT, 1024], F32).ap()
    out = nc.dram_tensor("out", [2048, 1024], F32, kind="ExternalOutput").ap()
    dbg = {}

    def dbg_out(name, shape, dt=F32):
        if name in debug:
            dbg[name] = nc.dram_tensor("dbg_" + name, list(shape), dt, kind="ExternalOutput").ap()
            return dbg[name]
        return None

    with ExitStack() as es:
        S = Sched(nc, es)

        uniq = [0]

        def sb(sc, name, shape, dt):
            uniq[0] += 1
            return sc.enter_context(nc.sbuf_tensor("s%d_%s" % (uniq[0], name), list(shape), dt))

        banks = [es.enter_context(nc.psum_tensor("bank%d" % i, [128, 512], F32)) for i in range(8)]
        Tb = [S.tile("bank%d" % i) for i in range(8)]
        Tdbg = S.tile("dbg")

        def dump(name, src_ap, reads):
            if name in dbg:
                S.dma("sync", dbg[name], src_ap, reads=reads, writes=[Tdbg], semtile=Tdbg)

        def bcast_load(dst, src, T, n):
            S.dma("sync", dst, src.broadcast_to([128, n]), writes=[T])

        bcreg = es.enter_context(nc.gpsimd.register("bcreg"))
        brregs = {e: es.enter_context(getattr(nc, e).register("br_" + e)) for e in ENGS}

        def set_bcreg():
            S.ops["gpsimd"].append(([], lambda e: e.reg_mov(bcreg, NSLOT - 1), None))

        identb = sb(es, "identb", [128, 128], BF16); Tidb = S.tile("identb")
        S.dma("sync", identb[:], identb_d, writes=[Tidb])
        gb_attn = sb(es, "gb_attn", [128, 1024], F32); Tgba = S.tile("gba")
        bcast_load(gb_attn[:], g_attn, Tgba, 1024)
        OT = sb(es, "OT", [128, 8, 2048], BF16)
        TOT = [S.tile("OT%d" % q) for q in range(16)]

        def make_sweep(sc, pfx, pT_banks):
            st = {}
            st["xt"] = [sb(sc, pfx + "xt%d" % i, [128, 1024], F32) for i in range(4)]
            st["Txt"] = [S.tile() for _ in range(4)]
            st["hb"] = [sb(sc, pfx + "hb%d" % i, [128, 1024], BF16) for i in range(2)]
            st["Thb"] = [S.tile() for _ in range(2)]
            st["hT"] = [sb(sc, pfx + "hT%d" % i, [128, 8, 512], BF16) for i in range(3)]
            st["ThT"] = [[S.tile() for _ in range(4)] for _ in range(3)]
            st["junk"] = sb(sc, pfx + "junk", [128, 1024], BF16)
            st["Tjunk"] = S.tile()
            st["ss"] = [sb(sc, pfx + "ss%d" % i, [128, 4], F32) for i in range(3)]
            st["Tss"] = [[S.tile() for _ in range(4)] for _ in range(3)]
            st["sq"] = [sb(sc, pfx + "sq%d" % i, [128, 4], F32) for i in range(3)]
            st["Tsq"] = [S.tile() for _ in range(3)]
            st["rs"] = [sb(sc, pfx + "rs%d" % i, [128, 4], F32) for i in range(3)]
            st["Trs"] = [S.tile() for _ in range(3)]
            st["pT"] = pT_banks
            return st

        def sweep_stage1(st, x_ap, blk, gb, Tgb, deferred=False):
            b2 = blk % 3
            for tt in range(4):
                n = blk * 4 + tt
                xt, Txt = st["xt"][n % 4], st["Txt"][n % 4]
                S.dma("sync", xt[:], x_ap[n * 128:(n + 1) * 128, :], writes=[Txt])
                S.op("scalar", lambda e, xt=xt, tt=tt: e.activation(out=st["junk"][:], in_=xt[:], func=AF.Square,
                                                                    accum_out=st["ss"][b2][:, tt:tt + 1]),
                     reads=[Txt], writes=[st["Tss"][b2][tt], st["Tjunk"]])
            S.op("scalar", lambda e: e.activation(out=st["sq"][b2][:], in_=st["ss"][b2][:], func=AF.Sqrt,
                                                  scale=1.0 / 1024, bias=EPS),
                 reads=st["Tss"][b2], writes=[st["Tsq"][b2]])
            S.op("vector", lambda e: e.reciprocal(out=st["rs"][b2][:], in_=st["sq"][b2][:]),
                 reads=[st["Tsq"][b2]], writes=[st["Trs"][b2]])
            def stt(tt):
                n = blk * 4 + tt
                xt, Txt = st["xt"][n % 4], st["Txt"][n % 4]
                hb, Thb = st["hb"][n % 2], st["Thb"][n % 2]
                S.op("vector", lambda e, xt=xt, hb=hb, tt=tt: e.scalar_tensor_tensor(
                    out=hb[:], in0=xt[:], scalar=st["rs"][b2][:, tt:tt + 1], in1=gb[:], op0=ALU.mult, op1=ALU.mult),
                    reads=[Txt, st["Trs"][b2], Tgb], writes=[Thb])

            def tr(tt):
                n = blk * 4 + tt
                hb, Thb = st["hb"][n % 2], st["Thb"][n % 2]
                bi = st["pT"][n % 2]
                pTv = banks[bi][:].bitcast(BF16)
                for c in range(8):
                    S.op("tensor", lambda e, c=c, hb=hb, pTv=pTv: e.transpose(
                        out=pTv[:, c * 128:(c + 1) * 128], in_=hb[:, c * 128:(c + 1) * 128], identity=identb[:]),
                        reads=[Thb, Tidb], writes=[Tb[bi]], signal=(c == 7))

            def ev(tt):
                n = blk * 4 + tt
                bi = st["pT"][n % 2]
                pTv = banks[bi][:].bitcast(BF16)
                S.op("vector", lambda e, tt=tt, pTv=pTv: e.tensor_copy(
                    out=st["hT"][b2][:, :, tt * 128:(tt + 1) * 128], in_=pTv.rearrange("p (c t) -> p c t", c=8)),
                    reads=[Tb[bi]], writes=[st["ThT"][b2][tt]])
            if not deferred:
                for f, a in ((stt, 0), (stt, 1), (tr, 0), (ev, 0), (stt, 2), (tr, 1), (ev, 1), (stt, 3), (tr, 2), (ev, 2), (tr, 3), (ev, 3)):
                    f(a)
                return []
            stt(0)
            return [lambda: (stt(1), tr(0), ev(0)), lambda: (stt(2), tr(1), ev(1)),
                    lambda: (stt(3), tr(2), ev(2)), lambda: (tr(3), ev(3))]

        def wload(dst, src_cols, T):
            S.dma("gpsimd", dst, src_cols.rearrange("(c p) n -> p c n", p=128), writes=[T])

        def mmgroup(out_ap, pairs, reads, writes):
            n = len(pairs)
            for k, (l, r) in enumerate(pairs):
                S.op("tensor", lambda e, l=l, r=r, k=k: e.matmul(out=out_ap, lhsT=l, rhs=r, start=(k == 0), stop=(k == n - 1)),
                     reads=reads, writes=writes, signal=(k == n - 1))

        def run_attention(units, PT, TPT, KT_of, QT_of, V_of, W, exp_emit, evac_emit, mask_emit, s_banks, o_banks,
                          OTs=None, TOTs=None, smr=None, Tsmr=None, identf=None, Tidf=None, Vsum_of=None):
            gctr = [0]
            dv = W - 1
            MO = dv if Vsum_of is not None else W
            deferred = []

            def st_list(ui, u):
                buf = ui % 2
                nk = nk_of(u["i"])
                L = []
                for p in range(nk // 2):
                    def f(p=p):
                        bi = s_banks[gctr[0] % len(s_banks)]
                        gctr[0] += 1
                        for j in range(2):
                            kb = 2 * p + j
                            kap, Tk = KT_of(u, kb)
                            qap, Tq = QT_of(u)
                            S.op("tensor", lambda e, kap=kap, qap=qap, j=j, bi=bi: e.matmul(
                                out=banks[bi][:, j * 256:(j + 1) * 256], lhsT=kap, rhs=qap, start=True, stop=True),
                                reads=[Tk, Tq], writes=[Tb[bi]], signal=(j == 1))
                        exp_emit(u, p, bi, PT[buf], TPT[buf][p])
                    L.append(f)
                L.append(lambda: mask_emit(u, PT[buf], TPT[buf]))
                return L

            def pv_list(ui, u):
                buf = ui % 2
                nk = nk_of(u["i"])
                ba = o_banks[(ui % 2) * 2]
                bb = o_banks[(ui % 2) * 2 + 1]
                o2 = ui % 2
                L = []
                for k0 in range(0, nk, 4):
                    def f(k0=k0):
                        for kb in range(k0, min(nk, k0 + 4)):
                            vap, Tv = V_of(u, kb)
                            S.op("tensor", lambda e, kb=kb, vap=vap: e.matmul(
                                out=banks[ba][0:MO, 0:256], lhsT=vap, rhs=PT[buf][:, kb, :], start=(kb == 0), stop=(kb == nk - 1)),
                                reads=[TPT[buf][kb // 2], Tv], writes=[Tb[ba]], signal=(kb == nk - 1))
                    L.append(f)
                if Vsum_of is not None:
                    for k0 in range(0, nk, 4):
                        def g(k0=k0):
                            for kb in range(k0, min(nk, k0 + 4)):
                                vap, Tv = Vsum_of(u, kb)
                                S.op("tensor", lambda e, kb=kb, vap=vap: e.matmul(
                                    out=banks[bb][0:1, 256:512], lhsT=vap, rhs=PT[buf][:, kb, :], start=(kb == 0), stop=(kb == nk - 1)),
                                    reads=[TPT[buf][kb // 2], Tv], writes=[Tb[bb]], signal=(kb == nk - 1))
                        L.append(g)

                def copy_out():
                    S.op("vector", lambda e: e.tensor_copy(out=OTs[o2][0:MO, :], in_=banks[ba][0:MO, 0:256]), reads=[Tb[ba]], writes=[TOTs[o2]])
                    if Vsum_of is not None:
                        S.op("vector", lambda e: e.tensor_copy(out=smr[o2][:], in_=banks[bb][0:1, 256:512]), reads=[Tb[bb]], writes=[Tsmr[o2]])

                def transpose_back():
                    for s in range(2):
                        last = (Vsum_of is None)
                        S.op("tensor", lambda e, s=s: e.transpose(out=banks[bb][:, s * W:s * W + MO], in_=OTs[o2][0:MO, s * 128:(s + 1) * 128],
                                                                  identity=identf[0:MO, 0:MO]),
                             reads=[TOTs[o2], Tidf], writes=[Tb[bb]], signal=(last and s == 1))
                        if Vsum_of is not None:
                            S.op("tensor", lambda e, s=s: e.transpose(out=banks[bb][:, s * W + dv:s * W + W], in_=smr[o2][0:1, s * 128:(s + 1) * 128],
                                                                      identity=identf[0:1, 0:1]),
                                 reads=[Tsmr[o2], Tidf], writes=[Tb[bb]], signal=(s == 1))
                    evac_emit(u, bb)
                L.append(copy_out)
                deferred.append(transpose_back)
                return L

            for ui in range(len(units) + 1):
                A = st_list(ui, units[ui]) if ui < len(units) else []
                B = pv_list(ui - 1, units[ui - 1]) if ui >= 1 else []
                if len(deferred) > (1 if ui >= 1 else 0):
                    B.insert(min(2, len(B)), deferred.pop(0))
                interleave(A, B)
            while deferred:
                deferred.pop(0)()

        with ExitStack() as pa:
            KT = sb(pa, "KTd", [128, 4, 4096], BF16)
            TKT = [[S.tile() for _ in range(8)] for _ in range(4)]
            QT = sb(pa, "QTd", [128, 4, 2048], BF16)
            TQT = [[S.tile() for _ in range(4)] for _ in range(4)]
            Vd = sb(pa, "Vd", [128, 32, 4, 130], BF16)
            TV = [S.tile() for _ in range(32)]
            Tvones = S.tile()
            S.op("vector", lambda e: e.memset(Vd[:, :, :, 128:130], 1.0), writes=TV)
            lamt = sb(pa, "lamt", [128, 256], F32); Tlam = S.tile()
            bcast_load(lamt[:], lam_d, Tlam, 256)
            lj = sb(pa, "lj", [128, 64], F32)
            ls = sb(pa, "ls", [128, 2], F32); Tls = S.tile()
            le = sb(pa, "le", [128, 2], F32); Tle = S.tile()
            neglam = sb(pa, "neglam", [128, 1], F32); Tnl = S.tile()
            for z in range(2):
                S.op("vector", lambda e, z=z: e.scalar_tensor_tensor(
                    out=lj[:], in0=lamt[:, z * 128:z * 128 + 64], scalar=1.0, in1=lamt[:, z * 128 + 64:z * 128 + 128],
                    op0=ALU.mult, op1=ALU.mult, accum_out=ls[:, z:z + 1]), reads=[Tlam, Tls], writes=[Tls])
            S.op("scalar", lambda e: e.activation(out=le[:], in_=ls[:], func=AF.Exp), reads=[Tls], writes=[Tle])
            S.op("vector", lambda e: e.tensor_tensor(out=neglam[:], in0=le[:, 1:2], in1=le[:, 0:1], op=ALU.subtract),
                 reads=[Tle], writes=[Tnl])
            S.op("vector", lambda e: e.tensor_scalar(out=neglam[:], in0=neglam[:], scalar1=-LAM_INIT, scalar2=None, op0=ALU.add),
                 reads=[Tnl], writes=[Tnl])
            gsc = sb(pa, "gsc", [128, 128], F32); Tgsc = S.tile()
            bcast_load(gsc[:], dng, Tgsc, 128)
            S.op("vector", lambda e: e.tensor_scalar(out=gsc[:], in0=gsc[:], scalar1=1.0 - LAM_INIT, scalar2=None, op0=ALU.mult),
                 reads=[Tgsc], writes=[Tgsc])
            Txs = S.tile("xs")

            with ExitStack() as sw:
                st = make_sweep(sw, "a", [0, 1])
                wA = sb(sw, "wA", [128, 8, 512], BF16); TwA = S.tile()
                wB = sb(sw, "wB", [128, 8, 512], BF16); TwB = S.tile()
                wC = sb(sw, "wC", [128, 8, 512], BF16); TwC = S.tile()
                wload(wA[:], w_in[:, 512:1024], TwA)
                wload(wB[:], wks_d, TwB)
                wload(wC[:], w_in[:, 1024:1536], TwC)
                ct = [sb(sw, "ct%d" % i, [128, 512], F32) for i in range(2)]; Tct = [S.tile() for _ in range(2)]
                sn = [sb(sw, "sn%d" % i, [128, 512], F32) for i in range(2)]; Tsn = [S.tile() for _ in range(2)]
                t1 = [sb(sw, "t1%d" % i, [128, 512], F32) for i in range(2)]; Tt1 = [S.tile() for _ in range(2)]
                t2 = [sb(sw, "t2%d" % i, [128, 512], F32) for i in range(2)]; Tt2 = [S.tile() for _ in range(2)]
                rctr = [0]

                def rope_proj(blk, hT, ThT, wq_, Tw_, ws_, Tws_, cos_d, sin_d, dstT, TdstT, slots=()):
                    b2 = blk % 2
                    S.dma("sync", ct[b2][:], cos_d[:, blk * 512:(blk + 1) * 512], writes=[Tct[b2]])
                    S.dma("sync", sn[b2][:], sin_d[:, blk * 512:(blk + 1) * 512], writes=[Tsn[b2]])
                    for h in range(4):
                        ba, bb = (2, 3) if h % 2 == 0 else (4, 5)
                        mmgroup(banks[ba][:], [(wq_[:, c, h * 128:(h + 1) * 128], hT[:, c, :]) for c in range(8)],
                                reads=list(ThT) + [Tw_], writes=[Tb[ba]])
                        mmgroup(banks[bb][:], [(ws_[:, c, h * 128:(h + 1) * 128], hT[:, c, :]) for c in range(8)],
                                reads=list(ThT) + [Tws_], writes=[Tb[bb]])
                        r = rctr[0] % 2
                        rctr[0] += 1
                        S.op("vector", lambda e, r=r, ba=ba: e.tensor_tensor(out=t1[r][:], in0=banks[ba][:], in1=ct[b2][:], op=ALU.mult),
                             reads=[Tb[ba], Tct[b2]], writes=[Tt1[r]])
                        S.op("vector", lambda e, r=r, bb=bb: e.tensor_tensor(out=t2[r][:], in0=banks[bb][:], in1=sn[b2][:], op=ALU.mult),
                             reads=[Tb[bb], Tsn[b2]], writes=[Tt2[r]])
                        S.op("gpsimd", lambda e, r=r, h=h: e.tensor_tensor(out=dstT[:, h, blk * 512:(blk + 1) * 512], in0=t1[r][:], in1=t2[r][:], op=ALU.add),
                             reads=[Tt1[r], Tt2[r]], writes=[TdstT[h][blk]])
                        if h < len(slots):
                            slots[h]()

                def kv_proj(blk, hT, ThT, slots=()):
                    rope_proj(blk, hT, ThT, wA, TwA, wB, TwB, cosk, sink, KT, TKT, slots)
                    for tt in range(4):
                        n = blk * 4 + tt
                        bv = 6 + (tt % 2)
                        mmgroup(banks[bv][:], [(hT[:, c, tt * 128:(tt + 1) * 128], wC[:, c, :]) for c in range(8)],
                                reads=[ThT[tt], TwC], writes=[Tb[bv]])
                        S.op("scalar", lambda e, n=n, bv=bv: e.activation(
                            out=Vd[:, n, :, 0:128], in_=banks[bv][:].rearrange("p (h d) -> p h d", h=4), func=AF.Copy),
                            reads=[Tb[bv]], writes=[TV[n]])

                sweep_stage1(st, xb, 0, gb_attn, Tgba)
                sweep_stage1(st, xb, 1, gb_attn, Tgba)
                for blk in range(8):
                    sl = sweep_stage1(st, xb, blk + 2, gb_attn, Tgba, deferred=True) if blk + 2 < 8 else []
                    kv_proj(blk, st["hT"][blk % 3], st["ThT"][blk % 3], sl)
                wload(wA[:], w_in[:, 0:512], TwA)
                wload(wB[:], wqs_d, TwB)
                sweep_stage1(st, xo, 0, gb_attn, Tgba)
                sweep_stage1(st, xo, 1, gb_attn, Tgba)
                for blk in range(4):
                    sl = sweep_stage1(st, xo, blk + 2, gb_attn, Tgba, deferred=True) if blk + 2 < 4 else []
                    rope_proj(blk, st["hT"][blk % 3], st["ThT"][blk % 3], wA, TwA, wB, TwB, cosq, sinq, QT, TQT, sl)
                S.barrier()
                if "KTd" in debug:
                    dbg_out("KTd", [128, 4, 4096], BF16); dump("KTd", KT[:], [])
                    dbg_out("QTd", [128, 4, 2048], BF16); dump("QTd", QT[:], [])
                    dbg_out("Vd", [128, 32, 4, 130], BF16); dump("Vd", Vd[:], [])
                    S.barrier()
                S.emit()
            if stop_after == "Aproj":
                S.barrier(); S.emit()
                return nc

            with ExitStack() as at:
                maskd = sb(at, "maskd", [128, 2, 1024], BF16); Tmd = S.tile()
                S.dma("sync", maskd[:], maskd_d, writes=[Tmd])
                if moe == "sparse":
                    zer = sb(at, "zer", [128, 2048], BF16); Tzer = S.tile()
                    S.op("gpsimd", lambda e: e.memset(zer[:], 0.0), writes=[Tzer])
                    for n in range(NSLOT // 256):
                        S.dma("gpsimd", xs_d[n * 256:(n + 1) * 256, :].rearrange("(p r) d -> p (r d)", r=2), zer[:], reads=[Tzer], writes=[Txs], semtile=Tzer)
                PT = [sb(at, "PT%d" % i, [128, 32, 256], BF16) for i in range(2)]
                TPT = [[S.tile() for _ in range(16)] for _ in range(2)]
                oc = sb(at, "oc", [128, 16, 4, 128], F32); Toc = [[S.tile() for _ in range(4)] for _ in range(16)]
                ssq = sb(at, "ssq", [128, 64], F32); Tssq = S.tile()
                A1 = [sb(at, "A1%d" % i, [128, 128], F32) for i in range(2)]; TA1 = [S.tile() for _ in range(2)]
                rr = sb(at, "rr", [128, 4], F32); Trr = [S.tile() for _ in range(4)]
                sjunk = sb(at, "sjunk", [128, 128], F32); Tsjunk = S.tile()
                units = [dict(h=h, i=i, z=z) for h in range(4) for i in range(8) for z in range(2)]

                def KT_of(u, kb):
                    r0 = 64 * u["z"]
                    return KT[r0:r0 + 64, u["h"], kb * 128:(kb + 1) * 128], TKT[u["h"]][kb // 4]

                def QT_of(u):
                    r0 = 64 * u["z"]
                    return QT[r0:r0 + 64, u["h"], u["i"] * 256:(u["i"] + 1) * 256], TQT[u["h"]][u["i"] // 2]

                def V_of(u, kb):
                    return Vd[:, kb, u["h"], 0:128], TV[kb]

                def exp_emit(u, p, bi, PTb, Tp):
                    S.op("scalar", lambda e: e.activation(out=PTb[:, 2 * p:2 * p + 2, :].rearrange("p a b -> p (a b)"),
                                                          in_=banks[bi][:], func=AF.Exp),
                         reads=[Tb[bi]], writes=[Tp])

                def mask_emit(u, PTb, TPb):
                    i = u["i"]
                    lo = nk_of(i) - 4
                    S.op("vector", lambda e: e.tensor_tensor(out=PTb[:, lo:lo + 4, :], in0=PTb[:, lo:lo + 4, :],
                                                             in1=maskd[:, i % 2, :].rearrange("p (a b) -> p a b", a=4), op=ALU.min),
                         reads=[Tmd, TPb[lo // 2], TPb[lo // 2 + 1]], writes=[TPb[lo // 2], TPb[lo // 2 + 1]])

                def evac_emit(u, ob):
                    h, i, z = u["h"], u["i"], u["z"]
                    for s in range(2):
                        qb = 2 * i + s
                        o_ap = banks[ob][:, s * 129:s * 129 + 128]
                        sm_ap = banks[ob][:, s * 129 + 128:s * 129 + 129]
                        ri = 2 * z + s
                        S.op("vector", lambda e, ri=ri, sm_ap=sm_ap: e.reciprocal(out=rr[:, ri:ri + 1], in_=sm_ap),
                             reads=[Tb[ob]], writes=[Trr[ri]])
                        if z == 0:
                            S.op("vector", lambda e, ri=ri, o_ap=o_ap, s=s: e.tensor_scalar(
                                out=A1[s][:], in0=o_ap, scalar1=rr[:, ri:ri + 1], scalar2=None, op0=ALU.mult),
                                reads=[Tb[ob], Trr[ri]], writes=[TA1[s]])
                        else:
                            S.op("vector", lambda e, ri=ri: e.tensor_tensor(out=rr[:, ri:ri + 1], in0=rr[:, ri:ri + 1], in1=neglam[:], op=ALU.mult),
                                 reads=[Trr[ri], Tnl], writes=[Trr[ri]])
                            S.op("vector", lambda e, ri=ri, o_ap=o_ap, s=s, qb=qb: e.scalar_tensor_tensor(
                                out=oc[:, qb, h, :], in0=o_ap, scalar=rr[:, ri:ri + 1], in1=A1[s][:], op0=ALU.mult, op1=ALU.add),
                                reads=[Tb[ob], Trr[ri], TA1[s]], writes=[Toc[qb][h]])
                            S.op("vector", lambda e, qb=qb: e.scalar_tensor_tensor(
                                out=sjunk[:], in0=oc[:, qb, h, :], scalar=1.0, in1=oc[:, qb, h, :], op0=ALU.mult, op1=ALU.mult,
                                accum_out=ssq[:, qb * 4 + h:qb * 4 + h + 1]),
                                reads=[Toc[qb][h], Tssq], writes=[Tssq, Tsjunk])

                OTs = [sb(at, "OTs%d" % i, [128, 256], F32) for i in range(2)]; TOTs = [S.tile() for _ in range(2)]
                smr = [sb(at, "smr%d" % i, [1, 256], F32) for i in range(2)]; Tsmr = [S.tile() for _ in range(2)]
                identf_a = sb(at, "identf_a", [128, 128], F32); Tidf_a = S.tile()
                S.dma("sync", identf_a[:], identf_d, writes=[Tidf_a])

                def Vsum_of(u, kb):
                    return Vd[:, kb, u["h"], 128:129], TV[kb]
                run_attention(units, PT, TPT, KT_of, QT_of, V_of, 129, exp_emit, evac_emit, mask_emit,
                              s_banks=[0, 1, 2, 3], o_banks=[4, 5, 6, 7], OTs=OTs, TOTs=TOTs, smr=smr, Tsmr=Tsmr,
                              identf=identf_a, Tidf=Tidf_a, Vsum_of=Vsum_of)
                S.op("scalar", lambda e: e.activation(out=ssq[:], in_=ssq[:], func=AF.Sqrt, scale=1.0 / 128, bias=EPS),
                     reads=[Tssq], writes=[Tssq])
                S.op("vector", lambda e: e.reciprocal(out=ssq[:], in_=ssq[:]), reads=[Tssq], writes=[Tssq])
                Otok = [sb(at, "Otok%d" % i, [128, 512], BF16) for i in range(2)]; TOtok = [S.tile() for _ in range(2)]
                for qb in range(16):
                    o2 = qb % 2
                    for h in range(4):
                        S.op("vector", lambda e, qb=qb, h=h, o2=o2: e.scalar_tensor_tensor(
                            out=Otok[o2][:, h * 128:(h + 1) * 128], in0=oc[:, qb, h, :], scalar=ssq[:, qb * 4 + h:qb * 4 + h + 1],
                            in1=gsc[:], op0=ALU.mult, op1=ALU.mult),
                            reads=[Toc[qb][h], Tssq, Tgsc], writes=[TOtok[o2]])
                    bi = o2
                    pTv = banks[bi][:].bitcast(BF16)
                    for c in range(4):
                        S.op("tensor", lambda e, c=c, o2=o2, pTv=pTv: e.transpose(
                            out=pTv[:, c * 128:(c + 1) * 128], in_=Otok[o2][:, c * 128:(c + 1) * 128], identity=identb[:]),
                            reads=[TOtok[o2], Tidb], writes=[Tb[bi]], signal=(c == 3))
                    S.op("vector", lambda e, qb=qb, pTv=pTv: e.tensor_copy(
                        out=OT[:, 0:4, qb * 128:(qb + 1) * 128], in_=pTv[:, 0:512].rearrange("p (c t) -> p c t", c=4)),
                        reads=[Tb[bi]], writes=[TOT[qb]])
                S.barrier()
                if "OTa" in debug:
                    dbg_out("OTa", [128, 8, 2048], BF16); dump("OTa", OT[:], []); S.barrier()
                S.emit()
        if stop_after == "A":
            return nc

        with ExitStack() as pb:
            KT = sb(pb, "KTf", [128, 4, 4096], BF16)
            TKT = [[S.tile() for _ in range(8)] for _ in range(4)]
            QT = sb(pb, "QTf", [128, 4, 2048], BF16)
            TQT = [[S.tile() for _ in range(4)] for _ in range(4)]
            Vf = sb(pb, "Vf", [128, 32, 8, 66], BF16)
            TV = [S.tile() for _ in range(32)]
            S.op("vector", lambda e: e.memset(Vf[:, :, :, 64:66], 1.0), writes=TV)
            zt = sb(pb, "zt", [128, 32, 8], F32); Tzt = [S.tile() for _ in range(32)]
            Fpos = sb(pb, "Fpos", [128, 32, 8], F32); TF = [S.tile() for _ in range(32)]
            Cpos = sb(pb, "Cpos", [128, 33, 8], F32); TC = [S.tile() for _ in range(33)]
            maskf = sb(pb, "maskf", [128, 2, 1024], BF16); Tmf = S.tile()
            S.dma("sync", maskf[:], maskf_d, writes=[Tmf])
            bfb = sb(pb, "bfb", [128, 8], F32); Tbfb = S.tile()
            bcast_load(bfb[:], bfor, Tbfb, 8)
            csel = sb(pb, "csel", [128, 8, 33], F32); Tcsel = S.tile()
            bcast_load(csel[:].rearrange("p a b -> p (a b)"), csel_d, Tcsel, 8 * 33)
            ctmp = sb(pb, "ctmp", [128, 8, 33], F32); Tctmp = S.tile()
            cq = sb(pb, "cq", [128, 8, 8], F32); Tcq = S.tile()
            tri = sb(pb, "tri", [128, 128], F32); Ttri = S.tile()
            S.dma("sync", tri[:], tri_d, writes=[Ttri])
            onesf = sb(pb, "onesf", [128, 128], F32); Tones = S.tile()
            S.dma("sync", onesf[:], onesf_d, writes=[Tones])

            with ExitStack() as sw:
                st = make_sweep(sw, "b", [0, 1])
                wA = sb(sw, "wA", [128, 8, 512], BF16); TwA = S.tile()
                wB = sb(sw, "wB", [128, 8, 512], BF16); TwB = S.tile()
                wF = sb(sw, "wF", [128, 8, 8], BF16); TwF = S.tile()
                wload(wA[:], w_in[:, 2048:2560], TwA)
                wload(wB[:], w_in[:, 2560:3072], TwB)
                wload(wF[:], w_in[:, 3072:3080], TwF)
                kctr = [0]

                def plain_proj(blk, hT, ThT, w_, Tw_, dstT, TdstT, scale, slots=()):
                    for hp in range(4):
                        bk = 2 + (kctr[0] % 3)
                        kctr[0] += 1
                        mmgroup(banks[bk][:], [(w_[:, c, hp * 128:(hp + 1) * 128], hT[:, c, :]) for c in range(8)],
                                reads=list(ThT) + [Tw_], writes=[Tb[bk]])
                        S.op("scalar", lambda e, bk=bk, hp=hp: e.activation(
                            out=dstT[:, hp, blk * 512:(blk + 1) * 512], in_=banks[bk][:], func=AF.Copy, scale=scale),
                            reads=[Tb[bk]], writes=[TdstT[hp][blk]])
                        if hp < len(slots):
                            slots[hp]()

                def kvf_proj(blk, hT, ThT, slots=()):
                    plain_proj(blk, hT, ThT, wA, TwA, KT, TKT, 1.0, slots)
                    for tt in range(4):
                        n = blk * 4 + tt
                        bv = 6 + (tt % 2)
                        mmgroup(banks[bv][:], [(hT[:, c, tt * 128:(tt + 1) * 128], wB[:, c, :]) for c in range(8)],
                                reads=[ThT[tt], TwB], writes=[Tb[bv]])
                        S.op("vector", lambda e, n=n, bv=bv: e.tensor_copy(
                            out=Vf[:, n, :, 0:64], in_=banks[bv][:].rearrange("p (h d) -> p h d", h=8)),
                            reads=[Tb[bv]], writes=[TV[n]])
                        mmgroup(banks[5][:, 0:8], [(hT[:, c, tt * 128:(tt + 1) * 128], wF[:, c, :]) for c in range(8)],
                                reads=[ThT[tt], TwF], writes=[Tb[5]])
                        S.op("vector", lambda e, n=n: e.tensor_tensor(out=zt[:, n, :], in0=banks[5][:, 0:8], in1=bfb[:], op=ALU.add),
                             reads=[Tb[5], Tbfb], writes=[Tzt[n]])

                sweep_stage1(st, xb, 0, gb_attn, Tgba)
                sweep_stage1(st, xb, 1, gb_attn, Tgba)
                for blk in range(8):
                    sl = sweep_stage1(st, xb, blk + 2, gb_attn, Tgba, deferred=True) if blk + 2 < 8 else []
                    kvf_proj(blk, st["hT"][blk % 3], st["ThT"][blk % 3], sl)
                wload(wA[:], w_in[:, 1536:2048], TwA)
                sweep_stage1(st, xo, 0, gb_attn, Tgba)
                sweep_stage1(st, xo, 1, gb_attn, Tgba)
                for blk in range(4):
                    sl = sweep_stage1(st, xo, blk + 2, gb_attn, Tgba, deferred=True) if blk + 2 < 4 else []
                    plain_proj(blk, st["hT"][blk % 3], st["ThT"][blk % 3], wA, TwA, QT, TQT, 0.125, sl)
                ztf = zt[:].rearrange("p a b -> p (a b)")
                S.op("scalar", lambda e: e.activation(out=ztf, in_=ztf, func=AF.Exp, scale=-1.0), reads=Tzt, writes=Tzt)
                S.op("scalar", lambda e: e.activation(out=ztf, in_=ztf, func=AF.Ln, bias=1.0), reads=Tzt, writes=Tzt)
                S.op("vector", lambda e: e.memset(Cpos[:, 0, :], 0.0), writes=[TC[0]])
                for n in range(32):
                    bc = 2 + (n % 2)
                    S.op("tensor", lambda e, n=n, bc=bc: e.matmul(out=banks[bc][:, 0:8], lhsT=tri[:], rhs=zt[:, n, :], start=True, stop=True),
                         reads=[Ttri, Tzt[n]], writes=[Tb[bc]], signal=False)
                    S.op("tensor", lambda e, n=n, bc=bc: e.matmul(out=banks[bc][:, 8:16], lhsT=onesf[:], rhs=zt[:, n, :], start=True, stop=True),
                         reads=[Tones, Tzt[n]], writes=[Tb[bc]], signal=True)
                    S.op("vector", lambda e, n=n, bc=bc: e.tensor_tensor(out=Fpos[:, n, :], in0=banks[bc][:, 0:8], in1=Cpos[:, n, :], op=ALU.add),
                         reads=[Tb[bc], TC[n]], writes=[TF[n]])
                    S.op("vector", lambda e, n=n, bc=bc: e.tensor_tensor(out=Cpos[:, n + 1, :], in0=banks[bc][:, 8:16], in1=Cpos[:, n, :], op=ALU.add),
                         reads=[Tb[bc], TC[n]], writes=[TC[n + 1]])
                for i in range(8):
                    S.op("vector", lambda e, i=i: e.tensor_tensor(out=ctmp[:], in0=Cpos[:].rearrange("p n h -> p h n"),
                                                                  in1=csel[:, i, :].unsqueeze(1).broadcast_to([128, 8, 33]), op=ALU.mult),
                         reads=TC + [Tcsel, Tctmp], writes=[Tctmp])
                    S.op("vector", lambda e, i=i: e.tensor_reduce(out=cq[:, i, :], in_=ctmp[:], axis=AX.X, op=ALU.add),
                         reads=[Tctmp], writes=[Tcq])
                S.barrier()
                if "Fpos" in debug:
                    dbg_out("Fpos", [128, 32, 8]); dump("Fpos", Fpos[:], []); S.barrier()
                S.emit()

            with ExitStack() as at:
                PT = [sb(at, "PT%d" % i, [128, 32, 256], BF16) for i in range(2)]
                TPT = [[S.tile() for _ in range(16)] for _ in range(2)]
                Otf = sb(at, "Otf", [128, 16, 512], BF16); TOtf = [S.tile() for _ in range(16)]
                rr = sb(at, "rrf", [128, 2], F32); Trr = [S.tile() for _ in range(2)]
                biasb = [sb(at, "biasb%d" % i, [128, 32], F32) for i in range(2)]; Tbias = [S.tile() for _ in range(2)]
                units = [dict(hp=hp, hh=hh, i=i, head=2 * hp + hh) for hp in range(4) for hh in range(2) for i in range(8)]
                for ui, u in enumerate(units):
                    u["ui"] = ui

                def KT_of(u, kb):
                    r0 = 64 * u["hh"]
                    return KT[r0:r0 + 64, u["hp"], kb * 128:(kb + 1) * 128], TKT[u["hp"]][kb // 4]

                def QT_of(u):
                    r0 = 64 * u["hh"]
                    return QT[r0:r0 + 64, u["hp"], u["i"] * 256:(u["i"] + 1) * 256], TQT[u["hp"]][u["i"] // 2]

                ebb = [sb(at, "ebb%d" % i, [128, 32], F32) for i in range(2)]; Teb = [S.tile() for _ in range(2)]
                Vp = [sb(at, "Vp%d" % i, [128, 32, 65], BF16) for i in range(2)]; TVp = [S.tile() for _ in range(2)]

                def V_of(u, kb):
                    return Vp[u["ui"] % 2][:, kb, :], TVp[u["ui"] % 2]

                def exp_emit(u, p, bi, PTb, Tp):
                    b2 = u["ui"] % 2
                    hd = u["head"]
                    nk = nk_of(u["i"])
                    if p == 0:
                        S.op("vector", lambda e: e.tensor_scalar(out=biasb[b2][:, 0:nk], in0=Fpos[:, 0:nk, hd], scalar1=cq[:, u["i"], hd:hd + 1],
                                                                 scalar2=70.0, op0=ALU.subtract, op1=ALU.min),
                             reads=TF[0:nk] + [Tcq], writes=[Tbias[b2]])
                        S.op("scalar", lambda e: e.activation(out=ebb[b2][:, 0:nk], in_=biasb[b2][:, 0:nk], func=AF.Exp),
                             reads=[Tbias[b2]], writes=[Teb[b2]])
                        S.op("vector", lambda e: e.tensor_tensor(out=Vp[b2][:, 0:nk, :], in0=Vf[:, 0:nk, hd, 0:65],
                                                                 in1=ebb[b2][:, 0:nk].unsqueeze(2).broadcast_to([128, nk, 65]), op=ALU.mult),
                             reads=TV[0:nk] + [Teb[b2]], writes=[TVp[b2]])
                    S.op("scalar", lambda e: e.activation(out=PTb[:, 2 * p:2 * p + 2, :].rearrange("p a b -> p (a b)"),
                                                          in_=banks[bi][:], func=AF.Exp),
                         reads=[Tb[bi]], writes=[Tp])

                def mask_emit(u, PTb, TPb):
                    i = u["i"]
                    lo = nk_of(i) - 4
                    S.op("vector", lambda e: e.tensor_tensor(out=PTb[:, lo:lo + 4, :], in0=PTb[:, lo:lo + 4, :],
                                                             in1=maskf[:, i % 2, :].rearrange("p (a b) -> p a b", a=4), op=ALU.min),
                         reads=[Tmf, TPb[lo // 2], TPb[lo // 2 + 1]], writes=[TPb[lo // 2], TPb[lo // 2 + 1]])

                def evac_emit(u, ob):
                    hd, i = u["head"], u["i"]
                    for s in range(2):
                        qb = 2 * i + s
                        S.op("vector", lambda e, s=s: e.reciprocal(out=rr[:, s:s + 1], in_=banks[ob][:, s * 65 + 64:s * 65 + 65]),
                             reads=[Tb[ob]], writes=[Trr[s]])
                        S.op("vector", lambda e, s=s, qb=qb: e.tensor_scalar(
                            out=Otf[:, qb, hd * 64:(hd + 1) * 64], in0=banks[ob][:, s * 65:s * 65 + 64], scalar1=rr[:, s:s + 1],
                            scalar2=None, op0=ALU.mult),
                            reads=[Tb[ob], Trr[s]], writes=[TOtf[qb]])

                OTs = [sb(at, "OTsf%d" % i, [128, 256], F32) for i in range(2)]; TOTs = [S.tile() for _ in range(2)]
                identf_b = sb(at, "identf_b", [128, 128], F32); Tidf_b = S.tile()
                S.dma("sync", identf_b[:], identf_d, writes=[Tidf_b])
                run_attention(units, PT, TPT, KT_of, QT_of, V_of, 65, exp_emit, evac_emit, mask_emit,
                              s_banks=[0, 1, 2, 3], o_banks=[4, 5, 6, 7], OTs=OTs, TOTs=TOTs, identf=identf_b, Tidf=Tidf_b)
                for qb in range(16):
                    bi = qb % 2
                    pTv = banks[bi][:].bitcast(BF16)
                    for c in range(4):
                        S.op("tensor", lambda e, c=c, qb=qb, pTv=pTv: e.transpose(
                            out=pTv[:, c * 128:(c + 1) * 128], in_=Otf[:, qb, c * 128:(c + 1) * 128], identity=identb[:]),
                            reads=[TOtf[qb], Tidb], writes=[Tb[bi]], signal=(c == 3))
                    S.op("vector", lambda e, qb=qb, pTv=pTv: e.tensor_copy(
                        out=OT[:, 4:8, qb * 128:(qb + 1) * 128], in_=pTv[:, 0:512].rearrange("p (c t) -> p c t", c=4)),
                        reads=[Tb[bi]], writes=[TOT[qb]])
                S.barrier()
                if "OTb" in debug:
                    dbg_out("OTb", [128, 8, 2048], BF16); dump("OTb", OT[:], []); S.barrier()
                S.emit()
        if stop_after == "B":
            return nc

        with ExitStack() as pc:
            x2 = sb(pc, "x2", [128, 16, 1024], F32); Tx2 = [[S.tile() for _ in range(2)] for _ in range(16)]
            hmT = OT
            ThmT = TOT
            ovf = sb(pc, "ovf", [128, 1], I32); Tovf = S.tile()
            w12 = sb(pc, "w12", [128, 2, 16], F32); Tw12 = S.tile()
            pos = sb(pc, "pos", [128, 2, 16], I32); Tpos = S.tile()
            comb = sb(pc, "comb", [128, 16, 16], F32); Tcomb = S.tile()
            junk = sb(pc, "junkc", [128, 1024], BF16); Tjunkc = S.tile()
            ssc = sb(pc, "ssc", [128, 16], F32); Tssc = [S.tile() for _ in range(16)]; Tsscall = S.tile()
            with ExitStack() as c1:
                wo = sb(c1, "wo", [128, 8, 1024], BF16); Two = S.tile()
                wload(wo[:], w_out, Two)
                xt = [sb(c1, "xc%d" % i, [128, 1024], F32) for i in range(2)]; Txt = [S.tile() for _ in range(2)]
                rw32 = sb(c1, "rw32", [128, 8, 20], F32); Trw = S.tile()
                S.dma("sync", rw32[:], rw_d.rearrange("(c p) n -> p c n", p=128), writes=[Trw])
                rbb = sb(c1, "rbb", [128, 20], F32); Trbb = S.tile()
                bcast_load(rbb[:], rb_d, Trbb, 20)
                gbf = sb(c1, "gbf", [128, 1024], F32); Tgbf = S.tile()
                bcast_load(gbf[:], g_ffn, Tgbf, 1024)
                identf = sb(c1, "identf", [128, 128], F32); Tidf = S.tile()
                S.dma("sync", identf[:], identf_d, writes=[Tidf])
                hm32 = [sb(c1, "hm32%d" % i, [128, 1024], F32) for i in range(2)]; Thm32 = [S.tile() for _ in range(2)]
                hmT32 = [sb(c1, "hmT32%d" % i, [128, 8, 128], F32) for i in range(2)]; ThmT32 = [S.tile() for _ in range(2)]
                Lall = sb(c1, "Lall", [128, 16, 20], F32); TL = [S.tile() for _ in range(16)]
                if moe == "sparse":
                    hmb = sb(c1, "hmb", [128, 16, 1024], BF16); Thmb = [S.tile() for _ in range(16)]
                for t in range(16):
                    S.dma("sync", xt[t % 2][:], xo[t * 128:(t + 1) * 128, :], writes=[Txt[t % 2]])
                    for hf in range(2):
                        mmgroup(banks[hf][:], [(OT[:, c, t * 128:(t + 1) * 128], wo[:, c, hf * 512:(hf + 1) * 512]) for c in range(8)],
                                reads=[TOT[t], Two], writes=[Tb[hf]])
                        S.op("vector", lambda e, t=t, hf=hf: e.tensor_tensor(
                            out=x2[:, t, hf * 512:(hf + 1) * 512], in0=banks[hf][:], in1=xt[t % 2][:, hf * 512:(hf + 1) * 512], op=ALU.add),
                            reads=[Tb[hf], Txt[t % 2]], writes=[Tx2[t][hf]])
                    S.op("scalar", lambda e, t=t: e.activation(out=junk[:], in_=x2[:, t, :], func=AF.Square, accum_out=ssc[:, t:t + 1]),
                         reads=Tx2[t], writes=[Tssc[t], Tjunkc])
                if "x2" in debug:
                    dbg_out("x2", [2048, 1024])
                    for t in range(16):
                        dump("x2", x2[:, t, :], Tx2[t]) if False else S.dma("sync", dbg["x2"][t * 128:(t + 1) * 128, :], x2[:, t, :], reads=Tx2[t], writes=[Tdbg], semtile=Tdbg)
                if stop_after == "C0":
                    S.barrier(); S.emit()
                    return nc
                S.op("scalar", lambda e: e.activation(out=ssc[:], in_=ssc[:], func=AF.Sqrt, scale=1.0 / 1024, bias=EPS),
                     reads=Tssc, writes=[Tsscall])
                S.op("vector", lambda e: e.reciprocal(out=ssc[:], in_=ssc[:]), reads=[Tsscall], writes=[Tsscall])
                for t in range(16):
                    t2 = t % 2
                    S.op("vector", lambda e, t=t, t2=t2: e.scalar_tensor_tensor(
                        out=hm32[t2][:], in0=x2[:, t, :], scalar=ssc[:, t:t + 1], in1=gbf[:], op0=ALU.mult, op1=ALU.mult),
                        reads=Tx2[t] + [Tsscall, Tgbf], writes=[Thm32[t2]])
                    ba, bb = (2, 3) if t2 == 0 else (4, 5)
                    for c in range(8):
                        bk = ba if c < 4 else bb
                        S.op("tensor", lambda e, c=c, t2=t2, bk=bk: e.transpose(
                            out=banks[bk][:, (c % 4) * 128:(c % 4 + 1) * 128], in_=hm32[t2][:, c * 128:(c + 1) * 128], identity=identf[:]),
                            reads=[Thm32[t2], Tidf], writes=[Tb[bk]], signal=(c % 4 == 3))
                    import os
                    CUT = int(os.environ.get("C1CUT", "9"))
                    if CUT < 2:
                        continue
                    for k, bk in enumerate((ba, bb)):
                        src = banks[bk][:].rearrange("p (c t) -> p c t", c=4)
                        S.op("scalar", lambda e, k=k, t2=t2, src=src: e.activation(out=hmT32[t2][:, 4 * k:4 * k + 4, :], in_=src, func=AF.Copy),
                             reads=[Tb[bk]], writes=[ThmT32[t2]])
                        S.op("vector", lambda e, k=k, t=t, t2=t2: e.tensor_copy(out=hmT[:, 4 * k:4 * k + 4, t * 128:(t + 1) * 128], in_=hmT32[t2][:, 4 * k:4 * k + 4, :]),
                             reads=[ThmT32[t2]], writes=[ThmT[t]])
                    if moe == "sparse":
                        S.op("gpsimd", lambda e, t=t, t2=t2: e.tensor_copy(out=hmb[:, t, :], in_=hm32[t2][:]), reads=[Thm32[t2]], writes=[Thmb[t]])
                    if CUT < 3:
                        continue
                    br = 6 + t2
                    mmgroup(banks[br][:, 0:20], [(hmT32[t2][:, c, :], rw32[:, c, :]) for c in range(8)],
                            reads=[ThmT32[t2], Trw], writes=[Tb[br]])
                    S.op("vector", lambda e, t=t, br=br: e.tensor_tensor(out=Lall[:, t, :], in0=banks[br][:, 0:20], in1=rbb[:], op=ALU.add),
                         reads=[Tb[br], Trbb], writes=[TL[t]])
                if stop_after == "C1a":
                    if "Lall" in debug:
                        dbg_out("Lall", [128, 320])
                        S.dma("sync", dbg["Lall"], Lall[:].rearrange("p a b -> p (a b)"), reads=[], writes=[Tdbg], semtile=Tdbg)
                    S.barrier(); S.emit()
                    return nc
                TR = S.tile()

                def rt(name, shape):
                    return sb(c1, "rt_" + name, shape, F32)
                gmax = rt("gmax", [128, 16]); gm = rt("gm", [128, 16, 4]); gd = rt("gd", [128, 16, 4])
                gsum = rt("gsum", [128, 16]); gw = rt("gw", [128, 16]); pen = rt("pen", [128, 16, 4])
                EL = rt("EL", [128, 16, 16]); EL2 = rt("EL2", [128, 16, 16]); m1 = rt("m1", [128, 16]); m2 = rt("m2", [128, 16])
                oh1 = rt("oh1", [128, 16, 16]); oh2 = rt("oh2", [128, 16, 16]); dd = rt("dd", [128, 16]); w1 = rt("w1", [128, 16]); w2 = rt("w2", [128, 16])
                LG = Lall[:, :, 0:4]
                LE4 = Lall[:, :, 4:20].rearrange("p t (g e) -> p t g e", g=4)
                EL4 = EL[:].rearrange("p t (g e) -> p t g e", g=4)

                def vop(fn, first=False):
                    S.op("vector", fn, reads=(TL + [TR]) if first else [TR], writes=[TR])

                def bc3(a, n):
                    return a[:].unsqueeze(2).broadcast_to([128, 16, n])
                vop(lambda e: e.tensor_reduce(out=gmax[:], in_=LG, axis=AX.X, op=ALU.max), first=True)
                vop(lambda e: e.tensor_tensor(out=gm[:], in0=LG, in1=bc3(gmax, 4), op=ALU.is_equal))
                vop(lambda e: e.tensor_tensor(out=gd[:], in0=LG, in1=bc3(gmax, 4), op=ALU.subtract))
                S.op("scalar", lambda e: e.activation(out=gd[:], in_=gd[:], func=AF.Exp), reads=[TR], writes=[TR])
                vop(lambda e: e.tensor_reduce(out=gsum[:], in_=gd[:], axis=AX.X, op=ALU.add))
                vop(lambda e: e.reciprocal(out=gw[:], in_=gsum[:]))
                vop(lambda e: e.tensor_scalar(out=pen[:], in0=gm[:], scalar1=1.0, scalar2=1e30, op0=ALU.subtract, op1=ALU.mult))
                vop(lambda e: e.tensor_tensor(out=EL4, in0=LE4, in1=gm[:].unsqueeze(3).broadcast_to([128, 16, 4, 4]), op=ALU.mult))
                vop(lambda e: e.tensor_tensor(out=EL4, in0=EL4, in1=pen[:].unsqueeze(3).broadcast_to([128, 16, 4, 4]), op=ALU.add))
                vop(lambda e: e.tensor_reduce(out=m1[:], in_=EL[:], axis=AX.X, op=ALU.max))
                vop(lambda e: e.tensor_tensor(out=oh1[:], in0=EL[:], in1=bc3(m1, 16), op=ALU.is_equal))
                vop(lambda e: e.scalar_tensor_tensor(out=EL2[:], in0=oh1[:], scalar=-1e30, in1=EL[:], op0=ALU.mult, op1=ALU.add))
                vop(lambda e: e.tensor_reduce(out=m2[:], in_=EL2[:], axis=AX.X, op=ALU.max))
                vop(lambda e: e.tensor_tensor(out=oh2[:], in0=EL2[:], in1=bc3(m2, 16), op=ALU.is_equal))
                vop(lambda e: e.tensor_tensor(out=dd[:], in0=m2[:], in1=m1[:], op=ALU.subtract))
                S.op("scalar", lambda e: e.activation(out=dd[:], in_=dd[:], func=AF.Exp), reads=[TR], writes=[TR])
                vop(lambda e: e.tensor_scalar(out=w1[:], in0=dd[:], scalar1=1.0, scalar2=None, op0=ALU.add))
                vop(lambda e: e.reciprocal(out=w1[:], in_=w1[:]))
                vop(lambda e: e.tensor_tensor(out=w1[:], in0=w1[:], in1=gw[:], op=ALU.mult))
                vop(lambda e: e.tensor_tensor(out=w2[:], in0=dd[:], in1=w1[:], op=ALU.mult))
                if moe == "sparse":
                    Mb = sb(c1, "Mb", [128, 16, 16], BF16)
                    ustrict = sb(c1, "ustrict", [128, 128], BF16); Tus = S.tile()
                    S.dma("sync", ustrict[:], ustrict_d, writes=[Tus])
                    onesb = sb(c1, "onesb", [128, 128], BF16); Tob_ = S.tile()
                    S.dma("sync", onesb[:], onesb_d, writes=[Tob_])
                    ebase = sb(c1, "ebase", [128, 16, 16], F32); Teb = S.tile()
                    S.dma("sync", ebase[:].rearrange("p a b -> p (a b)"), ebase_d, writes=[Teb])
                    slotf = rt("slotf", [128, 16, 16]); okf = rt("okf", [128, 16, 16]); posf = rt("posf", [128, 2, 16])
                    vop(lambda e: e.tensor_tensor(out=Mb[:], in0=oh1[:], in1=oh2[:], op=ALU.add))
                    for t in range(16):
                        prs = [(onesb[:], Mb[:, tp, :]) for tp in range(t)] + [(ustrict[:], Mb[:, t, :])]
                        n_ = len(prs)
                        for k_, (l_, r_) in enumerate(prs):
                            S.op("tensor", lambda e, l_=l_, r_=r_, k_=k_, n_=n_, t=t: e.matmul(out=banks[0][:, t * 16:(t + 1) * 16], lhsT=l_, rhs=r_,
                                                                                         start=(k_ == 0), stop=(k_ == n_ - 1)),
                                 reads=[TR, Tus, Tob_], writes=[Tb[0]], signal=(k_ == n_ - 1))
                    for tp in range(16):
                        S.op("tensor", lambda e, tp=tp: e.matmul(out=banks[1][:, 0:16], lhsT=onesb[:], rhs=Mb[:, tp, :], start=(tp == 0), stop=(tp == 15)),
                             reads=[TR, Tob_], writes=[Tb[1]], signal=(tp == 15))
                    cmax = rt("cmax", [128, 1])
                    S.op("vector", lambda e: e.tensor_reduce(out=cmax[:], in_=banks[1][:, 0:16], axis=AX.X, op=ALU.max), reads=[Tb[1], TR], writes=[TR])
                    import os as _os
                    thr = -1.0 if _os.environ.get("FORCE_DENSE") else float(CAP)
                    vop(lambda e: e.tensor_scalar(out=cmax[:], in0=cmax[:], scalar1=thr, scalar2=None, op0=ALU.is_gt))
                    S.op("vector", lambda e: e.tensor_copy(out=ovf[:], in_=cmax[:]), reads=[TR], writes=[Tovf])
                    rank = banks[0][:, 0:256].rearrange("p (a b) -> p a b", a=16)
                    S.op("vector", lambda e: e.tensor_tensor(out=slotf[:], in0=rank, in1=ebase[:], op=ALU.add), reads=[Tb[0], Teb, TR], writes=[TR])
                    vop(lambda e: e.tensor_scalar(out=okf[:], in0=slotf[:], scalar1=None, scalar2=None, op0=ALU.bypass) if False else
                        e.tensor_tensor(out=okf[:], in0=slotf[:], in1=ebase[:], op=ALU.subtract))
                    vop(lambda e: e.tensor_scalar(out=okf[:], in0=okf[:], scalar1=float(CAP), scalar2=1.0e6, op0=ALU.is_ge, op1=ALU.mult))
                    vop(lambda e: e.tensor_tensor(out=slotf[:], in0=slotf[:], in1=okf[:], op=ALU.add))
                    vop(lambda e: e.tensor_tensor(out=okf[:], in0=slotf[:], in1=oh1[:], op=ALU.mult))
                    vop(lambda e: e.tensor_reduce(out=posf[:, 0, :], in_=okf[:], axis=AX.X, op=ALU.add))
                    vop(lambda e: e.tensor_tensor(out=okf[:], in0=slotf[:], in1=oh2[:], op=ALU.mult))
                    vop(lambda e: e.tensor_reduce(out=posf[:, 1, :], in_=okf[:], axis=AX.X, op=ALU.add))
                    S.op("vector", lambda e: e.tensor_copy(out=pos[:], in_=posf[:]), reads=[TR], writes=[Tpos])
                    S.op("vector", lambda e: e.tensor_copy(out=w12[:, 0, :], in_=w1[:]), reads=[TR, Tw12], writes=[Tw12])
                    S.op("vector", lambda e: e.tensor_copy(out=w12[:, 1, :], in_=w2[:]), reads=[TR, Tw12], writes=[Tw12])
                    Tsc = [S.tile() for _ in range(32)]
                    set_bcreg()
                    for t in range(16):
                        for k_ in range(2):
                            S.dma_fn("gpsimd", lambda e, t=t, k_=k_: e.indirect_dma_start(
                                out=xs_d[:, :], out_offset=bass.IndirectOffsetOnAxis(ap=pos[:, k_, t:t + 1], axis=0),
                                in_=hmb[:, t, :], in_offset=None, bounds_check=bcreg, oob_is_err=False),
                                reads=[Thmb[t], Tpos, Txs], writes=[Tsc[2 * t + k_]], semtile=Thmb[t])
                vop(lambda e: e.tensor_tensor(out=oh1[:], in0=oh1[:], in1=bc3(w1, 16), op=ALU.mult))
                vop(lambda e: e.tensor_tensor(out=oh2[:], in0=oh2[:], in1=bc3(w2, 16), op=ALU.mult))
                S.op("vector", lambda e: e.tensor_tensor(out=comb[:], in0=oh1[:], in1=oh2[:], op=ALU.add), reads=[TR], writes=[Tcomb])
                S.barrier()
                if "comb" in debug:
                    dbg_out("comb", [128, 256])
                    S.dma("sync", dbg["comb"], comb[:].rearrange("p a b -> p (a b)"), reads=[Tcomb], writes=[Tdbg], semtile=Tdbg)
                    S.barrier()
                S.emit()
            if stop_after == "C1":
                return nc

            with ExitStack() as c2:
                wgb = [sb(c2, "wgb%d" % i, [128, 8, 512], BF16) for i in range(2)]; Twg4 = [[S.tile() for _ in range(4)] for _ in range(2)]
                wub = [sb(c2, "wub%d" % i, [128, 8, 512], BF16) for i in range(2)]; Twu4 = [[S.tile() for _ in range(4)] for _ in range(2)]
                wdb = [sb(c2, "wdb%d" % i, [128, 4, 1024], BF16) for i in range(2)]; Twd4 = [[S.tile() for _ in range(4)] for _ in range(2)]
                stg = [sb(c2, "stg%d" % i, [128, 1024], F32) for i in range(3)]; Tstg = [S.tile() for _ in range(3)]
                sq_ = [0]
                aT = [sb(c2, "aT%d" % i, [128, 4, 512], BF16) for i in range(2)]; TaT = [[S.tile() for _ in range(4)] for _ in range(2)]
                sg = [sb(c2, "sg%d" % i, [128, 512], F32) for i in range(2)]; Tsg = [S.tile() for _ in range(2)]
                xg = [sb(c2, "xg%d" % i, [128, 1024], BF16) for i in range(4)]; Txg = [S.tile() for _ in range(4)]
                xgT = [sb(c2, "xgT%d" % i, [128, 8, CAP], BF16) for i in range(2)]; TxgT = [[S.tile() for _ in range(CAP // 128)] for _ in range(2)]
                ysb = [sb(c2, "ysb%d" % i, [128, 1024], F32) for i in range(2)]; Tysb = [S.tile() for _ in range(2)]
                NJ = CAP // 128
                Tys = [S.tile() for _ in range(NEXP * NJ)]

                def w_steps(ex):
                    b2 = ex % 2
                    dmas, casts = [], []
                    for k in range(12):
                        def mk(k=k):
                            if k < 8:
                                srcw = (wg_d if k < 4 else wu_d)[ex]
                                kk = k % 4
                                src = srcw[kk * 256:(kk + 1) * 256, :].rearrange("(c p) n -> p c n", p=128)
                                dst_of = lambda: (wgb if k < 4 else wub)[b2][:, 2 * kk:2 * kk + 2, :]
                                Td = (Twg4 if k < 4 else Twu4)[b2][kk]
                                view = lambda t_: t_[:].rearrange("p (c n) -> p c n", c=2)
                            else:
                                kk = k - 8
                                src = wd_d[ex][kk * 128:(kk + 1) * 128, :]
                                dst_of = lambda: wdb[b2][:, kk, :]
                                Td = Twd4[b2][kk]
                                view = lambda t_: t_[:]
                            cell = {}

                            def d():
                                si = sq_[0] % 3
                                sq_[0] += 1
                                cell["si"] = si
                                S.dma("sync", view(stg[si]), src, writes=[Tstg[si]])

                            def c():
                                si = cell["si"]
                                sv = view(stg[si])
                                dstb = dst_of()
                                if k % 2 == 1:
                                    S.op("scalar", lambda e: e.activation(out=dstb, in_=sv, func=AF.Copy), reads=[Tstg[si]], writes=[Td])
                                else:
                                    S.op("vector", lambda e: e.tensor_copy(out=dstb, in_=sv), reads=[Tstg[si]], writes=[Td])
                            return d, c
                        d, c = mk()
                        dmas.append(d)
                        casts.append(c)
                    steps = dmas[0:3]
                    for k in range(12):
                        steps.append(casts[k])
                        if k + 3 < 12:
                            steps.append(dmas[k + 3])
                    return steps

                def load_w(ex):
                    for f in w_steps(ex):
                        f()

                def gate_up(b2, rhs_of, Trhs, width, gq, slot=None):
                    for ft in range(4):
                        bg, bu = (0, 1) if gq[0] % 2 == 0 else (2, 3)
                        s2 = gq[0] % 2
                        gq[0] += 1
                        mmgroup(banks[bg][:, 0:width], [(wgb[b2][:, c, ft * 128:(ft + 1) * 128], rhs_of(c)) for c in range(8)],
                                reads=Trhs + Twg4[b2], writes=[Tb[bg]])
                        mmgroup(banks[bu][:, 0:width], [(wub[b2][:, c, ft * 128:(ft + 1) * 128], rhs_of(c)) for c in range(8)],
                                reads=Trhs + Twu4[b2], writes=[Tb[bu]])
                        S.op("scalar", lambda e, bg=bg, s2=s2: e.activation(out=sg[s2][:, 0:width], in_=banks[bg][:, 0:width], func=AF.Silu),
                             reads=[Tb[bg]], writes=[Tsg[s2]])
                        S.op("vector", lambda e, bu=bu, s2=s2, ft=ft: e.tensor_tensor(out=aT[b2][:, ft, 0:width], in0=sg[s2][:, 0:width], in1=banks[bu][:, 0:width], op=ALU.mult),
                             reads=[Tsg[s2], Tb[bu]], writes=[TaT[b2][ft]])
                        if slot is not None:
                            slot()

                S.branch_begin()
                gq = [0]; yq = [0]; xq = [0]

                def prep(ex):
                    b2 = ex % 2
                    for j in range(NJ):
                        xi = xq[0] % 4
                        xq[0] += 1
                        r0 = ex * CAP + j * 128
                        S.dma("gpsimd", xg[xi][:], xs_d[r0:r0 + 128, :], reads=Tsc + [Txs], writes=[Txg[xi]])
                        bi = 6 + (xq[0] % 2)
                        pTv = banks[bi][:].bitcast(BF16)
                        for c in range(8):
                            S.op("tensor", lambda e, c=c, xi=xi, pTv=pTv: e.transpose(
                                out=pTv[:, c * 128:(c + 1) * 128], in_=xg[xi][:, c * 128:(c + 1) * 128], identity=identb[:]),
                                reads=[Txg[xi], Tidb], writes=[Tb[bi]], signal=(c == 7))
                        S.op("vector", lambda e, j=j, b2=b2, pTv=pTv: e.tensor_copy(
                            out=xgT[b2][:, :, j * 128:(j + 1) * 128], in_=pTv.rearrange("p (c t) -> p c t", c=8)),
                            reads=[Tb[bi]], writes=[TxgT[b2][j]])
                prep(0)
                load_w(0)
                for ex in range(NEXP):
                    b2 = ex % 2
                    wq = w_steps(ex + 1) if ex + 1 < NEXP else []

                    def pop(n):
                        for _ in range(n):
                            if wq:
                                wq.pop(0)()
                    pop(3)
                    gate_up(b2, lambda c, b2=b2: xgT[b2][:, c, :], TxgT[b2], CAP, gq, slot=lambda: pop(3))
                    if ex + 1 < NEXP:
                        prep(ex + 1)
                    for j in range(NJ):
                        y2 = yq[0] % 2
                        yq[0] += 1
                        for hf in range(2):
                            by = 4 + hf
                            mmgroup(banks[by][:], [(aT[b2][:, ft, j * 128:(j + 1) * 128], wdb[b2][:, ft, hf * 512:(hf + 1) * 512]) for ft in range(4)],
                                    reads=TaT[b2] + Twd4[b2], writes=[Tb[by]])
                            if hf == 0:
                                S.op("vector", lambda e, y2=y2, by=by: e.tensor_copy(out=ysb[y2][:, 0:512], in_=banks[by][:]),
                                     reads=[Tb[by]], writes=[Tysb[y2]])
                            else:
                                S.op("scalar", lambda e, y2=y2, by=by: e.activation(out=ysb[y2][:, 512:1024], in_=banks[by][:], func=AF.Copy),
                                     reads=[Tb[by], Tysb[y2]], writes=[Tysb[y2]])
                        r0 = ex * CAP + j * 128
                        S.dma("gpsimd", ys_d[r0:r0 + 128, :], ysb[y2][:], reads=[Tysb[y2]], writes=[Tys[ex * NJ + j]], semtile=Tysb[y2])
                        pop(3)
                    pop(99)
                S.barrier()
                ygl = []
                for wb in wgb + wub + wdb:
                    v = wb[:].rearrange("p a b -> p (a b)").bitcast(F32)
                    ygl += [v[:, 0:1024], v[:, 1024:2048]]
                Tyg = [S.tile() for _ in ygl]
                set_bcreg()
                def gath(i):
                    t, k_ = i // 2, i % 2
                    gi = i % len(ygl)
                    S.dma_fn("gpsimd", lambda e, t=t, k_=k_, gi=gi: e.indirect_dma_start(
                        out=ygl[gi], out_offset=None, in_=ys_d[:, :],
                        in_offset=bass.IndirectOffsetOnAxis(ap=pos[:, k_, t:t + 1], axis=0),
                        bounds_check=bcreg, oob_is_err=False),
                        reads=Tys + [Tpos], writes=[Tyg[gi]], semtile=Tyg[gi])

                def acc(i):
                    t, k_ = i // 2, i % 2
                    gi = i % len(ygl)
                    for hf in range(2):
                        S.op("vector", lambda e, t=t, k_=k_, gi=gi, hf=hf: e.scalar_tensor_tensor(
                            out=x2[:, t, hf * 512:(hf + 1) * 512], in0=ygl[gi][:, hf * 512:(hf + 1) * 512], scalar=w12[:, k_, t:t + 1],
                            in1=x2[:, t, hf * 512:(hf + 1) * 512], op0=ALU.mult, op1=ALU.add),
                            reads=[Tyg[gi], Tw12, Tx2[t][hf]], writes=[Tx2[t][hf]])
                depth = len(ygl) - 1
                for i in range(32 + depth):
                    if i < 32:
                        gath(i)
                    if i - depth >= 0:
                        acc(i - depth)
                S.branch_mid()
                gq = [0]; yq = [0]
                load_w(0)
                for ex in range(NEXP):
                    b2 = ex % 2
                    if ex + 1 < NEXP:
                        load_w(ex + 1)
                    for tb in range(4):
                        gate_up(b2, lambda c, tb=tb: hmT[:, c, tb * 512:(tb + 1) * 512], ThmT[tb * 4:tb * 4 + 4], 512, gq)
                        for tt in range(4):
                            t = tb * 4 + tt
                            for hf in range(2):
                                by = 4 + (yq[0] % 4)
                                yq[0] += 1
                                mmgroup(banks[by][:], [(aT[b2][:, ft, tt * 128:(tt + 1) * 128], wdb[b2][:, ft, hf * 512:(hf + 1) * 512]) for ft in range(4)],
                                        reads=TaT[b2] + Twd4[b2], writes=[Tb[by]])
                                S.op("vector", lambda e, t=t, hf=hf, by=by, ex=ex: e.scalar_tensor_tensor(
                                    out=x2[:, t, hf * 512:(hf + 1) * 512], in0=banks[by][:], scalar=comb[:, t, ex:ex + 1],
                                    in1=x2[:, t, hf * 512:(hf + 1) * 512], op0=ALU.mult, op1=ALU.add),
                                    reads=[Tb[by], Tcomb, Tx2[t][hf]], writes=[Tx2[t][hf]])
                S.branch_end(ovf[0:1, 0:1], brregs)
                S.emit()

            with ExitStack() as c3:
                gbn = sb(c3, "gbn", [128, 1024], F32); Tgbn = S.tile()
                bcast_load(gbn[:], g_fin, Tgbn, 1024)
                ob = [sb(c3, "ob%d" % i, [128, 1024], F32) for i in range(2)]; Tob = [S.tile() for _ in range(2)]
                Tout = S.tile()
                for t in range(16):
                    S.op("scalar", lambda e, t=t: e.activation(out=junk[:], in_=x2[:, t, :], func=AF.Square, accum_out=ssc[:, t:t + 1]),
                         reads=Tx2[t] + [Tsscall], writes=[Tssc[t], Tjunkc])
                S.op("scalar", lambda e: e.activation(out=ssc[:], in_=ssc[:], func=AF.Sqrt, scale=1.0 / 1024, bias=EPS),
                     reads=Tssc, writes=[Tsscall])
                S.op("vector", lambda e: e.reciprocal(out=ssc[:], in_=ssc[:]), reads=[Tsscall], writes=[Tsscall])
                for t in range(16):
                    S.op("vector", lambda e, t=t: e.scalar_tensor_tensor(
                        out=ob[t % 2][:], in0=x2[:, t, :], scalar=ssc[:, t:t + 1], in1=gbn[:], op0=ALU.mult, op1=ALU.mult),
                        reads=Tx2[t] + [Tsscall, Tgbn], writes=[Tob[t % 2]])
                    S.dma("sync", out[t * 128:(t + 1) * 128, :], ob[t % 2][:], reads=[Tob[t % 2]], writes=[Tout], semtile=Tob[t % 2])
                S.barrier()
                S.emit()
    return nc


def _const_tables():
    f32 = np.float32
    inv_freq = (f32(1.0) / (f32(10000.0) ** (np.arange(0, 64, 2, dtype=f32) / f32(64)))).astype(f32)
    pos = np.arange(4096, dtype=f32)
    ang = (pos[:, None] * inv_freq[None, :]).astype(f32)
    cos = np.cos(ang).astype(f32)
    sin = np.sin(ang).astype(f32)
    r = np.arange(128)
    dh = r % 64
    cosT = cos[:, dh % 32].T.copy()
    sgn = np.where(dh < 32, -1.0, 1.0).astype(f32)
    sinT = (sin[:, dh % 32].T * sgn[:, None]).astype(f32)
    return cosT, sinT


def _masks(hf):
    k = np.arange(128)[:, None, None]
    r = np.arange(4)[None, :, None]
    q = np.arange(256)[None, None, :]
    md = np.zeros((128, 2, 4, 256), np.float32)
    mf = np.zeros((128, 2, 4, 256), np.float32)
    for par in range(2):
        if par == 0:
            kb = r
            j = 0 if hf == 0 else 1
        else:
            kb = 4 + r
            j = 3 if hf == 0 else 2
        s = kb * 128 + k
        t = j * 256 + q
        mf[:, par] = np.where(s <= t, 3e38, 0.0)
        md[:, par] = np.where((s // 64) <= (t // 64), 3e38, 0.0)
    return (md.reshape(128, 2, 1024).astype(ml_dtypes.bfloat16),
            mf.reshape(128, 2, 1024).astype(ml_dtypes.bfloat16))


def own_tokens(hf):
    return np.concatenate([np.arange(j * 256, (j + 1) * 256) for j in own_qtiles(hf)])


def prep(inputs):
    f32 = np.float32
    x = np.asarray(inputs["x"], f32)
    w_in = np.ascontiguousarray(np.asarray(inputs["w_in"], f32)[0])

    def swap_cols(w):
        return np.ascontiguousarray(w.reshape(1024, 8, 2, 32)[:, :, ::-1, :].reshape(1024, 512))

    cosT, sinT = _const_tables()
    common = {
        "w_in": w_in,
        "wqs": swap_cols(w_in[:, 0:512]),
        "wks": swap_cols(w_in[:, 512:1024]),
        "cosk": cosT, "sink": sinT,
        "identb": np.eye(128, dtype=f32).astype(ml_dtypes.bfloat16),
        "identf": np.eye(128, dtype=f32),
        "tri": np.triu(np.ones((128, 128), f32)),
        "onesf": np.ones((128, 128), f32),
        "onesb": np.ones((128, 128), f32).astype(ml_dtypes.bfloat16),
        "ustrict": np.triu(np.ones((128, 128), f32), 1).astype(ml_dtypes.bfloat16),
        "ebase": np.ascontiguousarray(np.broadcast_to((np.arange(16, dtype=f32) * CAP)[None, None, :], (128, 16, 16)).reshape(128, 256)),
        "g_attn": np.asarray(inputs["norm_attn_g"], f32).reshape(1, 1024),
        "g_ffn": np.asarray(inputs["norm_ffn_g"], f32).reshape(1, 1024),
        "g_fin": np.asarray(inputs["norm_final_g"], f32).reshape(1, 1024),
        "bfor": np.asarray(inputs["b_forget"], f32).reshape(1, 8),
        "lamv": np.concatenate([np.asarray(inputs[k], f32).reshape(1, 64) for k in
                                ("lambda_q1", "lambda_k1", "lambda_q2", "lambda_k2")], axis=1),
        "dng": np.asarray(inputs["diff_norm_g"], f32).reshape(1, 128),
        "w_out": np.ascontiguousarray(np.asarray(inputs["w_out"], f32)[0]),
        "rw": np.ascontiguousarray(np.concatenate([np.asarray(inputs["router_group_w"], f32)[0],
                                                   np.asarray(inputs["router_expert_w"], f32)[0]], axis=1)),
        "rb": np.concatenate([np.asarray(inputs["router_group_b"], f32).reshape(1, 4),
                              np.asarray(inputs["router_expert_b"], f32).reshape(1, 16)], axis=1),
        "wg": np.ascontiguousarray(np.asarray(inputs["w_gate"], f32)[0]),
        "wu": np.ascontiguousarray(np.asarray(inputs["w_up"], f32)[0]),
        "wd": np.ascontiguousarray(np.asarray(inputs["w_down"], f32)[0]),
    }
    in_maps = []
    for c in range(8):
        b, hf = c // 2, c % 2
        tok = own_tokens(hf)
        md, mf = _masks(hf)
        m = dict(common)
        m["xb"] = np.ascontiguousarray(x[b])
        m["xo"] = np.ascontiguousarray(x[b][tok])
        m["cosq"] = np.ascontiguousarray(cosT[:, tok] * f32(0.125))
        m["sinq"] = np.ascontiguousarray(sinT[:, tok] * f32(0.125))
        cs = np.zeros((8, 33), f32)
        for i, j in enumerate(own_qtiles(hf)):
            cs[i, 2 * j + 1] = 1.0
        m["csel"] = cs.reshape(1, 8 * 33)
        m["maskd"] = md
        m["maskf"] = mf
        in_maps.append(m)
    return in_maps


def kernel(**inputs):
    in_maps = prep(inputs)
    nc = build()
    res = run_bass_kernel_spmd(nc, in_maps, core_ids=list(range(8)))
    out = np.zeros((4, 4096, 1024), np.float32)
    for c in range(8):
        b, hf = c // 2, c % 2
        out[b, own_tokens(hf)] = res.results[c]["out"]
    return out
```

```python
import numpy as np
import ml_dtypes
from contextlib import ExitStack
import concourse.bass as bass
import concourse.mybir as mybir
from concourse.bass_utils import run_bass_kernel_spmd

F32 = mybir.dt.float32
BF16 = mybir.dt.bfloat16
I32 = mybir.dt.int32
AF = mybir.ActivationFunctionType
ALU = mybir.AluOpType
AX = mybir.AxisListType

ENGS = ("sync", "scalar", "vector", "gpsimd", "tensor")
EPS = 1e-6
LAM_INIT = 0.8 - 0.6 * 1.0
NEXP = 16
CAP = 512
NSLOT = NEXP * CAP


class Tile:
    __slots__ = ("name", "last_w", "readers", "dsem")

    def __init__(self, name):
        self.name = name
        self.last_w = None
        self.readers = {}
        self.dsem = None


class Sched:
    def __init__(self, nc, es):
        self.nc = nc
        self.es = es
        self.ops = {e: [] for e in ENGS}
        self.sems = {}
        self.cnt = {}
        self.waited = {e: {} for e in ENGS}
        self.pending = {e: False for e in ENGS}
        for e in ENGS:
            self._mksem("E:" + e)
        self.n_dsem = 0
        self.nops = 0
        self.tiles = []

    def _mksem(self, key):
        self.sems[key] = self.es.enter_context(self.nc.semaphore(key.replace(":", "_")))
        self.cnt[key] = 0

    def tile(self, name="t"):
        t = Tile(name)
        self.tiles.append(t)
        return t

    def _snapshot(self):
        return (dict(self.cnt), {e: dict(w) for e, w in self.waited.items()},
                [(t, t.last_w, dict(t.readers)) for t in self.tiles])

    def _restore(self, snap):
        self.cnt = dict(snap[0])
        for k in self.sems:
            self.cnt.setdefault(k, 0)
        self.waited = {e: dict(w) for e, w in snap[1].items()}
        for t, lw, rd in snap[2]:
            t.last_w = lw
            t.readers = dict(rd)

    def branch_begin(self):
        self.barrier()
        self._outer_ops = self.ops
        self.ops = {e: [] for e in ENGS}
        self._snap = self._snapshot()

    def branch_mid(self):
        self.barrier()
        self._A = (self.ops, dict(self.cnt))
        self.ops = {e: [] for e in ENGS}
        self._restore(self._snap)

    def branch_end(self, flag_ap, regs):
        self.barrier()
        opsA, cntA = self._A
        opsB, cntB = self.ops, dict(self.cnt)
        target = {k: max(cntA.get(k, 0), cntB.get(k, 0)) for k in set(cntA) | set(cntB)}

        def pads(cntX):
            out = {e: [] for e in ENGS}
            for k, v in target.items():
                d = v - cntX.get(k, 0)
                if d > 0:
                    owner = k[2:] if k.startswith("E:") else "gpsimd"
                    out[owner].append((k, d))
            return out
        pA, pB = pads(cntA), pads(cntB)
        self.ops = self._outer_ops
        for e in ENGS:
            self.ops[e].append(("branch", flag_ap, regs[e], opsA[e], pA[e], opsB[e], pB[e]))
        self.cnt = target
        for e in ENGS:
            self.waited[e] = dict(target)
        for t in self.tiles:
            t.last_w = None
            t.readers = {}

    def dsem_for(self, t):
        if t.dsem is None:
            key = "D:%d" % self.n_dsem
            self.n_dsem += 1
            self._mksem(key)
            t.dsem = key
        return t.dsem

    def _need(self, eng, waits, key, val):
        if eng == "tensor" and key == "E:tensor":
            return
        if self.cnt[key] < val:
            raise RuntimeError("wait on un-signalled event %s %d (cnt %d) from %s" % (key, val, self.cnt[key], eng))
        if self.waited[eng].get(key, 0) >= val:
            return
        self.waited[eng][key] = val
        waits[key] = max(waits.get(key, 0), val)

    def _deps(self, eng, reads, writes):
        waits = {}
        for t in reads:
            if t.last_w is not None:
                self._need(eng, waits, *t.last_w)
        for t in writes:
            if t.last_w is not None:
                self._need(eng, waits, *t.last_w)
            for k, v in t.readers.items():
                self._need(eng, waits, k, v)
        return list(waits.items())

    def _record(self, ev, reads, writes):
        for t in writes:
            t.last_w = ev
            t.readers = {}
        for t in reads:
            if t not in writes:
                if t.readers.get(ev[0], 0) < ev[1]:
                    t.readers[ev[0]] = ev[1]

    def op(self, eng, fn, reads=(), writes=(), signal=True):
        waits = self._deps(eng, reads, writes)
        key = "E:" + eng
        if signal:
            self.cnt[key] += 1
            ev = (key, self.cnt[key])
            inc = (key, 1)
            self.pending[eng] = False
        else:
            ev = (key, self.cnt[key] + 1)
            inc = None
            self.pending[eng] = True
        self._record(ev, reads, writes)
        self.ops[eng].append((waits, fn, inc))
        self.nops += 1

    def dma(self, eng, out, in_, reads=(), writes=(), semtile=None, **kw):
        waits = self._deps(eng, reads, writes)
        if semtile is None:
            semtile = writes[0] if writes else reads[0]
        key = self.dsem_for(semtile)
        self.cnt[key] += 16
        ev = (key, self.cnt[key])
        self._record(ev, reads, writes)

        def fn(e, out=out, in_=in_, kw=kw):
            return e.dma_start(out=out, in_=in_, **kw)
        self.ops[eng].append((waits, fn, (key, 16)))
        self.nops += 1

    def dma_fn(self, eng, fn, reads=(), writes=(), semtile=None):
        waits = self._deps(eng, reads, writes)
        key = self.dsem_for(semtile)
        self.cnt[key] += 16
        ev = (key, self.cnt[key])
        self._record(ev, reads, writes)
        self.ops[eng].append((waits, fn, (key, 16)))
        self.nops += 1

    def barrier(self, engs=ENGS):
        for e in ENGS:
            assert not self.pending[e], e
        for e in engs:
            waits = {}
            for key, c in self.cnt.items():
                if c > 0:
                    self._need(e, waits, key, c)
            self.ops[e].append((list(waits.items()), None, None))

    def emit(self):
        nc = self.nc
        sems = self.sems
        ops = self.ops
        with nc.Block() as block:
            def replay(e, lst):
                for ent in lst:
                    if ent[0] == "branch":
                        _, flag_ap, reg, oA, pA, oB, pB = ent
                        e.reg_load(reg, flag_ap)
                        with e.If_eq(reg, 0):
                            replay(e, oA)
                            for k, d in pA:
                                e.sem_inc(sems[k], d)
                            e.nop()
                        with e.Else():
                            replay(e, oB)
                            for k, d in pB:
                                e.sem_inc(sems[k], d)
                            e.nop()
                        continue
                    waits, fn, inc = ent
                    for key, val in waits:
                        e.wait_ge(sems[key], val)
                    if fn is None:
                        continue
                    inst = fn(e)
                    if inc is not None:
                        inst.then_inc(sems[inc[0]], inc[1])

            def mk(name):
                def body(e):
                    replay(e, ops[name])
                return body
            block.sync(mk("sync"))
            block.scalar(mk("scalar"))
            block.vector(mk("vector"))
            block.gpsimd(mk("gpsimd"))
            block.tensor(mk("tensor"))
        self.ops = {e: [] for e in ENGS}


def own_qtiles(hf):
    js = []
    for m in range(4):
        js += ([4 * m, 4 * m + 3] if hf == 0 else [4 * m + 1, 4 * m + 2])
    return js


def nk_of(i):
    return 8 * (i // 2) + (4 if i % 2 == 0 else 8)


def interleave(A, B):
    a, b = len(A), len(B)
    if a == 0:
        for f in B:
            f()
        return
    done = 0
    for k, f in enumerate(A):
        f()
        upto = ((k + 1) * b) // a
        while done < upto:
            B[done]()
            done += 1
    while done < b:
        B[done]()
        done += 1


def build(debug=(), stop_after=None, moe="sparse"):
    nc = bass.Bass("TRN2", target_bir_lowering=False)

    def din(name, shape, dt=F32):
        return nc.dram_tensor(name, list(shape), dt, kind="ExternalInput").ap()

    xb = din("xb", [4096, 1024])
    xo = din("xo", [2048, 1024])
    w_in = din("w_in", [1024, 3080])
    wqs_d = din("wqs", [1024, 512])
    wks_d = din("wks", [1024, 512])
    cosk = din("cosk", [128, 4096])
    sink = din("sink", [128, 4096])
    cosq = din("cosq", [128, 2048])
    sinq = din("sinq", [128, 2048])
    maskd_d = din("maskd", [128, 2, 1024], BF16)
    maskf_d = din("maskf", [128, 2, 1024], BF16)
    identb_d = din("identb", [128, 128], BF16)
    identf_d = din("identf", [128, 128])
    tri_d = din("tri", [128, 128])
    onesf_d = din("onesf", [128, 128])
    g_attn = din("g_attn", [1, 1024])
    g_ffn = din("g_ffn", [1, 1024])
    g_fin = din("g_fin", [1, 1024])
    bfor = din("bfor", [1, 8])
    lam_d = din("lamv", [1, 256])
    csel_d = din("csel", [1, 8 * 33])
    dng = din("dng", [1, 128])
    w_out = din("w_out", [1024, 1024])
    rw_d = din("rw", [1024, 20])
    rb_d = din("rb", [1, 20])
    wg_d = din("wg", [16, 1024, 512])
    wu_d = din("wu", [16, 1024, 512])
    wd_d = din("wd", [16, 512, 1024])
    ebase_d = din("ebase", [128, 256])
    ustrict_d = din("ustrict", [128, 128], BF16)
    onesb_d = din("onesb", [128, 128], BF16)
    xs_d = nc.dram_tensor("xs_scratch", [NSLOT, 1024], BF16).ap()
    ys_d = nc.dram_tensor("ys_scratch", [NSLOT, 1024], F32).ap()
    out = nc.dram_tensor("out", [2048, 1024], F32, kind="ExternalOutput").ap()
    dbg = {}

    def dbg_out(name, shape, dt=F32):
        if name in debug:
            dbg[name] = nc.dram_tensor("dbg_" + name, list(shape), dt, kind="ExternalOutput").ap()
            return dbg[name]
        return None

    with ExitStack() as es:
        S = Sched(nc, es)

        uniq = [0]

        def sb(sc, name, shape, dt):
            uniq[0] += 1
            return sc.enter_context(nc.sbuf_tensor("s%d_%s" % (uniq[0], name), list(shape), dt))

        banks = [es.enter_context(nc.psum_tensor("bank%d" % i, [128, 512], F32)) for i in range(8)]
        Tb = [S.tile("bank%d" % i) for i in range(8)]
        Tdbg = S.tile("dbg")

        def dump(name, src_ap, reads):
            if name in dbg:
                S.dma("sync", dbg[name], src_ap, reads=reads, writes=[Tdbg], semtile=Tdbg)

        def bcast_load(dst, src, T, n):
            S.dma("sync", dst, src.broadcast_to([128, n]), writes=[T])

        bcreg = es.enter_context(nc.gpsimd.register("bcreg"))
        brregs = {e: es.enter_context(getattr(nc, e).register("br_" + e)) for e in ENGS}

        def set_bcreg():
            S.ops["gpsimd"].append(([], lambda e: e.reg_mov(bcreg, NSLOT - 1), None))

        identb = sb(es, "identb", [128, 128], BF16); Tidb = S.tile("identb")
        S.dma("sync", identb[:], identb_d, writes=[Tidb])
        gb_attn = sb(es, "gb_attn", [128, 1024], F32); Tgba = S.tile("gba")
        bcast_load(gb_attn[:], g_attn, Tgba, 1024)
        OT = sb(es, "OT", [128, 8, 2048], BF16)
        TOT = [S.tile("OT%d" % q) for q in range(16)]

        def make_sweep(sc, pfx, pT_banks):
            st = {}
            st["xt"] = [sb(sc, pfx + "xt%d" % i, [128, 1024], F32) for i in range(4)]
            st["Txt"] = [S.tile() for _ in range(4)]
            st["hb"] = [sb(sc, pfx + "hb%d" % i, [128, 1024], BF16) for i in range(2)]
            st["Thb"] = [S.tile() for _ in range(2)]
            st["hT"] = [sb(sc, pfx + "hT%d" % i, [128, 8, 512], BF16) for i in range(3)]
            st["ThT"] = [[S.tile() for _ in range(4)] for _ in range(3)]
            st["junk"] = sb(sc, pfx + "junk", [128, 1024], BF16)
            st["Tjunk"] = S.tile()
            st["ss"] = [sb(sc, pfx + "ss%d" % i, [128, 4], F32) for i in range(3)]
            st["Tss"] = [[S.tile() for _ in range(4)] for _ in range(3)]
            st["sq"] = [sb(sc, pfx + "sq%d" % i, [128, 4], F32) for i in range(3)]
            st["Tsq"] = [S.tile() for _ in range(3)]
            st["rs"] = [sb(sc, pfx + "rs%d" % i, [128, 4], F32) for i in range(3)]
            st["Trs"] = [S.tile() for _ in range(3)]
            st["pT"] = pT_banks
            return st

        def sweep_stage1(st, x_ap, blk, gb, Tgb, deferred=False):
            b2 = blk % 3
            for tt in range(4):
                n = blk * 4 + tt
                xt, Txt = st["xt"][n % 4], st["Txt"][n % 4]
                S.dma("sync", xt[:], x_ap[n * 128:(n + 1) * 128, :], writes=[Txt])
                S.op("scalar", lambda e, xt=xt, tt=tt: e.activation(out=st["junk"][:], in_=xt[:], func=AF.Square,
                                                                    accum_out=st["ss"][b2][:, tt:tt + 1]),
                     reads=[Txt], writes=[st["Tss"][b2][tt], st["Tjunk"]])
            S.op("scalar", lambda e: e.activation(out=st["sq"][b2][:], in_=st["ss"][b2][:], func=AF.Sqrt,
                                                  scale=1.0 / 1024, bias=EPS),
                 reads=st["Tss"][b2], writes=[st["Tsq"][b2]])
            S.op("vector", lambda e: e.reciprocal(out=st["rs"][b2][:], in_=st["sq"][b2][:]),
                 reads=[st["Tsq"][b2]], writes=[st["Trs"][b2]])
            def stt(tt):
                n = blk * 4 + tt
                xt, Txt = st["xt"][n % 4], st["Txt"][n % 4]
                hb, Thb = st["hb"][n % 2], st["Thb"][n % 2]
                S.op("vector", lambda e, xt=xt, hb=hb, tt=tt: e.scalar_tensor_tensor(
                    out=hb[:], in0=xt[:], scalar=st["rs"][b2][:, tt:tt + 1], in1=gb[:], op0=ALU.mult, op1=ALU.mult),
                    reads=[Txt, st["Trs"][b2], Tgb], writes=[Thb])

            def tr(tt):
                n = blk * 4 + tt
                hb, Thb = st["hb"][n % 2], st["Thb"][n % 2]
                bi = st["pT"][n % 2]
                pTv = banks[bi][:].bitcast(BF16)
                for c in range(8):
                    S.op("tensor", lambda e, c=c, hb=hb, pTv=pTv: e.transpose(
                        out=pTv[:, c * 128:(c + 1) * 128], in_=hb[:, c * 128:(c + 1) * 128], identity=identb[:]),
                        reads=[Thb, Tidb], writes=[Tb[bi]], signal=(c == 7))

            def ev(tt):
                n = blk * 4 + tt
                bi = st["pT"][n % 2]
                pTv = banks[bi][:].bitcast(BF16)
                S.op("vector", lambda e, tt=tt, pTv=pTv: e.tensor_copy(
                    out=st["hT"][b2][:, :, tt * 128:(tt + 1) * 128], in_=pTv.rearrange("p (c t) -> p c t", c=8)),
                    reads=[Tb[bi]], writes=[st["ThT"][b2][tt]])
            if not deferred:
                for f, a in ((stt, 0), (stt, 1), (tr, 0), (ev, 0), (stt, 2), (tr, 1), (ev, 1), (stt, 3), (tr, 2), (ev, 2), (tr, 3), (ev, 3)):
                    f(a)
                return []
            stt(0)
            return [lambda: (stt(1), tr(0), ev(0)), lambda: (stt(2), tr(1), ev(1)),
                    lambda: (stt(3), tr(2), ev(2)), lambda: (tr(3), ev(3))]

        def wload(dst, src_cols, T):
            S.dma("gpsimd", dst, src_cols.rearrange("(c p) n -> p c n", p=128), writes=[T])

        def mmgroup(out_ap, pairs, reads, writes):
            n = len(pairs)
            for k, (l, r) in enumerate(pairs):
                S.op("tensor", lambda e, l=l, r=r, k=k: e.matmul(out=out_ap, lhsT=l, rhs=r, start=(k == 0), stop=(k == n - 1)),
                     reads=reads, writes=writes, signal=(k == n - 1))

        def run_attention(units, PT, TPT, KT_of, QT_of, V_of, W, exp_emit, evac_emit, mask_emit, s_banks, o_banks,
                          OTs=None, TOTs=None, smr=None, Tsmr=None, identf=None, Tidf=None, Vsum_of=None):
            gctr = [0]
            dv = W - 1
            MO = dv if Vsum_of is not None else W
            deferred = []

            def st_list(ui, u):
                buf = ui % 2
                nk = nk_of(u["i"])
                L = []
                for p in range(nk // 2):
                    def f(p=p):
                        bi = s_banks[gctr[0] % len(s_banks)]
                        gctr[0] += 1
                        for j in range(2):
                            kb = 2 * p + j
                            kap, Tk = KT_of(u, kb)
                            qap, Tq = QT_of(u)
                            S.op("tensor", lambda e, kap=kap, qap=qap, j=j, bi=bi: e.matmul(
                                out=banks[bi][:, j * 256:(j + 1) * 256], lhsT=kap, rhs=qap, start=True, stop=True),
                                reads=[Tk, Tq], writes=[Tb[bi]], signal=(j == 1))
                        exp_emit(u, p, bi, PT[buf], TPT[buf][p])
                    L.append(f)
                L.append(lambda: mask_emit(u, PT[buf], TPT[buf]))
                return L

            def pv_list(ui, u):
                buf = ui % 2
                nk = nk_of(u["i"])
                ba = o_banks[(ui % 2) * 2]
                bb = o_banks[(ui % 2) * 2 + 1]
                o2 = ui % 2
                L = []
                for k0 in range(0, nk, 4):
                    def f(k0=k0):
                        for kb in range(k0, min(nk, k0 + 4)):
                            vap, Tv = V_of(u, kb)
                            S.op("tensor", lambda e, kb=kb, vap=vap: e.matmul(
                                out=banks[ba][0:MO, 0:256], lhsT=vap, rhs=PT[buf][:, kb, :], start=(kb == 0), stop=(kb == nk - 1)),
                                reads=[TPT[buf][kb // 2], Tv], writes=[Tb[ba]], signal=(kb == nk - 1))
                    L.append(f)
                if Vsum_of is not None:
                    for k0 in range(0, nk, 4):
                        def g(k0=k0):
                            for kb in range(k0, min(nk, k0 + 4)):
                                vap, Tv = Vsum_of(u, kb)
                                S.op("tensor", lambda e, kb=kb, vap=vap: e.matmul(
                                    out=banks[bb][0:1, 256:512], lhsT=vap, rhs=PT[buf][:, kb, :], start=(kb == 0), stop=(kb == nk - 1)),
                                    reads=[TPT[buf][kb // 2], Tv], writes=[Tb[bb]], signal=(kb == nk - 1))
                        L.append(g)

                def copy_out():
                    S.op("vector", lambda e: e.tensor_copy(out=OTs[o2][0:MO, :], in_=banks[ba][0:MO, 0:256]), reads=[Tb[ba]], writes=[TOTs[o2]])
                    if Vsum_of is not None:
                        S.op("vector", lambda e: e.tensor_copy(out=smr[o2][:], in_=banks[bb][0:1, 256:512]), reads=[Tb[bb]], writes=[Tsmr[o2]])

                def transpose_back():
                    for s in range(2):
                        last = (Vsum_of is None)
                        S.op("tensor", lambda e, s=s: e.transpose(out=banks[bb][:, s * W:s * W + MO], in_=OTs[o2][0:MO, s * 128:(s + 1) * 128],
                                                                  identity=identf[0:MO, 0:MO]),
                             reads=[TOTs[o2], Tidf], writes=[Tb[bb]], signal=(last and s == 1))
                        if Vsum_of is not None:
                            S.op("tensor", lambda e, s=s: e.transpose(out=banks[bb][:, s * W + dv:s * W + W], in_=smr[o2][0:1, s * 128:(s + 1) * 128],
                                                                      identity=identf[0:1, 0:1]),
                                 reads=[Tsmr[o2], Tidf], writes=[Tb[bb]], signal=(s == 1))
                    evac_emit(u, bb)
                L.append(copy_out)
                deferred.append(transpose_back)
                return L

            for ui in range(len(units) + 1):
                A = st_list(ui, units[ui]) if ui < len(units) else []
                B = pv_list(ui - 1, units[ui - 1]) if ui >= 1 else []
                if len(deferred) > (1 if ui >= 1 else 0):
                    B.insert(min(2, len(B)), deferred.pop(0))
                interleave(A, B)
            while deferred:
                deferred.pop(0)()

        with ExitStack() as pa:
            KT = sb(pa, "KTd", [128, 4, 4096], BF16)
            TKT = [[S.tile() for _ in range(8)] for _ in range(4)]
            QT = sb(pa, "QTd", [128, 4, 2048], BF16)
            TQT = [[S.tile() for _ in range(4)] for _ in range(4)]
            Vd = sb(pa, "Vd", [128, 32, 4, 130], BF16)
            TV = [S.tile() for _ in range(32)]
            Tvones = S.tile()
            S.op("vector", lambda e: e.memset(Vd[:, :, :, 128:130], 1.0), writes=TV)
            lamt = sb(pa, "lamt", [128, 256], F32); Tlam = S.tile()
            bcast_load(lamt[:], lam_d, Tlam, 256)
            lj = sb(pa, "lj", [128, 64], F32)
            ls = sb(pa, "ls", [128, 2], F32); Tls = S.tile()
            le = sb(pa, "le", [128, 2], F32); Tle = S.tile()
            neglam = sb(pa, "neglam", [128, 1], F32); Tnl = S.tile()
            for z in range(2):
                S.op("vector", lambda e, z=z: e.scalar_tensor_tensor(
                    out=lj[:], in0=lamt[:, z * 128:z * 128 + 64], scalar=1.0, in1=lamt[:, z * 128 + 64:z * 128 + 128],
                    op0=ALU.mult, op1=ALU.mult, accum_out=ls[:, z:z + 1]), reads=[Tlam, Tls], writes=[Tls])
            S.op("scalar", lambda e: e.activation(out=le[:], in_=ls[:], func=AF.Exp), reads=[Tls], writes=[Tle])
            S.op("vector", lambda e: e.tensor_tensor(out=neglam[:], in0=le[:, 1:2], in1=le[:, 0:1], op=ALU.subtract),
                 reads=[Tle], writes=[Tnl])
            S.op("vector", lambda e: e.tensor_scalar(out=neglam[:], in0=neglam[:], scalar1=-LAM_INIT, scalar2=None, op0=ALU.add),
                 reads=[Tnl], writes=[Tnl])
            gsc = sb(pa, "gsc", [128, 128], F32); Tgsc = S.tile()
            bcast_load(gsc[:], dng, Tgsc, 128)
            S.op("vector", lambda e: e.tensor_scalar(out=gsc[:], in0=gsc[:], scalar1=1.0 - LAM_INIT, scalar2=None, op0=ALU.mult),
                 reads=[Tgsc], writes=[Tgsc])
            Txs = S.tile("xs")

            with ExitStack() as sw:
                st = make_sweep(sw, "a", [0, 1])
                wA = sb(sw, "wA", [128, 8, 512], BF16); TwA = S.tile()
                wB = sb(sw, "wB", [128, 8, 512], BF16); TwB = S.tile()
                wC = sb(sw, "wC", [128, 8, 512], BF16); TwC = S.tile()
                wload(wA[:], w_in[:, 512:1024], TwA)
                wload(wB[:], wks_d, TwB)
                wload(wC[:], w_in[:, 1024:1536], TwC)
                ct = [sb(sw, "ct%d" % i, [128, 512], F32) for i in range(2)]; Tct = [S.tile() for _ in range(2)]
                sn = [sb(sw, "sn%d" % i, [128, 512], F32) for i in range(2)]; Tsn = [S.tile() for _ in range(2)]
                t1 = [sb(sw, "t1%d" % i, [128, 512], F32) for i in range(2)]; Tt1 = [S.tile() for _ in range(2)]
                t2 = [sb(sw, "t2%d" % i, [128, 512], F32) for i in range(2)]; Tt2 = [S.tile() for _ in range(2)]
                rctr = [0]

                def rope_proj(blk, hT, ThT, wq_, Tw_, ws_, Tws_, cos_d, sin_d, dstT, TdstT, slots=()):
                    b2 = blk % 2
                    S.dma("sync", ct[b2][:], cos_d[:, blk * 512:(blk + 1) * 512], writes=[Tct[b2]])
                    S.dma("sync", sn[b2][:], sin_d[:, blk * 512:(blk + 1) * 512], writes=[Tsn[b2]])
                    for h in range(4):
                        ba, bb = (2, 3) if h % 2 == 0 else (4, 5)
                        mmgroup(banks[ba][:], [(wq_[:, c, h * 128:(h + 1) * 128], hT[:, c, :]) for c in range(8)],
                                reads=list(ThT) + [Tw_], writes=[Tb[ba]])
                        mmgroup(banks[bb][:], [(ws_[:, c, h * 128:(h + 1) * 128], hT[:, c, :]) for c in range(8)],
                                reads=list(ThT) + [Tws_], writes=[Tb[bb]])
                        r = rctr[0] % 2
                        rctr[0] += 1
                        S.op("vector", lambda e, r=r, ba=ba: e.tensor_tensor(out=t1[r][:], in0=banks[ba][:], in1=ct[b2][:], op=ALU.mult),
                             reads=[Tb[ba], Tct[b2]], writes=[Tt1[r]])
                        S.op("vector", lambda e, r=r, bb=bb: e.tensor_tensor(out=t2[r][:], in0=banks[bb][:], in1=sn[b2][:], op=ALU.mult),
                             reads=[Tb[bb], Tsn[b2]], writes=[Tt2[r]])
                        S.op("gpsimd", lambda e, r=r, h=h: e.tensor_tensor(out=dstT[:, h, blk * 512:(blk + 1) * 512], in0=t1[r][:], in1=t2[r][:], op=ALU.add),
                             reads=[Tt1[r], Tt2[r]], writes=[TdstT[h][blk]])
                        if h < len(slots):
                            slots[h]()

                def kv_proj(blk, hT, ThT, slots=()):
                    rope_proj(blk, hT, ThT, wA, TwA, wB, TwB, cosk, sink, KT, TKT, slots)
                    for tt in range(4):
                        n = blk * 4 + tt
                        bv = 6 + (tt % 2)
                        mmgroup(banks[bv][:], [(hT[:, c, tt * 128:(tt + 1) * 128], wC[:, c, :]) for c in range(8)],
                                reads=[ThT[tt], TwC], writes=[Tb[bv]])
                        S.op("scalar", lambda e, n=n, bv=bv: e.activation(
                            out=Vd[:, n, :, 0:128], in_=banks[bv][:].rearrange("p (h d) -> p h d", h=4), func=AF.Copy),
                            reads=[Tb[bv]], writes=[TV[n]])

                sweep_stage1(st, xb, 0, gb_attn, Tgba)
                sweep_stage1(st, xb, 1, gb_attn, Tgba)
                for blk in range(8):
                    sl = sweep_stage1(st, xb, blk + 2, gb_attn, Tgba, deferred=True) if blk + 2 < 8 else []
                    kv_proj(blk, st["hT"][blk % 3], st["ThT"][blk % 3], sl)
                wload(wA[:], w_in[:, 0:512], TwA)
                wload(wB[:], wqs_d, TwB)
                sweep_stage1(st, xo, 0, gb_attn, Tgba)
                sweep_stage1(st, xo, 1, gb_attn, Tgba)
                for blk in range(4):
                    sl = sweep_stage1(st, xo, blk + 2, gb_attn, Tgba, deferred=True) if blk + 2 < 4 else []
                    rope_proj(blk, st["hT"][blk % 3], st["ThT"][blk % 3], wA, TwA, wB, TwB, cosq, sinq, QT, TQT, sl)
                S.barrier()
                if "KTd" in debug:
                    dbg_out("KTd", [128, 4, 4096], BF16); dump("KTd", KT[:], [])
                    dbg_out("QTd", [128, 4, 2048], BF16); dump("QTd", QT[:], [])
                    dbg_out("Vd", [128, 32, 4, 130], BF16); dump("Vd", Vd[:], [])
                    S.barrier()
                S.emit()
            if stop_after == "Aproj":
                S.barrier(); S.emit()
                return nc

            with ExitStack() as at:
                maskd = sb(at, "maskd", [128, 2, 1024], BF16); Tmd = S.tile()
                S.dma("sync", maskd[:], maskd_d, writes=[Tmd])
                if moe == "sparse":
                    zer = sb(at, "zer", [128, 2048], BF16); Tzer = S.tile()
                    S.op("gpsimd", lambda e: e.memset(zer[:], 0.0), writes=[Tzer])
                    for n in range(NSLOT // 256):
                        S.dma("gpsimd", xs_d[n * 256:(n + 1) * 256, :].rearrange("(p r) d -> p (r d)", r=2), zer[:], reads=[Tzer], writes=[Txs], semtile=Tzer)
                PT = [sb(at, "PT%d" % i, [128, 32, 256], BF16) for i in range(2)]
                TPT = [[S.tile() for _ in range(16)] for _ in range(2)]
                oc = sb(at, "oc", [128, 16, 4, 128], F32); Toc = [[S.tile() for _ in range(4)] for _ in range(16)]
                ssq = sb(at, "ssq", [128, 64], F32); Tssq = S.tile()
                A1 = [sb(at, "A1%d" % i, [128, 128], F32) for i in range(2)]; TA1 = [S.tile() for _ in range(2)]
                rr = sb(at, "rr", [128, 4], F32); Trr = [S.tile() for _ in range(4)]
                sjunk = sb(at, "sjunk", [128, 128], F32); Tsjunk = S.tile()
                units = [dict(h=h, i=i, z=z) for h in range(4) for i in range(8) for z in range(2)]

                def KT_of(u, kb):
                    r0 = 64 * u["z"]
                    return KT[r0:r0 + 64, u["h"], kb * 128:(kb + 1) * 128], TKT[u["h"]][kb // 4]

                def QT_of(u):
                    r0 = 64 * u["z"]
                    return QT[r0:r0 + 64, u["h"], u["i"] * 256:(u["i"] + 1) * 256], TQT[u["h"]][u["i"] // 2]

                def V_of(u, kb):
                    return Vd[:, kb, u["h"], 0:128], TV[kb]

                def exp_emit(u, p, bi, PTb, Tp):
                    S.op("scalar", lambda e: e.activation(out=PTb[:, 2 * p:2 * p + 2, :].rearrange("p a b -> p (a b)"),
                                                          in_=banks[bi][:], func=AF.Exp),
                         reads=[Tb[bi]], writes=[Tp])

                def mask_emit(u, PTb, TPb):
                    i = u["i"]
                    lo = nk_of(i) - 4
                    S.op("vector", lambda e: e.tensor_tensor(out=PTb[:, lo:lo + 4, :], in0=PTb[:, lo:lo + 4, :],
                                                             in1=maskd[:, i % 2, :].rearrange("p (a b) -> p a b", a=4), op=ALU.min),
                         reads=[Tmd, TPb[lo // 2], TPb[lo // 2 + 1]], writes=[TPb[lo // 2], TPb[lo // 2 + 1]])

                def evac_emit(u, ob):
                    h, i, z = u["h"], u["i"], u["z"]
                    for s in range(2):
                        qb = 2 * i + s
                        o_ap = banks[ob][:, s * 129:s * 129 + 128]
                        sm_ap = banks[ob][:, s * 129 + 128:s * 129 + 129]
                        ri = 2 * z + s
                        S.op("vector", lambda e, ri=ri, sm_ap=sm_ap: e.reciprocal(out=rr[:, ri:ri + 1], in_=sm_ap),
                             reads=[Tb[ob]], writes=[Trr[ri]])
                        if z == 0:
                            S.op("vector", lambda e, ri=ri, o_ap=o_ap, s=s: e.tensor_scalar(
                                out=A1[s][:], in0=o_ap, scalar1=rr[:, ri:ri + 1], scalar2=None, op0=ALU.mult),
                                reads=[Tb[ob], Trr[ri]], writes=[TA1[s]])
                        else:
                            S.op("vector", lambda e, ri=ri: e.tensor_tensor(out=rr[:, ri:ri + 1], in0=rr[:, ri:ri + 1], in1=neglam[:], op=ALU.mult),
                                 reads=[Trr[ri], Tnl], writes=[Trr[ri]])
                            S.op("vector", lambda e, ri=ri, o_ap=o_ap, s=s, qb=qb: e.scalar_tensor_tensor(
                                out=oc[:, qb, h, :], in0=o_ap, scalar=rr[:, ri:ri + 1], in1=A1[s][:], op0=ALU.mult, op1=ALU.add),
                                reads=[Tb[ob], Trr[ri], TA1[s]], writes=[Toc[qb][h]])
                            S.op("vector", lambda e, qb=qb: e.scalar_tensor_tensor(
                                out=sjunk[:], in0=oc[:, qb, h, :], scalar=1.0, in1=oc[:, qb, h, :], op0=ALU.mult, op1=ALU.mult,
                                accum_out=ssq[:, qb * 4 + h:qb * 4 + h + 1]),
                                reads=[Toc[qb][h], Tssq], writes=[Tssq, Tsjunk])

                OTs = [sb(at, "OTs%d" % i, [128, 256], F32) for i in range(2)]; TOTs = [S.tile() for _ in range(2)]
                smr = [sb(at, "smr%d" % i, [1, 256], F32) for i in range(2)]; Tsmr = [S.tile() for _ in range(2)]
                identf_a = sb(at, "identf_a", [128, 128], F32); Tidf_a = S.tile()
                S.dma("sync", identf_a[:], identf_d, writes=[Tidf_a])

                def Vsum_of(u, kb):
                    return Vd[:, kb, u["h"], 128:129], TV[kb]
                run_attention(units, PT, TPT, KT_of, QT_of, V_of, 129, exp_emit, evac_emit, mask_emit,
                              s_banks=[0, 1, 2, 3], o_banks=[4, 5, 6, 7], OTs=OTs, TOTs=TOTs, smr=smr, Tsmr=Tsmr,
                              identf=identf_a, Tidf=Tidf_a, Vsum_of=Vsum_of)
                S.op("scalar", lambda e: e.activation(out=ssq[:], in_=ssq[:], func=AF.Sqrt, scale=1.0 / 128, bias=EPS),
                     reads=[Tssq], writes=[Tssq])
                S.op("vector", lambda e: e.reciprocal(out=ssq[:], in_=ssq[:]), reads=[Tssq], writes=[Tssq])
                Otok = [sb(at, "Otok%d" % i, [128, 512], BF16) for i in range(2)]; TOtok = [S.tile() for _ in range(2)]
                for qb in range(16):
                    o2 = qb % 2
                    for h in range(4):
                        S.op("vector", lambda e, qb=qb, h=h, o2=o2: e.scalar_tensor_tensor(
                            out=Otok[o2][:, h * 128:(h + 1) * 128], in0=oc[:, qb, h, :], scalar=ssq[:, qb * 4 + h:qb * 4 + h + 1],
                            in1=gsc[:], op0=ALU.mult, op1=ALU.mult),
                            reads=[Toc[qb][h], Tssq, Tgsc], writes=[TOtok[o2]])
                    bi = o2
                    pTv = banks[bi][:].bitcast(BF16)
                    for c in range(4):
                        S.op("tensor", lambda e, c=c, o2=o2, pTv=pTv: e.transpose(
                            out=pTv[:, c * 128:(c + 1) * 128], in_=Otok[o2][:, c * 128:(c + 1) * 128], identity=identb[:]),
                            reads=[TOtok[o2], Tidb], writes=[Tb[bi]], signal=(c == 3))
                    S.op("vector", lambda e, qb=qb, pTv=pTv: e.tensor_copy(
                        out=OT[:, 0:4, qb * 128:(qb + 1) * 128], in_=pTv[:, 0:512].rearrange("p (c t) -> p c t", c=4)),
                        reads=[Tb[bi]], writes=[TOT[qb]])
                S.barrier()
                if "OTa" in debug:
                    dbg_out("OTa", [128, 8, 2048], BF16); dump("OTa", OT[:], []); S.barrier()
                S.emit()
        if stop_after == "A":
            return nc

        with ExitStack() as pb:
            KT = sb(pb, "KTf", [128, 4, 4096], BF16)
            TKT = [[S.tile() for _ in range(8)] for _ in range(4)]
            QT = sb(pb, "QTf", [128, 4, 2048], BF16)
            TQT = [[S.tile() for _ in range(4)] for _ in range(4)]
            Vf = sb(pb, "Vf", [128, 32, 8, 66], BF16)
            TV = [S.tile() for _ in range(32)]
            S.op("vector", lambda e: e.memset(Vf[:, :, :, 64:66], 1.0), writes=TV)
            zt = sb(pb, "zt", [128, 32, 8], F32); Tzt = [S.tile() for _ in range(32)]
            Fpos = sb(pb, "Fpos", [128, 32, 8], F32); TF = [S.tile() for _ in range(32)]
            Cpos = sb(pb, "Cpos", [128, 33, 8], F32); TC = [S.tile() for _ in range(33)]
            maskf = sb(pb, "maskf", [128, 2, 1024], BF16); Tmf = S.tile()
            S.dma("sync", maskf[:], maskf_d, writes=[Tmf])
            bfb = sb(pb, "bfb", [128, 8], F32); Tbfb = S.tile()
            bcast_load(bfb[:], bfor, Tbfb, 8)
            csel = sb(pb, "csel", [128, 8, 33], F32); Tcsel = S.tile()
            bcast_load(csel[:].rearrange("p a b -> p (a b)"), csel_d, Tcsel, 8 * 33)
            ctmp = sb(pb, "ctmp", [128, 8, 33], F32); Tctmp = S.tile()
            cq = sb(pb, "cq", [128, 8, 8], F32); Tcq = S.tile()
            tri = sb(pb, "tri", [128, 128], F32); Ttri = S.tile()
            S.dma("sync", tri[:], tri_d, writes=[Ttri])
            onesf = sb(pb, "onesf", [128, 128], F32); Tones = S.tile()
            S.dma("sync", onesf[:], onesf_d, writes=[Tones])

            with ExitStack() as sw:
                st = make_sweep(sw, "b", [0, 1])
                wA = sb(sw, "wA", [128, 8, 512], BF16); TwA = S.tile()
                wB = sb(sw, "wB", [128, 8, 512], BF16); TwB = S.tile()
                wF = sb(sw, "wF", [128, 8, 8], BF16); TwF = S.tile()
                wload(wA[:], w_in[:, 2048:2560], TwA)
                wload(wB[:], w_in[:, 2560:3072], TwB)
                wload(wF[:], w_in[:, 3072:3080], TwF)
                kctr = [0]

                def plain_proj(blk, hT, ThT, w_, Tw_, dstT, TdstT, scale, slots=()):
                    for hp in range(4):
                        bk = 2 + (kctr[0] % 3)
                        kctr[0] += 1
                        mmgroup(banks[bk][:], [(w_[:, c, hp * 128:(hp + 1) * 128], hT[:, c, :]) for c in range(8)],
                                reads=list(ThT) + [Tw_], writes=[Tb[bk]])
                        S.op("scalar", lambda e, bk=bk, hp=hp: e.activation(
                            out=dstT[:, hp, blk * 512:(blk + 1) * 512], in_=banks[bk][:], func=AF.Copy, scale=scale),
                            reads=[Tb[bk]], writes=[TdstT[hp][blk]])
                        if hp < len(slots):
                            slots[hp]()

                def kvf_proj(blk, hT, ThT, slots=()):
                    plain_proj(blk, hT, ThT, wA, TwA, KT, TKT, 1.0, slots)
                    for tt in range(4):
                        n = blk * 4 + tt
                        bv = 6 + (tt % 2)
                        mmgroup(banks[bv][:], [(hT[:, c, tt * 128:(tt + 1) * 128], wB[:, c, :]) for c in range(8)],
                                reads=[ThT[tt], TwB], writes=[Tb[bv]])
                        S.op("vector", lambda e, n=n, bv=bv: e.tensor_copy(
                            out=Vf[:, n, :, 0:64], in_=banks[bv][:].rearrange("p (h d) -> p h d", h=8)),
                            reads=[Tb[bv]], writes=[TV[n]])
                        mmgroup(banks[5][:, 0:8], [(hT[:, c, tt * 128:(tt + 1) * 128], wF[:, c, :]) for c in range(8)],
                                reads=[ThT[tt], TwF], writes=[Tb[5]])
                        S.op("vector", lambda e, n=n: e.tensor_tensor(out=zt[:, n, :], in0=banks[5][:, 0:8], in1=bfb[:], op=ALU.add),
                             reads=[Tb[5], Tbfb], writes=[Tzt[n]])

                sweep_stage1(st, xb, 0, gb_attn, Tgba)
                sweep_stage1(st, xb, 1, gb_attn, Tgba)
                for blk in range(8):
                    sl = sweep_stage1(st, xb, blk + 2, gb_attn, Tgba, deferred=True) if blk + 2 < 8 else []
                    kvf_proj(blk, st["hT"][blk % 3], st["ThT"][blk % 3], sl)
                wload(wA[:], w_in[:, 1536:2048], TwA)
                sweep_stage1(st, xo, 0, gb_attn, Tgba)
                sweep_stage1(st, xo, 1, gb_attn, Tgba)
                for blk in range(4):
                    sl = sweep_stage1(st, xo, blk + 2, gb_attn, Tgba, deferred=True) if blk + 2 < 4 else []
                    plain_proj(blk, st["hT"][blk % 3], st["ThT"][blk % 3], wA, TwA, QT, TQT, 0.125, sl)
                ztf = zt[:].rearrange("p a b -> p (a b)")
                S.op("scalar", lambda e: e.activation(out=ztf, in_=ztf, func=AF.Exp, scale=-1.0), reads=Tzt, writes=Tzt)
                S.op("scalar", lambda e: e.activation(out=ztf, in_=ztf, func=AF.Ln, bias=1.0), reads=Tzt, writes=Tzt)
                S.op("vector", lambda e: e.memset(Cpos[:, 0, :], 0.0), writes=[TC[0]])
                for n in range(32):
                    bc = 2 + (n % 2)
                    S.op("tensor", lambda e, n=n, bc=bc: e.matmul(out=banks[bc][:, 0:8], lhsT=tri[:], rhs=zt[:, n, :], start=True, stop=True),
                         reads=[Ttri, Tzt[n]], writes=[Tb[bc]], signal=False)
                    S.op("tensor", lambda e, n=n, bc=bc: e.matmul(out=banks[bc][:, 8:16], lhsT=onesf[:], rhs=zt[:, n, :], start=True, stop=True),
                         reads=[Tones, Tzt[n]], writes=[Tb[bc]], signal=True)
                    S.op("vector", lambda e, n=n, bc=bc: e.tensor_tensor(out=Fpos[:, n, :], in0=banks[bc][:, 0:8], in1=Cpos[:, n, :], op=ALU.add),
                         reads=[Tb[bc], TC[n]], writes=[TF[n]])
                    S.op("vector", lambda e, n=n, bc=bc: e.tensor_tensor(out=Cpos[:, n + 1, :], in0=banks[bc][:, 8:16], in1=Cpos[:, n, :], op=ALU.add),
                         reads=[Tb[bc], TC[n]], writes=[TC[n + 1]])
                for i in range(8):
                    S.op("vector", lambda e, i=i: e.tensor_tensor(out=ctmp[:], in0=Cpos[:].rearrange("p n h -> p h n"),
                                                                  in1=csel[:, i, :].unsqueeze(1).broadcast_to([128, 8, 33]), op=ALU.mult),
                         reads=TC + [Tcsel, Tctmp], writes=[Tctmp])
                    S.op("vector", lambda e, i=i: e.tensor_reduce(out=cq[:, i, :], in_=ctmp[:], axis=AX.X, op=ALU.add),
                         reads=[Tctmp], writes=[Tcq])
                S.barrier()
                if "Fpos" in debug:
                    dbg_out("Fpos", [128, 32, 8]); dump("Fpos", Fpos[:], []); S.barrier()
                S.emit()

            with ExitStack() as at:
                PT = [sb(at, "PT%d" % i, [128, 32, 256], BF16) for i in range(2)]
                TPT = [[S.tile() for _ in range(16)] for _ in range(2)]
                Otf = sb(at, "Otf", [128, 16, 512], BF16); TOtf = [S.tile() for _ in range(16)]
                rr = sb(at, "rrf", [128, 2], F32); Trr = [S.tile() for _ in range(2)]
                biasb = [sb(at, "biasb%d" % i, [128, 32], F32) for i in range(2)]; Tbias = [S.tile() for _ in range(2)]
                units = [dict(hp=hp, hh=hh, i=i, head=2 * hp + hh) for hp in range(4) for hh in range(2) for i in range(8)]
                for ui, u in enumerate(units):
                    u["ui"] = ui

                def KT_of(u, kb):
                    r0 = 64 * u["hh"]
                    return KT[r0:r0 + 64, u["hp"], kb * 128:(kb + 1) * 128], TKT[u["hp"]][kb // 4]

                def QT_of(u):
                    r0 = 64 * u["hh"]
                    return QT[r0:r0 + 64, u["hp"], u["i"] * 256:(u["i"] + 1) * 256], TQT[u["hp"]][u["i"] // 2]

                ebb = [sb(at, "ebb%d" % i, [128, 32], F32) for i in range(2)]; Teb = [S.tile() for _ in range(2)]
                Vp = [sb(at, "Vp%d" % i, [128, 32, 65], BF16) for i in range(2)]; TVp = [S.tile() for _ in range(2)]

                def V_of(u, kb):
                    return Vp[u["ui"] % 2][:, kb, :], TVp[u["ui"] % 2]

                def exp_emit(u, p, bi, PTb, Tp):
                    b2 = u["ui"] % 2
                    hd = u["head"]
                    nk = nk_of(u["i"])
                    if p == 0:
                        S.op("vector", lambda e: e.tensor_scalar(out=biasb[b2][:, 0:nk], in0=Fpos[:, 0:nk, hd], scalar1=cq[:, u["i"], hd:hd + 1],
                                                                 scalar2=70.0, op0=ALU.subtract, op1=ALU.min),
                             reads=TF[0:nk] + [Tcq], writes=[Tbias[b2]])
                        S.op("scalar", lambda e: e.activation(out=ebb[b2][:, 0:nk], in_=biasb[b2][:, 0:nk], func=AF.Exp),
                             reads=[Tbias[b2]], writes=[Teb[b2]])
                        S.op("vector", lambda e: e.tensor_tensor(out=Vp[b2][:, 0:nk, :], in0=Vf[:, 0:nk, hd, 0:65],
                                                                 in1=ebb[b2][:, 0:nk].unsqueeze(2).broadcast_to([128, nk, 65]), op=ALU.mult),
                             reads=TV[0:nk] + [Teb[b2]], writes=[TVp[b2]])
                    S.op("scalar", lambda e: e.activation(out=PTb[:, 2 * p:2 * p + 2, :].rearrange("p a b -> p (a b)"),
                                                          in_=banks[bi][:], func=AF.Exp),
                         reads=[Tb[bi]], writes=[Tp])

                def mask_emit(u, PTb, TPb):
                    i = u["i"]
                    lo = nk_of(i) - 4
                    S.op("vector", lambda e: e.tensor_tensor(out=PTb[:, lo:lo + 4, :], in0=PTb[:, lo:lo + 4, :],
                                                             in1=maskf[:, i % 2, :].rearrange("p (a b) -> p a b", a=4), op=ALU.min),
                         reads=[Tmf, TPb[lo // 2], TPb[lo // 2 + 1]], writes=[TPb[lo // 2], TPb[lo // 2 + 1]])

                def evac_emit(u, ob):
                    hd, i = u["head"], u["i"]
                    for s in range(2):
                        qb = 2 * i + s
                        S.op("vector", lambda e, s=s: e.reciprocal(out=rr[:, s:s + 1], in_=banks[ob][:, s * 65 + 64:s * 65 + 65]),
                             reads=[Tb[ob]], writes=[Trr[s]])
                        S.op("vector", lambda e, s=s, qb=qb: e.tensor_scalar(
                            out=Otf[:, qb, hd * 64:(hd + 1) * 64], in0=banks[ob][:, s * 65:s * 65 + 64], scalar1=rr[:, s:s + 1],
                            scalar2=None, op0=ALU.mult),
                            reads=[Tb[ob], Trr[s]], writes=[TOtf[qb]])

                OTs = [sb(at, "OTsf%d" % i, [128, 256], F32) for i in range(2)]; TOTs = [S.tile() for _ in range(2)]
                identf_b = sb(at, "identf_b", [128, 128], F32); Tidf_b = S.tile()
                S.dma("sync", identf_b[:], identf_d, writes=[Tidf_b])
                run_attention(units, PT, TPT, KT_of, QT_of, V_of, 65, exp_emit, evac_emit, mask_emit,
                              s_banks=[0, 1, 2, 3], o_banks=[4, 5, 6, 7], OTs=OTs, TOTs=TOTs, identf=identf_b, Tidf=Tidf_b)
                for qb in range(16):
                    bi = qb % 2
                    pTv = banks[bi][:].bitcast(BF16)
                    for c in range(4):
                        S.op("tensor", lambda e, c=c, qb=qb, pTv=pTv: e.transpose(
                            out=pTv[:, c * 128:(c + 1) * 128], in_=Otf[:, qb, c * 128:(c + 1) * 128], identity=identb[:]),
                            reads=[TOtf[qb], Tidb], writes=[Tb[bi]], signal=(c == 3))
                    S.op("vector", lambda e, qb=qb, pTv=pTv: e.tensor_copy(
                        out=OT[:, 4:8, qb * 128:(qb + 1) * 128], in_=pTv[:, 0:512].rearrange("p (c t) -> p c t", c=4)),
                        reads=[Tb[bi]], writes=[TOT[qb]])
                S.barrier()
                if "OTb" in debug:
                    dbg_out("OTb", [128, 8, 2048], BF16); dump("OTb", OT[:], []); S.barrier()
                S.emit()
        if stop_after == "B":
            return nc

        with ExitStack() as pc:
            x2 = sb(pc, "x2", [128, 16, 1024], F32); Tx2 = [[S.tile() for _ in range(2)] for _ in range(16)]
            hmT = OT
            ThmT = TOT
            ovf = sb(pc, "ovf", [128, 1], I32); Tovf = S.tile()
            w12 = sb(pc, "w12", [128, 2, 16], F32); Tw12 = S.tile()
            pos = sb(pc, "pos", [128, 2, 16], I32); Tpos = S.tile()
            comb = sb(pc, "comb", [128, 16, 16], F32); Tcomb = S.tile()
            junk = sb(pc, "junkc", [128, 1024], BF16); Tjunkc = S.tile()
            ssc = sb(pc, "ssc", [128, 16], F32); Tssc = [S.tile() for _ in range(16)]; Tsscall = S.tile()
            with ExitStack() as c1:
                wo = sb(c1, "wo", [128, 8, 1024], BF16); Two = S.tile()
                wload(wo[:], w_out, Two)
                xt = [sb(c1, "xc%d" % i, [128, 1024], F32) for i in range(2)]; Txt = [S.tile() for _ in range(2)]
                rw32 = sb(c1, "rw32", [128, 8, 20], F32); Trw = S.tile()
                S.dma("sync", rw32[:], rw_d.rearrange("(c p) n -> p c n", p=128), writes=[Trw])
                rbb = sb(c1, "rbb", [128, 20], F32); Trbb = S.tile()
                bcast_load(rbb[:], rb_d, Trbb, 20)
                gbf = sb(c1, "gbf", [128, 1024], F32); Tgbf = S.tile()
                bcast_load(gbf[:], g_ffn, Tgbf, 1024)
                identf = sb(c1, "identf", [128, 128], F32); Tidf = S.tile()
                S.dma("sync", identf[:], identf_d, writes=[Tidf])
                hm32 = [sb(c1, "hm32%d" % i, [128, 1024], F32) for i in range(2)]; Thm32 = [S.tile() for _ in range(2)]
                hmT32 = [sb(c1, "hmT32%d" % i, [128, 8, 128], F32) for i in range(2)]; ThmT32 = [S.tile() for _ in range(2)]
                Lall = sb(c1, "Lall", [128, 16, 20], F32); TL = [S.tile() for _ in range(16)]
                if moe == "sparse":
                    hmb = sb(c1, "hmb", [128, 16, 1024], BF16); Thmb = [S.tile() for _ in range(16)]
                for t in range(16):
                    S.dma("sync", xt[t % 2][:], xo[t * 128:(t + 1) * 128, :], writes=[Txt[t % 2]])
                    for hf in range(2):
                        mmgroup(banks[hf][:], [(OT[:, c, t * 128:(t + 1) * 128], wo[:, c, hf * 512:(hf + 1) * 512]) for c in range(8)],
                                reads=[TOT[t], Two], writes=[Tb[hf]])
                        S.op("vector", lambda e, t=t, hf=hf: e.tensor_tensor(
                            out=x2[:, t, hf * 512:(hf + 1) * 512], in0=banks[hf][:], in1=xt[t % 2][:, hf * 512:(hf + 1) * 512], op=ALU.add),
                            reads=[Tb[hf], Txt[t % 2]], writes=[Tx2[t][hf]])
                    S.op("scalar", lambda e, t=t: e.activation(out=junk[:], in_=x2[:, t, :], func=AF.Square, accum_out=ssc[:, t:t + 1]),
                         reads=Tx2[t], writes=[Tssc[t], Tjunkc])
                if "x2" in debug:
                    dbg_out("x2", [2048, 1024])
                    for t in range(16):
                        dump("x2", x2[:, t, :], Tx2[t]) if False else S.dma("sync", dbg["x2"][t * 128:(t + 1) * 128, :], x2[:, t, :], reads=Tx2[t], writes=[Tdbg], semtile=Tdbg)
                if stop_after == "C0":
                    S.barrier(); S.emit()
                    return nc
                S.op("scalar", lambda e: e.activation(out=ssc[:], in_=ssc[:], func=AF.Sqrt, scale=1.0 / 1024, bias=EPS),
                     reads=Tssc, writes=[Tsscall])
                S.op("vector", lambda e: e.reciprocal(out=ssc[:], in_=ssc[:]), reads=[Tsscall], writes=[Tsscall])
                def hm_front(t):
                    t2 = t % 2
                    S.op("vector", lambda e, t=t, t2=t2: e.scalar_tensor_tensor(
                        out=hm32[t2][:], in0=x2[:, t, :], scalar=ssc[:, t:t + 1], in1=gbf[:], op0=ALU.mult, op1=ALU.mult),
                        reads=Tx2[t] + [Tsscall, Tgbf], writes=[Thm32[t2]])
                    ba, bb = (2, 3) if t2 == 0 else (4, 5)
                    for c in range(8):
                        bk = ba if c < 4 else bb
                        S.op("tensor", lambda e, c=c, t2=t2, bk=bk: e.transpose(
                            out=banks[bk][:, (c % 4) * 128:(c % 4 + 1) * 128], in_=hm32[t2][:, c * 128:(c + 1) * 128], identity=identf[:]),
                            reads=[Thm32[t2], Tidf], writes=[Tb[bk]], signal=(c % 4 == 3))

                def hm_back(t):
                    t2 = t % 2
                    ba, bb = (2, 3) if t2 == 0 else (4, 5)
                    for k, bk in enumerate((ba, bb)):
                        src = banks[bk][:].rearrange("p (c t) -> p c t", c=4)
                        S.op("scalar", lambda e, k=k, t2=t2, src=src: e.activation(out=hmT32[t2][:, 4 * k:4 * k + 4, :], in_=src, func=AF.Copy),
                             reads=[Tb[bk]], writes=[ThmT32[t2]])
                        S.op("vector", lambda e, k=k, t=t, t2=t2: e.tensor_copy(out=hmT[:, 4 * k:4 * k + 4, t * 128:(t + 1) * 128], in_=hmT32[t2][:, 4 * k:4 * k + 4, :]),
                             reads=[ThmT32[t2]], writes=[ThmT[t]])
                    if moe == "sparse":
                        S.op("scalar", lambda e, t=t, t2=t2: e.activation(out=hmb[:, t, :], in_=hm32[t2][:], func=AF.Copy), reads=[Thm32[t2]], writes=[Thmb[t]])

                def hm_router(t):
                    t2 = t % 2
                    br = 6 + t2
                    mmgroup(banks[br][:, 0:20], [(hmT32[t2][:, c, :], rw32[:, c, :]) for c in range(8)],
                            reads=[ThmT32[t2], Trw], writes=[Tb[br]])
                    S.op("vector", lambda e, t=t, br=br: e.tensor_tensor(out=Lall[:, t, :], in0=banks[br][:, 0:20], in1=rbb[:], op=ALU.add),
                         reads=[Tb[br], Trbb], writes=[TL[t]])
                hm_front(0)
                for t in range(16):
                    hm_back(t)
                    if t + 1 < 16:
                        hm_front(t + 1)
                    hm_router(t)
                if stop_after == "C1a":
                    if "Lall" in debug:
                        dbg_out("Lall", [128, 320])
                        S.dma("sync", dbg["Lall"], Lall[:].rearrange("p a b -> p (a b)"), reads=[], writes=[Tdbg], semtile=Tdbg)
                    S.barrier(); S.emit()
                    return nc
                TR = S.tile()

                def rt(name, shape):
                    return sb(c1, "rt_" + name, shape, F32)
                gmax = rt("gmax", [128, 16]); gm = rt("gm", [128, 16, 4]); gd = rt("gd", [128, 16, 4])
                gsum = rt("gsum", [128, 16]); gw = rt("gw", [128, 16]); pen = rt("pen", [128, 16, 4])
                EL = rt("EL", [128, 16, 16]); EL2 = rt("EL2", [128, 16, 16]); m1 = rt("m1", [128, 16]); m2 = rt("m2", [128, 16])
                oh1 = rt("oh1", [128, 16, 16]); oh2 = rt("oh2", [128, 16, 16]); dd = rt("dd", [128, 16]); w1 = rt("w1", [128, 16]); w2 = rt("w2", [128, 16])
                LG = Lall[:, :, 0:4]
                LE4 = Lall[:, :, 4:20].rearrange("p t (g e) -> p t g e", g=4)
                EL4 = EL[:].rearrange("p t (g e) -> p t g e", g=4)

                def vop(fn, first=False):
                    S.op("vector", fn, reads=(TL + [TR]) if first else [TR], writes=[TR])

                def bc3(a, n):
                    return a[:].unsqueeze(2).broadcast_to([128, 16, n])
                vop(lambda e: e.tensor_reduce(out=gmax[:], in_=LG, axis=AX.X, op=ALU.max), first=True)
                vop(lambda e: e.tensor_tensor(out=gm[:], in0=LG, in1=bc3(gmax, 4), op=ALU.is_equal))
                vop(lambda e: e.tensor_tensor(out=gd[:], in0=LG, in1=bc3(gmax, 4), op=ALU.subtract))
                S.op("scalar", lambda e: e.activation(out=gd[:], in_=gd[:], func=AF.Exp), reads=[TR], writes=[TR])
                vop(lambda e: e.tensor_reduce(out=gsum[:], in_=gd[:], axis=AX.X, op=ALU.add))
                vop(lambda e: e.reciprocal(out=gw[:], in_=gsum[:]))
                vop(lambda e: e.tensor_scalar(out=pen[:], in0=gm[:], scalar1=1.0, scalar2=1e30, op0=ALU.subtract, op1=ALU.mult))
                vop(lambda e: e.tensor_tensor(out=EL4, in0=LE4, in1=gm[:].unsqueeze(3).broadcast_to([128, 16, 4, 4]), op=ALU.mult))
                vop(lambda e: e.tensor_tensor(out=EL4, in0=EL4, in1=pen[:].unsqueeze(3).broadcast_to([128, 16, 4, 4]), op=ALU.add))
                vop(lambda e: e.tensor_reduce(out=m1[:], in_=EL[:], axis=AX.X, op=ALU.max))
                vop(lambda e: e.tensor_tensor(out=oh1[:], in0=EL[:], in1=bc3(m1, 16), op=ALU.is_equal))
                vop(lambda e: e.scalar_tensor_tensor(out=EL2[:], in0=oh1[:], scalar=-1e30, in1=EL[:], op0=ALU.mult, op1=ALU.add))
                vop(lambda e: e.tensor_reduce(out=m2[:], in_=EL2[:], axis=AX.X, op=ALU.max))
                vop(lambda e: e.tensor_tensor(out=oh2[:], in0=EL2[:], in1=bc3(m2, 16), op=ALU.is_equal))
                vop(lambda e: e.tensor_tensor(out=dd[:], in0=m2[:], in1=m1[:], op=ALU.subtract))
                S.op("scalar", lambda e: e.activation(out=dd[:], in_=dd[:], func=AF.Exp), reads=[TR], writes=[TR])
                vop(lambda e: e.tensor_scalar(out=w1[:], in0=dd[:], scalar1=1.0, scalar2=None, op0=ALU.add))
                vop(lambda e: e.reciprocal(out=w1[:], in_=w1[:]))
                vop(lambda e: e.tensor_tensor(out=w1[:], in0=w1[:], in1=gw[:], op=ALU.mult))
                vop(lambda e: e.tensor_tensor(out=w2[:], in0=dd[:], in1=w1[:], op=ALU.mult))
                if moe == "sparse":
                    Mb = sb(c1, "Mb", [128, 16, 16], BF16)
                    ustrict = sb(c1, "ustrict", [128, 128], BF16); Tus = S.tile()
                    S.dma("sync", ustrict[:], ustrict_d, writes=[Tus])
                    onesb = sb(c1, "onesb", [128, 128], BF16); Tob_ = S.tile()
                    S.dma("sync", onesb[:], onesb_d, writes=[Tob_])
                    ebase = sb(c1, "ebase", [128, 16, 16], F32); Teb = S.tile()
                    S.dma("sync", ebase[:].rearrange("p a b -> p (a b)"), ebase_d, writes=[Teb])
                    slotf = rt("slotf", [128, 16, 16]); okf = rt("okf", [128, 16, 16]); posf = rt("posf", [128, 2, 16])
                    vop(lambda e: e.tensor_tensor(out=Mb[:], in0=oh1[:], in1=oh2[:], op=ALU.add))
                    for t in range(16):
                        prs = [(onesb[:], Mb[:, tp, :]) for tp in range(t)] + [(ustrict[:], Mb[:, t, :])]
                        n_ = len(prs)
                        for k_, (l_, r_) in enumerate(prs):
                            S.op("tensor", lambda e, l_=l_, r_=r_, k_=k_, n_=n_, t=t: e.matmul(out=banks[0][:, t * 16:(t + 1) * 16], lhsT=l_, rhs=r_,
                                                                                         start=(k_ == 0), stop=(k_ == n_ - 1)),
                                 reads=[TR, Tus, Tob_], writes=[Tb[0]], signal=(k_ == n_ - 1))
                    for tp in range(16):
                        S.op("tensor", lambda e, tp=tp: e.matmul(out=banks[1][:, 0:16], lhsT=onesb[:], rhs=Mb[:, tp, :], start=(tp == 0), stop=(tp == 15)),
                             reads=[TR, Tob_], writes=[Tb[1]], signal=(tp == 15))
                    cmax = rt("cmax", [128, 1])
                    S.op("vector", lambda e: e.tensor_reduce(out=cmax[:], in_=banks[1][:, 0:16], axis=AX.X, op=ALU.max), reads=[Tb[1], TR], writes=[TR])
                    import os as _os
                    thr = -1.0 if _os.environ.get("FORCE_DENSE") else float(CAP)
                    vop(lambda e: e.tensor_scalar(out=cmax[:], in0=cmax[:], scalar1=thr, scalar2=None, op0=ALU.is_gt))
                    S.op("vector", lambda e: e.tensor_copy(out=ovf[:], in_=cmax[:]), reads=[TR], writes=[Tovf])
                    rank = banks[0][:, 0:256].rearrange("p (a b) -> p a b", a=16)
                    S.op("vector", lambda e: e.tensor_tensor(out=slotf[:], in0=rank, in1=ebase[:], op=ALU.add), reads=[Tb[0], Teb, TR], writes=[TR])
                    vop(lambda e: e.tensor_scalar(out=okf[:], in0=slotf[:], scalar1=None, scalar2=None, op0=ALU.bypass) if False else
                        e.tensor_tensor(out=okf[:], in0=slotf[:], in1=ebase[:], op=ALU.subtract))
                    vop(lambda e: e.tensor_scalar(out=okf[:], in0=okf[:], scalar1=float(CAP), scalar2=1.0e6, op0=ALU.is_ge, op1=ALU.mult))
                    vop(lambda e: e.tensor_tensor(out=slotf[:], in0=slotf[:], in1=okf[:], op=ALU.add))
                    vop(lambda e: e.tensor_tensor(out=okf[:], in0=slotf[:], in1=oh1[:], op=ALU.mult))
                    vop(lambda e: e.tensor_reduce(out=posf[:, 0, :], in_=okf[:], axis=AX.X, op=ALU.add))
                    vop(lambda e: e.tensor_tensor(out=okf[:], in0=slotf[:], in1=oh2[:], op=ALU.mult))
                    vop(lambda e: e.tensor_reduce(out=posf[:, 1, :], in_=okf[:], axis=AX.X, op=ALU.add))
                    S.op("vector", lambda e: e.tensor_copy(out=pos[:], in_=posf[:]), reads=[TR], writes=[Tpos])
                    S.op("vector", lambda e: e.tensor_copy(out=w12[:, 0, :], in_=w1[:]), reads=[TR, Tw12], writes=[Tw12])
                    S.op("vector", lambda e: e.tensor_copy(out=w12[:, 1, :], in_=w2[:]), reads=[TR, Tw12], writes=[Tw12])
                    Tsc = [S.tile() for _ in range(32)]
                    set_bcreg()
                    for t in range(16):
                        for k_ in range(2):
                            S.dma_fn("gpsimd", lambda e, t=t, k_=k_: e.indirect_dma_start(
                                out=xs_d[:, :], out_offset=bass.IndirectOffsetOnAxis(ap=pos[:, k_, t:t + 1], axis=0),
                                in_=hmb[:, t, :], in_offset=None, bounds_check=bcreg, oob_is_err=False),
                                reads=[Thmb[t], Tpos, Txs], writes=[Tsc[2 * t + k_]], semtile=Thmb[t])
                vop(lambda e: e.tensor_tensor(out=oh1[:], in0=oh1[:], in1=bc3(w1, 16), op=ALU.mult))
                vop(lambda e: e.tensor_tensor(out=oh2[:], in0=oh2[:], in1=bc3(w2, 16), op=ALU.mult))
                S.op("vector", lambda e: e.tensor_tensor(out=comb[:], in0=oh1[:], in1=oh2[:], op=ALU.add), reads=[TR], writes=[Tcomb])
                S.barrier()
                if "comb" in debug:
                    dbg_out("comb", [128, 256])
                    S.dma("sync", dbg["comb"], comb[:].rearrange("p a b -> p (a b)"), reads=[Tcomb], writes=[Tdbg], semtile=Tdbg)
                    S.barrier()
                S.emit()
            if stop_after == "C1":
                return nc

            with ExitStack() as c2:
                wgb = [sb(c2, "wgb%d" % i, [128, 8, 512], BF16) for i in range(2)]; Twg4 = [[S.tile() for _ in range(4)] for _ in range(2)]
                wub = [sb(c2, "wub%d" % i, [128, 8, 512], BF16) for i in range(2)]; Twu4 = [[S.tile() for _ in range(4)] for _ in range(2)]
                wdb = [sb(c2, "wdb%d" % i, [128, 4, 1024], BF16) for i in range(2)]; Twd4 = [[S.tile() for _ in range(4)] for _ in range(2)]
                stg = [sb(c2, "stg%d" % i, [128, 1024], F32) for i in range(3)]; Tstg = [S.tile() for _ in range(3)]
                sq_ = [0]
                aT = [sb(c2, "aT%d" % i, [128, 4, 512], BF16) for i in range(2)]; TaT = [[S.tile() for _ in range(4)] for _ in range(2)]
                sg = [sb(c2, "sg%d" % i, [128, 512], F32) for i in range(2)]; Tsg = [S.tile() for _ in range(2)]
                xg = [sb(c2, "xg%d" % i, [128, 1024], BF16) for i in range(4)]; Txg = [S.tile() for _ in range(4)]
                xgT = [sb(c2, "xgT%d" % i, [128, 8, CAP], BF16) for i in range(2)]; TxgT = [[S.tile() for _ in range(CAP // 128)] for _ in range(2)]
                ysb = [sb(c2, "ysb%d" % i, [128, 1024], F32) for i in range(2)]; Tysb = [S.tile() for _ in range(2)]
                NJ = CAP // 128
                Tys = [S.tile() for _ in range(NEXP * NJ)]

                def w_steps(ex):
                    b2 = ex % 2
                    dmas, casts = [], []
                    for k in range(12):
                        def mk(k=k):
                            if k < 8:
                                srcw = (wg_d if k < 4 else wu_d)[ex]
                                kk = k % 4
                                src = srcw[kk * 256:(kk + 1) * 256, :].rearrange("(c p) n -> p c n", p=128)
                                dst_of = lambda: (wgb if k < 4 else wub)[b2][:, 2 * kk:2 * kk + 2, :]
                                Td = (Twg4 if k < 4 else Twu4)[b2][kk]
                                view = lambda t_: t_[:].rearrange("p (c n) -> p c n", c=2)
                            else:
                                kk = k - 8
                                src = wd_d[ex][kk * 128:(kk + 1) * 128, :]
                                dst_of = lambda: wdb[b2][:, kk, :]
                                Td = Twd4[b2][kk]
                                view = lambda t_: t_[:]
                            cell = {}

                            def d():
                                si = sq_[0] % 3
                                sq_[0] += 1
                                cell["si"] = si
                                S.dma("sync", view(stg[si]), src, writes=[Tstg[si]])

                            def c():
                                si = cell["si"]
                                sv = view(stg[si])
                                dstb = dst_of()
                                if k % 2 == 1:
                                    S.op("scalar", lambda e: e.activation(out=dstb, in_=sv, func=AF.Copy), reads=[Tstg[si]], writes=[Td])
                                else:
                                    S.op("vector", lambda e: e.tensor_copy(out=dstb, in_=sv), reads=[Tstg[si]], writes=[Td])
                            return d, c
                        d, c = mk()
                        dmas.append(d)
                        casts.append(c)
                    steps = dmas[0:3]
                    for k in range(12):
                        steps.append(casts[k])
                        if k + 3 < 12:
                            steps.append(dmas[k + 3])
                    return steps

                def load_w(ex):
                    for f in w_steps(ex):
                        f()

                def gate_up(b2, rhs_of, Trhs, width, gq, slot=None):
                    for ft in range(4):
                        bg, bu = (0, 1) if gq[0] % 2 == 0 else (2, 3)
                        s2 = gq[0] % 2
                        gq[0] += 1
                        mmgroup(banks[bg][:, 0:width], [(wgb[b2][:, c, ft * 128:(ft + 1) * 128], rhs_of(c)) for c in range(8)],
                                reads=Trhs + Twg4[b2], writes=[Tb[bg]])
                        mmgroup(banks[bu][:, 0:width], [(wub[b2][:, c, ft * 128:(ft + 1) * 128], rhs_of(c)) for c in range(8)],
                                reads=Trhs + Twu4[b2], writes=[Tb[bu]])
                        S.op("scalar", lambda e, bg=bg, s2=s2: e.activation(out=sg[s2][:, 0:width], in_=banks[bg][:, 0:width], func=AF.Silu),
                             reads=[Tb[bg]], writes=[Tsg[s2]])
                        S.op("vector", lambda e, bu=bu, s2=s2, ft=ft: e.tensor_tensor(out=aT[b2][:, ft, 0:width], in0=sg[s2][:, 0:width], in1=banks[bu][:, 0:width], op=ALU.mult),
                             reads=[Tsg[s2], Tb[bu]], writes=[TaT[b2][ft]])
                        if slot is not None:
                            slot()

                S.branch_begin()
                gq = [0]; yq = [0]; xq = [0]

                def prep(ex):
                    b2 = ex % 2
                    for j in range(NJ):
                        xi = xq[0] % 4
                        xq[0] += 1
                        r0 = ex * CAP + j * 128
                        S.dma("gpsimd", xg[xi][:], xs_d[r0:r0 + 128, :], reads=Tsc + [Txs], writes=[Txg[xi]])
                        bi = 6 + (xq[0] % 2)
                        pTv = banks[bi][:].bitcast(BF16)
                        for c in range(8):
                            S.op("tensor", lambda e, c=c, xi=xi, pTv=pTv: e.transpose(
                                out=pTv[:, c * 128:(c + 1) * 128], in_=xg[xi][:, c * 128:(c + 1) * 128], identity=identb[:]),
                                reads=[Txg[xi], Tidb], writes=[Tb[bi]], signal=(c == 7))
                        S.op("vector", lambda e, j=j, b2=b2, pTv=pTv: e.tensor_copy(
                            out=xgT[b2][:, :, j * 128:(j + 1) * 128], in_=pTv.rearrange("p (c t) -> p c t", c=8)),
                            reads=[Tb[bi]], writes=[TxgT[b2][j]])
                prep(0)
                load_w(0)
                for ex in range(NEXP):
                    b2 = ex % 2
                    wq = w_steps(ex + 1) if ex + 1 < NEXP else []

                    def pop(n):
                        for _ in range(n):
                            if wq:
                                wq.pop(0)()
                    pop(3)
                    gate_up(b2, lambda c, b2=b2: xgT[b2][:, c, :], TxgT[b2], CAP, gq, slot=lambda: pop(3))
                    if ex + 1 < NEXP:
                        prep(ex + 1)
                    for j in range(NJ):
                        y2 = yq[0] % 2
                        yq[0] += 1
                        for hf in range(2):
                            by = 4 + hf
                            mmgroup(banks[by][:], [(aT[b2][:, ft, j * 128:(j + 1) * 128], wdb[b2][:, ft, hf * 512:(hf + 1) * 512]) for ft in range(4)],
                                    reads=TaT[b2] + Twd4[b2], writes=[Tb[by]])
                            if hf == 0:
                                S.op("vector", lambda e, y2=y2, by=by: e.tensor_copy(out=ysb[y2][:, 0:512], in_=banks[by][:]),
                                     reads=[Tb[by]], writes=[Tysb[y2]])
                            else:
                                S.op("scalar", lambda e, y2=y2, by=by: e.activation(out=ysb[y2][:, 512:1024], in_=banks[by][:], func=AF.Copy),
                                     reads=[Tb[by], Tysb[y2]], writes=[Tysb[y2]])
                        r0 = ex * CAP + j * 128
                        S.dma("gpsimd", ys_d[r0:r0 + 128, :], ysb[y2][:], reads=[Tysb[y2]], writes=[Tys[ex * NJ + j]], semtile=Tysb[y2])
                        pop(3)
                    pop(99)
                S.barrier()
                ygl = []
                for wb in wgb + wub + wdb:
                    v = wb[:].rearrange("p a b -> p (a b)").bitcast(F32)
                    ygl += [v[:, 0:1024], v[:, 1024:2048]]
                Tyg = [S.tile() for _ in ygl]
                set_bcreg()
                def gath(i):
                    t, k_ = i // 2, i % 2
                    gi = i % len(ygl)
                    S.dma_fn("gpsimd", lambda e, t=t, k_=k_, gi=gi: e.indirect_dma_start(
                        out=ygl[gi], out_offset=None, in_=ys_d[:, :],
                        in_offset=bass.IndirectOffsetOnAxis(ap=pos[:, k_, t:t + 1], axis=0),
                        bounds_check=bcreg, oob_is_err=False),
                        reads=Tys + [Tpos], writes=[Tyg[gi]], semtile=Tyg[gi])

                def acc(i):
                    t, k_ = i // 2, i % 2
                    gi = i % len(ygl)
                    for hf in range(2):
                        S.op("vector", lambda e, t=t, k_=k_, gi=gi, hf=hf: e.scalar_tensor_tensor(
                            out=x2[:, t, hf * 512:(hf + 1) * 512], in0=ygl[gi][:, hf * 512:(hf + 1) * 512], scalar=w12[:, k_, t:t + 1],
                            in1=x2[:, t, hf * 512:(hf + 1) * 512], op0=ALU.mult, op1=ALU.add),
                            reads=[Tyg[gi], Tw12, Tx2[t][hf]], writes=[Tx2[t][hf]])
                depth = len(ygl) - 1
                for i in range(32 + depth):
                    if i < 32:
                        gath(i)
                    if i - depth >= 0:
                        acc(i - depth)
                S.branch_mid()
                gq = [0]; yq = [0]
                load_w(0)
                for ex in range(NEXP):
                    b2 = ex % 2
                    if ex + 1 < NEXP:
                        load_w(ex + 1)
                    for tb in range(4):
                        gate_up(b2, lambda c, tb=tb: hmT[:, c, tb * 512:(tb + 1) * 512], ThmT[tb * 4:tb * 4 + 4], 512, gq)
                        for tt in range(4):
                            t = tb * 4 + tt
                            for hf in range(2):
                                by = 4 + (yq[0] % 4)
                                yq[0] += 1
                                mmgroup(banks[by][:], [(aT[b2][:, ft, tt * 128:(tt + 1) * 128], wdb[b2][:, ft, hf * 512:(hf + 1) * 512]) for ft in range(4)],
                                        reads=TaT[b2] + Twd4[b2], writes=[Tb[by]])
                                S.op("vector", lambda e, t=t, hf=hf, by=by, ex=ex: e.scalar_tensor_tensor(
                                    out=x2[:, t, hf * 512:(hf + 1) * 512], in0=banks[by][:], scalar=comb[:, t, ex:ex + 1],
                                    in1=x2[:, t, hf * 512:(hf + 1) * 512], op0=ALU.mult, op1=ALU.add),
                                    reads=[Tb[by], Tcomb, Tx2[t][hf]], writes=[Tx2[t][hf]])
                S.branch_end(ovf[0:1, 0:1], brregs)
                S.emit()

            with ExitStack() as c3:
                gbn = sb(c3, "gbn", [128, 1024], F32); Tgbn = S.tile()
                bcast_load(gbn[:], g_fin, Tgbn, 1024)
                ob = [sb(c3, "ob%d" % i, [128, 1024], F32) for i in range(2)]; Tob = [S.tile() for _ in range(2)]
                Tout = S.tile()
                for t in range(16):
                    S.op("scalar", lambda e, t=t: e.activation(out=junk[:], in_=x2[:, t, :], func=AF.Square, accum_out=ssc[:, t:t + 1]),
                         reads=Tx2[t] + [Tsscall], writes=[Tssc[t], Tjunkc])
                S.op("scalar", lambda e: e.activation(out=ssc[:], in_=ssc[:], func=AF.Sqrt, scale=1.0 / 1024, bias=EPS),
                     reads=Tssc, writes=[Tsscall])
                S.op("vector", lambda e: e.reciprocal(out=ssc[:], in_=ssc[:]), reads=[Tsscall], writes=[Tsscall])
                for t in range(16):
                    S.op("vector", lambda e, t=t: e.scalar_tensor_tensor(
                        out=ob[t % 2][:], in0=x2[:, t, :], scalar=ssc[:, t:t + 1], in1=gbn[:], op0=ALU.mult, op1=ALU.mult),
                        reads=Tx2[t] + [Tsscall, Tgbn], writes=[Tob[t % 2]])
                    S.dma("sync", out[t * 128:(t + 1) * 128, :], ob[t % 2][:], reads=[Tob[t % 2]], writes=[Tout], semtile=Tob[t % 2])
                S.barrier()
                S.emit()
    return nc


def _const_tables():
    f32 = np.float32
    inv_freq = (f32(1.0) / (f32(10000.0) ** (np.arange(0, 64, 2, dtype=f32) / f32(64)))).astype(f32)
    pos = np.arange(4096, dtype=f32)
    ang = (pos[:, None] * inv_freq[None, :]).astype(f32)
    cos = np.cos(ang).astype(f32)
    sin = np.sin(ang).astype(f32)
    r = np.arange(128)
    dh = r % 64
    cosT = cos[:, dh % 32].T.copy()
    sgn = np.where(dh < 32, -1.0, 1.0).astype(f32)
    sinT = (sin[:, dh % 32].T * sgn[:, None]).astype(f32)
    return cosT, sinT


def _masks(hf):
    k = np.arange(128)[:, None, None]
    r = np.arange(4)[None, :, None]
    q = np.arange(256)[None, None, :]
    md = np.zeros((128, 2, 4, 256), np.float32)
    mf = np.zeros((128, 2, 4, 256), np.float32)
    for par in range(2):
        if par == 0:
            kb = r
            j = 0 if hf == 0 else 1
        else:
            kb = 4 + r
            j = 3 if hf == 0 else 2
        s = kb * 128 + k
        t = j * 256 + q
        mf[:, par] = np.where(s <= t, 3e38, 0.0)
        md[:, par] = np.where((s // 64) <= (t // 64), 3e38, 0.0)
    return (md.reshape(128, 2, 1024).astype(ml_dtypes.bfloat16),
            mf.reshape(128, 2, 1024).astype(ml_dtypes.bfloat16))


def own_tokens(hf):
    return np.concatenate([np.arange(j * 256, (j + 1) * 256) for j in own_qtiles(hf)])


def prep(inputs):
    f32 = np.float32
    x = np.asarray(inputs["x"], f32)
    w_in = np.ascontiguousarray(np.asarray(inputs["w_in"], f32)[0])

    def swap_cols(w):
        return np.ascontiguousarray(w.reshape(1024, 8, 2, 32)[:, :, ::-1, :].reshape(1024, 512))

    cosT, sinT = _const_tables()
    common = {
        "w_in": w_in,
        "wqs": swap_cols(w_in[:, 0:512]),
        "wks": swap_cols(w_in[:, 512:1024]),
        "cosk": cosT, "sink": sinT,
        "identb": np.eye(128, dtype=f32).astype(ml_dtypes.bfloat16),
        "identf": np.eye(128, dtype=f32),
        "tri": np.triu(np.ones((128, 128), f32)),
        "onesf": np.ones((128, 128), f32),
        "onesb": np.ones((128, 128), f32).astype(ml_dtypes.bfloat16),
        "ustrict": np.triu(np.ones((128, 128), f32), 1).astype(ml_dtypes.bfloat16),
        "ebase": np.ascontiguousarray(np.broadcast_to((np.arange(16, dtype=f32) * CAP)[None, None, :], (128, 16, 16)).reshape(128, 256)),
        "g_attn": np.asarray(inputs["norm_attn_g"], f32).reshape(1, 1024),
        "g_ffn": np.asarray(inputs["norm_ffn_g"], f32).reshape(1, 1024),
        "g_fin": np.asarray(inputs["norm_final_g"], f32).reshape(1, 1024),
        "bfor": np.asarray(inputs["b_forget"], f32).reshape(1, 8),
        "lamv": np.concatenate([np.asarray(inputs[k], f32).reshape(1, 64) for k in
                                ("lambda_q1", "lambda_k1", "lambda_q2", "lambda_k2")], axis=1),
        "dng": np.asarray(inputs["diff_norm_g"], f32).reshape(1, 128),
        "w_out": np.ascontiguousarray(np.asarray(inputs["w_out"], f32)[0]),
        "rw": np.ascontiguousarray(np.concatenate([np.asarray(inputs["router_group_w"], f32)[0],
                                                   np.asarray(inputs["router_expert_w"], f32)[0]], axis=1)),
        "rb": np.concatenate([np.asarray(inputs["router_group_b"], f32).reshape(1, 4),
                              np.asarray(inputs["router_expert_b"], f32).reshape(1, 16)], axis=1),
        "wg": np.ascontiguousarray(np.asarray(inputs["w_gate"], f32)[0]),
        "wu": np.ascontiguousarray(np.asarray(inputs["w_up"], f32)[0]),
        "wd": np.ascontiguousarray(np.asarray(inputs["w_down"], f32)[0]),
    }
    in_maps = []
    for c in range(8):
        b, hf = c // 2, c % 2
        tok = own_tokens(hf)
        md, mf = _masks(hf)
        m = dict(common)
        m["xb"] = np.ascontiguousarray(x[b])
        m["xo"] = np.ascontiguousarray(x[b][tok])
        m["cosq"] = np.ascontiguousarray(cosT[:, tok] * f32(0.125))
        m["sinq"] = np.ascontiguousarray(sinT[:, tok] * f32(0.125))
        cs = np.zeros((8, 33), f32)
        for i, j in enumerate(own_qtiles(hf)):
            cs[i, 2 * j + 1] = 1.0
        m["csel"] = cs.reshape(1, 8 * 33)
        m["maskd"] = md
        m["maskf"] = mf
        in_maps.append(m)
    return in_maps


def kernel(**inputs):
    in_maps = prep(inputs)
    nc = build()
    res = run_bass_kernel_spmd(nc, in_maps, core_ids=list(range(8)))
    out = np.zeros((4, 4096, 1024), np.float32)
    for c in range(8):
        b, hf = c // 2, c % 2
        out[b, own_tokens(hf)] = res.results[c]["out"]
    return out
```

```python
import numpy as np
import ml_dtypes
from contextlib import ExitStack
import concourse.bass as bass
import concourse.mybir as mybir
from concourse.bass_utils import run_bass_kernel_spmd

F32 = mybir.dt.float32
BF16 = mybir.dt.bfloat16
I32 = mybir.dt.int32
AF = mybir.ActivationFunctionType
ALU = mybir.AluOpType
AX = mybir.AxisListType

ENGS = ("sync", "scalar", "vector", "gpsimd", "tensor")
EPS = 1e-6
LAM_INIT = 0.8 - 0.6 * 1.0
NEXP = 16
CAP = 512
NSLOT = NEXP * CAP


class Tile:
    __slots__ = ("name", "last_w", "readers", "dsem")

    def __init__(self, name):
        self.name = name
        self.last_w = None
        self.readers = {}
        self.dsem = None


class Sched:
    def __init__(self, nc, es):
        self.nc = nc
        self.es = es
        self.ops = {e: [] for e in ENGS}
        self.sems = {}
        self.cnt = {}
        self.waited = {e: {} for e in ENGS}
        self.pending = {e: False for e in ENGS}
        for e in ENGS:
            self._mksem("E:" + e)
        self.n_dsem = 0
        self.nops = 0
        self.tiles = []

    def _mksem(self, key):
        self.sems[key] = self.es.enter_context(self.nc.semaphore(key.replace(":", "_")))
        self.cnt[key] = 0

    def tile(self, name="t"):
        t = Tile(name)
        self.tiles.append(t)
        return t

    def _snapshot(self):
        return (dict(self.cnt), {e: dict(w) for e, w in self.waited.items()},
                [(t, t.last_w, dict(t.readers)) for t in self.tiles])

    def _restore(self, snap):
        self.cnt = dict(snap[0])
        for k in self.sems:
            self.cnt.setdefault(k, 0)
        self.waited = {e: dict(w) for e, w in snap[1].items()}
        for t, lw, rd in snap[2]:
            t.last_w = lw
            t.readers = dict(rd)

    def branch_begin(self):
        self.barrier()
        self._outer_ops = self.ops
        self.ops = {e: [] for e in ENGS}
        self._snap = self._snapshot()

    def branch_mid(self):
        self.barrier()
        self._A = (self.ops, dict(self.cnt))
        self.ops = {e: [] for e in ENGS}
        self._restore(self._snap)

    def branch_end(self, flag_ap, regs):
        self.barrier()
        opsA, cntA = self._A
        opsB, cntB = self.ops, dict(self.cnt)
        target = {k: max(cntA.get(k, 0), cntB.get(k, 0)) for k in set(cntA) | set(cntB)}

        def pads(cntX):
            out = {e: [] for e in ENGS}
            for k, v in target.items():
                d = v - cntX.get(k, 0)
                if d > 0:
                    owner = k[2:] if k.startswith("E:") else "gpsimd"
                    out[owner].append((k, d))
            return out
        pA, pB = pads(cntA), pads(cntB)
        self.ops = self._outer_ops
        for e in ENGS:
            self.ops[e].append(("branch", flag_ap, regs[e], opsA[e], pA[e], opsB[e], pB[e]))
        self.cnt = target
        for e in ENGS:
            self.waited[e] = dict(target)
        for t in self.tiles:
            t.last_w = None
            t.readers = {}

    def dsem_for(self, t):
        if t.dsem is None:
            key = "D:%d" % self.n_dsem
            self.n_dsem += 1
            self._mksem(key)
            t.dsem = key
        return t.dsem

    def _need(self, eng, waits, key, val):
        if eng == "tensor" and key == "E:tensor":
            return
        if self.cnt[key] < val:
            raise RuntimeError("wait on un-signalled event %s %d (cnt %d) from %s" % (key, val, self.cnt[key], eng))
        if self.waited[eng].get(key, 0) >= val:
            return
        self.waited[eng][key] = val
        waits[key] = max(waits.get(key, 0), val)

    def _deps(self, eng, reads, writes):
        waits = {}
        for t in reads:
            if t.last_w is not None:
                self._need(eng, waits, *t.last_w)
        for t in writes:
            if t.last_w is not None:
                self._need(eng, waits, *t.last_w)
            for k, v in t.readers.items():
                self._need(eng, waits, k, v)
        return list(waits.items())

    def _record(self, ev, reads, writes):
        for t in writes:
            t.last_w = ev
            t.readers = {}
        for t in reads:
            if t not in writes:
                if t.readers.get(ev[0], 0) < ev[1]:
                    t.readers[ev[0]] = ev[1]

    def op(self, eng, fn, reads=(), writes=(), signal=True):
        waits = self._deps(eng, reads, writes)
        key = "E:" + eng
        if signal:
            self.cnt[key] += 1
            ev = (key, self.cnt[key])
            inc = (key, 1)
            self.pending[eng] = False
        else:
            ev = (key, self.cnt[key] + 1)
            inc = None
            self.pending[eng] = True
        self._record(ev, reads, writes)
        self.ops[eng].append((waits, fn, inc))
        self.nops += 1

    def dma(self, eng, out, in_, reads=(), writes=(), semtile=None, **kw):
        waits = self._deps(eng, reads, writes)
        if semtile is None:
            semtile = writes[0] if writes else reads[0]
        key = self.dsem_for(semtile)
        self.cnt[key] += 16
        ev = (key, self.cnt[key])
        self._record(ev, reads, writes)

        def fn(e, out=out, in_=in_, kw=kw):
            return e.dma_start(out=out, in_=in_, **kw)
        self.ops[eng].append((waits, fn, (key, 16)))
        self.nops += 1

    def dma_fn(self, eng, fn, reads=(), writes=(), semtile=None):
        waits = self._deps(eng, reads, writes)
        key = self.dsem_for(semtile)
        self.cnt[key] += 16
        ev = (key, self.cnt[key])
        self._record(ev, reads, writes)
        self.ops[eng].append((waits, fn, (key, 16)))
        self.nops += 1

    def barrier(self, engs=ENGS):
        for e in ENGS:
            assert not self.pending[e], e
        for e in engs:
            waits = {}
            for key, c in self.cnt.items():
                if c > 0:
                    self._need(e, waits, key, c)
            self.ops[e].append((list(waits.items()), None, None))

    def emit(self):
        nc = self.nc
        sems = self.sems
        ops = self.ops
        with nc.Block() as block:
            def replay(e, lst):
                for ent in lst:
                    if ent[0] == "branch":
                        _, flag_ap, reg, oA, pA, oB, pB = ent
                        e.reg_load(reg, flag_ap)
                        with e.If_eq(reg, 0):
                            replay(e, oA)
                            for k, d in pA:
                                e.sem_inc(sems[k], d)
                            e.nop()
                        with e.Else():
                            replay(e, oB)
                            for k, d in pB:
                                e.sem_inc(sems[k], d)
                            e.nop()
                        continue
                    waits, fn, inc = ent
                    for key, val in waits:
                        e.wait_ge(sems[key], val)
                    if fn is None:
                        continue
                    inst = fn(e)
                    if inc is not None:
                        inst.then_inc(sems[inc[0]], inc[1])

            def mk(name):
                def body(e):
                    replay(e, ops[name])
                return body
            block.sync(mk("sync"))
            block.scalar(mk("scalar"))
            block.vector(mk("vector"))
            block.gpsimd(mk("gpsimd"))
            block.tensor(mk("tensor"))
        self.ops = {e: [] for e in ENGS}


def own_qtiles(hf):
    js = []
    for m in range(4):
        js += ([4 * m, 4 * m + 3] if hf == 0 else [4 * m + 1, 4 * m + 2])
    return js


def nk_of(i):
    return 8 * (i // 2) + (4 if i % 2 == 0 else 8)


def interleave(A, B):
    a, b = len(A), len(B)
    if a == 0:
        for f in B:
            f()
        return
    done = 0
    for k, f in enumerate(A):
        f()
        upto = ((k + 1) * b) // a
        while done < upto:
            B[done]()
            done += 1
    while done < b:
        B[done]()
        done += 1


def build(debug=(), stop_after=None, moe="sparse"):
    nc = bass.Bass("TRN2", target_bir_lowering=False)

    def din(name, shape, dt=F32):
        return nc.dram_tensor(name, list(shape), dt, kind="ExternalInput").ap()

    xb = din("xb", [4096, 1024])
    xo = din("xo", [2048, 1024])
    w_in = din("w_in", [1024, 3080])
    wqs_d = din("wqs", [1024, 512])
    wks_d = din("wks", [1024, 512])
    cosk = din("cosk", [128, 4096])
    sink = din("sink", [128, 4096])
    cosq = din("cosq", [128, 2048])
    sinq = din("sinq", [128, 2048])
    maskd_d = din("maskd", [128, 2, 1024], BF16)
    maskf_d = din("maskf", [128, 2, 1024], BF16)
    identb_d = din("identb", [128, 128], BF16)
    identf_d = din("identf", [128, 128])
    tri_d = din("tri", [128, 128])
    onesf_d = din("onesf", [128, 128])
    g_attn = din("g_attn", [1, 1024])
    g_ffn = din("g_ffn", [1, 1024])
    g_fin = din("g_fin", [1, 1024])
    bfor = din("bfor", [1, 8])
    lam_d = din("lamv", [1, 256])
    csel_d = din("csel", [1, 8 * 33])
    dng = din("dng", [1, 128])
    w_out = din("w_out", [1024, 1024])
    rw_d = din("rw", [1024, 20])
    rb_d = din("rb", [1, 20])
    wg_d = din("wg", [16, 1024, 512])
    wu_d = din("wu", [16, 1024, 512])
    wd_d = din("wd", [16, 512, 1024])
    ebase_d = din("ebase", [128, 256])
    ustrict_d = din("ustrict", [128, 128], BF16)
    onesb_d = din("onesb", [128, 128], BF16)
    xs_d = nc.dram_tensor("xs_scratch", [NSLOT, 1024], BF16).ap()
    ys_d = nc.dram_tensor("ys_scratch", [NSLOT, 1024], F32).ap()
    out = nc.dram_tensor("out", [2048, 1024], F32, kind="ExternalOutput").ap()
    dbg = {}

    def dbg_out(name, shape, dt=F32):
        if name in debug:
            dbg[name] = nc.dram_tensor("dbg_" + name, list(shape), dt, kind="ExternalOutput").ap()
            return dbg[name]
        return None

    with ExitStack() as es:
        S = Sched(nc, es)

        uniq = [0]

        def sb(sc, name, shape, dt):
            uniq[0] += 1
            return sc.enter_context(nc.sbuf_tensor("s%d_%s" % (uniq[0], name), list(shape), dt))

        banks = [es.enter_context(nc.psum_tensor("bank%d" % i, [128, 512], F32)) for i in range(8)]
        Tb = [S.tile("bank%d" % i) for i in range(8)]
        Tdbg = S.tile("dbg")

        def dump(name, src_ap, reads):
            if name in dbg:
                S.dma("sync", dbg[name], src_ap, reads=reads, writes=[Tdbg], semtile=Tdbg)

        def bcast_load(dst, src, T, n):
            S.dma("sync", dst, src.broadcast_to([128, n]), writes=[T])

        bcreg = es.enter_context(nc.gpsimd.register("bcreg"))
        brregs = {e: es.enter_context(getattr(nc, e).register("br_" + e)) for e in ENGS}

        def set_bcreg():
            S.ops["gpsimd"].append(([], lambda e: e.reg_mov(bcreg, NSLOT - 1), None))

        identb = sb(es, "identb", [128, 128], BF16); Tidb = S.tile("identb")
        S.dma("sync", identb[:], identb_d, writes=[Tidb])
        gb_attn = sb(es, "gb_attn", [128, 1024], F32); Tgba = S.tile("gba")
        bcast_load(gb_attn[:], g_attn, Tgba, 1024)
        OT = sb(es, "OT", [128, 8, 2048], BF16)
        TOT = [S.tile("OT%d" % q) for q in range(16)]

        def make_sweep(sc, pfx, pT_banks):
            st = {}
            st["xt"] = [sb(sc, pfx + "xt%d" % i, [128, 1024], F32) for i in range(4)]
            st["Txt"] = [S.tile() for _ in range(4)]
            st["hb"] = [sb(sc, pfx + "hb%d" % i, [128, 1024], BF16) for i in range(2)]
            st["Thb"] = [S.tile() for _ in range(2)]
            st["hT"] = [sb(sc, pfx + "hT%d" % i, [128, 8, 512], BF16) for i in range(3)]
            st["ThT"] = [[S.tile() for _ in range(4)] for _ in range(3)]
            st["junk"] = sb(sc, pfx + "junk", [128, 1024], BF16)
            st["Tjunk"] = S.tile()
            st["ss"] = [sb(sc, pfx + "ss%d" % i, [128, 4], F32) for i in range(3)]
            st["Tss"] = [[S.tile() for _ in range(4)] for _ in range(3)]
            st["sq"] = [sb(sc, pfx + "sq%d" % i, [128, 4], F32) for i in range(3)]
            st["Tsq"] = [S.tile() for _ in range(3)]
            st["rs"] = [sb(sc, pfx + "rs%d" % i, [128, 4], F32) for i in range(3)]
            st["Trs"] = [S.tile() for _ in range(3)]
            st["pT"] = pT_banks
            return st

        def sweep_stage1(st, x_ap, blk, gb, Tgb, deferred=False):
            b2 = blk % 3
            for tt in range(4):
                n = blk * 4 + tt
                xt, Txt = st["xt"][n % 4], st["Txt"][n % 4]
                S.dma("sync", xt[:], x_ap[n * 128:(n + 1) * 128, :], writes=[Txt])
                S.op("scalar", lambda e, xt=xt, tt=tt: e.activation(out=st["junk"][:], in_=xt[:], func=AF.Square,
                                                                    accum_out=st["ss"][b2][:, tt:tt + 1]),
                     reads=[Txt], writes=[st["Tss"][b2][tt], st["Tjunk"]])
            S.op("scalar", lambda e: e.activation(out=st["sq"][b2][:], in_=st["ss"][b2][:], func=AF.Sqrt,
                                                  scale=1.0 / 1024, bias=EPS),
                 reads=st["Tss"][b2], writes=[st["Tsq"][b2]])
            S.op("vector", lambda e: e.reciprocal(out=st["rs"][b2][:], in_=st["sq"][b2][:]),
                 reads=[st["Tsq"][b2]], writes=[st["Trs"][b2]])
            def stt(tt):
                n = blk * 4 + tt
                xt, Txt = st["xt"][n % 4], st["Txt"][n % 4]
                hb, Thb = st["hb"][n % 2], st["Thb"][n % 2]
                S.op("vector", lambda e, xt=xt, hb=hb, tt=tt: e.scalar_tensor_tensor(
                    out=hb[:], in0=xt[:], scalar=st["rs"][b2][:, tt:tt + 1], in1=gb[:], op0=ALU.mult, op1=ALU.mult),
                    reads=[Txt, st["Trs"][b2], Tgb], writes=[Thb])

            def tr(tt):
                n = blk * 4 + tt
                hb, Thb = st["hb"][n % 2], st["Thb"][n % 2]
                bi = st["pT"][n % 2]
                pTv = banks[bi][:].bitcast(BF16)
                for c in range(8):
                    S.op("tensor", lambda e, c=c, hb=hb, pTv=pTv: e.transpose(
                        out=pTv[:, c * 128:(c + 1) * 128], in_=hb[:, c * 128:(c + 1) * 128], identity=identb[:]),
                        reads=[Thb, Tidb], writes=[Tb[bi]], signal=(c == 7))

            def ev(tt):
                n = blk * 4 + tt
                bi = st["pT"][n % 2]
                pTv = banks[bi][:].bitcast(BF16)
                S.op("vector", lambda e, tt=tt, pTv=pTv: e.tensor_copy(
                    out=st["hT"][b2][:, :, tt * 128:(tt + 1) * 128], in_=pTv.rearrange("p (c t) -> p c t", c=8)),
                    reads=[Tb[bi]], writes=[st["ThT"][b2][tt]])
            if not deferred:
                for f, a in ((stt, 0), (stt, 1), (tr, 0), (ev, 0), (stt, 2), (tr, 1), (ev, 1), (stt, 3), (tr, 2), (ev, 2), (tr, 3), (ev, 3)):
                    f(a)
                return []
            stt(0)
            return [lambda: (stt(1), tr(0), ev(0)), lambda: (stt(2), tr(1), ev(1)),
                    lambda: (stt(3), tr(2), ev(2)), lambda: (tr(3), ev(3))]

        def wload(dst, src_cols, T):
            S.dma("gpsimd", dst, src_cols.rearrange("(c p) n -> p c n", p=128), writes=[T])

        def mmgroup(out_ap, pairs, reads, writes):
            n = len(pairs)
            for k, (l, r) in enumerate(pairs):
                S.op("tensor", lambda e, l=l, r=r, k=k: e.matmul(out=out_ap, lhsT=l, rhs=r, start=(k == 0), stop=(k == n - 1)),
                     reads=reads, writes=writes, signal=(k == n - 1))

        def run_attention(units, PT, TPT, KT_of, QT_of, V_of, W, exp_emit, evac_emit, mask_emit, s_banks, o_banks,
                          OTs=None, TOTs=None, smr=None, Tsmr=None, identf=None, Tidf=None, Vsum_of=None):
            gctr = [0]
            dv = W - 1
            MO = dv if Vsum_of is not None else W
            deferred = []

            def st_list(ui, u):
                buf = ui % 2
                nk = nk_of(u["i"])
                L = []
                for p in range(nk // 2):
                    def f(p=p):
                        bi = s_banks[gctr[0] % len(s_banks)]
                        gctr[0] += 1
                        for j in range(2):
                            kb = 2 * p + j
                            kap, Tk = KT_of(u, kb)
                            qap, Tq = QT_of(u)
                            S.op("tensor", lambda e, kap=kap, qap=qap, j=j, bi=bi: e.matmul(
                                out=banks[bi][:, j * 256:(j + 1) * 256], lhsT=kap, rhs=qap, start=True, stop=True),
                                reads=[Tk, Tq], writes=[Tb[bi]], signal=(j == 1))
                        exp_emit(u, p, bi, PT[buf], TPT[buf][p])
                    L.append(f)
                L.append(lambda: mask_emit(u, PT[buf], TPT[buf]))
                return L

            def pv_list(ui, u):
                buf = ui % 2
                nk = nk_of(u["i"])
                ba = o_banks[(ui % 2) * 2]
                bb = o_banks[(ui % 2) * 2 + 1]
                o2 = ui % 2
                L = []
                for k0 in range(0, nk, 4):
                    def f(k0=k0):
                        for kb in range(k0, min(nk, k0 + 4)):
                            vap, Tv = V_of(u, kb)
                            S.op("tensor", lambda e, kb=kb, vap=vap: e.matmul(
                                out=banks[ba][0:MO, 0:256], lhsT=vap, rhs=PT[buf][:, kb, :], start=(kb == 0), stop=(kb == nk - 1)),
                                reads=[TPT[buf][kb // 2], Tv], writes=[Tb[ba]], signal=(kb == nk - 1))
                    L.append(f)
                if Vsum_of is not None:
                    for k0 in range(0, nk, 4):
                        def g(k0=k0):
                            for kb in range(k0, min(nk, k0 + 4)):
                                vap, Tv = Vsum_of(u, kb)
                                S.op("tensor", lambda e, kb=kb, vap=vap: e.matmul(
                                    out=banks[bb][0:1, 256:512], lhsT=vap, rhs=PT[buf][:, kb, :], start=(kb == 0), stop=(kb == nk - 1)),
                                    reads=[TPT[buf][kb // 2], Tv], writes=[Tb[bb]], signal=(kb == nk - 1))
                        L.append(g)

                def copy_out():
                    S.op("vector", lambda e: e.tensor_copy(out=OTs[o2][0:MO, :], in_=banks[ba][0:MO, 0:256]), reads=[Tb[ba]], writes=[TOTs[o2]])
                    if Vsum_of is not None:
                        S.op("vector", lambda e: e.tensor_copy(out=smr[o2][:], in_=banks[bb][0:1, 256:512]), reads=[Tb[bb]], writes=[Tsmr[o2]])

                def transpose_back():
                    for s in range(2):
                        last = (Vsum_of is None)
                        S.op("tensor", lambda e, s=s: e.transpose(out=banks[bb][:, s * W:s * W + MO], in_=OTs[o2][0:MO, s * 128:(s + 1) * 128],
                                                                  identity=identf[0:MO, 0:MO]),
                             reads=[TOTs[o2], Tidf], writes=[Tb[bb]], signal=(last and s == 1))
                        if Vsum_of is not None:
                            S.op("tensor", lambda e, s=s: e.transpose(out=banks[bb][:, s * W + dv:s * W + W], in_=smr[o2][0:1, s * 128:(s + 1) * 128],
                                                                      identity=identf[0:1, 0:1]),
                                 reads=[Tsmr[o2], Tidf], writes=[Tb[bb]], signal=(s == 1))
                    evac_emit(u, bb)
                L.append(copy_out)
                deferred.append(transpose_back)
                return L

            for ui in range(len(units) + 1):
                A = st_list(ui, units[ui]) if ui < len(units) else []
                B = pv_list(ui - 1, units[ui - 1]) if ui >= 1 else []
                if len(deferred) > (1 if ui >= 1 else 0):
                    B.insert(min(2, len(B)), deferred.pop(0))
                interleave(A, B)
            while deferred:
                deferred.pop(0)()

        with ExitStack() as pa:
            KT = sb(pa, "KTd", [128, 4, 4096], BF16)
            TKT = [[S.tile() for _ in range(8)] for _ in range(4)]
            QT = sb(pa, "QTd", [128, 4, 2048], BF16)
            TQT = [[S.tile() for _ in range(4)] for _ in range(4)]
            Vd = sb(pa, "Vd", [128, 32, 4, 130], BF16)
            TV = [S.tile() for _ in range(32)]
            Tvones = S.tile()
            S.op("vector", lambda e: e.memset(Vd[:, :, :, 128:130], 1.0), writes=TV)
            lamt = sb(pa, "lamt", [128, 256], F32); Tlam = S.tile()
            bcast_load(lamt[:], lam_d, Tlam, 256)
            lj = sb(pa, "lj", [128, 64], F32)
            ls = sb(pa, "ls", [128, 2], F32); Tls = S.tile()
            le = sb(pa, "le", [128, 2], F32); Tle = S.tile()
            neglam = sb(pa, "neglam", [128, 1], F32); Tnl = S.tile()
            for z in range(2):
                S.op("vector", lambda e, z=z: e.scalar_tensor_tensor(
                    out=lj[:], in0=lamt[:, z * 128:z * 128 + 64], scalar=1.0, in1=lamt[:, z * 128 + 64:z * 128 + 128],
                    op0=ALU.mult, op1=ALU.mult, accum_out=ls[:, z:z + 1]), reads=[Tlam, Tls], writes=[Tls])
            S.op("scalar", lambda e: e.activation(out=le[:], in_=ls[:], func=AF.Exp), reads=[Tls], writes=[Tle])
            S.op("vector", lambda e: e.tensor_tensor(out=neglam[:], in0=le[:, 1:2], in1=le[:, 0:1], op=ALU.subtract),
                 reads=[Tle], writes=[Tnl])
            S.op("vector", lambda e: e.tensor_scalar(out=neglam[:], in0=neglam[:], scalar1=-LAM_INIT, scalar2=None, op0=ALU.add),
                 reads=[Tnl], writes=[Tnl])
            gsc = sb(pa, "gsc", [128, 128], F32); Tgsc = S.tile()
            bcast_load(gsc[:], dng, Tgsc, 128)
            S.op("vector", lambda e: e.tensor_scalar(out=gsc[:], in0=gsc[:], scalar1=1.0 - LAM_INIT, scalar2=None, op0=ALU.mult),
                 reads=[Tgsc], writes=[Tgsc])
            Txs = S.tile("xs")

            with ExitStack() as sw:
                st = make_sweep(sw, "a", [0, 1])
                wA = sb(sw, "wA", [128, 8, 512], BF16); TwA = S.tile()
                wB = sb(sw, "wB", [128, 8, 512], BF16); TwB = S.tile()
                wC = sb(sw, "wC", [128, 8, 512], BF16); TwC = S.tile()
                wload(wA[:], w_in[:, 512:1024], TwA)
                wload(wB[:], wks_d, TwB)
                wload(wC[:], w_in[:, 1024:1536], TwC)
                ct = [sb(sw, "ct%d" % i, [128, 512], F32) for i in range(2)]; Tct = [S.tile() for _ in range(2)]
                sn = [sb(sw, "sn%d" % i, [128, 512], F32) for i in range(2)]; Tsn = [S.tile() for _ in range(2)]
                t1 = [sb(sw, "t1%d" % i, [128, 512], F32) for i in range(2)]; Tt1 = [S.tile() for _ in range(2)]
                t2 = [sb(sw, "t2%d" % i, [128, 512], F32) for i in range(2)]; Tt2 = [S.tile() for _ in range(2)]
                rctr = [0]

                def rope_proj(blk, hT, ThT, wq_, Tw_, ws_, Tws_, cos_d, sin_d, dstT, TdstT, slots=()):
                    b2 = blk % 2
                    S.dma("sync", ct[b2][:], cos_d[:, blk * 512:(blk + 1) * 512], writes=[Tct[b2]])
                    S.dma("sync", sn[b2][:], sin_d[:, blk * 512:(blk + 1) * 512], writes=[Tsn[b2]])
                    for h in range(4):
                        ba, bb = (2, 3) if h % 2 == 0 else (4, 5)
                        mmgroup(banks[ba][:], [(wq_[:, c, h * 128:(h + 1) * 128], hT[:, c, :]) for c in range(8)],
                                reads=list(ThT) + [Tw_], writes=[Tb[ba]])
                        mmgroup(banks[bb][:], [(ws_[:, c, h * 128:(h + 1) * 128], hT[:, c, :]) for c in range(8)],
                                reads=list(ThT) + [Tws_], writes=[Tb[bb]])
                        r = rctr[0] % 2
                        rctr[0] += 1
                        S.op("vector", lambda e, r=r, ba=ba: e.tensor_tensor(out=t1[r][:], in0=banks[ba][:], in1=ct[b2][:], op=ALU.mult),
                             reads=[Tb[ba], Tct[b2]], writes=[Tt1[r]])
                        S.op("vector", lambda e, r=r, bb=bb: e.tensor_tensor(out=t2[r][:], in0=banks[bb][:], in1=sn[b2][:], op=ALU.mult),
                             reads=[Tb[bb], Tsn[b2]], writes=[Tt2[r]])
                        S.op("gpsimd", lambda e, r=r, h=h: e.tensor_tensor(out=dstT[:, h, blk * 512:(blk + 1) * 512], in0=t1[r][:], in1=t2[r][:], op=ALU.add),
                             reads=[Tt1[r], Tt2[r]], writes=[TdstT[h][blk]])
                        if h < len(slots):
                            slots[h]()

                def kv_proj(blk, hT, ThT, slots=()):
                    rope_proj(blk, hT, ThT, wA, TwA, wB, TwB, cosk, sink, KT, TKT, slots)
                    for tt in range(4):
                        n = blk * 4 + tt
                        bv = 6 + (tt % 2)
                        mmgroup(banks[bv][:], [(hT[:, c, tt * 128:(tt + 1) * 128], wC[:, c, :]) for c in range(8)],
                                reads=[ThT[tt], TwC], writes=[Tb[bv]])
                        S.op("scalar", lambda e, n=n, bv=bv: e.activation(
                            out=Vd[:, n, :, 0:128], in_=banks[bv][:].rearrange("p (h d) -> p h d", h=4), func=AF.Copy),
                            reads=[Tb[bv]], writes=[TV[n]])

                sweep_stage1(st, xb, 0, gb_attn, Tgba)
                sweep_stage1(st, xb, 1, gb_attn, Tgba)
                for blk in range(8):
                    sl = sweep_stage1(st, xb, blk + 2, gb_attn, Tgba, deferred=True) if blk + 2 < 8 else []
                    kv_proj(blk, st["hT"][blk % 3], st["ThT"][blk % 3], sl)
                wload(wA[:], w_in[:, 0:512], TwA)
                wload(wB[:], wqs_d, TwB)
                sweep_stage1(st, xo, 0, gb_attn, Tgba)
                sweep_stage1(st, xo, 1, gb_attn, Tgba)
                for blk in range(4):
                    sl = sweep_stage1(st, xo, blk + 2, gb_attn, Tgba, deferred=True) if blk + 2 < 4 else []
                    rope_proj(blk, st["hT"][blk % 3], st["ThT"][blk % 3], wA, TwA, wB, TwB, cosq, sinq, QT, TQT, sl)
                S.barrier()
                if "KTd" in debug:
                    dbg_out("KTd", [128, 4, 4096], BF16); dump("KTd", KT[:], [])
                    dbg_out("QTd", [128, 4, 2048], BF16); dump("QTd", QT[:], [])
                    dbg_out("Vd", [128, 32, 4, 130], BF16); dump("Vd", Vd[:], [])
                    S.barrier()
                S.emit()
            if stop_after == "Aproj":
                S.barrier(); S.emit()
                return nc

            with ExitStack() as at:
                maskd = sb(at, "maskd", [128, 2, 1024], BF16); Tmd = S.tile()
                S.dma("sync", maskd[:], maskd_d, writes=[Tmd])
                if moe == "sparse":
                    zer = sb(at, "zer", [128, 2048], BF16); Tzer = S.tile()
                    S.op("gpsimd", lambda e: e.memset(zer[:], 0.0), writes=[Tzer])
                    for n in range(NSLOT // 256):
                        S.dma("gpsimd", xs_d[n * 256:(n + 1) * 256, :].rearrange("(p r) d -> p (r d)", r=2), zer[:], reads=[Tzer], writes=[Txs], semtile=Tzer)
                PT = [sb(at, "PT%d" % i, [128, 32, 256], BF16) for i in range(2)]
                TPT = [[S.tile() for _ in range(16)] for _ in range(2)]
                oc = sb(at, "oc", [128, 16, 4, 128], F32); Toc = [[S.tile() for _ in range(4)] for _ in range(16)]
                ssq = sb(at, "ssq", [128, 64], F32); Tssq = S.tile()
                A1 = [sb(at, "A1%d" % i, [128, 128], F32) for i in range(2)]; TA1 = [S.tile() for _ in range(2)]
                rr = sb(at, "rr", [128, 4], F32); Trr = [S.tile() for _ in range(4)]
                sjunk = sb(at, "sjunk", [128, 128], F32); Tsjunk = S.tile()
                units = [dict(h=h, i=i, z=z) for h in range(4) for i in range(8) for z in range(2)]

                def KT_of(u, kb):
                    r0 = 64 * u["z"]
                    return KT[r0:r0 + 64, u["h"], kb * 128:(kb + 1) * 128], TKT[u["h"]][kb // 4]

                def QT_of(u):
                    r0 = 64 * u["z"]
                    return QT[r0:r0 + 64, u["h"], u["i"] * 256:(u["i"] + 1) * 256], TQT[u["h"]][u["i"] // 2]

                def V_of(u, kb):
                    return Vd[:, kb, u["h"], 0:128], TV[kb]

                def exp_emit(u, p, bi, PTb, Tp):
                    S.op("scalar", lambda e: e.activation(out=PTb[:, 2 * p:2 * p + 2, :].rearrange("p a b -> p (a b)"),
                                                          in_=banks[bi][:], func=AF.Exp),
                         reads=[Tb[bi]], writes=[Tp])

                def mask_emit(u, PTb, TPb):
                    i = u["i"]
                    lo = nk_of(i) - 4
                    S.op("vector", lambda e: e.tensor_tensor(out=PTb[:, lo:lo + 4, :], in0=PTb[:, lo:lo + 4, :],
                                                             in1=maskd[:, i % 2, :].rearrange("p (a b) -> p a b", a=4), op=ALU.min),
                         reads=[Tmd, TPb[lo // 2], TPb[lo // 2 + 1]], writes=[TPb[lo // 2], TPb[lo // 2 + 1]])

                def evac_emit(u, ob):
                    h, i, z = u["h"], u["i"], u["z"]
                    for s in range(2):
                        qb = 2 * i + s
                        o_ap = banks[ob][:, s * 129:s * 129 + 128]
                        sm_ap = banks[ob][:, s * 129 + 128:s * 129 + 129]
                        ri = 2 * z + s
                        S.op("vector", lambda e, ri=ri, sm_ap=sm_ap: e.reciprocal(out=rr[:, ri:ri + 1], in_=sm_ap),
                             reads=[Tb[ob]], writes=[Trr[ri]])
                        if z == 0:
                            S.op("vector", lambda e, ri=ri, o_ap=o_ap, s=s: e.tensor_scalar(
                                out=A1[s][:], in0=o_ap, scalar1=rr[:, ri:ri + 1], scalar2=None, op0=ALU.mult),
                                reads=[Tb[ob], Trr[ri]], writes=[TA1[s]])
                        else:
                            S.op("vector", lambda e, ri=ri: e.tensor_tensor(out=rr[:, ri:ri + 1], in0=rr[:, ri:ri + 1], in1=neglam[:], op=ALU.mult),
                                 reads=[Trr[ri], Tnl], writes=[Trr[ri]])
                            S.op("vector", lambda e, ri=ri, o_ap=o_ap, s=s, qb=qb: e.scalar_tensor_tensor(
                                out=oc[:, qb, h, :], in0=o_ap, scalar=rr[:, ri:ri + 1], in1=A1[s][:], op0=ALU.mult, op1=ALU.add),
                                reads=[Tb[ob], Trr[ri], TA1[s]], writes=[Toc[qb][h]])
                            S.op("vector", lambda e, qb=qb: e.scalar_tensor_tensor(
                                out=sjunk[:], in0=oc[:, qb, h, :], scalar=1.0, in1=oc[:, qb, h, :], op0=ALU.mult, op1=ALU.mult,
                                accum_out=ssq[:, qb * 4 + h:qb * 4 + h + 1]),
                                reads=[Toc[qb][h], Tssq], writes=[Tssq, Tsjunk])

                OTs = [sb(at, "OTs%d" % i, [128, 256], F32) for i in range(2)]; TOTs = [S.tile() for _ in range(2)]
                smr = [sb(at, "smr%d" % i, [1, 256], F32) for i in range(2)]; Tsmr = [S.tile() for _ in range(2)]
                identf_a = sb(at, "identf_a", [128, 128], F32); Tidf_a = S.tile()
                S.dma("sync", identf_a[:], identf_d, writes=[Tidf_a])

                def Vsum_of(u, kb):
                    return Vd[:, kb, u["h"], 128:129], TV[kb]
                run_attention(units, PT, TPT, KT_of, QT_of, V_of, 129, exp_emit, evac_emit, mask_emit,
                              s_banks=[0, 1, 2, 3], o_banks=[4, 5, 6, 7], OTs=OTs, TOTs=TOTs, smr=smr, Tsmr=Tsmr,
                              identf=identf_a, Tidf=Tidf_a, Vsum_of=Vsum_of)
                S.op("scalar", lambda e: e.activation(out=ssq[:], in_=ssq[:], func=AF.Sqrt, scale=1.0 / 128, bias=EPS),
                     reads=[Tssq], writes=[Tssq])
                S.op("vector", lambda e: e.reciprocal(out=ssq[:], in_=ssq[:]), reads=[Tssq], writes=[Tssq])
                Otok = [sb(at, "Otok%d" % i, [128, 512], BF16) for i in range(2)]; TOtok = [S.tile() for _ in range(2)]
                for qb in range(16):
                    o2 = qb % 2
                    for h in range(4):
                        S.op("vector", lambda e, qb=qb, h=h, o2=o2: e.scalar_tensor_tensor(
                            out=Otok[o2][:, h * 128:(h + 1) * 128], in0=oc[:, qb, h, :], scalar=ssq[:, qb * 4 + h:qb * 4 + h + 1],
                            in1=gsc[:], op0=ALU.mult, op1=ALU.mult),
                            reads=[Toc[qb][h], Tssq, Tgsc], writes=[TOtok[o2]])
                    bi = o2
                    pTv = banks[bi][:].bitcast(BF16)
                    for c in range(4):
                        S.op("tensor", lambda e, c=c, o2=o2, pTv=pTv: e.transpose(
                            out=pTv[:, c * 128:(c + 1) * 128], in_=Otok[o2][:, c * 128:(c + 1) * 128], identity=identb[:]),
                            reads=[TOtok[o2], Tidb], writes=[Tb[bi]], signal=(c == 3))
                    S.op("vector", lambda e, qb=qb, pTv=pTv: e.tensor_copy(
                        out=OT[:, 0:4, qb * 128:(qb + 1) * 128], in_=pTv[:, 0:512].rearrange("p (c t) -> p c t", c=4)),
                        reads=[Tb[bi]], writes=[TOT[qb]])
                S.barrier()
                if "OTa" in debug:
                    dbg_out("OTa", [128, 8, 2048], BF16); dump("OTa", OT[:], []); S.barrier()
                S.emit()
        if stop_after == "A":
            return nc

        with ExitStack() as pb:
            KT = sb(pb, "KTf", [128, 4, 4096], BF16)
            TKT = [[S.tile() for _ in range(8)] for _ in range(4)]
            QT = sb(pb, "QTf", [128, 4, 2048], BF16)
            TQT = [[S.tile() for _ in range(4)] for _ in range(4)]
            Vf = sb(pb, "Vf", [128, 32, 8, 66], BF16)
            TV = [S.tile() for _ in range(32)]
            S.op("vector", lambda e: e.memset(Vf[:, :, :, 64:66], 1.0), writes=TV)
            zt = sb(pb, "zt", [128, 32, 8], F32); Tzt = [S.tile() for _ in range(32)]
            Fpos = sb(pb, "Fpos", [128, 32, 8], F32); TF = [S.tile() for _ in range(32)]
            Cpos = sb(pb, "Cpos", [128, 33, 8], F32); TC = [S.tile() for _ in range(33)]
            maskf = sb(pb, "maskf", [128, 2, 1024], BF16); Tmf = S.tile()
            S.dma("sync", maskf[:], maskf_d, writes=[Tmf])
            bfb = sb(pb, "bfb", [128, 8], F32); Tbfb = S.tile()
            bcast_load(bfb[:], bfor, Tbfb, 8)
            csel = sb(pb, "csel", [128, 8, 33], F32); Tcsel = S.tile()
            bcast_load(csel[:].rearrange("p a b -> p (a b)"), csel_d, Tcsel, 8 * 33)
            ctmp = sb(pb, "ctmp", [128, 8, 33], F32); Tctmp = S.tile()
            cq = sb(pb, "cq", [128, 8, 8], F32); Tcq = S.tile()
            tri = sb(pb, "tri", [128, 128], F32); Ttri = S.tile()
            S.dma("sync", tri[:], tri_d, writes=[Ttri])
            onesf = sb(pb, "onesf", [128, 128], F32); Tones = S.tile()
            S.dma("sync", onesf[:], onesf_d, writes=[Tones])

            with ExitStack() as sw:
                st = make_sweep(sw, "b", [0, 1])
                wA = sb(sw, "wA", [128, 8, 512], BF16); TwA = S.tile()
                wB = sb(sw, "wB", [128, 8, 512], BF16); TwB = S.tile()
                wF = sb(sw, "wF", [128, 8, 8], BF16); TwF = S.tile()
                wload(wA[:], w_in[:, 2048:2560], TwA)
                wload(wB[:], w_in[:, 2560:3072], TwB)
                wload(wF[:], w_in[:, 3072:3080], TwF)
                kctr = [0]

                def plain_proj(blk, hT, ThT, w_, Tw_, dstT, TdstT, scale, slots=()):
                    for hp in range(4):
                        bk = 2 + (kctr[0] % 3)
                        kctr[0] += 1
                        mmgroup(banks[bk][:], [(w_[:, c, hp * 128:(hp + 1) * 128], hT[:, c, :]) for c in range(8)],
                                reads=list(ThT) + [Tw_], writes=[Tb[bk]])
                        S.op("scalar", lambda e, bk=bk, hp=hp: e.activation(
                            out=dstT[:, hp, blk * 512:(blk + 1) * 512], in_=banks[bk][:], func=AF.Copy, scale=scale),
                            reads=[Tb[bk]], writes=[TdstT[hp][blk]])
                        if hp < len(slots):
                            slots[hp]()

                def kvf_proj(blk, hT, ThT, slots=()):
                    plain_proj(blk, hT, ThT, wA, TwA, KT, TKT, 1.0, slots)
                    for tt in range(4):
                        n = blk * 4 + tt
                        bv = 6 + (tt % 2)
                        mmgroup(banks[bv][:], [(hT[:, c, tt * 128:(tt + 1) * 128], wB[:, c, :]) for c in range(8)],
                                reads=[ThT[tt], TwB], writes=[Tb[bv]])
                        S.op("vector", lambda e, n=n, bv=bv: e.tensor_copy(
                            out=Vf[:, n, :, 0:64], in_=banks[bv][:].rearrange("p (h d) -> p h d", h=8)),
                            reads=[Tb[bv]], writes=[TV[n]])
                        mmgroup(banks[5][:, 0:8], [(hT[:, c, tt * 128:(tt + 1) * 128], wF[:, c, :]) for c in range(8)],
                                reads=[ThT[tt], TwF], writes=[Tb[5]])
                        S.op("vector", lambda e, n=n: e.tensor_tensor(out=zt[:, n, :], in0=banks[5][:, 0:8], in1=bfb[:], op=ALU.add),
                             reads=[Tb[5], Tbfb], writes=[Tzt[n]])

                sweep_stage1(st, xb, 0, gb_attn, Tgba)
                sweep_stage1(st, xb, 1, gb_attn, Tgba)
                for blk in range(8):
                    sl = sweep_stage1(st, xb, blk + 2, gb_attn, Tgba, deferred=True) if blk + 2 < 8 else []
                    kvf_proj(blk, st["hT"][blk % 3], st["ThT"][blk % 3], sl)
                wload(wA[:], w_in[:, 1536:2048], TwA)
                sweep_stage1(st, xo, 0, gb_attn, Tgba)
                sweep_stage1(st, xo, 1, gb_attn, Tgba)
                for blk in range(4):
                    sl = sweep_stage1(st, xo, blk + 2, gb_attn, Tgba, deferred=True) if blk + 2 < 4 else []
                    plain_proj(blk, st["hT"][blk % 3], st["ThT"][blk % 3], wA, TwA, QT, TQT, 0.125, sl)
                ztf = zt[:].rearrange("p a b -> p (a b)")
                S.op("scalar", lambda e: e.activation(out=ztf, in_=ztf, func=AF.Exp, scale=-1.0), reads=Tzt, writes=Tzt)
                S.op("scalar", lambda e: e.activation(out=ztf, in_=ztf, func=AF.Ln, bias=1.0), reads=Tzt, writes=Tzt)
                S.op("vector", lambda e: e.memset(Cpos[:, 0, :], 0.0), writes=[TC[0]])
                for n in range(32):
                    bc = 2 + (n % 2)
                    S.op("tensor", lambda e, n=n, bc=bc: e.matmul(out=banks[bc][:, 0:8], lhsT=tri[:], rhs=zt[:, n, :], start=True, stop=True),
                         reads=[Ttri, Tzt[n]], writes=[Tb[bc]], signal=False)
                    S.op("tensor", lambda e, n=n, bc=bc: e.matmul(out=banks[bc][:, 8:16], lhsT=onesf[:], rhs=zt[:, n, :], start=True, stop=True),
                         reads=[Tones, Tzt[n]], writes=[Tb[bc]], signal=True)
                    S.op("vector", lambda e, n=n, bc=bc: e.tensor_tensor(out=Fpos[:, n, :], in0=banks[bc][:, 0:8], in1=Cpos[:, n, :], op=ALU.add),
                         reads=[Tb[bc], TC[n]], writes=[TF[n]])
                    S.op("vector", lambda e, n=n, bc=bc: e.tensor_tensor(out=Cpos[:, n + 1, :], in0=banks[bc][:, 8:16], in1=Cpos[:, n, :], op=ALU.add),
                         reads=[Tb[bc], TC[n]], writes=[TC[n + 1]])
                for i in range(8):
                    S.op("vector", lambda e, i=i: e.tensor_tensor(out=ctmp[:], in0=Cpos[:].rearrange("p n h -> p h n"),
                                                                  in1=csel[:, i, :].unsqueeze(1).broadcast_to([128, 8, 33]), op=ALU.mult),
                         reads=TC + [Tcsel, Tctmp], writes=[Tctmp])
                    S.op("vector", lambda e, i=i: e.tensor_reduce(out=cq[:, i, :], in_=ctmp[:], axis=AX.X, op=ALU.add),
                         reads=[Tctmp], writes=[Tcq])
                S.barrier()
                if "Fpos" in debug:
                    dbg_out("Fpos", [128, 32, 8]); dump("Fpos", Fpos[:], []); S.barrier()
                S.emit()

            with ExitStack() as at:
                PT = [sb(at, "PT%d" % i, [128, 32, 256], BF16) for i in range(2)]
                TPT = [[S.tile() for _ in range(16)] for _ in range(2)]
                Otf = sb(at, "Otf", [128, 16, 512], BF16); TOtf = [S.tile() for _ in range(16)]
                rr = sb(at, "rrf", [128, 2], F32); Trr = [S.tile() for _ in range(2)]
                biasb = [sb(at, "biasb%d" % i, [128, 32], F32) for i in range(2)]; Tbias = [S.tile() for _ in range(2)]
                units = [dict(hp=hp, hh=hh, i=i, head=2 * hp + hh) for hp in range(4) for hh in range(2) for i in range(8)]
                for ui, u in enumerate(units):
                    u["ui"] = ui

                def KT_of(u, kb):
                    r0 = 64 * u["hh"]
                    return KT[r0:r0 + 64, u["hp"], kb * 128:(kb + 1) * 128], TKT[u["hp"]][kb // 4]

                def QT_of(u):
                    r0 = 64 * u["hh"]
                    return QT[r0:r0 + 64, u["hp"], u["i"] * 256:(u["i"] + 1) * 256], TQT[u["hp"]][u["i"] // 2]

                ebb = [sb(at, "ebb%d" % i, [128, 32], F32) for i in range(2)]; Teb = [S.tile() for _ in range(2)]
                Vp = [sb(at, "Vp%d" % i, [128, 32, 65], BF16) for i in range(2)]; TVp = [S.tile() for _ in range(2)]

                def V_of(u, kb):
                    return Vp[u["ui"] % 2][:, kb, :], TVp[u["ui"] % 2]

                def exp_emit(u, p, bi, PTb, Tp):
                    b2 = u["ui"] % 2
                    hd = u["head"]
                    nk = nk_of(u["i"])
                    if p == 0:
                        S.op("vector", lambda e: e.tensor_scalar(out=biasb[b2][:, 0:nk], in0=Fpos[:, 0:nk, hd], scalar1=cq[:, u["i"], hd:hd + 1],
                                                                 scalar2=70.0, op0=ALU.subtract, op1=ALU.min),
                             reads=TF[0:nk] + [Tcq], writes=[Tbias[b2]])
                        S.op("scalar", lambda e: e.activation(out=ebb[b2][:, 0:nk], in_=biasb[b2][:, 0:nk], func=AF.Exp),
                             reads=[Tbias[b2]], writes=[Teb[b2]])
                        S.op("vector", lambda e: e.tensor_tensor(out=Vp[b2][:, 0:nk, :], in0=Vf[:, 0:nk, hd, 0:65],
                                                                 in1=ebb[b2][:, 0:nk].unsqueeze(2).broadcast_to([128, nk, 65]), op=ALU.mult),
                             reads=TV[0:nk] + [Teb[b2]], writes=[TVp[b2]])
                    S.op("scalar", lambda e: e.activation(out=PTb[:, 2 * p:2 * p + 2, :].rearrange("p a b -> p (a b)"),
                                                          in_=banks[bi][:], func=AF.Exp),
                         reads=[Tb[bi]], writes=[Tp])

                def mask_emit(u, PTb, TPb):
                    i = u["i"]
                    lo = nk_of(i) - 4
                    S.op("vector", lambda e: e.tensor_tensor(out=PTb[:, lo:lo + 4, :], in0=PTb[:, lo:lo + 4, :],
                                                             in1=maskf[:, i % 2, :].rearrange("p (a b) -> p a b", a=4), op=ALU.min),
                         reads=[Tmf, TPb[lo // 2], TPb[lo // 2 + 1]], writes=[TPb[lo // 2], TPb[lo // 2 + 1]])

                def evac_emit(u, ob):
                    hd, i = u["head"], u["i"]
                    for s in range(2):
                        qb = 2 * i + s
                        S.op("vector", lambda e, s=s: e.reciprocal(out=rr[:, s:s + 1], in_=banks[ob][:, s * 65 + 64:s * 65 + 65]),
                             reads=[Tb[ob]], writes=[Trr[s]])
                        S.op("vector", lambda e, s=s, qb=qb: e.tensor_scalar(
                            out=Otf[:, qb, hd * 64:(hd + 1) * 64], in0=banks[ob][:, s * 65:s * 65 + 64], scalar1=rr[:, s:s + 1],
                            scalar2=None, op0=ALU.mult),
                            reads=[Tb[ob], Trr[s]], writes=[TOtf[qb]])

                OTs = [sb(at, "OTsf%d" % i, [128, 256], F32) for i in range(2)]; TOTs = [S.tile() for _ in range(2)]
                identf_b = sb(at, "identf_b", [128, 128], F32); Tidf_b = S.tile()
                S.dma("sync", identf_b[:], identf_d, writes=[Tidf_b])
                run_attention(units, PT, TPT, KT_of, QT_of, V_of, 65, exp_emit, evac_emit, mask_emit,
                              s_banks=[0, 1, 2, 3], o_banks=[4, 5, 6, 7], OTs=OTs, TOTs=TOTs, identf=identf_b, Tidf=Tidf_b)
                for qb in range(16):
                    bi = qb % 2
                    pTv = banks[bi][:].bitcast(BF16)
                    for c in range(4):
                        S.op("tensor", lambda e, c=c, qb=qb, pTv=pTv: e.transpose(
                            out=pTv[:, c * 128:(c + 1) * 128], in_=Otf[:, qb, c * 128:(c + 1) * 128], identity=identb[:]),
                            reads=[TOtf[qb], Tidb], writes=[Tb[bi]], signal=(c == 3))
                    S.op("vector", lambda e, qb=qb, pTv=pTv: e.tensor_copy(
                        out=OT[:, 4:8, qb * 128:(qb + 1) * 128], in_=pTv[:, 0:512].rearrange("p (c t) -> p c t", c=4)),
                        reads=[Tb[bi]], writes=[TOT[qb]])
                S.barrier()
                if "OTb" in debug:
                    dbg_out("OTb", [128, 8, 2048], BF16); dump("OTb", OT[:], []); S.barrier()
                S.emit()
        if stop_after == "B":
            return nc

        with ExitStack() as pc:
            x2 = sb(pc, "x2", [128, 16, 1024], F32); Tx2 = [[S.tile() for _ in range(2)] for _ in range(16)]
            hmT = OT
            ThmT = TOT
            ovf = sb(pc, "ovf", [128, 1], I32); Tovf = S.tile()
            w12 = sb(pc, "w12", [128, 2, 16], F32); Tw12 = S.tile()
            pos = sb(pc, "pos", [128, 2, 16], I32); Tpos = S.tile()
            comb = sb(pc, "comb", [128, 16, 16], F32); Tcomb = S.tile()
            junk = sb(pc, "junkc", [128, 1024], BF16); Tjunkc = S.tile()
            ssc = sb(pc, "ssc", [128, 16], F32); Tssc = [S.tile() for _ in range(16)]; Tsscall = S.tile()
            with ExitStack() as c1:
                wo = sb(c1, "wo", [128, 8, 1024], BF16); Two = S.tile()
                wload(wo[:], w_out, Two)
                xt = [sb(c1, "xc%d" % i, [128, 1024], F32) for i in range(2)]; Txt = [S.tile() for _ in range(2)]
                rw32 = sb(c1, "rw32", [128, 8, 20], F32); Trw = S.tile()
                S.dma("sync", rw32[:], rw_d.rearrange("(c p) n -> p c n", p=128), writes=[Trw])
                rbb = sb(c1, "rbb", [128, 20], F32); Trbb = S.tile()
                bcast_load(rbb[:], rb_d, Trbb, 20)
                gbf = sb(c1, "gbf", [128, 1024], F32); Tgbf = S.tile()
                bcast_load(gbf[:], g_ffn, Tgbf, 1024)
                identf = sb(c1, "identf", [128, 128], F32); Tidf = S.tile()
                S.dma("sync", identf[:], identf_d, writes=[Tidf])
                hm32 = [sb(c1, "hm32%d" % i, [128, 1024], F32) for i in range(2)]; Thm32 = [S.tile() for _ in range(2)]
                hmT32 = [sb(c1, "hmT32%d" % i, [128, 8, 128], F32) for i in range(2)]; ThmT32 = [S.tile() for _ in range(2)]
                Lall = sb(c1, "Lall", [128, 16, 20], F32); TL = [S.tile() for _ in range(16)]
                if moe == "sparse":
                    hmb = sb(c1, "hmb", [128, 16, 1024], BF16); Thmb = [S.tile() for _ in range(16)]
                for t in range(16):
                    S.dma("sync", xt[t % 2][:], xo[t * 128:(t + 1) * 128, :], writes=[Txt[t % 2]])
                    for hf in range(2):
                        mmgroup(banks[hf][:], [(OT[:, c, t * 128:(t + 1) * 128], wo[:, c, hf * 512:(hf + 1) * 512]) for c in range(8)],
                                reads=[TOT[t], Two], writes=[Tb[hf]])
                        S.op("vector", lambda e, t=t, hf=hf: e.tensor_tensor(
                            out=x2[:, t, hf * 512:(hf + 1) * 512], in0=banks[hf][:], in1=xt[t % 2][:, hf * 512:(hf + 1) * 512], op=ALU.add),
                            reads=[Tb[hf], Txt[t % 2]], writes=[Tx2[t][hf]])
                    S.op("scalar", lambda e, t=t: e.activation(out=junk[:], in_=x2[:, t, :], func=AF.Square, accum_out=ssc[:, t:t + 1]),
                         reads=Tx2[t], writes=[Tssc[t], Tjunkc])
                if "x2" in debug:
                    dbg_out("x2", [2048, 1024])
                    for t in range(16):
                        dump("x2", x2[:, t, :], Tx2[t]) if False else S.dma("sync", dbg["x2"][t * 128:(t + 1) * 128, :], x2[:, t, :], reads=Tx2[t], writes=[Tdbg], semtile=Tdbg)
                if stop_after == "C0":
                    S.barrier(); S.emit()
                    return nc
                S.op("scalar", lambda e: e.activation(out=ssc[:], in_=ssc[:], func=AF.Sqrt, scale=1.0 / 1024, bias=EPS),
                     reads=Tssc, writes=[Tsscall])
                S.op("vector", lambda e: e.reciprocal(out=ssc[:], in_=ssc[:]), reads=[Tsscall], writes=[Tsscall])
                def hm_front(t):
                    t2 = t % 2
                    S.op("vector", lambda e, t=t, t2=t2: e.scalar_tensor_tensor(
                        out=hm32[t2][:], in0=x2[:, t, :], scalar=ssc[:, t:t + 1], in1=gbf[:], op0=ALU.mult, op1=ALU.mult),
                        reads=Tx2[t] + [Tsscall, Tgbf], writes=[Thm32[t2]])
                    ba, bb = (2, 3) if t2 == 0 else (4, 5)
                    for c in range(8):
                        bk = ba if c < 4 else bb
                        S.op("tensor", lambda e, c=c, t2=t2, bk=bk: e.transpose(
                            out=banks[bk][:, (c % 4) * 128:(c % 4 + 1) * 128], in_=hm32[t2][:, c * 128:(c + 1) * 128], identity=identf[:]),
                            reads=[Thm32[t2], Tidf], writes=[Tb[bk]], signal=(c % 4 == 3))

                def hm_back(t):
                    t2 = t % 2
                    ba, bb = (2, 3) if t2 == 0 else (4, 5)
                    for k, bk in enumerate((ba, bb)):
                        src = banks[bk][:].rearrange("p (c t) -> p c t", c=4)
                        S.op("scalar", lambda e, k=k, t2=t2, src=src: e.activation(out=hmT32[t2][:, 4 * k:4 * k + 4, :], in_=src, func=AF.Copy),
                             reads=[Tb[bk]], writes=[ThmT32[t2]])
                        S.op("gpsimd", lambda e, k=k, t=t, t2=t2: e.tensor_copy(out=hmT[:, 4 * k:4 * k + 4, t * 128:(t + 1) * 128], in_=hmT32[t2][:, 4 * k:4 * k + 4, :]),
                             reads=[ThmT32[t2]], writes=[ThmT[t]])
                    if moe == "sparse":
                        S.op("scalar", lambda e, t=t, t2=t2: e.activation(out=hmb[:, t, :], in_=hm32[t2][:], func=AF.Copy), reads=[Thm32[t2]], writes=[Thmb[t]])

                def hm_router(t):
                    t2 = t % 2
                    br = 6 + t2
                    mmgroup(banks[br][:, 0:20], [(hmT32[t2][:, c, :], rw32[:, c, :]) for c in range(8)],
                            reads=[ThmT32[t2], Trw], writes=[Tb[br]])
                    S.op("vector", lambda e, t=t, br=br: e.tensor_tensor(out=Lall[:, t, :], in0=banks[br][:, 0:20], in1=rbb[:], op=ALU.add),
                         reads=[Tb[br], Trbb], writes=[TL[t]])
                hm_front(0)
                for t in range(16):
                    hm_back(t)
                    if t + 1 < 16:
                        hm_front(t + 1)
                    hm_router(t)
                if stop_after == "C1a":
                    if "Lall" in debug:
                        dbg_out("Lall", [128, 320])
                        S.dma("sync", dbg["Lall"], Lall[:].rearrange("p a b -> p (a b)"), reads=[], writes=[Tdbg], semtile=Tdbg)
                    S.barrier(); S.emit()
                    return nc
                TR = S.tile()

                def rt(name, shape):
                    return sb(c1, "rt_" + name, shape, F32)
                gmax = rt("gmax", [128, 16]); gm = rt("gm", [128, 16, 4]); gd = rt("gd", [128, 16, 4])
                gsum = rt("gsum", [128, 16]); gw = rt("gw", [128, 16]); pen = rt("pen", [128, 16, 4])
                EL = rt("EL", [128, 16, 16]); EL2 = rt("EL2", [128, 16, 16]); m1 = rt("m1", [128, 16]); m2 = rt("m2", [128, 16])
                oh1 = rt("oh1", [128, 16, 16]); oh2 = rt("oh2", [128, 16, 16]); dd = rt("dd", [128, 16]); w1 = rt("w1", [128, 16]); w2 = rt("w2", [128, 16])
                LG = Lall[:, :, 0:4]
                LE4 = Lall[:, :, 4:20].rearrange("p t (g e) -> p t g e", g=4)
                EL4 = EL[:].rearrange("p t (g e) -> p t g e", g=4)

                def vop(fn, first=False):
                    S.op("vector", fn, reads=(TL + [TR]) if first else [TR], writes=[TR])

                def bc3(a, n):
                    return a[:].unsqueeze(2).broadcast_to([128, 16, n])
                vop(lambda e: e.tensor_reduce(out=gmax[:], in_=LG, axis=AX.X, op=ALU.max), first=True)
                vop(lambda e: e.tensor_tensor(out=gm[:], in0=LG, in1=bc3(gmax, 4), op=ALU.is_equal))
                vop(lambda e: e.tensor_tensor(out=gd[:], in0=LG, in1=bc3(gmax, 4), op=ALU.subtract))
                S.op("scalar", lambda e: e.activation(out=gd[:], in_=gd[:], func=AF.Exp), reads=[TR], writes=[TR])
                vop(lambda e: e.tensor_reduce(out=gsum[:], in_=gd[:], axis=AX.X, op=ALU.add))
                vop(lambda e: e.reciprocal(out=gw[:], in_=gsum[:]))
                vop(lambda e: e.tensor_scalar(out=pen[:], in0=gm[:], scalar1=1.0, scalar2=1e30, op0=ALU.subtract, op1=ALU.mult))
                vop(lambda e: e.tensor_tensor(out=EL4, in0=LE4, in1=gm[:].unsqueeze(3).broadcast_to([128, 16, 4, 4]), op=ALU.mult))
                vop(lambda e: e.tensor_tensor(out=EL4, in0=EL4, in1=pen[:].unsqueeze(3).broadcast_to([128, 16, 4, 4]), op=ALU.add))
                vop(lambda e: e.tensor_reduce(out=m1[:], in_=EL[:], axis=AX.X, op=ALU.max))
                vop(lambda e: e.tensor_tensor(out=oh1[:], in0=EL[:], in1=bc3(m1, 16), op=ALU.is_equal))
                vop(lambda e: e.scalar_tensor_tensor(out=EL2[:], in0=oh1[:], scalar=-1e30, in1=EL[:], op0=ALU.mult, op1=ALU.add))
                vop(lambda e: e.tensor_reduce(out=m2[:], in_=EL2[:], axis=AX.X, op=ALU.max))
                vop(lambda e: e.tensor_tensor(out=oh2[:], in0=EL2[:], in1=bc3(m2, 16), op=ALU.is_equal))
                vop(lambda e: e.tensor_tensor(out=dd[:], in0=m2[:], in1=m1[:], op=ALU.subtract))
                S.op("scalar", lambda e: e.activation(out=dd[:], in_=dd[:], func=AF.Exp), reads=[TR], writes=[TR])
                vop(lambda e: e.tensor_scalar(out=w1[:], in0=dd[:], scalar1=1.0, scalar2=None, op0=ALU.add))
                vop(lambda e: e.reciprocal(out=w1[:], in_=w1[:]))
                vop(lambda e: e.tensor_tensor(out=w1[:], in0=w1[:], in1=gw[:], op=ALU.mult))
                vop(lambda e: e.tensor_tensor(out=w2[:], in0=dd[:], in1=w1[:], op=ALU.mult))
                if moe == "sparse":
                    Mb = sb(c1, "Mb", [128, 16, 16], BF16)
                    ustrict = sb(c1, "ustrict", [128, 128], BF16); Tus = S.tile()
                    S.dma("sync", ustrict[:], ustrict_d, writes=[Tus])
                    onesb = sb(c1, "onesb", [128, 128], BF16); Tob_ = S.tile()
                    S.dma("sync", onesb[:], onesb_d, writes=[Tob_])
                    ebase = sb(c1, "ebase", [128, 16, 16], F32); Teb = S.tile()
                    S.dma("sync", ebase[:].rearrange("p a b -> p (a b)"), ebase_d, writes=[Teb])
                    slotf = rt("slotf", [128, 16, 16]); okf = rt("okf", [128, 16, 16]); posf = rt("posf", [128, 2, 16])
                    vop(lambda e: e.tensor_tensor(out=Mb[:], in0=oh1[:], in1=oh2[:], op=ALU.add))
                    for t in range(16):
                        prs = [(onesb[:], Mb[:, tp, :]) for tp in range(t)] + [(ustrict[:], Mb[:, t, :])]
                        n_ = len(prs)
                        for k_, (l_, r_) in enumerate(prs):
                            S.op("tensor", lambda e, l_=l_, r_=r_, k_=k_, n_=n_, t=t: e.matmul(out=banks[0][:, t * 16:(t + 1) * 16], lhsT=l_, rhs=r_,
                                                                                         start=(k_ == 0), stop=(k_ == n_ - 1)),
                                 reads=[TR, Tus, Tob_], writes=[Tb[0]], signal=(k_ == n_ - 1))
                    for tp in range(16):
                        S.op("tensor", lambda e, tp=tp: e.matmul(out=banks[1][:, 0:16], lhsT=onesb[:], rhs=Mb[:, tp, :], start=(tp == 0), stop=(tp == 15)),
                             reads=[TR, Tob_], writes=[Tb[1]], signal=(tp == 15))
                    cmax = rt("cmax", [128, 1])
                    S.op("vector", lambda e: e.tensor_reduce(out=cmax[:], in_=banks[1][:, 0:16], axis=AX.X, op=ALU.max), reads=[Tb[1], TR], writes=[TR])
                    import os as _os
                    thr = -1.0 if _os.environ.get("FORCE_DENSE") else float(CAP)
                    vop(lambda e: e.tensor_scalar(out=cmax[:], in0=cmax[:], scalar1=thr, scalar2=None, op0=ALU.is_gt))
                    S.op("vector", lambda e: e.tensor_copy(out=ovf[:], in_=cmax[:]), reads=[TR], writes=[Tovf])
                    rank = banks[0][:, 0:256].rearrange("p (a b) -> p a b", a=16)
                    S.op("vector", lambda e: e.tensor_tensor(out=slotf[:], in0=rank, in1=ebase[:], op=ALU.add), reads=[Tb[0], Teb, TR], writes=[TR])
                    vop(lambda e: e.tensor_scalar(out=okf[:], in0=slotf[:], scalar1=None, scalar2=None, op0=ALU.bypass) if False else
                        e.tensor_tensor(out=okf[:], in0=slotf[:], in1=ebase[:], op=ALU.subtract))
                    vop(lambda e: e.tensor_scalar(out=okf[:], in0=okf[:], scalar1=float(CAP), scalar2=1.0e6, op0=ALU.is_ge, op1=ALU.mult))
                    vop(lambda e: e.tensor_tensor(out=slotf[:], in0=slotf[:], in1=okf[:], op=ALU.add))
                    vop(lambda e: e.tensor_tensor(out=okf[:], in0=slotf[:], in1=oh1[:], op=ALU.mult))
                    vop(lambda e: e.tensor_reduce(out=posf[:, 0, :], in_=okf[:], axis=AX.X, op=ALU.add))
                    vop(lambda e: e.tensor_tensor(out=okf[:], in0=slotf[:], in1=oh2[:], op=ALU.mult))
                    vop(lambda e: e.tensor_reduce(out=posf[:, 1, :], in_=okf[:], axis=AX.X, op=ALU.add))
                    S.op("vector", lambda e: e.tensor_copy(out=pos[:], in_=posf[:]), reads=[TR], writes=[Tpos])
                    S.op("vector", lambda e: e.tensor_copy(out=w12[:, 0, :], in_=w1[:]), reads=[TR, Tw12], writes=[Tw12])
                    S.op("vector", lambda e: e.tensor_copy(out=w12[:, 1, :], in_=w2[:]), reads=[TR, Tw12], writes=[Tw12])
                    Tsc = [S.tile() for _ in range(32)]
                    set_bcreg()
                    for t in range(16):
                        for k_ in range(2):
                            S.dma_fn("gpsimd", lambda e, t=t, k_=k_: e.indirect_dma_start(
                                out=xs_d[:, :], out_offset=bass.IndirectOffsetOnAxis(ap=pos[:, k_, t:t + 1], axis=0),
                                in_=hmb[:, t, :], in_offset=None, bounds_check=bcreg, oob_is_err=False),
                                reads=[Thmb[t], Tpos, Txs], writes=[Tsc[2 * t + k_]], semtile=Thmb[t])
                vop(lambda e: e.tensor_tensor(out=oh1[:], in0=oh1[:], in1=bc3(w1, 16), op=ALU.mult))
                vop(lambda e: e.tensor_tensor(out=oh2[:], in0=oh2[:], in1=bc3(w2, 16), op=ALU.mult))
                S.op("vector", lambda e: e.tensor_tensor(out=comb[:], in0=oh1[:], in1=oh2[:], op=ALU.add), reads=[TR], writes=[Tcomb])
                S.barrier()
                if "comb" in debug:
                    dbg_out("comb", [128, 256])
                    S.dma("sync", dbg["comb"], comb[:].rearrange("p a b -> p (a b)"), reads=[Tcomb], writes=[Tdbg], semtile=Tdbg)
                    S.barrier()
                S.emit()
            if stop_after == "C1":
                return nc

            with ExitStack() as c2:
                wgb = [sb(c2, "wgb%d" % i, [128, 8, 512], BF16) for i in range(2)]; Twg4 = [[S.tile() for _ in range(4)] for _ in range(2)]
                wub = [sb(c2, "wub%d" % i, [128, 8, 512], BF16) for i in range(2)]; Twu4 = [[S.tile() for _ in range(4)] for _ in range(2)]
                wdb = [sb(c2, "wdb%d" % i, [128, 4, 1024], BF16) for i in range(2)]; Twd4 = [[S.tile() for _ in range(4)] for _ in range(2)]
                stg = [sb(c2, "stg%d" % i, [128, 1024], F32) for i in range(3)]; Tstg = [S.tile() for _ in range(3)]
                sq_ = [0]
                aT = [sb(c2, "aT%d" % i, [128, 4, 512], BF16) for i in range(2)]; TaT = [[S.tile() for _ in range(4)] for _ in range(2)]
                sg = [sb(c2, "sg%d" % i, [128, 512], F32) for i in range(2)]; Tsg = [S.tile() for _ in range(2)]
                xg = [sb(c2, "xg%d" % i, [128, 1024], BF16) for i in range(4)]; Txg = [S.tile() for _ in range(4)]
                xgT = [sb(c2, "xgT%d" % i, [128, 8, CAP], BF16) for i in range(2)]; TxgT = [[S.tile() for _ in range(CAP // 128)] for _ in range(2)]
                ysb = [sb(c2, "ysb%d" % i, [128, 1024], F32) for i in range(2)]; Tysb = [S.tile() for _ in range(2)]
                NJ = CAP // 128
                Tys = [S.tile() for _ in range(NEXP * NJ)]

                def w_steps(ex):
                    b2 = ex % 2
                    dmas, casts = [], []
                    for k in range(12):
                        def mk(k=k):
                            if k < 8:
                                srcw = (wg_d if k < 4 else wu_d)[ex]
                                kk = k % 4
                                src = srcw[kk * 256:(kk + 1) * 256, :].rearrange("(c p) n -> p c n", p=128)
                                dst_of = lambda: (wgb if k < 4 else wub)[b2][:, 2 * kk:2 * kk + 2, :]
                                Td = (Twg4 if k < 4 else Twu4)[b2][kk]
                                view = lambda t_: t_[:].rearrange("p (c n) -> p c n", c=2)
                            else:
                                kk = k - 8
                                src = wd_d[ex][kk * 128:(kk + 1) * 128, :]
                                dst_of = lambda: wdb[b2][:, kk, :]
                                Td = Twd4[b2][kk]
                                view = lambda t_: t_[:]
                            cell = {}

                            def d():
                                si = sq_[0] % 3
                                sq_[0] += 1
                                cell["si"] = si
                                S.dma("sync", view(stg[si]), src, writes=[Tstg[si]])

                            def c():
                                si = cell["si"]
                                sv = view(stg[si])
                                dstb = dst_of()
                                if k % 2 == 1:
                                    S.op("scalar", lambda e: e.activation(out=dstb, in_=sv, func=AF.Copy), reads=[Tstg[si]], writes=[Td])
                                else:
                                    S.op("vector", lambda e: e.tensor_copy(out=dstb, in_=sv), reads=[Tstg[si]], writes=[Td])
                            return d, c
                        d, c = mk()
                        dmas.append(d)
                        casts.append(c)
                    steps = dmas[0:3]
                    for k in range(12):
                        steps.append(casts[k])
                        if k + 3 < 12:
                            steps.append(dmas[k + 3])
                    return steps

                def load_w(ex):
                    for f in w_steps(ex):
                        f()

                def gate_up(b2, rhs_of, Trhs, width, gq, slot=None):
                    for ft in range(4):
                        bg, bu = (0, 1) if gq[0] % 2 == 0 else (2, 3)
                        s2 = gq[0] % 2
                        gq[0] += 1
                        mmgroup(banks[bg][:, 0:width], [(wgb[b2][:, c, ft * 128:(ft + 1) * 128], rhs_of(c)) for c in range(8)],
                                reads=Trhs + Twg4[b2], writes=[Tb[bg]])
                        mmgroup(banks[bu][:, 0:width], [(wub[b2][:, c, ft * 128:(ft + 1) * 128], rhs_of(c)) for c in range(8)],
                                reads=Trhs + Twu4[b2], writes=[Tb[bu]])
                        S.op("scalar", lambda e, bg=bg, s2=s2: e.activation(out=sg[s2][:, 0:width], in_=banks[bg][:, 0:width], func=AF.Silu),
                             reads=[Tb[bg]], writes=[Tsg[s2]])
                        S.op("vector", lambda e, bu=bu, s2=s2, ft=ft: e.tensor_tensor(out=aT[b2][:, ft, 0:width], in0=sg[s2][:, 0:width], in1=banks[bu][:, 0:width], op=ALU.mult),
                             reads=[Tsg[s2], Tb[bu]], writes=[TaT[b2][ft]])
                        if slot is not None:
                            slot()

                S.branch_begin()
                gq = [0]; yq = [0]; xq = [0]

                def prep(ex):
                    b2 = ex % 2
                    for j in range(NJ):
                        xi = xq[0] % 4
                        xq[0] += 1
                        r0 = ex * CAP + j * 128
                        S.dma("gpsimd", xg[xi][:], xs_d[r0:r0 + 128, :], reads=Tsc + [Txs], writes=[Txg[xi]])
                        bi = 6 + (xq[0] % 2)
                        pTv = banks[bi][:].bitcast(BF16)
                        for c in range(8):
                            S.op("tensor", lambda e, c=c, xi=xi, pTv=pTv: e.transpose(
                                out=pTv[:, c * 128:(c + 1) * 128], in_=xg[xi][:, c * 128:(c + 1) * 128], identity=identb[:]),
                                reads=[Txg[xi], Tidb], writes=[Tb[bi]], signal=(c == 7))
                        S.op("vector", lambda e, j=j, b2=b2, pTv=pTv: e.tensor_copy(
                            out=xgT[b2][:, :, j * 128:(j + 1) * 128], in_=pTv.rearrange("p (c t) -> p c t", c=8)),
                            reads=[Tb[bi]], writes=[TxgT[b2][j]])
                prep(0)
                load_w(0)
                for ex in range(NEXP):
                    b2 = ex % 2
                    wq = w_steps(ex + 1) if ex + 1 < NEXP else []

                    def pop(n):
                        for _ in range(n):
                            if wq:
                                wq.pop(0)()
                    pop(3)
                    gate_up(b2, lambda c, b2=b2: xgT[b2][:, c, :], TxgT[b2], CAP, gq, slot=lambda: pop(3))
                    if ex + 1 < NEXP:
                        prep(ex + 1)
                    for j in range(NJ):
                        y2 = yq[0] % 2
                        yq[0] += 1
                        for hf in range(2):
                            by = 4 + hf
                            mmgroup(banks[by][:], [(aT[b2][:, ft, j * 128:(j + 1) * 128], wdb[b2][:, ft, hf * 512:(hf + 1) * 512]) for ft in range(4)],
                                    reads=TaT[b2] + Twd4[b2], writes=[Tb[by]])
                            if hf == 0:
                                S.op("vector", lambda e, y2=y2, by=by: e.tensor_copy(out=ysb[y2][:, 0:512], in_=banks[by][:]),
                                     reads=[Tb[by]], writes=[Tysb[y2]])
                            else:
                                S.op("scalar", lambda e, y2=y2, by=by: e.activation(out=ysb[y2][:, 512:1024], in_=banks[by][:], func=AF.Copy),
                                     reads=[Tb[by], Tysb[y2]], writes=[Tysb[y2]])
                        r0 = ex * CAP + j * 128
                        S.dma("gpsimd", ys_d[r0:r0 + 128, :], ysb[y2][:], reads=[Tysb[y2]], writes=[Tys[ex * NJ + j]], semtile=Tysb[y2])
                        pop(3)
                    pop(99)
                S.barrier()
                ygl = []
                for wb in wgb + wub + wdb:
                    v = wb[:].rearrange("p a b -> p (a b)").bitcast(F32)
                    ygl += [v[:, 0:1024], v[:, 1024:2048]]
                Tyg = [S.tile() for _ in ygl]
                set_bcreg()
                def gath(i):
                    t, k_ = i // 2, i % 2
                    gi = i % len(ygl)
                    S.dma_fn("gpsimd", lambda e, t=t, k_=k_, gi=gi: e.indirect_dma_start(
                        out=ygl[gi], out_offset=None, in_=ys_d[:, :],
                        in_offset=bass.IndirectOffsetOnAxis(ap=pos[:, k_, t:t + 1], axis=0),
                        bounds_check=bcreg, oob_is_err=False),
                        reads=Tys + [Tpos], writes=[Tyg[gi]], semtile=Tyg[gi])

                def acc(i):
                    t, k_ = i // 2, i % 2
                    gi = i % len(ygl)
                    for hf in range(2):
                        S.op("vector", lambda e, t=t, k_=k_, gi=gi, hf=hf: e.scalar_tensor_tensor(
                            out=x2[:, t, hf * 512:(hf + 1) * 512], in0=ygl[gi][:, hf * 512:(hf + 1) * 512], scalar=w12[:, k_, t:t + 1],
                            in1=x2[:, t, hf * 512:(hf + 1) * 512], op0=ALU.mult, op1=ALU.add),
                            reads=[Tyg[gi], Tw12, Tx2[t][hf]], writes=[Tx2[t][hf]])
                depth = len(ygl) - 1
                for i in range(32 + depth):
                    if i < 32:
                        gath(i)
                    if i - depth >= 0:
                        acc(i - depth)
                S.branch_mid()
                gq = [0]; yq = [0]
                load_w(0)
                for ex in range(NEXP):
                    b2 = ex % 2
                    if ex + 1 < NEXP:
                        load_w(ex + 1)
                    for tb in range(4):
                        gate_up(b2, lambda c, tb=tb: hmT[:, c, tb * 512:(tb + 1) * 512], ThmT[tb * 4:tb * 4 + 4], 512, gq)
                        for tt in range(4):
                            t = tb * 4 + tt
                            for hf in range(2):
                                by = 4 + (yq[0] % 4)
                                yq[0] += 1
                                mmgroup(banks[by][:], [(aT[b2][:, ft, tt * 128:(tt + 1) * 128], wdb[b2][:, ft, hf * 512:(hf + 1) * 512]) for ft in range(4)],
                                        reads=TaT[b2] + Twd4[b2], writes=[Tb[by]])
                                S.op("vector", lambda e, t=t, hf=hf, by=by, ex=ex: e.scalar_tensor_tensor(
                                    out=x2[:, t, hf * 512:(hf + 1) * 512], in0=banks[by][:], scalar=comb[:, t, ex:ex + 1],
                                    in1=x2[:, t, hf * 512:(hf + 1) * 512], op0=ALU.mult, op1=ALU.add),
                                    reads=[Tb[by], Tcomb, Tx2[t][hf]], writes=[Tx2[t][hf]])
                S.branch_end(ovf[0:1, 0:1], brregs)
                S.emit()

            with ExitStack() as c3:
                gbn = sb(c3, "gbn", [128, 1024], F32); Tgbn = S.tile()
                bcast_load(gbn[:], g_fin, Tgbn, 1024)
                ob = [sb(c3, "ob%d" % i, [128, 1024], F32) for i in range(2)]; Tob = [S.tile() for _ in range(2)]
                Tout = S.tile()
                for t in range(16):
                    S.op("scalar", lambda e, t=t: e.activation(out=junk[:], in_=x2[:, t, :], func=AF.Square, accum_out=ssc[:, t:t + 1]),
                         reads=Tx2[t] + [Tsscall], writes=[Tssc[t], Tjunkc])
                S.op("scalar", lambda e: e.activation(out=ssc[:], in_=ssc[:], func=AF.Sqrt, scale=1.0 / 1024, bias=EPS),
                     reads=Tssc, writes=[Tsscall])
                S.op("vector", lambda e: e.reciprocal(out=ssc[:], in_=ssc[:]), reads=[Tsscall], writes=[Tsscall])
                for t in range(16):
                    S.op("vector", lambda e, t=t: e.scalar_tensor_tensor(
                        out=ob[t % 2][:], in0=x2[:, t, :], scalar=ssc[:, t:t + 1], in1=gbn[:], op0=ALU.mult, op1=ALU.mult),
                        reads=Tx2[t] + [Tsscall, Tgbn], writes=[Tob[t % 2]])
                    S.dma("sync", out[t * 128:(t + 1) * 128, :], ob[t % 2][:], reads=[Tob[t % 2]], writes=[Tout], semtile=Tob[t % 2])
                S.barrier()
                S.emit()
    return nc


def _const_tables():
    f32 = np.float32
    inv_freq = (f32(1.0) / (f32(10000.0) ** (np.arange(0, 64, 2, dtype=f32) / f32(64)))).astype(f32)
    pos = np.arange(4096, dtype=f32)
    ang = (pos[:, None] * inv_freq[None, :]).astype(f32)
    cos = np.cos(ang).astype(f32)
    sin = np.sin(ang).astype(f32)
    r = np.arange(128)
    dh = r % 64
    cosT = cos[:, dh % 32].T.copy()
    sgn = np.where(dh < 32, -1.0, 1.0).astype(f32)
    sinT = (sin[:, dh % 32].T * sgn[:, None]).astype(f32)
    return cosT, sinT


def _masks(hf):
    k = np.arange(128)[:, None, None]
    r = np.arange(4)[None, :, None]
    q = np.arange(256)[None, None, :]
    md = np.zeros((128, 2, 4, 256), np.float32)
    mf = np.zeros((128, 2, 4, 256), np.float32)
    for par in range(2):
        if par == 0:
            kb = r
            j = 0 if hf == 0 else 1
        else:
            kb = 4 + r
            j = 3 if hf == 0 else 2
        s = kb * 128 + k
        t = j * 256 + q
        mf[:, par] = np.where(s <= t, 3e38, 0.0)
        md[:, par] = np.where((s // 64) <= (t // 64), 3e38, 0.0)
    return (md.reshape(128, 2, 1024).astype(ml_dtypes.bfloat16),
            mf.reshape(128, 2, 1024).astype(ml_dtypes.bfloat16))


def own_tokens(hf):
    return np.concatenate([np.arange(j * 256, (j + 1) * 256) for j in own_qtiles(hf)])


def prep(inputs):
    f32 = np.float32
    x = np.asarray(inputs["x"], f32)
    w_in = np.ascontiguousarray(np.asarray(inputs["w_in"], f32)[0])

    def swap_cols(w):
        return np.ascontiguousarray(w.reshape(1024, 8, 2, 32)[:, :, ::-1, :].reshape(1024, 512))

    cosT, sinT = _const_tables()
    common = {
        "w_in": w_in,
        "wqs": swap_cols(w_in[:, 0:512]),
        "wks": swap_cols(w_in[:, 512:1024]),
        "cosk": cosT, "sink": sinT,
        "identb": np.eye(128, dtype=f32).astype(ml_dtypes.bfloat16),
        "identf": np.eye(128, dtype=f32),
        "tri": np.triu(np.ones((128, 128), f32)),
        "onesf": np.ones((128, 128), f32),
        "onesb": np.ones((128, 128), f32).astype(ml_dtypes.bfloat16),
        "ustrict": np.triu(np.ones((128, 128), f32), 1).astype(ml_dtypes.bfloat16),
        "ebase": np.ascontiguousarray(np.broadcast_to((np.arange(16, dtype=f32) * CAP)[None, None, :], (128, 16, 16)).reshape(128, 256)),
        "g_attn": np.asarray(inputs["norm_attn_g"], f32).reshape(1, 1024),
        "g_ffn": np.asarray(inputs["norm_ffn_g"], f32).reshape(1, 1024),
        "g_fin": np.asarray(inputs["norm_final_g"], f32).reshape(1, 1024),
        "bfor": np.asarray(inputs["b_forget"], f32).reshape(1, 8),
        "lamv": np.concatenate([np.asarray(inputs[k], f32).reshape(1, 64) for k in
                                ("lambda_q1", "lambda_k1", "lambda_q2", "lambda_k2")], axis=1),
        "dng": np.asarray(inputs["diff_norm_g"], f32).reshape(1, 128),
        "w_out": np.ascontiguousarray(np.asarray(inputs["w_out"], f32)[0]),
        "rw": np.ascontiguousarray(np.concatenate([np.asarray(inputs["router_group_w"], f32)[0],
                                                   np.asarray(inputs["router_expert_w"], f32)[0]], axis=1)),
        "rb": np.concatenate([np.asarray(inputs["router_group_b"], f32).reshape(1, 4),
                              np.asarray(inputs["router_expert_b"], f32).reshape(1, 16)], axis=1),
        "wg": np.ascontiguousarray(np.asarray(inputs["w_gate"], f32)[0]),
        "wu": np.ascontiguousarray(np.asarray(inputs["w_up"], f32)[0]),
        "wd": np.ascontiguousarray(np.asarray(inputs["w_down"], f32)[0]),
    }
    in_maps = []
    for c in range(8):
        b, hf = c // 2, c % 2
        tok = own_tokens(hf)
        md, mf = _masks(hf)
        m = dict(common)
        m["xb"] = np.ascontiguousarray(x[b])
        m["xo"] = np.ascontiguousarray(x[b][tok])
        m["cosq"] = np.ascontiguousarray(cosT[:, tok] * f32(0.125))
        m["sinq"] = np.ascontiguousarray(sinT[:, tok] * f32(0.125))
        cs = np.zeros((8, 33), f32)
        for i, j in enumerate(own_qtiles(hf)):
            cs[i, 2 * j + 1] = 1.0
        m["csel"] = cs.reshape(1, 8 * 33)
        m["maskd"] = md
        m["maskf"] = mf
        in_maps.append(m)
    return in_maps


def kernel(**inputs):
    in_maps = prep(inputs)
    nc = build()
    res = run_bass_kernel_spmd(nc, in_maps, core_ids=list(range(8)))
    out = np.zeros((4, 4096, 1024), np.float32)
    for c in range(8):
        b, hf = c // 2, c % 2
        out[b, own_tokens(hf)] = res.results[c]["out"]
    return out
```
